# Optimizing a Trainium2 kernel written in Bass

```python
import math
import jax
import jax.numpy as jnp
from jax import lax
import numpy as np

D_MODEL = 1024
BATCH = 2
SEQ = 8192
DEPTH = 4

HEAD_DIM = 64
Q_BLOCK = 128
GLA_HEADS = 4
GLA_DK = 64
GLA_DV = 128
GLA_GATE_RANK = 16
GLA_GATE_TAU = 16.0
GLA_CHUNK = 64
DSA_HEADS = 8
IDX_HEADS = 8
IDX_DIM = 64
DSA_TOPK_MAX = 256
FOX_HEADS = 8
FORGET_BIAS_MEAN = 2.0
DIL_HEADS = 8
DIL_PATTERNS = ((128, 1), (512, 4), (2048, 16))
REL_BUCKETS = 32
REL_MAX_DIST = 2048
REL_HEADS = 8
D_FF = 2816
N_EXPERTS = 8
TOP_K = 2
D_FF_EXPERT = 3584
ALPHA = (2 * DEPTH) ** 0.25
BETA = (8 * DEPTH) ** -0.25
LN_EPS = 1e-5

AB_SPLITS = (GLA_HEADS * GLA_DK, GLA_HEADS * GLA_DK, GLA_HEADS * GLA_DV, GLA_HEADS * GLA_DV, GLA_GATE_RANK,
             DSA_HEADS * HEAD_DIM, DSA_HEADS * HEAD_DIM, DSA_HEADS * HEAD_DIM,
             IDX_HEADS * IDX_DIM, IDX_DIM, IDX_HEADS)
CD_SPLITS = (FOX_HEADS * HEAD_DIM, FOX_HEADS * HEAD_DIM, FOX_HEADS * HEAD_DIM, FOX_HEADS,
             DIL_HEADS * HEAD_DIM, DIL_HEADS * HEAD_DIM, DIL_HEADS * HEAD_DIM)
W_AB = sum(AB_SPLITS)
W_CD = sum(CD_SPLITS)
MIX_AB = GLA_HEADS * GLA_DV + DSA_HEADS * HEAD_DIM
MIX_CD = (FOX_HEADS + DIL_HEADS) * HEAD_DIM

kernel_name = "hybrid_gla_dsa_fox_dilated_moe"


def _split(y, sizes):
    idx = [int(v) for v in np.cumsum(sizes)[:-1]]
    return jnp.split(y, idx, axis=-1)


def _layernorm(x, g, b):
    xf = x.astype(jnp.float32)
    mu = jnp.mean(xf, axis=-1, keepdims=True)
    var = jnp.mean(jnp.square(xf - mu), axis=-1, keepdims=True)
    return ((xf - mu) * lax.rsqrt(var + LN_EPS) * g + b).astype(x.dtype)


def _rel_bucket(dist):
    max_exact = REL_BUCKETS // 2
    d = jnp.maximum(dist, 0)
    df = jnp.maximum(d, 1).astype(jnp.float32)
    large = max_exact + (jnp.log(df / max_exact) / math.log(REL_MAX_DIST / max_exact)
                         * (REL_BUCKETS - max_exact)).astype(jnp.int32)
    large = jnp.minimum(large, REL_BUCKETS - 1)
    return jnp.where(d < max_exact, d, large)


def _gla(q, k, v, g_log):
    B, L, H, dk = q.shape
    dv = v.shape[-1]
    C = GLA_CHUNK
    n = L // C

    def to_chunks(t):
        return t.reshape(B, n, C, H, t.shape[-1]).transpose(1, 0, 3, 2, 4)

    qc, kc, vc, gc = (to_chunks(q * dk ** -0.5), to_chunks(k), to_chunks(v), to_chunks(g_log))
    causal = jnp.tril(jnp.ones((C, C), dtype=bool))[None, None, :, :, None]

    def step(S, inp):
        qi, ki, vi, gi = inp
        G = jnp.cumsum(gi.astype(jnp.float32), axis=2)
        o_inter = jnp.einsum('bhck,bhkv->bhcv', qi * jnp.exp(G), S)
        diff = G[:, :, :, None, :] - G[:, :, None, :, :]
        decay = jnp.exp(jnp.where(causal, diff, -jnp.inf))
        A = jnp.einsum('bhtk,bhsk,bhtsk->bhts', qi, ki, decay)
        o_intra = jnp.einsum('bhts,bhsv->bhtv', A, vi)
        G_last = G[:, :, -1:, :]
        S_new = (jnp.exp(G_last[:, :, 0, :, None]) * S
                 + jnp.einsum('bhsk,bhsv->bhkv', ki * jnp.exp(G_last - G), vi))
        return S_new, o_inter + o_intra

    S0 = jnp.zeros((B, H, dk, dv), jnp.float32)
    _, o = lax.scan(step, S0, (qc, kc, vc, gc))
    return o.transpose(1, 0, 3, 2, 4).reshape(B, L, H, dv).astype(v.dtype)


def _dsa(q, k, v, q_idx, k_idx, w_idx, rel_table):
    B, L, H, dh = q.shape
    topk = min(DSA_TOPK_MAX, L // 4)
    pos = jnp.arange(L)
    gather = jax.vmap(lambda t, i: t[i])

    def block(i):
        q0 = i * Q_BLOCK
        tq = q0 + jnp.arange(Q_BLOCK)
        qib = lax.dynamic_slice_in_dim(q_idx, q0, Q_BLOCK, axis=1)
        wib = lax.dynamic_slice_in_dim(w_idx, q0, Q_BLOCK, axis=1)
        s = jnp.einsum('bqhd,bkd->bhqk', qib, k_idx).astype(jnp.float32)
        score = jnp.einsum('bqh,bhqk->bqk', wib.astype(jnp.float32), jax.nn.relu(s))
        causal = pos[None, :] <= tq[:, None]
        score = jnp.where(causal[None], score, -jnp.inf)
        _, sel = lax.top_k(score, topk)
        valid = sel <= tq[None, :, None]
        ksel = gather(k, sel)
        vsel = gather(v, sel)
        qb = lax.dynamic_slice_in_dim(q, q0, Q_BLOCK, axis=1)
        logits = jnp.einsum('bqhd,bqkhd->bhqk', qb, ksel).astype(jnp.float32) * dh ** -0.5
        bias = rel_table[_rel_bucket(tq[None, :, None] - sel)]
        logits = logits + bias.transpose(0, 3, 1, 2).astype(jnp.float32)
        logits = jnp.where(valid[:, None], logits, -jnp.inf)
        p = jax.nn.softmax(logits, axis=-1)
        return jnp.einsum('bhqk,bqkhd->bqhd', p.astype(v.dtype), vsel)

    out = lax.map(block, jnp.arange(L // Q_BLOCK))
    return out.transpose(1, 0, 2, 3, 4).reshape(B, L, H, dh)


def _fox(q, k, v, log_f):
    B, L, H, dh = q.shape
    F = jnp.cumsum(log_f, axis=1).transpose(0, 2, 1)
    pos = jnp.arange(L)

    def block(i):
        q0 = i * Q_BLOCK
        tq = q0 + jnp.arange(Q_BLOCK)
        qb = lax.dynamic_slice_in_dim(q, q0, Q_BLOCK, axis=1)
        Fq = lax.dynamic_slice_in_dim(F, q0, Q_BLOCK, axis=2)
        logits = (jnp.einsum('bqhd,bkhd->bhqk', qb, k).astype(jnp.float32) * dh ** -0.5
                  + Fq[..., None] - F[:, :, None, :])
        causal = pos[None, :] <= tq[:, None]
        logits = jnp.where(causal[None, None], logits, -jnp.inf)
        p = jax.nn.softmax(logits, axis=-1)
        return jnp.einsum('bhqk,bkhd->bqhd', p.astype(v.dtype), v)

    out = lax.map(block, jnp.arange(L // Q_BLOCK))
    return out.transpose(1, 0, 2, 3, 4).reshape(B, L, H, dh)


def _dilated(q, k, v, rel_table):
    B, L, H, dh = q.shape

    def block(i):
        q0 = i * Q_BLOCK
        tq = q0 + jnp.arange(Q_BLOCK)
        qb = lax.dynamic_slice_in_dim(q, q0, Q_BLOCK, axis=1)
        outs, log_den = [], []
        for window, dil in DIL_PATTERNS:
            offs = dil * jnp.arange(window // dil + 1)
            idx = tq[:, None] - offs[None, :]
            valid = idx >= 0
            idxc = jnp.maximum(idx, 0)
            ksel = k[:, idxc]
            vsel = v[:, idxc]
            bias = rel_table[_rel_bucket(offs)].T.astype(jnp.float32)
            lg = (jnp.einsum('bqhd,bqjhd->bhqj', qb, ksel).astype(jnp.float32) * dh ** -0.5
                  + bias[None, :, None, :])
            lg = jnp.where(valid[None, None], lg, -jnp.inf)
            m = jnp.max(lg, axis=-1, keepdims=True)
            e = jnp.exp(lg - m)
            s = jnp.sum(e, axis=-1, keepdims=True)
            outs.append(jnp.einsum('bhqj,bqjhd->bhqd', e, vsel.astype(jnp.float32)) / s)
            log_den.append(m + jnp.log(s))
        wts = jax.nn.softmax(jnp.stack(log_den, 0), axis=0)
        o = jnp.sum(wts * jnp.stack(outs, 0), axis=0)
        return o.transpose(0, 2, 1, 3).astype(v.dtype)

    out = lax.map(block, jnp.arange(L // Q_BLOCK))
    return out.transpose(1, 0, 2, 3, 4).reshape(B, L, H, dh)


def _mixer_ab(x, w_in, w_gate, b_gate, g_norm, w_out, rel_table):
    B, L, _ = x.shape
    y = x @ w_in
    qa, ka, va, ra, ga, qb, kb, vb, qi, ki, wi = _split(y, AB_SPLITS)
    g_log = jax.nn.log_sigmoid((ga @ w_gate + b_gate).astype(jnp.float32)) / GLA_GATE_TAU
    oa = _gla(qa.reshape(B, L, GLA_HEADS, GLA_DK), ka.reshape(B, L, GLA_HEADS, GLA_DK),
              va.reshape(B, L, GLA_HEADS, GLA_DV), g_log.reshape(B, L, GLA_HEADS, GLA_DK))
    of = oa.astype(jnp.float32)
    of = of * lax.rsqrt(jnp.mean(jnp.square(of), axis=-1, keepdims=True) + LN_EPS) * g_norm
    oa = of.astype(x.dtype) * jax.nn.silu(ra.reshape(B, L, GLA_HEADS, GLA_DV))
    ob = _dsa(qb.reshape(B, L, DSA_HEADS, HEAD_DIM), kb.reshape(B, L, DSA_HEADS, HEAD_DIM),
              vb.reshape(B, L, DSA_HEADS, HEAD_DIM), qi.reshape(B, L, IDX_HEADS, IDX_DIM), ki, wi, rel_table)
    o = jnp.concatenate([oa.reshape(B, L, -1), ob.reshape(B, L, -1)], axis=-1)
    return o @ w_out


def _mixer_cd(x, w_in, b_forget, w_out, rel_table):
    B, L, _ = x.shape
    y = x @ w_in
    qc, kc, vc, fc, qd, kd, vd = _split(y, CD_SPLITS)
    log_f = jax.nn.log_sigmoid((fc + b_forget).astype(jnp.float32))
    oc = _fox(qc.reshape(B, L, FOX_HEADS, HEAD_DIM), kc.reshape(B, L, FOX_HEADS, HEAD_DIM),
              vc.reshape(B, L, FOX_HEADS, HEAD_DIM), log_f)
    od = _dilated(qd.reshape(B, L, DIL_HEADS, HEAD_DIM), kd.reshape(B, L, DIL_HEADS, HEAD_DIM),
                  vd.reshape(B, L, DIL_HEADS, HEAD_DIM), rel_table)
    o = jnp.concatenate([oc.reshape(B, L, -1), od.reshape(B, L, -1)], axis=-1)
    return o @ w_out


def _swiglu(x, w1, w3, w2):
    return (jax.nn.silu(x @ w1) * (x @ w3)) @ w2


def _moe(x, w_router, w1, w3, w2):
    logits = (x @ w_router).astype(jnp.float32)
    top_v, top_i = lax.top_k(logits, TOP_K)
    gates = jax.nn.softmax(top_v, axis=-1)
    combine = jnp.sum(jax.nn.one_hot(top_i, N_EXPERTS, dtype=jnp.float32) * gates[..., None], axis=-2)
    combine = combine.astype(x.dtype)
    y = jnp.zeros_like(x)
    for e in range(N_EXPERTS):
        y = y + _swiglu(x, w1[e], w3[e], w2[e]) * combine[..., e:e + 1]
    return y


def setup_inputs(seed: int = 0) -> dict:
    key = jax.random.key(seed)
    ks = iter(jax.random.split(key, 32))
    n_e = (DEPTH + 1) // 2
    n_o = DEPTH // 2

    def nrm(shape, scale):
        return jax.random.normal(next(ks), shape, jnp.float32) * scale

    return {
        "x": nrm((BATCH, SEQ, D_MODEL), 1.0),
        "ln_g": 1.0 + nrm((DEPTH, 2, D_MODEL), 0.02),
        "ln_b": nrm((DEPTH, 2, D_MODEL), 0.02),
        "rel_table": nrm((REL_BUCKETS, REL_HEADS), 0.2),
        "w_in_ab": nrm((n_e, D_MODEL, W_AB), D_MODEL ** -0.5),
        "w_gate_a": nrm((n_e, GLA_GATE_RANK, GLA_HEADS * GLA_DK), GLA_GATE_RANK ** -0.5),
        "b_gate_a": nrm((n_e, GLA_HEADS * GLA_DK), 0.1),
        "g_norm_a": 1.0 + nrm((n_e, GLA_DV), 0.02),
        "w_out_ab": nrm((n_e, MIX_AB, D_MODEL), MIX_AB ** -0.5 * BETA),
        "w_in_cd": nrm((n_o, D_MODEL, W_CD), D_MODEL ** -0.5),
        "b_forget": FORGET_BIAS_MEAN + nrm((n_o, FOX_HEADS), 0.1),
        "w_out_cd": nrm((n_o, MIX_CD, D_MODEL), MIX_CD ** -0.5 * BETA),
        "w1_dense": nrm((n_e, D_MODEL, D_FF), D_MODEL ** -0.5),
        "w3_dense": nrm((n_e, D_MODEL, D_FF), D_MODEL ** -0.5),
        "w2_dense": nrm((n_e, D_FF, D_MODEL), D_FF ** -0.5 * BETA),
        "w_router": nrm((n_o, D_MODEL, N_EXPERTS), D_MODEL ** -0.5),
        "w1_moe": nrm((n_o, N_EXPERTS, D_MODEL, D_FF_EXPERT), D_MODEL ** -0.5),
        "w3_moe": nrm((n_o, N_EXPERTS, D_MODEL, D_FF_EXPERT), D_MODEL ** -0.5),
        "w2_moe": nrm((n_o, N_EXPERTS, D_FF_EXPERT, D_MODEL), D_FF_EXPERT ** -0.5 * BETA),
    }


def reference(x, ln_g, ln_b, rel_table, w_in_ab, w_gate_a, b_gate_a, g_norm_a, w_out_ab,
              w_in_cd, b_forget, w_out_cd, w1_dense, w3_dense, w2_dense,
              w_router, w1_moe, w3_moe, w2_moe):
    for layer in range(DEPTH):
        j = layer // 2
        if layer % 2 == 0:
            mix = _mixer_ab(x, w_in_ab[j], w_gate_a[j], b_gate_a[j], g_norm_a[j], w_out_ab[j], rel_table)
        else:
            mix = _mixer_cd(x, w_in_cd[j], b_forget[j], w_out_cd[j], rel_table)
        x = _layernorm(ALPHA * x + mix, ln_g[layer, 0], ln_b[layer, 0])
        if layer % 2 == 0:
            ffn = _swiglu(x, w1_dense[j], w3_dense[j], w2_dense[j])
        else:
            ffn = _moe(x, w_router[j], w1_moe[j], w3_moe[j], w2_moe[j])
        x = _layernorm(ALPHA * x + ffn, ln_g[layer, 1], ln_b[layer, 1])
    return x
```

```python
import math
from contextlib import ExitStack
import numpy as np
import ml_dtypes
import concourse.bass as bass
import concourse.mybir as mybir
from concourse.bass_utils import run_bass_kernel_spmd

F32 = mybir.dt.float32
BF16 = mybir.dt.bfloat16
AF = mybir.ActivationFunctionType
ALU = mybir.AluOpType
AX = mybir.AxisListType
NPBF = ml_dtypes.bfloat16

NCORES = 8
D = 1024
SEQ = 8192
BATCH = 2
DEPTH = 4
ALPHA = (2 * DEPTH) ** 0.25
LN_EPS = 1e-5
NEG = -1.0e30


class Buf:
    def __init__(self, t):
        self.t = t
        self.w = None
        self.r = []

    def __getitem__(self, idx):
        return self.t[idx]


class KB:
    NDS = 6

    def __init__(self):
        self.nc = bass.Bass("TRN2", target_bir_lowering=False)
        nc = self.nc
        self.es = ExitStack()
        self.eng = {"pe": nc.tensor, "dve": nc.vector, "act": nc.scalar, "pool": nc.gpsimd, "sp": nc.sync}
        self.esem = {}
        self.ecnt = {}
        self.seen = {e: {} for e in self.eng}
        for e in self.eng:
            self.esem[e] = self.es.enter_context(nc.semaphore("sem_" + e))
            self.ecnt[e] = 0
        self.dsem = {}
        self.dcnt = {}
        self.dnext = {}
        for q in ("sp", "act", "pool"):
            self.dsem[q] = [self.es.enter_context(nc.semaphore("dsem_%s%d" % (q, i))) for i in range(self.NDS)]
            self.dcnt[q] = [0] * self.NDS
            self.dnext[q] = 0
        self.n_names = 0
        self.outs = []

    def _nm(self, p):
        self.n_names += 1
        return "%s_%d" % (p, self.n_names)

    def dram_in(self, name, shape, dt):
        return Buf(self.nc.dram_tensor(name, list(shape), dt, kind="ExternalInput").ap())

    def dram_out(self, name, shape, dt):
        b = Buf(self.nc.dram_tensor(name, list(shape), dt, kind="ExternalOutput").ap())
        self.outs.append(b)
        return b

    def dram_tmp(self, name, shape, dt):
        return Buf(self.nc.dram_tensor(name, list(shape), dt, kind="Internal").ap())

    def sb(self, shape, dt, name="sb"):
        return Buf(self.es.enter_context(self.nc.sbuf_tensor(self._nm(name), list(shape), dt)))

    def ps(self, shape, dt=F32, name="ps"):
        return Buf(self.es.enter_context(self.nc.psum_tensor(self._nm(name), list(shape), dt)))

    def _wait(self, e, ev):
        if ev is None:
            return
        sem, val, key = ev
        if self.seen[e].get(key, 0) >= val:
            return
        self.eng[e].wait_ge(sem, val)
        self.seen[e][key] = val

    def _deps(self, e, R, W):
        evs = []
        for b in R:
            if b.w is not None:
                evs.append(b.w)
        for b in W:
            if b.w is not None:
                evs.append(b.w)
            evs.extend(b.r)
        best = {}
        for ev in evs:
            k = ev[2]
            if e == "pe" and k == "pe":
                continue
            if k not in best or best[k][1] < ev[1]:
                best[k] = ev
        for ev in best.values():
            self._wait(e, ev)

    def _mark(self, ev, R, W):
        for b in R:
            b.r.append(ev)
            if len(b.r) > 24:
                best = {}
                for x in b.r:
                    if x[2] not in best or best[x[2]][1] < x[1]:
                        best[x[2]] = x
                b.r = list(best.values())
        for b in W:
            b.w = ev
            b.r = []

    def op(self, e, fn, R=(), W=()):
        self._deps(e, R, W)
        ins = fn()
        self.ecnt[e] += 1
        ins.then_inc(self.esem[e], 1)
        ev = (self.esem[e], self.ecnt[e], e)
        self.seen[e][e] = max(self.seen[e].get(e, 0), 0)
        self._mark(ev, R, W)
        return ev

    def dma(self, q, out, in_, R=(), W=(), **kw):
        self._deps(q, R, W)
        i = self.dnext[q]
        self.dnext[q] = (i + 1) % self.NDS
        key = "d%s%d" % (q, i)
        if self.dcnt[q][i] > 0:
            self._wait(q, (self.dsem[q][i], self.dcnt[q][i], key))
        self.dcnt[q][i] += 16
        self.eng[q].dma_start(out=out, in_=in_, **kw).then_inc(self.dsem[q][i], 16)
        ev = (self.dsem[q][i], self.dcnt[q][i], key)
        self._mark(ev, R, W)
        return ev

    def finish(self):
        for q in ("sp", "act", "pool"):
            for i in range(self.NDS):
                if self.dcnt[q][i] > 0:
                    self._wait("sp", (self.dsem[q][i], self.dcnt[q][i], "d%s%d" % (q, i)))
        for e in ("pe", "dve", "act", "pool"):
            if self.ecnt[e] > 0:
                self._wait("sp", (self.esem[e], self.ecnt[e], e))
        self.es.close()
        return self.nc


def run_spmd(nc, in_maps):
    res = run_bass_kernel_spmd(nc, in_maps, core_ids=list(range(NCORES)))
    return res.results


def build_cast(n):
    kb = KB()
    nc = kb.nc
    CH = 2048
    src = kb.dram_in("src", [128, n], F32)
    dst = kb.dram_out("dst", [128, n], BF16)
    NB = 3
    tin = [kb.sb([128, CH], F32, "tin") for _ in range(NB)]
    tout = [kb.sb([128, CH], BF16, "tout") for _ in range(NB)]
    nch = (n + CH - 1) // CH
    for c in range(nch):
        c0 = c * CH
        w = min(CH, n - c0)
        a, b = tin[c % NB], tout[c % NB]
        kb.dma("sp", a[:, :w], src[:, c0:c0 + w], R=[src], W=[a])
        if c % 2 == 0:
            kb.op("dve", lambda: nc.vector.tensor_copy(out=b[:, :w], in_=a[:, :w]), R=[a], W=[b])
        else:
            kb.op("act", lambda: nc.scalar.copy(out=b[:, :w], in_=a[:, :w]), R=[a], W=[b])
        kb.dma("pool", dst[:, c0:c0 + w], b[:, :w], R=[b], W=[dst])
    return kb.finish()


def cast_weights(arrs):
    flats = [np.ascontiguousarray(a).reshape(NCORES, 128, -1) for a in arrs]
    ns = [f.shape[2] for f in flats]
    cat = np.concatenate(flats, axis=2)
    n = cat.shape[2]
    nc = build_cast(n)
    res = run_spmd(nc, [{"src": np.ascontiguousarray(cat[c])} for c in range(NCORES)])
    out = np.stack([res[c]["dst"] for c in range(NCORES)], axis=0)
    outs = []
    o = 0
    for a, k in zip(arrs, ns):
        outs.append(out[:, :, o:o + k].reshape(a.shape))
        o += k
    return outs


TPC = 2048


def build_A(W, fm, tmb, tmf, gate=None):
    kb = KB()
    nc = kb.nc
    NT = TPC // 128
    n_fm = sum(n for _, n in fm)
    n_tmb = sum(n for _, n in tmb)
    n_tmf = sum(n for _, n in tmf) + (256 if gate is not None else 0)
    x = kb.dram_in("x", [TPC, D], F32)
    w = kb.dram_in("w", [128, 8, W], BF16)
    ident_d = kb.dram_in("ident", [128, 128], F32)
    yT = kb.dram_out("yT", [n_fm, TPC], BF16)
    ytb = kb.dram_out("ytb", [TPC, max(n_tmb, 1)], BF16)
    ytf = kb.dram_out("ytf", [TPC, max(n_tmf, 1)], F32)
    wsb = kb.sb([128, 8, W], BF16, "w")
    ident = kb.sb([128, 128], F32, "ident")
    xT = kb.sb([128, 8, TPC], BF16, "xT")
    kb.dma("sp", ident[:, :], ident_d[:, :], R=[ident_d], W=[ident])
    for kc in range(8):
        kb.dma("pool" if kc % 2 else "sp", wsb[:, kc, :], w[:, kc, :], R=[w], W=[wsb])
    if gate is not None:
        wg_d = kb.dram_in("wg", [16, 256], F32)
        bg_d = kb.dram_in("bg", [128, 256], F32)
        wg = kb.sb([16, 256], F32, "wg")
        bg = kb.sb([128, 256], F32, "bg")
        gaT = kb.sb([16, TPC], F32, "gaT")
        w32 = kb.sb([128, 8, 16], F32, "w32")
        kb.dma("sp", wg[:, :], wg_d[:, :], R=[wg_d], W=[wg])
        kb.dma("sp", bg[:, :], bg_d[:, :], R=[bg_d], W=[bg])
    xin = [kb.sb([128, D], F32, "xin") for _ in range(2)]
    pst = [kb.ps([128, 1024], F32, "pst")]
    for tt in range(NT):
        xi = xin[tt % 2]
        kb.dma("sp", xi[:, :], x[tt * 128:(tt + 1) * 128, :], R=[x], W=[xi])
        pt = pst[0]
        for kc in range(8):
            kb.op("pe", lambda: nc.tensor.transpose(pt[:, kc * 128:(kc + 1) * 128], xi[:, kc * 128:(kc + 1) * 128],
                                                    ident[:, :]), R=[xi, ident], W=[pt])
        for hf in range(2):
            src = pt[:, hf * 512:(hf + 1) * 512].rearrange("p (k t) -> p k t", k=4)
            dst = xT[:, hf * 4:(hf + 1) * 4, tt * 128:(tt + 1) * 128]
            if hf == 0:
                kb.op("dve", lambda: nc.vector.tensor_copy(out=dst, in_=src), R=[pt], W=[xT])
            else:
                kb.op("act", lambda: nc.scalar.copy(out=dst, in_=src), R=[pt], W=[xT])
    psf = [kb.ps([128, 512], F32, "psf") for _ in range(2)]
    stf = [kb.sb([128, 512], BF16, "stf") for _ in range(3)]
    cnt = 0
    row = 0
    blocks = list(fm)
    for (c0, ncol) in blocks:
        for tg in range(TPC // 512):
            pp = psf[cnt % 2]
            st = stf[cnt % 3]
            for kc in range(8):
                kb.op("pe", lambda: nc.tensor.matmul(pp[:ncol, :], lhsT=wsb[:, kc, c0:c0 + ncol],
                                                     rhs=xT[:, kc, tg * 512:(tg + 1) * 512],
                                                     start=(kc == 0), stop=(kc == 7)), R=[wsb, xT], W=[pp])
            if cnt % 2 == 0:
                kb.op("dve", lambda: nc.vector.tensor_copy(out=st[:ncol, :], in_=pp[:ncol, :]), R=[pp], W=[st])
            else:
                kb.op("act", lambda: nc.scalar.copy(out=st[:ncol, :], in_=pp[:ncol, :]), R=[pp], W=[st])
            kb.dma("pool" if cnt % 2 else "sp", yT[row:row + ncol, tg * 512:(tg + 1) * 512], st[:ncol, :],
                   R=[st], W=[yT])
            cnt += 1
        row += ncol
    if gate is not None:
        g0 = gate[0]
        for tg in range(TPC // 512):
            pp = psf[cnt % 2]
            for kc in range(8):
                kb.op("pe", lambda: nc.tensor.matmul(pp[:16, :], lhsT=wsb[:, kc, g0:g0 + 16],
                                                     rhs=xT[:, kc, tg * 512:(tg + 1) * 512],
                                                     start=(kc == 0), stop=(kc == 7)), R=[wsb, xT], W=[pp])
            kb.op("dve", lambda: nc.vector.tensor_copy(out=gaT[:, tg * 512:(tg + 1) * 512], in_=pp[:16, :]),
                  R=[pp], W=[gaT])
            cnt += 1
    pstm = [kb.ps([128, 512], F32, "pstm") for _ in range(2)]
    stb = [kb.sb([128, 512], BF16, "stb") for _ in range(3)]
    st32 = [kb.sb([128, 512], F32, "st32") for _ in range(3)]
    cnt = 0
    for tt in range(NT):
        for kind, lst, dst_d in (("b", tmb, ytb), ("f", tmf, ytf)):
            off = 0
            for (c0, ncol) in lst:
                pp = pstm[cnt % 2]
                st = (stb if kind == "b" else st32)[cnt % 3]
                for kc in range(8):
                    kb.op("pe", lambda: nc.tensor.matmul(pp[:, :ncol], lhsT=xT[:, kc, tt * 128:(tt + 1) * 128],
                                                         rhs=wsb[:, kc, c0:c0 + ncol],
                                                         start=(kc == 0), stop=(kc == 7)), R=[wsb, xT], W=[pp])
                if cnt % 2 == 0:
                    kb.op("dve", lambda: nc.vector.tensor_copy(out=st[:, :ncol], in_=pp[:, :ncol]), R=[pp], W=[st])
                else:
                    kb.op("act", lambda: nc.scalar.copy(out=st[:, :ncol], in_=pp[:, :ncol]), R=[pp], W=[st])
                kb.dma("pool" if cnt % 2 else "sp", dst_d[tt * 128:(tt + 1) * 128, off:off + ncol], st[:, :ncol],
                       R=[st], W=[dst_d])
                off += ncol
                cnt += 1
        if gate is not None:
            off = sum(n for _, n in tmf)
            pp = pstm[cnt % 2]
            st = st32[cnt % 3]
            kb.op("pe", lambda: nc.tensor.matmul(pp[:, :256], lhsT=gaT[:, tt * 128:(tt + 1) * 128], rhs=wg[:, :],
                                                 start=True, stop=True), R=[gaT, wg], W=[pp])
            kb.op("dve", lambda: nc.vector.tensor_tensor(out=st[:, :256], in0=pp[:, :256], in1=bg[:, :], op=ALU.add),
                  R=[pp, bg], W=[st])
            kb.op("act", lambda: nc.scalar.activation(out=st[:, :256], in_=st[:, :256], func=AF.Exp, scale=-1.0),
                  R=[st], W=[st])
            kb.op("act", lambda: nc.scalar.activation(out=st[:, :256], in_=st[:, :256], func=AF.Ln, bias=1.0),
                  R=[st], W=[st])
            kb.op("dve", lambda: nc.vector.tensor_scalar(out=st[:, :256], in0=st[:, :256], scalar1=-1.0 / 16.0,
                                                         scalar2=None, op0=ALU.mult), R=[st], W=[st])
            kb.dma("sp", ytf[tt * 128:(tt + 1) * 128, off:off + 256], st[:, :256], R=[st], W=[ytf])
            cnt += 1
    return kb.finish()


def blocks_of(c0, n, bs):
    out = []
    while n > 0:
        k = min(bs, n)
        out.append((c0, k))
        c0 += k
        n -= k
    return out


def w_kc_layout(w):
    W = w.shape[1]
    return np.ascontiguousarray(w.reshape(8, 128, W).transpose(1, 0, 2))


IDENT = np.eye(128, dtype=np.float32)


def run_A(x_flat, w_bf, fm_ranges, tmb_ranges, tmf_ranges, gate=None, wg=None, bg=None):
    W = w_bf.shape[1]
    fm = [b for (c0, n) in fm_ranges for b in blocks_of(c0, n, 128)]
    tmb = [b for (c0, n) in tmb_ranges for b in blocks_of(c0, n, 512)]
    tmf = [b for (c0, n) in tmf_ranges for b in blocks_of(c0, n, 512)]
    nc = build_A(W, fm, tmb, tmf, gate)
    wl = w_kc_layout(w_bf)
    maps = []
    for c in range(NCORES):
        m = {"x": np.ascontiguousarray(x_flat[c * TPC:(c + 1) * TPC]), "w": wl, "ident": IDENT}
        if gate is not None:
            m["wg"] = np.ascontiguousarray(wg)
            m["bg"] = np.ascontiguousarray(np.broadcast_to(bg[None, :], (128, 256)))
        maps.append(m)
    res = run_spmd(nc, maps)
    yT = np.concatenate([res[c]["yT"] for c in range(NCORES)], axis=1)
    ytb = np.concatenate([res[c]["ytb"] for c in range(NCORES)], axis=0)
    ytf = np.concatenate([res[c]["ytf"] for c in range(NCORES)], axis=0)
    return yT, ytb, ytf


def layer_norm(kb, h, gt, bt, out_ap, out_buf, scr):
    nc = kb.nc
    stats, mv, sd = scr
    for c in range(2):
        kb.op("dve", lambda: nc.vector.bn_stats(out=stats[:, c, :], in_=h[:, c * 512:(c + 1) * 512]), R=[h], W=[stats])
    kb.op("dve", lambda: nc.vector.bn_aggr(out=mv[:, :], in_=stats[:, :, :].rearrange("p a b -> p (a b)")),
          R=[stats], W=[mv])
    kb.op("act", lambda: nc.scalar.activation(out=sd[:, 0:1], in_=mv[:, 1:2], func=AF.Sqrt, bias=kb.eps_col[:, 0:1]),
          R=[mv, kb.eps_col], W=[sd])
    kb.op("dve", lambda: nc.vector.reciprocal(out=sd[:, 1:2], in_=sd[:, 0:1]), R=[sd], W=[sd])
    kb.op("dve", lambda: nc.vector.tensor_scalar(out=h[:, :], in0=h[:, :], scalar1=mv[:, 0:1], scalar2=sd[:, 1:2],
                                                 op0=ALU.subtract, op1=ALU.mult), R=[h, mv, sd], W=[h])
    kb.op("pool", lambda: nc.gpsimd.tensor_tensor(out=h[:, :], in0=h[:, :], in1=gt[1], op=ALU.mult),
          R=[h, gt[0]], W=[h])
    kb.op("dve", lambda: nc.vector.tensor_tensor(out=out_ap, in0=h[:, :], in1=bt[1], op=ALU.add),
          R=[h, bt[0]], W=[out_buf])


def build_C(n_exp, nf_unit, n_units_per_exp):
    kb = KB()
    nc = kb.nc
    moe = n_exp > 1
    NT = TPC // 128
    TG = 512
    NG = TPC // TG
    nfu = nf_unit
    n_chunks = n_exp * n_units_per_exp * nfu
    oT_d = kb.dram_in("oT", [128, 8, TPC], BF16)
    x_d = kb.dram_in("x", [TPC, D], F32)
    wo_d = kb.dram_in("wo", [128, 8, D], BF16)
    lnp_d = kb.dram_in("lnp", [128, 4, D], F32)
    wf_d = kb.dram_in("wf", [n_chunks, 128, 3072], BF16)
    ident_d = kb.dram_in("ident", [128, 128], F32)
    out_d = kb.dram_out("out", [TPC, D], F32)
    ident = kb.sb([128, 128], F32, "ident")
    wo = kb.sb([128, 8, D], BF16, "wo")
    lnp = kb.sb([128, 4, D], F32, "lnp")
    kb.eps_col = kb.sb([128, 1], F32, "eps")
    kb.op("dve", lambda: nc.vector.memset(kb.eps_col[:, :], LN_EPS), W=[kb.eps_col])
    kb.dma("sp", ident[:, :], ident_d[:, :], R=[ident_d], W=[ident])
    kb.dma("sp", wo[:, :, :], wo_d[:, :, :], R=[wo_d], W=[wo])
    kb.dma("pool", lnp[:, :, :], lnp_d[:, :, :], R=[lnp_d], W=[lnp])
    if moe:
        wr_d = kb.dram_in("wr", [128, 8, 8], F32)
        wr = kb.sb([128, 8, 8], F32, "wr")
        kb.dma("sp", wr[:, :, :], wr_d[:, :, :], R=[wr_d], W=[wr])
        x1T32 = kb.sb([128, 8, 128], F32, "x1T32")
        comb = [kb.sb([128, 8], F32, "comb") for _ in range(4)]
        rt = kb.sb([128, 40], F32, "rt")
    oT = [kb.sb([128, 8, TG], BF16, "oT") for _ in range(2)]
    xt = [kb.sb([128, D], F32, "xt") for _ in range(2)]
    h = kb.sb([128, D], F32, "h")
    x1g = [kb.sb([128, D], F32, "x1g") for _ in range(4)]
    x1T = kb.sb([128, 8, TG], BF16, "x1T")
    aT = [kb.sb([128, TG], BF16, "aT") for _ in range(nfu)]
    w2 = [kb.sb([128, D], BF16, "w2") for _ in range(nfu)]
    w13 = [kb.sb([128, 2048], BF16, "w13") for _ in range(3)]
    yacc = [kb.sb([128, D], F32, "yacc") for _ in range(4)]
    sil = [kb.sb([128, TG], F32, "sil") for _ in range(2)]
    ost = [kb.sb([128, D], F32, "ost") for _ in range(2)]
    scr = (kb.sb([128, 2, 6], F32, "stats"), kb.sb([128, 2], F32, "mv"), kb.sb([128, 2], F32, "sd"))
    X = [kb.ps([128, 512], F32, "X") for _ in range(4)]
    Y = [kb.ps([128, 512], F32, "Y") for _ in range(2)]
    wcnt = 0
    for g in range(NG):
        og = oT[g % 2]
        kb.dma("pool", og[:, :, :], oT_d[:, :, g * TG:(g + 1) * TG], R=[oT_d], W=[og])
        for tt in range(4):
            tok0 = g * TG + tt * 128
            xi = xt[tt % 2]
            kb.dma("sp", xi[:, :], x_d[tok0:tok0 + 128, :], R=[x_d], W=[xi])
            for hf in range(2):
                for kc in range(8):
                    kb.op("pe", lambda: nc.tensor.matmul(Y[hf][:, :], lhsT=og[:, kc, tt * 128:(tt + 1) * 128],
                                                         rhs=wo[:, kc, hf * 512:(hf + 1) * 512],
                                                         start=(kc == 0), stop=(kc == 7)), R=[og, wo], W=[Y[hf]])
                kb.op("dve", lambda: nc.vector.scalar_tensor_tensor(out=h[:, hf * 512:(hf + 1) * 512],
                                                                    in0=xi[:, hf * 512:(hf + 1) * 512], scalar=ALPHA,
                                                                    in1=Y[hf][:, :], op0=ALU.mult, op1=ALU.add),
                      R=[xi, Y[hf]], W=[h])
            x1 = x1g[tt]
            layer_norm(kb, h, (lnp, lnp[:, 0, :]), (lnp, lnp[:, 1, :]), x1[:, :], x1, scr)
            for kc in range(8):
                pt = X[kc // 4]
                kb.op("pe", lambda: nc.tensor.transpose(pt[:, (kc % 4) * 128:(kc % 4 + 1) * 128],
                                                        x1[:, kc * 128:(kc + 1) * 128], ident[:, :]),
                      R=[x1, ident], W=[pt])
            for hf in range(2):
                src = X[hf][:, :].rearrange("p (k t) -> p k t", k=4)
                dst = x1T[:, hf * 4:(hf + 1) * 4, tt * 128:(tt + 1) * 128]
                if hf == 0:
                    kb.op("dve", lambda: nc.vector.tensor_copy(out=dst, in_=src), R=[X[hf]], W=[x1T])
                else:
                    kb.op("act", lambda: nc.scalar.copy(out=dst, in_=src), R=[X[hf]], W=[x1T])
                if moe:
                    kb.op("pool" if False else "dve",
                          lambda: nc.vector.tensor_copy(out=x1T32[:, hf * 4:(hf + 1) * 4, :], in_=src),
                          R=[X[hf]], W=[x1T32])
            if moe:
                pr = X[2]
                for kc in range(8):
                    kb.op("pe", lambda: nc.tensor.matmul(pr[:, :8], lhsT=x1T32[:, kc, :], rhs=wr[:, kc, :],
                                                         start=(kc == 0), stop=(kc == 7)), R=[x1T32, wr], W=[pr])
                cb = comb[tt]
                lg, mx, tmp, oh = rt[:, 0:8], rt[:, 8:16], rt[:, 16:24], rt[:, 24:32]
                sc = rt[:, 32:40]
                kb.op("dve", lambda: nc.vector.tensor_copy(out=lg, in_=pr[:, :8]), R=[pr], W=[rt])
                kb.op("dve", lambda: nc.vector.max(out=mx, in_=lg), R=[rt], W=[rt])
                kb.op("dve", lambda: nc.vector.tensor_tensor(out=sc[:, 0:1], in0=mx[:, 1:2], in1=mx[:, 0:1],
                                                             op=ALU.subtract), R=[rt], W=[rt])
                kb.op("act", lambda: nc.scalar.activation(out=sc[:, 1:2], in_=sc[:, 0:1], func=AF.Exp), R=[rt], W=[rt])
                kb.op("dve", lambda: nc.vector.tensor_scalar(out=sc[:, 2:3], in0=sc[:, 1:2], scalar1=1.0, scalar2=None,
                                                             op0=ALU.add), R=[rt], W=[rt])
                kb.op("dve", lambda: nc.vector.reciprocal(out=sc[:, 3:4], in_=sc[:, 2:3]), R=[rt], W=[rt])
                kb.op("dve", lambda: nc.vector.tensor_tensor(out=sc[:, 4:5], in0=sc[:, 1:2], in1=sc[:, 3:4],
                                                             op=ALU.mult), R=[rt], W=[rt])
                kb.op("dve", lambda: nc.vector.tensor_scalar(out=tmp, in0=lg, scalar1=mx[:, 0:1], scalar2=sc[:, 3:4],
                                                             op0=ALU.is_equal, op1=ALU.mult), R=[rt], W=[rt])
                kb.op("dve", lambda: nc.vector.tensor_scalar(out=oh, in0=lg, scalar1=mx[:, 1:2], scalar2=sc[:, 4:5],
                                                             op0=ALU.is_equal, op1=ALU.mult), R=[rt], W=[rt])
                kb.op("dve", lambda: nc.vector.tensor_tensor(out=cb[:, :], in0=tmp, in1=oh, op=ALU.add),
                      R=[rt], W=[cb])
        first = True
        for e in range(n_exp):
            for u in range(n_units_per_exp):
                base = (e * n_units_per_exp + u) * nfu
                for f in range(nfu):
                    wc = w13[wcnt % 3]
                    q = "sp" if wcnt % 2 == 0 else "pool"
                    kb.dma(q, wc[:, :], wf_d[base + f, :, 0:2048], R=[wf_d], W=[wc])
                    kb.dma("pool" if wcnt % 2 == 0 else "sp", w2[f][:, :], wf_d[base + f, :, 2048:3072],
                           R=[wf_d], W=[w2[f]])
                    h1, h3 = X[(wcnt % 2) * 2], X[(wcnt % 2) * 2 + 1]
                    for kc in range(8):
                        kb.op("pe", lambda: nc.tensor.matmul(h1[:, :], lhsT=wc[:, kc * 128:(kc + 1) * 128],
                                                             rhs=x1T[:, kc, :], start=(kc == 0), stop=(kc == 7)),
                              R=[wc, x1T], W=[h1])
                    for kc in range(8):
                        kb.op("pe", lambda: nc.tensor.matmul(h3[:, :], lhsT=wc[:, 1024 + kc * 128:1024 + (kc + 1) * 128],
                                                             rhs=x1T[:, kc, :], start=(kc == 0), stop=(kc == 7)),
                              R=[wc, x1T], W=[h3])
                    s = sil[wcnt % 2]
                    kb.op("act", lambda: nc.scalar.activation(out=s[:, :], in_=h1[:, :], func=AF.Silu), R=[h1], W=[s])
                    kb.op("dve", lambda: nc.vector.tensor_tensor(out=aT[f][:, :], in0=s[:, :], in1=h3[:, :],
                                                                 op=ALU.mult), R=[s, h3], W=[aT[f]])
                    wcnt += 1
                for tt in range(4):
                    for hf in range(2):
                        py = Y[hf]
                        for f in range(nfu):
                            kb.op("pe", lambda: nc.tensor.matmul(py[:, :], lhsT=aT[f][:, tt * 128:(tt + 1) * 128],
                                                                 rhs=w2[f][:, hf * 512:(hf + 1) * 512],
                                                                 start=(f == 0), stop=(f == nfu - 1)),
                                  R=[aT[f], w2[f]], W=[py])
                        ya = yacc[tt]
                        ysl = ya[:, hf * 512:(hf + 1) * 512]
                        if moe:
                            cs = comb[tt][:, e:e + 1]
                            if first:
                                kb.op("dve", lambda: nc.vector.tensor_scalar(out=ysl, in0=py[:, :], scalar1=cs,
                                                                             scalar2=None, op0=ALU.mult),
                                      R=[py, comb[tt]], W=[ya])
                            else:
                                kb.op("dve", lambda: nc.vector.scalar_tensor_tensor(out=ysl, in0=py[:, :], scalar=cs,
                                                                                    in1=ysl, op0=ALU.mult, op1=ALU.add),
                                      R=[py, comb[tt], ya], W=[ya])
                        else:
                            if first:
                                kb.op("dve", lambda: nc.vector.tensor_copy(out=ysl, in_=py[:, :]), R=[py], W=[ya])
                            else:
                                kb.op("dve", lambda: nc.vector.tensor_tensor(out=ysl, in0=py[:, :], in1=ysl, op=ALU.add),
                                      R=[py, ya], W=[ya])
                first = False
        for tt in range(4):
            tok0 = g * TG + tt * 128
            kb.op("dve", lambda: nc.vector.scalar_tensor_tensor(out=h[:, :], in0=x1g[tt][:, :], scalar=ALPHA,
                                                                in1=yacc[tt][:, :], op0=ALU.mult, op1=ALU.add),
                  R=[x1g[tt], yacc[tt]], W=[h])
            o = ost[tt % 2]
            layer_norm(kb, h, (lnp, lnp[:, 2, :]), (lnp, lnp[:, 3, :]), o[:, :], o, scr)
            kb.dma("sp", out_d[tok0:tok0 + 128, :], o[:, :], R=[o], W=[out_d])
    return kb.finish()


def ffn_chunk_layout(w1, w3, w2):
    F = w1.shape[1]
    nf = F // 128
    a = w1.reshape(8, 128, nf, 128).transpose(2, 1, 0, 3).reshape(nf, 128, 1024)
    b = w3.reshape(8, 128, nf, 128).transpose(2, 1, 0, 3).reshape(nf, 128, 1024)
    c = w2.reshape(nf, 128, 1024)
    return np.ascontiguousarray(np.concatenate([a, b, c], axis=2))


def run_C(o_flat_bf, x_flat, wo_bf, lnp4, wf, n_exp, nf_unit, n_units, wr=None):
    nc = build_C(n_exp, nf_unit, n_units)
    wol = w_kc_layout(wo_bf)
    lnb = np.ascontiguousarray(np.broadcast_to(lnp4[None, :, :], (128, 4, D))).astype(np.float32)
    maps = []
    for c in range(NCORES):
        oc = o_flat_bf[c * TPC:(c + 1) * TPC]
        oT = np.ascontiguousarray(oc.reshape(TPC, 8, 128).transpose(2, 1, 0))
        m = {"oT": oT, "x": np.ascontiguousarray(x_flat[c * TPC:(c + 1) * TPC]), "wo": wol, "lnp": lnb,
             "wf": wf, "ident": IDENT}
        if wr is not None:
            m["wr"] = np.ascontiguousarray(wr.reshape(8, 128, 8).transpose(1, 0, 2))
        maps.append(m)
    res = run_spmd(nc, maps)
    return np.concatenate([res[c]["out"] for c in range(NCORES)], axis=0)


from concourse.bass_types import AP as _AP

NQB = SEQ // 128
NDEL = 2304


def rel_bucket_np(d):
    d = np.maximum(d, 0)
    df = np.maximum(d, 1).astype(np.float32)
    large = 16 + (np.log(df / np.float32(16.0)).astype(np.float32) / np.float32(math.log(2048 / 16))
                  * np.float32(16)).astype(np.int32)
    large = np.minimum(large, 31)
    return np.where(d < 16, d, large)


def dil_const():
    C = np.zeros((32, NDEL), np.float32)
    for idx in range(NDEL):
        dl = idx - 127
        if dl < 0 or dl > 2048:
            continue
        mult = 0
        for (w, dd) in ((128, 1), (512, 4), (2048, 16)):
            if dl <= w and dl % dd == 0:
                mult += 1
        if mult:
            C[int(rel_bucket_np(np.array([dl]))[0]), idx] = mult
    return C


class AttnCtx:
    def __init__(self, kb, n_s=4, n_o=2):
        self.kb = kb
        self.S = [kb.ps([128, 512], F32, "S") for _ in range(n_s)]
        self.O = [kb.ps([128, 512], F32, "O") for _ in range(n_o)]
        self.P32 = [kb.sb([128, 128], F32, "P32") for _ in range(4)]
        self.Pb = [kb.sb([128, 128], BF16, "Pb") for _ in range(6)]
        self.pending = []
        self.LA = 3
        self.rc = [kb.sb([128, 1], F32, "rc") for _ in range(2)]
        self.cs = 0
        self.co = 0
        self.cp = 0


def attn_qblock(ax, qT, kT, Vaug, h, i, jlist, bias_of, mask_of, ost, ocol, after=None):
    kb = ax.kb
    nc = kb.nc
    O = ax.O[ax.co % len(ax.O)]
    rc = ax.rc[ax.co % 2]
    ax.co += 1
    hp = slice(h * 64, (h + 1) * 64)
    nj = len(jlist)
    for n, j in enumerate(jlist):
        S = ax.S[ax.cs % len(ax.S)]
        ax.cs += 1
        kb.op("pe", lambda: nc.tensor.matmul(S[:, :128], lhsT=kT[hp, j * 128:(j + 1) * 128],
                                             rhs=qT[hp, i * 128:(i + 1) * 128], start=True, stop=True),
              R=[kT, qT], W=[S])
        Pb = ax.Pb[ax.cp % len(ax.Pb)]
        b = bias_of(j) if bias_of is not None else None
        m = mask_of(j) if mask_of is not None else None
        if m is not None and not isinstance(m, list):
            m = [m]
        if m is not None and len(m) == 0:
            m = None
        tgt = Pb if m is None else ax.P32[ax.cp % len(ax.P32)]
        ax.cp += 1
        if b is None:
            kb.op("act", lambda: nc.scalar.activation(out=tgt[:, :], in_=S[:, :128], func=AF.Exp, scale=0.125),
                  R=[S], W=[tgt])
        else:
            kb.op("act", lambda: nc.scalar.activation(out=tgt[:, :], in_=S[:, :128], func=AF.Exp, bias=b[1],
                                                      scale=0.125), R=[S, b[0]], W=[tgt])
        if m is not None:
            for mm in m[:-1]:
                kb.op("pool", lambda: nc.gpsimd.tensor_tensor(out=tgt[:, :], in0=tgt[:, :], in1=mm[1], op=ALU.mult),
                      R=[tgt, mm[0]], W=[tgt])
            kb.op("dve", lambda: nc.vector.tensor_tensor(out=Pb[:, :], in0=tgt[:, :], in1=m[-1][1], op=ALU.mult),
                  R=[tgt, m[-1][0]], W=[Pb])

        def pv(Pb=Pb, j=j, n=n):
            kb.op("pe", lambda: nc.tensor.matmul(O[:, :65], lhsT=Pb[:, :], rhs=Vaug[:, j, h, :],
                                                 start=(n == 0), stop=(n == nj - 1)), R=[Pb, Vaug], W=[O])
            if n == nj - 1:
                kb.op("dve", lambda: nc.vector.reciprocal(out=rc[:, :], in_=O[:, 64:65]), R=[O], W=[rc])
                kb.op("dve", lambda: nc.vector.tensor_scalar(out=ost[:, ocol:ocol + 64], in0=O[:, 0:64],
                                                             scalar1=rc[:, 0:1], scalar2=None, op0=ALU.mult),
                      R=[O, rc], W=[ost])
                if after is not None:
                    after()

        ax.pending.append(pv)
        while len(ax.pending) > ax.LA:
            ax.pending.pop(0)()


def attn_flush(ax):
    while ax.pending:
        ax.pending.pop(0)()


def load_vaug(kb, v_d, name):
    nc = kb.nc
    Vaug = kb.sb([128, NQB, 2, 65], BF16, name)
    kb.op("pool", lambda: nc.gpsimd.memset(Vaug[:, :, :, :], 1.0), W=[Vaug])
    for c in range(4):
        js = slice(c * 16, (c + 1) * 16)
        kb.dma("sp" if c % 2 == 0 else "pool", Vaug[:, js, :, 0:64],
               v_d[:, js, :].rearrange("p j (h d) -> p j h d", h=2), R=[v_d], W=[Vaug])
    return Vaug


def build_B_cd():
    kb = KB()
    nc = kb.nc
    qc_d = kb.dram_in("qc", [128, SEQ], BF16)
    kc_d = kb.dram_in("kc", [128, SEQ], BF16)
    vc_d = kb.dram_in("vc", [128, NQB, 128], BF16)
    qd_d = kb.dram_in("qd", [128, SEQ], BF16)
    kd_d = kb.dram_in("kd", [128, SEQ], BF16)
    vd_d = kb.dram_in("vd", [128, NQB, 128], BF16)
    fc_d = kb.dram_in("fc", [128, NQB, 2], F32)
    bf_d = kb.dram_in("bf", [128, 2], F32)
    tri_d = kb.dram_in("tri", [128, 128], F32)
    rel_d = kb.dram_in("rel", [32, 2], F32)
    C_d = kb.dram_in("dilc", [32, NDEL], F32)
    oc_d = kb.dram_out("oc", [SEQ, 128], BF16)
    od_d = kb.dram_out("od", [SEQ, 128], BF16)
    E_d = kb.dram_tmp("Escr", [2, NDEL], F32)

    tri = kb.sb([128, 128], F32, "tri")
    ones = kb.sb([128, 128], F32, "ones")
    kb.dma("sp", tri[:, :], tri_d[:, :], R=[tri_d], W=[tri])
    kb.op("dve", lambda: nc.vector.memset(ones[:, :], 1.0), W=[ones])
    ax = AttnCtx(kb)
    rel = kb.sb([32, 2], F32, "rel")
    Cs = kb.sb([32, NDEL], F32, "Cs")
    Es = kb.sb([2, NDEL], F32, "Es")
    kb.dma("sp", rel[:, :], rel_d[:, :], R=[rel_d], W=[rel])
    kb.dma("sp", Cs[:, :], C_d[:, :], R=[C_d], W=[Cs])
    kb.op("act", lambda: nc.scalar.activation(out=rel[:, :], in_=rel[:, :], func=AF.Exp), R=[rel], W=[rel])
    for c in range((NDEL + 511) // 512):
        w = min(512, NDEL - c * 512)
        pp = ax.S[c % 3]
        kb.op("pe", lambda: nc.tensor.matmul(pp[:2, :w], lhsT=rel[:, :], rhs=Cs[:, c * 512:c * 512 + w],
                                             start=True, stop=True), R=[rel, Cs], W=[pp])
        kb.op("dve", lambda: nc.vector.tensor_copy(out=Es[:, c * 512:c * 512 + w], in_=pp[:2, :w]), R=[pp], W=[Es])
    kb.dma("sp", E_d[:, :], Es[:, :], R=[Es], W=[E_d])
    TT = [kb.sb([128, 17 * 128], F32, "TT") for _ in range(2)]
    for h in range(2):
        src = _AP(tensor=E_d.t.tensor, offset=h * NDEL, ap=[[1, 128], [1, 17 * 128]])
        kb.dma("sp", TT[h][:, :], src, R=[E_d], W=[TT[h]])
    fc = kb.sb([128, NQB, 2], F32, "fc")
    bf = kb.sb([128, 2], F32, "bf")
    lf = kb.sb([128, 2, NQB], F32, "lf")
    kb.dma("sp", fc[:, :, :], fc_d[:, :, :], R=[fc_d], W=[fc])
    kb.dma("sp", bf[:, :], bf_d[:, :], R=[bf_d], W=[bf])
    for h in range(2):
        kb.op("dve", lambda: nc.vector.tensor_scalar(out=lf[:, h, :], in0=fc[:, :, h], scalar1=bf[:, h:h + 1],
                                                     scalar2=None, op0=ALU.add), R=[fc, bf], W=[lf])
    kb.op("act", lambda: nc.scalar.activation(out=lf[:, :, :], in_=lf[:, :, :], func=AF.Exp, scale=-1.0), R=[lf], W=[lf])
    kb.op("act", lambda: nc.scalar.activation(out=lf[:, :, :], in_=lf[:, :, :], func=AF.Ln, bias=1.0), R=[lf], W=[lf])
    kb.op("dve", lambda: nc.vector.tensor_scalar(out=lf[:, :, :], in0=lf[:, :, :], scalar1=-1.0, scalar2=None,
                                                 op0=ALU.mult), R=[lf], W=[lf])
    lf2 = lf[:, :, :].rearrange("p h j -> p (h j)")
    p1, p2 = ax.O[0], ax.O[1]
    kb.op("pe", lambda: nc.tensor.matmul(p1[:, :128], lhsT=tri[:, :], rhs=lf2, start=True, stop=True),
          R=[tri, lf], W=[p1])
    kb.op("pe", lambda: nc.tensor.matmul(p2[:, :128], lhsT=ones[:, :], rhs=lf2, start=True, stop=True),
          R=[ones, lf], W=[p2])
    tot = kb.sb([128, 2, NQB], F32, "tot")
    carry = kb.sb([128, 2, NQB], F32, "carry")
    negF = kb.sb([128, 2, NQB], F32, "negF")
    kb.op("dve", lambda: nc.vector.tensor_copy(out=tot[:, :, :].rearrange("p h j -> p (h j)"), in_=p2[:, :128]),
          R=[p2], W=[tot])
    for h in range(2):
        kb.op("dve", lambda: nc.vector.tensor_tensor_scan(out=carry[:, h, :], data0=ones[:, :NQB], data1=tot[:, h, :],
                                                          initial=0.0, op0=ALU.mult, op1=ALU.add),
              R=[ones, tot], W=[carry])
    kb.op("dve", lambda: nc.vector.tensor_tensor(out=carry[:, :, :], in0=carry[:, :, :], in1=tot[:, :, :],
                                                 op=ALU.subtract), R=[carry, tot], W=[carry])
    kb.op("dve", lambda: nc.vector.tensor_tensor(out=negF[:, :, :].rearrange("p h j -> p (h j)"), in0=p1[:, :128],
                                                 in1=carry[:, :, :].rearrange("p h j -> p (h j)"), op=ALU.add),
          R=[p1, carry], W=[negF])
    kb.op("dve", lambda: nc.vector.tensor_scalar(out=negF[:, :, :], in0=negF[:, :, :], scalar1=-1.0, scalar2=None,
                                                 op0=ALU.mult), R=[negF], W=[negF])
    qc = kb.sb([128, SEQ], BF16, "qc")
    kc = kb.sb([128, SEQ], BF16, "kc")
    qd = kb.sb([128, SEQ], BF16, "qd")
    kd = kb.sb([128, SEQ], BF16, "kd")
    for n, (s, d_) in enumerate(((qd, qd_d), (kd, kd_d), (qc, qc_d), (kc, kc_d))):
        for c in range(2):
            kb.dma("sp" if (n + c) % 2 == 0 else "pool", s[:, c * 4096:(c + 1) * 4096], d_[:, c * 4096:(c + 1) * 4096],
                   R=[d_], W=[s])
    Vd = load_vaug(kb, vd_d, "Vd")
    Vc = load_vaug(kb, vc_d, "Vc")
    ostd = [kb.sb([128, 128], BF16, "ostd") for _ in range(2)]
    ostc = [kb.sb([128, 128], BF16, "ostc") for _ in range(2)]
    Bi = [kb.sb([128, NQB], F32, "Bi") for _ in range(3)]
    nb = 0
    for i in range(NQB):
        od = ostd[i % 2]
        for h in range(2):
            j0 = max(0, i - 16)
            attn_qblock(ax, qd, kd, Vd, h, i, list(range(j0, i + 1)), None,
                        lambda j, h=h, i=i: (TT[h], TT[h][:, (i - j) * 128:(i - j + 1) * 128]), od, h * 64,
                        after=(None if h == 0 else
                               (lambda i=i, od=od: kb.dma("pool", od_d[i * 128:(i + 1) * 128, :], od[:, :],
                                                          R=[od], W=[od_d]))))
        oc = ostc[i % 2]
        for h in range(2):
            B = Bi[nb % 3]
            nb += 1
            kb.op("dve", lambda: nc.vector.tensor_scalar(out=B[:, :i + 1], in0=negF[:, h, :i + 1],
                                                         scalar1=carry[:, h, i:i + 1], scalar2=None, op0=ALU.add),
                  R=[negF, carry], W=[B])
            attn_qblock(ax, qc, kc, Vc, h, i, list(range(0, i + 1)),
                        lambda j, B=B: (B, B[:, j:j + 1]),
                        lambda j, i=i: ((tri, tri[:, :]) if j == i else None), oc, h * 64,
                        after=(None if h == 0 else
                               (lambda i=i, oc=oc: kb.dma("sp", oc_d[i * 128:(i + 1) * 128, :], oc[:, :],
                                                          R=[oc], W=[oc_d]))))
    attn_flush(ax)
    return kb.finish()


TRI = np.triu(np.ones((128, 128), np.float32))


def to_pj(a):
    n = a.shape[1]
    return np.ascontiguousarray(a.reshape(NQB, 128, n).transpose(1, 0, 2))


def run_B_cd(yT, ytb, ytf, b_forget, rel_table):
    nc = build_B_cd()
    C = dil_const()
    maps = []
    for c in range(NCORES):
        b, m = c // 4, c % 4
        ts = slice(b * SEQ, (b + 1) * SEQ)
        rs = lambda base: slice(base + m * 128, base + (m + 1) * 128)
        maps.append({
            "qc": np.ascontiguousarray(yT[rs(0), ts]), "kc": np.ascontiguousarray(yT[rs(512), ts]),
            "qd": np.ascontiguousarray(yT[rs(1024), ts]),
            "kd": np.ascontiguousarray(yT[rs(1536), ts].reshape(128, NQB, 128)[:, :, ::-1].reshape(128, SEQ)),
            "vc": to_pj(ytb[ts, m * 128:(m + 1) * 128]), "vd": np.ascontiguousarray(to_pj(ytb[ts, 512 + m * 128:512 + (m + 1) * 128])[::-1]),
            "fc": to_pj(ytf[ts, 2 * m:2 * m + 2]),
            "bf": np.ascontiguousarray(np.broadcast_to(b_forget[None, 2 * m:2 * m + 2], (128, 2))).astype(np.float32),
            "tri": TRI, "rel": np.ascontiguousarray(rel_table[:, 2 * m:2 * m + 2]), "dilc": C,
        })
    res = run_spmd(nc, maps)
    o = np.zeros((BATCH * SEQ, D), NPBF)
    for c in range(NCORES):
        b, m = c // 4, c % 4
        o[b * SEQ:(b + 1) * SEQ, m * 128:(m + 1) * 128] = res[c]["oc"]
        o[b * SEQ:(b + 1) * SEQ, 512 + m * 128:512 + (m + 1) * 128] = res[c]["od"]
    return o


def build_B_gla():
    kb = KB()
    nc = kb.nc
    qT_d = kb.dram_in("qT", [64, SEQ], BF16)
    kT_d = kb.dram_in("kT", [64, SEQ], BF16)
    k_d = kb.dram_in("k", [128, NQB, 64], BF16)
    v_d = kb.dram_in("v", [128, NQB, 128], BF16)
    r_d = kb.dram_in("r", [128, NQB, 128], F32)
    g_d = kb.dram_in("g", [128, NQB, 64], F32)
    gn_d = kb.dram_in("gn", [128, 128], F32)
    tri_d = kb.dram_in("tri", [128, 128], F32)
    o_d = kb.dram_out("o", [SEQ, 128], BF16)
    qT = kb.sb([64, SEQ], BF16, "qT")
    kT = kb.sb([64, SEQ], BF16, "kT")
    ktm = kb.sb([128, NQB, 64], BF16, "ktm")
    v = kb.sb([128, NQB, 128], BF16, "v")
    r = kb.sb([128, NQB, 128], F32, "r")
    g = kb.sb([128, NQB, 64], F32, "g")
    gn = kb.sb([128, 128], F32, "gn")
    tri = kb.sb([128, 128], F32, "tri")
    eps = kb.sb([128, 1], F32, "eps")
    kb.op("dve", lambda: nc.vector.memset(eps[:, :], LN_EPS), W=[eps])
    kb.dma("sp", tri[:, :], tri_d[:, :], R=[tri_d], W=[tri])
    kb.dma("sp", g[:, :, :], g_d[:, :, :], R=[g_d], W=[g])
    kb.dma("pool", qT[:, :], qT_d[:, :], R=[qT_d], W=[qT])
    kb.dma("sp", kT[:, :], kT_d[:, :], R=[kT_d], W=[kT])
    kb.dma("pool", ktm[:, :, :], k_d[:, :, :], R=[k_d], W=[ktm])
    kb.dma("sp", v[:, :, :], v_d[:, :, :], R=[v_d], W=[v])
    kb.dma("pool", r[:, :, :], r_d[:, :, :], R=[r_d], W=[r])
    kb.dma("sp", gn[:, :], gn_d[:, :], R=[gn_d], W=[gn])
    kb.op("act", lambda: nc.scalar.activation(out=r[:, :, :], in_=r[:, :, :], func=AF.Silu), R=[r], W=[r])
    PG = [kb.ps([128, 512], F32, "PG") for _ in range(2)]
    PGT = [kb.ps([128, 512], F32, "PGT") for _ in range(2)]
    PA = [kb.ps([128, 512], F32, "PA") for _ in range(2)]
    PO = kb.ps([128, 512], F32, "PO")
    PU = kb.ps([128, 512], F32, "PU")
    eGT = [kb.sb([64, 128], F32, "eGT") for _ in range(2)]
    enGT = [kb.sb([64, 128], F32, "enGT") for _ in range(2)]
    enG = [kb.sb([128, 64], F32, "enG") for _ in range(2)]
    qgT = [kb.sb([64, 128], BF16, "qgT") for _ in range(2)]
    kgT = [kb.sb([64, 128], BF16, "kgT") for _ in range(2)]
    kg = [kb.sb([128, 64], BF16, "kg") for _ in range(2)]
    Am = [kb.sb([128, 128], BF16, "Am") for _ in range(2)]
    S32 = kb.sb([64, 128], F32, "S32")
    Sbf = kb.sb([64, 128], BF16, "Sbf")
    st6 = [kb.sb([128, 6], F32, "st6") for _ in range(2)]
    mv = [kb.sb([128, 4], F32, "mv") for _ in range(2)]
    of = [kb.sb([128, 128], F32, "of") for _ in range(2)]
    ost = [kb.sb([128, 128], BF16, "ost") for _ in range(2)]
    for c in range(NQB):
        p = c % 2
        cs = slice(c * 128, (c + 1) * 128)
        kb.op("pe", lambda: nc.tensor.matmul(PG[p][:, :64], lhsT=tri[:, :], rhs=g[:, c, :], start=True, stop=True),
              R=[tri, g], W=[PG[p]])
        kb.op("pe", lambda: nc.tensor.matmul(PGT[p][:64, :128], lhsT=g[:, c, :], rhs=tri[:, :], start=True, stop=True),
              R=[tri, g], W=[PGT[p]])
        kb.op("act", lambda: nc.scalar.activation(out=eGT[p][:, :], in_=PGT[p][:64, :128], func=AF.Exp),
              R=[PGT[p]], W=[eGT[p]])
        kb.op("act", lambda: nc.scalar.activation(out=enGT[p][:, :], in_=PGT[p][:64, :128], func=AF.Exp, scale=-1.0),
              R=[PGT[p]], W=[enGT[p]])
        kb.op("act", lambda: nc.scalar.activation(out=enG[p][:, :], in_=PG[p][:, :64], func=AF.Exp, scale=-1.0),
              R=[PG[p]], W=[enG[p]])
        kb.op("dve", lambda: nc.vector.scalar_tensor_tensor(out=qgT[p][:, :], in0=qT[:, cs], scalar=0.125,
                                                            in1=eGT[p][:, :], op0=ALU.mult, op1=ALU.mult),
              R=[qT, eGT[p]], W=[qgT[p]])
        kb.op("dve", lambda: nc.vector.tensor_tensor(out=kgT[p][:, :], in0=kT[:, cs], in1=enGT[p][:, :], op=ALU.mult),
              R=[kT, enGT[p]], W=[kgT[p]])
        kb.op("dve", lambda: nc.vector.tensor_tensor(out=kg[p][:, :], in0=ktm[:, c, :], in1=enG[p][:, :], op=ALU.mult),
              R=[ktm, enG[p]], W=[kg[p]])
        kb.op("pe", lambda: nc.tensor.matmul(PA[p][:, :128], lhsT=kgT[p][:, :], rhs=qgT[p][:, :], start=True, stop=True),
              R=[kgT[p], qgT[p]], W=[PA[p]])
        kb.op("dve", lambda: nc.vector.tensor_tensor(out=Am[p][:, :], in0=PA[p][:, :128], in1=tri[:, :], op=ALU.mult),
              R=[PA[p], tri], W=[Am[p]])
        kb.op("pe", lambda: nc.tensor.matmul(PO[:, :128], lhsT=Am[p][:, :], rhs=v[:, c, :], start=True, stop=(c == 0)),
              R=[Am[p], v], W=[PO])
        if c > 0:
            kb.op("pe", lambda: nc.tensor.matmul(PO[:, :128], lhsT=qgT[p][:, :], rhs=Sbf[:, :], start=False, stop=True),
                  R=[qgT[p], Sbf], W=[PO])
        if c < NQB - 1:
            kb.op("pe", lambda: nc.tensor.matmul(PU[:64, :128], lhsT=kg[p][:, :], rhs=v[:, c, :], start=True, stop=True),
                  R=[kg[p], v], W=[PU])
            eGl = eGT[p][:, 127:128]
            if c == 0:
                kb.op("dve", lambda: nc.vector.tensor_scalar(out=S32[:, :], in0=PU[:64, :128], scalar1=eGl, scalar2=None,
                                                             op0=ALU.mult), R=[PU, eGT[p]], W=[S32])
            else:
                kb.op("dve", lambda: nc.vector.tensor_scalar(out=S32[:, :], in0=S32[:, :], scalar1=eGl, scalar2=None,
                                                             op0=ALU.mult), R=[S32, eGT[p]], W=[S32])
                kb.op("dve", lambda: nc.vector.scalar_tensor_tensor(out=S32[:, :], in0=PU[:64, :128], scalar=eGl,
                                                                    in1=S32[:, :], op0=ALU.mult, op1=ALU.add),
                      R=[PU, eGT[p], S32], W=[S32])
            kb.op("dve", lambda: nc.vector.tensor_copy(out=Sbf[:, :], in_=S32[:, :]), R=[S32], W=[Sbf])
        kb.op("dve", lambda: nc.vector.bn_stats(out=st6[p][:, :], in_=PO[:, :128]), R=[PO], W=[st6[p]])
        kb.op("dve", lambda: nc.vector.bn_aggr(out=mv[p][:, 0:2], in_=st6[p][:, :]), R=[st6[p]], W=[mv[p]])
        kb.op("dve", lambda: nc.vector.scalar_tensor_tensor(out=mv[p][:, 2:3], in0=mv[p][:, 0:1], scalar=mv[p][:, 0:1],
                                                            in1=mv[p][:, 1:2], op0=ALU.mult, op1=ALU.add),
              R=[mv[p]], W=[mv[p]])
        kb.op("act", lambda: nc.scalar.activation(out=mv[p][:, 3:4], in_=mv[p][:, 2:3], func=AF.Ln, bias=eps[:, 0:1]),
              R=[mv[p], eps], W=[mv[p]])
        kb.op("act", lambda: nc.scalar.activation(out=mv[p][:, 3:4], in_=mv[p][:, 3:4], func=AF.Exp, scale=-0.5),
              R=[mv[p]], W=[mv[p]])
        kb.op("dve", lambda: nc.vector.scalar_tensor_tensor(out=of[p][:, :], in0=PO[:, :128], scalar=mv[p][:, 3:4],
                                                            in1=gn[:, :], op0=ALU.mult, op1=ALU.mult),
              R=[PO, mv[p], gn], W=[of[p]])
        kb.op("pool", lambda: nc.gpsimd.tensor_tensor(out=ost[p][:, :], in0=of[p][:, :], in1=r[:, c, :], op=ALU.mult),
              R=[of[p], r], W=[ost[p]])
        kb.dma("sp", o_d[cs, :], ost[p][:, :], R=[ost[p]], W=[o_d])
    return kb.finish()


def run_B_gla(yT, ytb, ytf, g_norm):
    nc = build_B_gla()
    maps = []
    for c in range(NCORES):
        b, h = c // 4, c % 4
        ts = slice(b * SEQ, (b + 1) * SEQ)
        maps.append({
            "qT": np.ascontiguousarray(yT[h * 64:(h + 1) * 64, ts]),
            "kT": np.ascontiguousarray(yT[256 + h * 64:256 + (h + 1) * 64, ts]),
            "k": to_pj(ytb[ts, h * 64:(h + 1) * 64]),
            "v": to_pj(ytb[ts, 256 + h * 128:256 + (h + 1) * 128]),
            "r": to_pj(ytf[ts, h * 128:(h + 1) * 128]),
            "g": to_pj(ytf[ts, 520 + h * 64:520 + (h + 1) * 64]),
            "gn": np.ascontiguousarray(np.broadcast_to(g_norm[None, :], (128, 128))).astype(np.float32),
            "tri": TRI,
        })
    res = run_spmd(nc, maps)
    o = np.zeros((BATCH * SEQ, 512), NPBF)
    for c in range(NCORES):
        b, h = c // 4, c % 4
        o[b * SEQ:(b + 1) * SEQ, h * 128:(h + 1) * 128] = res[c]["o"]
    return o


NBIS = 24
TOPK = 256


def build_B_dsa1(act_split=True):
    kb = KB()
    nc = kb.nc
    NK = 16
    qiT_d = kb.dram_in("qiT", [64, 8, NK * 128], BF16)
    kiT_d = kb.dram_in("kiT", [64, SEQ], BF16)
    wi_d = kb.dram_in("wi", [128, NK, 8], F32)
    cm_d = kb.dram_in("cmask", [128, 512], F32)
    idb_d = kb.dram_in("identb", [128, 128], BF16)
    stp_d = kb.dram_in("steps", [128, NBIS], F32)
    M_d = kb.dram_out("M", [NK, 128, SEQ], BF16)
    qiT = kb.sb([64, 8, NK * 128], BF16, "qiT")
    kiT = kb.sb([64, SEQ], BF16, "kiT")
    wi = kb.sb([128, NK, 8], F32, "wi")
    absw = kb.sb([128, NK, 8], F32, "absw")
    sgn = kb.sb([128, NK, 8], F32, "sgn")
    cm = kb.sb([128, 512], F32, "cm")
    idb = kb.sb([128, 128], BF16, "idb")
    stp = kb.sb([128, NBIS], F32, "stp")
    kb.dma("sp", qiT[:, :, :], qiT_d[:, :, :], R=[qiT_d], W=[qiT])
    kb.dma("pool", kiT[:, :], kiT_d[:, :], R=[kiT_d], W=[kiT])
    kb.dma("sp", wi[:, :, :], wi_d[:, :, :], R=[wi_d], W=[wi])
    kb.dma("sp", cm[:, :], cm_d[:, :], R=[cm_d], W=[cm])
    kb.dma("sp", idb[:, :], idb_d[:, :], R=[idb_d], W=[idb])
    kb.dma("sp", stp[:, :], stp_d[:, :], R=[stp_d], W=[stp])
    kb.op("act", lambda: nc.scalar.activation(out=absw[:, :, :], in_=wi[:, :, :], func=AF.Abs), R=[wi], W=[absw])
    kb.op("act", lambda: nc.scalar.activation(out=sgn[:, :, :], in_=wi[:, :, :], func=AF.Sign), R=[wi], W=[sgn])
    PS = [kb.ps([128, 512], F32, "PS") for _ in range(3)]
    PT = [kb.ps([128, 1024], BF16, "PT") for _ in range(2)]

    def write_mask(k, mt, Lk):
        kb.dma("sp", M_d[k, :, :Lk], mt[:, :Lk], R=[mt], W=[M_d])

    dsa1_body(kb, qiT, kiT, absw, sgn, cm, idb, stp, PS, PT, write_mask, act_split)
    return kb.finish()


def dsa1_body(kb, qiT, kiT, absw, sgn, cm, idb, stp, PS, PT, write_mask, act_split=True):
    nc = kb.nc
    NK = 16
    score = [kb.sb([128, SEQ], F32, "score") for _ in range(2)]
    selb = kb.sb([128, SEQ], BF16, "selb")
    junk2 = kb.sb([128, SEQ // 2], BF16, "junk2")
    MT = [kb.sb([128, SEQ], BF16, "MT") for _ in range(2)]
    rl = [kb.sb([128, 512], F32, "rl") for _ in range(3)]
    bs = [kb.sb([128, 16], F32, "bs") for _ in range(2)]
    st = {"rl": 0, "pt": 0}

    def scoring_units(k):
        sc_ = score[k % 2]
        units = []
        for c in range(k + 1):
            for hi in range(8):
                def u(c=c, hi=hi):
                    pp = PS[st["rl"] % 3]
                    r_ = rl[st["rl"] % 3]
                    st["rl"] += 1
                    kb.op("pe", lambda: nc.tensor.matmul(pp[:, :], lhsT=qiT[:, hi, k * 128:(k + 1) * 128],
                                                         rhs=kiT[:, c * 512:(c + 1) * 512], start=True, stop=True),
                          R=[qiT, kiT], W=[pp])
                    kb.op("act", lambda: nc.scalar.activation(out=r_[:, :], in_=pp[:, :], func=AF.Relu,
                                                              scale=absw[:, k, hi:hi + 1]), R=[pp, absw], W=[r_])
                    ssl = sc_[:, c * 512:(c + 1) * 512]
                    if hi == 0:
                        kb.op("dve", lambda: nc.vector.tensor_scalar(out=ssl, in0=r_[:, :], scalar1=sgn[:, k, 0:1],
                                                                     scalar2=None, op0=ALU.mult), R=[r_, sgn], W=[sc_])
                    else:
                        kb.op("dve", lambda: nc.vector.scalar_tensor_tensor(out=ssl, in0=r_[:, :],
                                                                            scalar=sgn[:, k, hi:hi + 1], in1=ssl,
                                                                            op0=ALU.mult, op1=ALU.add),
                              R=[r_, sgn, sc_], W=[sc_])
                units.append(u)
        return units

    for u in scoring_units(0):
        u()
    for k in range(NK):
        L = 512 * (k + 1)
        sc_ = score[k % 2]
        b_ = bs[k % 2]
        nxt = scoring_units(k + 1) if k + 1 < NK else []
        per_it = (len(nxt) + NBIS - 1) // NBIS
        kb.op("dve", lambda: nc.vector.tensor_reduce(out=b_[:, 0:1], in_=sc_[:, :L], axis=AX.X, op=ALU.min),
              R=[sc_], W=[b_])
        kb.op("dve", lambda: nc.vector.tensor_reduce(out=b_[:, 1:2], in_=sc_[:, :L], axis=AX.X, op=ALU.max),
              R=[sc_], W=[b_])
        kb.op("dve", lambda: nc.vector.tensor_tensor(out=sc_[:, L - 512:L], in0=sc_[:, L - 512:L], in1=cm[:, :],
                                                     op=ALU.add), R=[sc_, cm], W=[sc_])
        kb.op("dve", lambda: nc.vector.tensor_tensor(out=b_[:, 2:3], in0=b_[:, 1:2], in1=b_[:, 0:1], op=ALU.subtract),
              R=[b_], W=[b_])
        Ld = L // 2 if act_split else L
        for n in range(NBIS):
            kb.op("dve", lambda: nc.vector.tensor_scalar(out=b_[:, 7:8], in0=b_[:, 2:3], scalar1=stp[:, n:n + 1],
                                                         scalar2=None, op0=ALU.mult), R=[b_, stp], W=[b_])
            kb.op("dve", lambda: nc.vector.tensor_tensor(out=b_[:, 3:4], in0=b_[:, 0:1], in1=b_[:, 7:8], op=ALU.add),
                  R=[b_], W=[b_])
            if act_split:
                kb.op("act", lambda: nc.scalar.activation(out=junk2[:, :L - Ld], in_=sc_[:, Ld:L], func=AF.Sign,
                                                          bias=b_[:, 3:4], scale=-1.0, accum_out=b_[:, 6:7]),
                      R=[sc_, b_], W=[junk2, b_])
            kb.op("dve", lambda: nc.vector.tensor_scalar(out=selb[:, :Ld], in0=sc_[:, :Ld], scalar1=b_[:, 3:4],
                                                         scalar2=0.0, op0=ALU.is_ge, op1=ALU.add, accum_out=b_[:, 4:5]),
                  R=[sc_, b_], W=[selb, b_])
            for u in nxt[n * per_it:(n + 1) * per_it]:
                u()
            if act_split:
                kb.op("dve", lambda: nc.vector.tensor_scalar(out=b_[:, 6:7], in0=b_[:, 6:7], scalar1=-0.5,
                                                             scalar2=0.5 * (L - Ld), op0=ALU.mult, op1=ALU.add),
                      R=[b_], W=[b_])
                kb.op("dve", lambda: nc.vector.tensor_tensor(out=b_[:, 4:5], in0=b_[:, 4:5], in1=b_[:, 6:7], op=ALU.add),
                      R=[b_], W=[b_])
            kb.op("dve", lambda: nc.vector.tensor_scalar(out=b_[:, 5:6], in0=b_[:, 4:5], scalar1=TOPK - 0.25,
                                                         scalar2=b_[:, 7:8], op0=ALU.is_ge, op1=ALU.mult),
                  R=[b_], W=[b_])
            kb.op("dve", lambda: nc.vector.tensor_tensor(out=b_[:, 0:1], in0=b_[:, 0:1], in1=b_[:, 5:6], op=ALU.add),
                  R=[b_], W=[b_])
        for u in nxt[NBIS * per_it:]:
            u()
        kb.op("dve", lambda: nc.vector.tensor_scalar(out=selb[:, :L], in0=sc_[:, :L], scalar1=b_[:, 0:1], scalar2=None,
                                                     op0=ALU.is_ge), R=[sc_, b_], W=[selb])
        mt = MT[k % 2]
        nkb = 4 * (k + 1)
        for j0 in range(0, nkb, 8):
            pt = PT[st["pt"] % 2]
            for jj in range(8):
                j = j0 + jj
                if j >= nkb:
                    break
                kb.op("pe", lambda: nc.tensor.transpose(pt[:, jj * 128:(jj + 1) * 128], selb[:, j * 128:(j + 1) * 128],
                                                        idb[:, :]), R=[selb, idb], W=[pt])
            wdt = min(8, nkb - j0) * 128
            if st["pt"] % 2 == 0:
                kb.op("act", lambda: nc.scalar.copy(out=mt[:, j0 * 128:j0 * 128 + wdt], in_=pt[:, :wdt]), R=[pt], W=[mt])
            else:
                kb.op("dve", lambda: nc.vector.tensor_copy(out=mt[:, j0 * 128:j0 * 128 + wdt], in_=pt[:, :wdt]),
                      R=[pt], W=[mt])
            st["pt"] += 1
        write_mask(k, mt, L)


def run_B_dsa1(yT, ytf):
    nc = build_B_dsa1()
    steps = np.ascontiguousarray(np.broadcast_to((0.5 ** np.arange(1, NBIS + 1))[None, :], (128, NBIS))).astype(np.float32)
    maps = []
    for c in range(NCORES):
        b, m = c // 4, c % 4
        ts = slice(b * SEQ, (b + 1) * SEQ)
        tok = (np.arange(16)[:, None] * 512 + m * 128 + np.arange(128)[None, :]).reshape(-1) + b * SEQ
        qi = yT[1536:2048][:, tok].reshape(8, 64, 2048).transpose(1, 0, 2)
        wi = ytf[tok, 512:520].reshape(16, 128, 8).transpose(1, 0, 2)
        cm = np.where(np.arange(512)[None, :] <= (128 * m + np.arange(128))[:, None], 0.0, NEG).astype(np.float32)
        maps.append({"qiT": np.ascontiguousarray(qi), "kiT": np.ascontiguousarray(yT[2048:2112, ts]),
                     "wi": np.ascontiguousarray(wi), "cmask": cm, "identb": IDENT.astype(NPBF), "steps": steps})
    res = run_spmd(nc, maps)
    out = []
    for b in range(BATCH):
        M = np.zeros((NQB, 128, SEQ), NPBF)
        for m in range(4):
            M[m::4] = res[b * 4 + m]["M"]
        out.append(M)
    return out


def dsa_const():
    C = np.zeros((32, NDEL), np.float32)
    dl = np.arange(NDEL) - 127
    ok = dl >= 0
    C[rel_bucket_np(dl)[ok], np.arange(NDEL)[ok]] = 1.0
    return C


NPACK = NQB * (NQB + 1) // 2


def build_B_dsa2():
    kb = KB()
    nc = kb.nc
    q_d = kb.dram_in("q", [128, SEQ], BF16)
    k_d = kb.dram_in("k", [128, SEQ], BF16)
    v_d = kb.dram_in("v", [128, NQB, 128], BF16)
    rel_d = kb.dram_in("rel", [32, 2], F32)
    rf_d = kb.dram_in("relfar", [128, 2], F32)
    C_d = kb.dram_in("dsac", [32, NDEL], F32)
    M_d = kb.dram_in("Mp", [NPACK * 128 * 128], BF16)
    o_d = kb.dram_out("o", [SEQ, 128], BF16)
    E_d = kb.dram_tmp("Escr", [2, NDEL], F32)
    ax = AttnCtx(kb)
    rel = kb.sb([32, 2], F32, "rel")
    rf = kb.sb([128, 2], F32, "rf")
    Cs = kb.sb([32, NDEL], F32, "Cs")
    Es = kb.sb([2, NDEL], F32, "Es")
    kb.dma("sp", rel[:, :], rel_d[:, :], R=[rel_d], W=[rel])
    kb.dma("sp", rf[:, :], rf_d[:, :], R=[rf_d], W=[rf])
    kb.dma("sp", Cs[:, :], C_d[:, :], R=[C_d], W=[Cs])
    kb.op("act", lambda: nc.scalar.activation(out=rel[:, :], in_=rel[:, :], func=AF.Exp), R=[rel], W=[rel])
    for c in range((NDEL + 511) // 512):
        w = min(512, NDEL - c * 512)
        pp = ax.S[c % 3]
        kb.op("pe", lambda: nc.tensor.matmul(pp[:2, :w], lhsT=rel[:, :], rhs=Cs[:, c * 512:c * 512 + w],
                                             start=True, stop=True), R=[rel, Cs], W=[pp])
        kb.op("dve", lambda: nc.vector.tensor_copy(out=Es[:, c * 512:c * 512 + w], in_=pp[:2, :w]), R=[pp], W=[Es])
    kb.dma("sp", E_d[:, :], Es[:, :], R=[Es], W=[E_d])
    TT = [kb.sb([128, 17 * 128], F32, "TT") for _ in range(2)]
    for h in range(2):
        src = _AP(tensor=E_d.t.tensor, offset=h * NDEL, ap=[[1, 128], [1, 17 * 128]])
        kb.dma("sp", TT[h][:, :], src, R=[E_d], W=[TT[h]])
    q = kb.sb([128, SEQ], BF16, "q")
    k = kb.sb([128, SEQ], BF16, "k")
    for n, (s, d_) in enumerate(((q, q_d), (k, k_d))):
        for c in range(2):
            kb.dma("sp" if (n + c) % 2 == 0 else "pool", s[:, c * 4096:(c + 1) * 4096], d_[:, c * 4096:(c + 1) * 4096],
                   R=[d_], W=[s])
    V = load_vaug(kb, v_d, "V")
    Ms = [kb.sb([128, SEQ], BF16, "Ms") for _ in range(2)]
    ost = [kb.sb([128, 128], BF16, "ost") for _ in range(2)]
    for i in range(NQB):
        ms = Ms[i % 2]
        W_ = (i + 1) * 128
        off = (i * (i + 1) // 2) * 128 * 128
        src = _AP(tensor=M_d.t.tensor, offset=off, ap=[[W_, 128], [1, W_]])
        kb.dma("pool" if i % 2 else "sp", ms[:, :W_], src, R=[M_d], W=[ms])
        o = ost[i % 2]
        for h in range(2):
            def bias_of(j, h=h, i=i):
                return (rf, rf[:, h:h + 1]) if i - j > 16 else None

            def mask_of(j, h=h, i=i, ms=ms):
                sel = (ms, ms[:, j * 128:(j + 1) * 128])
                if i - j > 16:
                    return [sel]
                return [(TT[h], TT[h][:, (i - j) * 128:(i - j + 1) * 128]), sel]

            attn_qblock(ax, q, k, V, h, i, list(range(0, i + 1)), bias_of, mask_of, o, h * 64,
                        after=(None if h == 0 else
                               (lambda i=i, o=o: kb.dma("sp", o_d[i * 128:(i + 1) * 128, :], o[:, :], R=[o], W=[o_d]))))
    attn_flush(ax)
    return kb.finish()


def run_B_dsa2(yT, ytb, Msel, rel_table):
    nc = build_B_dsa2()
    C = dsa_const()
    packed = []
    for b in range(BATCH):
        M = Msel[b][:, ::-1, :]
        packed.append(np.concatenate([np.ascontiguousarray(M[i][:, :(i + 1) * 128]).reshape(-1) for i in range(NQB)]))
    maps = []
    for c in range(NCORES):
        b, m = c // 4, c % 4
        ts = slice(b * SEQ, (b + 1) * SEQ)
        maps.append({
            "q": np.ascontiguousarray(yT[512 + m * 128:512 + (m + 1) * 128, ts]),
            "k": np.ascontiguousarray(yT[1024 + m * 128:1024 + (m + 1) * 128, ts].reshape(128, NQB, 128)[:, :, ::-1]
                                      .reshape(128, SEQ)),
            "v": np.ascontiguousarray(to_pj(ytb[ts, 768 + m * 128:768 + (m + 1) * 128])[::-1]),
            "rel": np.ascontiguousarray(rel_table[:, 2 * m:2 * m + 2]),
            "relfar": np.ascontiguousarray(np.broadcast_to(rel_table[31:32, 2 * m:2 * m + 2], (128, 2))).astype(np.float32),
            "dsac": C, "Mp": packed[b],
        })
    res = run_spmd(nc, maps)
    o = np.zeros((BATCH * SEQ, 512), NPBF)
    for c in range(NCORES):
        b, m = c // 4, c % 4
        o[b * SEQ:(b + 1) * SEQ, m * 128:(m + 1) * 128] = res[c]["o"]
    return o


def kernel_unfused(x, ln_g, ln_b, rel_table, w_in_ab, w_gate_a, b_gate_a, g_norm_a, w_out_ab,
           w_in_cd, b_forget, w_out_cd, w1_dense, w3_dense, w2_dense,
           w_router, w1_moe, w3_moe, w2_moe):
    f32 = lambda a: np.ascontiguousarray(np.asarray(a, dtype=np.float32))
    x = f32(x)
    rel_table = f32(rel_table)
    big = [f32(w_in_ab), f32(w_in_cd), f32(w_out_ab), f32(w_out_cd), f32(w1_dense), f32(w3_dense), f32(w2_dense),
           f32(w1_moe), f32(w3_moe), f32(w2_moe)]
    (wi_ab, wi_cd, wo_ab, wo_cd, w1d, w3d, w2d, w1m, w3m, w2m) = cast_weights(big)
    del big
    xf = x.reshape(BATCH * SEQ, D)
    for layer in range(DEPTH):
        j = layer // 2
        lnp4 = np.stack([ln_g[layer, 0], ln_b[layer, 0], ln_g[layer, 1], ln_b[layer, 1]]).astype(np.float32)
        if layer % 2 == 0:
            yT, ytb, ytf = run_A(xf, wi_ab[j],
                                 [(0, 256), (256, 256), (1552, 512), (2064, 512), (3088, 512), (3600, 64)],
                                 [(256, 256), (512, 512), (2576, 512)],
                                 [(1024, 512), (3664, 8)],
                                 gate=(1536,), wg=f32(w_gate_a[j]), bg=f32(b_gate_a[j]))
            oa = run_B_gla(yT, ytb, ytf, f32(g_norm_a[j]))
            Msel = run_B_dsa1(yT, ytf)
            ob = run_B_dsa2(yT, ytb, Msel, rel_table)
            del Msel
            o = np.concatenate([oa, ob], axis=1)
            wf = ffn_chunk_layout(w1d[j], w3d[j], w2d[j])
            xf = run_C(o, xf, wo_ab[j], lnp4, wf, 1, 11, 2)
        else:
            yT, ytb, ytf = run_A(xf, wi_cd[j],
                                 [(0, 512), (512, 512), (1544, 512), (2056, 512)],
                                 [(1024, 512), (2568, 512)],
                                 [(1536, 8)])
            o = run_B_cd(yT, ytb, ytf, f32(b_forget[j]), rel_table)
            wf = np.concatenate([ffn_chunk_layout(w1m[j, e], w3m[j, e], w2m[j, e]) for e in range(8)], axis=0)
            xf = run_C(o, xf, wo_cd[j], lnp4, wf, 8, 14, 2, wr=f32(w_router[j]))
    return xf.reshape(BATCH, SEQ, D).astype(np.float32)


I32 = mybir.dt.int32

import os
SKIP = os.environ.get('FZ_SKIP', '')
GROUPS = [[0, 1, 2, 3], [4, 5, 6, 7]]
LK = [512 * (k + 1) for k in range(16)]
MOFF = [128 * sum(LK[:k]) for k in range(16)]
MTOT = 128 * sum(LK)
MPARTS = []
_g = 0
for _k in range(16):
    _r0 = MOFF[_k] // 512
    _n = 128 * LK[_k] // 512
    _halves = 1 if _k < 8 else 2
    for _h in range(_halves):
        _nh = _n // _halves
        MPARTS.append((_k, _h, _r0 + _h * _nh, _nh, _g))
        _g += 4 * _nh
MG = {(p[0], p[1]): p for p in MPARTS}


class KBF(KB):
    def __init__(self):
        super().__init__()
        self.ccsem = self.es.enter_context(self.nc.semaphore("ccsem"))
        self.cccnt = 0
        self.stack = [self.es]

    def sb(self, shape, dt, name="sb"):
        return Buf(self.stack[-1].enter_context(self.nc.sbuf_tensor(self._nm(name), list(shape), dt)))

    def ps(self, shape, dt=F32, name="ps"):
        b = Buf(self.stack[-1].enter_context(self.nc.psum_tensor(self._nm(name), list(shape), dt)))
        b.psum = True
        return b

    def barrier(self):
        evs = []
        for q in ("sp", "act", "pool"):
            for i in range(self.NDS):
                if self.dcnt[q][i] > 0:
                    evs.append((self.dsem[q][i], self.dcnt[q][i], "d%s%d" % (q, i)))
        for e in ("pe", "dve", "act", "pool", "sp"):
            if self.ecnt[e] > 0:
                evs.append((self.esem[e], self.ecnt[e], e))
        if self.cccnt > 0:
            evs.append((self.ccsem, self.cccnt, "cc"))
        for e in ("pe", "dve", "act", "pool", "sp"):
            for ev in evs:
                if ev[2] == e:
                    continue
                self._wait(e, ev)

    def scope(self):
        kb = self

        class _S:
            def __enter__(s):
                st = ExitStack()
                kb.stack.append(st)
                return st

            def __exit__(s, *a):
                kb.barrier()
                st = kb.stack.pop()
                st.close()
                return False

        return _S()

    def dram_tmp(self, name, shape, dt):
        return Buf(self.nc.dram_tensor(name, list(shape), dt).ap())

    def collective(self, kind, src, dst, src_ap=None, dst_ap=None):
        self._deps("pool", [src], [dst])
        sa = src.t if src_ap is None else src_ap
        da = dst.t if dst_ap is None else dst_ap
        ins = self.nc.gpsimd.collective_compute(kind, ALU.bypass, replica_groups=GROUPS,
                                                ins=[sa.opt()], outs=[da.opt()])
        self.cccnt += CC_INC
        ins.then_inc(self.ccsem, CC_INC)
        ev = (self.ccsem, self.cccnt, "cc")
        _kbf_mark(self, ev, [src], [dst])
        return ev


CC_INC = 1


def load_w_cast(kb, dst, src_d, ncols):
    for kc in range(8):
        kb.dma("pool", dst[:, kc, :ncols], src_d[:, kc, :ncols], R=[src_d], Wd=[dst])


def load_xT_chunk(kb, x_, xT_all, c):
    r, t0 = c // 4, (c % 4) * 512
    for cf in range(4):
        row0 = (cf * 4 + r) * 256
        kb.dma("sp", x_[:, 2 * cf:2 * cf + 2, :],
               xT_all[row0:row0 + 256, t0:t0 + 512].rearrange("(k p) t -> p k t", p=128),
               R=[xT_all], Wd=[x_] if cf else (), W=[x_] if cf == 0 else ())


def emit_xT(kb, ax_ps, ident, xtile, tt, stg, xT_loc, xTs_loc):
    nc = kb.nc
    for kc in range(8):
        pt = ax_ps[kc // 4]
        kb.op("pe", lambda: nc.tensor.transpose(pt[:, (kc % 4) * 128:(kc % 4 + 1) * 128],
                                                xtile[:, kc * 128:(kc + 1) * 128], ident[:, :]),
              R=[xtile, ident], W=[pt])
    q = tt % 4
    for hf in range(2):
        src = ax_ps[hf][:, :].rearrange("p (k t) -> p k t", k=4)
        dst = stg[:, hf * 4:(hf + 1) * 4, q * 128:(q + 1) * 128]
        if hf == 0:
            kb.op("dve", lambda: nc.vector.tensor_copy(out=dst, in_=src), R=[ax_ps[hf]], W=[stg])
        else:
            kb.op("act", lambda: nc.scalar.copy(out=dst, in_=src), R=[ax_ps[hf]], W=[stg])
    if q == 3:
        g = tt // 4
        kb.dma("sp", xT_loc[:, g * 512:(g + 1) * 512].rearrange("(k p) t -> p k t", p=128), stg[:, :, :],
               R=[stg], Wd=[xT_loc])
        if xTs_loc is not None:
            for j in range(4):
                kb.dma("sp", xTs_loc[j * D:(j + 1) * D, g * 128:(g + 1) * 128].rearrange("(k p) t -> p k t", p=128),
                       stg[:, :, j * 128:(j + 1) * 128], R=[stg], Wd=[xTs_loc])


def toeplitz_load(kb, TT, E_d, h, q="act"):
    for s in range(128):
        kb.dma(q if s % 2 == 0 else "sp", TT[s:s + 1, :], E_d[h:h + 1, 127 - s:127 - s + 17 * 128], R=[E_d], Wd=[TT])


def build_E_table(kb, ax, rel_d, C_d, E_d):
    nc = kb.nc
    rel = kb.sb([32, 2], F32, "rel")
    Cs = kb.sb([32, NDEL], F32, "Cs")
    Es = kb.sb([2, NDEL], F32, "Es")
    kb.dma("sp", rel[:, :], rel_d[:, :], R=[rel_d], W=[rel])
    kb.dma("sp", Cs[:, :], C_d[:, :], R=[C_d], W=[Cs])
    kb.op("act", lambda: nc.scalar.activation(out=rel[:, :], in_=rel[:, :], func=AF.Exp), R=[rel], W=[rel])
    for c in range((NDEL + 511) // 512):
        w = min(512, NDEL - c * 512)
        pp = ax.S[c % 3]
        kb.op("pe", lambda: nc.tensor.matmul(pp[:2, :w], lhsT=rel[:, :], rhs=Cs[:, c * 512:c * 512 + w],
                                             start=True, stop=True), R=[rel, Cs], W=[pp])
        kb.op("dve", lambda: nc.vector.tensor_copy(out=Es[:, c * 512:c * 512 + w], in_=pp[:2, :w]), R=[pp], W=[Es])
    kb.dma("sp", E_d[:, :], Es[:, :], R=[Es], W=[E_d])


class OTOut:
    def __init__(self, kb, identb, oT_loc, row0, name):
        self.kb, self.identb, self.oT_loc, self.row0 = kb, identb, oT_loc, row0
        self.pt = [kb.ps([128, 1024], BF16, "ptO" + name) for _ in range(1)]
        self.stg = [kb.sb([128, 512], BF16, "stgO" + name) for _ in range(2)]
        self.n = 0

    def put(self, i, ost):
        kb, nc = self.kb, self.kb.nc
        g = i // 4
        st = self.stg[g % 2]
        pt = self.pt[0]
        q = i % 4
        kb.op("pe", lambda: nc.tensor.transpose(pt[:, q * 128:(q + 1) * 128], ost[:, :], self.identb[:, :]),
              R=[ost, self.identb], W=[pt])
        if q == 3:
            kb.op("act", lambda: nc.scalar.copy(out=st[:, :], in_=pt[:, :512]), R=[pt], W=[st])
            rank, grp = g // 4, g % 4
            r0 = (rank * 4 + grp) * 256 + self.row0
            kb.dma("sp", self.oT_loc[r0:r0 + 128, :], st[:, :], R=[st], Wd=[self.oT_loc])


def phase_cd(kb, L, xT_all, oT_loc, cst):
    nc = kb.nc
    with kb.scope():
        wcd_d = L["wcd"]
        NCOL = 770
        w = kb.sb([128, 8, NCOL], BF16, "wcd")
        load_w_cast(kb, w, wcd_d, NCOL)
        tri = kb.sb([128, 128], F32, "tri")
        ones = kb.sb([128, 128], F32, "ones")
        identb = kb.sb([128, 128], BF16, "identb")
        kb.dma("sp", tri[:, :], cst["tri"][:, :], R=[cst["tri"]], W=[tri])
        kb.dma("sp", identb[:, :], cst["identb"][:, :], R=[cst["identb"]], W=[identb])
        kb.op("dve", lambda: nc.vector.memset(ones[:, :], 1.0), W=[ones])
        ax = AttnCtx(kb, n_s=4, n_o=2)
        E_d = L["Escr"]
        build_E_table(kb, ax, L["rel2"], cst["dilc"], E_d)
        TT = [kb.sb([128, 17 * 128], F32, "TT") for _ in range(2)]
        for h in range(2):
            if "toep" in SKIP:
                kb.op("dve", lambda: nc.vector.memset(TT[h][:, :], 1.0), W=[TT[h]])
            else:
                toeplitz_load(kb, TT[h], E_d, h)
        qc = kb.sb([128, SEQ], BF16, "qc")
        kc_ = kb.sb([128, SEQ], BF16, "kc")
        qd = kb.sb([128, SEQ], BF16, "qd")
        kd = kb.sb([128, SEQ], BF16, "kd")
        Vc = kb.sb([128, NQB, 2, 65], BF16, "Vc")
        Vd = kb.sb([128, NQB, 2, 65], BF16, "Vd")
        fc = kb.sb([128, NQB, 2], F32, "fc")
        kb.op("pool", lambda: nc.gpsimd.memset(Vc[:, :, :, :], 1.0), W=[Vc])
        kb.op("pool", lambda: nc.gpsimd.memset(Vd[:, :, :, :], 1.0), W=[Vd])
        xc = [kb.sb([128, 8, 512], BF16, "xc") for _ in range(2)]
        n_ev = 0
        passes = [(True, True)] if "twopass" not in SKIP else [(True, False), (False, True)]
        NCH = int(os.environ.get("FZ_NCH", "16"))
        for c2 in range((NCH if "proj" not in SKIP else 0) * len(passes)):
            c = c2 % NCH
            do_fm, do_tm = passes[c2 // NCH]
            x_ = xc[c % 2]
            load_xT_chunk(kb, x_, xT_all, c)
            for bi, dst in enumerate((qc, kc_, qd, kd) if ("projfm" not in SKIP and do_fm) else ()):
                pp = ax.S[n_ev % 4]
                for k8 in range(8):
                    kb.op("pe", lambda: nc.tensor.matmul(pp[:, :], lhsT=w[:, k8, bi * 128:(bi + 1) * 128],
                                                         rhs=x_[:, k8, :], start=(k8 == 0), stop=(k8 == 7)),
                          R=[w, x_], W=[pp])
                d_ = dst[:, c * 512:(c + 1) * 512]
                if n_ev % 2 == 0 or "fmdve" in SKIP:
                    kb.op("dve", lambda: nc.vector.tensor_copy(out=d_, in_=pp[:, :]), R=[pp], W=[dst])
                else:
                    kb.op("act", lambda: nc.scalar.copy(out=d_, in_=pp[:, :]), R=[pp], W=[dst])
                n_ev += 1
            for t4 in range(4 if ("projtm" not in SKIP and do_tm) else 0):
                j = c * 4 + t4
                pp = ax.S[n_ev % 4]
                n_ev += 1
                for k8 in range(8):
                    kb.op("pe", lambda: nc.tensor.matmul(pp[:, :258], lhsT=x_[:, k8, t4 * 128:(t4 + 1) * 128],
                                                         rhs=w[:, k8, 512:770], start=(k8 == 0), stop=(k8 == 7)),
                          R=[w, x_], W=[pp])
                kb.op("dve", lambda: nc.vector.tensor_copy(out=Vc[:, j, :, 0:64],
                                                           in_=pp[:, 0:128].rearrange("p (h d) -> p h d", h=2)),
                      R=[pp], W=[Vc])
                if "vddve" in SKIP:
                    kb.op("dve", lambda: nc.vector.tensor_copy(out=Vd[:, j, :, 0:64],
                                                               in_=pp[:, 128:256].rearrange("p (h d) -> p h d", h=2)),
                          R=[pp], W=[Vd])
                else:
                    kb.op("act", lambda: nc.scalar.copy(out=Vd[:, j, :, 0:64],
                                                        in_=pp[:, 128:256].rearrange("p (h d) -> p h d", h=2)),
                          R=[pp], W=[Vd])
                kb.op("dve", lambda: nc.vector.tensor_copy(out=fc[:, j, :], in_=pp[:, 256:258]), R=[pp], W=[fc])
        bf = kb.sb([128, 2], F32, "bf")
        lf = kb.sb([128, 2, NQB], F32, "lf")
        kb.dma("sp", bf[:, :], L["bf"][:, :], R=[L["bf"]], W=[bf])
        for h in range(2):
            kb.op("dve", lambda: nc.vector.tensor_scalar(out=lf[:, h, :], in0=fc[:, :, h], scalar1=bf[:, h:h + 1],
                                                         scalar2=None, op0=ALU.add), R=[fc, bf], W=[lf])
        kb.op("act", lambda: nc.scalar.activation(out=lf[:, :, :], in_=lf[:, :, :], func=AF.Exp, scale=-1.0),
              R=[lf], W=[lf])
        kb.op("act", lambda: nc.scalar.activation(out=lf[:, :, :], in_=lf[:, :, :], func=AF.Ln, bias=1.0),
              R=[lf], W=[lf])
        kb.op("dve", lambda: nc.vector.tensor_scalar(out=lf[:, :, :], in0=lf[:, :, :], scalar1=-1.0, scalar2=None,
                                                     op0=ALU.mult), R=[lf], W=[lf])
        lf2 = lf[:, :, :].rearrange("p h j -> p (h j)")
        p1, p2 = ax.O[0], ax.O[1]
        kb.op("pe", lambda: nc.tensor.matmul(p1[:, :128], lhsT=tri[:, :], rhs=lf2, start=True, stop=True),
              R=[tri, lf], W=[p1])
        kb.op("pe", lambda: nc.tensor.matmul(p2[:, :128], lhsT=ones[:, :], rhs=lf2, start=True, stop=True),
              R=[ones, lf], W=[p2])
        tot = kb.sb([128, 2, NQB], F32, "tot")
        carry = kb.sb([128, 2, NQB], F32, "carry")
        negF = kb.sb([128, 2, NQB], F32, "negF")
        kb.op("dve", lambda: nc.vector.tensor_copy(out=tot[:, :, :].rearrange("p h j -> p (h j)"), in_=p2[:, :128]),
              R=[p2], W=[tot])
        for h in range(2):
            kb.op("dve", lambda: nc.vector.tensor_tensor_scan(out=carry[:, h, :], data0=ones[:, :NQB],
                                                              data1=tot[:, h, :], initial=0.0, op0=ALU.mult,
                                                              op1=ALU.add), R=[ones, tot], W=[carry])
        kb.op("dve", lambda: nc.vector.tensor_tensor(out=carry[:, :, :], in0=carry[:, :, :], in1=tot[:, :, :],
                                                     op=ALU.subtract), R=[carry, tot], W=[carry])
        kb.op("dve", lambda: nc.vector.tensor_tensor(out=negF[:, :, :].rearrange("p h j -> p (h j)"), in0=p1[:, :128],
                                                     in1=carry[:, :, :].rearrange("p h j -> p (h j)"), op=ALU.add),
              R=[p1, carry], W=[negF])
        kb.op("dve", lambda: nc.vector.tensor_scalar(out=negF[:, :, :], in0=negF[:, :, :], scalar1=-1.0, scalar2=None,
                                                     op0=ALU.mult), R=[negF], W=[negF])
        outc = OTOut(kb, identb, oT_loc, 0, "c")
        outd = OTOut(kb, identb, oT_loc, 128, "d")
        ostd = [kb.sb([128, 128], BF16, "ostd") for _ in range(2)]
        ostc = [kb.sb([128, 128], BF16, "ostc") for _ in range(2)]
        Bi = [kb.sb([128, NQB], F32, "Bi") for _ in range(3)]
        nb = 0
        for i in range(NQB if "attn" not in SKIP else 0):
            od = ostd[i % 2]
            for h in range(2):
                j0 = max(0, i - 16)
                attn_qblock(ax, qd, kd, Vd, h, i, list(range(j0, i + 1)), None,
                            lambda j, h=h, i=i: (TT[h], TT[h][:, (i - j) * 128:(i - j + 1) * 128]), od, h * 64,
                            after=(None if h == 0 else (lambda i=i, od=od: outd.put(i, od))))
            oc = ostc[i % 2]
            for h in range(2):
                B = Bi[nb % 3]
                nb += 1
                kb.op("dve", lambda: nc.vector.tensor_scalar(out=B[:, :i + 1], in0=negF[:, h, :i + 1],
                                                             scalar1=carry[:, h, i:i + 1], scalar2=None, op0=ALU.add),
                      R=[negF, carry], W=[B])
                attn_qblock(ax, qc, kc_, Vc, h, i, list(range(0, i + 1)),
                            lambda j, B=B: (B, B[:, j:j + 1]),
                            lambda j, i=i: ((tri, tri[:, :]) if j == i else None), oc, h * 64,
                            after=(None if h == 0 else (lambda i=i, oc=oc: outc.put(i, oc))))
        attn_flush(ax)


def phase_gla(kb, L, xT_all, oT_loc, cst):
    nc = kb.nc
    with kb.scope():
        tri = kb.sb([128, 128], F32, "tri")
        identb = kb.sb([128, 128], BF16, "identb")
        gn = kb.sb([128, 128], F32, "gn")
        eps = kb.sb([128, 1], F32, "eps")
        kb.dma("sp", tri[:, :], cst["tri"][:, :], R=[cst["tri"]], W=[tri])
        kb.dma("sp", identb[:, :], cst["identb"][:, :], R=[cst["identb"]], W=[identb])
        kb.dma("sp", gn[:, :], L["gn"][:, :], R=[L["gn"]], W=[gn])
        kb.op("dve", lambda: nc.vector.memset(eps[:, :], LN_EPS), W=[eps])
        qT = kb.sb([64, SEQ], BF16, "qT")
        kT = kb.sb([64, SEQ], BF16, "kT")
        ktm = kb.sb([128, NQB, 64], BF16, "ktm")
        v = kb.sb([128, NQB, 128], BF16, "v")
        r = kb.sb([128, NQB, 128], F32, "r")
        g = kb.sb([128, NQB, 64], F32, "g")
        PG = [kb.ps([128, 512], F32, "PG")]
        PGT = [kb.ps([128, 512], F32, "PGT")]
        PA = [kb.ps([128, 512], F32, "PA") for _ in range(2)]
        PO = kb.ps([128, 512], F32, "PO")
        PU = kb.ps([128, 512], F32, "PU")
        out = OTOut(kb, identb, oT_loc, 0, "g")
        with kb.scope():
            NCOL = 464
            w = kb.sb([128, 8, NCOL], BF16, "wgl")
            load_w_cast(kb, w, L["wgl"], NCOL)
            wg = kb.sb([16, 64], F32, "wg")
            bg = kb.sb([128, 64], F32, "bg")
            kb.dma("sp", wg[:, :], L["wg"][:, :], R=[L["wg"]], W=[wg])
            kb.dma("sp", bg[:, :], L["bg"][:, :], R=[L["bg"]], W=[bg])
            xc = [kb.sb([128, 8, 512], BF16, "xc") for _ in range(2)]
            gaT = [kb.sb([16, 512], F32, "gaT") for _ in range(2)]
            zt = [kb.sb([128, 64], F32, "zt") for _ in range(2)]
            pr = [PA[0], PA[1], PO]
            n_ev = 0
            for c in range(16):
                x_ = xc[c % 2]
                ga_ = gaT[c % 2]
                load_xT_chunk(kb, x_, xT_all, c)
                for (c0, ncol, dst) in ((0, 64, qT), (64, 64, kT), (128, 16, ga_)):
                    pp = pr[n_ev % 3]
                    for k8 in range(8):
                        kb.op("pe", lambda: nc.tensor.matmul(pp[:ncol, :], lhsT=w[:, k8, c0:c0 + ncol], rhs=x_[:, k8, :],
                                                             start=(k8 == 0), stop=(k8 == 7)), R=[w, x_], W=[pp])
                    d_ = dst[:, c * 512:(c + 1) * 512] if dst is not ga_ else ga_[:, :]
                    if n_ev % 2 == 0:
                        kb.op("dve", lambda: nc.vector.tensor_copy(out=d_, in_=pp[:ncol, :]), R=[pp], W=[dst])
                    else:
                        kb.op("act", lambda: nc.scalar.copy(out=d_, in_=pp[:ncol, :]), R=[pp], W=[dst])
                    n_ev += 1
                for t4 in range(4):
                    j = c * 4 + t4
                    pp = pr[n_ev % 3]
                    n_ev += 1
                    for k8 in range(8):
                        kb.op("pe", lambda: nc.tensor.matmul(pp[:, :320], lhsT=x_[:, k8, t4 * 128:(t4 + 1) * 128],
                                                             rhs=w[:, k8, 144:464], start=(k8 == 0), stop=(k8 == 7)),
                              R=[w, x_], W=[pp])
                    kb.op("dve", lambda: nc.vector.tensor_copy(out=ktm[:, j, :], in_=pp[:, 0:64]), R=[pp], W=[ktm])
                    kb.op("dve", lambda: nc.vector.tensor_copy(out=v[:, j, :], in_=pp[:, 64:192]), R=[pp], W=[v])
                    kb.op("act", lambda: nc.scalar.activation(out=r[:, j, :], in_=pp[:, 192:320], func=AF.Silu),
                          R=[pp], W=[r])
                    pq = pr[n_ev % 3]
                    n_ev += 1
                    z = zt[j % 2]
                    kb.op("pe", lambda: nc.tensor.matmul(pq[:, :64], lhsT=ga_[:, t4 * 128:(t4 + 1) * 128], rhs=wg[:, :],
                                                         start=True, stop=True), R=[ga_, wg], W=[pq])
                    kb.op("dve", lambda: nc.vector.tensor_tensor(out=z[:, :], in0=pq[:, :64], in1=bg[:, :], op=ALU.add),
                          R=[pq, bg], W=[z])
                    kb.op("act", lambda: nc.scalar.activation(out=z[:, :], in_=z[:, :], func=AF.Exp, scale=-1.0),
                          R=[z], W=[z])
                    kb.op("act", lambda: nc.scalar.activation(out=z[:, :], in_=z[:, :], func=AF.Ln, bias=1.0),
                          R=[z], W=[z])
                    kb.op("dve", lambda: nc.vector.tensor_scalar(out=g[:, j, :], in0=z[:, :], scalar1=-1.0 / 16.0,
                                                                 scalar2=None, op0=ALU.mult), R=[z], W=[g])
        eGT = [kb.sb([64, 128], F32, "eGT") for _ in range(2)]
        enGT = [kb.sb([64, 128], F32, "enGT") for _ in range(2)]
        enG = [kb.sb([128, 64], F32, "enG") for _ in range(2)]
        qgT = [kb.sb([64, 128], BF16, "qgT") for _ in range(2)]
        kgT = [kb.sb([64, 128], BF16, "kgT") for _ in range(2)]
        kg = [kb.sb([128, 64], BF16, "kg") for _ in range(2)]
        Am = [kb.sb([128, 128], BF16, "Am") for _ in range(2)]
        S32 = kb.sb([64, 128], F32, "S32")
        Sbf = kb.sb([64, 128], BF16, "Sbf")
        st6 = [kb.sb([128, 6], F32, "st6") for _ in range(2)]
        mv = [kb.sb([128, 4], F32, "mv") for _ in range(2)]
        of = [kb.sb([128, 128], F32, "of") for _ in range(2)]
        ost = [kb.sb([128, 128], BF16, "ost") for _ in range(2)]
        for c in range(NQB):
            p = c % 2
            cs = slice(c * 128, (c + 1) * 128)
            kb.op("pe", lambda: nc.tensor.matmul(PG[0][:, :64], lhsT=tri[:, :], rhs=g[:, c, :], start=True, stop=True),
                  R=[tri, g], W=[PG[0]])
            kb.op("pe", lambda: nc.tensor.matmul(PGT[0][:64, :128], lhsT=g[:, c, :], rhs=tri[:, :], start=True, stop=True),
                  R=[tri, g], W=[PGT[0]])
            kb.op("act", lambda: nc.scalar.activation(out=eGT[p][:, :], in_=PGT[0][:64, :128], func=AF.Exp),
                  R=[PGT[0]], W=[eGT[p]])
            kb.op("act", lambda: nc.scalar.activation(out=enGT[p][:, :], in_=PGT[0][:64, :128], func=AF.Exp, scale=-1.0),
                  R=[PGT[0]], W=[enGT[p]])
            kb.op("act", lambda: nc.scalar.activation(out=enG[p][:, :], in_=PG[0][:, :64], func=AF.Exp, scale=-1.0),
                  R=[PG[0]], W=[enG[p]])
            kb.op("dve", lambda: nc.vector.scalar_tensor_tensor(out=qgT[p][:, :], in0=qT[:, cs], scalar=0.125,
                                                                in1=eGT[p][:, :], op0=ALU.mult, op1=ALU.mult),
                  R=[qT, eGT[p]], W=[qgT[p]])
            kb.op("dve", lambda: nc.vector.tensor_tensor(out=kgT[p][:, :], in0=kT[:, cs], in1=enGT[p][:, :], op=ALU.mult),
                  R=[kT, enGT[p]], W=[kgT[p]])
            kb.op("dve", lambda: nc.vector.tensor_tensor(out=kg[p][:, :], in0=ktm[:, c, :], in1=enG[p][:, :], op=ALU.mult),
                  R=[ktm, enG[p]], W=[kg[p]])
            kb.op("pe", lambda: nc.tensor.matmul(PA[p][:, :128], lhsT=kgT[p][:, :], rhs=qgT[p][:, :], start=True, stop=True),
                  R=[kgT[p], qgT[p]], W=[PA[p]])
            kb.op("dve", lambda: nc.vector.tensor_tensor(out=Am[p][:, :], in0=PA[p][:, :128], in1=tri[:, :], op=ALU.mult),
                  R=[PA[p], tri], W=[Am[p]])
            kb.op("pe", lambda: nc.tensor.matmul(PO[:, :128], lhsT=Am[p][:, :], rhs=v[:, c, :], start=True, stop=(c == 0)),
                  R=[Am[p], v], W=[PO])
            if c > 0:
                kb.op("pe", lambda: nc.tensor.matmul(PO[:, :128], lhsT=qgT[p][:, :], rhs=Sbf[:, :], start=False, stop=True),
                      R=[qgT[p], Sbf], W=[PO])
            if c < NQB - 1:
                kb.op("pe", lambda: nc.tensor.matmul(PU[:64, :128], lhsT=kg[p][:, :], rhs=v[:, c, :], start=True, stop=True),
                      R=[kg[p], v], W=[PU])
                eGl = eGT[p][:, 127:128]
                if c == 0:
                    kb.op("dve", lambda: nc.vector.tensor_scalar(out=S32[:, :], in0=PU[:64, :128], scalar1=eGl,
                                                                 scalar2=None, op0=ALU.mult), R=[PU, eGT[p]], W=[S32])
                else:
                    kb.op("dve", lambda: nc.vector.tensor_scalar(out=S32[:, :], in0=S32[:, :], scalar1=eGl, scalar2=None,
                                                                 op0=ALU.mult), R=[S32, eGT[p]], W=[S32])
                    kb.op("dve", lambda: nc.vector.scalar_tensor_tensor(out=S32[:, :], in0=PU[:64, :128], scalar=eGl,
                                                                        in1=S32[:, :], op0=ALU.mult, op1=ALU.add),
                          R=[PU, eGT[p], S32], W=[S32])
                kb.op("dve", lambda: nc.vector.tensor_copy(out=Sbf[:, :], in_=S32[:, :]), R=[S32], W=[Sbf])
            kb.op("dve", lambda: nc.vector.bn_stats(out=st6[p][:, :], in_=PO[:, :128]), R=[PO], W=[st6[p]])
            kb.op("dve", lambda: nc.vector.bn_aggr(out=mv[p][:, 0:2], in_=st6[p][:, :]), R=[st6[p]], W=[mv[p]])
            kb.op("dve", lambda: nc.vector.scalar_tensor_tensor(out=mv[p][:, 2:3], in0=mv[p][:, 0:1], scalar=mv[p][:, 0:1],
                                                                in1=mv[p][:, 1:2], op0=ALU.mult, op1=ALU.add),
                  R=[mv[p]], W=[mv[p]])
            kb.op("act", lambda: nc.scalar.activation(out=mv[p][:, 3:4], in_=mv[p][:, 2:3], func=AF.Ln, bias=eps[:, 0:1]),
                  R=[mv[p], eps], W=[mv[p]])
            kb.op("act", lambda: nc.scalar.activation(out=mv[p][:, 3:4], in_=mv[p][:, 3:4], func=AF.Exp, scale=-0.5),
                  R=[mv[p]], W=[mv[p]])
            kb.op("dve", lambda: nc.vector.scalar_tensor_tensor(out=of[p][:, :], in0=PO[:, :128], scalar=mv[p][:, 3:4],
                                                                in1=gn[:, :], op0=ALU.mult, op1=ALU.mult),
                  R=[PO, mv[p], gn], W=[of[p]])
            kb.op("pool", lambda: nc.gpsimd.tensor_tensor(out=ost[p][:, :], in0=of[p][:, :], in1=r[:, c, :], op=ALU.mult),
                  R=[of[p], r], W=[ost[p]])
            out.put(c, ost[p])


def _kbf_deps(self, e, R, W, Wd=()):
    evs = []
    for b in R:
        evs.extend(b.wl())
        if getattr(b, "psum", False):
            evs.extend(x for x in b.r if x[2] != e)
    for b in W:
        evs.extend(b.wl())
        evs.extend(b.r)
    for b in Wd:
        evs.extend(b.r)
        evs.extend(getattr(b, "w_excl", []))
    best = {}
    for ev in evs:
        k = ev[2]
        if e == "pe" and k == "pe":
            continue
        if k not in best or best[k][1] < ev[1]:
            best[k] = ev
    for ev in best.values():
        self._wait(e, ev)


def _buf_wl(self):
    if self.w is None:
        return []
    return self.w if isinstance(self.w, list) else [self.w]


Buf.wl = _buf_wl


def _kbf_mark(self, ev, R, W, Wd=()):
    KB._mark(self, ev, R, W)
    for b in W:
        b.w = [ev]
        b.w_excl = [ev]
    for b in Wd:
        cur = b.wl()
        cur = [x for x in cur if x[2] != ev[2]] + [ev]
        b.w = cur
        b.r = []


def _kbf_dma(self, q, out, in_, R=(), W=(), Wd=(), **kw):
    _kbf_deps(self, q, R, W, Wd)
    i = self.dnext[q]
    self.dnext[q] = (i + 1) % self.NDS
    key = "d%s%d" % (q, i)
    if self.dcnt[q][i] > 0:
        self._wait(q, (self.dsem[q][i], self.dcnt[q][i], key))
    self.dcnt[q][i] += 16
    self.eng[q].dma_start(out=out, in_=in_, **kw).then_inc(self.dsem[q][i], 16)
    ev = (self.dsem[q][i], self.dcnt[q][i], key)
    _kbf_mark(self, ev, R, W, Wd)
    return ev


def _kbf_op(self, e, fn, R=(), W=()):
    _kbf_deps(self, e, R, W)
    ins = fn()
    self.ecnt[e] += 1
    ins.then_inc(self.esem[e], 1)
    ev = (self.esem[e], self.ecnt[e], e)
    _kbf_mark(self, ev, R, W)
    return ev


def _kbf_idma(self, out, in_, idx_ap, R=(), W=(), Wd=()):
    q = "pool"
    _kbf_deps(self, q, R, W, Wd)
    i = self.dnext[q]
    self.dnext[q] = (i + 1) % self.NDS
    key = "d%s%d" % (q, i)
    if self.dcnt[q][i] > 0:
        self._wait(q, (self.dsem[q][i], self.dcnt[q][i], key))
    self.dcnt[q][i] += 16
    self.nc.gpsimd.indirect_dma_start(out=out, out_offset=None, in_=in_,
                                      in_offset=bass.IndirectOffsetOnAxis(ap=idx_ap, axis=0)
                                      ).then_inc(self.dsem[q][i], 16)
    ev = (self.dsem[q][i], self.dcnt[q][i], key)
    _kbf_mark(self, ev, R, W, Wd)
    return ev


KBF.idma = _kbf_idma
KBF._deps = lambda self, e, R, W: _kbf_deps(self, e, R, W)
KBF.dma = _kbf_dma
KBF.op = _kbf_op


def phase_dsa1(kb, L, xT_all, xTs_all, M_loc, M_all, cst, act_split=True):
    nc = kb.nc
    NK = 16
    with kb.scope():
        qiT = kb.sb([64, 8, NK * 128], BF16, "qiT")
        kiT = kb.sb([64, SEQ], BF16, "kiT")
        wi = kb.sb([128, NK, 8], F32, "wi")
        absw = kb.sb([128, NK, 8], F32, "absw")
        sgn = kb.sb([128, NK, 8], F32, "sgn")
        cm = kb.sb([128, 512], F32, "cm")
        idb = kb.sb([128, 128], BF16, "idb")
        stp = kb.sb([128, NBIS], F32, "stp")
        kb.dma("sp", cm[:, :], cst["cmask"][:, :], R=[cst["cmask"]], W=[cm])
        kb.dma("sp", idb[:, :], cst["identb"][:, :], R=[cst["identb"]], W=[idb])
        kb.dma("sp", stp[:, :], cst["steps"][:, :], R=[cst["steps"]], W=[stp])
        idxs = kb.sb([128, 32], I32, "idxs")
        kb.dma("sp", idxs[:, :], cst["idx_s"][:, :], R=[cst["idx_s"]], W=[idxs])
        PS = [kb.ps([128, 512], F32, "PS") for _ in range(3)]
        PT = [kb.ps([128, 1024], BF16, "PT") for _ in range(2)]
        with kb.scope():
            NCOL = 584
            w = kb.sb([128, 8, NCOL], BF16, "wd1")
            load_w_cast(kb, w, L["wd1"], NCOL)
            xc = [kb.sb([128, 8, 512], BF16, "xc") for _ in range(2)]
            n_ev = 0
            for c in range(16):
                x_ = xc[c % 2]
                load_xT_chunk(kb, x_, xT_all, c)
                pp = PS[n_ev % 3]
                n_ev += 1
                for k8 in range(8):
                    kb.op("pe", lambda: nc.tensor.matmul(pp[:64, :], lhsT=w[:, k8, 512:576], rhs=x_[:, k8, :],
                                                         start=(k8 == 0), stop=(k8 == 7)), R=[w, x_], W=[pp])
                kb.op("dve", lambda: nc.vector.tensor_copy(out=kiT[:, c * 512:(c + 1) * 512], in_=pp[:64, :]),
                      R=[pp], W=[kiT])
            for r in range(4):
                x_ = xc[r % 2]
                for k8 in range(8):
                    kb.idma(x_[:, k8, :], xTs_all[:, :], idxs[:, r * 8 + k8:r * 8 + k8 + 1], R=[xTs_all, idxs],
                            Wd=[x_] if k8 else (), W=[x_] if k8 == 0 else ())
                for hi in range(8):
                    pp = PS[n_ev % 3]
                    n_ev += 1
                    for k8 in range(8):
                        kb.op("pe", lambda: nc.tensor.matmul(pp[:64, :], lhsT=w[:, k8, hi * 64:(hi + 1) * 64],
                                                             rhs=x_[:, k8, :], start=(k8 == 0), stop=(k8 == 7)),
                              R=[w, x_], W=[pp])
                    if hi % 2 == 0:
                        kb.op("dve", lambda: nc.vector.tensor_copy(out=qiT[:, hi, r * 512:(r + 1) * 512], in_=pp[:64, :]),
                              R=[pp], W=[qiT])
                    else:
                        kb.op("act", lambda: nc.scalar.copy(out=qiT[:, hi, r * 512:(r + 1) * 512], in_=pp[:64, :]),
                              R=[pp], W=[qiT])
                for t4 in range(4):
                    pp = PS[n_ev % 3]
                    n_ev += 1
                    for k8 in range(8):
                        kb.op("pe", lambda: nc.tensor.matmul(pp[:, :8], lhsT=x_[:, k8, t4 * 128:(t4 + 1) * 128],
                                                             rhs=w[:, k8, 576:584], start=(k8 == 0), stop=(k8 == 7)),
                              R=[w, x_], W=[pp])
                    kb.op("dve", lambda: nc.vector.tensor_copy(out=wi[:, r * 4 + t4, :], in_=pp[:, :8]), R=[pp], W=[wi])
        kb.op("act", lambda: nc.scalar.activation(out=absw[:, :, :], in_=wi[:, :, :], func=AF.Abs), R=[wi], W=[absw])
        kb.op("act", lambda: nc.scalar.activation(out=sgn[:, :, :], in_=wi[:, :, :], func=AF.Sign), R=[wi], W=[sgn])
        def write_mask(k, mt, Lk):
            dst = _AP(tensor=M_loc.t.tensor, offset=MOFF[k], ap=[[Lk, 128], [1, Lk]])
            kb.dma("sp", dst, mt[:, :Lk], R=[mt], Wd=[M_loc])
            for (k2, hf, lrow, nrow, grow) in MPARTS:
                if k2 == k:
                    kb.collective("AllGather", M_loc, M_all, M_loc[lrow:lrow + nrow, :],
                                  M_all[grow:grow + 4 * nrow, :])

        dsa1_body(kb, qiT, kiT, absw, sgn, cm, idb, stp, PS, PT, write_mask, act_split)


def phase_dsa2(kb, L, xT_all, M_all, oT_loc, cst):
    nc = kb.nc
    with kb.scope():
        identb = kb.sb([128, 128], BF16, "identb")
        kb.dma("sp", identb[:, :], cst["identb"][:, :], R=[cst["identb"]], W=[identb])
        rf = kb.sb([128, 2], F32, "rf")
        kb.dma("sp", rf[:, :], L["relfar"][:, :], R=[L["relfar"]], W=[rf])
        ax = AttnCtx(kb, n_s=4, n_o=2)
        E_d = L["Escr"]
        build_E_table(kb, ax, L["rel2"], cst["dsac"], E_d)
        TT = [kb.sb([128, 17 * 128], F32, "TT") for _ in range(2)]
        for h in range(2):
            toeplitz_load(kb, TT[h], E_d, h)
        q = kb.sb([128, SEQ], BF16, "q")
        k = kb.sb([128, SEQ], BF16, "k")
        V = kb.sb([128, NQB, 2, 65], BF16, "V")
        kb.op("pool", lambda: nc.gpsimd.memset(V[:, :, :, :], 1.0), W=[V])
        with kb.scope():
            NCOL = 384
            w = kb.sb([128, 8, NCOL], BF16, "wd2")
            load_w_cast(kb, w, L["wd2"], NCOL)
            xc = [kb.sb([128, 8, 512], BF16, "xc") for _ in range(2)]
            n_ev = 0
            for c in range(16):
                x_ = xc[c % 2]
                load_xT_chunk(kb, x_, xT_all, c)
                for bi, dst in enumerate((q, k)):
                    pp = ax.S[n_ev % 3]
                    for k8 in range(8):
                        kb.op("pe", lambda: nc.tensor.matmul(pp[:, :], lhsT=w[:, k8, bi * 128:(bi + 1) * 128],
                                                             rhs=x_[:, k8, :], start=(k8 == 0), stop=(k8 == 7)),
                              R=[w, x_], W=[pp])
                    d_ = dst[:, c * 512:(c + 1) * 512]
                    if n_ev % 2 == 0:
                        kb.op("dve", lambda: nc.vector.tensor_copy(out=d_, in_=pp[:, :]), R=[pp], W=[dst])
                    else:
                        kb.op("act", lambda: nc.scalar.copy(out=d_, in_=pp[:, :]), R=[pp], W=[dst])
                    n_ev += 1
                for t4 in range(4):
                    j = c * 4 + t4
                    pp = ax.S[n_ev % 3]
                    n_ev += 1
                    for k8 in range(8):
                        kb.op("pe", lambda: nc.tensor.matmul(pp[:, :128], lhsT=x_[:, k8, t4 * 128:(t4 + 1) * 128],
                                                             rhs=w[:, k8, 256:384], start=(k8 == 0), stop=(k8 == 7)),
                              R=[w, x_], W=[pp])
                    kb.op("dve" if t4 % 2 else "act",
                          (lambda: nc.vector.tensor_copy(out=V[:, j, :, 0:64],
                                                         in_=pp[:, 0:128].rearrange("p (h d) -> p h d", h=2)))
                          if t4 % 2 else
                          (lambda: nc.scalar.copy(out=V[:, j, :, 0:64],
                                                  in_=pp[:, 0:128].rearrange("p (h d) -> p h d", h=2))),
                          R=[pp], W=[V])
        Ms = [kb.sb([128, SEQ], BF16, "Ms") for _ in range(2)]
        ost = [kb.sb([128, 128], BF16, "ost") for _ in range(2)]
        out = OTOut(kb, identb, oT_loc, 128, "b")
        for i in range(NQB):
            ms = Ms[i % 2]
            W_ = (i + 1) * 128
            r_, k_ = i % 4, i // 4
            nh = 1 if k_ < 8 else 2
            for hf in range(nh):
                (_, _, lrow, nrow, grow) = MG[(k_, hf)]
                ns = 128 // nh
                src = _AP(tensor=M_all.t.tensor, offset=(grow + r_ * nrow) * 512, ap=[[LK[k_], ns], [1, W_]])
                kb.dma("sp", ms[hf * ns:(hf + 1) * ns, :W_], src, R=[M_all],
                       Wd=[ms] if hf else (), W=[ms] if hf == 0 else ())
            o = ost[i % 2]
            for h in range(2):
                def bias_of(j, h=h, i=i):
                    return (rf, rf[:, h:h + 1]) if i - j > 16 else None

                def mask_of(j, h=h, i=i, ms=ms):
                    sel = (ms, ms[:, j * 128:(j + 1) * 128])
                    if i - j > 16:
                        return [sel]
                    return [(TT[h], TT[h][:, (i - j) * 128:(i - j + 1) * 128]), sel]

                attn_qblock(ax, q, k, V, h, i, list(range(0, i + 1)), bias_of, mask_of, o, h * 64,
                            after=(None if h == 0 else (lambda i=i, o=o: out.put(i, o))))
        attn_flush(ax)


def phase_c(kb, L, moe, oT_all, x_src, x_dst, xT_loc, xTs_loc, cst, last):
    nc = kb.nc
    n_exp, nfu, n_units = (8, 14, 2) if moe else (1, 11, 2)
    TG = 512
    NG = TPC // TG
    with kb.scope():
        wf_d = L["wf"]
        ident = kb.sb([128, 128], F32, "ident")
        wo = kb.sb([128, 8, D], BF16, "wo")
        lnp = kb.sb([128, 4, D], F32, "lnp")
        kb.eps_col = kb.sb([128, 1], F32, "eps")
        kb.op("dve", lambda: nc.vector.memset(kb.eps_col[:, :], LN_EPS), W=[kb.eps_col])
        kb.dma("sp", ident[:, :], cst["ident"][:, :], R=[cst["ident"]], W=[ident])
        load_w_cast(kb, wo, L["wo"], D)
        kb.dma("sp", lnp[:, :, :], L["lnp"][:, :, :], R=[L["lnp"]], W=[lnp])
        idxo = kb.sb([128, 32], I32, "idxo")
        kb.dma("sp", idxo[:, :], cst["idx_o"][:, :], R=[cst["idx_o"]], W=[idxo])
        if moe:
            wr = kb.sb([128, 8, 8], F32, "wr")
            kb.dma("sp", wr[:, :, :], L["wr"][:, :, :], R=[L["wr"]], W=[wr])
            x1T32 = kb.sb([128, 8, 128], F32, "x1T32")
            comb = [kb.sb([128, 8], F32, "comb") for _ in range(4)]
            rt = kb.sb([128, 40], F32, "rt")
        oT = [kb.sb([128, 8, TG], BF16, "oT") for _ in range(1)]
        xt = [kb.sb([128, D], F32, "xt") for _ in range(2)]
        h = kb.sb([128, D], F32, "h")
        x1g = [kb.sb([128, D], F32, "x1g") for _ in range(4)]
        x1T = kb.sb([128, 8, TG], BF16, "x1T")
        aT = [kb.sb([128, TG], BF16, "aT") for _ in range(nfu)]
        w2 = [kb.sb([128, D], BF16, "w2") for _ in range(nfu)]
        w13 = [kb.sb([128, 2048], BF16, "w13") for _ in range(3)]
        yacc = [kb.sb([128, D], F32, "yacc") for _ in range(4)]
        sil = [kb.sb([128, TG], F32, "sil") for _ in range(2)]
        ost = [kb.sb([128, D], F32, "ost") for _ in range(2)]
        stg = kb.sb([128, 8, 512], BF16, "stgx")
        scr = (kb.sb([128, 2, 6], F32, "stats"), kb.sb([128, 2], F32, "mv"), kb.sb([128, 2], F32, "sd"))
        X = [kb.ps([128, 512], F32, "X") for _ in range(4)]
        Y = [kb.ps([128, 512], F32, "Y") for _ in range(2)]
        wcnt = 0
        for g in range(NG):
            og = oT[0]
            for k8 in range(8):
                kb.idma(og[:, k8, :], oT_all[:, :], idxo[:, g * 8 + k8:g * 8 + k8 + 1], R=[oT_all, idxo],
                        Wd=[og] if k8 else (), W=[og] if k8 == 0 else ())
            for tt in range(4):
                tok0 = g * TG + tt * 128
                xi = xt[tt % 2]
                kb.dma("sp", xi[:, :], x_src[tok0:tok0 + 128, :], R=[x_src], W=[xi])
                for hf in range(2):
                    for kc in range(8):
                        kb.op("pe", lambda: nc.tensor.matmul(Y[hf][:, :], lhsT=og[:, kc, tt * 128:(tt + 1) * 128],
                                                             rhs=wo[:, kc, hf * 512:(hf + 1) * 512],
                                                             start=(kc == 0), stop=(kc == 7)), R=[og, wo], W=[Y[hf]])
                    kb.op("dve", lambda: nc.vector.scalar_tensor_tensor(out=h[:, hf * 512:(hf + 1) * 512],
                                                                        in0=xi[:, hf * 512:(hf + 1) * 512], scalar=ALPHA,
                                                                        in1=Y[hf][:, :], op0=ALU.mult, op1=ALU.add),
                          R=[xi, Y[hf]], W=[h])
                x1 = x1g[tt]
                layer_norm(kb, h, (lnp, lnp[:, 0, :]), (lnp, lnp[:, 1, :]), x1[:, :], x1, scr)
                for kc in range(8):
                    pt = X[kc // 4]
                    kb.op("pe", lambda: nc.tensor.transpose(pt[:, (kc % 4) * 128:(kc % 4 + 1) * 128],
                                                            x1[:, kc * 128:(kc + 1) * 128], ident[:, :]),
                          R=[x1, ident], W=[pt])
                for hf in range(2):
                    src = X[hf][:, :].rearrange("p (k t) -> p k t", k=4)
                    dst = x1T[:, hf * 4:(hf + 1) * 4, tt * 128:(tt + 1) * 128]
                    if hf == 0:
                        kb.op("dve", lambda: nc.vector.tensor_copy(out=dst, in_=src), R=[X[hf]], W=[x1T])
                    else:
                        kb.op("act", lambda: nc.scalar.copy(out=dst, in_=src), R=[X[hf]], W=[x1T])
                    if moe:
                        kb.op("dve", lambda: nc.vector.tensor_copy(out=x1T32[:, hf * 4:(hf + 1) * 4, :], in_=src),
                              R=[X[hf]], W=[x1T32])
                if moe:
                    pr = X[2]
                    for kc in range(8):
                        kb.op("pe", lambda: nc.tensor.matmul(pr[:, :8], lhsT=x1T32[:, kc, :], rhs=wr[:, kc, :],
                                                             start=(kc == 0), stop=(kc == 7)), R=[x1T32, wr], W=[pr])
                    cb = comb[tt]
                    lg, mx, tmp, oh = rt[:, 0:8], rt[:, 8:16], rt[:, 16:24], rt[:, 24:32]
                    sc = rt[:, 32:40]
                    kb.op("dve", lambda: nc.vector.tensor_copy(out=lg, in_=pr[:, :8]), R=[pr], W=[rt])
                    kb.op("dve", lambda: nc.vector.max(out=mx, in_=lg), R=[rt], W=[rt])
                    kb.op("dve", lambda: nc.vector.tensor_tensor(out=sc[:, 0:1], in0=mx[:, 1:2], in1=mx[:, 0:1],
                                                                 op=ALU.subtract), R=[rt], W=[rt])
                    kb.op("act", lambda: nc.scalar.activation(out=sc[:, 1:2], in_=sc[:, 0:1], func=AF.Exp),
                          R=[rt], W=[rt])
                    kb.op("dve", lambda: nc.vector.tensor_scalar(out=sc[:, 2:3], in0=sc[:, 1:2], scalar1=1.0,
                                                                 scalar2=None, op0=ALU.add), R=[rt], W=[rt])
                    kb.op("dve", lambda: nc.vector.reciprocal(out=sc[:, 3:4], in_=sc[:, 2:3]), R=[rt], W=[rt])
                    kb.op("dve", lambda: nc.vector.tensor_tensor(out=sc[:, 4:5], in0=sc[:, 1:2], in1=sc[:, 3:4],
                                                                 op=ALU.mult), R=[rt], W=[rt])
                    kb.op("dve", lambda: nc.vector.tensor_scalar(out=tmp, in0=lg, scalar1=mx[:, 0:1],
                                                                 scalar2=sc[:, 3:4], op0=ALU.is_equal, op1=ALU.mult),
                          R=[rt], W=[rt])
                    kb.op("dve", lambda: nc.vector.tensor_scalar(out=oh, in0=lg, scalar1=mx[:, 1:2],
                                                                 scalar2=sc[:, 4:5], op0=ALU.is_equal, op1=ALU.mult),
                          R=[rt], W=[rt])
                    kb.op("dve", lambda: nc.vector.tensor_tensor(out=cb[:, :], in0=tmp, in1=oh, op=ALU.add),
                          R=[rt], W=[cb])
            first = True
            for e in range(n_exp):
                for u in range(n_units):
                    base = (e * n_units + u) * nfu
                    for f in range(nfu):
                        wc = w13[wcnt % 3]
                        kb.dma("pool", wc[:, :], wf_d[base + f, :, 0:2048], R=[wf_d], W=[wc])
                        kb.dma("pool", w2[f][:, :], wf_d[base + f, :, 2048:3072], R=[wf_d], W=[w2[f]])
                        h1, h3 = X[(wcnt % 2) * 2], X[(wcnt % 2) * 2 + 1]
                        for kc in range(8):
                            kb.op("pe", lambda: nc.tensor.matmul(h1[:, :], lhsT=wc[:, kc * 128:(kc + 1) * 128],
                                                                 rhs=x1T[:, kc, :], start=(kc == 0), stop=(kc == 7)),
                                  R=[wc, x1T], W=[h1])
                        for kc in range(8):
                            kb.op("pe", lambda: nc.tensor.matmul(h3[:, :],
                                                                 lhsT=wc[:, 1024 + kc * 128:1024 + (kc + 1) * 128],
                                                                 rhs=x1T[:, kc, :], start=(kc == 0), stop=(kc == 7)),
                                  R=[wc, x1T], W=[h3])
                        s = sil[wcnt % 2]
                        kb.op("act", lambda: nc.scalar.activation(out=s[:, :], in_=h1[:, :], func=AF.Silu),
                              R=[h1], W=[s])
                        kb.op("dve", lambda: nc.vector.tensor_tensor(out=aT[f][:, :], in0=s[:, :], in1=h3[:, :],
                                                                     op=ALU.mult), R=[s, h3], W=[aT[f]])
                        wcnt += 1
                    for tt in range(4):
                        for hf in range(2):
                            py = Y[hf]
                            for f in range(nfu):
                                kb.op("pe", lambda: nc.tensor.matmul(py[:, :], lhsT=aT[f][:, tt * 128:(tt + 1) * 128],
                                                                     rhs=w2[f][:, hf * 512:(hf + 1) * 512],
                                                                     start=(f == 0), stop=(f == nfu - 1)),
                                      R=[aT[f], w2[f]], W=[py])
                            ya = yacc[tt]
                            ysl = ya[:, hf * 512:(hf + 1) * 512]
                            if moe:
                                cs = comb[tt][:, e:e + 1]
                                if first:
                                    kb.op("dve", lambda: nc.vector.tensor_scalar(out=ysl, in0=py[:, :], scalar1=cs,
                                                                                 scalar2=None, op0=ALU.mult),
                                          R=[py, comb[tt]], W=[ya])
                                else:
                                    kb.op("dve", lambda: nc.vector.scalar_tensor_tensor(out=ysl, in0=py[:, :], scalar=cs,
                                                                                        in1=ysl, op0=ALU.mult,
                                                                                        op1=ALU.add),
                                          R=[py, comb[tt], ya], W=[ya])
                            else:
                                if first:
                                    kb.op("dve", lambda: nc.vector.tensor_copy(out=ysl, in_=py[:, :]), R=[py], W=[ya])
                                else:
                                    kb.op("dve", lambda: nc.vector.tensor_tensor(out=ysl, in0=py[:, :], in1=ysl,
                                                                                 op=ALU.add), R=[py, ya], W=[ya])
                    first = False
            for tt in range(4):
                tok0 = g * TG + tt * 128
                kb.op("dve", lambda: nc.vector.scalar_tensor_tensor(out=h[:, :], in0=x1g[tt][:, :], scalar=ALPHA,
                                                                    in1=yacc[tt][:, :], op0=ALU.mult, op1=ALU.add),
                      R=[x1g[tt], yacc[tt]], W=[h])
                o = ost[tt % 2]
                layer_norm(kb, h, (lnp, lnp[:, 2, :]), (lnp, lnp[:, 3, :]), o[:, :], o, scr)
                kb.dma("sp", x_dst[tok0:tok0 + 128, :], o[:, :], R=[o], Wd=[x_dst])
                if not last:
                    emit_xT(kb, X, ident, o, g * 4 + tt, stg, xT_loc, xTs_loc)


def _dbg_out(kb, x_d, out_d):
    for tt in range(4):
        kb.dma("sp", out_d[tt * 512:(tt + 1) * 512, :], x_d[tt * 512:(tt + 1) * 512, :], R=[x_d], Wd=[out_d])
    return kb.finish()


def build_fused(layers=(0, 1, 2, 3), upto=9):
    kb = KBF()
    nc = kb.nc
    x_d = kb.dram_in("x", [TPC, D], F32)
    out_d = kb.dram_out("out", [TPC, D], F32)
    cst = {"tri": kb.dram_in("tri", [128, 128], F32), "identb": kb.dram_in("identb", [128, 128], BF16),
           "ident": kb.dram_in("ident", [128, 128], F32), "dilc": kb.dram_in("dilc", [32, NDEL], F32),
           "dsac": kb.dram_in("dsac", [32, NDEL], F32), "cmask": kb.dram_in("cmask", [128, 512], F32),
           "steps": kb.dram_in("steps", [128, NBIS], F32), "idx_s": kb.dram_in("idx_s", [128, 32], I32),
           "idx_o": kb.dram_in("idx_o", [128, 32], I32)}
    Ls = {}
    for l in layers:
        L = {}
        pre = "L%d_" % l
        moe = (l % 2 == 1)
        if l % 2 == 0:
            L["wgl"] = kb.dram_in(pre + "wgl", [128, 8, 464], F32)
            L["wg"] = kb.dram_in(pre + "wg", [16, 64], F32)
            L["bg"] = kb.dram_in(pre + "bg", [128, 64], F32)
            L["gn"] = kb.dram_in(pre + "gn", [128, 128], F32)
            L["wd1"] = kb.dram_in(pre + "wd1", [128, 8, 584], F32)
            L["wd2"] = kb.dram_in(pre + "wd2", [128, 8, 384], F32)
            L["relfar"] = kb.dram_in(pre + "relfar", [128, 2], F32)
        else:
            L["wcd"] = kb.dram_in(pre + "wcd", [128, 8, 770], F32)
            L["bf"] = kb.dram_in(pre + "bf", [128, 2], F32)
            L["wr"] = kb.dram_in(pre + "wr", [128, 8, 8], F32)
        L["rel2"] = kb.dram_in(pre + "rel2", [32, 2], F32)
        L["wo"] = kb.dram_in(pre + "wo", [128, 8, D], F32)
        L["lnp"] = kb.dram_in(pre + "lnp", [128, 4, D], F32)
        L["wf"] = kb.dram_in(pre + "wf", [(224 if moe else 22) if upto >= 9 else 1, 128, 3072], F32)
        L["Escr"] = kb.dram_tmp(pre + "Escr", [2, NDEL], F32)
        Ls[l] = L
    xres = [kb.dram_tmp("xres0", [TPC, D], F32), kb.dram_tmp("xres1", [TPC, D], F32)]
    xT_loc = kb.dram_tmp("xT_loc", [D, TPC], BF16)
    xT_all = kb.dram_tmp("xT_all", [4 * D, TPC], BF16)
    xTs_loc = kb.dram_tmp("xTs_loc", [4 * D, 512], BF16)
    xTs_all = kb.dram_tmp("xTs_all", [16 * D, 512], BF16)
    oT_loc = kb.dram_tmp("oT_loc", [16 * 256, 512], BF16)
    oT_all = kb.dram_tmp("oT_all", [64 * 256, 512], BF16)
    M_loc = kb.dram_tmp("M_loc", [MTOT // 512, 512], BF16)
    M_all = kb.dram_tmp("M_all", [4 * MTOT // 512, 512], BF16)
    assert MPARTS[-1][4] + 4 * MPARTS[-1][3] == 4 * MTOT // 512
    with kb.scope():
        ident = kb.sb([128, 128], F32, "ident")
        kb.dma("sp", ident[:, :], cst["ident"][:, :], R=[cst["ident"]], W=[ident])
        xin = [kb.sb([128, D], F32, "xin") for _ in range(2)]
        stg = kb.sb([128, 8, 512], BF16, "stgx")
        X = [kb.ps([128, 512], F32, "X") for _ in range(2)]
        for tt in range(TPC // 128):
            xi = xin[tt % 2]
            kb.dma("sp", xi[:, :], x_d[tt * 128:(tt + 1) * 128, :], R=[x_d], W=[xi])
            emit_xT(kb, X, ident, xi, tt, stg, xT_loc, xTs_loc)
    if upto == 0:
        return _dbg_out(kb, x_d, out_d)
    x_src = x_d
    for n, l in enumerate(layers):
        L = Ls[l]
        last = (n == len(layers) - 1)
        x_dst = out_d if last else xres[n % 2]
        for cf in range(4):
            kb.collective("AllGather", xT_loc, xT_all, xT_loc[cf * 256:(cf + 1) * 256, :],
                          xT_all[cf * 1024:(cf + 1) * 1024, :])
        if upto == 1:
            return _dbg_out(kb, x_d, out_d)
        if l % 2 == 0:
            for j in range(4):
                kb.collective("AllGather", xTs_loc, xTs_all, xTs_loc[j * 1024:(j + 1) * 1024, :],
                              xTs_all[j * 4096:(j + 1) * 4096, :])
            phase_gla(kb, L, xT_all, oT_loc, cst)
            phase_dsa1(kb, L, xT_all, xTs_all, M_loc, M_all, cst)
            phase_dsa2(kb, L, xT_all, M_all, oT_loc, cst)
        else:
            phase_cd(kb, L, xT_all, oT_loc, cst)
        if upto == 2:
            return _dbg_out(kb, x_d, out_d)
        for j in range(4):
            kb.collective("AllGather", oT_loc, oT_all, oT_loc[j * 1024:(j + 1) * 1024, :],
                          oT_all[j * 4096:(j + 1) * 4096, :])
        if upto == 3:
            return _dbg_out(kb, x_d, out_d)
        phase_c(kb, L, l % 2 == 1, oT_all, x_src, x_dst, xT_loc, xTs_loc, cst, last)
        x_src = x_dst
    return kb.finish()


def fused_inputs(x, ln_g, ln_b, rel_table, w_in_ab, w_gate_a, b_gate_a, g_norm_a, w_out_ab,
                 w_in_cd, b_forget, w_out_cd, w1_dense, w3_dense, w2_dense,
                 w_router, w1_moe, w3_moe, w2_moe, layers=(0, 1, 2, 3)):
    f32 = lambda a: np.ascontiguousarray(np.asarray(a, dtype=np.float32))
    bc = lambda v, n=128: np.ascontiguousarray(np.broadcast_to(np.asarray(v, np.float32)[None, :], (n, len(v))))
    xf = f32(x).reshape(BATCH * SEQ, D)
    rel_table = f32(rel_table)
    steps = bc(0.5 ** np.arange(1, NBIS + 1))
    perm = np.zeros(D, np.int64)
    for src in range(4):
        for rr in range(256):
            perm[src * 256 + rr] = src * 128 + rr if rr < 128 else 512 + src * 128 + (rr - 128)
    shared = {"tri": TRI, "identb": IDENT.astype(NPBF), "ident": IDENT, "dilc": dil_const(), "dsac": dsa_const(),
              "steps": steps}
    per_layer_shared = {}
    for l in layers:
        j = l // 2
        pre = "L%d_" % l
        d = {}
        d[pre + "lnp"] = np.ascontiguousarray(np.broadcast_to(
            np.stack([ln_g[l, 0], ln_b[l, 0], ln_g[l, 1], ln_b[l, 1]]).astype(np.float32)[None], (128, 4, D)))
        if l % 2 == 0:
            d[pre + "wo"] = w_kc_layout(f32(w_out_ab[j])[perm])
            d[pre + "wf"] = ffn_chunk_layout(f32(w1_dense[j]), f32(w3_dense[j]), f32(w2_dense[j]))
            d[pre + "gn"] = bc(g_norm_a[j])
        else:
            d[pre + "wo"] = w_kc_layout(f32(w_out_cd[j])[perm])
            d[pre + "wf"] = np.concatenate([ffn_chunk_layout(f32(w1_moe[j, e]), f32(w3_moe[j, e]), f32(w2_moe[j, e]))
                                            for e in range(8)], axis=0)
            d[pre + "wr"] = np.ascontiguousarray(f32(w_router[j]).reshape(8, 128, 8).transpose(1, 0, 2))
        per_layer_shared.update(d)
    maps = []
    for c in range(NCORES):
        b, m = c // 4, c % 4
        mp = dict(shared)
        mp.update(per_layer_shared)
        mp["x"] = np.ascontiguousarray(xf[c * TPC:(c + 1) * TPC])
        pp_ = np.arange(128)[:, None]
        rr_, k8_ = np.arange(4)[None, :, None], np.arange(8)[None, None, :]
        mp["idx_s"] = np.ascontiguousarray(((m * 4 + rr_) * 1024 + k8_ * 128 + pp_[:, :, None]).reshape(128, 32)
                                           .astype(np.int32))
        G_ = k8_ * 128 + pp_[:, :, None]
        mp["idx_o"] = np.ascontiguousarray(((m * 4 + G_ // 256) * 1024 + rr_ * 256 + G_ % 256).reshape(128, 32)
                                           .astype(np.int32))
        mp["cmask"] = np.where(np.arange(512)[None, :] <= (128 * m + np.arange(128))[:, None], 0.0, NEG).astype(np.float32)
        for l in layers:
            j = l // 2
            pre = "L%d_" % l
            mp[pre + "rel2"] = np.ascontiguousarray(rel_table[:, 2 * m:2 * m + 2])
            if l % 2 == 0:
                w = f32(w_in_ab[j])
                cs = lambda a, n: w[:, a:a + n]
                wgl = np.concatenate([cs(m * 64, 64), cs(256 + m * 64, 64), cs(1536, 16), cs(256 + m * 64, 64),
                                      cs(512 + m * 128, 128), cs(1024 + m * 128, 128)], axis=1)
                wd1 = np.concatenate([cs(3088, 512), cs(3600, 64), cs(3664, 8)], axis=1)
                wd2 = np.concatenate([cs(1552 + m * 128, 128), cs(2064 + m * 128, 128), cs(2576 + m * 128, 128)], axis=1)
                mp[pre + "wgl"] = w_kc_layout(wgl)
                mp[pre + "wd1"] = w_kc_layout(wd1)
                mp[pre + "wd2"] = w_kc_layout(wd2)
                mp[pre + "wg"] = np.ascontiguousarray(f32(w_gate_a[j])[:, m * 64:(m + 1) * 64])
                mp[pre + "bg"] = bc(f32(b_gate_a[j])[m * 64:(m + 1) * 64])
                mp[pre + "relfar"] = bc(rel_table[31, 2 * m:2 * m + 2])
            else:
                w = f32(w_in_cd[j])
                cs = lambda a, n: w[:, a:a + n]
                wcd = np.concatenate([cs(m * 128, 128), cs(512 + m * 128, 128), cs(1544 + m * 128, 128),
                                      cs(2056 + m * 128, 128), cs(1024 + m * 128, 128), cs(2568 + m * 128, 128),
                                      cs(1536 + 2 * m, 2)], axis=1)
                mp[pre + "wcd"] = w_kc_layout(wcd)
                mp[pre + "bf"] = bc(f32(b_forget[j])[2 * m:2 * m + 2])
        maps.append(mp)
    return maps


def kernel_fused(**inputs):
    nc = build_fused()
    maps = fused_inputs(**inputs)
    res = run_spmd(nc, maps)
    out = np.concatenate([res[c]["out"] for c in range(NCORES)], axis=0)
    return out.reshape(BATCH, SEQ, D).astype(np.float32)


def kernel(**inputs):
    return kernel_fused(**inputs)
```

```python
import math
from contextlib import ExitStack
import numpy as np
import ml_dtypes
import concourse.bass as bass
import concourse.mybir as mybir
from concourse.bass_utils import run_bass_kernel_spmd

F32 = mybir.dt.float32
BF16 = mybir.dt.bfloat16
AF = mybir.ActivationFunctionType
ALU = mybir.AluOpType
AX = mybir.AxisListType
NPBF = ml_dtypes.bfloat16

NCORES = 8
D = 1024
SEQ = 8192
BATCH = 2
DEPTH = 4
ALPHA = (2 * DEPTH) ** 0.25
LN_EPS = 1e-5
NEG = -1.0e30


class Buf:
    def __init__(self, t):
        self.t = t
        self.w = None
        self.r = []

    def __getitem__(self, idx):
        return self.t[idx]


class KB:
    NDS = 6

    def __init__(self):
        self.nc = bass.Bass("TRN2", target_bir_lowering=False)
        nc = self.nc
        self.es = ExitStack()
        self.eng = {"pe": nc.tensor, "dve": nc.vector, "act": nc.scalar, "pool": nc.gpsimd, "sp": nc.sync}
        self.esem = {}
        self.ecnt = {}
        self.seen = {e: {} for e in self.eng}
        for e in self.eng:
            self.esem[e] = self.es.enter_context(nc.semaphore("sem_" + e))
            self.ecnt[e] = 0
        self.dsem = {}
        self.dcnt = {}
        self.dnext = {}
        for q in ("sp", "act", "pool"):
            self.dsem[q] = [self.es.enter_context(nc.semaphore("dsem_%s%d" % (q, i))) for i in range(self.NDS)]
            self.dcnt[q] = [0] * self.NDS
            self.dnext[q] = 0
        self.n_names = 0
        self.outs = []

    def _nm(self, p):
        self.n_names += 1
        return "%s_%d" % (p, self.n_names)

    def dram_in(self, name, shape, dt):
        return Buf(self.nc.dram_tensor(name, list(shape), dt, kind="ExternalInput").ap())

    def dram_out(self, name, shape, dt):
        b = Buf(self.nc.dram_tensor(name, list(shape), dt, kind="ExternalOutput").ap())
        self.outs.append(b)
        return b

    def dram_tmp(self, name, shape, dt):
        return Buf(self.nc.dram_tensor(name, list(shape), dt, kind="Internal").ap())

    def sb(self, shape, dt, name="sb"):
        return Buf(self.es.enter_context(self.nc.sbuf_tensor(self._nm(name), list(shape), dt)))

    def ps(self, shape, dt=F32, name="ps"):
        return Buf(self.es.enter_context(self.nc.psum_tensor(self._nm(name), list(shape), dt)))

    def _wait(self, e, ev):
        if ev is None:
            return
        sem, val, key = ev
        if self.seen[e].get(key, 0) >= val:
            return
        self.eng[e].wait_ge(sem, val)
        self.seen[e][key] = val

    def _deps(self, e, R, W):
        evs = []
        for b in R:
            if b.w is not None:
                evs.append(b.w)
        for b in W:
            if b.w is not None:
                evs.append(b.w)
            evs.extend(b.r)
        best = {}
        for ev in evs:
            k = ev[2]
            if e == "pe" and k == "pe":
                continue
            if k not in best or best[k][1] < ev[1]:
                best[k] = ev
        for ev in best.values():
            self._wait(e, ev)

    def _mark(self, ev, R, W):
        for b in R:
            b.r.append(ev)
            if len(b.r) > 24:
                best = {}
                for x in b.r:
                    if x[2] not in best or best[x[2]][1] < x[1]:
                        best[x[2]] = x
                b.r = list(best.values())
        for b in W:
            b.w = ev
            b.r = []

    def op(self, e, fn, R=(), W=()):
        self._deps(e, R, W)
        ins = fn()
        self.ecnt[e] += 1
        ins.then_inc(self.esem[e], 1)
        ev = (self.esem[e], self.ecnt[e], e)
        self.seen[e][e] = max(self.seen[e].get(e, 0), 0)
        self._mark(ev, R, W)
        return ev

    def dma(self, q, out, in_, R=(), W=(), **kw):
        self._deps(q, R, W)
        i = self.dnext[q]
        self.dnext[q] = (i + 1) % self.NDS
        key = "d%s%d" % (q, i)
        if self.dcnt[q][i] > 0:
            self._wait(q, (self.dsem[q][i], self.dcnt[q][i], key))
        self.dcnt[q][i] += 16
        self.eng[q].dma_start(out=out, in_=in_, **kw).then_inc(self.dsem[q][i], 16)
        ev = (self.dsem[q][i], self.dcnt[q][i], key)
        self._mark(ev, R, W)
        return ev

    def finish(self):
        for q in ("sp", "act", "pool"):
            for i in range(self.NDS):
                if self.dcnt[q][i] > 0:
                    self._wait("sp", (self.dsem[q][i], self.dcnt[q][i], "d%s%d" % (q, i)))
        for e in ("pe", "dve", "act", "pool"):
            if self.ecnt[e] > 0:
                self._wait("sp", (self.esem[e], self.ecnt[e], e))
        self.es.close()
        return self.nc


def run_spmd(nc, in_maps):
    res = run_bass_kernel_spmd(nc, in_maps, core_ids=list(range(NCORES)))
    return res.results


def build_cast(n):
    kb = KB()
    nc = kb.nc
    CH = 2048
    src = kb.dram_in("src", [128, n], F32)
    dst = kb.dram_out("dst", [128, n], BF16)
    NB = 3
    tin = [kb.sb([128, CH], F32, "tin") for _ in range(NB)]
    tout = [kb.sb([128, CH], BF16, "tout") for _ in range(NB)]
    nch = (n + CH - 1) // CH
    for c in range(nch):
        c0 = c * CH
        w = min(CH, n - c0)
        a, b = tin[c % NB], tout[c % NB]
        kb.dma("sp", a[:, :w], src[:, c0:c0 + w], R=[src], W=[a])
        if c % 2 == 0:
            kb.op("dve", lambda: nc.vector.tensor_copy(out=b[:, :w], in_=a[:, :w]), R=[a], W=[b])
        else:
            kb.op("act", lambda: nc.scalar.copy(out=b[:, :w], in_=a[:, :w]), R=[a], W=[b])
        kb.dma("pool", dst[:, c0:c0 + w], b[:, :w], R=[b], W=[dst])
    return kb.finish()


def cast_weights(arrs):
    flats = [np.ascontiguousarray(a).reshape(NCORES, 128, -1) for a in arrs]
    ns = [f.shape[2] for f in flats]
    cat = np.concatenate(flats, axis=2)
    n = cat.shape[2]
    nc = build_cast(n)
    res = run_spmd(nc, [{"src": np.ascontiguousarray(cat[c])} for c in range(NCORES)])
    out = np.stack([res[c]["dst"] for c in range(NCORES)], axis=0)
    outs = []
    o = 0
    for a, k in zip(arrs, ns):
        outs.append(out[:, :, o:o + k].reshape(a.shape))
        o += k
    return outs


TPC = 2048


def build_A(W, fm, tmb, tmf, gate=None):
    kb = KB()
    nc = kb.nc
    NT = TPC // 128
    n_fm = sum(n for _, n in fm)
    n_tmb = sum(n for _, n in tmb)
    n_tmf = sum(n for _, n in tmf) + (256 if gate is not None else 0)
    x = kb.dram_in("x", [TPC, D], F32)
    w = kb.dram_in("w", [128, 8, W], BF16)
    ident_d = kb.dram_in("ident", [128, 128], F32)
    yT = kb.dram_out("yT", [n_fm, TPC], BF16)
    ytb = kb.dram_out("ytb", [TPC, max(n_tmb, 1)], BF16)
    ytf = kb.dram_out("ytf", [TPC, max(n_tmf, 1)], F32)
    wsb = kb.sb([128, 8, W], BF16, "w")
    ident = kb.sb([128, 128], F32, "ident")
    xT = kb.sb([128, 8, TPC], BF16, "xT")
    kb.dma("sp", ident[:, :], ident_d[:, :], R=[ident_d], W=[ident])
    for kc in range(8):
        kb.dma("pool" if kc % 2 else "sp", wsb[:, kc, :], w[:, kc, :], R=[w], W=[wsb])
    if gate is not None:
        wg_d = kb.dram_in("wg", [16, 256], F32)
        bg_d = kb.dram_in("bg", [128, 256], F32)
        wg = kb.sb([16, 256], F32, "wg")
        bg = kb.sb([128, 256], F32, "bg")
        gaT = kb.sb([16, TPC], F32, "gaT")
        w32 = kb.sb([128, 8, 16], F32, "w32")
        kb.dma("sp", wg[:, :], wg_d[:, :], R=[wg_d], W=[wg])
        kb.dma("sp", bg[:, :], bg_d[:, :], R=[bg_d], W=[bg])
    xin = [kb.sb([128, D], F32, "xin") for _ in range(2)]
    pst = [kb.ps([128, 1024], F32, "pst")]
    for tt in range(NT):
        xi = xin[tt % 2]
        kb.dma("sp", xi[:, :], x[tt * 128:(tt + 1) * 128, :], R=[x], W=[xi])
        pt = pst[0]
        for kc in range(8):
            kb.op("pe", lambda: nc.tensor.transpose(pt[:, kc * 128:(kc + 1) * 128], xi[:, kc * 128:(kc + 1) * 128],
                                                    ident[:, :]), R=[xi, ident], W=[pt])
        for hf in range(2):
            src = pt[:, hf * 512:(hf + 1) * 512].rearrange("p (k t) -> p k t", k=4)
            dst = xT[:, hf * 4:(hf + 1) * 4, tt * 128:(tt + 1) * 128]
            if hf == 0:
                kb.op("dve", lambda: nc.vector.tensor_copy(out=dst, in_=src), R=[pt], W=[xT])
            else:
                kb.op("act", lambda: nc.scalar.copy(out=dst, in_=src), R=[pt], W=[xT])
    psf = [kb.ps([128, 512], F32, "psf") for _ in range(2)]
    stf = [kb.sb([128, 512], BF16, "stf") for _ in range(3)]
    cnt = 0
    row = 0
    blocks = list(fm)
    for (c0, ncol) in blocks:
        for tg in range(TPC // 512):
            pp = psf[cnt % 2]
            st = stf[cnt % 3]
            for kc in range(8):
                kb.op("pe", lambda: nc.tensor.matmul(pp[:ncol, :], lhsT=wsb[:, kc, c0:c0 + ncol],
                                                     rhs=xT[:, kc, tg * 512:(tg + 1) * 512],
                                                     start=(kc == 0), stop=(kc == 7)), R=[wsb, xT], W=[pp])
            if cnt % 2 == 0:
                kb.op("dve", lambda: nc.vector.tensor_copy(out=st[:ncol, :], in_=pp[:ncol, :]), R=[pp], W=[st])
            else:
                kb.op("act", lambda: nc.scalar.copy(out=st[:ncol, :], in_=pp[:ncol, :]), R=[pp], W=[st])
            kb.dma("pool" if cnt % 2 else "sp", yT[row:row + ncol, tg * 512:(tg + 1) * 512], st[:ncol, :],
                   R=[st], W=[yT])
            cnt += 1
        row += ncol
    if gate is not None:
        g0 = gate[0]
        for tg in range(TPC // 512):
            pp = psf[cnt % 2]
            for kc in range(8):
                kb.op("pe", lambda: nc.tensor.matmul(pp[:16, :], lhsT=wsb[:, kc, g0:g0 + 16],
                                                     rhs=xT[:, kc, tg * 512:(tg + 1) * 512],
                                                     start=(kc == 0), stop=(kc == 7)), R=[wsb, xT], W=[pp])
            kb.op("dve", lambda: nc.vector.tensor_copy(out=gaT[:, tg * 512:(tg + 1) * 512], in_=pp[:16, :]),
                  R=[pp], W=[gaT])
            cnt += 1
    pstm = [kb.ps([128, 512], F32, "pstm") for _ in range(2)]
    stb = [kb.sb([128, 512], BF16, "stb") for _ in range(3)]
    st32 = [kb.sb([128, 512], F32, "st32") for _ in range(3)]
    cnt = 0
    for tt in range(NT):
        for kind, lst, dst_d in (("b", tmb, ytb), ("f", tmf, ytf)):
            off = 0
            for (c0, ncol) in lst:
                pp = pstm[cnt % 2]
                st = (stb if kind == "b" else st32)[cnt % 3]
                for kc in range(8):
                    kb.op("pe", lambda: nc.tensor.matmul(pp[:, :ncol], lhsT=xT[:, kc, tt * 128:(tt + 1) * 128],
                                                         rhs=wsb[:, kc, c0:c0 + ncol],
                                                         start=(kc == 0), stop=(kc == 7)), R=[wsb, xT], W=[pp])
                if cnt % 2 == 0:
                    kb.op("dve", lambda: nc.vector.tensor_copy(out=st[:, :ncol], in_=pp[:, :ncol]), R=[pp], W=[st])
                else:
                    kb.op("act", lambda: nc.scalar.copy(out=st[:, :ncol], in_=pp[:, :ncol]), R=[pp], W=[st])
                kb.dma("pool" if cnt % 2 else "sp", dst_d[tt * 128:(tt + 1) * 128, off:off + ncol], st[:, :ncol],
                       R=[st], W=[dst_d])
                off += ncol
                cnt += 1
        if gate is not None:
            off = sum(n for _, n in tmf)
            pp = pstm[cnt % 2]
            st = st32[cnt % 3]
            kb.op("pe", lambda: nc.tensor.matmul(pp[:, :256], lhsT=gaT[:, tt * 128:(tt + 1) * 128], rhs=wg[:, :],
                                                 start=True, stop=True), R=[gaT, wg], W=[pp])
            kb.op("dve", lambda: nc.vector.tensor_tensor(out=st[:, :256], in0=pp[:, :256], in1=bg[:, :], op=ALU.add),
                  R=[pp, bg], W=[st])
            kb.op("act", lambda: nc.scalar.activation(out=st[:, :256], in_=st[:, :256], func=AF.Exp, scale=-1.0),
                  R=[st], W=[st])
            kb.op("act", lambda: nc.scalar.activation(out=st[:, :256], in_=st[:, :256], func=AF.Ln, bias=1.0),
                  R=[st], W=[st])
            kb.op("dve", lambda: nc.vector.tensor_scalar(out=st[:, :256], in0=st[:, :256], scalar1=-1.0 / 16.0,
                                                         scalar2=None, op0=ALU.mult), R=[st], W=[st])
            kb.dma("sp", ytf[tt * 128:(tt + 1) * 128, off:off + 256], st[:, :256], R=[st], W=[ytf])
            cnt += 1
    return kb.finish()


def blocks_of(c0, n, bs):
    out = []
    while n > 0:
        k = min(bs, n)
        out.append((c0, k))
        c0 += k
        n -= k
    return out


def w_kc_layout(w):
    W = w.shape[1]
    return np.ascontiguousarray(w.reshape(8, 128, W).transpose(1, 0, 2))


IDENT = np.eye(128, dtype=np.float32)


def run_A(x_flat, w_bf, fm_ranges, tmb_ranges, tmf_ranges, gate=None, wg=None, bg=None):
    W = w_bf.shape[1]
    fm = [b for (c0, n) in fm_ranges for b in blocks_of(c0, n, 128)]
    tmb = [b for (c0, n) in tmb_ranges for b in blocks_of(c0, n, 512)]
    tmf = [b for (c0, n) in tmf_ranges for b in blocks_of(c0, n, 512)]
    nc = build_A(W, fm, tmb, tmf, gate)
    wl = w_kc_layout(w_bf)
    maps = []
    for c in range(NCORES):
        m = {"x": np.ascontiguousarray(x_flat[c * TPC:(c + 1) * TPC]), "w": wl, "ident": IDENT}
        if gate is not None:
            m["wg"] = np.ascontiguousarray(wg)
            m["bg"] = np.ascontiguousarray(np.broadcast_to(bg[None, :], (128, 256)))
        maps.append(m)
    res = run_spmd(nc, maps)
    yT = np.concatenate([res[c]["yT"] for c in range(NCORES)], axis=1)
    ytb = np.concatenate([res[c]["ytb"] for c in range(NCORES)], axis=0)
    ytf = np.concatenate([res[c]["ytf"] for c in range(NCORES)], axis=0)
    return yT, ytb, ytf


def layer_norm(kb, h, gt, bt, out_ap, out_buf, scr):
    nc = kb.nc
    stats, mv, sd = scr
    for c in range(2):
        kb.op("dve", lambda: nc.vector.bn_stats(out=stats[:, c, :], in_=h[:, c * 512:(c + 1) * 512]), R=[h], W=[stats])
    kb.op("dve", lambda: nc.vector.bn_aggr(out=mv[:, :], in_=stats[:, :, :].rearrange("p a b -> p (a b)")),
          R=[stats], W=[mv])
    kb.op("act", lambda: nc.scalar.activation(out=sd[:, 0:1], in_=mv[:, 1:2], func=AF.Sqrt, bias=kb.eps_col[:, 0:1]),
          R=[mv, kb.eps_col], W=[sd])
    kb.op("dve", lambda: nc.vector.reciprocal(out=sd[:, 1:2], in_=sd[:, 0:1]), R=[sd], W=[sd])
    kb.op("dve", lambda: nc.vector.tensor_scalar(out=h[:, :], in0=h[:, :], scalar1=mv[:, 0:1], scalar2=sd[:, 1:2],
                                                 op0=ALU.subtract, op1=ALU.mult), R=[h, mv, sd], W=[h])
    kb.op("pool", lambda: nc.gpsimd.tensor_tensor(out=h[:, :], in0=h[:, :], in1=gt[1], op=ALU.mult),
          R=[h, gt[0]], W=[h])
    kb.op("dve", lambda: nc.vector.tensor_tensor(out=out_ap, in0=h[:, :], in1=bt[1], op=ALU.add),
          R=[h, bt[0]], W=[out_buf])


def build_C(n_exp, nf_unit, n_units_per_exp):
    kb = KB()
    nc = kb.nc
    moe = n_exp > 1
    NT = TPC // 128
    TG = 512
    NG = TPC // TG
    nfu = nf_unit
    n_chunks = n_exp * n_units_per_exp * nfu
    oT_d = kb.dram_in("oT", [128, 8, TPC], BF16)
    x_d = kb.dram_in("x", [TPC, D], F32)
    wo_d = kb.dram_in("wo", [128, 8, D], BF16)
    lnp_d = kb.dram_in("lnp", [128, 4, D], F32)
    wf_d = kb.dram_in("wf", [n_chunks, 128, 3072], BF16)
    ident_d = kb.dram_in("ident", [128, 128], F32)
    out_d = kb.dram_out("out", [TPC, D], F32)
    ident = kb.sb([128, 128], F32, "ident")
    wo = kb.sb([128, 8, D], BF16, "wo")
    lnp = kb.sb([128, 4, D], F32, "lnp")
    kb.eps_col = kb.sb([128, 1], F32, "eps")
    kb.op("dve", lambda: nc.vector.memset(kb.eps_col[:, :], LN_EPS), W=[kb.eps_col])
    kb.dma("sp", ident[:, :], ident_d[:, :], R=[ident_d], W=[ident])
    kb.dma("sp", wo[:, :, :], wo_d[:, :, :], R=[wo_d], W=[wo])
    kb.dma("pool", lnp[:, :, :], lnp_d[:, :, :], R=[lnp_d], W=[lnp])
    if moe:
        wr_d = kb.dram_in("wr", [128, 8, 8], F32)
        wr = kb.sb([128, 8, 8], F32, "wr")
        kb.dma("sp", wr[:, :, :], wr_d[:, :, :], R=[wr_d], W=[wr])
        x1T32 = kb.sb([128, 8, 128], F32, "x1T32")
        comb = [kb.sb([128, 8], F32, "comb") for _ in range(4)]
        rt = kb.sb([128, 40], F32, "rt")
    oT = [kb.sb([128, 8, TG], BF16, "oT") for _ in range(2)]
    xt = [kb.sb([128, D], F32, "xt") for _ in range(2)]
    h = kb.sb([128, D], F32, "h")
    x1g = [kb.sb([128, D], F32, "x1g") for _ in range(4)]
    x1T = kb.sb([128, 8, TG], BF16, "x1T")
    aT = [kb.sb([128, TG], BF16, "aT") for _ in range(nfu)]
    w2 = [kb.sb([128, D], BF16, "w2") for _ in range(nfu)]
    w13 = [kb.sb([128, 2048], BF16, "w13") for _ in range(3)]
    yacc = [kb.sb([128, D], F32, "yacc") for _ in range(4)]
    sil = [kb.sb([128, TG], F32, "sil") for _ in range(2)]
    ost = [kb.sb([128, D], F32, "ost") for _ in range(2)]
    scr = (kb.sb([128, 2, 6], F32, "stats"), kb.sb([128, 2], F32, "mv"), kb.sb([128, 2], F32, "sd"))
    X = [kb.ps([128, 512], F32, "X") for _ in range(4)]
    Y = [kb.ps([128, 512], F32, "Y") for _ in range(2)]
    wcnt = 0
    for g in range(NG):
        og = oT[g % 2]
        kb.dma("pool", og[:, :, :], oT_d[:, :, g * TG:(g + 1) * TG], R=[oT_d], W=[og])
        for tt in range(4):
            tok0 = g * TG + tt * 128
            xi = xt[tt % 2]
            kb.dma("sp", xi[:, :], x_d[tok0:tok0 + 128, :], R=[x_d], W=[xi])
            for hf in range(2):
                for kc in range(8):
                    kb.op("pe", lambda: nc.tensor.matmul(Y[hf][:, :], lhsT=og[:, kc, tt * 128:(tt + 1) * 128],
                                                         rhs=wo[:, kc, hf * 512:(hf + 1) * 512],
                                                         start=(kc == 0), stop=(kc == 7)), R=[og, wo], W=[Y[hf]])
                kb.op("dve", lambda: nc.vector.scalar_tensor_tensor(out=h[:, hf * 512:(hf + 1) * 512],
                                                                    in0=xi[:, hf * 512:(hf + 1) * 512], scalar=ALPHA,
                                                                    in1=Y[hf][:, :], op0=ALU.mult, op1=ALU.add),
                      R=[xi, Y[hf]], W=[h])
            x1 = x1g[tt]
            layer_norm(kb, h, (lnp, lnp[:, 0, :]), (lnp, lnp[:, 1, :]), x1[:, :], x1, scr)
            for kc in range(8):
                pt = X[kc // 4]
                kb.op("pe", lambda: nc.tensor.transpose(pt[:, (kc % 4) * 128:(kc % 4 + 1) * 128],
                                                        x1[:, kc * 128:(kc + 1) * 128], ident[:, :]),
                      R=[x1, ident], W=[pt])
            for hf in range(2):
                src = X[hf][:, :].rearrange("p (k t) -> p k t", k=4)
                dst = x1T[:, hf * 4:(hf + 1) * 4, tt * 128:(tt + 1) * 128]
                if hf == 0:
                    kb.op("dve", lambda: nc.vector.tensor_copy(out=dst, in_=src), R=[X[hf]], W=[x1T])
                else:
                    kb.op("act", lambda: nc.scalar.copy(out=dst, in_=src), R=[X[hf]], W=[x1T])
                if moe:
                    kb.op("pool" if False else "dve",
                          lambda: nc.vector.tensor_copy(out=x1T32[:, hf * 4:(hf + 1) * 4, :], in_=src),
                          R=[X[hf]], W=[x1T32])
            if moe:
                pr = X[2]
                for kc in range(8):
                    kb.op("pe", lambda: nc.tensor.matmul(pr[:, :8], lhsT=x1T32[:, kc, :], rhs=wr[:, kc, :],
                                                         start=(kc == 0), stop=(kc == 7)), R=[x1T32, wr], W=[pr])
                cb = comb[tt]
                lg, mx, tmp, oh = rt[:, 0:8], rt[:, 8:16], rt[:, 16:24], rt[:, 24:32]
                sc = rt[:, 32:40]
                kb.op("dve", lambda: nc.vector.tensor_copy(out=lg, in_=pr[:, :8]), R=[pr], W=[rt])
                kb.op("dve", lambda: nc.vector.max(out=mx, in_=lg), R=[rt], W=[rt])
                kb.op("dve", lambda: nc.vector.tensor_tensor(out=sc[:, 0:1], in0=mx[:, 1:2], in1=mx[:, 0:1],
                                                             op=ALU.subtract), R=[rt], W=[rt])
                kb.op("act", lambda: nc.scalar.activation(out=sc[:, 1:2], in_=sc[:, 0:1], func=AF.Exp), R=[rt], W=[rt])
                kb.op("dve", lambda: nc.vector.tensor_scalar(out=sc[:, 2:3], in0=sc[:, 1:2], scalar1=1.0, scalar2=None,
                                                             op0=ALU.add), R=[rt], W=[rt])
                kb.op("dve", lambda: nc.vector.reciprocal(out=sc[:, 3:4], in_=sc[:, 2:3]), R=[rt], W=[rt])
                kb.op("dve", lambda: nc.vector.tensor_tensor(out=sc[:, 4:5], in0=sc[:, 1:2], in1=sc[:, 3:4],
                                                             op=ALU.mult), R=[rt], W=[rt])
                kb.op("dve", lambda: nc.vector.tensor_scalar(out=tmp, in0=lg, scalar1=mx[:, 0:1], scalar2=sc[:, 3:4],
                                                             op0=ALU.is_equal, op1=ALU.mult), R=[rt], W=[rt])
                kb.op("dve", lambda: nc.vector.tensor_scalar(out=oh, in0=lg, scalar1=mx[:, 1:2], scalar2=sc[:, 4:5],
                                                             op0=ALU.is_equal, op1=ALU.mult), R=[rt], W=[rt])
                kb.op("dve", lambda: nc.vector.tensor_tensor(out=cb[:, :], in0=tmp, in1=oh, op=ALU.add),
                      R=[rt], W=[cb])
        first = True
        for e in range(n_exp):
            for u in range(n_units_per_exp):
                base = (e * n_units_per_exp + u) * nfu
                for f in range(nfu):
                    wc = w13[wcnt % 3]
                    q = "sp" if wcnt % 2 == 0 else "pool"
                    kb.dma(q, wc[:, :], wf_d[base + f, :, 0:2048], R=[wf_d], W=[wc])
                    kb.dma("pool" if wcnt % 2 == 0 else "sp", w2[f][:, :], wf_d[base + f, :, 2048:3072],
                           R=[wf_d], W=[w2[f]])
                    h1, h3 = X[(wcnt % 2) * 2], X[(wcnt % 2) * 2 + 1]
                    for kc in range(8):
                        kb.op("pe", lambda: nc.tensor.matmul(h1[:, :], lhsT=wc[:, kc * 128:(kc + 1) * 128],
                                                             rhs=x1T[:, kc, :], start=(kc == 0), stop=(kc == 7)),
                              R=[wc, x1T], W=[h1])
                    for kc in range(8):
                        kb.op("pe", lambda: nc.tensor.matmul(h3[:, :], lhsT=wc[:, 1024 + kc * 128:1024 + (kc + 1) * 128],
                                                             rhs=x1T[:, kc, :], start=(kc == 0), stop=(kc == 7)),
                              R=[wc, x1T], W=[h3])
                    s = sil[wcnt % 2]
                    kb.op("act", lambda: nc.scalar.activation(out=s[:, :], in_=h1[:, :], func=AF.Silu), R=[h1], W=[s])
                    kb.op("dve", lambda: nc.vector.tensor_tensor(out=aT[f][:, :], in0=s[:, :], in1=h3[:, :],
                                                                 op=ALU.mult), R=[s, h3], W=[aT[f]])
                    wcnt += 1
                for tt in range(4):
                    for hf in range(2):
                        py = Y[hf]
                        for f in range(nfu):
                            kb.op("pe", lambda: nc.tensor.matmul(py[:, :], lhsT=aT[f][:, tt * 128:(tt + 1) * 128],
                                                                 rhs=w2[f][:, hf * 512:(hf + 1) * 512],
                                                                 start=(f == 0), stop=(f == nfu - 1)),
                                  R=[aT[f], w2[f]], W=[py])
                        ya = yacc[tt]
                        ysl = ya[:, hf * 512:(hf + 1) * 512]
                        if moe:
                            cs = comb[tt][:, e:e + 1]
                            if first:
                                kb.op("dve", lambda: nc.vector.tensor_scalar(out=ysl, in0=py[:, :], scalar1=cs,
                                                                             scalar2=None, op0=ALU.mult),
                                      R=[py, comb[tt]], W=[ya])
                            else:
                                kb.op("dve", lambda: nc.vector.scalar_tensor_tensor(out=ysl, in0=py[:, :], scalar=cs,
                                                                                    in1=ysl, op0=ALU.mult, op1=ALU.add),
                                      R=[py, comb[tt], ya], W=[ya])
                        else:
                            if first:
                                kb.op("dve", lambda: nc.vector.tensor_copy(out=ysl, in_=py[:, :]), R=[py], W=[ya])
                            else:
                                kb.op("dve", lambda: nc.vector.tensor_tensor(out=ysl, in0=py[:, :], in1=ysl, op=ALU.add),
                                      R=[py, ya], W=[ya])
                first = False
        for tt in range(4):
            tok0 = g * TG + tt * 128
            kb.op("dve", lambda: nc.vector.scalar_tensor_tensor(out=h[:, :], in0=x1g[tt][:, :], scalar=ALPHA,
                                                                in1=yacc[tt][:, :], op0=ALU.mult, op1=ALU.add),
                  R=[x1g[tt], yacc[tt]], W=[h])
            o = ost[tt % 2]
            layer_norm(kb, h, (lnp, lnp[:, 2, :]), (lnp, lnp[:, 3, :]), o[:, :], o, scr)
            kb.dma("sp", out_d[tok0:tok0 + 128, :], o[:, :], R=[o], W=[out_d])
    return kb.finish()


def ffn_chunk_layout(w1, w3, w2):
    F = w1.shape[1]
    nf = F // 128
    a = w1.reshape(8, 128, nf, 128).transpose(2, 1, 0, 3).reshape(nf, 128, 1024)
    b = w3.reshape(8, 128, nf, 128).transpose(2, 1, 0, 3).reshape(nf, 128, 1024)
    c = w2.reshape(nf, 128, 1024)
    return np.ascontiguousarray(np.concatenate([a, b, c], axis=2))


def run_C(o_flat_bf, x_flat, wo_bf, lnp4, wf, n_exp, nf_unit, n_units, wr=None):
    nc = build_C(n_exp, nf_unit, n_units)
    wol = w_kc_layout(wo_bf)
    lnb = np.ascontiguousarray(np.broadcast_to(lnp4[None, :, :], (128, 4, D))).astype(np.float32)
    maps = []
    for c in range(NCORES):
        oc = o_flat_bf[c * TPC:(c + 1) * TPC]
        oT = np.ascontiguousarray(oc.reshape(TPC, 8, 128).transpose(2, 1, 0))
        m = {"oT": oT, "x": np.ascontiguousarray(x_flat[c * TPC:(c + 1) * TPC]), "wo": wol, "lnp": lnb,
             "wf": wf, "ident": IDENT}
        if wr is not None:
            m["wr"] = np.ascontiguousarray(wr.reshape(8, 128, 8).transpose(1, 0, 2))
        maps.append(m)
    res = run_spmd(nc, maps)
    return np.concatenate([res[c]["out"] for c in range(NCORES)], axis=0)


from concourse.bass_types import AP as _AP

NQB = SEQ // 128
NDEL = 2304


def rel_bucket_np(d):
    d = np.maximum(d, 0)
    df = np.maximum(d, 1).astype(np.float32)
    large = 16 + (np.log(df / np.float32(16.0)).astype(np.float32) / np.float32(math.log(2048 / 16))
                  * np.float32(16)).astype(np.int32)
    large = np.minimum(large, 31)
    return np.where(d < 16, d, large)


def dil_const():
    C = np.zeros((32, NDEL), np.float32)
    for idx in range(NDEL):
        dl = idx - 127
        if dl < 0 or dl > 2048:
            continue
        mult = 0
        for (w, dd) in ((128, 1), (512, 4), (2048, 16)):
            if dl <= w and dl % dd == 0:
                mult += 1
        if mult:
            C[int(rel_bucket_np(np.array([dl]))[0]), idx] = mult
    return C


class AttnCtx:
    def __init__(self, kb, n_s=4, n_o=2, grouped=False):
        self.kb = kb
        self.S = [kb.ps([128, 512], F32, "S") for _ in range(n_s)]
        self.O = [kb.ps([128, 512], F32, "O") for _ in range(n_o)]
        self.P32 = [kb.sb([128, 128], F32, "P32") for _ in range(4)]
        self.Pb = [kb.sb([128, 128], BF16, "Pb") for _ in range(6)]
        self.pending = []
        self.LA = 3
        self.LAG = 2
        self.cpg = 0
        if grouped:
            self.P32g = [kb.sb([128, 512], F32, "P32g") for _ in range(3)]
            self.Pbg = [kb.sb([128, 512], BF16, "Pbg") for _ in range(4)]
        self.rc = [kb.sb([128, 1], F32, "rc") for _ in range(2)]
        self.cs = 0
        self.co = 0
        self.cp = 0


def attn_qblock(ax, qT, kT, Vaug, h, i, jlist, bias_of, mask_of, ost, ocol, after=None):
    kb = ax.kb
    nc = kb.nc
    O = ax.O[ax.co % len(ax.O)]
    rc = ax.rc[ax.co % 2]
    ax.co += 1
    hp = slice(h * 64, (h + 1) * 64)
    nj = len(jlist)
    for n, j in enumerate(jlist):
        S = ax.S[ax.cs % len(ax.S)]
        ax.cs += 1
        kb.op("pe", lambda: nc.tensor.matmul(S[:, :128], lhsT=kT[hp, j * 128:(j + 1) * 128],
                                             rhs=qT[hp, i * 128:(i + 1) * 128], start=True, stop=True),
              R=[kT, qT], W=[S])
        Pb = ax.Pb[ax.cp % len(ax.Pb)]
        b = bias_of(j) if bias_of is not None else None
        m = mask_of(j) if mask_of is not None else None
        if m is not None and not isinstance(m, list):
            m = [m]
        if m is not None and len(m) == 0:
            m = None
        tgt = Pb if m is None else ax.P32[ax.cp % len(ax.P32)]
        ax.cp += 1
        if b is None:
            kb.op("act", lambda: nc.scalar.activation(out=tgt[:, :], in_=S[:, :128], func=AF.Exp, scale=0.125),
                  R=[S], W=[tgt])
        else:
            kb.op("act", lambda: nc.scalar.activation(out=tgt[:, :], in_=S[:, :128], func=AF.Exp, bias=b[1],
                                                      scale=0.125), R=[S, b[0]], W=[tgt])
        if m is not None:
            for mm in m[:-1]:
                kb.op("pool", lambda: nc.gpsimd.tensor_tensor(out=tgt[:, :], in0=tgt[:, :], in1=mm[1], op=ALU.mult),
                      R=[tgt, mm[0]], W=[tgt])
            kb.op("dve", lambda: nc.vector.tensor_tensor(out=Pb[:, :], in0=tgt[:, :], in1=m[-1][1], op=ALU.mult),
                  R=[tgt, m[-1][0]], W=[Pb])

        def pv(Pb=Pb, j=j, n=n):
            kb.op("pe", lambda: nc.tensor.matmul(O[:, :65], lhsT=Pb[:, :], rhs=Vaug[:, j, h, :],
                                                 start=(n == 0), stop=(n == nj - 1)), R=[Pb, Vaug], W=[O])
            if n == nj - 1:
                kb.op("dve", lambda: nc.vector.reciprocal(out=rc[:, :], in_=O[:, 64:65]), R=[O], W=[rc])
                kb.op("dve", lambda: nc.vector.tensor_scalar(out=ost[:, ocol:ocol + 64], in0=O[:, 0:64],
                                                             scalar1=rc[:, 0:1], scalar2=None, op0=ALU.mult),
                      R=[O, rc], W=[ost])
                if after is not None:
                    after()

        ax.pending.append(pv)
        while len(ax.pending) > ax.LA:
            ax.pending.pop(0)()


def attn_qgroup(ax, qT, kT, Vaug, h, i, groups, ost, ocol, after=None):
    kb = ax.kb
    nc = kb.nc
    O = ax.O[ax.co % len(ax.O)]
    rc = ax.rc[ax.co % 2]
    ax.co += 1
    hp = slice(h * 64, (h + 1) * 64)
    ng = len(groups)
    for gi, g in enumerate(groups):
        js = g["js"]
        wd = 128 * len(js)
        S = ax.S[ax.cs % len(ax.S)]
        ax.cs += 1
        for n, j in enumerate(js):
            kb.op("pe", lambda: nc.tensor.matmul(S[:, n * 128:(n + 1) * 128], lhsT=kT[hp, j * 128:(j + 1) * 128],
                                                 rhs=qT[hp, i * 128:(i + 1) * 128], start=True, stop=True),
                  R=[kT, qT], W=[S])
        Pb = ax.Pbg[ax.cpg % len(ax.Pbg)]
        has_mask = (g.get("pool_masks") is not None) or (g.get("dve_mask") is not None)
        tgt = ax.P32g[ax.cpg % len(ax.P32g)] if has_mask else Pb
        ax.cpg += 1
        b = g.get("bias")
        if b is None:
            kb.op("act", lambda: nc.scalar.activation(out=tgt[:, :wd], in_=S[:, :wd], func=AF.Exp, scale=0.125),
                  R=[S], W=[tgt])
        else:
            kb.op("act", lambda: nc.scalar.activation(out=tgt[:, :wd], in_=S[:, :wd], func=AF.Exp, bias=b[1],
                                                      scale=0.125), R=[S, b[0]], W=[tgt])
        if has_mask:
            pm = g.get("pool_masks")
            dm = g.get("dve_mask")
            if pm is not None:
                for n, mm in enumerate(pm):
                    last = (dm is None) and False
                    kb.op("pool", lambda: nc.gpsimd.tensor_tensor(out=tgt[:, n * 128:(n + 1) * 128],
                                                                  in0=tgt[:, n * 128:(n + 1) * 128], in1=mm[1],
                                                                  op=ALU.mult), R=[tgt, mm[0]], W=[tgt])
            if dm is not None:
                kb.op("dve", lambda: nc.vector.tensor_tensor(out=Pb[:, :wd], in0=tgt[:, :wd], in1=dm[1], op=ALU.mult),
                      R=[tgt, dm[0]], W=[Pb])
            else:
                kb.op("dve", lambda: nc.vector.tensor_copy(out=Pb[:, :wd], in_=tgt[:, :wd]), R=[tgt], W=[Pb])

        def pv(Pb=Pb, js=js, gi=gi):
            for n, j in enumerate(js):
                kb.op("pe", lambda: nc.tensor.matmul(O[:, :65], lhsT=Pb[:, n * 128:(n + 1) * 128], rhs=Vaug[:, j, h, :],
                                                     start=(gi == 0 and n == 0),
                                                     stop=(gi == ng - 1 and n == len(js) - 1)), R=[Pb, Vaug], W=[O])
            if gi == ng - 1:
                kb.op("dve", lambda: nc.vector.reciprocal(out=rc[:, :], in_=O[:, 64:65]), R=[O], W=[rc])
                kb.op("dve", lambda: nc.vector.tensor_scalar(out=ost[:, ocol:ocol + 64], in0=O[:, 0:64],
                                                             scalar1=rc[:, 0:1], scalar2=None, op0=ALU.mult),
                      R=[O, rc], W=[ost])
                if after is not None:
                    after()

        ax.pending.append(pv)
        while len(ax.pending) > ax.LAG:
            ax.pending.pop(0)()


def attn_flush(ax):
    while ax.pending:
        ax.pending.pop(0)()


def load_vaug(kb, v_d, name):
    nc = kb.nc
    Vaug = kb.sb([128, NQB, 2, 65], BF16, name)
    kb.op("pool", lambda: nc.gpsimd.memset(Vaug[:, :, :, :], 1.0), W=[Vaug])
    for c in range(4):
        js = slice(c * 16, (c + 1) * 16)
        kb.dma("sp" if c % 2 == 0 else "pool", Vaug[:, js, :, 0:64],
               v_d[:, js, :].rearrange("p j (h d) -> p j h d", h=2), R=[v_d], W=[Vaug])
    return Vaug


def build_B_cd():
    kb = KB()
    nc = kb.nc
    qc_d = kb.dram_in("qc", [128, SEQ], BF16)
    kc_d = kb.dram_in("kc", [128, SEQ], BF16)
    vc_d = kb.dram_in("vc", [128, NQB, 128], BF16)
    qd_d = kb.dram_in("qd", [128, SEQ], BF16)
    kd_d = kb.dram_in("kd", [128, SEQ], BF16)
    vd_d = kb.dram_in("vd", [128, NQB, 128], BF16)
    fc_d = kb.dram_in("fc", [128, NQB, 2], F32)
    bf_d = kb.dram_in("bf", [128, 2], F32)
    tri_d = kb.dram_in("tri", [128, 128], F32)
    rel_d = kb.dram_in("rel", [32, 2], F32)
    C_d = kb.dram_in("dilc", [32, NDEL], F32)
    oc_d = kb.dram_out("oc", [SEQ, 128], BF16)
    od_d = kb.dram_out("od", [SEQ, 128], BF16)
    E_d = kb.dram_tmp("Escr", [2, NDEL], F32)

    tri = kb.sb([128, 128], F32, "tri")
    ones = kb.sb([128, 128], F32, "ones")
    kb.dma("sp", tri[:, :], tri_d[:, :], R=[tri_d], W=[tri])
    kb.op("dve", lambda: nc.vector.memset(ones[:, :], 1.0), W=[ones])
    ax = AttnCtx(kb)
    rel = kb.sb([32, 2], F32, "rel")
    Cs = kb.sb([32, NDEL], F32, "Cs")
    Es = kb.sb([2, NDEL], F32, "Es")
    kb.dma("sp", rel[:, :], rel_d[:, :], R=[rel_d], W=[rel])
    kb.dma("sp", Cs[:, :], C_d[:, :], R=[C_d], W=[Cs])
    kb.op("act", lambda: nc.scalar.activation(out=rel[:, :], in_=rel[:, :], func=AF.Exp), R=[rel], W=[rel])
    for c in range((NDEL + 511) // 512):
        w = min(512, NDEL - c * 512)
        pp = ax.S[c % 3]
        kb.op("pe", lambda: nc.tensor.matmul(pp[:2, :w], lhsT=rel[:, :], rhs=Cs[:, c * 512:c * 512 + w],
                                             start=True, stop=True), R=[rel, Cs], W=[pp])
        kb.op("dve", lambda: nc.vector.tensor_copy(out=Es[:, c * 512:c * 512 + w], in_=pp[:2, :w]), R=[pp], W=[Es])
    kb.dma("sp", E_d[:, :], Es[:, :], R=[Es], W=[E_d])
    TT = [kb.sb([128, 17 * 128], F32, "TT") for _ in range(2)]
    for h in range(2):
        src = _AP(tensor=E_d.t.tensor, offset=h * NDEL, ap=[[1, 128], [1, 17 * 128]])
        kb.dma("sp", TT[h][:, :], src, R=[E_d], W=[TT[h]])
    fc = kb.sb([128, NQB, 2], F32, "fc")
    bf = kb.sb([128, 2], F32, "bf")
    lf = kb.sb([128, 2, NQB], F32, "lf")
    kb.dma("sp", fc[:, :, :], fc_d[:, :, :], R=[fc_d], W=[fc])
    kb.dma("sp", bf[:, :], bf_d[:, :], R=[bf_d], W=[bf])
    for h in range(2):
        kb.op("dve", lambda: nc.vector.tensor_scalar(out=lf[:, h, :], in0=fc[:, :, h], scalar1=bf[:, h:h + 1],
                                                     scalar2=None, op0=ALU.add), R=[fc, bf], W=[lf])
    kb.op("act", lambda: nc.scalar.activation(out=lf[:, :, :], in_=lf[:, :, :], func=AF.Exp, scale=-1.0), R=[lf], W=[lf])
    kb.op("act", lambda: nc.scalar.activation(out=lf[:, :, :], in_=lf[:, :, :], func=AF.Ln, bias=1.0), R=[lf], W=[lf])
    kb.op("dve", lambda: nc.vector.tensor_scalar(out=lf[:, :, :], in0=lf[:, :, :], scalar1=-1.0, scalar2=None,
                                                 op0=ALU.mult), R=[lf], W=[lf])
    lf2 = lf[:, :, :].rearrange("p h j -> p (h j)")
    p1, p2 = ax.O[0], ax.O[1]
    kb.op("pe", lambda: nc.tensor.matmul(p1[:, :128], lhsT=tri[:, :], rhs=lf2, start=True, stop=True),
          R=[tri, lf], W=[p1])
    kb.op("pe", lambda: nc.tensor.matmul(p2[:, :128], lhsT=ones[:, :], rhs=lf2, start=True, stop=True),
          R=[ones, lf], W=[p2])
    tot = kb.sb([128, 2, NQB], F32, "tot")
    carry = kb.sb([128, 2, NQB], F32, "carry")
    negF = kb.sb([128, 2, NQB], F32, "negF")
    kb.op("dve", lambda: nc.vector.tensor_copy(out=tot[:, :, :].rearrange("p h j -> p (h j)"), in_=p2[:, :128]),
          R=[p2], W=[tot])
    for h in range(2):
        kb.op("dve", lambda: nc.vector.tensor_tensor_scan(out=carry[:, h, :], data0=ones[:, :NQB], data1=tot[:, h, :],
                                                          initial=0.0, op0=ALU.mult, op1=ALU.add),
              R=[ones, tot], W=[carry])
    kb.op("dve", lambda: nc.vector.tensor_tensor(out=carry[:, :, :], in0=carry[:, :, :], in1=tot[:, :, :],
                                                 op=ALU.subtract), R=[carry, tot], W=[carry])
    kb.op("dve", lambda: nc.vector.tensor_tensor(out=negF[:, :, :].rearrange("p h j -> p (h j)"), in0=p1[:, :128],
                                                 in1=carry[:, :, :].rearrange("p h j -> p (h j)"), op=ALU.add),
          R=[p1, carry], W=[negF])
    kb.op("dve", lambda: nc.vector.tensor_scalar(out=negF[:, :, :], in0=negF[:, :, :], scalar1=-1.0, scalar2=None,
                                                 op0=ALU.mult), R=[negF], W=[negF])
    qc = kb.sb([128, SEQ], BF16, "qc")
    kc = kb.sb([128, SEQ], BF16, "kc")
    qd = kb.sb([128, SEQ], BF16, "qd")
    kd = kb.sb([128, SEQ], BF16, "kd")
    for n, (s, d_) in enumerate(((qd, qd_d), (kd, kd_d), (qc, qc_d), (kc, kc_d))):
        for c in range(2):
            kb.dma("sp" if (n + c) % 2 == 0 else "pool", s[:, c * 4096:(c + 1) * 4096], d_[:, c * 4096:(c + 1) * 4096],
                   R=[d_], W=[s])
    Vd = load_vaug(kb, vd_d, "Vd")
    Vc = load_vaug(kb, vc_d, "Vc")
    ostd = [kb.sb([128, 128], BF16, "ostd") for _ in range(2)]
    ostc = [kb.sb([128, 128], BF16, "ostc") for _ in range(2)]
    Bi = [kb.sb([128, NQB], F32, "Bi") for _ in range(3)]
    nb = 0
    for i in range(NQB):
        od = ostd[i % 2]
        for h in range(2):
            j0 = max(0, i - 16)
            attn_qblock(ax, qd, kd, Vd, h, i, list(range(j0, i + 1)), None,
                        lambda j, h=h, i=i: (TT[h], TT[h][:, (i - j) * 128:(i - j + 1) * 128]), od, h * 64,
                        after=(None if h == 0 else
                               (lambda i=i, od=od: kb.dma("pool", od_d[i * 128:(i + 1) * 128, :], od[:, :],
                                                          R=[od], W=[od_d]))))
        oc = ostc[i % 2]
        for h in range(2):
            B = Bi[nb % 3]
            nb += 1
            kb.op("dve", lambda: nc.vector.tensor_scalar(out=B[:, :i + 1], in0=negF[:, h, :i + 1],
                                                         scalar1=carry[:, h, i:i + 1], scalar2=None, op0=ALU.add),
                  R=[negF, carry], W=[B])
            attn_qblock(ax, qc, kc, Vc, h, i, list(range(0, i + 1)),
                        lambda j, B=B: (B, B[:, j:j + 1]),
                        lambda j, i=i: ((tri, tri[:, :]) if j == i else None), oc, h * 64,
                        after=(None if h == 0 else
                               (lambda i=i, oc=oc: kb.dma("sp", oc_d[i * 128:(i + 1) * 128, :], oc[:, :],
                                                          R=[oc], W=[oc_d]))))
    attn_flush(ax)
    return kb.finish()


TRI = np.triu(np.ones((128, 128), np.float32))


def to_pj(a):
    n = a.shape[1]
    return np.ascontiguousarray(a.reshape(NQB, 128, n).transpose(1, 0, 2))


def run_B_cd(yT, ytb, ytf, b_forget, rel_table):
    nc = build_B_cd()
    C = dil_const()
    maps = []
    for c in range(NCORES):
        b, m = c // 4, c % 4
        ts = slice(b * SEQ, (b + 1) * SEQ)
        rs = lambda base: slice(base + m * 128, base + (m + 1) * 128)
        maps.append({
            "qc": np.ascontiguousarray(yT[rs(0), ts]), "kc": np.ascontiguousarray(yT[rs(512), ts]),
            "qd": np.ascontiguousarray(yT[rs(1024), ts]),
            "kd": np.ascontiguousarray(yT[rs(1536), ts].reshape(128, NQB, 128)[:, :, ::-1].reshape(128, SEQ)),
            "vc": to_pj(ytb[ts, m * 128:(m + 1) * 128]), "vd": np.ascontiguousarray(to_pj(ytb[ts, 512 + m * 128:512 + (m + 1) * 128])[::-1]),
            "fc": to_pj(ytf[ts, 2 * m:2 * m + 2]),
            "bf": np.ascontiguousarray(np.broadcast_to(b_forget[None, 2 * m:2 * m + 2], (128, 2))).astype(np.float32),
            "tri": TRI, "rel": np.ascontiguousarray(rel_table[:, 2 * m:2 * m + 2]), "dilc": C,
        })
    res = run_spmd(nc, maps)
    o = np.zeros((BATCH * SEQ, D), NPBF)
    for c in range(NCORES):
        b, m = c // 4, c % 4
        o[b * SEQ:(b + 1) * SEQ, m * 128:(m + 1) * 128] = res[c]["oc"]
        o[b * SEQ:(b + 1) * SEQ, 512 + m * 128:512 + (m + 1) * 128] = res[c]["od"]
    return o


def build_B_gla():
    kb = KB()
    nc = kb.nc
    qT_d = kb.dram_in("qT", [64, SEQ], BF16)
    kT_d = kb.dram_in("kT", [64, SEQ], BF16)
    k_d = kb.dram_in("k", [128, NQB, 64], BF16)
    v_d = kb.dram_in("v", [128, NQB, 128], BF16)
    r_d = kb.dram_in("r", [128, NQB, 128], F32)
    g_d = kb.dram_in("g", [128, NQB, 64], F32)
    gn_d = kb.dram_in("gn", [128, 128], F32)
    tri_d = kb.dram_in("tri", [128, 128], F32)
    o_d = kb.dram_out("o", [SEQ, 128], BF16)
    qT = kb.sb([64, SEQ], BF16, "qT")
    kT = kb.sb([64, SEQ], BF16, "kT")
    ktm = kb.sb([128, NQB, 64], BF16, "ktm")
    v = kb.sb([128, NQB, 128], BF16, "v")
    r = kb.sb([128, NQB, 128], F32, "r")
    g = kb.sb([128, NQB, 64], F32, "g")
    gn = kb.sb([128, 128], F32, "gn")
    tri = kb.sb([128, 128], F32, "tri")
    eps = kb.sb([128, 1], F32, "eps")
    kb.op("dve", lambda: nc.vector.memset(eps[:, :], LN_EPS), W=[eps])
    kb.dma("sp", tri[:, :], tri_d[:, :], R=[tri_d], W=[tri])
    kb.dma("sp", g[:, :, :], g_d[:, :, :], R=[g_d], W=[g])
    kb.dma("pool", qT[:, :], qT_d[:, :], R=[qT_d], W=[qT])
    kb.dma("sp", kT[:, :], kT_d[:, :], R=[kT_d], W=[kT])
    kb.dma("pool", ktm[:, :, :], k_d[:, :, :], R=[k_d], W=[ktm])
    kb.dma("sp", v[:, :, :], v_d[:, :, :], R=[v_d], W=[v])
    kb.dma("pool", r[:, :, :], r_d[:, :, :], R=[r_d], W=[r])
    kb.dma("sp", gn[:, :], gn_d[:, :], R=[gn_d], W=[gn])
    kb.op("act", lambda: nc.scalar.activation(out=r[:, :, :], in_=r[:, :, :], func=AF.Silu), R=[r], W=[r])
    PG = [kb.ps([128, 512], F32, "PG") for _ in range(2)]
    PGT = [kb.ps([128, 512], F32, "PGT") for _ in range(2)]
    PA = [kb.ps([128, 512], F32, "PA") for _ in range(2)]
    PO = kb.ps([128, 512], F32, "PO")
    PU = kb.ps([128, 512], F32, "PU")
    eGT = [kb.sb([64, 128], F32, "eGT") for _ in range(2)]
    enGT = [kb.sb([64, 128], F32, "enGT") for _ in range(2)]
    enG = [kb.sb([128, 64], F32, "enG") for _ in range(2)]
    qgT = [kb.sb([64, 128], BF16, "qgT") for _ in range(2)]
    kgT = [kb.sb([64, 128], BF16, "kgT") for _ in range(2)]
    kg = [kb.sb([128, 64], BF16, "kg") for _ in range(2)]
    Am = [kb.sb([128, 128], BF16, "Am") for _ in range(2)]
    S32 = kb.sb([64, 128], F32, "S32")
    Sbf = kb.sb([64, 128], BF16, "Sbf")
    st6 = [kb.sb([128, 6], F32, "st6") for _ in range(2)]
    mv = [kb.sb([128, 4], F32, "mv") for _ in range(2)]
    of = [kb.sb([128, 128], F32, "of") for _ in range(2)]
    ost = [kb.sb([128, 128], BF16, "ost") for _ in range(2)]
    for c in range(NQB):
        p = c % 2
        cs = slice(c * 128, (c + 1) * 128)
        kb.op("pe", lambda: nc.tensor.matmul(PG[p][:, :64], lhsT=tri[:, :], rhs=g[:, c, :], start=True, stop=True),
              R=[tri, g], W=[PG[p]])
        kb.op("pe", lambda: nc.tensor.matmul(PGT[p][:64, :128], lhsT=g[:, c, :], rhs=tri[:, :], start=True, stop=True),
              R=[tri, g], W=[PGT[p]])
        kb.op("act", lambda: nc.scalar.activation(out=eGT[p][:, :], in_=PGT[p][:64, :128], func=AF.Exp),
              R=[PGT[p]], W=[eGT[p]])
        kb.op("act", lambda: nc.scalar.activation(out=enGT[p][:, :], in_=PGT[p][:64, :128], func=AF.Exp, scale=-1.0),
              R=[PGT[p]], W=[enGT[p]])
        kb.op("act", lambda: nc.scalar.activation(out=enG[p][:, :], in_=PG[p][:, :64], func=AF.Exp, scale=-1.0),
              R=[PG[p]], W=[enG[p]])
        kb.op("dve", lambda: nc.vector.scalar_tensor_tensor(out=qgT[p][:, :], in0=qT[:, cs], scalar=0.125,
                                                            in1=eGT[p][:, :], op0=ALU.mult, op1=ALU.mult),
              R=[qT, eGT[p]], W=[qgT[p]])
        kb.op("dve", lambda: nc.vector.tensor_tensor(out=kgT[p][:, :], in0=kT[:, cs], in1=enGT[p][:, :], op=ALU.mult),
              R=[kT, enGT[p]], W=[kgT[p]])
        kb.op("dve", lambda: nc.vector.tensor_tensor(out=kg[p][:, :], in0=ktm[:, c, :], in1=enG[p][:, :], op=ALU.mult),
              R=[ktm, enG[p]], W=[kg[p]])
        kb.op("pe", lambda: nc.tensor.matmul(PA[p][:, :128], lhsT=kgT[p][:, :], rhs=qgT[p][:, :], start=True, stop=True),
              R=[kgT[p], qgT[p]], W=[PA[p]])
        kb.op("dve", lambda: nc.vector.tensor_tensor(out=Am[p][:, :], in0=PA[p][:, :128], in1=tri[:, :], op=ALU.mult),
              R=[PA[p], tri], W=[Am[p]])
        kb.op("pe", lambda: nc.tensor.matmul(PO[:, :128], lhsT=Am[p][:, :], rhs=v[:, c, :], start=True, stop=(c == 0)),
              R=[Am[p], v], W=[PO])
        if c > 0:
            kb.op("pe", lambda: nc.tensor.matmul(PO[:, :128], lhsT=qgT[p][:, :], rhs=Sbf[:, :], start=False, stop=True),
                  R=[qgT[p], Sbf], W=[PO])
        if c < NQB - 1:
            kb.op("pe", lambda: nc.tensor.matmul(PU[:64, :128], lhsT=kg[p][:, :], rhs=v[:, c, :], start=True, stop=True),
                  R=[kg[p], v], W=[PU])
            eGl = eGT[p][:, 127:128]
            if c == 0:
                kb.op("dve", lambda: nc.vector.tensor_scalar(out=S32[:, :], in0=PU[:64, :128], scalar1=eGl, scalar2=None,
                                                             op0=ALU.mult), R=[PU, eGT[p]], W=[S32])
            else:
                kb.op("dve", lambda: nc.vector.tensor_scalar(out=S32[:, :], in0=S32[:, :], scalar1=eGl, scalar2=None,
                                                             op0=ALU.mult), R=[S32, eGT[p]], W=[S32])
                kb.op("dve", lambda: nc.vector.scalar_tensor_tensor(out=S32[:, :], in0=PU[:64, :128], scalar=eGl,
                                                                    in1=S32[:, :], op0=ALU.mult, op1=ALU.add),
                      R=[PU, eGT[p], S32], W=[S32])
            kb.op("dve", lambda: nc.vector.tensor_copy(out=Sbf[:, :], in_=S32[:, :]), R=[S32], W=[Sbf])
        kb.op("dve", lambda: nc.vector.bn_stats(out=st6[p][:, :], in_=PO[:, :128]), R=[PO], W=[st6[p]])
        kb.op("dve", lambda: nc.vector.bn_aggr(out=mv[p][:, 0:2], in_=st6[p][:, :]), R=[st6[p]], W=[mv[p]])
        kb.op("dve", lambda: nc.vector.scalar_tensor_tensor(out=mv[p][:, 2:3], in0=mv[p][:, 0:1], scalar=mv[p][:, 0:1],
                                                            in1=mv[p][:, 1:2], op0=ALU.mult, op1=ALU.add),
              R=[mv[p]], W=[mv[p]])
        kb.op("act", lambda: nc.scalar.activation(out=mv[p][:, 3:4], in_=mv[p][:, 2:3], func=AF.Ln, bias=eps[:, 0:1]),
              R=[mv[p], eps], W=[mv[p]])
        kb.op("act", lambda: nc.scalar.activation(out=mv[p][:, 3:4], in_=mv[p][:, 3:4], func=AF.Exp, scale=-0.5),
              R=[mv[p]], W=[mv[p]])
        kb.op("dve", lambda: nc.vector.scalar_tensor_tensor(out=of[p][:, :], in0=PO[:, :128], scalar=mv[p][:, 3:4],
                                                            in1=gn[:, :], op0=ALU.mult, op1=ALU.mult),
              R=[PO, mv[p], gn], W=[of[p]])
        kb.op("pool", lambda: nc.gpsimd.tensor_tensor(out=ost[p][:, :], in0=of[p][:, :], in1=r[:, c, :], op=ALU.mult),
              R=[of[p], r], W=[ost[p]])
        kb.dma("sp", o_d[cs, :], ost[p][:, :], R=[ost[p]], W=[o_d])
    return kb.finish()


def run_B_gla(yT, ytb, ytf, g_norm):
    nc = build_B_gla()
    maps = []
    for c in range(NCORES):
        b, h = c // 4, c % 4
        ts = slice(b * SEQ, (b + 1) * SEQ)
        maps.append({
            "qT": np.ascontiguousarray(yT[h * 64:(h + 1) * 64, ts]),
            "kT": np.ascontiguousarray(yT[256 + h * 64:256 + (h + 1) * 64, ts]),
            "k": to_pj(ytb[ts, h * 64:(h + 1) * 64]),
            "v": to_pj(ytb[ts, 256 + h * 128:256 + (h + 1) * 128]),
            "r": to_pj(ytf[ts, h * 128:(h + 1) * 128]),
            "g": to_pj(ytf[ts, 520 + h * 64:520 + (h + 1) * 64]),
            "gn": np.ascontiguousarray(np.broadcast_to(g_norm[None, :], (128, 128))).astype(np.float32),
            "tri": TRI,
        })
    res = run_spmd(nc, maps)
    o = np.zeros((BATCH * SEQ, 512), NPBF)
    for c in range(NCORES):
        b, h = c // 4, c % 4
        o[b * SEQ:(b + 1) * SEQ, h * 128:(h + 1) * 128] = res[c]["o"]
    return o


NBIS = 24
TOPK = 256


def build_B_dsa1(act_split=True):
    kb = KB()
    nc = kb.nc
    NK = 16
    qiT_d = kb.dram_in("qiT", [64, 8, NK * 128], BF16)
    kiT_d = kb.dram_in("kiT", [64, SEQ], BF16)
    wi_d = kb.dram_in("wi", [128, NK, 8], F32)
    cm_d = kb.dram_in("cmask", [128, 512], F32)
    idb_d = kb.dram_in("identb", [128, 128], BF16)
    stp_d = kb.dram_in("steps", [128, NBIS], F32)
    M_d = kb.dram_out("M", [NK, 128, SEQ], BF16)
    qiT = kb.sb([64, 8, NK * 128], BF16, "qiT")
    kiT = kb.sb([64, SEQ], BF16, "kiT")
    wi = kb.sb([128, NK, 8], F32, "wi")
    absw = kb.sb([128, NK, 8], F32, "absw")
    sgn = kb.sb([128, NK, 8], F32, "sgn")
    cm = kb.sb([128, 512], F32, "cm")
    idb = kb.sb([128, 128], BF16, "idb")
    stp = kb.sb([128, NBIS], F32, "stp")
    kb.dma("sp", qiT[:, :, :], qiT_d[:, :, :], R=[qiT_d], W=[qiT])
    kb.dma("pool", kiT[:, :], kiT_d[:, :], R=[kiT_d], W=[kiT])
    kb.dma("sp", wi[:, :, :], wi_d[:, :, :], R=[wi_d], W=[wi])
    kb.dma("sp", cm[:, :], cm_d[:, :], R=[cm_d], W=[cm])
    kb.dma("sp", idb[:, :], idb_d[:, :], R=[idb_d], W=[idb])
    kb.dma("sp", stp[:, :], stp_d[:, :], R=[stp_d], W=[stp])
    kb.op("act", lambda: nc.scalar.activation(out=absw[:, :, :], in_=wi[:, :, :], func=AF.Abs), R=[wi], W=[absw])
    kb.op("act", lambda: nc.scalar.activation(out=sgn[:, :, :], in_=wi[:, :, :], func=AF.Sign), R=[wi], W=[sgn])
    PS = [kb.ps([128, 512], F32, "PS") for _ in range(3)]
    PT = [kb.ps([128, 1024], BF16, "PT") for _ in range(2)]

    def write_mask(k, mt, Lk):
        kb.dma("sp", M_d[k, :, :Lk], mt[:, :Lk], R=[mt], W=[M_d])

    dsa1_body(kb, qiT, kiT, absw, sgn, cm, idb, stp, PS, PT, write_mask, act_split)
    return kb.finish()


def dsa1_body(kb, qiT, kiT, absw, sgn, cm, idb, stp, PS, PT, write_mask, act_split=True):
    nc = kb.nc
    NK = 16
    score = [kb.sb([128, SEQ], F32, "score") for _ in range(2)]
    selb = kb.sb([128, SEQ], BF16, "selb")
    junk2 = kb.sb([128, SEQ // 2], BF16, "junk2")
    MT = [kb.sb([128, SEQ], BF16, "MT") for _ in range(2)]
    rl = [kb.sb([128, 512], F32, "rl") for _ in range(3)]
    bs = [kb.sb([128, 16], F32, "bs") for _ in range(2)]
    st = {"rl": 0, "pt": 0}

    def scoring_units(k):
        sc_ = score[k % 2]
        units = []
        for c in range(k + 1):
            for hi in range(8):
                def u(c=c, hi=hi):
                    pp = PS[st["rl"] % 3]
                    r_ = rl[st["rl"] % 3]
                    st["rl"] += 1
                    kb.op("pe", lambda: nc.tensor.matmul(pp[:, :], lhsT=qiT[:, hi, k * 128:(k + 1) * 128],
                                                         rhs=kiT[:, c * 512:(c + 1) * 512], start=True, stop=True),
                          R=[qiT, kiT], W=[pp])
                    kb.op("act", lambda: nc.scalar.activation(out=r_[:, :], in_=pp[:, :], func=AF.Relu,
                                                              scale=absw[:, k, hi:hi + 1]), R=[pp, absw], W=[r_])
                    ssl = sc_[:, c * 512:(c + 1) * 512]
                    if hi == 0:
                        kb.op("dve", lambda: nc.vector.tensor_scalar(out=ssl, in0=r_[:, :], scalar1=sgn[:, k, 0:1],
                                                                     scalar2=None, op0=ALU.mult), R=[r_, sgn], W=[sc_])
                    else:
                        kb.op("dve", lambda: nc.vector.scalar_tensor_tensor(out=ssl, in0=r_[:, :],
                                                                            scalar=sgn[:, k, hi:hi + 1], in1=ssl,
                                                                            op0=ALU.mult, op1=ALU.add),
                              R=[r_, sgn, sc_], W=[sc_])
                units.append(u)
        return units

    for u in scoring_units(0):
        u()
    for k in range(NK):
        L = 512 * (k + 1)
        sc_ = score[k % 2]
        b_ = bs[k % 2]
        nxt = scoring_units(k + 1) if k + 1 < NK else []
        per_it = (len(nxt) + NBIS - 1) // NBIS
        kb.op("dve", lambda: nc.vector.tensor_reduce(out=b_[:, 0:1], in_=sc_[:, :L], axis=AX.X, op=ALU.min),
              R=[sc_], W=[b_])
        kb.op("dve", lambda: nc.vector.tensor_reduce(out=b_[:, 1:2], in_=sc_[:, :L], axis=AX.X, op=ALU.max),
              R=[sc_], W=[b_])
        kb.op("dve", lambda: nc.vector.tensor_tensor(out=sc_[:, L - 512:L], in0=sc_[:, L - 512:L], in1=cm[:, :],
                                                     op=ALU.add), R=[sc_, cm], W=[sc_])
        kb.op("dve", lambda: nc.vector.tensor_tensor(out=b_[:, 2:3], in0=b_[:, 1:2], in1=b_[:, 0:1], op=ALU.subtract),
              R=[b_], W=[b_])
        Ld = L // 2 if act_split else L
        for n in range(NBIS):
            kb.op("dve", lambda: nc.vector.tensor_scalar(out=b_[:, 7:8], in0=b_[:, 2:3], scalar1=stp[:, n:n + 1],
                                                         scalar2=None, op0=ALU.mult), R=[b_, stp], W=[b_])
            kb.op("dve", lambda: nc.vector.tensor_tensor(out=b_[:, 3:4], in0=b_[:, 0:1], in1=b_[:, 7:8], op=ALU.add),
                  R=[b_], W=[b_])
            if act_split:
                kb.op("act", lambda: nc.scalar.activation(out=junk2[:, :L - Ld], in_=sc_[:, Ld:L], func=AF.Sign,
                                                          bias=b_[:, 3:4], scale=-1.0, accum_out=b_[:, 6:7]),
                      R=[sc_, b_], W=[junk2, b_])
            kb.op("dve", lambda: nc.vector.tensor_scalar(out=selb[:, :Ld], in0=sc_[:, :Ld], scalar1=b_[:, 3:4],
                                                         scalar2=0.0, op0=ALU.is_ge, op1=ALU.add, accum_out=b_[:, 4:5]),
                  R=[sc_, b_], W=[selb, b_])
            for u in nxt[n * per_it:(n + 1) * per_it]:
                u()
            if act_split:
                kb.op("dve", lambda: nc.vector.tensor_scalar(out=b_[:, 6:7], in0=b_[:, 6:7], scalar1=-0.5,
                                                             scalar2=0.5 * (L - Ld), op0=ALU.mult, op1=ALU.add),
                      R=[b_], W=[b_])
                kb.op("dve", lambda: nc.vector.tensor_tensor(out=b_[:, 4:5], in0=b_[:, 4:5], in1=b_[:, 6:7], op=ALU.add),
                      R=[b_], W=[b_])
            kb.op("dve", lambda: nc.vector.tensor_scalar(out=b_[:, 5:6], in0=b_[:, 4:5], scalar1=TOPK - 0.25,
                                                         scalar2=b_[:, 7:8], op0=ALU.is_ge, op1=ALU.mult),
                  R=[b_], W=[b_])
            kb.op("dve", lambda: nc.vector.tensor_tensor(out=b_[:, 0:1], in0=b_[:, 0:1], in1=b_[:, 5:6], op=ALU.add),
                  R=[b_], W=[b_])
        for u in nxt[NBIS * per_it:]:
            u()
        kb.op("dve", lambda: nc.vector.tensor_scalar(out=selb[:, :L], in0=sc_[:, :L], scalar1=b_[:, 0:1], scalar2=None,
                                                     op0=ALU.is_ge), R=[sc_, b_], W=[selb])
        mt = MT[k % 2]
        nkb = 4 * (k + 1)
        for j0 in range(0, nkb, 8):
            pt = PT[st["pt"] % 2]
            for jj in range(8):
                j = j0 + jj
                if j >= nkb:
                    break
                kb.op("pe", lambda: nc.tensor.transpose(pt[:, jj * 128:(jj + 1) * 128], selb[:, j * 128:(j + 1) * 128],
                                                        idb[:, :]), R=[selb, idb], W=[pt])
            wdt = min(8, nkb - j0) * 128
            if st["pt"] % 2 == 0:
                kb.op("act", lambda: nc.scalar.copy(out=mt[:, j0 * 128:j0 * 128 + wdt], in_=pt[:, :wdt]), R=[pt], W=[mt])
            else:
                kb.op("dve", lambda: nc.vector.tensor_copy(out=mt[:, j0 * 128:j0 * 128 + wdt], in_=pt[:, :wdt]),
                      R=[pt], W=[mt])
            st["pt"] += 1
        write_mask(k, mt, L)


def run_B_dsa1(yT, ytf):
    nc = build_B_dsa1()
    steps = np.ascontiguousarray(np.broadcast_to((0.5 ** np.arange(1, NBIS + 1))[None, :], (128, NBIS))).astype(np.float32)
    maps = []
    for c in range(NCORES):
        b, m = c // 4, c % 4
        ts = slice(b * SEQ, (b + 1) * SEQ)
        tok = (np.arange(16)[:, None] * 512 + m * 128 + np.arange(128)[None, :]).reshape(-1) + b * SEQ
        qi = yT[1536:2048][:, tok].reshape(8, 64, 2048).transpose(1, 0, 2)
        wi = ytf[tok, 512:520].reshape(16, 128, 8).transpose(1, 0, 2)
        cm = np.where(np.arange(512)[None, :] <= (128 * m + np.arange(128))[:, None], 0.0, NEG).astype(np.float32)
        maps.append({"qiT": np.ascontiguousarray(qi), "kiT": np.ascontiguousarray(yT[2048:2112, ts]),
                     "wi": np.ascontiguousarray(wi), "cmask": cm, "identb": IDENT.astype(NPBF), "steps": steps})
    res = run_spmd(nc, maps)
    out = []
    for b in range(BATCH):
        M = np.zeros((NQB, 128, SEQ), NPBF)
        for m in range(4):
            M[m::4] = res[b * 4 + m]["M"]
        out.append(M)
    return out


def dsa_const():
    C = np.zeros((32, NDEL), np.float32)
    dl = np.arange(NDEL) - 127
    ok = dl >= 0
    C[rel_bucket_np(dl)[ok], np.arange(NDEL)[ok]] = 1.0
    return C


NPACK = NQB * (NQB + 1) // 2


def build_B_dsa2():
    kb = KB()
    nc = kb.nc
    q_d = kb.dram_in("q", [128, SEQ], BF16)
    k_d = kb.dram_in("k", [128, SEQ], BF16)
    v_d = kb.dram_in("v", [128, NQB, 128], BF16)
    rel_d = kb.dram_in("rel", [32, 2], F32)
    rf_d = kb.dram_in("relfar", [128, 2], F32)
    C_d = kb.dram_in("dsac", [32, NDEL], F32)
    M_d = kb.dram_in("Mp", [NPACK * 128 * 128], BF16)
    o_d = kb.dram_out("o", [SEQ, 128], BF16)
    E_d = kb.dram_tmp("Escr", [2, NDEL], F32)
    ax = AttnCtx(kb)
    rel = kb.sb([32, 2], F32, "rel")
    rf = kb.sb([128, 2], F32, "rf")
    Cs = kb.sb([32, NDEL], F32, "Cs")
    Es = kb.sb([2, NDEL], F32, "Es")
    kb.dma("sp", rel[:, :], rel_d[:, :], R=[rel_d], W=[rel])
    kb.dma("sp", rf[:, :], rf_d[:, :], R=[rf_d], W=[rf])
    kb.dma("sp", Cs[:, :], C_d[:, :], R=[C_d], W=[Cs])
    kb.op("act", lambda: nc.scalar.activation(out=rel[:, :], in_=rel[:, :], func=AF.Exp), R=[rel], W=[rel])
    for c in range((NDEL + 511) // 512):
        w = min(512, NDEL - c * 512)
        pp = ax.S[c % 3]
        kb.op("pe", lambda: nc.tensor.matmul(pp[:2, :w], lhsT=rel[:, :], rhs=Cs[:, c * 512:c * 512 + w],
                                             start=True, stop=True), R=[rel, Cs], W=[pp])
        kb.op("dve", lambda: nc.vector.tensor_copy(out=Es[:, c * 512:c * 512 + w], in_=pp[:2, :w]), R=[pp], W=[Es])
    kb.dma("sp", E_d[:, :], Es[:, :], R=[Es], W=[E_d])
    TT = [kb.sb([128, 17 * 128], F32, "TT") for _ in range(2)]
    for h in range(2):
        src = _AP(tensor=E_d.t.tensor, offset=h * NDEL, ap=[[1, 128], [1, 17 * 128]])
        kb.dma("sp", TT[h][:, :], src, R=[E_d], W=[TT[h]])
    q = kb.sb([128, SEQ], BF16, "q")
    k = kb.sb([128, SEQ], BF16, "k")
    for n, (s, d_) in enumerate(((q, q_d), (k, k_d))):
        for c in range(2):
            kb.dma("sp" if (n + c) % 2 == 0 else "pool", s[:, c * 4096:(c + 1) * 4096], d_[:, c * 4096:(c + 1) * 4096],
                   R=[d_], W=[s])
    V = load_vaug(kb, v_d, "V")
    Ms = [kb.sb([128, SEQ], BF16, "Ms") for _ in range(2)]
    ost = [kb.sb([128, 128], BF16, "ost") for _ in range(2)]
    for i in range(NQB):
        ms = Ms[i % 2]
        W_ = (i + 1) * 128
        off = (i * (i + 1) // 2) * 128 * 128
        src = _AP(tensor=M_d.t.tensor, offset=off, ap=[[W_, 128], [1, W_]])
        kb.dma("pool" if i % 2 else "sp", ms[:, :W_], src, R=[M_d], W=[ms])
        o = ost[i % 2]
        for h in range(2):
            def bias_of(j, h=h, i=i):
                return (rf, rf[:, h:h + 1]) if i - j > 16 else None

            def mask_of(j, h=h, i=i, ms=ms):
                sel = (ms, ms[:, j * 128:(j + 1) * 128])
                if i - j > 16:
                    return [sel]
                return [(TT[h], TT[h][:, (i - j) * 128:(i - j + 1) * 128]), sel]

            attn_qblock(ax, q, k, V, h, i, list(range(0, i + 1)), bias_of, mask_of, o, h * 64,
                        after=(None if h == 0 else
                               (lambda i=i, o=o: kb.dma("sp", o_d[i * 128:(i + 1) * 128, :], o[:, :], R=[o], W=[o_d]))))
    attn_flush(ax)
    return kb.finish()


def run_B_dsa2(yT, ytb, Msel, rel_table):
    nc = build_B_dsa2()
    C = dsa_const()
    packed = []
    for b in range(BATCH):
        M = Msel[b][:, ::-1, :]
        packed.append(np.concatenate([np.ascontiguousarray(M[i][:, :(i + 1) * 128]).reshape(-1) for i in range(NQB)]))
    maps = []
    for c in range(NCORES):
        b, m = c // 4, c % 4
        ts = slice(b * SEQ, (b + 1) * SEQ)
        maps.append({
            "q": np.ascontiguousarray(yT[512 + m * 128:512 + (m + 1) * 128, ts]),
            "k": np.ascontiguousarray(yT[1024 + m * 128:1024 + (m + 1) * 128, ts].reshape(128, NQB, 128)[:, :, ::-1]
                                      .reshape(128, SEQ)),
            "v": np.ascontiguousarray(to_pj(ytb[ts, 768 + m * 128:768 + (m + 1) * 128])[::-1]),
            "rel": np.ascontiguousarray(rel_table[:, 2 * m:2 * m + 2]),
            "relfar": np.ascontiguousarray(np.broadcast_to(rel_table[31:32, 2 * m:2 * m + 2], (128, 2))).astype(np.float32),
            "dsac": C, "Mp": packed[b],
        })
    res = run_spmd(nc, maps)
    o = np.zeros((BATCH * SEQ, 512), NPBF)
    for c in range(NCORES):
        b, m = c // 4, c % 4
        o[b * SEQ:(b + 1) * SEQ, m * 128:(m + 1) * 128] = res[c]["o"]
    return o


def kernel_unfused(x, ln_g, ln_b, rel_table, w_in_ab, w_gate_a, b_gate_a, g_norm_a, w_out_ab,
           w_in_cd, b_forget, w_out_cd, w1_dense, w3_dense, w2_dense,
           w_router, w1_moe, w3_moe, w2_moe):
    f32 = lambda a: np.ascontiguousarray(np.asarray(a, dtype=np.float32))
    x = f32(x)
    rel_table = f32(rel_table)
    big = [f32(w_in_ab), f32(w_in_cd), f32(w_out_ab), f32(w_out_cd), f32(w1_dense), f32(w3_dense), f32(w2_dense),
           f32(w1_moe), f32(w3_moe), f32(w2_moe)]
    (wi_ab, wi_cd, wo_ab, wo_cd, w1d, w3d, w2d, w1m, w3m, w2m) = cast_weights(big)
    del big
    xf = x.reshape(BATCH * SEQ, D)
    for layer in range(DEPTH):
        j = layer // 2
        lnp4 = np.stack([ln_g[layer, 0], ln_b[layer, 0], ln_g[layer, 1], ln_b[layer, 1]]).astype(np.float32)
        if layer % 2 == 0:
            yT, ytb, ytf = run_A(xf, wi_ab[j],
                                 [(0, 256), (256, 256), (1552, 512), (2064, 512), (3088, 512), (3600, 64)],
                                 [(256, 256), (512, 512), (2576, 512)],
                                 [(1024, 512), (3664, 8)],
                                 gate=(1536,), wg=f32(w_gate_a[j]), bg=f32(b_gate_a[j]))
            oa = run_B_gla(yT, ytb, ytf, f32(g_norm_a[j]))
            Msel = run_B_dsa1(yT, ytf)
            ob = run_B_dsa2(yT, ytb, Msel, rel_table)
            del Msel
            o = np.concatenate([oa, ob], axis=1)
            wf = ffn_chunk_layout(w1d[j], w3d[j], w2d[j])
            xf = run_C(o, xf, wo_ab[j], lnp4, wf, 1, 11, 2)
        else:
            yT, ytb, ytf = run_A(xf, wi_cd[j],
                                 [(0, 512), (512, 512), (1544, 512), (2056, 512)],
                                 [(1024, 512), (2568, 512)],
                                 [(1536, 8)])
            o = run_B_cd(yT, ytb, ytf, f32(b_forget[j]), rel_table)
            wf = np.concatenate([ffn_chunk_layout(w1m[j, e], w3m[j, e], w2m[j, e]) for e in range(8)], axis=0)
            xf = run_C(o, xf, wo_cd[j], lnp4, wf, 8, 14, 2, wr=f32(w_router[j]))
    return xf.reshape(BATCH, SEQ, D).astype(np.float32)


I32 = mybir.dt.int32

import os
SKIP = os.environ.get('FZ_SKIP', '')
GROUPS = [[0, 1, 2, 3], [4, 5, 6, 7]]
LK = [512 * (k + 1) for k in range(16)]
MOFF = [128 * sum(LK[:k]) for k in range(16)]
MTOT = 128 * sum(LK)
MPARTS = []
_g = 0
for _k in range(16):
    _r0 = MOFF[_k] // 512
    _n = 128 * LK[_k] // 512
    _halves = 1 if _k < 8 else 2
    for _h in range(_halves):
        _nh = _n // _halves
        MPARTS.append((_k, _h, _r0 + _h * _nh, _nh, _g))
        _g += 4 * _nh
MG = {(p[0], p[1]): p for p in MPARTS}


class KBF(KB):
    def __init__(self):
        super().__init__()
        self.ccsem = self.es.enter_context(self.nc.semaphore("ccsem"))
        self.cccnt = 0
        self.stack = [self.es]

    def sb(self, shape, dt, name="sb"):
        return Buf(self.stack[-1].enter_context(self.nc.sbuf_tensor(self._nm(name), list(shape), dt)))

    def ps(self, shape, dt=F32, name="ps"):
        b = Buf(self.stack[-1].enter_context(self.nc.psum_tensor(self._nm(name), list(shape), dt)))
        b.psum = True
        return b

    def barrier(self):
        evs = []
        for q in ("sp", "act", "pool"):
            for i in range(self.NDS):
                if self.dcnt[q][i] > 0:
                    evs.append((self.dsem[q][i], self.dcnt[q][i], "d%s%d" % (q, i)))
        for e in ("pe", "dve", "act", "pool", "sp"):
            if self.ecnt[e] > 0:
                evs.append((self.esem[e], self.ecnt[e], e))
        if self.cccnt > 0:
            evs.append((self.ccsem, self.cccnt, "cc"))
        for e in ("pe", "dve", "act", "pool", "sp"):
            for ev in evs:
                if ev[2] == e:
                    continue
                self._wait(e, ev)

    def scope(self):
        kb = self

        class _S:
            def __enter__(s):
                st = ExitStack()
                kb.stack.append(st)
                return st

            def __exit__(s, *a):
                kb.barrier()
                st = kb.stack.pop()
                st.close()
                return False

        return _S()

    def dram_tmp(self, name, shape, dt):
        return Buf(self.nc.dram_tensor(name, list(shape), dt).ap())

    def collective(self, kind, src, dst, src_ap=None, dst_ap=None):
        self._deps("pool", [src], [dst])
        sa = src.t if src_ap is None else src_ap
        da = dst.t if dst_ap is None else dst_ap
        ins = self.nc.gpsimd.collective_compute(kind, ALU.bypass, replica_groups=GROUPS,
                                                ins=[sa.opt()], outs=[da.opt()])
        self.cccnt += CC_INC
        ins.then_inc(self.ccsem, CC_INC)
        ev = (self.ccsem, self.cccnt, "cc")
        _kbf_mark(self, ev, [src], [dst])
        return ev


CC_INC = 1


def load_w_cast(kb, dst, src_d, ncols):
    for kc in range(8):
        kb.dma("pool", dst[:, kc, :ncols], src_d[:, kc, :ncols], R=[src_d], Wd=[dst])


def load_xT_chunk(kb, x_, xT_all, c):
    r, t0 = c // 4, (c % 4) * 512
    for cf in range(4):
        row0 = (cf * 4 + r) * 256
        kb.dma("sp", x_[:, 2 * cf:2 * cf + 2, :],
               xT_all[row0:row0 + 256, t0:t0 + 512].rearrange("(k p) t -> p k t", p=128),
               R=[xT_all], Wd=[x_] if cf else (), W=[x_] if cf == 0 else ())


def emit_xT(kb, ax_ps, ident, xtile, tt, stg, xT_loc, xTs_loc):
    nc = kb.nc
    for kc in range(8):
        pt = ax_ps[kc // 4]
        kb.op("pe", lambda: nc.tensor.transpose(pt[:, (kc % 4) * 128:(kc % 4 + 1) * 128],
                                                xtile[:, kc * 128:(kc + 1) * 128], ident[:, :]),
              R=[xtile, ident], W=[pt])
    q = tt % 4
    for hf in range(2):
        src = ax_ps[hf][:, :].rearrange("p (k t) -> p k t", k=4)
        dst = stg[:, hf * 4:(hf + 1) * 4, q * 128:(q + 1) * 128]
        if hf == 0:
            kb.op("dve", lambda: nc.vector.tensor_copy(out=dst, in_=src), R=[ax_ps[hf]], W=[stg])
        else:
            kb.op("act", lambda: nc.scalar.copy(out=dst, in_=src), R=[ax_ps[hf]], W=[stg])
    if q == 3:
        g = tt // 4
        kb.dma("sp", xT_loc[:, g * 512:(g + 1) * 512].rearrange("(k p) t -> p k t", p=128), stg[:, :, :],
               R=[stg], Wd=[xT_loc])
        if xTs_loc is not None:
            for j in range(4):
                kb.dma("sp", xTs_loc[j * D:(j + 1) * D, g * 128:(g + 1) * 128].rearrange("(k p) t -> p k t", p=128),
                       stg[:, :, j * 128:(j + 1) * 128], R=[stg], Wd=[xTs_loc])


def toeplitz_load(kb, TT, E_d, h, q="act"):
    for s in range(128):
        kb.dma(q if s % 2 == 0 else "sp", TT[s:s + 1, :], E_d[h:h + 1, 127 - s:127 - s + 17 * 128], R=[E_d], Wd=[TT])


def build_E_table(kb, ax, rel_d, C_d, E_d):
    nc = kb.nc
    rel = kb.sb([32, 2], F32, "rel")
    Cs = kb.sb([32, NDEL], F32, "Cs")
    Es = kb.sb([2, NDEL], F32, "Es")
    kb.dma("sp", rel[:, :], rel_d[:, :], R=[rel_d], W=[rel])
    kb.dma("sp", Cs[:, :], C_d[:, :], R=[C_d], W=[Cs])
    kb.op("act", lambda: nc.scalar.activation(out=rel[:, :], in_=rel[:, :], func=AF.Exp), R=[rel], W=[rel])
    for c in range((NDEL + 511) // 512):
        w = min(512, NDEL - c * 512)
        pp = ax.S[c % 3]
        kb.op("pe", lambda: nc.tensor.matmul(pp[:2, :w], lhsT=rel[:, :], rhs=Cs[:, c * 512:c * 512 + w],
                                             start=True, stop=True), R=[rel, Cs], W=[pp])
        kb.op("dve", lambda: nc.vector.tensor_copy(out=Es[:, c * 512:c * 512 + w], in_=pp[:2, :w]), R=[pp], W=[Es])
    kb.dma("sp", E_d[:, :], Es[:, :], R=[Es], W=[E_d])


class OTOut:
    def __init__(self, kb, identb, oT_loc, row0, name):
        self.kb, self.identb, self.oT_loc, self.row0 = kb, identb, oT_loc, row0
        self.pt = [kb.ps([128, 1024], BF16, "ptO" + name) for _ in range(1)]
        self.stg = [kb.sb([128, 512], BF16, "stgO" + name) for _ in range(2)]
        self.n = 0

    def put(self, i, ost):
        kb, nc = self.kb, self.kb.nc
        g = i // 4
        st = self.stg[g % 2]
        pt = self.pt[0]
        q = i % 4
        kb.op("pe", lambda: nc.tensor.transpose(pt[:, q * 128:(q + 1) * 128], ost[:, :], self.identb[:, :]),
              R=[ost, self.identb], W=[pt])
        if q == 3:
            kb.op("act", lambda: nc.scalar.copy(out=st[:, :], in_=pt[:, :512]), R=[pt], W=[st])
            rank, grp = g // 4, g % 4
            r0 = (rank * 4 + grp) * 256 + self.row0
            kb.dma("sp", self.oT_loc[r0:r0 + 128, :], st[:, :], R=[st], Wd=[self.oT_loc])


def phase_cd(kb, L, xT_all, oT_loc, cst):
    nc = kb.nc
    with kb.scope():
        wcd_d = L["wcd"]
        NCOL = 770
        w = kb.sb([128, 8, NCOL], BF16, "wcd")
        load_w_cast(kb, w, wcd_d, NCOL)
        tri = kb.sb([128, 128], F32, "tri")
        ones = kb.sb([128, 128], F32, "ones")
        identb = kb.sb([128, 128], BF16, "identb")
        kb.dma("sp", tri[:, :], cst["tri"][:, :], R=[cst["tri"]], W=[tri])
        kb.dma("sp", identb[:, :], cst["identb"][:, :], R=[cst["identb"]], W=[identb])
        kb.op("dve", lambda: nc.vector.memset(ones[:, :], 1.0), W=[ones])
        ax = AttnCtx(kb, n_s=4, n_o=2, grouped=True)
        E_d = L["Escr"]
        with kb.scope():
            build_E_table(kb, ax, L["rel2"], cst["dilc"], E_d)
        TT = [kb.sb([128, 17 * 128], F32, "TT") for _ in range(2)]
        for h in range(2):
            if "toep" in SKIP:
                kb.op("dve", lambda: nc.vector.memset(TT[h][:, :], 1.0), W=[TT[h]])
            else:
                toeplitz_load(kb, TT[h], E_d, h)
        qc = kb.sb([128, SEQ], BF16, "qc")
        kc_ = kb.sb([128, SEQ], BF16, "kc")
        qd = kb.sb([128, SEQ], BF16, "qd")
        kd = kb.sb([128, SEQ], BF16, "kd")
        Vc = kb.sb([128, NQB, 2, 65], BF16, "Vc")
        Vd = kb.sb([128, NQB, 2, 65], BF16, "Vd")
        fc = kb.sb([128, NQB, 2], F32, "fc")
        kb.op("pool", lambda: nc.gpsimd.memset(Vc[:, :, :, :], 1.0), W=[Vc])
        kb.op("pool", lambda: nc.gpsimd.memset(Vd[:, :, :, :], 1.0), W=[Vd])
        xc = [kb.sb([128, 8, 512], BF16, "xc") for _ in range(2)]
        n_ev = 0
        passes = [(True, True)] if "twopass" not in SKIP else [(True, False), (False, True)]
        NCH = int(os.environ.get("FZ_NCH", "16"))
        for c2 in range((NCH if "proj" not in SKIP else 0) * len(passes)):
            c = c2 % NCH
            do_fm, do_tm = passes[c2 // NCH]
            x_ = xc[c % 2]
            load_xT_chunk(kb, x_, xT_all, c)
            for bi, dst in enumerate((qc, kc_, qd, kd) if ("projfm" not in SKIP and do_fm) else ()):
                pp = ax.S[n_ev % 4]
                for k8 in range(8):
                    kb.op("pe", lambda: nc.tensor.matmul(pp[:, :], lhsT=w[:, k8, bi * 128:(bi + 1) * 128],
                                                         rhs=x_[:, k8, :], start=(k8 == 0), stop=(k8 == 7)),
                          R=[w, x_], W=[pp])
                d_ = dst[:, c * 512:(c + 1) * 512]
                if n_ev % 2 == 0 or "fmdve" in SKIP:
                    kb.op("dve", lambda: nc.vector.tensor_copy(out=d_, in_=pp[:, :]), R=[pp], W=[dst])
                else:
                    kb.op("act", lambda: nc.scalar.copy(out=d_, in_=pp[:, :]), R=[pp], W=[dst])
                n_ev += 1
            for t4 in range(4 if ("projtm" not in SKIP and do_tm) else 0):
                j = c * 4 + t4
                pp = ax.S[n_ev % 4]
                n_ev += 1
                for k8 in range(8):
                    kb.op("pe", lambda: nc.tensor.matmul(pp[:, :258], lhsT=x_[:, k8, t4 * 128:(t4 + 1) * 128],
                                                         rhs=w[:, k8, 512:770], start=(k8 == 0), stop=(k8 == 7)),
                          R=[w, x_], W=[pp])
                kb.op("dve", lambda: nc.vector.tensor_copy(out=Vc[:, j, :, 0:64],
                                                           in_=pp[:, 0:128].rearrange("p (h d) -> p h d", h=2)),
                      R=[pp], W=[Vc])
                if "vddve" in SKIP:
                    kb.op("dve", lambda: nc.vector.tensor_copy(out=Vd[:, j, :, 0:64],
                                                               in_=pp[:, 128:256].rearrange("p (h d) -> p h d", h=2)),
                          R=[pp], W=[Vd])
                else:
                    kb.op("act", lambda: nc.scalar.copy(out=Vd[:, j, :, 0:64],
                                                        in_=pp[:, 128:256].rearrange("p (h d) -> p h d", h=2)),
                          R=[pp], W=[Vd])
                kb.op("dve", lambda: nc.vector.tensor_copy(out=fc[:, j, :], in_=pp[:, 256:258]), R=[pp], W=[fc])
        bf = kb.sb([128, 2], F32, "bf")
        lf = kb.sb([128, 2, NQB], F32, "lf")
        kb.dma("sp", bf[:, :], L["bf"][:, :], R=[L["bf"]], W=[bf])
        for h in range(2):
            kb.op("dve", lambda: nc.vector.tensor_scalar(out=lf[:, h, :], in0=fc[:, :, h], scalar1=bf[:, h:h + 1],
                                                         scalar2=None, op0=ALU.add), R=[fc, bf], W=[lf])
        kb.op("act", lambda: nc.scalar.activation(out=lf[:, :, :], in_=lf[:, :, :], func=AF.Exp, scale=-1.0),
              R=[lf], W=[lf])
        kb.op("act", lambda: nc.scalar.activation(out=lf[:, :, :], in_=lf[:, :, :], func=AF.Ln, bias=1.0),
              R=[lf], W=[lf])
        kb.op("dve", lambda: nc.vector.tensor_scalar(out=lf[:, :, :], in0=lf[:, :, :], scalar1=-1.0, scalar2=None,
                                                     op0=ALU.mult), R=[lf], W=[lf])
        lf2 = lf[:, :, :].rearrange("p h j -> p (h j)")
        p1, p2 = ax.O[0], ax.O[1]
        kb.op("pe", lambda: nc.tensor.matmul(p1[:, :128], lhsT=tri[:, :], rhs=lf2, start=True, stop=True),
              R=[tri, lf], W=[p1])
        kb.op("pe", lambda: nc.tensor.matmul(p2[:, :128], lhsT=ones[:, :], rhs=lf2, start=True, stop=True),
              R=[ones, lf], W=[p2])
        tot = kb.sb([128, 2, NQB], F32, "tot")
        carry = kb.sb([128, 2, NQB], F32, "carry")
        negF = kb.sb([128, 2, NQB], F32, "negF")
        kb.op("dve", lambda: nc.vector.tensor_copy(out=tot[:, :, :].rearrange("p h j -> p (h j)"), in_=p2[:, :128]),
              R=[p2], W=[tot])
        for h in range(2):
            kb.op("dve", lambda: nc.vector.tensor_tensor_scan(out=carry[:, h, :], data0=ones[:, :NQB],
                                                              data1=tot[:, h, :], initial=0.0, op0=ALU.mult,
                                                              op1=ALU.add), R=[ones, tot], W=[carry])
        kb.op("dve", lambda: nc.vector.tensor_tensor(out=carry[:, :, :], in0=carry[:, :, :], in1=tot[:, :, :],
                                                     op=ALU.subtract), R=[carry, tot], W=[carry])
        kb.op("dve", lambda: nc.vector.tensor_tensor(out=negF[:, :, :].rearrange("p h j -> p (h j)"), in0=p1[:, :128],
                                                     in1=carry[:, :, :].rearrange("p h j -> p (h j)"), op=ALU.add),
              R=[p1, carry], W=[negF])
        kb.op("dve", lambda: nc.vector.tensor_scalar(out=negF[:, :, :], in0=negF[:, :, :], scalar1=-1.0, scalar2=None,
                                                     op0=ALU.mult), R=[negF], W=[negF])
        outc = OTOut(kb, identb, oT_loc, 0, "c")
        outd = OTOut(kb, identb, oT_loc, 128, "d")
        ostd = [kb.sb([128, 128], BF16, "ostd") for _ in range(2)]
        ostc = [kb.sb([128, 128], BF16, "ostc") for _ in range(2)]
        Bi = [kb.sb([128, NQB], F32, "Bi") for _ in range(3)]
        nb = 0
        for i in range(NQB if "attn" not in SKIP else 0):
            od = ostd[i % 2]
            for h in range(2):
                jdesc = list(range(i, max(0, i - 16) - 1, -1))
                groups = []
                for a in range(0, len(jdesc), 4):
                    js = jdesc[a:a + 4]
                    d0 = i - js[0]
                    groups.append({"js": js, "dve_mask": (TT[h], TT[h][:, d0 * 128:(d0 + len(js)) * 128])})
                attn_qgroup(ax, qd, kd, Vd, h, i, groups, od, h * 64,
                            after=(None if h == 0 else (lambda i=i, od=od: outd.put(i, od))))
            oc = ostc[i % 2]
            for h in range(2):
                B = Bi[nb % 3]
                nb += 1
                kb.op("dve", lambda: nc.vector.tensor_scalar(out=B[:, :i + 1], in0=negF[:, h, :i + 1],
                                                             scalar1=carry[:, h, i:i + 1], scalar2=None, op0=ALU.add),
                      R=[negF, carry], W=[B])
                attn_qblock(ax, qc, kc_, Vc, h, i, list(range(0, i + 1)),
                            lambda j, B=B: (B, B[:, j:j + 1]),
                            lambda j, i=i: ((tri, tri[:, :]) if j == i else None), oc, h * 64,
                            after=(None if h == 0 else (lambda i=i, oc=oc: outc.put(i, oc))))
        attn_flush(ax)


def phase_gla(kb, L, xT_all, oT_loc, cst):
    nc = kb.nc
    with kb.scope():
        tri = kb.sb([128, 128], F32, "tri")
        identb = kb.sb([128, 128], BF16, "identb")
        gn = kb.sb([128, 128], F32, "gn")
        eps = kb.sb([128, 1], F32, "eps")
        kb.dma("sp", tri[:, :], cst["tri"][:, :], R=[cst["tri"]], W=[tri])
        kb.dma("sp", identb[:, :], cst["identb"][:, :], R=[cst["identb"]], W=[identb])
        kb.dma("sp", gn[:, :], L["gn"][:, :], R=[L["gn"]], W=[gn])
        kb.op("dve", lambda: nc.vector.memset(eps[:, :], LN_EPS), W=[eps])
        qT = kb.sb([64, SEQ], BF16, "qT")
        kT = kb.sb([64, SEQ], BF16, "kT")
        ktm = kb.sb([128, NQB, 64], BF16, "ktm")
        v = kb.sb([128, NQB, 128], BF16, "v")
        r = kb.sb([128, NQB, 128], F32, "r")
        g = kb.sb([128, NQB, 64], F32, "g")
        PG = [kb.ps([128, 512], F32, "PG")]
        PGT = [kb.ps([128, 512], F32, "PGT")]
        PA = [kb.ps([128, 512], F32, "PA") for _ in range(2)]
        PO = kb.ps([128, 512], F32, "PO")
        PU = kb.ps([128, 512], F32, "PU")
        out = OTOut(kb, identb, oT_loc, 0, "g")
        with kb.scope():
            NCOL = 464
            w = kb.sb([128, 8, NCOL], BF16, "wgl")
            load_w_cast(kb, w, L["wgl"], NCOL)
            wg = kb.sb([16, 64], F32, "wg")
            bg = kb.sb([128, 64], F32, "bg")
            kb.dma("sp", wg[:, :], L["wg"][:, :], R=[L["wg"]], W=[wg])
            kb.dma("sp", bg[:, :], L["bg"][:, :], R=[L["bg"]], W=[bg])
            xc = [kb.sb([128, 8, 512], BF16, "xc") for _ in range(2)]
            gaT = [kb.sb([16, 512], F32, "gaT") for _ in range(2)]
            zt = [kb.sb([128, 64], F32, "zt") for _ in range(2)]
            pr = [PA[0], PA[1], PO]
            n_ev = 0
            for c in range(16):
                x_ = xc[c % 2]
                ga_ = gaT[c % 2]
                load_xT_chunk(kb, x_, xT_all, c)
                for (c0, ncol, dst) in ((0, 64, qT), (64, 64, kT), (128, 16, ga_)):
                    pp = pr[n_ev % 3]
                    for k8 in range(8):
                        kb.op("pe", lambda: nc.tensor.matmul(pp[:ncol, :], lhsT=w[:, k8, c0:c0 + ncol], rhs=x_[:, k8, :],
                                                             start=(k8 == 0), stop=(k8 == 7)), R=[w, x_], W=[pp])
                    d_ = dst[:, c * 512:(c + 1) * 512] if dst is not ga_ else ga_[:, :]
                    if n_ev % 2 == 0:
                        kb.op("dve", lambda: nc.vector.tensor_copy(out=d_, in_=pp[:ncol, :]), R=[pp], W=[dst])
                    else:
                        kb.op("act", lambda: nc.scalar.copy(out=d_, in_=pp[:ncol, :]), R=[pp], W=[dst])
                    n_ev += 1
                for t4 in range(4):
                    j = c * 4 + t4
                    pp = pr[n_ev % 3]
                    n_ev += 1
                    for k8 in range(8):
                        kb.op("pe", lambda: nc.tensor.matmul(pp[:, :320], lhsT=x_[:, k8, t4 * 128:(t4 + 1) * 128],
                                                             rhs=w[:, k8, 144:464], start=(k8 == 0), stop=(k8 == 7)),
                              R=[w, x_], W=[pp])
                    kb.op("dve", lambda: nc.vector.tensor_copy(out=ktm[:, j, :], in_=pp[:, 0:64]), R=[pp], W=[ktm])
                    kb.op("dve", lambda: nc.vector.tensor_copy(out=v[:, j, :], in_=pp[:, 64:192]), R=[pp], W=[v])
                    kb.op("act", lambda: nc.scalar.activation(out=r[:, j, :], in_=pp[:, 192:320], func=AF.Silu),
                          R=[pp], W=[r])
                    pq = pr[n_ev % 3]
                    n_ev += 1
                    z = zt[j % 2]
                    kb.op("pe", lambda: nc.tensor.matmul(pq[:, :64], lhsT=ga_[:, t4 * 128:(t4 + 1) * 128], rhs=wg[:, :],
                                                         start=True, stop=True), R=[ga_, wg], W=[pq])
                    kb.op("dve", lambda: nc.vector.tensor_tensor(out=z[:, :], in0=pq[:, :64], in1=bg[:, :], op=ALU.add),
                          R=[pq, bg], W=[z])
                    kb.op("act", lambda: nc.scalar.activation(out=z[:, :], in_=z[:, :], func=AF.Exp, scale=-1.0),
                          R=[z], W=[z])
                    kb.op("act", lambda: nc.scalar.activation(out=z[:, :], in_=z[:, :], func=AF.Ln, bias=1.0),
                          R=[z], W=[z])
                    kb.op("dve", lambda: nc.vector.tensor_scalar(out=g[:, j, :], in0=z[:, :], scalar1=-1.0 / 16.0,
                                                                 scalar2=None, op0=ALU.mult), R=[z], W=[g])
        eGT = [kb.sb([64, 128], F32, "eGT") for _ in range(2)]
        enGT = [kb.sb([64, 128], F32, "enGT") for _ in range(2)]
        enG = [kb.sb([128, 64], F32, "enG") for _ in range(2)]
        qgT = [kb.sb([64, 128], BF16, "qgT") for _ in range(2)]
        kgT = [kb.sb([64, 128], BF16, "kgT") for _ in range(2)]
        kg = [kb.sb([128, 64], BF16, "kg") for _ in range(2)]
        Am = [kb.sb([128, 128], BF16, "Am") for _ in range(2)]
        S32 = kb.sb([64, 128], F32, "S32")
        Sbf = kb.sb([64, 128], BF16, "Sbf")
        st6 = [kb.sb([128, 6], F32, "st6") for _ in range(2)]
        mv = [kb.sb([128, 4], F32, "mv") for _ in range(2)]
        of = [kb.sb([128, 128], F32, "of") for _ in range(2)]
        ost = [kb.sb([128, 128], BF16, "ost") for _ in range(2)]
        for c in range(NQB):
            p = c % 2
            cs = slice(c * 128, (c + 1) * 128)
            kb.op("pe", lambda: nc.tensor.matmul(PG[0][:, :64], lhsT=tri[:, :], rhs=g[:, c, :], start=True, stop=True),
                  R=[tri, g], W=[PG[0]])
            kb.op("pe", lambda: nc.tensor.matmul(PGT[0][:64, :128], lhsT=g[:, c, :], rhs=tri[:, :], start=True, stop=True),
                  R=[tri, g], W=[PGT[0]])
            kb.op("act", lambda: nc.scalar.activation(out=eGT[p][:, :], in_=PGT[0][:64, :128], func=AF.Exp),
                  R=[PGT[0]], W=[eGT[p]])
            kb.op("act", lambda: nc.scalar.activation(out=enGT[p][:, :], in_=PGT[0][:64, :128], func=AF.Exp, scale=-1.0),
                  R=[PGT[0]], W=[enGT[p]])
            kb.op("act", lambda: nc.scalar.activation(out=enG[p][:, :], in_=PG[0][:, :64], func=AF.Exp, scale=-1.0),
                  R=[PG[0]], W=[enG[p]])
            kb.op("dve", lambda: nc.vector.scalar_tensor_tensor(out=qgT[p][:, :], in0=qT[:, cs], scalar=0.125,
                                                                in1=eGT[p][:, :], op0=ALU.mult, op1=ALU.mult),
                  R=[qT, eGT[p]], W=[qgT[p]])
            kb.op("dve", lambda: nc.vector.tensor_tensor(out=kgT[p][:, :], in0=kT[:, cs], in1=enGT[p][:, :], op=ALU.mult),
                  R=[kT, enGT[p]], W=[kgT[p]])
            kb.op("dve", lambda: nc.vector.tensor_tensor(out=kg[p][:, :], in0=ktm[:, c, :], in1=enG[p][:, :], op=ALU.mult),
                  R=[ktm, enG[p]], W=[kg[p]])
            kb.op("pe", lambda: nc.tensor.matmul(PA[p][:, :128], lhsT=kgT[p][:, :], rhs=qgT[p][:, :], start=True, stop=True),
                  R=[kgT[p], qgT[p]], W=[PA[p]])
            kb.op("dve", lambda: nc.vector.tensor_tensor(out=Am[p][:, :], in0=PA[p][:, :128], in1=tri[:, :], op=ALU.mult),
                  R=[PA[p], tri], W=[Am[p]])
            kb.op("pe", lambda: nc.tensor.matmul(PO[:, :128], lhsT=Am[p][:, :], rhs=v[:, c, :], start=True, stop=(c == 0)),
                  R=[Am[p], v], W=[PO])
            if c > 0:
                kb.op("pe", lambda: nc.tensor.matmul(PO[:, :128], lhsT=qgT[p][:, :], rhs=Sbf[:, :], start=False, stop=True),
                      R=[qgT[p], Sbf], W=[PO])
            if c < NQB - 1:
                kb.op("pe", lambda: nc.tensor.matmul(PU[:64, :128], lhsT=kg[p][:, :], rhs=v[:, c, :], start=True, stop=True),
                      R=[kg[p], v], W=[PU])
                eGl = eGT[p][:, 127:128]
                if c == 0:
                    kb.op("dve", lambda: nc.vector.tensor_scalar(out=S32[:, :], in0=PU[:64, :128], scalar1=eGl,
                                                                 scalar2=None, op0=ALU.mult), R=[PU, eGT[p]], W=[S32])
                else:
                    kb.op("dve", lambda: nc.vector.tensor_scalar(out=S32[:, :], in0=S32[:, :], scalar1=eGl, scalar2=None,
                                                                 op0=ALU.mult), R=[S32, eGT[p]], W=[S32])
                    kb.op("dve", lambda: nc.vector.scalar_tensor_tensor(out=S32[:, :], in0=PU[:64, :128], scalar=eGl,
                                                                        in1=S32[:, :], op0=ALU.mult, op1=ALU.add),
                          R=[PU, eGT[p], S32], W=[S32])
                kb.op("dve", lambda: nc.vector.tensor_copy(out=Sbf[:, :], in_=S32[:, :]), R=[S32], W=[Sbf])
            kb.op("dve", lambda: nc.vector.bn_stats(out=st6[p][:, :], in_=PO[:, :128]), R=[PO], W=[st6[p]])
            kb.op("dve", lambda: nc.vector.bn_aggr(out=mv[p][:, 0:2], in_=st6[p][:, :]), R=[st6[p]], W=[mv[p]])
            kb.op("dve", lambda: nc.vector.scalar_tensor_tensor(out=mv[p][:, 2:3], in0=mv[p][:, 0:1], scalar=mv[p][:, 0:1],
                                                                in1=mv[p][:, 1:2], op0=ALU.mult, op1=ALU.add),
                  R=[mv[p]], W=[mv[p]])
            kb.op("act", lambda: nc.scalar.activation(out=mv[p][:, 3:4], in_=mv[p][:, 2:3], func=AF.Ln, bias=eps[:, 0:1]),
                  R=[mv[p], eps], W=[mv[p]])
            kb.op("act", lambda: nc.scalar.activation(out=mv[p][:, 3:4], in_=mv[p][:, 3:4], func=AF.Exp, scale=-0.5),
                  R=[mv[p]], W=[mv[p]])
            kb.op("dve", lambda: nc.vector.scalar_tensor_tensor(out=of[p][:, :], in0=PO[:, :128], scalar=mv[p][:, 3:4],
                                                                in1=gn[:, :], op0=ALU.mult, op1=ALU.mult),
                  R=[PO, mv[p], gn], W=[of[p]])
            kb.op("pool", lambda: nc.gpsimd.tensor_tensor(out=ost[p][:, :], in0=of[p][:, :], in1=r[:, c, :], op=ALU.mult),
                  R=[of[p], r], W=[ost[p]])
            out.put(c, ost[p])


def _kbf_deps(self, e, R, W, Wd=()):
    evs = []
    for b in R:
        evs.extend(b.wl())
        if getattr(b, "psum", False):
            evs.extend(x for x in b.r if x[2] != e)
    for b in W:
        evs.extend(b.wl())
        evs.extend(b.r)
    for b in Wd:
        evs.extend(b.r)
        evs.extend(getattr(b, "w_excl", []))
    best = {}
    for ev in evs:
        k = ev[2]
        if e == "pe" and k == "pe":
            continue
        if k not in best or best[k][1] < ev[1]:
            best[k] = ev
    for ev in best.values():
        self._wait(e, ev)


def _buf_wl(self):
    if self.w is None:
        return []
    return self.w if isinstance(self.w, list) else [self.w]


Buf.wl = _buf_wl


def _kbf_mark(self, ev, R, W, Wd=()):
    KB._mark(self, ev, R, W)
    for b in W:
        b.w = [ev]
        b.w_excl = [ev]
    for b in Wd:
        cur = b.wl()
        cur = [x for x in cur if x[2] != ev[2]] + [ev]
        b.w = cur
        b.r = []


def _kbf_dma(self, q, out, in_, R=(), W=(), Wd=(), **kw):
    _kbf_deps(self, q, R, W, Wd)
    i = self.dnext[q]
    self.dnext[q] = (i + 1) % self.NDS
    key = "d%s%d" % (q, i)
    if self.dcnt[q][i] > 0:
        self._wait(q, (self.dsem[q][i], self.dcnt[q][i], key))
    self.dcnt[q][i] += 16
    self.eng[q].dma_start(out=out, in_=in_, **kw).then_inc(self.dsem[q][i], 16)
    ev = (self.dsem[q][i], self.dcnt[q][i], key)
    _kbf_mark(self, ev, R, W, Wd)
    return ev


def _kbf_op(self, e, fn, R=(), W=()):
    _kbf_deps(self, e, R, W)
    ins = fn()
    self.ecnt[e] += 1
    ins.then_inc(self.esem[e], 1)
    ev = (self.esem[e], self.ecnt[e], e)
    _kbf_mark(self, ev, R, W)
    return ev


def _kbf_idma(self, out, in_, idx_ap, R=(), W=(), Wd=()):
    q = "pool"
    _kbf_deps(self, q, R, W, Wd)
    i = self.dnext[q]
    self.dnext[q] = (i + 1) % self.NDS
    key = "d%s%d" % (q, i)
    if self.dcnt[q][i] > 0:
        self._wait(q, (self.dsem[q][i], self.dcnt[q][i], key))
    self.dcnt[q][i] += 16
    self.nc.gpsimd.indirect_dma_start(out=out, out_offset=None, in_=in_,
                                      in_offset=bass.IndirectOffsetOnAxis(ap=idx_ap, axis=0)
                                      ).then_inc(self.dsem[q][i], 16)
    ev = (self.dsem[q][i], self.dcnt[q][i], key)
    _kbf_mark(self, ev, R, W, Wd)
    return ev


KBF.idma = _kbf_idma
KBF._deps = lambda self, e, R, W: _kbf_deps(self, e, R, W)
KBF.dma = _kbf_dma
KBF.op = _kbf_op


def phase_dsa1(kb, L, xT_all, xTs_all, M_loc, M_all, cst, act_split=True):
    nc = kb.nc
    NK = 16
    with kb.scope():
        qiT = kb.sb([64, 8, NK * 128], BF16, "qiT")
        kiT = kb.sb([64, SEQ], BF16, "kiT")
        wi = kb.sb([128, NK, 8], F32, "wi")
        absw = kb.sb([128, NK, 8], F32, "absw")
        sgn = kb.sb([128, NK, 8], F32, "sgn")
        cm = kb.sb([128, 512], F32, "cm")
        idb = kb.sb([128, 128], BF16, "idb")
        stp = kb.sb([128, NBIS], F32, "stp")
        kb.dma("sp", cm[:, :], cst["cmask"][:, :], R=[cst["cmask"]], W=[cm])
        kb.dma("sp", idb[:, :], cst["identb"][:, :], R=[cst["identb"]], W=[idb])
        kb.dma("sp", stp[:, :], cst["steps"][:, :], R=[cst["steps"]], W=[stp])
        idxs = kb.sb([128, 32], I32, "idxs")
        kb.dma("sp", idxs[:, :], cst["idx_s"][:, :], R=[cst["idx_s"]], W=[idxs])
        PS = [kb.ps([128, 512], F32, "PS") for _ in range(3)]
        PT = [kb.ps([128, 1024], BF16, "PT") for _ in range(2)]
        with kb.scope():
            NCOL = 584
            w = kb.sb([128, 8, NCOL], BF16, "wd1")
            load_w_cast(kb, w, L["wd1"], NCOL)
            xc = [kb.sb([128, 8, 512], BF16, "xc") for _ in range(2)]
            n_ev = 0
            for c in range(16):
                x_ = xc[c % 2]
                load_xT_chunk(kb, x_, xT_all, c)
                pp = PS[n_ev % 3]
                n_ev += 1
                for k8 in range(8):
                    kb.op("pe", lambda: nc.tensor.matmul(pp[:64, :], lhsT=w[:, k8, 512:576], rhs=x_[:, k8, :],
                                                         start=(k8 == 0), stop=(k8 == 7)), R=[w, x_], W=[pp])
                kb.op("dve", lambda: nc.vector.tensor_copy(out=kiT[:, c * 512:(c + 1) * 512], in_=pp[:64, :]),
                      R=[pp], W=[kiT])
            for r in range(4):
                x_ = xc[r % 2]
                for k8 in range(8):
                    kb.idma(x_[:, k8, :], xTs_all[:, :], idxs[:, r * 8 + k8:r * 8 + k8 + 1], R=[xTs_all, idxs],
                            Wd=[x_] if k8 else (), W=[x_] if k8 == 0 else ())
                for hi in range(8):
                    pp = PS[n_ev % 3]
                    n_ev += 1
                    for k8 in range(8):
                        kb.op("pe", lambda: nc.tensor.matmul(pp[:64, :], lhsT=w[:, k8, hi * 64:(hi + 1) * 64],
                                                             rhs=x_[:, k8, :], start=(k8 == 0), stop=(k8 == 7)),
                              R=[w, x_], W=[pp])
                    if hi % 2 == 0:
                        kb.op("dve", lambda: nc.vector.tensor_copy(out=qiT[:, hi, r * 512:(r + 1) * 512], in_=pp[:64, :]),
                              R=[pp], W=[qiT])
                    else:
                        kb.op("act", lambda: nc.scalar.copy(out=qiT[:, hi, r * 512:(r + 1) * 512], in_=pp[:64, :]),
                              R=[pp], W=[qiT])
                for t4 in range(4):
                    pp = PS[n_ev % 3]
                    n_ev += 1
                    for k8 in range(8):
                        kb.op("pe", lambda: nc.tensor.matmul(pp[:, :8], lhsT=x_[:, k8, t4 * 128:(t4 + 1) * 128],
                                                             rhs=w[:, k8, 576:584], start=(k8 == 0), stop=(k8 == 7)),
                              R=[w, x_], W=[pp])
                    kb.op("dve", lambda: nc.vector.tensor_copy(out=wi[:, r * 4 + t4, :], in_=pp[:, :8]), R=[pp], W=[wi])
        kb.op("act", lambda: nc.scalar.activation(out=absw[:, :, :], in_=wi[:, :, :], func=AF.Abs), R=[wi], W=[absw])
        kb.op("act", lambda: nc.scalar.activation(out=sgn[:, :, :], in_=wi[:, :, :], func=AF.Sign), R=[wi], W=[sgn])
        def write_mask(k, mt, Lk):
            dst = _AP(tensor=M_loc.t.tensor, offset=MOFF[k], ap=[[Lk, 128], [1, Lk]])
            kb.dma("sp", dst, mt[:, :Lk], R=[mt], Wd=[M_loc])
            for (k2, hf, lrow, nrow, grow) in MPARTS:
                if k2 == k:
                    kb.collective("AllGather", M_loc, M_all, M_loc[lrow:lrow + nrow, :],
                                  M_all[grow:grow + 4 * nrow, :])

        dsa1_body(kb, qiT, kiT, absw, sgn, cm, idb, stp, PS, PT, write_mask, act_split)


def phase_dsa2(kb, L, xT_all, M_all, oT_loc, cst):
    nc = kb.nc
    with kb.scope():
        identb = kb.sb([128, 128], BF16, "identb")
        kb.dma("sp", identb[:, :], cst["identb"][:, :], R=[cst["identb"]], W=[identb])
        rf = kb.sb([128, 2], F32, "rf")
        kb.dma("sp", rf[:, :], L["relfar"][:, :], R=[L["relfar"]], W=[rf])
        ax = AttnCtx(kb, n_s=4, n_o=2, grouped=True)
        E_d = L["Escr"]
        with kb.scope():
            build_E_table(kb, ax, L["rel2"], cst["dsac"], E_d)
        TT = [kb.sb([128, 17 * 128], F32, "TT") for _ in range(2)]
        for h in range(2):
            toeplitz_load(kb, TT[h], E_d, h)
        q = kb.sb([128, SEQ], BF16, "q")
        k = kb.sb([128, SEQ], BF16, "k")
        V = kb.sb([128, NQB, 2, 65], BF16, "V")
        kb.op("pool", lambda: nc.gpsimd.memset(V[:, :, :, :], 1.0), W=[V])
        with kb.scope():
            NCOL = 384
            w = kb.sb([128, 8, NCOL], BF16, "wd2")
            load_w_cast(kb, w, L["wd2"], NCOL)
            xc = [kb.sb([128, 8, 512], BF16, "xc") for _ in range(2)]
            n_ev = 0
            for c in range(16):
                x_ = xc[c % 2]
                load_xT_chunk(kb, x_, xT_all, c)
                for bi, dst in enumerate((q, k)):
                    pp = ax.S[n_ev % 3]
                    for k8 in range(8):
                        kb.op("pe", lambda: nc.tensor.matmul(pp[:, :], lhsT=w[:, k8, bi * 128:(bi + 1) * 128],
                                                             rhs=x_[:, k8, :], start=(k8 == 0), stop=(k8 == 7)),
                              R=[w, x_], W=[pp])
                    d_ = dst[:, c * 512:(c + 1) * 512]
                    if n_ev % 2 == 0:
                        kb.op("dve", lambda: nc.vector.tensor_copy(out=d_, in_=pp[:, :]), R=[pp], W=[dst])
                    else:
                        kb.op("act", lambda: nc.scalar.copy(out=d_, in_=pp[:, :]), R=[pp], W=[dst])
                    n_ev += 1
                for t4 in range(4):
                    j = c * 4 + t4
                    pp = ax.S[n_ev % 3]
                    n_ev += 1
                    for k8 in range(8):
                        kb.op("pe", lambda: nc.tensor.matmul(pp[:, :128], lhsT=x_[:, k8, t4 * 128:(t4 + 1) * 128],
                                                             rhs=w[:, k8, 256:384], start=(k8 == 0), stop=(k8 == 7)),
                              R=[w, x_], W=[pp])
                    kb.op("dve" if t4 % 2 else "act",
                          (lambda: nc.vector.tensor_copy(out=V[:, j, :, 0:64],
                                                         in_=pp[:, 0:128].rearrange("p (h d) -> p h d", h=2)))
                          if t4 % 2 else
                          (lambda: nc.scalar.copy(out=V[:, j, :, 0:64],
                                                  in_=pp[:, 0:128].rearrange("p (h d) -> p h d", h=2))),
                          R=[pp], W=[V])
        Ms = [kb.sb([128, SEQ], BF16, "Ms") for _ in range(2)]
        ost = [kb.sb([128, 128], BF16, "ost") for _ in range(2)]
        out = OTOut(kb, identb, oT_loc, 128, "b")
        for i in range(NQB):
            ms = Ms[i % 2]
            W_ = (i + 1) * 128
            r_, k_ = i % 4, i // 4
            nh = 1 if k_ < 8 else 2
            for hf in range(nh):
                (_, _, lrow, nrow, grow) = MG[(k_, hf)]
                ns = 128 // nh
                src = _AP(tensor=M_all.t.tensor, offset=(grow + r_ * nrow) * 512, ap=[[LK[k_], ns], [1, W_]])
                kb.dma("sp", ms[hf * ns:(hf + 1) * ns, :W_], src, R=[M_all],
                       Wd=[ms] if hf else (), W=[ms] if hf == 0 else ())
            o = ost[i % 2]
            for h in range(2):
                groups = []
                jn0 = max(0, i - 16)
                for a in range(0, jn0, 4):
                    js = list(range(a, min(a + 4, jn0)))
                    groups.append({"js": js, "bias": (rf, rf[:, h:h + 1]),
                                   "dve_mask": (ms, ms[:, js[0] * 128:(js[-1] + 1) * 128])})
                for a in range(jn0, i + 1, 4):
                    js = list(range(a, min(a + 4, i + 1)))
                    groups.append({"js": js,
                                   "pool_masks": [(TT[h], TT[h][:, (i - j) * 128:(i - j + 1) * 128]) for j in js],
                                   "dve_mask": (ms, ms[:, js[0] * 128:(js[-1] + 1) * 128])})
                attn_qgroup(ax, q, k, V, h, i, groups, o, h * 64,
                            after=(None if h == 0 else (lambda i=i, o=o: out.put(i, o))))
        attn_flush(ax)


def phase_c(kb, L, moe, oT_all, x_src, x_dst, xT_loc, xTs_loc, cst, last):
    nc = kb.nc
    n_exp, nfu, n_units = (8, 14, 2) if moe else (1, 11, 2)
    TG = 512
    NG = TPC // TG
    with kb.scope():
        wf_d = L["wf"]
        ident = kb.sb([128, 128], F32, "ident")
        wo = kb.sb([128, 8, D], BF16, "wo")
        lnp = kb.sb([128, 4, D], F32, "lnp")
        kb.eps_col = kb.sb([128, 1], F32, "eps")
        kb.op("dve", lambda: nc.vector.memset(kb.eps_col[:, :], LN_EPS), W=[kb.eps_col])
        kb.dma("sp", ident[:, :], cst["ident"][:, :], R=[cst["ident"]], W=[ident])
        load_w_cast(kb, wo, L["wo"], D)
        kb.dma("sp", lnp[:, :, :], L["lnp"][:, :, :], R=[L["lnp"]], W=[lnp])
        idxo = kb.sb([128, 32], I32, "idxo")
        kb.dma("sp", idxo[:, :], cst["idx_o"][:, :], R=[cst["idx_o"]], W=[idxo])
        if moe:
            wr = kb.sb([128, 8, 8], F32, "wr")
            kb.dma("sp", wr[:, :, :], L["wr"][:, :, :], R=[L["wr"]], W=[wr])
            x1T32 = kb.sb([128, 8, 128], F32, "x1T32")
            comb = [kb.sb([128, 8], F32, "comb") for _ in range(4)]
            rt = kb.sb([128, 40], F32, "rt")
        oT = [kb.sb([128, 8, TG], BF16, "oT") for _ in range(1)]
        xt = [kb.sb([128, D], F32, "xt") for _ in range(2)]
        h = kb.sb([128, D], F32, "h")
        x1g = [kb.sb([128, D], F32, "x1g") for _ in range(4)]
        x1T = kb.sb([128, 8, TG], BF16, "x1T")
        aT = [kb.sb([128, TG], BF16, "aT") for _ in range(nfu)]
        w2 = [kb.sb([128, D], BF16, "w2") for _ in range(nfu)]
        w13 = [kb.sb([128, 2048], BF16, "w13") for _ in range(3)]
        yacc = [kb.sb([128, D], F32, "yacc") for _ in range(4)]
        sil = [kb.sb([128, TG], F32, "sil") for _ in range(2)]
        ost = [kb.sb([128, D], F32, "ost") for _ in range(2)]
        stg = kb.sb([128, 8, 512], BF16, "stgx")
        scr = (kb.sb([128, 2, 6], F32, "stats"), kb.sb([128, 2], F32, "mv"), kb.sb([128, 2], F32, "sd"))
        X = [kb.ps([128, 512], F32, "X") for _ in range(4)]
        Y = [kb.ps([128, 512], F32, "Y") for _ in range(2)]
        wcnt = 0
        for g in range(NG):
            og = oT[0]
            for k8 in range(8):
                kb.idma(og[:, k8, :], oT_all[:, :], idxo[:, g * 8 + k8:g * 8 + k8 + 1], R=[oT_all, idxo],
                        Wd=[og] if k8 else (), W=[og] if k8 == 0 else ())
            for tt in range(4):
                tok0 = g * TG + tt * 128
                xi = xt[tt % 2]
                kb.dma("sp", xi[:, :], x_src[tok0:tok0 + 128, :], R=[x_src], W=[xi])
                for hf in range(2):
                    for kc in range(8):
                        kb.op("pe", lambda: nc.tensor.matmul(Y[hf][:, :], lhsT=og[:, kc, tt * 128:(tt + 1) * 128],
                                                             rhs=wo[:, kc, hf * 512:(hf + 1) * 512],
                                                             start=(kc == 0), stop=(kc == 7)), R=[og, wo], W=[Y[hf]])
                    kb.op("dve", lambda: nc.vector.scalar_tensor_tensor(out=h[:, hf * 512:(hf + 1) * 512],
                                                                        in0=xi[:, hf * 512:(hf + 1) * 512], scalar=ALPHA,
                                                                        in1=Y[hf][:, :], op0=ALU.mult, op1=ALU.add),
                          R=[xi, Y[hf]], W=[h])
                x1 = x1g[tt]
                layer_norm(kb, h, (lnp, lnp[:, 0, :]), (lnp, lnp[:, 1, :]), x1[:, :], x1, scr)
                for kc in range(8):
                    pt = X[kc // 4]
                    kb.op("pe", lambda: nc.tensor.transpose(pt[:, (kc % 4) * 128:(kc % 4 + 1) * 128],
                                                            x1[:, kc * 128:(kc + 1) * 128], ident[:, :]),
                          R=[x1, ident], W=[pt])
                for hf in range(2):
                    src = X[hf][:, :].rearrange("p (k t) -> p k t", k=4)
                    dst = x1T[:, hf * 4:(hf + 1) * 4, tt * 128:(tt + 1) * 128]
                    if hf == 0:
                        kb.op("dve", lambda: nc.vector.tensor_copy(out=dst, in_=src), R=[X[hf]], W=[x1T])
                    else:
                        kb.op("act", lambda: nc.scalar.copy(out=dst, in_=src), R=[X[hf]], W=[x1T])
                    if moe:
                        kb.op("dve", lambda: nc.vector.tensor_copy(out=x1T32[:, hf * 4:(hf + 1) * 4, :], in_=src),
                              R=[X[hf]], W=[x1T32])
                if moe:
                    pr = X[2]
                    for kc in range(8):
                        kb.op("pe", lambda: nc.tensor.matmul(pr[:, :8], lhsT=x1T32[:, kc, :], rhs=wr[:, kc, :],
                                                             start=(kc == 0), stop=(kc == 7)), R=[x1T32, wr], W=[pr])
                    cb = comb[tt]
                    lg, mx, tmp, oh = rt[:, 0:8], rt[:, 8:16], rt[:, 16:24], rt[:, 24:32]
                    sc = rt[:, 32:40]
                    kb.op("dve", lambda: nc.vector.tensor_copy(out=lg, in_=pr[:, :8]), R=[pr], W=[rt])
                    kb.op("dve", lambda: nc.vector.max(out=mx, in_=lg), R=[rt], W=[rt])
                    kb.op("dve", lambda: nc.vector.tensor_tensor(out=sc[:, 0:1], in0=mx[:, 1:2], in1=mx[:, 0:1],
                                                                 op=ALU.subtract), R=[rt], W=[rt])
                    kb.op("act", lambda: nc.scalar.activation(out=sc[:, 1:2], in_=sc[:, 0:1], func=AF.Exp),
                          R=[rt], W=[rt])
                    kb.op("dve", lambda: nc.vector.tensor_scalar(out=sc[:, 2:3], in0=sc[:, 1:2], scalar1=1.0,
                                                                 scalar2=None, op0=ALU.add), R=[rt], W=[rt])
                    kb.op("dve", lambda: nc.vector.reciprocal(out=sc[:, 3:4], in_=sc[:, 2:3]), R=[rt], W=[rt])
                    kb.op("dve", lambda: nc.vector.tensor_tensor(out=sc[:, 4:5], in0=sc[:, 1:2], in1=sc[:, 3:4],
                                                                 op=ALU.mult), R=[rt], W=[rt])
                    kb.op("dve", lambda: nc.vector.tensor_scalar(out=tmp, in0=lg, scalar1=mx[:, 0:1],
                                                                 scalar2=sc[:, 3:4], op0=ALU.is_equal, op1=ALU.mult),
                          R=[rt], W=[rt])
                    kb.op("dve", lambda: nc.vector.tensor_scalar(out=oh, in0=lg, scalar1=mx[:, 1:2],
                                                                 scalar2=sc[:, 4:5], op0=ALU.is_equal, op1=ALU.mult),
                          R=[rt], W=[rt])
                    kb.op("dve", lambda: nc.vector.tensor_tensor(out=cb[:, :], in0=tmp, in1=oh, op=ALU.add),
                          R=[rt], W=[cb])
            first = True
            for e in range(n_exp):
                for u in range(n_units):
                    base = (e * n_units + u) * nfu
                    for f in range(nfu):
                        wc = w13[wcnt % 3]
                        kb.dma("pool", wc[:, :], wf_d[base + f, :, 0:2048], R=[wf_d], W=[wc])
                        kb.dma("pool", w2[f][:, :], wf_d[base + f, :, 2048:3072], R=[wf_d], W=[w2[f]])
                        h1, h3 = X[(wcnt % 2) * 2], X[(wcnt % 2) * 2 + 1]
                        for kc in range(8):
                            kb.op("pe", lambda: nc.tensor.matmul(h1[:, :], lhsT=wc[:, kc * 128:(kc + 1) * 128],
                                                                 rhs=x1T[:, kc, :], start=(kc == 0), stop=(kc == 7)),
                                  R=[wc, x1T], W=[h1])
                        for kc in range(8):
                            kb.op("pe", lambda: nc.tensor.matmul(h3[:, :],
                                                                 lhsT=wc[:, 1024 + kc * 128:1024 + (kc + 1) * 128],
                                                                 rhs=x1T[:, kc, :], start=(kc == 0), stop=(kc == 7)),
                                  R=[wc, x1T], W=[h3])
                        s = sil[wcnt % 2]
                        kb.op("act", lambda: nc.scalar.activation(out=s[:, :], in_=h1[:, :], func=AF.Silu),
                              R=[h1], W=[s])
                        kb.op("dve", lambda: nc.vector.tensor_tensor(out=aT[f][:, :], in0=s[:, :], in1=h3[:, :],
                                                                     op=ALU.mult), R=[s, h3], W=[aT[f]])
                        wcnt += 1
                    for tt in range(4):
                        for hf in range(2):
                            py = Y[hf]
                            for f in range(nfu):
                                kb.op("pe", lambda: nc.tensor.matmul(py[:, :], lhsT=aT[f][:, tt * 128:(tt + 1) * 128],
                                                                     rhs=w2[f][:, hf * 512:(hf + 1) * 512],
                                                                     start=(f == 0), stop=(f == nfu - 1)),
                                      R=[aT[f], w2[f]], W=[py])
                            ya = yacc[tt]
                            ysl = ya[:, hf * 512:(hf + 1) * 512]
                            if moe:
                                cs = comb[tt][:, e:e + 1]
                                if first:
                                    kb.op("dve", lambda: nc.vector.tensor_scalar(out=ysl, in0=py[:, :], scalar1=cs,
                                                                                 scalar2=None, op0=ALU.mult),
                                          R=[py, comb[tt]], W=[ya])
                                else:
                                    kb.op("dve", lambda: nc.vector.scalar_tensor_tensor(out=ysl, in0=py[:, :], scalar=cs,
                                                                                        in1=ysl, op0=ALU.mult,
                                                                                        op1=ALU.add),
                                          R=[py, comb[tt], ya], W=[ya])
                            else:
                                if first:
                                    kb.op("dve", lambda: nc.vector.tensor_copy(out=ysl, in_=py[:, :]), R=[py], W=[ya])
                                else:
                                    kb.op("dve", lambda: nc.vector.tensor_tensor(out=ysl, in0=py[:, :], in1=ysl,
                                                                                 op=ALU.add), R=[py, ya], W=[ya])
                    first = False
            for tt in range(4):
                tok0 = g * TG + tt * 128
                kb.op("dve", lambda: nc.vector.scalar_tensor_tensor(out=h[:, :], in0=x1g[tt][:, :], scalar=ALPHA,
                                                                    in1=yacc[tt][:, :], op0=ALU.mult, op1=ALU.add),
                      R=[x1g[tt], yacc[tt]], W=[h])
                o = ost[tt % 2]
                layer_norm(kb, h, (lnp, lnp[:, 2, :]), (lnp, lnp[:, 3, :]), o[:, :], o, scr)
                kb.dma("sp", x_dst[tok0:tok0 + 128, :], o[:, :], R=[o], Wd=[x_dst])
                if not last:
                    emit_xT(kb, X, ident, o, g * 4 + tt, stg, xT_loc, xTs_loc)


def _dbg_out(kb, x_d, out_d):
    for tt in range(4):
        kb.dma("sp", out_d[tt * 512:(tt + 1) * 512, :], x_d[tt * 512:(tt + 1) * 512, :], R=[x_d], Wd=[out_d])
    return kb.finish()


def build_fused(layers=(0, 1, 2, 3), upto=9):
    kb = KBF()
    nc = kb.nc
    x_d = kb.dram_in("x", [TPC, D], F32)
    out_d = kb.dram_out("out", [TPC, D], F32)
    cst = {"tri": kb.dram_in("tri", [128, 128], F32), "identb": kb.dram_in("identb", [128, 128], BF16),
           "ident": kb.dram_in("ident", [128, 128], F32), "dilc": kb.dram_in("dilc", [32, NDEL], F32),
           "dsac": kb.dram_in("dsac", [32, NDEL], F32), "cmask": kb.dram_in("cmask", [128, 512], F32),
           "steps": kb.dram_in("steps", [128, NBIS], F32), "idx_s": kb.dram_in("idx_s", [128, 32], I32),
           "idx_o": kb.dram_in("idx_o", [128, 32], I32)}
    Ls = {}
    for l in layers:
        L = {}
        pre = "L%d_" % l
        moe = (l % 2 == 1)
        if l % 2 == 0:
            L["wgl"] = kb.dram_in(pre + "wgl", [128, 8, 464], F32)
            L["wg"] = kb.dram_in(pre + "wg", [16, 64], F32)
            L["bg"] = kb.dram_in(pre + "bg", [128, 64], F32)
            L["gn"] = kb.dram_in(pre + "gn", [128, 128], F32)
            L["wd1"] = kb.dram_in(pre + "wd1", [128, 8, 584], F32)
            L["wd2"] = kb.dram_in(pre + "wd2", [128, 8, 384], F32)
            L["relfar"] = kb.dram_in(pre + "relfar", [128, 2], F32)
        else:
            L["wcd"] = kb.dram_in(pre + "wcd", [128, 8, 770], F32)
            L["bf"] = kb.dram_in(pre + "bf", [128, 2], F32)
            L["wr"] = kb.dram_in(pre + "wr", [128, 8, 8], F32)
        L["rel2"] = kb.dram_in(pre + "rel2", [32, 2], F32)
        L["wo"] = kb.dram_in(pre + "wo", [128, 8, D], F32)
        L["lnp"] = kb.dram_in(pre + "lnp", [128, 4, D], F32)
        L["wf"] = kb.dram_in(pre + "wf", [(224 if moe else 22) if upto >= 9 else 1, 128, 3072], F32)
        L["Escr"] = kb.dram_tmp(pre + "Escr", [2, NDEL], F32)
        Ls[l] = L
    xres = [kb.dram_tmp("xres0", [TPC, D], F32), kb.dram_tmp("xres1", [TPC, D], F32)]
    xT_loc = kb.dram_tmp("xT_loc", [D, TPC], BF16)
    xT_all = kb.dram_tmp("xT_all", [4 * D, TPC], BF16)
    xTs_loc = kb.dram_tmp("xTs_loc", [4 * D, 512], BF16)
    xTs_all = kb.dram_tmp("xTs_all", [16 * D, 512], BF16)
    oT_loc = kb.dram_tmp("oT_loc", [16 * 256, 512], BF16)
    oT_all = kb.dram_tmp("oT_all", [64 * 256, 512], BF16)
    M_loc = kb.dram_tmp("M_loc", [MTOT // 512, 512], BF16)
    M_all = kb.dram_tmp("M_all", [4 * MTOT // 512, 512], BF16)
    assert MPARTS[-1][4] + 4 * MPARTS[-1][3] == 4 * MTOT // 512
    with kb.scope():
        ident = kb.sb([128, 128], F32, "ident")
        kb.dma("sp", ident[:, :], cst["ident"][:, :], R=[cst["ident"]], W=[ident])
        xin = [kb.sb([128, D], F32, "xin") for _ in range(2)]
        stg = kb.sb([128, 8, 512], BF16, "stgx")
        X = [kb.ps([128, 512], F32, "X") for _ in range(2)]
        for tt in range(TPC // 128):
            xi = xin[tt % 2]
            kb.dma("sp", xi[:, :], x_d[tt * 128:(tt + 1) * 128, :], R=[x_d], W=[xi])
            emit_xT(kb, X, ident, xi, tt, stg, xT_loc, xTs_loc)
    if upto == 0:
        return _dbg_out(kb, x_d, out_d)
    x_src = x_d
    for n, l in enumerate(layers):
        L = Ls[l]
        last = (n == len(layers) - 1)
        x_dst = out_d if last else xres[n % 2]
        for cf in range(4):
            kb.collective("AllGather", xT_loc, xT_all, xT_loc[cf * 256:(cf + 1) * 256, :],
                          xT_all[cf * 1024:(cf + 1) * 1024, :])
        if upto == 1:
            return _dbg_out(kb, x_d, out_d)
        if l % 2 == 0:
            for j in range(4):
                kb.collective("AllGather", xTs_loc, xTs_all, xTs_loc[j * 1024:(j + 1) * 1024, :],
                              xTs_all[j * 4096:(j + 1) * 4096, :])
            phase_gla(kb, L, xT_all, oT_loc, cst)
            phase_dsa1(kb, L, xT_all, xTs_all, M_loc, M_all, cst)
            phase_dsa2(kb, L, xT_all, M_all, oT_loc, cst)
        else:
            phase_cd(kb, L, xT_all, oT_loc, cst)
        if upto == 2:
            return _dbg_out(kb, x_d, out_d)
        for j in range(4):
            kb.collective("AllGather", oT_loc, oT_all, oT_loc[j * 1024:(j + 1) * 1024, :],
                          oT_all[j * 4096:(j + 1) * 4096, :])
        if upto == 3:
            return _dbg_out(kb, x_d, out_d)
        phase_c(kb, L, l % 2 == 1, oT_all, x_src, x_dst, xT_loc, xTs_loc, cst, last)
        x_src = x_dst
    return kb.finish()


def fused_inputs(x, ln_g, ln_b, rel_table, w_in_ab, w_gate_a, b_gate_a, g_norm_a, w_out_ab,
                 w_in_cd, b_forget, w_out_cd, w1_dense, w3_dense, w2_dense,
                 w_router, w1_moe, w3_moe, w2_moe, layers=(0, 1, 2, 3)):
    f32 = lambda a: np.ascontiguousarray(np.asarray(a, dtype=np.float32))
    bc = lambda v, n=128: np.ascontiguousarray(np.broadcast_to(np.asarray(v, np.float32)[None, :], (n, len(v))))
    xf = f32(x).reshape(BATCH * SEQ, D)
    rel_table = f32(rel_table)
    steps = bc(0.5 ** np.arange(1, NBIS + 1))
    perm = np.zeros(D, np.int64)
    for src in range(4):
        for rr in range(256):
            perm[src * 256 + rr] = src * 128 + rr if rr < 128 else 512 + src * 128 + (rr - 128)
    shared = {"tri": TRI, "identb": IDENT.astype(NPBF), "ident": IDENT, "dilc": dil_const(), "dsac": dsa_const(),
              "steps": steps}
    per_layer_shared = {}
    for l in layers:
        j = l // 2
        pre = "L%d_" % l
        d = {}
        d[pre + "lnp"] = np.ascontiguousarray(np.broadcast_to(
            np.stack([ln_g[l, 0], ln_b[l, 0], ln_g[l, 1], ln_b[l, 1]]).astype(np.float32)[None], (128, 4, D)))
        if l % 2 == 0:
            d[pre + "wo"] = w_kc_layout(f32(w_out_ab[j])[perm])
            d[pre + "wf"] = ffn_chunk_layout(f32(w1_dense[j]), f32(w3_dense[j]), f32(w2_dense[j]))
            d[pre + "gn"] = bc(g_norm_a[j])
        else:
            d[pre + "wo"] = w_kc_layout(f32(w_out_cd[j])[perm])
            d[pre + "wf"] = np.concatenate([ffn_chunk_layout(f32(w1_moe[j, e]), f32(w3_moe[j, e]), f32(w2_moe[j, e]))
                                            for e in range(8)], axis=0)
            d[pre + "wr"] = np.ascontiguousarray(f32(w_router[j]).reshape(8, 128, 8).transpose(1, 0, 2))
        per_layer_shared.update(d)
    maps = []
    for c in range(NCORES):
        b, m = c // 4, c % 4
        mp = dict(shared)
        mp.update(per_layer_shared)
        mp["x"] = np.ascontiguousarray(xf[c * TPC:(c + 1) * TPC])
        pp_ = np.arange(128)[:, None]
        rr_, k8_ = np.arange(4)[None, :, None], np.arange(8)[None, None, :]
        mp["idx_s"] = np.ascontiguousarray(((m * 4 + rr_) * 1024 + k8_ * 128 + pp_[:, :, None]).reshape(128, 32)
                                           .astype(np.int32))
        G_ = k8_ * 128 + pp_[:, :, None]
        mp["idx_o"] = np.ascontiguousarray(((m * 4 + G_ // 256) * 1024 + rr_ * 256 + G_ % 256).reshape(128, 32)
                                           .astype(np.int32))
        mp["cmask"] = np.where(np.arange(512)[None, :] <= (128 * m + np.arange(128))[:, None], 0.0, NEG).astype(np.float32)
        for l in layers:
            j = l // 2
            pre = "L%d_" % l
            mp[pre + "rel2"] = np.ascontiguousarray(rel_table[:, 2 * m:2 * m + 2])
            if l % 2 == 0:
                w = f32(w_in_ab[j])
                cs = lambda a, n: w[:, a:a + n]
                wgl = np.concatenate([cs(m * 64, 64), cs(256 + m * 64, 64), cs(1536, 16), cs(256 + m * 64, 64),
                                      cs(512 + m * 128, 128), cs(1024 + m * 128, 128)], axis=1)
                wd1 = np.concatenate([cs(3088, 512), cs(3600, 64), cs(3664, 8)], axis=1)
                wd2 = np.concatenate([cs(1552 + m * 128, 128), cs(2064 + m * 128, 128), cs(2576 + m * 128, 128)], axis=1)
                mp[pre + "wgl"] = w_kc_layout(wgl)
                mp[pre + "wd1"] = w_kc_layout(wd1)
                mp[pre + "wd2"] = w_kc_layout(wd2)
                mp[pre + "wg"] = np.ascontiguousarray(f32(w_gate_a[j])[:, m * 64:(m + 1) * 64])
                mp[pre + "bg"] = bc(f32(b_gate_a[j])[m * 64:(m + 1) * 64])
                mp[pre + "relfar"] = bc(rel_table[31, 2 * m:2 * m + 2])
            else:
                w = f32(w_in_cd[j])
                cs = lambda a, n: w[:, a:a + n]
                wcd = np.concatenate([cs(m * 128, 128), cs(512 + m * 128, 128), cs(1544 + m * 128, 128),
                                      cs(2056 + m * 128, 128), cs(1024 + m * 128, 128), cs(2568 + m * 128, 128),
                                      cs(1536 + 2 * m, 2)], axis=1)
                mp[pre + "wcd"] = w_kc_layout(wcd)
                mp[pre + "bf"] = bc(f32(b_forget[j])[2 * m:2 * m + 2])
        maps.append(mp)
    return maps


def kernel_fused(**inputs):
    nc = build_fused()
    maps = fused_inputs(**inputs)
    res = run_spmd(nc, maps)
    out = np.concatenate([res[c]["out"] for c in range(NCORES)], axis=0)
    return out.reshape(BATCH, SEQ, D).astype(np.float32)


def kernel(**inputs):
    return kernel_fused(**inputs)
```

```python
import math
from contextlib import ExitStack
import numpy as np
import ml_dtypes
import concourse.bass as bass
import concourse.mybir as mybir
from concourse.bass_utils import run_bass_kernel_spmd

F32 = mybir.dt.float32
BF16 = mybir.dt.bfloat16
AF = mybir.ActivationFunctionType
ALU = mybir.AluOpType
AX = mybir.AxisListType
NPBF = ml_dtypes.bfloat16

NCORES = 8
D = 1024
SEQ = 8192
BATCH = 2
DEPTH = 4
ALPHA = (2 * DEPTH) ** 0.25
LN_EPS = 1e-5
NEG = -1.0e30


class Buf:
    def __init__(self, t):
        self.t = t
        self.w = None
        self.r = []

    def __getitem__(self, idx):
        return self.t[idx]


class KB:
    NDS = 6

    def __init__(self):
        self.nc = bass.Bass("TRN2", target_bir_lowering=False)
        nc = self.nc
        self.es = ExitStack()
        self.eng = {"pe": nc.tensor, "dve": nc.vector, "act": nc.scalar, "pool": nc.gpsimd, "sp": nc.sync}
        self.esem = {}
        self.ecnt = {}
        self.seen = {e: {} for e in self.eng}
        for e in self.eng:
            self.esem[e] = self.es.enter_context(nc.semaphore("sem_" + e))
            self.ecnt[e] = 0
        self.dsem = {}
        self.dcnt = {}
        self.dnext = {}
        for q in ("sp", "act", "pool"):
            self.dsem[q] = [self.es.enter_context(nc.semaphore("dsem_%s%d" % (q, i))) for i in range(self.NDS)]
            self.dcnt[q] = [0] * self.NDS
            self.dnext[q] = 0
        self.n_names = 0
        self.outs = []

    def _nm(self, p):
        self.n_names += 1
        return "%s_%d" % (p, self.n_names)

    def dram_in(self, name, shape, dt):
        return Buf(self.nc.dram_tensor(name, list(shape), dt, kind="ExternalInput").ap())

    def dram_out(self, name, shape, dt):
        b = Buf(self.nc.dram_tensor(name, list(shape), dt, kind="ExternalOutput").ap())
        self.outs.append(b)
        return b

    def dram_tmp(self, name, shape, dt):
        return Buf(self.nc.dram_tensor(name, list(shape), dt, kind="Internal").ap())

    def sb(self, shape, dt, name="sb"):
        return Buf(self.es.enter_context(self.nc.sbuf_tensor(self._nm(name), list(shape), dt)))

    def ps(self, shape, dt=F32, name="ps"):
        return Buf(self.es.enter_context(self.nc.psum_tensor(self._nm(name), list(shape), dt)))

    def _wait(self, e, ev):
        if ev is None:
            return
        sem, val, key = ev
        if self.seen[e].get(key, 0) >= val:
            return
        self.eng[e].wait_ge(sem, val)
        self.seen[e][key] = val

    def _deps(self, e, R, W):
        evs = []
        for b in R:
            if b.w is not None:
                evs.append(b.w)
        for b in W:
            if b.w is not None:
                evs.append(b.w)
            evs.extend(b.r)
        best = {}
        for ev in evs:
            k = ev[2]
            if e == "pe" and k == "pe":
                continue
            if k not in best or best[k][1] < ev[1]:
                best[k] = ev
        for ev in best.values():
            self._wait(e, ev)

    def _mark(self, ev, R, W):
        for b in R:
            b.r.append(ev)
            if len(b.r) > 24:
                best = {}
                for x in b.r:
                    if x[2] not in best or best[x[2]][1] < x[1]:
                        best[x[2]] = x
                b.r = list(best.values())
        for b in W:
            b.w = ev
            b.r = []

    def op(self, e, fn, R=(), W=()):
        self._deps(e, R, W)
        ins = fn()
        self.ecnt[e] += 1
        ins.then_inc(self.esem[e], 1)
        ev = (self.esem[e], self.ecnt[e], e)
        self.seen[e][e] = max(self.seen[e].get(e, 0), 0)
        self._mark(ev, R, W)
        return ev

    def dma(self, q, out, in_, R=(), W=(), **kw):
        self._deps(q, R, W)
        i = self.dnext[q]
        self.dnext[q] = (i + 1) % self.NDS
        key = "d%s%d" % (q, i)
        if self.dcnt[q][i] > 0:
            self._wait(q, (self.dsem[q][i], self.dcnt[q][i], key))
        self.dcnt[q][i] += 16
        self.eng[q].dma_start(out=out, in_=in_, **kw).then_inc(self.dsem[q][i], 16)
        ev = (self.dsem[q][i], self.dcnt[q][i], key)
        self._mark(ev, R, W)
        return ev

    def finish(self):
        for q in ("sp", "act", "pool"):
            for i in range(self.NDS):
                if self.dcnt[q][i] > 0:
                    self._wait("sp", (self.dsem[q][i], self.dcnt[q][i], "d%s%d" % (q, i)))
        for e in ("pe", "dve", "act", "pool"):
            if self.ecnt[e] > 0:
                self._wait("sp", (self.esem[e], self.ecnt[e], e))
        self.es.close()
        return self.nc


def run_spmd(nc, in_maps):
    res = run_bass_kernel_spmd(nc, in_maps, core_ids=list(range(NCORES)))
    return res.results


def build_cast(n):
    kb = KB()
    nc = kb.nc
    CH = 2048
    src = kb.dram_in("src", [128, n], F32)
    dst = kb.dram_out("dst", [128, n], BF16)
    NB = 3
    tin = [kb.sb([128, CH], F32, "tin") for _ in range(NB)]
    tout = [kb.sb([128, CH], BF16, "tout") for _ in range(NB)]
    nch = (n + CH - 1) // CH
    for c in range(nch):
        c0 = c * CH
        w = min(CH, n - c0)
        a, b = tin[c % NB], tout[c % NB]
        kb.dma("sp", a[:, :w], src[:, c0:c0 + w], R=[src], W=[a])
        if c % 2 == 0:
            kb.op("dve", lambda: nc.vector.tensor_copy(out=b[:, :w], in_=a[:, :w]), R=[a], W=[b])
        else:
            kb.op("act", lambda: nc.scalar.copy(out=b[:, :w], in_=a[:, :w]), R=[a], W=[b])
        kb.dma("pool", dst[:, c0:c0 + w], b[:, :w], R=[b], W=[dst])
    return kb.finish()


def cast_weights(arrs):
    flats = [np.ascontiguousarray(a).reshape(NCORES, 128, -1) for a in arrs]
    ns = [f.shape[2] for f in flats]
    cat = np.concatenate(flats, axis=2)
    n = cat.shape[2]
    nc = build_cast(n)
    res = run_spmd(nc, [{"src": np.ascontiguousarray(cat[c])} for c in range(NCORES)])
    out = np.stack([res[c]["dst"] for c in range(NCORES)], axis=0)
    outs = []
    o = 0
    for a, k in zip(arrs, ns):
        outs.append(out[:, :, o:o + k].reshape(a.shape))
        o += k
    return outs


TPC = 2048


def build_A(W, fm, tmb, tmf, gate=None):
    kb = KB()
    nc = kb.nc
    NT = TPC // 128
    n_fm = sum(n for _, n in fm)
    n_tmb = sum(n for _, n in tmb)
    n_tmf = sum(n for _, n in tmf) + (256 if gate is not None else 0)
    x = kb.dram_in("x", [TPC, D], F32)
    w = kb.dram_in("w", [128, 8, W], BF16)
    ident_d = kb.dram_in("ident", [128, 128], F32)
    yT = kb.dram_out("yT", [n_fm, TPC], BF16)
    ytb = kb.dram_out("ytb", [TPC, max(n_tmb, 1)], BF16)
    ytf = kb.dram_out("ytf", [TPC, max(n_tmf, 1)], F32)
    wsb = kb.sb([128, 8, W], BF16, "w")
    ident = kb.sb([128, 128], F32, "ident")
    xT = kb.sb([128, 8, TPC], BF16, "xT")
    kb.dma("sp", ident[:, :], ident_d[:, :], R=[ident_d], W=[ident])
    for kc in range(8):
        kb.dma("pool" if kc % 2 else "sp", wsb[:, kc, :], w[:, kc, :], R=[w], W=[wsb])
    if gate is not None:
        wg_d = kb.dram_in("wg", [16, 256], F32)
        bg_d = kb.dram_in("bg", [128, 256], F32)
        wg = kb.sb([16, 256], F32, "wg")
        bg = kb.sb([128, 256], F32, "bg")
        gaT = kb.sb([16, TPC], F32, "gaT")
        w32 = kb.sb([128, 8, 16], F32, "w32")
        kb.dma("sp", wg[:, :], wg_d[:, :], R=[wg_d], W=[wg])
        kb.dma("sp", bg[:, :], bg_d[:, :], R=[bg_d], W=[bg])
    xin = [kb.sb([128, D], F32, "xin") for _ in range(2)]
    pst = [kb.ps([128, 1024], F32, "pst")]
    for tt in range(NT):
        xi = xin[tt % 2]
        kb.dma("sp", xi[:, :], x[tt * 128:(tt + 1) * 128, :], R=[x], W=[xi])
        pt = pst[0]
        for kc in range(8):
            kb.op("pe", lambda: nc.tensor.transpose(pt[:, kc * 128:(kc + 1) * 128], xi[:, kc * 128:(kc + 1) * 128],
                                                    ident[:, :]), R=[xi, ident], W=[pt])
        for hf in range(2):
            src = pt[:, hf * 512:(hf + 1) * 512].rearrange("p (k t) -> p k t", k=4)
            dst = xT[:, hf * 4:(hf + 1) * 4, tt * 128:(tt + 1) * 128]
            if hf == 0:
                kb.op("dve", lambda: nc.vector.tensor_copy(out=dst, in_=src), R=[pt], W=[xT])
            else:
                kb.op("act", lambda: nc.scalar.copy(out=dst, in_=src), R=[pt], W=[xT])
    psf = [kb.ps([128, 512], F32, "psf") for _ in range(2)]
    stf = [kb.sb([128, 512], BF16, "stf") for _ in range(3)]
    cnt = 0
    row = 0
    blocks = list(fm)
    for (c0, ncol) in blocks:
        for tg in range(TPC // 512):
            pp = psf[cnt % 2]
            st = stf[cnt % 3]
            for kc in range(8):
                kb.op("pe", lambda: nc.tensor.matmul(pp[:ncol, :], lhsT=wsb[:, kc, c0:c0 + ncol],
                                                     rhs=xT[:, kc, tg * 512:(tg + 1) * 512],
                                                     start=(kc == 0), stop=(kc == 7)), R=[wsb, xT], W=[pp])
            if cnt % 2 == 0:
                kb.op("dve", lambda: nc.vector.tensor_copy(out=st[:ncol, :], in_=pp[:ncol, :]), R=[pp], W=[st])
            else:
                kb.op("act", lambda: nc.scalar.copy(out=st[:ncol, :], in_=pp[:ncol, :]), R=[pp], W=[st])
            kb.dma("pool" if cnt % 2 else "sp", yT[row:row + ncol, tg * 512:(tg + 1) * 512], st[:ncol, :],
                   R=[st], W=[yT])
            cnt += 1
        row += ncol
    if gate is not None:
        g0 = gate[0]
        for tg in range(TPC // 512):
            pp = psf[cnt % 2]
            for kc in range(8):
                kb.op("pe", lambda: nc.tensor.matmul(pp[:16, :], lhsT=wsb[:, kc, g0:g0 + 16],
                                                     rhs=xT[:, kc, tg * 512:(tg + 1) * 512],
                                                     start=(kc == 0), stop=(kc == 7)), R=[wsb, xT], W=[pp])
            kb.op("dve", lambda: nc.vector.tensor_copy(out=gaT[:, tg * 512:(tg + 1) * 512], in_=pp[:16, :]),
                  R=[pp], W=[gaT])
            cnt += 1
    pstm = [kb.ps([128, 512], F32, "pstm") for _ in range(2)]
    stb = [kb.sb([128, 512], BF16, "stb") for _ in range(3)]
    st32 = [kb.sb([128, 512], F32, "st32") for _ in range(3)]
    cnt = 0
    for tt in range(NT):
        for kind, lst, dst_d in (("b", tmb, ytb), ("f", tmf, ytf)):
            off = 0
            for (c0, ncol) in lst:
                pp = pstm[cnt % 2]
                st = (stb if kind == "b" else st32)[cnt % 3]
                for kc in range(8):
                    kb.op("pe", lambda: nc.tensor.matmul(pp[:, :ncol], lhsT=xT[:, kc, tt * 128:(tt + 1) * 128],
                                                         rhs=wsb[:, kc, c0:c0 + ncol],
                                                         start=(kc == 0), stop=(kc == 7)), R=[wsb, xT], W=[pp])
                if cnt % 2 == 0:
                    kb.op("dve", lambda: nc.vector.tensor_copy(out=st[:, :ncol], in_=pp[:, :ncol]), R=[pp], W=[st])
                else:
                    kb.op("act", lambda: nc.scalar.copy(out=st[:, :ncol], in_=pp[:, :ncol]), R=[pp], W=[st])
                kb.dma("pool" if cnt % 2 else "sp", dst_d[tt * 128:(tt + 1) * 128, off:off + ncol], st[:, :ncol],
                       R=[st], W=[dst_d])
                off += ncol
                cnt += 1
        if gate is not None:
            off = sum(n for _, n in tmf)
            pp = pstm[cnt % 2]
            st = st32[cnt % 3]
            kb.op("pe", lambda: nc.tensor.matmul(pp[:, :256], lhsT=gaT[:, tt * 128:(tt + 1) * 128], rhs=wg[:, :],
                                                 start=True, stop=True), R=[gaT, wg], W=[pp])
            kb.op("dve", lambda: nc.vector.tensor_tensor(out=st[:, :256], in0=pp[:, :256], in1=bg[:, :], op=ALU.add),
                  R=[pp, bg], W=[st])
            kb.op("act", lambda: nc.scalar.activation(out=st[:, :256], in_=st[:, :256], func=AF.Exp, scale=-1.0),
                  R=[st], W=[st])
            kb.op("act", lambda: nc.scalar.activation(out=st[:, :256], in_=st[:, :256], func=AF.Ln, bias=1.0),
                  R=[st], W=[st])
            kb.op("dve", lambda: nc.vector.tensor_scalar(out=st[:, :256], in0=st[:, :256], scalar1=-1.0 / 16.0,
                                                         scalar2=None, op0=ALU.mult), R=[st], W=[st])
            kb.dma("sp", ytf[tt * 128:(tt + 1) * 128, off:off + 256], st[:, :256], R=[st], W=[ytf])
            cnt += 1
    return kb.finish()


def blocks_of(c0, n, bs):
    out = []
    while n > 0:
        k = min(bs, n)
        out.append((c0, k))
        c0 += k
        n -= k
    return out


def w_kc_layout(w):
    W = w.shape[1]
    return np.ascontiguousarray(w.reshape(8, 128, W).transpose(1, 0, 2))


IDENT = np.eye(128, dtype=np.float32)


def run_A(x_flat, w_bf, fm_ranges, tmb_ranges, tmf_ranges, gate=None, wg=None, bg=None):
    W = w_bf.shape[1]
    fm = [b for (c0, n) in fm_ranges for b in blocks_of(c0, n, 128)]
    tmb = [b for (c0, n) in tmb_ranges for b in blocks_of(c0, n, 512)]
    tmf = [b for (c0, n) in tmf_ranges for b in blocks_of(c0, n, 512)]
    nc = build_A(W, fm, tmb, tmf, gate)
    wl = w_kc_layout(w_bf)
    maps = []
    for c in range(NCORES):
        m = {"x": np.ascontiguousarray(x_flat[c * TPC:(c + 1) * TPC]), "w": wl, "ident": IDENT}
        if gate is not None:
            m["wg"] = np.ascontiguousarray(wg)
            m["bg"] = np.ascontiguousarray(np.broadcast_to(bg[None, :], (128, 256)))
        maps.append(m)
    res = run_spmd(nc, maps)
    yT = np.concatenate([res[c]["yT"] for c in range(NCORES)], axis=1)
    ytb = np.concatenate([res[c]["ytb"] for c in range(NCORES)], axis=0)
    ytf = np.concatenate([res[c]["ytf"] for c in range(NCORES)], axis=0)
    return yT, ytb, ytf


def layer_norm(kb, h, gt, bt, out_ap, out_buf, scr):
    nc = kb.nc
    stats, mv, sd = scr
    for c in range(2):
        kb.op("dve", lambda: nc.vector.bn_stats(out=stats[:, c, :], in_=h[:, c * 512:(c + 1) * 512]), R=[h], W=[stats])
    kb.op("dve", lambda: nc.vector.bn_aggr(out=mv[:, :], in_=stats[:, :, :].rearrange("p a b -> p (a b)")),
          R=[stats], W=[mv])
    kb.op("act", lambda: nc.scalar.activation(out=sd[:, 0:1], in_=mv[:, 1:2], func=AF.Sqrt, bias=kb.eps_col[:, 0:1]),
          R=[mv, kb.eps_col], W=[sd])
    kb.op("dve", lambda: nc.vector.reciprocal(out=sd[:, 1:2], in_=sd[:, 0:1]), R=[sd], W=[sd])
    kb.op("dve", lambda: nc.vector.tensor_scalar(out=h[:, :], in0=h[:, :], scalar1=mv[:, 0:1], scalar2=sd[:, 1:2],
                                                 op0=ALU.subtract, op1=ALU.mult), R=[h, mv, sd], W=[h])
    kb.op("pool", lambda: nc.gpsimd.tensor_tensor(out=h[:, :], in0=h[:, :], in1=gt[1], op=ALU.mult),
          R=[h, gt[0]], W=[h])
    kb.op("dve", lambda: nc.vector.tensor_tensor(out=out_ap, in0=h[:, :], in1=bt[1], op=ALU.add),
          R=[h, bt[0]], W=[out_buf])


def build_C(n_exp, nf_unit, n_units_per_exp):
    kb = KB()
    nc = kb.nc
    moe = n_exp > 1
    NT = TPC // 128
    TG = 512
    NG = TPC // TG
    nfu = nf_unit
    n_chunks = n_exp * n_units_per_exp * nfu
    oT_d = kb.dram_in("oT", [128, 8, TPC], BF16)
    x_d = kb.dram_in("x", [TPC, D], F32)
    wo_d = kb.dram_in("wo", [128, 8, D], BF16)
    lnp_d = kb.dram_in("lnp", [128, 4, D], F32)
    wf_d = kb.dram_in("wf", [n_chunks, 128, 3072], BF16)
    ident_d = kb.dram_in("ident", [128, 128], F32)
    out_d = kb.dram_out("out", [TPC, D], F32)
    ident = kb.sb([128, 128], F32, "ident")
    wo = kb.sb([128, 8, D], BF16, "wo")
    lnp = kb.sb([128, 4, D], F32, "lnp")
    kb.eps_col = kb.sb([128, 1], F32, "eps")
    kb.op("dve", lambda: nc.vector.memset(kb.eps_col[:, :], LN_EPS), W=[kb.eps_col])
    kb.dma("sp", ident[:, :], ident_d[:, :], R=[ident_d], W=[ident])
    kb.dma("sp", wo[:, :, :], wo_d[:, :, :], R=[wo_d], W=[wo])
    kb.dma("pool", lnp[:, :, :], lnp_d[:, :, :], R=[lnp_d], W=[lnp])
    if moe:
        wr_d = kb.dram_in("wr", [128, 8, 8], F32)
        wr = kb.sb([128, 8, 8], F32, "wr")
        kb.dma("sp", wr[:, :, :], wr_d[:, :, :], R=[wr_d], W=[wr])
        x1T32 = kb.sb([128, 8, 128], F32, "x1T32")
        comb = [kb.sb([128, 8], F32, "comb") for _ in range(4)]
        rt = kb.sb([128, 40], F32, "rt")
    oT = [kb.sb([128, 8, TG], BF16, "oT") for _ in range(2)]
    xt = [kb.sb([128, D], F32, "xt") for _ in range(2)]
    h = kb.sb([128, D], F32, "h")
    x1g = [kb.sb([128, D], F32, "x1g") for _ in range(4)]
    x1T = kb.sb([128, 8, TG], BF16, "x1T")
    aT = [kb.sb([128, TG], BF16, "aT") for _ in range(nfu)]
    w2 = [kb.sb([128, D], BF16, "w2") for _ in range(nfu)]
    w13 = [kb.sb([128, 2048], BF16, "w13") for _ in range(3)]
    yacc = [kb.sb([128, D], F32, "yacc") for _ in range(4)]
    sil = [kb.sb([128, TG], F32, "sil") for _ in range(2)]
    ost = [kb.sb([128, D], F32, "ost") for _ in range(2)]
    scr = (kb.sb([128, 2, 6], F32, "stats"), kb.sb([128, 2], F32, "mv"), kb.sb([128, 2], F32, "sd"))
    X = [kb.ps([128, 512], F32, "X") for _ in range(4)]
    Y = [kb.ps([128, 512], F32, "Y") for _ in range(2)]
    wcnt = 0
    for g in range(NG):
        og = oT[g % 2]
        kb.dma("pool", og[:, :, :], oT_d[:, :, g * TG:(g + 1) * TG], R=[oT_d], W=[og])
        for tt in range(4):
            tok0 = g * TG + tt * 128
            xi = xt[tt % 2]
            kb.dma("sp", xi[:, :], x_d[tok0:tok0 + 128, :], R=[x_d], W=[xi])
            for hf in range(2):
                for kc in range(8):
                    kb.op("pe", lambda: nc.tensor.matmul(Y[hf][:, :], lhsT=og[:, kc, tt * 128:(tt + 1) * 128],
                                                         rhs=wo[:, kc, hf * 512:(hf + 1) * 512],
                                                         start=(kc == 0), stop=(kc == 7)), R=[og, wo], W=[Y[hf]])
                kb.op("dve", lambda: nc.vector.scalar_tensor_tensor(out=h[:, hf * 512:(hf + 1) * 512],
                                                                    in0=xi[:, hf * 512:(hf + 1) * 512], scalar=ALPHA,
                                                                    in1=Y[hf][:, :], op0=ALU.mult, op1=ALU.add),
                      R=[xi, Y[hf]], W=[h])
            x1 = x1g[tt]
            layer_norm(kb, h, (lnp, lnp[:, 0, :]), (lnp, lnp[:, 1, :]), x1[:, :], x1, scr)
            for kc in range(8):
                pt = X[kc // 4]
                kb.op("pe", lambda: nc.tensor.transpose(pt[:, (kc % 4) * 128:(kc % 4 + 1) * 128],
                                                        x1[:, kc * 128:(kc + 1) * 128], ident[:, :]),
                      R=[x1, ident], W=[pt])
            for hf in range(2):
                src = X[hf][:, :].rearrange("p (k t) -> p k t", k=4)
                dst = x1T[:, hf * 4:(hf + 1) * 4, tt * 128:(tt + 1) * 128]
                if hf == 0:
                    kb.op("dve", lambda: nc.vector.tensor_copy(out=dst, in_=src), R=[X[hf]], W=[x1T])
                else:
                    kb.op("act", lambda: nc.scalar.copy(out=dst, in_=src), R=[X[hf]], W=[x1T])
                if moe:
                    kb.op("pool" if False else "dve",
                          lambda: nc.vector.tensor_copy(out=x1T32[:, hf * 4:(hf + 1) * 4, :], in_=src),
                          R=[X[hf]], W=[x1T32])
            if moe:
                pr = X[2]
                for kc in range(8):
                    kb.op("pe", lambda: nc.tensor.matmul(pr[:, :8], lhsT=x1T32[:, kc, :], rhs=wr[:, kc, :],
                                                         start=(kc == 0), stop=(kc == 7)), R=[x1T32, wr], W=[pr])
                cb = comb[tt]
                lg, mx, tmp, oh = rt[:, 0:8], rt[:, 8:16], rt[:, 16:24], rt[:, 24:32]
                sc = rt[:, 32:40]
                kb.op("dve", lambda: nc.vector.tensor_copy(out=lg, in_=pr[:, :8]), R=[pr], W=[rt])
                kb.op("dve", lambda: nc.vector.max(out=mx, in_=lg), R=[rt], W=[rt])
                kb.op("dve", lambda: nc.vector.tensor_tensor(out=sc[:, 0:1], in0=mx[:, 1:2], in1=mx[:, 0:1],
                                                             op=ALU.subtract), R=[rt], W=[rt])
                kb.op("act", lambda: nc.scalar.activation(out=sc[:, 1:2], in_=sc[:, 0:1], func=AF.Exp), R=[rt], W=[rt])
                kb.op("dve", lambda: nc.vector.tensor_scalar(out=sc[:, 2:3], in0=sc[:, 1:2], scalar1=1.0, scalar2=None,
                                                             op0=ALU.add), R=[rt], W=[rt])
                kb.op("dve", lambda: nc.vector.reciprocal(out=sc[:, 3:4], in_=sc[:, 2:3]), R=[rt], W=[rt])
                kb.op("dve", lambda: nc.vector.tensor_tensor(out=sc[:, 4:5], in0=sc[:, 1:2], in1=sc[:, 3:4],
                                                             op=ALU.mult), R=[rt], W=[rt])
                kb.op("dve", lambda: nc.vector.tensor_scalar(out=tmp, in0=lg, scalar1=mx[:, 0:1], scalar2=sc[:, 3:4],
                                                             op0=ALU.is_equal, op1=ALU.mult), R=[rt], W=[rt])
                kb.op("dve", lambda: nc.vector.tensor_scalar(out=oh, in0=lg, scalar1=mx[:, 1:2], scalar2=sc[:, 4:5],
                                                             op0=ALU.is_equal, op1=ALU.mult), R=[rt], W=[rt])
                kb.op("dve", lambda: nc.vector.tensor_tensor(out=cb[:, :], in0=tmp, in1=oh, op=ALU.add),
                      R=[rt], W=[cb])
        first = True
        for e in range(n_exp):
            for u in range(n_units_per_exp):
                base = (e * n_units_per_exp + u) * nfu
                for f in range(nfu):
                    wc = w13[wcnt % 3]
                    q = "sp" if wcnt % 2 == 0 else "pool"
                    kb.dma(q, wc[:, :], wf_d[base + f, :, 0:2048], R=[wf_d], W=[wc])
                    kb.dma("pool" if wcnt % 2 == 0 else "sp", w2[f][:, :], wf_d[base + f, :, 2048:3072],
                           R=[wf_d], W=[w2[f]])
                    h1, h3 = X[(wcnt % 2) * 2], X[(wcnt % 2) * 2 + 1]
                    for kc in range(8):
                        kb.op("pe", lambda: nc.tensor.matmul(h1[:, :], lhsT=wc[:, kc * 128:(kc + 1) * 128],
                                                             rhs=x1T[:, kc, :], start=(kc == 0), stop=(kc == 7)),
                              R=[wc, x1T], W=[h1])
                    for kc in range(8):
                        kb.op("pe", lambda: nc.tensor.matmul(h3[:, :], lhsT=wc[:, 1024 + kc * 128:1024 + (kc + 1) * 128],
                                                             rhs=x1T[:, kc, :], start=(kc == 0), stop=(kc == 7)),
                              R=[wc, x1T], W=[h3])
                    s = sil[wcnt % 2]
                    kb.op("act", lambda: nc.scalar.activation(out=s[:, :], in_=h1[:, :], func=AF.Silu), R=[h1], W=[s])
                    kb.op("dve", lambda: nc.vector.tensor_tensor(out=aT[f][:, :], in0=s[:, :], in1=h3[:, :],
                                                                 op=ALU.mult), R=[s, h3], W=[aT[f]])
                    wcnt += 1
                for tt in range(4):
                    for hf in range(2):
                        py = Y[hf]
                        for f in range(nfu):
                            kb.op("pe", lambda: nc.tensor.matmul(py[:, :], lhsT=aT[f][:, tt * 128:(tt + 1) * 128],
                                                                 rhs=w2[f][:, hf * 512:(hf + 1) * 512],
                                                                 start=(f == 0), stop=(f == nfu - 1)),
                                  R=[aT[f], w2[f]], W=[py])
                        ya = yacc[tt]
                        ysl = ya[:, hf * 512:(hf + 1) * 512]
                        if moe:
                            cs = comb[tt][:, e:e + 1]
                            if first:
                                kb.op("dve", lambda: nc.vector.tensor_scalar(out=ysl, in0=py[:, :], scalar1=cs,
                                                                             scalar2=None, op0=ALU.mult),
                                      R=[py, comb[tt]], W=[ya])
                            else:
                                kb.op("dve", lambda: nc.vector.scalar_tensor_tensor(out=ysl, in0=py[:, :], scalar=cs,
                                                                                    in1=ysl, op0=ALU.mult, op1=ALU.add),
                                      R=[py, comb[tt], ya], W=[ya])
                        else:
                            if first:
                                kb.op("dve", lambda: nc.vector.tensor_copy(out=ysl, in_=py[:, :]), R=[py], W=[ya])
                            else:
                                kb.op("dve", lambda: nc.vector.tensor_tensor(out=ysl, in0=py[:, :], in1=ysl, op=ALU.add),
                                      R=[py, ya], W=[ya])
                first = False
        for tt in range(4):
            tok0 = g * TG + tt * 128
            kb.op("dve", lambda: nc.vector.scalar_tensor_tensor(out=h[:, :], in0=x1g[tt][:, :], scalar=ALPHA,
                                                                in1=yacc[tt][:, :], op0=ALU.mult, op1=ALU.add),
                  R=[x1g[tt], yacc[tt]], W=[h])
            o = ost[tt % 2]
            layer_norm(kb, h, (lnp, lnp[:, 2, :]), (lnp, lnp[:, 3, :]), o[:, :], o, scr)
            kb.dma("sp", out_d[tok0:tok0 + 128, :], o[:, :], R=[o], W=[out_d])
    return kb.finish()


def ffn_chunk_layout(w1, w3, w2):
    F = w1.shape[1]
    nf = F // 128
    a = w1.reshape(8, 128, nf, 128).transpose(2, 1, 0, 3).reshape(nf, 128, 1024)
    b = w3.reshape(8, 128, nf, 128).transpose(2, 1, 0, 3).reshape(nf, 128, 1024)
    c = w2.reshape(nf, 128, 1024)
    return np.ascontiguousarray(np.concatenate([a, b, c], axis=2))


def run_C(o_flat_bf, x_flat, wo_bf, lnp4, wf, n_exp, nf_unit, n_units, wr=None):
    nc = build_C(n_exp, nf_unit, n_units)
    wol = w_kc_layout(wo_bf)
    lnb = np.ascontiguousarray(np.broadcast_to(lnp4[None, :, :], (128, 4, D))).astype(np.float32)
    maps = []
    for c in range(NCORES):
        oc = o_flat_bf[c * TPC:(c + 1) * TPC]
        oT = np.ascontiguousarray(oc.reshape(TPC, 8, 128).transpose(2, 1, 0))
        m = {"oT": oT, "x": np.ascontiguousarray(x_flat[c * TPC:(c + 1) * TPC]), "wo": wol, "lnp": lnb,
             "wf": wf, "ident": IDENT}
        if wr is not None:
            m["wr"] = np.ascontiguousarray(wr.reshape(8, 128, 8).transpose(1, 0, 2))
        maps.append(m)
    res = run_spmd(nc, maps)
    return np.concatenate([res[c]["out"] for c in range(NCORES)], axis=0)


from concourse.bass_types import AP as _AP

NQB = SEQ // 128
NDEL = 2304


def rel_bucket_np(d):
    d = np.maximum(d, 0)
    df = np.maximum(d, 1).astype(np.float32)
    large = 16 + (np.log(df / np.float32(16.0)).astype(np.float32) / np.float32(math.log(2048 / 16))
                  * np.float32(16)).astype(np.int32)
    large = np.minimum(large, 31)
    return np.where(d < 16, d, large)


def dil_const():
    C = np.zeros((32, NDEL), np.float32)
    for idx in range(NDEL):
        dl = idx - 127
        if dl < 0 or dl > 2048:
            continue
        mult = 0
        for (w, dd) in ((128, 1), (512, 4), (2048, 16)):
            if dl <= w and dl % dd == 0:
                mult += 1
        if mult:
            C[int(rel_bucket_np(np.array([dl]))[0]), idx] = mult
    return C


class AttnCtx:
    def __init__(self, kb, n_s=4, n_o=2, grouped=False):
        self.kb = kb
        self.S = [kb.ps([128, 512], F32, "S") for _ in range(n_s)]
        self.O = [kb.ps([128, 512], F32, "O") for _ in range(n_o)]
        self.P32 = [kb.sb([128, 128], F32, "P32") for _ in range(4)]
        self.Pb = [kb.sb([128, 128], BF16, "Pb") for _ in range(6)]
        self.pending = []
        self.LA = 3
        self.LAG = 2
        self.cpg = 0
        if grouped:
            self.P32g = [kb.sb([128, 512], F32, "P32g") for _ in range(3)]
            self.Pbg = [kb.sb([128, 512], BF16, "Pbg") for _ in range(4)]
        self.rc = [kb.sb([128, 1], F32, "rc") for _ in range(2)]
        self.cs = 0
        self.co = 0
        self.cp = 0


def attn_qblock(ax, qT, kT, Vaug, h, i, jlist, bias_of, mask_of, ost, ocol, after=None):
    kb = ax.kb
    nc = kb.nc
    O = ax.O[ax.co % len(ax.O)]
    rc = ax.rc[ax.co % 2]
    ax.co += 1
    hp = slice(h * 64, (h + 1) * 64)
    nj = len(jlist)
    for n, j in enumerate(jlist):
        S = ax.S[ax.cs % len(ax.S)]
        ax.cs += 1
        kb.op("pe", lambda: nc.tensor.matmul(S[:, :128], lhsT=kT[hp, j * 128:(j + 1) * 128],
                                             rhs=qT[hp, i * 128:(i + 1) * 128], start=True, stop=True),
              R=[kT, qT], W=[S])
        Pb = ax.Pb[ax.cp % len(ax.Pb)]
        b = bias_of(j) if bias_of is not None else None
        m = mask_of(j) if mask_of is not None else None
        if m is not None and not isinstance(m, list):
            m = [m]
        if m is not None and len(m) == 0:
            m = None
        tgt = Pb if m is None else ax.P32[ax.cp % len(ax.P32)]
        ax.cp += 1
        if b is None:
            kb.op("act", lambda: nc.scalar.activation(out=tgt[:, :], in_=S[:, :128], func=AF.Exp, scale=0.125),
                  R=[S], W=[tgt])
        else:
            kb.op("act", lambda: nc.scalar.activation(out=tgt[:, :], in_=S[:, :128], func=AF.Exp, bias=b[1],
                                                      scale=0.125), R=[S, b[0]], W=[tgt])
        if m is not None:
            for mm in m[:-1]:
                kb.op("pool", lambda: nc.gpsimd.tensor_tensor(out=tgt[:, :], in0=tgt[:, :], in1=mm[1], op=ALU.mult),
                      R=[tgt, mm[0]], W=[tgt])
            kb.op("dve", lambda: nc.vector.tensor_tensor(out=Pb[:, :], in0=tgt[:, :], in1=m[-1][1], op=ALU.mult),
                  R=[tgt, m[-1][0]], W=[Pb])

        def pv(Pb=Pb, j=j, n=n):
            kb.op("pe", lambda: nc.tensor.matmul(O[:, :65], lhsT=Pb[:, :], rhs=Vaug[:, j, h, :],
                                                 start=(n == 0), stop=(n == nj - 1)), R=[Pb, Vaug], W=[O])
            if n == nj - 1:
                kb.op("dve", lambda: nc.vector.reciprocal(out=rc[:, :], in_=O[:, 64:65]), R=[O], W=[rc])
                kb.op("dve", lambda: nc.vector.tensor_scalar(out=ost[:, ocol:ocol + 64], in0=O[:, 0:64],
                                                             scalar1=rc[:, 0:1], scalar2=None, op0=ALU.mult),
                      R=[O, rc], W=[ost])
                if after is not None:
                    after()

        ax.pending.append(pv)
        while len(ax.pending) > ax.LA:
            ax.pending.pop(0)()


def attn_qgroup(ax, qT, kT, Vaug, h, i, groups, ost, ocol, after=None):
    kb = ax.kb
    nc = kb.nc
    O = ax.O[ax.co % len(ax.O)]
    rc = ax.rc[ax.co % 2]
    ax.co += 1
    hp = slice(h * 64, (h + 1) * 64)
    ng = len(groups)
    for gi, g in enumerate(groups):
        js = g["js"]
        wd = 128 * len(js)
        S = ax.S[ax.cs % len(ax.S)]
        ax.cs += 1
        for n, j in enumerate(js):
            kb.op("pe", lambda: nc.tensor.matmul(S[:, n * 128:(n + 1) * 128], lhsT=kT[hp, j * 128:(j + 1) * 128],
                                                 rhs=qT[hp, i * 128:(i + 1) * 128], start=True, stop=True),
                  R=[kT, qT], W=[S])
        Pb = ax.Pbg[ax.cpg % len(ax.Pbg)]
        has_mask = (g.get("pool_masks") is not None) or (g.get("dve_mask") is not None)
        tgt = ax.P32g[ax.cpg % len(ax.P32g)] if has_mask else Pb
        ax.cpg += 1
        b = g.get("bias")
        if b is None:
            kb.op("act", lambda: nc.scalar.activation(out=tgt[:, :wd], in_=S[:, :wd], func=AF.Exp, scale=0.125),
                  R=[S], W=[tgt])
        else:
            kb.op("act", lambda: nc.scalar.activation(out=tgt[:, :wd], in_=S[:, :wd], func=AF.Exp, bias=b[1],
                                                      scale=0.125), R=[S, b[0]], W=[tgt])
        if has_mask:
            pm = g.get("pool_masks")
            dm = g.get("dve_mask")
            if pm is not None:
                for n, mm in enumerate(pm):
                    last = (dm is None) and False
                    kb.op("pool", lambda: nc.gpsimd.tensor_tensor(out=tgt[:, n * 128:(n + 1) * 128],
                                                                  in0=tgt[:, n * 128:(n + 1) * 128], in1=mm[1],
                                                                  op=ALU.mult), R=[tgt, mm[0]], W=[tgt])
            if dm is not None:
                kb.op("dve", lambda: nc.vector.tensor_tensor(out=Pb[:, :wd], in0=tgt[:, :wd], in1=dm[1], op=ALU.mult),
                      R=[tgt, dm[0]], W=[Pb])
            else:
                kb.op("dve", lambda: nc.vector.tensor_copy(out=Pb[:, :wd], in_=tgt[:, :wd]), R=[tgt], W=[Pb])

        def pv(Pb=Pb, js=js, gi=gi):
            for n, j in enumerate(js):
                kb.op("pe", lambda: nc.tensor.matmul(O[:, :65], lhsT=Pb[:, n * 128:(n + 1) * 128], rhs=Vaug[:, j, h, :],
                                                     start=(gi == 0 and n == 0),
                                                     stop=(gi == ng - 1 and n == len(js) - 1)), R=[Pb, Vaug], W=[O])
            if gi == ng - 1:
                kb.op("dve", lambda: nc.vector.reciprocal(out=rc[:, :], in_=O[:, 64:65]), R=[O], W=[rc])
                kb.op("dve", lambda: nc.vector.tensor_scalar(out=ost[:, ocol:ocol + 64], in0=O[:, 0:64],
                                                             scalar1=rc[:, 0:1], scalar2=None, op0=ALU.mult),
                      R=[O, rc], W=[ost])
                if after is not None:
                    after()

        ax.pending.append(pv)
        while len(ax.pending) > ax.LAG:
            ax.pending.pop(0)()


def attn_flush(ax):
    while ax.pending:
        ax.pending.pop(0)()


def load_vaug(kb, v_d, name):
    nc = kb.nc
    Vaug = kb.sb([128, NQB, 2, 65], BF16, name)
    kb.op("pool", lambda: nc.gpsimd.memset(Vaug[:, :, :, :], 1.0), W=[Vaug])
    for c in range(4):
        js = slice(c * 16, (c + 1) * 16)
        kb.dma("sp" if c % 2 == 0 else "pool", Vaug[:, js, :, 0:64],
               v_d[:, js, :].rearrange("p j (h d) -> p j h d", h=2), R=[v_d], W=[Vaug])
    return Vaug


def build_B_cd():
    kb = KB()
    nc = kb.nc
    qc_d = kb.dram_in("qc", [128, SEQ], BF16)
    kc_d = kb.dram_in("kc", [128, SEQ], BF16)
    vc_d = kb.dram_in("vc", [128, NQB, 128], BF16)
    qd_d = kb.dram_in("qd", [128, SEQ], BF16)
    kd_d = kb.dram_in("kd", [128, SEQ], BF16)
    vd_d = kb.dram_in("vd", [128, NQB, 128], BF16)
    fc_d = kb.dram_in("fc", [128, NQB, 2], F32)
    bf_d = kb.dram_in("bf", [128, 2], F32)
    tri_d = kb.dram_in("tri", [128, 128], F32)
    rel_d = kb.dram_in("rel", [32, 2], F32)
    C_d = kb.dram_in("dilc", [32, NDEL], F32)
    oc_d = kb.dram_out("oc", [SEQ, 128], BF16)
    od_d = kb.dram_out("od", [SEQ, 128], BF16)
    E_d = kb.dram_tmp("Escr", [2, NDEL], F32)

    tri = kb.sb([128, 128], F32, "tri")
    ones = kb.sb([128, 128], F32, "ones")
    kb.dma("sp", tri[:, :], tri_d[:, :], R=[tri_d], W=[tri])
    kb.op("dve", lambda: nc.vector.memset(ones[:, :], 1.0), W=[ones])
    ax = AttnCtx(kb)
    rel = kb.sb([32, 2], F32, "rel")
    Cs = kb.sb([32, NDEL], F32, "Cs")
    Es = kb.sb([2, NDEL], F32, "Es")
    kb.dma("sp", rel[:, :], rel_d[:, :], R=[rel_d], W=[rel])
    kb.dma("sp", Cs[:, :], C_d[:, :], R=[C_d], W=[Cs])
    kb.op("act", lambda: nc.scalar.activation(out=rel[:, :], in_=rel[:, :], func=AF.Exp), R=[rel], W=[rel])
    for c in range((NDEL + 511) // 512):
        w = min(512, NDEL - c * 512)
        pp = ax.S[c % 3]
        kb.op("pe", lambda: nc.tensor.matmul(pp[:2, :w], lhsT=rel[:, :], rhs=Cs[:, c * 512:c * 512 + w],
                                             start=True, stop=True), R=[rel, Cs], W=[pp])
        kb.op("dve", lambda: nc.vector.tensor_copy(out=Es[:, c * 512:c * 512 + w], in_=pp[:2, :w]), R=[pp], W=[Es])
    kb.dma("sp", E_d[:, :], Es[:, :], R=[Es], W=[E_d])
    TT = [kb.sb([128, 17 * 128], F32, "TT") for _ in range(2)]
    for h in range(2):
        src = _AP(tensor=E_d.t.tensor, offset=h * NDEL, ap=[[1, 128], [1, 17 * 128]])
        kb.dma("sp", TT[h][:, :], src, R=[E_d], W=[TT[h]])
    fc = kb.sb([128, NQB, 2], F32, "fc")
    bf = kb.sb([128, 2], F32, "bf")
    lf = kb.sb([128, 2, NQB], F32, "lf")
    kb.dma("sp", fc[:, :, :], fc_d[:, :, :], R=[fc_d], W=[fc])
    kb.dma("sp", bf[:, :], bf_d[:, :], R=[bf_d], W=[bf])
    for h in range(2):
        kb.op("dve", lambda: nc.vector.tensor_scalar(out=lf[:, h, :], in0=fc[:, :, h], scalar1=bf[:, h:h + 1],
                                                     scalar2=None, op0=ALU.add), R=[fc, bf], W=[lf])
    kb.op("act", lambda: nc.scalar.activation(out=lf[:, :, :], in_=lf[:, :, :], func=AF.Exp, scale=-1.0), R=[lf], W=[lf])
    kb.op("act", lambda: nc.scalar.activation(out=lf[:, :, :], in_=lf[:, :, :], func=AF.Ln, bias=1.0), R=[lf], W=[lf])
    kb.op("dve", lambda: nc.vector.tensor_scalar(out=lf[:, :, :], in0=lf[:, :, :], scalar1=-1.0, scalar2=None,
                                                 op0=ALU.mult), R=[lf], W=[lf])
    lf2 = lf[:, :, :].rearrange("p h j -> p (h j)")
    p1, p2 = ax.O[0], ax.O[1]
    kb.op("pe", lambda: nc.tensor.matmul(p1[:, :128], lhsT=tri[:, :], rhs=lf2, start=True, stop=True),
          R=[tri, lf], W=[p1])
    kb.op("pe", lambda: nc.tensor.matmul(p2[:, :128], lhsT=ones[:, :], rhs=lf2, start=True, stop=True),
          R=[ones, lf], W=[p2])
    tot = kb.sb([128, 2, NQB], F32, "tot")
    carry = kb.sb([128, 2, NQB], F32, "carry")
    negF = kb.sb([128, 2, NQB], F32, "negF")
    kb.op("dve", lambda: nc.vector.tensor_copy(out=tot[:, :, :].rearrange("p h j -> p (h j)"), in_=p2[:, :128]),
          R=[p2], W=[tot])
    for h in range(2):
        kb.op("dve", lambda: nc.vector.tensor_tensor_scan(out=carry[:, h, :], data0=ones[:, :NQB], data1=tot[:, h, :],
                                                          initial=0.0, op0=ALU.mult, op1=ALU.add),
              R=[ones, tot], W=[carry])
    kb.op("dve", lambda: nc.vector.tensor_tensor(out=carry[:, :, :], in0=carry[:, :, :], in1=tot[:, :, :],
                                                 op=ALU.subtract), R=[carry, tot], W=[carry])
    kb.op("dve", lambda: nc.vector.tensor_tensor(out=negF[:, :, :].rearrange("p h j -> p (h j)"), in0=p1[:, :128],
                                                 in1=carry[:, :, :].rearrange("p h j -> p (h j)"), op=ALU.add),
          R=[p1, carry], W=[negF])
    kb.op("dve", lambda: nc.vector.tensor_scalar(out=negF[:, :, :], in0=negF[:, :, :], scalar1=-1.0, scalar2=None,
                                                 op0=ALU.mult), R=[negF], W=[negF])
    qc = kb.sb([128, SEQ], BF16, "qc")
    kc = kb.sb([128, SEQ], BF16, "kc")
    qd = kb.sb([128, SEQ], BF16, "qd")
    kd = kb.sb([128, SEQ], BF16, "kd")
    for n, (s, d_) in enumerate(((qd, qd_d), (kd, kd_d), (qc, qc_d), (kc, kc_d))):
        for c in range(2):
            kb.dma("sp" if (n + c) % 2 == 0 else "pool", s[:, c * 4096:(c + 1) * 4096], d_[:, c * 4096:(c + 1) * 4096],
                   R=[d_], W=[s])
    Vd = load_vaug(kb, vd_d, "Vd")
    Vc = load_vaug(kb, vc_d, "Vc")
    ostd = [kb.sb([128, 128], BF16, "ostd") for _ in range(2)]
    ostc = [kb.sb([128, 128], BF16, "ostc") for _ in range(2)]
    Bi = [kb.sb([128, NQB], F32, "Bi") for _ in range(3)]
    nb = 0
    for i in range(NQB):
        od = ostd[i % 2]
        for h in range(2):
            j0 = max(0, i - 16)
            attn_qblock(ax, qd, kd, Vd, h, i, list(range(j0, i + 1)), None,
                        lambda j, h=h, i=i: (TT[h], TT[h][:, (i - j) * 128:(i - j + 1) * 128]), od, h * 64,
                        after=(None if h == 0 else
                               (lambda i=i, od=od: kb.dma("pool", od_d[i * 128:(i + 1) * 128, :], od[:, :],
                                                          R=[od], W=[od_d]))))
        oc = ostc[i % 2]
        for h in range(2):
            B = Bi[nb % 3]
            nb += 1
            kb.op("dve", lambda: nc.vector.tensor_scalar(out=B[:, :i + 1], in0=negF[:, h, :i + 1],
                                                         scalar1=carry[:, h, i:i + 1], scalar2=None, op0=ALU.add),
                  R=[negF, carry], W=[B])
            attn_qblock(ax, qc, kc, Vc, h, i, list(range(0, i + 1)),
                        lambda j, B=B: (B, B[:, j:j + 1]),
                        lambda j, i=i: ((tri, tri[:, :]) if j == i else None), oc, h * 64,
                        after=(None if h == 0 else
                               (lambda i=i, oc=oc: kb.dma("sp", oc_d[i * 128:(i + 1) * 128, :], oc[:, :],
                                                          R=[oc], W=[oc_d]))))
    attn_flush(ax)
    return kb.finish()


TRI = np.triu(np.ones((128, 128), np.float32))


def to_pj(a):
    n = a.shape[1]
    return np.ascontiguousarray(a.reshape(NQB, 128, n).transpose(1, 0, 2))


def run_B_cd(yT, ytb, ytf, b_forget, rel_table):
    nc = build_B_cd()
    C = dil_const()
    maps = []
    for c in range(NCORES):
        b, m = c // 4, c % 4
        ts = slice(b * SEQ, (b + 1) * SEQ)
        rs = lambda base: slice(base + m * 128, base + (m + 1) * 128)
        maps.append({
            "qc": np.ascontiguousarray(yT[rs(0), ts]), "kc": np.ascontiguousarray(yT[rs(512), ts]),
            "qd": np.ascontiguousarray(yT[rs(1024), ts]),
            "kd": np.ascontiguousarray(yT[rs(1536), ts].reshape(128, NQB, 128)[:, :, ::-1].reshape(128, SEQ)),
            "vc": to_pj(ytb[ts, m * 128:(m + 1) * 128]), "vd": np.ascontiguousarray(to_pj(ytb[ts, 512 + m * 128:512 + (m + 1) * 128])[::-1]),
            "fc": to_pj(ytf[ts, 2 * m:2 * m + 2]),
            "bf": np.ascontiguousarray(np.broadcast_to(b_forget[None, 2 * m:2 * m + 2], (128, 2))).astype(np.float32),
            "tri": TRI, "rel": np.ascontiguousarray(rel_table[:, 2 * m:2 * m + 2]), "dilc": C,
        })
    res = run_spmd(nc, maps)
    o = np.zeros((BATCH * SEQ, D), NPBF)
    for c in range(NCORES):
        b, m = c // 4, c % 4
        o[b * SEQ:(b + 1) * SEQ, m * 128:(m + 1) * 128] = res[c]["oc"]
        o[b * SEQ:(b + 1) * SEQ, 512 + m * 128:512 + (m + 1) * 128] = res[c]["od"]
    return o


def build_B_gla():
    kb = KB()
    nc = kb.nc
    qT_d = kb.dram_in("qT", [64, SEQ], BF16)
    kT_d = kb.dram_in("kT", [64, SEQ], BF16)
    k_d = kb.dram_in("k", [128, NQB, 64], BF16)
    v_d = kb.dram_in("v", [128, NQB, 128], BF16)
    r_d = kb.dram_in("r", [128, NQB, 128], F32)
    g_d = kb.dram_in("g", [128, NQB, 64], F32)
    gn_d = kb.dram_in("gn", [128, 128], F32)
    tri_d = kb.dram_in("tri", [128, 128], F32)
    o_d = kb.dram_out("o", [SEQ, 128], BF16)
    qT = kb.sb([64, SEQ], BF16, "qT")
    kT = kb.sb([64, SEQ], BF16, "kT")
    ktm = kb.sb([128, NQB, 64], BF16, "ktm")
    v = kb.sb([128, NQB, 128], BF16, "v")
    r = kb.sb([128, NQB, 128], F32, "r")
    g = kb.sb([128, NQB, 64], F32, "g")
    gn = kb.sb([128, 128], F32, "gn")
    tri = kb.sb([128, 128], F32, "tri")
    eps = kb.sb([128, 1], F32, "eps")
    kb.op("dve", lambda: nc.vector.memset(eps[:, :], LN_EPS), W=[eps])
    kb.dma("sp", tri[:, :], tri_d[:, :], R=[tri_d], W=[tri])
    kb.dma("sp", g[:, :, :], g_d[:, :, :], R=[g_d], W=[g])
    kb.dma("pool", qT[:, :], qT_d[:, :], R=[qT_d], W=[qT])
    kb.dma("sp", kT[:, :], kT_d[:, :], R=[kT_d], W=[kT])
    kb.dma("pool", ktm[:, :, :], k_d[:, :, :], R=[k_d], W=[ktm])
    kb.dma("sp", v[:, :, :], v_d[:, :, :], R=[v_d], W=[v])
    kb.dma("pool", r[:, :, :], r_d[:, :, :], R=[r_d], W=[r])
    kb.dma("sp", gn[:, :], gn_d[:, :], R=[gn_d], W=[gn])
    kb.op("act", lambda: nc.scalar.activation(out=r[:, :, :], in_=r[:, :, :], func=AF.Silu), R=[r], W=[r])
    PG = [kb.ps([128, 512], F32, "PG") for _ in range(2)]
    PGT = [kb.ps([128, 512], F32, "PGT") for _ in range(2)]
    PA = [kb.ps([128, 512], F32, "PA") for _ in range(2)]
    PO = kb.ps([128, 512], F32, "PO")
    PU = kb.ps([128, 512], F32, "PU")
    eGT = [kb.sb([64, 128], F32, "eGT") for _ in range(2)]
    enGT = [kb.sb([64, 128], F32, "enGT") for _ in range(2)]
    enG = [kb.sb([128, 64], F32, "enG") for _ in range(2)]
    qgT = [kb.sb([64, 128], BF16, "qgT") for _ in range(2)]
    kgT = [kb.sb([64, 128], BF16, "kgT") for _ in range(2)]
    kg = [kb.sb([128, 64], BF16, "kg") for _ in range(2)]
    Am = [kb.sb([128, 128], BF16, "Am") for _ in range(2)]
    S32 = kb.sb([64, 128], F32, "S32")
    Sbf = kb.sb([64, 128], BF16, "Sbf")
    st6 = [kb.sb([128, 6], F32, "st6") for _ in range(2)]
    mv = [kb.sb([128, 4], F32, "mv") for _ in range(2)]
    of = [kb.sb([128, 128], F32, "of") for _ in range(2)]
    ost = [kb.sb([128, 128], BF16, "ost") for _ in range(2)]
    for c in range(NQB):
        p = c % 2
        cs = slice(c * 128, (c + 1) * 128)
        kb.op("pe", lambda: nc.tensor.matmul(PG[p][:, :64], lhsT=tri[:, :], rhs=g[:, c, :], start=True, stop=True),
              R=[tri, g], W=[PG[p]])
        kb.op("pe", lambda: nc.tensor.matmul(PGT[p][:64, :128], lhsT=g[:, c, :], rhs=tri[:, :], start=True, stop=True),
              R=[tri, g], W=[PGT[p]])
        kb.op("act", lambda: nc.scalar.activation(out=eGT[p][:, :], in_=PGT[p][:64, :128], func=AF.Exp),
              R=[PGT[p]], W=[eGT[p]])
        kb.op("act", lambda: nc.scalar.activation(out=enGT[p][:, :], in_=PGT[p][:64, :128], func=AF.Exp, scale=-1.0),
              R=[PGT[p]], W=[enGT[p]])
        kb.op("act", lambda: nc.scalar.activation(out=enG[p][:, :], in_=PG[p][:, :64], func=AF.Exp, scale=-1.0),
              R=[PG[p]], W=[enG[p]])
        kb.op("dve", lambda: nc.vector.scalar_tensor_tensor(out=qgT[p][:, :], in0=qT[:, cs], scalar=0.125,
                                                            in1=eGT[p][:, :], op0=ALU.mult, op1=ALU.mult),
              R=[qT, eGT[p]], W=[qgT[p]])
        kb.op("dve", lambda: nc.vector.tensor_tensor(out=kgT[p][:, :], in0=kT[:, cs], in1=enGT[p][:, :], op=ALU.mult),
              R=[kT, enGT[p]], W=[kgT[p]])
        kb.op("dve", lambda: nc.vector.tensor_tensor(out=kg[p][:, :], in0=ktm[:, c, :], in1=enG[p][:, :], op=ALU.mult),
              R=[ktm, enG[p]], W=[kg[p]])
        kb.op("pe", lambda: nc.tensor.matmul(PA[p][:, :128], lhsT=kgT[p][:, :], rhs=qgT[p][:, :], start=True, stop=True),
              R=[kgT[p], qgT[p]], W=[PA[p]])
        kb.op("dve", lambda: nc.vector.tensor_tensor(out=Am[p][:, :], in0=PA[p][:, :128], in1=tri[:, :], op=ALU.mult),
              R=[PA[p], tri], W=[Am[p]])
        kb.op("pe", lambda: nc.tensor.matmul(PO[:, :128], lhsT=Am[p][:, :], rhs=v[:, c, :], start=True, stop=(c == 0)),
              R=[Am[p], v], W=[PO])
        if c > 0:
            kb.op("pe", lambda: nc.tensor.matmul(PO[:, :128], lhsT=qgT[p][:, :], rhs=Sbf[:, :], start=False, stop=True),
                  R=[qgT[p], Sbf], W=[PO])
        if c < NQB - 1:
            kb.op("pe", lambda: nc.tensor.matmul(PU[:64, :128], lhsT=kg[p][:, :], rhs=v[:, c, :], start=True, stop=True),
                  R=[kg[p], v], W=[PU])
            eGl = eGT[p][:, 127:128]
            if c == 0:
                kb.op("dve", lambda: nc.vector.tensor_scalar(out=S32[:, :], in0=PU[:64, :128], scalar1=eGl, scalar2=None,
                                                             op0=ALU.mult), R=[PU, eGT[p]], W=[S32])
            else:
                kb.op("dve", lambda: nc.vector.tensor_scalar(out=S32[:, :], in0=S32[:, :], scalar1=eGl, scalar2=None,
                                                             op0=ALU.mult), R=[S32, eGT[p]], W=[S32])
                kb.op("dve", lambda: nc.vector.scalar_tensor_tensor(out=S32[:, :], in0=PU[:64, :128], scalar=eGl,
                                                                    in1=S32[:, :], op0=ALU.mult, op1=ALU.add),
                      R=[PU, eGT[p], S32], W=[S32])
            kb.op("dve", lambda: nc.vector.tensor_copy(out=Sbf[:, :], in_=S32[:, :]), R=[S32], W=[Sbf])
        kb.op("dve", lambda: nc.vector.bn_stats(out=st6[p][:, :], in_=PO[:, :128]), R=[PO], W=[st6[p]])
        kb.op("dve", lambda: nc.vector.bn_aggr(out=mv[p][:, 0:2], in_=st6[p][:, :]), R=[st6[p]], W=[mv[p]])
        kb.op("dve", lambda: nc.vector.scalar_tensor_tensor(out=mv[p][:, 2:3], in0=mv[p][:, 0:1], scalar=mv[p][:, 0:1],
                                                            in1=mv[p][:, 1:2], op0=ALU.mult, op1=ALU.add),
              R=[mv[p]], W=[mv[p]])
        kb.op("act", lambda: nc.scalar.activation(out=mv[p][:, 3:4], in_=mv[p][:, 2:3], func=AF.Ln, bias=eps[:, 0:1]),
              R=[mv[p], eps], W=[mv[p]])
        kb.op("act", lambda: nc.scalar.activation(out=mv[p][:, 3:4], in_=mv[p][:, 3:4], func=AF.Exp, scale=-0.5),
              R=[mv[p]], W=[mv[p]])
        kb.op("dve", lambda: nc.vector.scalar_tensor_tensor(out=of[p][:, :], in0=PO[:, :128], scalar=mv[p][:, 3:4],
                                                            in1=gn[:, :], op0=ALU.mult, op1=ALU.mult),
              R=[PO, mv[p], gn], W=[of[p]])
        kb.op("pool", lambda: nc.gpsimd.tensor_tensor(out=ost[p][:, :], in0=of[p][:, :], in1=r[:, c, :], op=ALU.mult),
              R=[of[p], r], W=[ost[p]])
        kb.dma("sp", o_d[cs, :], ost[p][:, :], R=[ost[p]], W=[o_d])
    return kb.finish()


def run_B_gla(yT, ytb, ytf, g_norm):
    nc = build_B_gla()
    maps = []
    for c in range(NCORES):
        b, h = c // 4, c % 4
        ts = slice(b * SEQ, (b + 1) * SEQ)
        maps.append({
            "qT": np.ascontiguousarray(yT[h * 64:(h + 1) * 64, ts]),
            "kT": np.ascontiguousarray(yT[256 + h * 64:256 + (h + 1) * 64, ts]),
            "k": to_pj(ytb[ts, h * 64:(h + 1) * 64]),
            "v": to_pj(ytb[ts, 256 + h * 128:256 + (h + 1) * 128]),
            "r": to_pj(ytf[ts, h * 128:(h + 1) * 128]),
            "g": to_pj(ytf[ts, 520 + h * 64:520 + (h + 1) * 64]),
            "gn": np.ascontiguousarray(np.broadcast_to(g_norm[None, :], (128, 128))).astype(np.float32),
            "tri": TRI,
        })
    res = run_spmd(nc, maps)
    o = np.zeros((BATCH * SEQ, 512), NPBF)
    for c in range(NCORES):
        b, h = c // 4, c % 4
        o[b * SEQ:(b + 1) * SEQ, h * 128:(h + 1) * 128] = res[c]["o"]
    return o


NBIS = 24
TOPK = 256


def build_B_dsa1(act_split=True):
    kb = KB()
    nc = kb.nc
    NK = 16
    qiT_d = kb.dram_in("qiT", [64, 8, NK * 128], BF16)
    kiT_d = kb.dram_in("kiT", [64, SEQ], BF16)
    wi_d = kb.dram_in("wi", [128, NK, 8], F32)
    cm_d = kb.dram_in("cmask", [128, 512], F32)
    idb_d = kb.dram_in("identb", [128, 128], BF16)
    stp_d = kb.dram_in("steps", [128, NBIS], F32)
    M_d = kb.dram_out("M", [NK, 128, SEQ], BF16)
    qiT = kb.sb([64, 8, NK * 128], BF16, "qiT")
    kiT = kb.sb([64, SEQ], BF16, "kiT")
    wi = kb.sb([128, NK, 8], F32, "wi")
    absw = kb.sb([128, NK, 8], F32, "absw")
    sgn = kb.sb([128, NK, 8], F32, "sgn")
    cm = kb.sb([128, 512], F32, "cm")
    idb = kb.sb([128, 128], BF16, "idb")
    stp = kb.sb([128, NBIS], F32, "stp")
    kb.dma("sp", qiT[:, :, :], qiT_d[:, :, :], R=[qiT_d], W=[qiT])
    kb.dma("pool", kiT[:, :], kiT_d[:, :], R=[kiT_d], W=[kiT])
    kb.dma("sp", wi[:, :, :], wi_d[:, :, :], R=[wi_d], W=[wi])
    kb.dma("sp", cm[:, :], cm_d[:, :], R=[cm_d], W=[cm])
    kb.dma("sp", idb[:, :], idb_d[:, :], R=[idb_d], W=[idb])
    kb.dma("sp", stp[:, :], stp_d[:, :], R=[stp_d], W=[stp])
    kb.op("act", lambda: nc.scalar.activation(out=absw[:, :, :], in_=wi[:, :, :], func=AF.Abs), R=[wi], W=[absw])
    kb.op("act", lambda: nc.scalar.activation(out=sgn[:, :, :], in_=wi[:, :, :], func=AF.Sign), R=[wi], W=[sgn])
    PS = [kb.ps([128, 512], F32, "PS") for _ in range(3)]
    PT = [kb.ps([128, 1024], BF16, "PT") for _ in range(2)]

    def write_mask(k, mt, Lk):
        kb.dma("sp", M_d[k, :, :Lk], mt[:, :Lk], R=[mt], W=[M_d])

    dsa1_body(kb, qiT, kiT, absw, sgn, cm, idb, stp, PS, PT, write_mask, act_split)
    return kb.finish()


def dsa1_body(kb, qiT, kiT, absw, sgn, cm, idb, stp, PS, PT, write_mask, act_split=True):
    nc = kb.nc
    NK = 16
    score = [kb.sb([128, SEQ], F32, "score") for _ in range(2)]
    selb = kb.sb([128, SEQ], BF16, "selb")
    junk2 = kb.sb([128, SEQ // 2], BF16, "junk2")
    MT = [kb.sb([128, SEQ], BF16, "MT") for _ in range(2)]
    rl = [kb.sb([128, 512], F32, "rl") for _ in range(3)]
    bs = [kb.sb([128, 16], F32, "bs") for _ in range(2)]
    stk = [kb.sb([128, NBIS], F32, "stk") for _ in range(2)]
    midb = [kb.sb([128, 1], F32, "midb") for _ in range(2)]
    cntd = [kb.sb([128, 1], F32, "cntd") for _ in range(2)]
    cnta = [kb.sb([128, 1], F32, "cnta") for _ in range(2)]
    tmpb = [kb.sb([128, 1], F32, "tmpb") for _ in range(2)]
    geb = [kb.sb([128, 1], F32, "geb") for _ in range(2)]
    st = {"rl": 0, "pt": 0}

    def scoring(k):
        sc_ = score[k % 2]
        for c in range(k + 1):
            for hi in range(8):
                pp = PS[st["rl"] % 3]
                r_ = rl[st["rl"] % 3]
                st["rl"] += 1
                kb.op("pe", lambda: nc.tensor.matmul(pp[:, :], lhsT=qiT[:, hi, k * 128:(k + 1) * 128],
                                                     rhs=kiT[:, c * 512:(c + 1) * 512], start=True, stop=True),
                      R=[qiT, kiT], W=[pp])
                kb.op("act", lambda: nc.scalar.activation(out=r_[:, :], in_=pp[:, :], func=AF.Relu,
                                                          scale=absw[:, k, hi:hi + 1]), R=[pp, absw], W=[r_])
                ssl = sc_[:, c * 512:(c + 1) * 512]
                if hi == 0:
                    kb.op("dve", lambda: nc.vector.tensor_scalar(out=ssl, in0=r_[:, :], scalar1=sgn[:, k, 0:1],
                                                                 scalar2=None, op0=ALU.mult), R=[r_, sgn], W=[sc_])
                else:
                    kb.op("dve", lambda: nc.vector.scalar_tensor_tensor(out=ssl, in0=r_[:, :],
                                                                        scalar=sgn[:, k, hi:hi + 1], in1=ssl,
                                                                        op0=ALU.mult, op1=ALU.add),
                          R=[r_, sgn, sc_], W=[sc_])

    def setup(k):
        L = 512 * (k + 1)
        sc_, b_, sk = score[k % 2], bs[k % 2], stk[k % 2]
        kb.op("dve", lambda: nc.vector.tensor_reduce(out=b_[:, 0:1], in_=sc_[:, :L], axis=AX.X, op=ALU.min),
              R=[sc_], W=[b_])
        kb.op("dve", lambda: nc.vector.tensor_reduce(out=b_[:, 1:2], in_=sc_[:, :L], axis=AX.X, op=ALU.max),
              R=[sc_], W=[b_])
        kb.op("dve", lambda: nc.vector.tensor_tensor(out=sc_[:, L - 512:L], in0=sc_[:, L - 512:L], in1=cm[:, :],
                                                     op=ALU.add), R=[sc_, cm], W=[sc_])
        kb.op("dve", lambda: nc.vector.tensor_tensor(out=b_[:, 2:3], in0=b_[:, 1:2], in1=b_[:, 0:1], op=ALU.subtract),
              R=[b_], W=[b_])
        kb.op("dve", lambda: nc.vector.tensor_scalar(out=sk[:, :], in0=stp[:, :], scalar1=b_[:, 2:3], scalar2=None,
                                                     op0=ALU.mult), R=[stp, b_], W=[sk])
        kb.op("dve", lambda: nc.vector.scalar_tensor_tensor(out=midb[k % 2][:, 0:1], in0=b_[:, 2:3], scalar=0.5,
                                                            in1=b_[:, 0:1], op0=ALU.mult, op1=ALU.add),
              R=[b_], W=[midb[k % 2]])

    def iteration(k, n):
        L = 512 * (k + 1)
        p = k % 2
        sc_, sk = score[p], stk[p]
        Ld = L // 2 if act_split else L
        if act_split:
            kb.op("act", lambda: nc.scalar.activation(out=junk2[:, :L - Ld], in_=sc_[:, Ld:L], func=AF.Sign,
                                                      bias=midb[p][:, 0:1], scale=-1.0, accum_out=cnta[p][:, 0:1]),
                  R=[sc_, midb[p]], W=[junk2, cnta[p]])
        kb.op("dve", lambda: nc.vector.tensor_scalar(out=selb[:, :Ld], in0=sc_[:, :Ld], scalar1=midb[p][:, 0:1],
                                                     scalar2=0.0, op0=ALU.is_ge, op1=ALU.add,
                                                     accum_out=cntd[p][:, 0:1]),
              R=[sc_, midb[p]], W=[selb, cntd[p]])
        thr = TOPK - 0.25
        if act_split:
            kb.op("dve", lambda: nc.vector.scalar_tensor_tensor(out=tmpb[p][:, 0:1], in0=cnta[p][:, 0:1], scalar=-0.5,
                                                                in1=cntd[p][:, 0:1], op0=ALU.mult, op1=ALU.add),
                  R=[cnta[p], cntd[p]], W=[tmpb[p]])
            thr -= 0.5 * (L - Ld)
            src, srcb = tmpb[p][:, 0:1], tmpb[p]
        else:
            src, srcb = cntd[p][:, 0:1], cntd[p]
        kb.op("dve", lambda: nc.vector.tensor_scalar(out=geb[p][:, 0:1], in0=src, scalar1=thr, scalar2=-0.5,
                                                     op0=ALU.is_ge, op1=ALU.add), R=[srcb], W=[geb[p]])
        kb.op("dve", lambda: nc.vector.scalar_tensor_tensor(out=midb[p][:, 0:1], in0=geb[p][:, 0:1],
                                                            scalar=sk[:, n:n + 1], in1=midb[p][:, 0:1],
                                                            op0=ALU.mult, op1=ALU.add),
              R=[geb[p], sk, midb[p]], W=[midb[p]])

    def finish_block(k):
        L = 512 * (k + 1)
        sc_, b_, sk = score[k % 2], bs[k % 2], stk[k % 2]
        kb.op("dve", lambda: nc.vector.scalar_tensor_tensor(out=b_[:, 8:9], in0=sk[:, NBIS - 1:NBIS], scalar=-0.5,
                                                            in1=midb[k % 2][:, 0:1], op0=ALU.mult, op1=ALU.add),
              R=[midb[k % 2], sk], W=[b_])
        kb.op("dve", lambda: nc.vector.tensor_scalar(out=selb[:, :L], in0=sc_[:, :L], scalar1=b_[:, 8:9], scalar2=None,
                                                     op0=ALU.is_ge), R=[sc_, b_], W=[selb])
        mt = MT[k % 2]
        nkb = 4 * (k + 1)
        for j0 in range(0, nkb, 8):
            pt = PT[st["pt"] % 2]
            for jj in range(8):
                j = j0 + jj
                if j >= nkb:
                    break
                kb.op("pe", lambda: nc.tensor.transpose(pt[:, jj * 128:(jj + 1) * 128], selb[:, j * 128:(j + 1) * 128],
                                                        idb[:, :]), R=[selb, idb], W=[pt])
            wdt = min(8, nkb - j0) * 128
            if st["pt"] % 2 == 0:
                kb.op("act", lambda: nc.scalar.copy(out=mt[:, j0 * 128:j0 * 128 + wdt], in_=pt[:, :wdt]), R=[pt], W=[mt])
            else:
                kb.op("dve", lambda: nc.vector.tensor_copy(out=mt[:, j0 * 128:j0 * 128 + wdt], in_=pt[:, :wdt]),
                      R=[pt], W=[mt])
            st["pt"] += 1
        write_mask(k, mt, L)

    for k0 in range(0, NK, 2):
        ks = (k0, k0 + 1)
        for k in ks:
            scoring(k)
        for k in ks:
            setup(k)
        for n in range(NBIS):
            for k in ks:
                iteration(k, n)
        for k in ks:
            finish_block(k)


def run_B_dsa1(yT, ytf):
    nc = build_B_dsa1()
    steps = np.ascontiguousarray(np.broadcast_to((0.5 ** np.arange(1, NBIS + 1))[None, :], (128, NBIS))).astype(np.float32)
    maps = []
    for c in range(NCORES):
        b, m = c // 4, c % 4
        ts = slice(b * SEQ, (b + 1) * SEQ)
        tok = (np.arange(16)[:, None] * 512 + m * 128 + np.arange(128)[None, :]).reshape(-1) + b * SEQ
        qi = yT[1536:2048][:, tok].reshape(8, 64, 2048).transpose(1, 0, 2)
        wi = ytf[tok, 512:520].reshape(16, 128, 8).transpose(1, 0, 2)
        cm = np.where(np.arange(512)[None, :] <= (128 * m + np.arange(128))[:, None], 0.0, NEG).astype(np.float32)
        maps.append({"qiT": np.ascontiguousarray(qi), "kiT": np.ascontiguousarray(yT[2048:2112, ts]),
                     "wi": np.ascontiguousarray(wi), "cmask": cm, "identb": IDENT.astype(NPBF), "steps": steps})
    res = run_spmd(nc, maps)
    out = []
    for b in range(BATCH):
        M = np.zeros((NQB, 128, SEQ), NPBF)
        for m in range(4):
            M[m::4] = res[b * 4 + m]["M"]
        out.append(M)
    return out


def dsa_const():
    C = np.zeros((32, NDEL), np.float32)
    dl = np.arange(NDEL) - 127
    ok = dl >= 0
    C[rel_bucket_np(dl)[ok], np.arange(NDEL)[ok]] = 1.0
    return C


NPACK = NQB * (NQB + 1) // 2


def build_B_dsa2():
    kb = KB()
    nc = kb.nc
    q_d = kb.dram_in("q", [128, SEQ], BF16)
    k_d = kb.dram_in("k", [128, SEQ], BF16)
    v_d = kb.dram_in("v", [128, NQB, 128], BF16)
    rel_d = kb.dram_in("rel", [32, 2], F32)
    rf_d = kb.dram_in("relfar", [128, 2], F32)
    C_d = kb.dram_in("dsac", [32, NDEL], F32)
    M_d = kb.dram_in("Mp", [NPACK * 128 * 128], BF16)
    o_d = kb.dram_out("o", [SEQ, 128], BF16)
    E_d = kb.dram_tmp("Escr", [2, NDEL], F32)
    ax = AttnCtx(kb)
    rel = kb.sb([32, 2], F32, "rel")
    rf = kb.sb([128, 2], F32, "rf")
    Cs = kb.sb([32, NDEL], F32, "Cs")
    Es = kb.sb([2, NDEL], F32, "Es")
    kb.dma("sp", rel[:, :], rel_d[:, :], R=[rel_d], W=[rel])
    kb.dma("sp", rf[:, :], rf_d[:, :], R=[rf_d], W=[rf])
    kb.dma("sp", Cs[:, :], C_d[:, :], R=[C_d], W=[Cs])
    kb.op("act", lambda: nc.scalar.activation(out=rel[:, :], in_=rel[:, :], func=AF.Exp), R=[rel], W=[rel])
    for c in range((NDEL + 511) // 512):
        w = min(512, NDEL - c * 512)
        pp = ax.S[c % 3]
        kb.op("pe", lambda: nc.tensor.matmul(pp[:2, :w], lhsT=rel[:, :], rhs=Cs[:, c * 512:c * 512 + w],
                                             start=True, stop=True), R=[rel, Cs], W=[pp])
        kb.op("dve", lambda: nc.vector.tensor_copy(out=Es[:, c * 512:c * 512 + w], in_=pp[:2, :w]), R=[pp], W=[Es])
    kb.dma("sp", E_d[:, :], Es[:, :], R=[Es], W=[E_d])
    TT = [kb.sb([128, 17 * 128], F32, "TT") for _ in range(2)]
    for h in range(2):
        src = _AP(tensor=E_d.t.tensor, offset=h * NDEL, ap=[[1, 128], [1, 17 * 128]])
        kb.dma("sp", TT[h][:, :], src, R=[E_d], W=[TT[h]])
    q = kb.sb([128, SEQ], BF16, "q")
    k = kb.sb([128, SEQ], BF16, "k")
    for n, (s, d_) in enumerate(((q, q_d), (k, k_d))):
        for c in range(2):
            kb.dma("sp" if (n + c) % 2 == 0 else "pool", s[:, c * 4096:(c + 1) * 4096], d_[:, c * 4096:(c + 1) * 4096],
                   R=[d_], W=[s])
    V = load_vaug(kb, v_d, "V")
    Ms = [kb.sb([128, SEQ], BF16, "Ms") for _ in range(2)]
    ost = [kb.sb([128, 128], BF16, "ost") for _ in range(2)]
    for i in range(NQB):
        ms = Ms[i % 2]
        W_ = (i + 1) * 128
        off = (i * (i + 1) // 2) * 128 * 128
        src = _AP(tensor=M_d.t.tensor, offset=off, ap=[[W_, 128], [1, W_]])
        kb.dma("pool" if i % 2 else "sp", ms[:, :W_], src, R=[M_d], W=[ms])
        o = ost[i % 2]
        for h in range(2):
            def bias_of(j, h=h, i=i):
                return (rf, rf[:, h:h + 1]) if i - j > 16 else None

            def mask_of(j, h=h, i=i, ms=ms):
                sel = (ms, ms[:, j * 128:(j + 1) * 128])
                if i - j > 16:
                    return [sel]
                return [(TT[h], TT[h][:, (i - j) * 128:(i - j + 1) * 128]), sel]

            attn_qblock(ax, q, k, V, h, i, list(range(0, i + 1)), bias_of, mask_of, o, h * 64,
                        after=(None if h == 0 else
                               (lambda i=i, o=o: kb.dma("sp", o_d[i * 128:(i + 1) * 128, :], o[:, :], R=[o], W=[o_d]))))
    attn_flush(ax)
    return kb.finish()


def run_B_dsa2(yT, ytb, Msel, rel_table):
    nc = build_B_dsa2()
    C = dsa_const()
    packed = []
    for b in range(BATCH):
        M = Msel[b][:, ::-1, :]
        packed.append(np.concatenate([np.ascontiguousarray(M[i][:, :(i + 1) * 128]).reshape(-1) for i in range(NQB)]))
    maps = []
    for c in range(NCORES):
        b, m = c // 4, c % 4
        ts = slice(b * SEQ, (b + 1) * SEQ)
        maps.append({
            "q": np.ascontiguousarray(yT[512 + m * 128:512 + (m + 1) * 128, ts]),
            "k": np.ascontiguousarray(yT[1024 + m * 128:1024 + (m + 1) * 128, ts].reshape(128, NQB, 128)[:, :, ::-1]
                                      .reshape(128, SEQ)),
            "v": np.ascontiguousarray(to_pj(ytb[ts, 768 + m * 128:768 + (m + 1) * 128])[::-1]),
            "rel": np.ascontiguousarray(rel_table[:, 2 * m:2 * m + 2]),
            "relfar": np.ascontiguousarray(np.broadcast_to(rel_table[31:32, 2 * m:2 * m + 2], (128, 2))).astype(np.float32),
            "dsac": C, "Mp": packed[b],
        })
    res = run_spmd(nc, maps)
    o = np.zeros((BATCH * SEQ, 512), NPBF)
    for c in range(NCORES):
        b, m = c // 4, c % 4
        o[b * SEQ:(b + 1) * SEQ, m * 128:(m + 1) * 128] = res[c]["o"]
    return o


def kernel_unfused(x, ln_g, ln_b, rel_table, w_in_ab, w_gate_a, b_gate_a, g_norm_a, w_out_ab,
           w_in_cd, b_forget, w_out_cd, w1_dense, w3_dense, w2_dense,
           w_router, w1_moe, w3_moe, w2_moe):
    f32 = lambda a: np.ascontiguousarray(np.asarray(a, dtype=np.float32))
    x = f32(x)
    rel_table = f32(rel_table)
    big = [f32(w_in_ab), f32(w_in_cd), f32(w_out_ab), f32(w_out_cd), f32(w1_dense), f32(w3_dense), f32(w2_dense),
           f32(w1_moe), f32(w3_moe), f32(w2_moe)]
    (wi_ab, wi_cd, wo_ab, wo_cd, w1d, w3d, w2d, w1m, w3m, w2m) = cast_weights(big)
    del big
    xf = x.reshape(BATCH * SEQ, D)
    for layer in range(DEPTH):
        j = layer // 2
        lnp4 = np.stack([ln_g[layer, 0], ln_b[layer, 0], ln_g[layer, 1], ln_b[layer, 1]]).astype(np.float32)
        if layer % 2 == 0:
            yT, ytb, ytf = run_A(xf, wi_ab[j],
                                 [(0, 256), (256, 256), (1552, 512), (2064, 512), (3088, 512), (3600, 64)],
                                 [(256, 256), (512, 512), (2576, 512)],
                                 [(1024, 512), (3664, 8)],
                                 gate=(1536,), wg=f32(w_gate_a[j]), bg=f32(b_gate_a[j]))
            oa = run_B_gla(yT, ytb, ytf, f32(g_norm_a[j]))
            Msel = run_B_dsa1(yT, ytf)
            ob = run_B_dsa2(yT, ytb, Msel, rel_table)
            del Msel
            o = np.concatenate([oa, ob], axis=1)
            wf = ffn_chunk_layout(w1d[j], w3d[j], w2d[j])
            xf = run_C(o, xf, wo_ab[j], lnp4, wf, 1, 11, 2)
        else:
            yT, ytb, ytf = run_A(xf, wi_cd[j],
                                 [(0, 512), (512, 512), (1544, 512), (2056, 512)],
                                 [(1024, 512), (2568, 512)],
                                 [(1536, 8)])
            o = run_B_cd(yT, ytb, ytf, f32(b_forget[j]), rel_table)
            wf = np.concatenate([ffn_chunk_layout(w1m[j, e], w3m[j, e], w2m[j, e]) for e in range(8)], axis=0)
            xf = run_C(o, xf, wo_cd[j], lnp4, wf, 8, 14, 2, wr=f32(w_router[j]))
    return xf.reshape(BATCH, SEQ, D).astype(np.float32)


I32 = mybir.dt.int32

import os
SKIP = os.environ.get('FZ_SKIP', '')
GROUPS = [[0, 1, 2, 3], [4, 5, 6, 7]]
LK = [512 * (k + 1) for k in range(16)]
MOFF = [128 * sum(LK[:k]) for k in range(16)]
MTOT = 128 * sum(LK)
MPARTS = []
_g = 0
for _k in range(16):
    _r0 = MOFF[_k] // 512
    _n = 128 * LK[_k] // 512
    _halves = 1 if _k < 8 else 2
    for _h in range(_halves):
        _nh = _n // _halves
        MPARTS.append((_k, _h, _r0 + _h * _nh, _nh, _g))
        _g += 4 * _nh
MG = {(p[0], p[1]): p for p in MPARTS}


class KBF(KB):
    def __init__(self):
        super().__init__()
        self.ccsem = self.es.enter_context(self.nc.semaphore("ccsem"))
        self.cccnt = 0
        self.stack = [self.es]

    def sb(self, shape, dt, name="sb"):
        return Buf(self.stack[-1].enter_context(self.nc.sbuf_tensor(self._nm(name), list(shape), dt)))

    def ps(self, shape, dt=F32, name="ps"):
        b = Buf(self.stack[-1].enter_context(self.nc.psum_tensor(self._nm(name), list(shape), dt)))
        b.psum = True
        return b

    def barrier(self):
        evs = []
        for q in ("sp", "act", "pool"):
            for i in range(self.NDS):
                if self.dcnt[q][i] > 0:
                    evs.append((self.dsem[q][i], self.dcnt[q][i], "d%s%d" % (q, i)))
        for e in ("pe", "dve", "act", "pool", "sp"):
            if self.ecnt[e] > 0:
                evs.append((self.esem[e], self.ecnt[e], e))
        if self.cccnt > 0:
            evs.append((self.ccsem, self.cccnt, "cc"))
        for e in ("pe", "dve", "act", "pool", "sp"):
            for ev in evs:
                if ev[2] == e:
                    continue
                self._wait(e, ev)

    def scope(self):
        kb = self

        class _S:
            def __enter__(s):
                st = ExitStack()
                kb.stack.append(st)
                return st

            def __exit__(s, *a):
                kb.barrier()
                st = kb.stack.pop()
                st.close()
                return False

        return _S()

    def dram_tmp(self, name, shape, dt):
        return Buf(self.nc.dram_tensor(name, list(shape), dt).ap())

    def collective(self, kind, src, dst, src_ap=None, dst_ap=None):
        self._deps("pool", [src], [dst])
        sa = src.t if src_ap is None else src_ap
        da = dst.t if dst_ap is None else dst_ap
        ins = self.nc.gpsimd.collective_compute(kind, ALU.bypass, replica_groups=GROUPS,
                                                ins=[sa.opt()], outs=[da.opt()])
        self.cccnt += CC_INC
        ins.then_inc(self.ccsem, CC_INC)
        ev = (self.ccsem, self.cccnt, "cc")
        _kbf_mark(self, ev, [src], [dst])
        return ev


CC_INC = 1


def load_w_cast(kb, dst, src_d, ncols):
    for kc in range(8):
        kb.dma("pool", dst[:, kc, :ncols], src_d[:, kc, :ncols], R=[src_d], Wd=[dst])


def load_xT_chunk(kb, x_, xT_all, c):
    r, t0 = c // 4, (c % 4) * 512
    for cf in range(4):
        row0 = (cf * 4 + r) * 256
        kb.dma("sp", x_[:, 2 * cf:2 * cf + 2, :],
               xT_all[row0:row0 + 256, t0:t0 + 512].rearrange("(k p) t -> p k t", p=128),
               R=[xT_all], Wd=[x_] if cf else (), W=[x_] if cf == 0 else ())


def emit_xT(kb, ax_ps, ident, xtile, tt, stg, xT_loc, xTs_loc):
    nc = kb.nc
    for kc in range(8):
        pt = ax_ps[kc // 4]
        kb.op("pe", lambda: nc.tensor.transpose(pt[:, (kc % 4) * 128:(kc % 4 + 1) * 128],
                                                xtile[:, kc * 128:(kc + 1) * 128], ident[:, :]),
              R=[xtile, ident], W=[pt])
    q = tt % 4
    for hf in range(2):
        src = ax_ps[hf][:, :].rearrange("p (k t) -> p k t", k=4)
        dst = stg[:, hf * 4:(hf + 1) * 4, q * 128:(q + 1) * 128]
        if hf == 0:
            kb.op("dve", lambda: nc.vector.tensor_copy(out=dst, in_=src), R=[ax_ps[hf]], W=[stg])
        else:
            kb.op("act", lambda: nc.scalar.copy(out=dst, in_=src), R=[ax_ps[hf]], W=[stg])
    if q == 3:
        g = tt // 4
        kb.dma("sp", xT_loc[:, g * 512:(g + 1) * 512].rearrange("(k p) t -> p k t", p=128), stg[:, :, :],
               R=[stg], Wd=[xT_loc])
        if xTs_loc is not None:
            for j in range(4):
                kb.dma("sp", xTs_loc[j * D:(j + 1) * D, g * 128:(g + 1) * 128].rearrange("(k p) t -> p k t", p=128),
                       stg[:, :, j * 128:(j + 1) * 128], R=[stg], Wd=[xTs_loc])


def toeplitz_load(kb, TT, E_d, h, q="act"):
    for s in range(128):
        kb.dma(q if s % 2 == 0 else "sp", TT[s:s + 1, :], E_d[h:h + 1, 127 - s:127 - s + 17 * 128], R=[E_d], Wd=[TT])


def build_E_table(kb, ax, rel_d, C_d, E_d):
    nc = kb.nc
    rel = kb.sb([32, 2], F32, "rel")
    Cs = kb.sb([32, NDEL], F32, "Cs")
    Es = kb.sb([2, NDEL], F32, "Es")
    kb.dma("sp", rel[:, :], rel_d[:, :], R=[rel_d], W=[rel])
    kb.dma("sp", Cs[:, :], C_d[:, :], R=[C_d], W=[Cs])
    kb.op("act", lambda: nc.scalar.activation(out=rel[:, :], in_=rel[:, :], func=AF.Exp), R=[rel], W=[rel])
    for c in range((NDEL + 511) // 512):
        w = min(512, NDEL - c * 512)
        pp = ax.S[c % 3]
        kb.op("pe", lambda: nc.tensor.matmul(pp[:2, :w], lhsT=rel[:, :], rhs=Cs[:, c * 512:c * 512 + w],
                                             start=True, stop=True), R=[rel, Cs], W=[pp])
        kb.op("dve", lambda: nc.vector.tensor_copy(out=Es[:, c * 512:c * 512 + w], in_=pp[:2, :w]), R=[pp], W=[Es])
    kb.dma("sp", E_d[:, :], Es[:, :], R=[Es], W=[E_d])


class OTOut:
    def __init__(self, kb, identb, oT_loc, row0, name):
        self.kb, self.identb, self.oT_loc, self.row0 = kb, identb, oT_loc, row0
        self.pt = [kb.ps([128, 1024], BF16, "ptO" + name) for _ in range(1)]
        self.stg = [kb.sb([128, 512], BF16, "stgO" + name) for _ in range(2)]
        self.n = 0

    def put(self, i, ost):
        kb, nc = self.kb, self.kb.nc
        g = i // 4
        st = self.stg[g % 2]
        pt = self.pt[0]
        q = i % 4
        kb.op("pe", lambda: nc.tensor.transpose(pt[:, q * 128:(q + 1) * 128], ost[:, :], self.identb[:, :]),
              R=[ost, self.identb], W=[pt])
        if q == 3:
            kb.op("act", lambda: nc.scalar.copy(out=st[:, :], in_=pt[:, :512]), R=[pt], W=[st])
            rank, grp = g // 4, g % 4
            r0 = (rank * 4 + grp) * 256 + self.row0
            kb.dma("sp", self.oT_loc[r0:r0 + 128, :], st[:, :], R=[st], Wd=[self.oT_loc])


def phase_cd(kb, L, xT_all, oT_loc, cst):
    nc = kb.nc
    with kb.scope():
        wcd_d = L["wcd"]
        NCOL = 770
        w = kb.sb([128, 8, NCOL], BF16, "wcd")
        load_w_cast(kb, w, wcd_d, NCOL)
        tri = kb.sb([128, 128], F32, "tri")
        ones = kb.sb([128, 128], F32, "ones")
        identb = kb.sb([128, 128], BF16, "identb")
        kb.dma("sp", tri[:, :], cst["tri"][:, :], R=[cst["tri"]], W=[tri])
        kb.dma("sp", identb[:, :], cst["identb"][:, :], R=[cst["identb"]], W=[identb])
        kb.op("dve", lambda: nc.vector.memset(ones[:, :], 1.0), W=[ones])
        ax = AttnCtx(kb, n_s=4, n_o=2, grouped=True)
        E_d = L["Escr"]
        with kb.scope():
            build_E_table(kb, ax, L["rel2"], cst["dilc"], E_d)
        TT = [kb.sb([128, 17 * 128], F32, "TT") for _ in range(2)]
        for h in range(2):
            if "toep" in SKIP:
                kb.op("dve", lambda: nc.vector.memset(TT[h][:, :], 1.0), W=[TT[h]])
            else:
                toeplitz_load(kb, TT[h], E_d, h)
        qc = kb.sb([128, SEQ], BF16, "qc")
        kc_ = kb.sb([128, SEQ], BF16, "kc")
        qd = kb.sb([128, SEQ], BF16, "qd")
        kd = kb.sb([128, SEQ], BF16, "kd")
        Vc = kb.sb([128, NQB, 2, 65], BF16, "Vc")
        Vd = kb.sb([128, NQB, 2, 65], BF16, "Vd")
        fc = kb.sb([128, NQB, 2], F32, "fc")
        kb.op("pool", lambda: nc.gpsimd.memset(Vc[:, :, :, :], 1.0), W=[Vc])
        kb.op("pool", lambda: nc.gpsimd.memset(Vd[:, :, :, :], 1.0), W=[Vd])
        xc = [kb.sb([128, 8, 512], BF16, "xc") for _ in range(2)]
        n_ev = 0
        passes = [(True, True)] if "twopass" not in SKIP else [(True, False), (False, True)]
        NCH = int(os.environ.get("FZ_NCH", "16"))
        for c2 in range((NCH if "proj" not in SKIP else 0) * len(passes)):
            c = c2 % NCH
            do_fm, do_tm = passes[c2 // NCH]
            x_ = xc[c % 2]
            load_xT_chunk(kb, x_, xT_all, c)
            for bi, dst in enumerate((qc, kc_, qd, kd) if ("projfm" not in SKIP and do_fm) else ()):
                pp = ax.S[n_ev % 4]
                for k8 in range(8):
                    kb.op("pe", lambda: nc.tensor.matmul(pp[:, :], lhsT=w[:, k8, bi * 128:(bi + 1) * 128],
                                                         rhs=x_[:, k8, :], start=(k8 == 0), stop=(k8 == 7)),
                          R=[w, x_], W=[pp])
                d_ = dst[:, c * 512:(c + 1) * 512]
                if n_ev % 2 == 0 or "fmdve" in SKIP:
                    kb.op("dve", lambda: nc.vector.tensor_copy(out=d_, in_=pp[:, :]), R=[pp], W=[dst])
                else:
                    kb.op("act", lambda: nc.scalar.copy(out=d_, in_=pp[:, :]), R=[pp], W=[dst])
                n_ev += 1
            for t4 in range(4 if ("projtm" not in SKIP and do_tm) else 0):
                j = c * 4 + t4
                pp = ax.S[n_ev % 4]
                n_ev += 1
                for k8 in range(8):
                    kb.op("pe", lambda: nc.tensor.matmul(pp[:, :258], lhsT=x_[:, k8, t4 * 128:(t4 + 1) * 128],
                                                         rhs=w[:, k8, 512:770], start=(k8 == 0), stop=(k8 == 7)),
                          R=[w, x_], W=[pp])
                kb.op("dve", lambda: nc.vector.tensor_copy(out=Vc[:, j, :, 0:64],
                                                           in_=pp[:, 0:128].rearrange("p (h d) -> p h d", h=2)),
                      R=[pp], W=[Vc])
                if "vddve" in SKIP:
                    kb.op("dve", lambda: nc.vector.tensor_copy(out=Vd[:, j, :, 0:64],
                                                               in_=pp[:, 128:256].rearrange("p (h d) -> p h d", h=2)),
                          R=[pp], W=[Vd])
                else:
                    kb.op("act", lambda: nc.scalar.copy(out=Vd[:, j, :, 0:64],
                                                        in_=pp[:, 128:256].rearrange("p (h d) -> p h d", h=2)),
                          R=[pp], W=[Vd])
                kb.op("dve", lambda: nc.vector.tensor_copy(out=fc[:, j, :], in_=pp[:, 256:258]), R=[pp], W=[fc])
        bf = kb.sb([128, 2], F32, "bf")
        lf = kb.sb([128, 2, NQB], F32, "lf")
        kb.dma("sp", bf[:, :], L["bf"][:, :], R=[L["bf"]], W=[bf])
        for h in range(2):
            kb.op("dve", lambda: nc.vector.tensor_scalar(out=lf[:, h, :], in0=fc[:, :, h], scalar1=bf[:, h:h + 1],
                                                         scalar2=None, op0=ALU.add), R=[fc, bf], W=[lf])
        kb.op("act", lambda: nc.scalar.activation(out=lf[:, :, :], in_=lf[:, :, :], func=AF.Exp, scale=-1.0),
              R=[lf], W=[lf])
        kb.op("act", lambda: nc.scalar.activation(out=lf[:, :, :], in_=lf[:, :, :], func=AF.Ln, bias=1.0),
              R=[lf], W=[lf])
        kb.op("dve", lambda: nc.vector.tensor_scalar(out=lf[:, :, :], in0=lf[:, :, :], scalar1=-1.0, scalar2=None,
                                                     op0=ALU.mult), R=[lf], W=[lf])
        lf2 = lf[:, :, :].rearrange("p h j -> p (h j)")
        p1, p2 = ax.O[0], ax.O[1]
        kb.op("pe", lambda: nc.tensor.matmul(p1[:, :128], lhsT=tri[:, :], rhs=lf2, start=True, stop=True),
              R=[tri, lf], W=[p1])
        kb.op("pe", lambda: nc.tensor.matmul(p2[:, :128], lhsT=ones[:, :], rhs=lf2, start=True, stop=True),
              R=[ones, lf], W=[p2])
        tot = kb.sb([128, 2, NQB], F32, "tot")
        carry = kb.sb([128, 2, NQB], F32, "carry")
        negF = kb.sb([128, 2, NQB], F32, "negF")
        kb.op("dve", lambda: nc.vector.tensor_copy(out=tot[:, :, :].rearrange("p h j -> p (h j)"), in_=p2[:, :128]),
              R=[p2], W=[tot])
        for h in range(2):
            kb.op("dve", lambda: nc.vector.tensor_tensor_scan(out=carry[:, h, :], data0=ones[:, :NQB],
                                                              data1=tot[:, h, :], initial=0.0, op0=ALU.mult,
                                                              op1=ALU.add), R=[ones, tot], W=[carry])
        kb.op("dve", lambda: nc.vector.tensor_tensor(out=carry[:, :, :], in0=carry[:, :, :], in1=tot[:, :, :],
                                                     op=ALU.subtract), R=[carry, tot], W=[carry])
        kb.op("dve", lambda: nc.vector.tensor_tensor(out=negF[:, :, :].rearrange("p h j -> p (h j)"), in0=p1[:, :128],
                                                     in1=carry[:, :, :].rearrange("p h j -> p (h j)"), op=ALU.add),
              R=[p1, carry], W=[negF])
        kb.op("dve", lambda: nc.vector.tensor_scalar(out=negF[:, :, :], in0=negF[:, :, :], scalar1=-1.0, scalar2=None,
                                                     op0=ALU.mult), R=[negF], W=[negF])
        outc = OTOut(kb, identb, oT_loc, 0, "c")
        outd = OTOut(kb, identb, oT_loc, 128, "d")
        ostd = [kb.sb([128, 128], BF16, "ostd") for _ in range(2)]
        ostc = [kb.sb([128, 128], BF16, "ostc") for _ in range(2)]
        Bi = [kb.sb([128, NQB], F32, "Bi") for _ in range(3)]
        nb = 0
        for i in range(NQB if "attn" not in SKIP else 0):
            od = ostd[i % 2]
            for h in range(2):
                jdesc = list(range(i, max(0, i - 16) - 1, -1))
                groups = []
                for a in range(0, len(jdesc), 4):
                    js = jdesc[a:a + 4]
                    d0 = i - js[0]
                    groups.append({"js": js, "dve_mask": (TT[h], TT[h][:, d0 * 128:(d0 + len(js)) * 128])})
                attn_qgroup(ax, qd, kd, Vd, h, i, groups, od, h * 64,
                            after=(None if h == 0 else (lambda i=i, od=od: outd.put(i, od))))
            oc = ostc[i % 2]
            for h in range(2):
                B = Bi[nb % 3]
                nb += 1
                kb.op("dve", lambda: nc.vector.tensor_scalar(out=B[:, :i + 1], in0=negF[:, h, :i + 1],
                                                             scalar1=carry[:, h, i:i + 1], scalar2=None, op0=ALU.add),
                      R=[negF, carry], W=[B])
                attn_qblock(ax, qc, kc_, Vc, h, i, list(range(0, i + 1)),
                            lambda j, B=B: (B, B[:, j:j + 1]),
                            lambda j, i=i: ((tri, tri[:, :]) if j == i else None), oc, h * 64,
                            after=(None if h == 0 else (lambda i=i, oc=oc: outc.put(i, oc))))
        attn_flush(ax)


def phase_gla(kb, L, xT_all, oT_loc, cst):
    nc = kb.nc
    with kb.scope():
        tri = kb.sb([128, 128], F32, "tri")
        identb = kb.sb([128, 128], BF16, "identb")
        gn = kb.sb([128, 128], F32, "gn")
        eps = kb.sb([128, 1], F32, "eps")
        kb.dma("sp", tri[:, :], cst["tri"][:, :], R=[cst["tri"]], W=[tri])
        kb.dma("sp", identb[:, :], cst["identb"][:, :], R=[cst["identb"]], W=[identb])
        kb.dma("sp", gn[:, :], L["gn"][:, :], R=[L["gn"]], W=[gn])
        kb.op("dve", lambda: nc.vector.memset(eps[:, :], LN_EPS), W=[eps])
        qT = kb.sb([64, SEQ], BF16, "qT")
        kT = kb.sb([64, SEQ], BF16, "kT")
        ktm = kb.sb([128, NQB, 64], BF16, "ktm")
        v = kb.sb([128, NQB, 128], BF16, "v")
        r = kb.sb([128, NQB, 128], F32, "r")
        g = kb.sb([128, NQB, 64], F32, "g")
        PG = [kb.ps([128, 512], F32, "PG")]
        PGT = [kb.ps([128, 512], F32, "PGT")]
        PA = [kb.ps([128, 512], F32, "PA") for _ in range(2)]
        PO = kb.ps([128, 512], F32, "PO")
        PU = kb.ps([128, 512], F32, "PU")
        out = OTOut(kb, identb, oT_loc, 0, "g")
        with kb.scope():
            NCOL = 464
            w = kb.sb([128, 8, NCOL], BF16, "wgl")
            load_w_cast(kb, w, L["wgl"], NCOL)
            wg = kb.sb([16, 64], F32, "wg")
            bg = kb.sb([128, 64], F32, "bg")
            kb.dma("sp", wg[:, :], L["wg"][:, :], R=[L["wg"]], W=[wg])
            kb.dma("sp", bg[:, :], L["bg"][:, :], R=[L["bg"]], W=[bg])
            xc = [kb.sb([128, 8, 512], BF16, "xc") for _ in range(2)]
            gaT = [kb.sb([16, 512], F32, "gaT") for _ in range(2)]
            zt = [kb.sb([128, 64], F32, "zt") for _ in range(2)]
            pr = [PA[0], PA[1], PO]
            n_ev = 0
            for c in range(16):
                x_ = xc[c % 2]
                ga_ = gaT[c % 2]
                load_xT_chunk(kb, x_, xT_all, c)
                for (c0, ncol, dst) in ((0, 64, qT), (64, 64, kT), (128, 16, ga_)):
                    pp = pr[n_ev % 3]
                    for k8 in range(8):
                        kb.op("pe", lambda: nc.tensor.matmul(pp[:ncol, :], lhsT=w[:, k8, c0:c0 + ncol], rhs=x_[:, k8, :],
                                                             start=(k8 == 0), stop=(k8 == 7)), R=[w, x_], W=[pp])
                    d_ = dst[:, c * 512:(c + 1) * 512] if dst is not ga_ else ga_[:, :]
                    if n_ev % 2 == 0:
                        kb.op("dve", lambda: nc.vector.tensor_copy(out=d_, in_=pp[:ncol, :]), R=[pp], W=[dst])
                    else:
                        kb.op("act", lambda: nc.scalar.copy(out=d_, in_=pp[:ncol, :]), R=[pp], W=[dst])
                    n_ev += 1
                for t4 in range(4):
                    j = c * 4 + t4
                    pp = pr[n_ev % 3]
                    n_ev += 1
                    for k8 in range(8):
                        kb.op("pe", lambda: nc.tensor.matmul(pp[:, :320], lhsT=x_[:, k8, t4 * 128:(t4 + 1) * 128],
                                                             rhs=w[:, k8, 144:464], start=(k8 == 0), stop=(k8 == 7)),
                              R=[w, x_], W=[pp])
                    kb.op("dve", lambda: nc.vector.tensor_copy(out=ktm[:, j, :], in_=pp[:, 0:64]), R=[pp], W=[ktm])
                    kb.op("dve", lambda: nc.vector.tensor_copy(out=v[:, j, :], in_=pp[:, 64:192]), R=[pp], W=[v])
                    kb.op("act", lambda: nc.scalar.activation(out=r[:, j, :], in_=pp[:, 192:320], func=AF.Silu),
                          R=[pp], W=[r])
                    pq = pr[n_ev % 3]
                    n_ev += 1
                    z = zt[j % 2]
                    kb.op("pe", lambda: nc.tensor.matmul(pq[:, :64], lhsT=ga_[:, t4 * 128:(t4 + 1) * 128], rhs=wg[:, :],
                                                         start=True, stop=True), R=[ga_, wg], W=[pq])
                    kb.op("dve", lambda: nc.vector.tensor_tensor(out=z[:, :], in0=pq[:, :64], in1=bg[:, :], op=ALU.add),
                          R=[pq, bg], W=[z])
                    kb.op("act", lambda: nc.scalar.activation(out=z[:, :], in_=z[:, :], func=AF.Exp, scale=-1.0),
                          R=[z], W=[z])
                    kb.op("act", lambda: nc.scalar.activation(out=z[:, :], in_=z[:, :], func=AF.Ln, bias=1.0),
                          R=[z], W=[z])
                    kb.op("dve", lambda: nc.vector.tensor_scalar(out=g[:, j, :], in0=z[:, :], scalar1=-1.0 / 16.0,
                                                                 scalar2=None, op0=ALU.mult), R=[z], W=[g])
        eGT = [kb.sb([64, 128], F32, "eGT") for _ in range(2)]
        enGT = [kb.sb([64, 128], F32, "enGT") for _ in range(2)]
        enG = [kb.sb([128, 64], F32, "enG") for _ in range(2)]
        qgT = [kb.sb([64, 128], BF16, "qgT") for _ in range(2)]
        kgT = [kb.sb([64, 128], BF16, "kgT") for _ in range(2)]
        kg = [kb.sb([128, 64], BF16, "kg") for _ in range(2)]
        Am = [kb.sb([128, 128], BF16, "Am") for _ in range(2)]
        S32 = kb.sb([64, 128], F32, "S32")
        Sbf = kb.sb([64, 128], BF16, "Sbf")
        st6 = [kb.sb([128, 6], F32, "st6") for _ in range(2)]
        mv = [kb.sb([128, 4], F32, "mv") for _ in range(2)]
        of = [kb.sb([128, 128], F32, "of") for _ in range(2)]
        ost = [kb.sb([128, 128], BF16, "ost") for _ in range(2)]
        for c in range(NQB):
            p = c % 2
            cs = slice(c * 128, (c + 1) * 128)
            kb.op("pe", lambda: nc.tensor.matmul(PG[0][:, :64], lhsT=tri[:, :], rhs=g[:, c, :], start=True, stop=True),
                  R=[tri, g], W=[PG[0]])
            kb.op("pe", lambda: nc.tensor.matmul(PGT[0][:64, :128], lhsT=g[:, c, :], rhs=tri[:, :], start=True, stop=True),
                  R=[tri, g], W=[PGT[0]])
            kb.op("act", lambda: nc.scalar.activation(out=eGT[p][:, :], in_=PGT[0][:64, :128], func=AF.Exp),
                  R=[PGT[0]], W=[eGT[p]])
            kb.op("act", lambda: nc.scalar.activation(out=enGT[p][:, :], in_=PGT[0][:64, :128], func=AF.Exp, scale=-1.0),
                  R=[PGT[0]], W=[enGT[p]])
            kb.op("act", lambda: nc.scalar.activation(out=enG[p][:, :], in_=PG[0][:, :64], func=AF.Exp, scale=-1.0),
                  R=[PG[0]], W=[enG[p]])
            kb.op("dve", lambda: nc.vector.scalar_tensor_tensor(out=qgT[p][:, :], in0=qT[:, cs], scalar=0.125,
                                                                in1=eGT[p][:, :], op0=ALU.mult, op1=ALU.mult),
                  R=[qT, eGT[p]], W=[qgT[p]])
            kb.op("dve", lambda: nc.vector.tensor_tensor(out=kgT[p][:, :], in0=kT[:, cs], in1=enGT[p][:, :], op=ALU.mult),
                  R=[kT, enGT[p]], W=[kgT[p]])
            kb.op("dve", lambda: nc.vector.tensor_tensor(out=kg[p][:, :], in0=ktm[:, c, :], in1=enG[p][:, :], op=ALU.mult),
                  R=[ktm, enG[p]], W=[kg[p]])
            kb.op("pe", lambda: nc.tensor.matmul(PA[p][:, :128], lhsT=kgT[p][:, :], rhs=qgT[p][:, :], start=True, stop=True),
                  R=[kgT[p], qgT[p]], W=[PA[p]])
            kb.op("dve", lambda: nc.vector.tensor_tensor(out=Am[p][:, :], in0=PA[p][:, :128], in1=tri[:, :], op=ALU.mult),
                  R=[PA[p], tri], W=[Am[p]])
            kb.op("pe", lambda: nc.tensor.matmul(PO[:, :128], lhsT=Am[p][:, :], rhs=v[:, c, :], start=True, stop=(c == 0)),
                  R=[Am[p], v], W=[PO])
            if c > 0:
                kb.op("pe", lambda: nc.tensor.matmul(PO[:, :128], lhsT=qgT[p][:, :], rhs=Sbf[:, :], start=False, stop=True),
                      R=[qgT[p], Sbf], W=[PO])
            if c < NQB - 1:
                kb.op("pe", lambda: nc.tensor.matmul(PU[:64, :128], lhsT=kg[p][:, :], rhs=v[:, c, :], start=True, stop=True),
                      R=[kg[p], v], W=[PU])
                eGl = eGT[p][:, 127:128]
                if c == 0:
                    kb.op("dve", lambda: nc.vector.tensor_scalar(out=S32[:, :], in0=PU[:64, :128], scalar1=eGl,
                                                                 scalar2=None, op0=ALU.mult), R=[PU, eGT[p]], W=[S32])
                else:
                    kb.op("dve", lambda: nc.vector.tensor_scalar(out=S32[:, :], in0=S32[:, :], scalar1=eGl, scalar2=None,
                                                                 op0=ALU.mult), R=[S32, eGT[p]], W=[S32])
                    kb.op("dve", lambda: nc.vector.scalar_tensor_tensor(out=S32[:, :], in0=PU[:64, :128], scalar=eGl,
                                                                        in1=S32[:, :], op0=ALU.mult, op1=ALU.add),
                          R=[PU, eGT[p], S32], W=[S32])
                kb.op("dve", lambda: nc.vector.tensor_copy(out=Sbf[:, :], in_=S32[:, :]), R=[S32], W=[Sbf])
            kb.op("dve", lambda: nc.vector.bn_stats(out=st6[p][:, :], in_=PO[:, :128]), R=[PO], W=[st6[p]])
            kb.op("dve", lambda: nc.vector.bn_aggr(out=mv[p][:, 0:2], in_=st6[p][:, :]), R=[st6[p]], W=[mv[p]])
            kb.op("dve", lambda: nc.vector.scalar_tensor_tensor(out=mv[p][:, 2:3], in0=mv[p][:, 0:1], scalar=mv[p][:, 0:1],
                                                                in1=mv[p][:, 1:2], op0=ALU.mult, op1=ALU.add),
                  R=[mv[p]], W=[mv[p]])
            kb.op("act", lambda: nc.scalar.activation(out=mv[p][:, 3:4], in_=mv[p][:, 2:3], func=AF.Ln, bias=eps[:, 0:1]),
                  R=[mv[p], eps], W=[mv[p]])
            kb.op("act", lambda: nc.scalar.activation(out=mv[p][:, 3:4], in_=mv[p][:, 3:4], func=AF.Exp, scale=-0.5),
                  R=[mv[p]], W=[mv[p]])
            kb.op("dve", lambda: nc.vector.scalar_tensor_tensor(out=of[p][:, :], in0=PO[:, :128], scalar=mv[p][:, 3:4],
                                                                in1=gn[:, :], op0=ALU.mult, op1=ALU.mult),
                  R=[PO, mv[p], gn], W=[of[p]])
            kb.op("pool", lambda: nc.gpsimd.tensor_tensor(out=ost[p][:, :], in0=of[p][:, :], in1=r[:, c, :], op=ALU.mult),
                  R=[of[p], r], W=[ost[p]])
            out.put(c, ost[p])


def _kbf_deps(self, e, R, W, Wd=()):
    evs = []
    for b in R:
        evs.extend(b.wl())
        if getattr(b, "psum", False):
            evs.extend(x for x in b.r if x[2] != e)
    for b in W:
        evs.extend(b.wl())
        evs.extend(b.r)
    for b in Wd:
        evs.extend(b.r)
        evs.extend(getattr(b, "w_excl", []))
    best = {}
    for ev in evs:
        k = ev[2]
        if e == "pe" and k == "pe":
            continue
        if k not in best or best[k][1] < ev[1]:
            best[k] = ev
    for ev in best.values():
        self._wait(e, ev)


def _buf_wl(self):
    if self.w is None:
        return []
    return self.w if isinstance(self.w, list) else [self.w]


Buf.wl = _buf_wl


def _kbf_mark(self, ev, R, W, Wd=()):
    KB._mark(self, ev, R, W)
    for b in W:
        b.w = [ev]
        b.w_excl = [ev]
    for b in Wd:
        cur = b.wl()
        cur = [x for x in cur if x[2] != ev[2]] + [ev]
        b.w = cur
        b.r = []


def _kbf_dma(self, q, out, in_, R=(), W=(), Wd=(), **kw):
    _kbf_deps(self, q, R, W, Wd)
    i = self.dnext[q]
    self.dnext[q] = (i + 1) % self.NDS
    key = "d%s%d" % (q, i)
    if self.dcnt[q][i] > 0:
        self._wait(q, (self.dsem[q][i], self.dcnt[q][i], key))
    self.dcnt[q][i] += 16
    self.eng[q].dma_start(out=out, in_=in_, **kw).then_inc(self.dsem[q][i], 16)
    ev = (self.dsem[q][i], self.dcnt[q][i], key)
    _kbf_mark(self, ev, R, W, Wd)
    return ev


def _kbf_op(self, e, fn, R=(), W=()):
    _kbf_deps(self, e, R, W)
    ins = fn()
    self.ecnt[e] += 1
    ins.then_inc(self.esem[e], 1)
    ev = (self.esem[e], self.ecnt[e], e)
    _kbf_mark(self, ev, R, W)
    return ev


def _kbf_idma(self, out, in_, idx_ap, R=(), W=(), Wd=()):
    q = "pool"
    _kbf_deps(self, q, R, W, Wd)
    i = self.dnext[q]
    self.dnext[q] = (i + 1) % self.NDS
    key = "d%s%d" % (q, i)
    if self.dcnt[q][i] > 0:
        self._wait(q, (self.dsem[q][i], self.dcnt[q][i], key))
    self.dcnt[q][i] += 16
    self.nc.gpsimd.indirect_dma_start(out=out, out_offset=None, in_=in_,
                                      in_offset=bass.IndirectOffsetOnAxis(ap=idx_ap, axis=0)
                                      ).then_inc(self.dsem[q][i], 16)
    ev = (self.dsem[q][i], self.dcnt[q][i], key)
    _kbf_mark(self, ev, R, W, Wd)
    return ev


KBF.idma = _kbf_idma
KBF._deps = lambda self, e, R, W: _kbf_deps(self, e, R, W)
KBF.dma = _kbf_dma
KBF.op = _kbf_op


def phase_dsa1(kb, L, xT_all, xTs_all, M_loc, M_all, cst, act_split=True):
    nc = kb.nc
    NK = 16
    with kb.scope():
        qiT = kb.sb([64, 8, NK * 128], BF16, "qiT")
        kiT = kb.sb([64, SEQ], BF16, "kiT")
        wi = kb.sb([128, NK, 8], F32, "wi")
        absw = kb.sb([128, NK, 8], F32, "absw")
        sgn = kb.sb([128, NK, 8], F32, "sgn")
        cm = kb.sb([128, 512], F32, "cm")
        idb = kb.sb([128, 128], BF16, "idb")
        stp = kb.sb([128, NBIS], F32, "stp")
        kb.dma("sp", cm[:, :], cst["cmask"][:, :], R=[cst["cmask"]], W=[cm])
        kb.dma("sp", idb[:, :], cst["identb"][:, :], R=[cst["identb"]], W=[idb])
        kb.dma("sp", stp[:, :], cst["steps"][:, :], R=[cst["steps"]], W=[stp])
        idxs = kb.sb([128, 32], I32, "idxs")
        kb.dma("sp", idxs[:, :], cst["idx_s"][:, :], R=[cst["idx_s"]], W=[idxs])
        PS = [kb.ps([128, 512], F32, "PS") for _ in range(3)]
        PT = [kb.ps([128, 1024], BF16, "PT") for _ in range(2)]
        with kb.scope():
            NCOL = 584
            w = kb.sb([128, 8, NCOL], BF16, "wd1")
            load_w_cast(kb, w, L["wd1"], NCOL)
            xc = [kb.sb([128, 8, 512], BF16, "xc") for _ in range(2)]
            n_ev = 0
            for c in range(16):
                x_ = xc[c % 2]
                load_xT_chunk(kb, x_, xT_all, c)
                pp = PS[n_ev % 3]
                n_ev += 1
                for k8 in range(8):
                    kb.op("pe", lambda: nc.tensor.matmul(pp[:64, :], lhsT=w[:, k8, 512:576], rhs=x_[:, k8, :],
                                                         start=(k8 == 0), stop=(k8 == 7)), R=[w, x_], W=[pp])
                kb.op("dve", lambda: nc.vector.tensor_copy(out=kiT[:, c * 512:(c + 1) * 512], in_=pp[:64, :]),
                      R=[pp], W=[kiT])
            for r in range(4):
                x_ = xc[r % 2]
                for k8 in range(8):
                    kb.idma(x_[:, k8, :], xTs_all[:, :], idxs[:, r * 8 + k8:r * 8 + k8 + 1], R=[xTs_all, idxs],
                            Wd=[x_] if k8 else (), W=[x_] if k8 == 0 else ())
                for hi in range(8):
                    pp = PS[n_ev % 3]
                    n_ev += 1
                    for k8 in range(8):
                        kb.op("pe", lambda: nc.tensor.matmul(pp[:64, :], lhsT=w[:, k8, hi * 64:(hi + 1) * 64],
                                                             rhs=x_[:, k8, :], start=(k8 == 0), stop=(k8 == 7)),
                              R=[w, x_], W=[pp])
                    if hi % 2 == 0:
                        kb.op("dve", lambda: nc.vector.tensor_copy(out=qiT[:, hi, r * 512:(r + 1) * 512], in_=pp[:64, :]),
                              R=[pp], W=[qiT])
                    else:
                        kb.op("act", lambda: nc.scalar.copy(out=qiT[:, hi, r * 512:(r + 1) * 512], in_=pp[:64, :]),
                              R=[pp], W=[qiT])
                for t4 in range(4):
                    pp = PS[n_ev % 3]
                    n_ev += 1
                    for k8 in range(8):
                        kb.op("pe", lambda: nc.tensor.matmul(pp[:, :8], lhsT=x_[:, k8, t4 * 128:(t4 + 1) * 128],
                                                             rhs=w[:, k8, 576:584], start=(k8 == 0), stop=(k8 == 7)),
                              R=[w, x_], W=[pp])
                    kb.op("dve", lambda: nc.vector.tensor_copy(out=wi[:, r * 4 + t4, :], in_=pp[:, :8]), R=[pp], W=[wi])
        kb.op("act", lambda: nc.scalar.activation(out=absw[:, :, :], in_=wi[:, :, :], func=AF.Abs), R=[wi], W=[absw])
        kb.op("act", lambda: nc.scalar.activation(out=sgn[:, :, :], in_=wi[:, :, :], func=AF.Sign), R=[wi], W=[sgn])
        def write_mask(k, mt, Lk):
            dst = _AP(tensor=M_loc.t.tensor, offset=MOFF[k], ap=[[Lk, 128], [1, Lk]])
            kb.dma("sp", dst, mt[:, :Lk], R=[mt], Wd=[M_loc])
            for (k2, hf, lrow, nrow, grow) in MPARTS:
                if k2 == k:
                    kb.collective("AllGather", M_loc, M_all, M_loc[lrow:lrow + nrow, :],
                                  M_all[grow:grow + 4 * nrow, :])

        dsa1_body(kb, qiT, kiT, absw, sgn, cm, idb, stp, PS, PT, write_mask, act_split)


def phase_dsa2(kb, L, xT_all, M_all, oT_loc, cst):
    nc = kb.nc
    with kb.scope():
        identb = kb.sb([128, 128], BF16, "identb")
        kb.dma("sp", identb[:, :], cst["identb"][:, :], R=[cst["identb"]], W=[identb])
        rf = kb.sb([128, 2], F32, "rf")
        kb.dma("sp", rf[:, :], L["relfar"][:, :], R=[L["relfar"]], W=[rf])
        ax = AttnCtx(kb, n_s=4, n_o=2, grouped=True)
        E_d = L["Escr"]
        with kb.scope():
            build_E_table(kb, ax, L["rel2"], cst["dsac"], E_d)
        TT = [kb.sb([128, 17 * 128], F32, "TT") for _ in range(2)]
        for h in range(2):
            toeplitz_load(kb, TT[h], E_d, h)
        q = kb.sb([128, SEQ], BF16, "q")
        k = kb.sb([128, SEQ], BF16, "k")
        V = kb.sb([128, NQB, 2, 65], BF16, "V")
        kb.op("pool", lambda: nc.gpsimd.memset(V[:, :, :, :], 1.0), W=[V])
        with kb.scope():
            NCOL = 384
            w = kb.sb([128, 8, NCOL], BF16, "wd2")
            load_w_cast(kb, w, L["wd2"], NCOL)
            xc = [kb.sb([128, 8, 512], BF16, "xc") for _ in range(2)]
            n_ev = 0
            for c in range(16):
                x_ = xc[c % 2]
                load_xT_chunk(kb, x_, xT_all, c)
                for bi, dst in enumerate((q, k)):
                    pp = ax.S[n_ev % 3]
                    for k8 in range(8):
                        kb.op("pe", lambda: nc.tensor.matmul(pp[:, :], lhsT=w[:, k8, bi * 128:(bi + 1) * 128],
                                                             rhs=x_[:, k8, :], start=(k8 == 0), stop=(k8 == 7)),
                              R=[w, x_], W=[pp])
                    d_ = dst[:, c * 512:(c + 1) * 512]
                    if n_ev % 2 == 0:
                        kb.op("dve", lambda: nc.vector.tensor_copy(out=d_, in_=pp[:, :]), R=[pp], W=[dst])
                    else:
                        kb.op("act", lambda: nc.scalar.copy(out=d_, in_=pp[:, :]), R=[pp], W=[dst])
                    n_ev += 1
                for t4 in range(4):
                    j = c * 4 + t4
                    pp = ax.S[n_ev % 3]
                    n_ev += 1
                    for k8 in range(8):
                        kb.op("pe", lambda: nc.tensor.matmul(pp[:, :128], lhsT=x_[:, k8, t4 * 128:(t4 + 1) * 128],
                                                             rhs=w[:, k8, 256:384], start=(k8 == 0), stop=(k8 == 7)),
                              R=[w, x_], W=[pp])
                    kb.op("dve" if t4 % 2 else "act",
                          (lambda: nc.vector.tensor_copy(out=V[:, j, :, 0:64],
                                                         in_=pp[:, 0:128].rearrange("p (h d) -> p h d", h=2)))
                          if t4 % 2 else
                          (lambda: nc.scalar.copy(out=V[:, j, :, 0:64],
                                                  in_=pp[:, 0:128].rearrange("p (h d) -> p h d", h=2))),
                          R=[pp], W=[V])
        Ms = [kb.sb([128, SEQ], BF16, "Ms") for _ in range(2)]
        ost = [kb.sb([128, 128], BF16, "ost") for _ in range(2)]
        out = OTOut(kb, identb, oT_loc, 128, "b")
        for i in range(NQB):
            ms = Ms[i % 2]
            W_ = (i + 1) * 128
            r_, k_ = i % 4, i // 4
            nh = 1 if k_ < 8 else 2
            for hf in range(nh):
                (_, _, lrow, nrow, grow) = MG[(k_, hf)]
                ns = 128 // nh
                src = _AP(tensor=M_all.t.tensor, offset=(grow + r_ * nrow) * 512, ap=[[LK[k_], ns], [1, W_]])
                kb.dma("sp", ms[hf * ns:(hf + 1) * ns, :W_], src, R=[M_all],
                       Wd=[ms] if hf else (), W=[ms] if hf == 0 else ())
            o = ost[i % 2]
            for h in range(2):
                groups = []
                jn0 = max(0, i - 16)
                for a in range(0, jn0, 4):
                    js = list(range(a, min(a + 4, jn0)))
                    groups.append({"js": js, "bias": (rf, rf[:, h:h + 1]),
                                   "dve_mask": (ms, ms[:, js[0] * 128:(js[-1] + 1) * 128])})
                for a in range(jn0, i + 1, 4):
                    js = list(range(a, min(a + 4, i + 1)))
                    groups.append({"js": js,
                                   "pool_masks": [(TT[h], TT[h][:, (i - j) * 128:(i - j + 1) * 128]) for j in js],
                                   "dve_mask": (ms, ms[:, js[0] * 128:(js[-1] + 1) * 128])})
                attn_qgroup(ax, q, k, V, h, i, groups, o, h * 64,
                            after=(None if h == 0 else (lambda i=i, o=o: out.put(i, o))))
        attn_flush(ax)


def phase_c(kb, L, moe, oT_all, x_src, x_dst, xT_loc, xTs_loc, cst, last):
    nc = kb.nc
    n_exp, nfu, n_units = (8, 14, 2) if moe else (1, 11, 2)
    TG = 512
    NG = TPC // TG
    with kb.scope():
        wf_d = L["wf"]
        ident = kb.sb([128, 128], F32, "ident")
        wo = kb.sb([128, 8, D], BF16, "wo")
        lnp = kb.sb([128, 4, D], F32, "lnp")
        kb.eps_col = kb.sb([128, 1], F32, "eps")
        kb.op("dve", lambda: nc.vector.memset(kb.eps_col[:, :], LN_EPS), W=[kb.eps_col])
        kb.dma("sp", ident[:, :], cst["ident"][:, :], R=[cst["ident"]], W=[ident])
        load_w_cast(kb, wo, L["wo"], D)
        kb.dma("sp", lnp[:, :, :], L["lnp"][:, :, :], R=[L["lnp"]], W=[lnp])
        idxo = kb.sb([128, 32], I32, "idxo")
        kb.dma("sp", idxo[:, :], cst["idx_o"][:, :], R=[cst["idx_o"]], W=[idxo])
        if moe:
            wr = kb.sb([128, 8, 8], F32, "wr")
            kb.dma("sp", wr[:, :, :], L["wr"][:, :, :], R=[L["wr"]], W=[wr])
            x1T32 = kb.sb([128, 8, 128], F32, "x1T32")
            comb = [kb.sb([128, 8], F32, "comb") for _ in range(4)]
            rt = kb.sb([128, 40], F32, "rt")
        oT = [kb.sb([128, 8, TG], BF16, "oT") for _ in range(1)]
        xt = [kb.sb([128, D], F32, "xt") for _ in range(2)]
        h = kb.sb([128, D], F32, "h")
        x1g = [kb.sb([128, D], F32, "x1g") for _ in range(4)]
        x1T = kb.sb([128, 8, TG], BF16, "x1T")
        aT = [kb.sb([128, TG], BF16, "aT") for _ in range(nfu)]
        w2 = [kb.sb([128, D], BF16, "w2") for _ in range(nfu)]
        w13 = [kb.sb([128, 2048], BF16, "w13") for _ in range(3)]
        yacc = [kb.sb([128, D], F32, "yacc") for _ in range(4)]
        sil = [kb.sb([128, TG], F32, "sil") for _ in range(2)]
        ost = [kb.sb([128, D], F32, "ost") for _ in range(2)]
        stg = kb.sb([128, 8, 512], BF16, "stgx")
        scr = (kb.sb([128, 2, 6], F32, "stats"), kb.sb([128, 2], F32, "mv"), kb.sb([128, 2], F32, "sd"))
        X = [kb.ps([128, 512], F32, "X") for _ in range(4)]
        Y = [kb.ps([128, 512], F32, "Y") for _ in range(2)]
        wcnt = 0
        for g in range(NG):
            og = oT[0]
            for k8 in range(8):
                kb.idma(og[:, k8, :], oT_all[:, :], idxo[:, g * 8 + k8:g * 8 + k8 + 1], R=[oT_all, idxo],
                        Wd=[og] if k8 else (), W=[og] if k8 == 0 else ())
            for tt in range(4):
                tok0 = g * TG + tt * 128
                xi = xt[tt % 2]
                kb.dma("sp", xi[:, :], x_src[tok0:tok0 + 128, :], R=[x_src], W=[xi])
                for hf in range(2):
                    for kc in range(8):
                        kb.op("pe", lambda: nc.tensor.matmul(Y[hf][:, :], lhsT=og[:, kc, tt * 128:(tt + 1) * 128],
                                                             rhs=wo[:, kc, hf * 512:(hf + 1) * 512],
                                                             start=(kc == 0), stop=(kc == 7)), R=[og, wo], W=[Y[hf]])
                    kb.op("dve", lambda: nc.vector.scalar_tensor_tensor(out=h[:, hf * 512:(hf + 1) * 512],
                                                                        in0=xi[:, hf * 512:(hf + 1) * 512], scalar=ALPHA,
                                                                        in1=Y[hf][:, :], op0=ALU.mult, op1=ALU.add),
                          R=[xi, Y[hf]], W=[h])
                x1 = x1g[tt]
                layer_norm(kb, h, (lnp, lnp[:, 0, :]), (lnp, lnp[:, 1, :]), x1[:, :], x1, scr)
                for kc in range(8):
                    pt = X[kc // 4]
                    kb.op("pe", lambda: nc.tensor.transpose(pt[:, (kc % 4) * 128:(kc % 4 + 1) * 128],
                                                            x1[:, kc * 128:(kc + 1) * 128], ident[:, :]),
                          R=[x1, ident], W=[pt])
                for hf in range(2):
                    src = X[hf][:, :].rearrange("p (k t) -> p k t", k=4)
                    dst = x1T[:, hf * 4:(hf + 1) * 4, tt * 128:(tt + 1) * 128]
                    if hf == 0:
                        kb.op("dve", lambda: nc.vector.tensor_copy(out=dst, in_=src), R=[X[hf]], W=[x1T])
                    else:
                        kb.op("act", lambda: nc.scalar.copy(out=dst, in_=src), R=[X[hf]], W=[x1T])
                    if moe:
                        kb.op("dve", lambda: nc.vector.tensor_copy(out=x1T32[:, hf * 4:(hf + 1) * 4, :], in_=src),
                              R=[X[hf]], W=[x1T32])
                if moe:
                    pr = X[2]
                    for kc in range(8):
                        kb.op("pe", lambda: nc.tensor.matmul(pr[:, :8], lhsT=x1T32[:, kc, :], rhs=wr[:, kc, :],
                                                             start=(kc == 0), stop=(kc == 7)), R=[x1T32, wr], W=[pr])
                    cb = comb[tt]
                    lg, mx, tmp, oh = rt[:, 0:8], rt[:, 8:16], rt[:, 16:24], rt[:, 24:32]
                    sc = rt[:, 32:40]
                    kb.op("dve", lambda: nc.vector.tensor_copy(out=lg, in_=pr[:, :8]), R=[pr], W=[rt])
                    kb.op("dve", lambda: nc.vector.max(out=mx, in_=lg), R=[rt], W=[rt])
                    kb.op("dve", lambda: nc.vector.tensor_tensor(out=sc[:, 0:1], in0=mx[:, 1:2], in1=mx[:, 0:1],
                                                                 op=ALU.subtract), R=[rt], W=[rt])
                    kb.op("act", lambda: nc.scalar.activation(out=sc[:, 1:2], in_=sc[:, 0:1], func=AF.Exp),
                          R=[rt], W=[rt])
                    kb.op("dve", lambda: nc.vector.tensor_scalar(out=sc[:, 2:3], in0=sc[:, 1:2], scalar1=1.0,
                                                                 scalar2=None, op0=ALU.add), R=[rt], W=[rt])
                    kb.op("dve", lambda: nc.vector.reciprocal(out=sc[:, 3:4], in_=sc[:, 2:3]), R=[rt], W=[rt])
                    kb.op("dve", lambda: nc.vector.tensor_tensor(out=sc[:, 4:5], in0=sc[:, 1:2], in1=sc[:, 3:4],
                                                                 op=ALU.mult), R=[rt], W=[rt])
                    kb.op("dve", lambda: nc.vector.tensor_scalar(out=tmp, in0=lg, scalar1=mx[:, 0:1],
                                                                 scalar2=sc[:, 3:4], op0=ALU.is_equal, op1=ALU.mult),
                          R=[rt], W=[rt])
                    kb.op("dve", lambda: nc.vector.tensor_scalar(out=oh, in0=lg, scalar1=mx[:, 1:2],
                                                                 scalar2=sc[:, 4:5], op0=ALU.is_equal, op1=ALU.mult),
                          R=[rt], W=[rt])
                    kb.op("dve", lambda: nc.vector.tensor_tensor(out=cb[:, :], in0=tmp, in1=oh, op=ALU.add),
                          R=[rt], W=[cb])
            first = True
            for e in range(n_exp):
                for u in range(n_units):
                    base = (e * n_units + u) * nfu
                    for f in range(nfu):
                        wc = w13[wcnt % 3]
                        kb.dma("pool", wc[:, :], wf_d[base + f, :, 0:2048], R=[wf_d], W=[wc])
                        kb.dma("pool", w2[f][:, :], wf_d[base + f, :, 2048:3072], R=[wf_d], W=[w2[f]])
                        h1, h3 = X[(wcnt % 2) * 2], X[(wcnt % 2) * 2 + 1]
                        for kc in range(8):
                            kb.op("pe", lambda: nc.tensor.matmul(h1[:, :], lhsT=wc[:, kc * 128:(kc + 1) * 128],
                                                                 rhs=x1T[:, kc, :], start=(kc == 0), stop=(kc == 7)),
                                  R=[wc, x1T], W=[h1])
                        for kc in range(8):
                            kb.op("pe", lambda: nc.tensor.matmul(h3[:, :],
                                                                 lhsT=wc[:, 1024 + kc * 128:1024 + (kc + 1) * 128],
                                                                 rhs=x1T[:, kc, :], start=(kc == 0), stop=(kc == 7)),
                                  R=[wc, x1T], W=[h3])
                        s = sil[wcnt % 2]
                        kb.op("act", lambda: nc.scalar.activation(out=s[:, :], in_=h1[:, :], func=AF.Silu),
                              R=[h1], W=[s])
                        kb.op("dve", lambda: nc.vector.tensor_tensor(out=aT[f][:, :], in0=s[:, :], in1=h3[:, :],
                                                                     op=ALU.mult), R=[s, h3], W=[aT[f]])
                        wcnt += 1
                    for tt in range(4):
                        for hf in range(2):
                            py = Y[hf]
                            for f in range(nfu):
                                kb.op("pe", lambda: nc.tensor.matmul(py[:, :], lhsT=aT[f][:, tt * 128:(tt + 1) * 128],
                                                                     rhs=w2[f][:, hf * 512:(hf + 1) * 512],
                                                                     start=(f == 0), stop=(f == nfu - 1)),
                                      R=[aT[f], w2[f]], W=[py])
                            ya = yacc[tt]
                            ysl = ya[:, hf * 512:(hf + 1) * 512]
                            if moe:
                                cs = comb[tt][:, e:e + 1]
                                if first:
                                    kb.op("dve", lambda: nc.vector.tensor_scalar(out=ysl, in0=py[:, :], scalar1=cs,
                                                                                 scalar2=None, op0=ALU.mult),
                                          R=[py, comb[tt]], W=[ya])
                                else:
                                    kb.op("dve", lambda: nc.vector.scalar_tensor_tensor(out=ysl, in0=py[:, :], scalar=cs,
                                                                                        in1=ysl, op0=ALU.mult,
                                                                                        op1=ALU.add),
                                          R=[py, comb[tt], ya], W=[ya])
                            else:
                                if first:
                                    kb.op("dve", lambda: nc.vector.tensor_copy(out=ysl, in_=py[:, :]), R=[py], W=[ya])
                                else:
                                    kb.op("dve", lambda: nc.vector.tensor_tensor(out=ysl, in0=py[:, :], in1=ysl,
                                                                                 op=ALU.add), R=[py, ya], W=[ya])
                    first = False
            for tt in range(4):
                tok0 = g * TG + tt * 128
                kb.op("dve", lambda: nc.vector.scalar_tensor_tensor(out=h[:, :], in0=x1g[tt][:, :], scalar=ALPHA,
                                                                    in1=yacc[tt][:, :], op0=ALU.mult, op1=ALU.add),
                      R=[x1g[tt], yacc[tt]], W=[h])
                o = ost[tt % 2]
                layer_norm(kb, h, (lnp, lnp[:, 2, :]), (lnp, lnp[:, 3, :]), o[:, :], o, scr)
                kb.dma("sp", x_dst[tok0:tok0 + 128, :], o[:, :], R=[o], Wd=[x_dst])
                if not last:
                    emit_xT(kb, X, ident, o, g * 4 + tt, stg, xT_loc, xTs_loc)


def _dbg_out(kb, x_d, out_d):
    for tt in range(4):
        kb.dma("sp", out_d[tt * 512:(tt + 1) * 512, :], x_d[tt * 512:(tt + 1) * 512, :], R=[x_d], Wd=[out_d])
    return kb.finish()


PROFILE_SCOPES = False


class _NullScope:
    def __enter__(self):
        return self

    def __exit__(self, *a):
        return False


def _scope(nc, name):
    return nc.named_scope(name) if PROFILE_SCOPES else _NullScope()


def build_fused(layers=(0, 1, 2, 3), upto=9):
    kb = KBF()
    nc = kb.nc
    x_d = kb.dram_in("x", [TPC, D], F32)
    out_d = kb.dram_out("out", [TPC, D], F32)
    cst = {"tri": kb.dram_in("tri", [128, 128], F32), "identb": kb.dram_in("identb", [128, 128], BF16),
           "ident": kb.dram_in("ident", [128, 128], F32), "dilc": kb.dram_in("dilc", [32, NDEL], F32),
           "dsac": kb.dram_in("dsac", [32, NDEL], F32), "cmask": kb.dram_in("cmask", [128, 512], F32),
           "steps": kb.dram_in("steps", [128, NBIS], F32), "idx_s": kb.dram_in("idx_s", [128, 32], I32),
           "idx_o": kb.dram_in("idx_o", [128, 32], I32)}
    Ls = {}
    for l in layers:
        L = {}
        pre = "L%d_" % l
        moe = (l % 2 == 1)
        if l % 2 == 0:
            L["wgl"] = kb.dram_in(pre + "wgl", [128, 8, 464], F32)
            L["wg"] = kb.dram_in(pre + "wg", [16, 64], F32)
            L["bg"] = kb.dram_in(pre + "bg", [128, 64], F32)
            L["gn"] = kb.dram_in(pre + "gn", [128, 128], F32)
            L["wd1"] = kb.dram_in(pre + "wd1", [128, 8, 584], F32)
            L["wd2"] = kb.dram_in(pre + "wd2", [128, 8, 384], F32)
            L["relfar"] = kb.dram_in(pre + "relfar", [128, 2], F32)
        else:
            L["wcd"] = kb.dram_in(pre + "wcd", [128, 8, 770], F32)
            L["bf"] = kb.dram_in(pre + "bf", [128, 2], F32)
            L["wr"] = kb.dram_in(pre + "wr", [128, 8, 8], F32)
        L["rel2"] = kb.dram_in(pre + "rel2", [32, 2], F32)
        L["wo"] = kb.dram_in(pre + "wo", [128, 8, D], F32)
        L["lnp"] = kb.dram_in(pre + "lnp", [128, 4, D], F32)
        L["wf"] = kb.dram_in(pre + "wf", [(224 if moe else 22) if upto >= 9 else 1, 128, 3072], F32)
        L["Escr"] = kb.dram_tmp(pre + "Escr", [2, NDEL], F32)
        Ls[l] = L
    xres = [kb.dram_tmp("xres0", [TPC, D], F32), kb.dram_tmp("xres1", [TPC, D], F32)]
    xT_loc = kb.dram_tmp("xT_loc", [D, TPC], BF16)
    xT_all = kb.dram_tmp("xT_all", [4 * D, TPC], BF16)
    xTs_loc = kb.dram_tmp("xTs_loc", [4 * D, 512], BF16)
    xTs_all = kb.dram_tmp("xTs_all", [16 * D, 512], BF16)
    oT_loc = kb.dram_tmp("oT_loc", [16 * 256, 512], BF16)
    oT_all = kb.dram_tmp("oT_all", [64 * 256, 512], BF16)
    M_loc = kb.dram_tmp("M_loc", [MTOT // 512, 512], BF16)
    M_all = kb.dram_tmp("M_all", [4 * MTOT // 512, 512], BF16)
    assert MPARTS[-1][4] + 4 * MPARTS[-1][3] == 4 * MTOT // 512
    with kb.scope():
        ident = kb.sb([128, 128], F32, "ident")
        kb.dma("sp", ident[:, :], cst["ident"][:, :], R=[cst["ident"]], W=[ident])
        xin = [kb.sb([128, D], F32, "xin") for _ in range(2)]
        stg = kb.sb([128, 8, 512], BF16, "stgx")
        X = [kb.ps([128, 512], F32, "X") for _ in range(2)]
        for tt in range(TPC // 128):
            xi = xin[tt % 2]
            kb.dma("sp", xi[:, :], x_d[tt * 128:(tt + 1) * 128, :], R=[x_d], W=[xi])
            emit_xT(kb, X, ident, xi, tt, stg, xT_loc, xTs_loc)
    if upto == 0:
        return _dbg_out(kb, x_d, out_d)
    x_src = x_d
    for n, l in enumerate(layers):
        L = Ls[l]
        last = (n == len(layers) - 1)
        x_dst = out_d if last else xres[n % 2]
        for cf in range(4):
            kb.collective("AllGather", xT_loc, xT_all, xT_loc[cf * 256:(cf + 1) * 256, :],
                          xT_all[cf * 1024:(cf + 1) * 1024, :])
        if upto == 1:
            return _dbg_out(kb, x_d, out_d)
        if l % 2 == 0:
            for j in range(4):
                kb.collective("AllGather", xTs_loc, xTs_all, xTs_loc[j * 1024:(j + 1) * 1024, :],
                              xTs_all[j * 4096:(j + 1) * 4096, :])
            with _scope(nc, "L%d_gla" % l):
                phase_gla(kb, L, xT_all, oT_loc, cst)
            with _scope(nc, "L%d_dsa1" % l):
                phase_dsa1(kb, L, xT_all, xTs_all, M_loc, M_all, cst)
            with _scope(nc, "L%d_dsa2" % l):
                phase_dsa2(kb, L, xT_all, M_all, oT_loc, cst)
        else:
            with _scope(nc, "L%d_cd" % l):
                phase_cd(kb, L, xT_all, oT_loc, cst)
        if upto == 2:
            return _dbg_out(kb, x_d, out_d)
        for j in range(4):
            kb.collective("AllGather", oT_loc, oT_all, oT_loc[j * 1024:(j + 1) * 1024, :],
                          oT_all[j * 4096:(j + 1) * 4096, :])
        if upto == 3:
            return _dbg_out(kb, x_d, out_d)
        with _scope(nc, "L%d_c" % l):
            phase_c(kb, L, l % 2 == 1, oT_all, x_src, x_dst, xT_loc, xTs_loc, cst, last)
        x_src = x_dst
    return kb.finish()


def fused_inputs(x, ln_g, ln_b, rel_table, w_in_ab, w_gate_a, b_gate_a, g_norm_a, w_out_ab,
                 w_in_cd, b_forget, w_out_cd, w1_dense, w3_dense, w2_dense,
                 w_router, w1_moe, w3_moe, w2_moe, layers=(0, 1, 2, 3)):
    f32 = lambda a: np.ascontiguousarray(np.asarray(a, dtype=np.float32))
    bc = lambda v, n=128: np.ascontiguousarray(np.broadcast_to(np.asarray(v, np.float32)[None, :], (n, len(v))))
    xf = f32(x).reshape(BATCH * SEQ, D)
    rel_table = f32(rel_table)
    steps = bc(0.5 ** np.arange(1, NBIS + 1))
    perm = np.zeros(D, np.int64)
    for src in range(4):
        for rr in range(256):
            perm[src * 256 + rr] = src * 128 + rr if rr < 128 else 512 + src * 128 + (rr - 128)
    shared = {"tri": TRI, "identb": IDENT.astype(NPBF), "ident": IDENT, "dilc": dil_const(), "dsac": dsa_const(),
              "steps": steps}
    per_layer_shared = {}
    for l in layers:
        j = l // 2
        pre = "L%d_" % l
        d = {}
        d[pre + "lnp"] = np.ascontiguousarray(np.broadcast_to(
            np.stack([ln_g[l, 0], ln_b[l, 0], ln_g[l, 1], ln_b[l, 1]]).astype(np.float32)[None], (128, 4, D)))
        if l % 2 == 0:
            d[pre + "wo"] = w_kc_layout(f32(w_out_ab[j])[perm])
            d[pre + "wf"] = ffn_chunk_layout(f32(w1_dense[j]), f32(w3_dense[j]), f32(w2_dense[j]))
            d[pre + "gn"] = bc(g_norm_a[j])
        else:
            d[pre + "wo"] = w_kc_layout(f32(w_out_cd[j])[perm])
            d[pre + "wf"] = np.concatenate([ffn_chunk_layout(f32(w1_moe[j, e]), f32(w3_moe[j, e]), f32(w2_moe[j, e]))
                                            for e in range(8)], axis=0)
            d[pre + "wr"] = np.ascontiguousarray(f32(w_router[j]).reshape(8, 128, 8).transpose(1, 0, 2))
        per_layer_shared.update(d)
    maps = []
    for c in range(NCORES):
        b, m = c // 4, c % 4
        mp = dict(shared)
        mp.update(per_layer_shared)
        mp["x"] = np.ascontiguousarray(xf[c * TPC:(c + 1) * TPC])
        pp_ = np.arange(128)[:, None]
        rr_, k8_ = np.arange(4)[None, :, None], np.arange(8)[None, None, :]
        mp["idx_s"] = np.ascontiguousarray(((m * 4 + rr_) * 1024 + k8_ * 128 + pp_[:, :, None]).reshape(128, 32)
                                           .astype(np.int32))
        G_ = k8_ * 128 + pp_[:, :, None]
        mp["idx_o"] = np.ascontiguousarray(((m * 4 + G_ // 256) * 1024 + rr_ * 256 + G_ % 256).reshape(128, 32)
                                           .astype(np.int32))
        mp["cmask"] = np.where(np.arange(512)[None, :] <= (128 * m + np.arange(128))[:, None], 0.0, NEG).astype(np.float32)
        for l in layers:
            j = l // 2
            pre = "L%d_" % l
            mp[pre + "rel2"] = np.ascontiguousarray(rel_table[:, 2 * m:2 * m + 2])
            if l % 2 == 0:
                w = f32(w_in_ab[j])
                cs = lambda a, n: w[:, a:a + n]
                wgl = np.concatenate([cs(m * 64, 64), cs(256 + m * 64, 64), cs(1536, 16), cs(256 + m * 64, 64),
                                      cs(512 + m * 128, 128), cs(1024 + m * 128, 128)], axis=1)
                wd1 = np.concatenate([cs(3088, 512), cs(3600, 64), cs(3664, 8)], axis=1)
                wd2 = np.concatenate([cs(1552 + m * 128, 128), cs(2064 + m * 128, 128), cs(2576 + m * 128, 128)], axis=1)
                mp[pre + "wgl"] = w_kc_layout(wgl)
                mp[pre + "wd1"] = w_kc_layout(wd1)
                mp[pre + "wd2"] = w_kc_layout(wd2)
                mp[pre + "wg"] = np.ascontiguousarray(f32(w_gate_a[j])[:, m * 64:(m + 1) * 64])
                mp[pre + "bg"] = bc(f32(b_gate_a[j])[m * 64:(m + 1) * 64])
                mp[pre + "relfar"] = bc(rel_table[31, 2 * m:2 * m + 2])
            else:
                w = f32(w_in_cd[j])
                cs = lambda a, n: w[:, a:a + n]
                wcd = np.concatenate([cs(m * 128, 128), cs(512 + m * 128, 128), cs(1544 + m * 128, 128),
                                      cs(2056 + m * 128, 128), cs(1024 + m * 128, 128), cs(2568 + m * 128, 128),
                                      cs(1536 + 2 * m, 2)], axis=1)
                mp[pre + "wcd"] = w_kc_layout(wcd)
                mp[pre + "bf"] = bc(f32(b_forget[j])[2 * m:2 * m + 2])
        maps.append(mp)
    return maps


def kernel_fused(**inputs):
    nc = build_fused()
    maps = fused_inputs(**inputs)
    res = run_spmd(nc, maps)
    out = np.concatenate([res[c]["out"] for c in range(NCORES)], axis=0)
    return out.reshape(BATCH, SEQ, D).astype(np.float32)


def kernel(**inputs):
    return kernel_fused(**inputs)
```

```python
import math
from contextlib import ExitStack
import numpy as np
import ml_dtypes
import concourse.bass as bass
import concourse.mybir as mybir
from concourse.bass_utils import run_bass_kernel_spmd

F32 = mybir.dt.float32
BF16 = mybir.dt.bfloat16
AF = mybir.ActivationFunctionType
ALU = mybir.AluOpType
AX = mybir.AxisListType
NPBF = ml_dtypes.bfloat16

NCORES = 8
D = 1024
SEQ = 8192
BATCH = 2
DEPTH = 4
ALPHA = (2 * DEPTH) ** 0.25
LN_EPS = 1e-5
NEG = -1.0e30


class Buf:
    def __init__(self, t):
        self.t = t
        self.w = None
        self.r = []

    def __getitem__(self, idx):
        return self.t[idx]


class KB:
    NDS = 6

    def __init__(self):
        self.nc = bass.Bass("TRN2", target_bir_lowering=False)
        nc = self.nc
        self.es = ExitStack()
        self.eng = {"pe": nc.tensor, "dve": nc.vector, "act": nc.scalar, "pool": nc.gpsimd, "sp": nc.sync}
        self.esem = {}
        self.ecnt = {}
        self.seen = {e: {} for e in self.eng}
        for e in self.eng:
            self.esem[e] = self.es.enter_context(nc.semaphore("sem_" + e))
            self.ecnt[e] = 0
        self.dsem = {}
        self.dcnt = {}
        self.dnext = {}
        for q in ("sp", "act", "pool"):
            self.dsem[q] = [self.es.enter_context(nc.semaphore("dsem_%s%d" % (q, i))) for i in range(self.NDS)]
            self.dcnt[q] = [0] * self.NDS
            self.dnext[q] = 0
        self.n_names = 0
        self.outs = []

    def _nm(self, p):
        self.n_names += 1
        return "%s_%d" % (p, self.n_names)

    def dram_in(self, name, shape, dt):
        return Buf(self.nc.dram_tensor(name, list(shape), dt, kind="ExternalInput").ap())

    def dram_out(self, name, shape, dt):
        b = Buf(self.nc.dram_tensor(name, list(shape), dt, kind="ExternalOutput").ap())
        self.outs.append(b)
        return b

    def dram_tmp(self, name, shape, dt):
        return Buf(self.nc.dram_tensor(name, list(shape), dt, kind="Internal").ap())

    def sb(self, shape, dt, name="sb"):
        return Buf(self.es.enter_context(self.nc.sbuf_tensor(self._nm(name), list(shape), dt)))

    def ps(self, shape, dt=F32, name="ps"):
        return Buf(self.es.enter_context(self.nc.psum_tensor(self._nm(name), list(shape), dt)))

    def _wait(self, e, ev):
        if ev is None:
            return
        sem, val, key = ev
        if self.seen[e].get(key, 0) >= val:
            return
        self.eng[e].wait_ge(sem, val)
        self.seen[e][key] = val

    def _deps(self, e, R, W):
        evs = []
        for b in R:
            if b.w is not None:
                evs.append(b.w)
        for b in W:
            if b.w is not None:
                evs.append(b.w)
            evs.extend(b.r)
        best = {}
        for ev in evs:
            k = ev[2]
            if e == "pe" and k == "pe":
                continue
            if k not in best or best[k][1] < ev[1]:
                best[k] = ev
        for ev in best.values():
            self._wait(e, ev)

    def _mark(self, ev, R, W):
        for b in R:
            b.r.append(ev)
            if len(b.r) > 24:
                best = {}
                for x in b.r:
                    if x[2] not in best or best[x[2]][1] < x[1]:
                        best[x[2]] = x
                b.r = list(best.values())
        for b in W:
            b.w = ev
            b.r = []

    def op(self, e, fn, R=(), W=()):
        self._deps(e, R, W)
        ins = fn()
        self.ecnt[e] += 1
        ins.then_inc(self.esem[e], 1)
        ev = (self.esem[e], self.ecnt[e], e)
        self.seen[e][e] = max(self.seen[e].get(e, 0), 0)
        self._mark(ev, R, W)
        return ev

    def dma(self, q, out, in_, R=(), W=(), **kw):
        self._deps(q, R, W)
        i = self.dnext[q]
        self.dnext[q] = (i + 1) % self.NDS
        key = "d%s%d" % (q, i)
        if self.dcnt[q][i] > 0:
            self._wait(q, (self.dsem[q][i], self.dcnt[q][i], key))
        self.dcnt[q][i] += 16
        self.eng[q].dma_start(out=out, in_=in_, **kw).then_inc(self.dsem[q][i], 16)
        ev = (self.dsem[q][i], self.dcnt[q][i], key)
        self._mark(ev, R, W)
        return ev

    def finish(self):
        for q in ("sp", "act", "pool"):
            for i in range(self.NDS):
                if self.dcnt[q][i] > 0:
                    self._wait("sp", (self.dsem[q][i], self.dcnt[q][i], "d%s%d" % (q, i)))
        for e in ("pe", "dve", "act", "pool"):
            if self.ecnt[e] > 0:
                self._wait("sp", (self.esem[e], self.ecnt[e], e))
        self.es.close()
        return self.nc


def run_spmd(nc, in_maps):
    res = run_bass_kernel_spmd(nc, in_maps, core_ids=list(range(NCORES)))
    return res.results


def build_cast(n):
    kb = KB()
    nc = kb.nc
    CH = 2048
    src = kb.dram_in("src", [128, n], F32)
    dst = kb.dram_out("dst", [128, n], BF16)
    NB = 3
    tin = [kb.sb([128, CH], F32, "tin") for _ in range(NB)]
    tout = [kb.sb([128, CH], BF16, "tout") for _ in range(NB)]
    nch = (n + CH - 1) // CH
    for c in range(nch):
        c0 = c * CH
        w = min(CH, n - c0)
        a, b = tin[c % NB], tout[c % NB]
        kb.dma("sp", a[:, :w], src[:, c0:c0 + w], R=[src], W=[a])
        if c % 2 == 0:
            kb.op("dve", lambda: nc.vector.tensor_copy(out=b[:, :w], in_=a[:, :w]), R=[a], W=[b])
        else:
            kb.op("act", lambda: nc.scalar.copy(out=b[:, :w], in_=a[:, :w]), R=[a], W=[b])
        kb.dma("pool", dst[:, c0:c0 + w], b[:, :w], R=[b], W=[dst])
    return kb.finish()


def cast_weights(arrs):
    flats = [np.ascontiguousarray(a).reshape(NCORES, 128, -1) for a in arrs]
    ns = [f.shape[2] for f in flats]
    cat = np.concatenate(flats, axis=2)
    n = cat.shape[2]
    nc = build_cast(n)
    res = run_spmd(nc, [{"src": np.ascontiguousarray(cat[c])} for c in range(NCORES)])
    out = np.stack([res[c]["dst"] for c in range(NCORES)], axis=0)
    outs = []
    o = 0
    for a, k in zip(arrs, ns):
        outs.append(out[:, :, o:o + k].reshape(a.shape))
        o += k
    return outs


TPC = 2048


def build_A(W, fm, tmb, tmf, gate=None):
    kb = KB()
    nc = kb.nc
    NT = TPC // 128
    n_fm = sum(n for _, n in fm)
    n_tmb = sum(n for _, n in tmb)
    n_tmf = sum(n for _, n in tmf) + (256 if gate is not None else 0)
    x = kb.dram_in("x", [TPC, D], F32)
    w = kb.dram_in("w", [128, 8, W], BF16)
    ident_d = kb.dram_in("ident", [128, 128], F32)
    yT = kb.dram_out("yT", [n_fm, TPC], BF16)
    ytb = kb.dram_out("ytb", [TPC, max(n_tmb, 1)], BF16)
    ytf = kb.dram_out("ytf", [TPC, max(n_tmf, 1)], F32)
    wsb = kb.sb([128, 8, W], BF16, "w")
    ident = kb.sb([128, 128], F32, "ident")
    xT = kb.sb([128, 8, TPC], BF16, "xT")
    kb.dma("sp", ident[:, :], ident_d[:, :], R=[ident_d], W=[ident])
    for kc in range(8):
        kb.dma("pool" if kc % 2 else "sp", wsb[:, kc, :], w[:, kc, :], R=[w], W=[wsb])
    if gate is not None:
        wg_d = kb.dram_in("wg", [16, 256], F32)
        bg_d = kb.dram_in("bg", [128, 256], F32)
        wg = kb.sb([16, 256], F32, "wg")
        bg = kb.sb([128, 256], F32, "bg")
        gaT = kb.sb([16, TPC], F32, "gaT")
        w32 = kb.sb([128, 8, 16], F32, "w32")
        kb.dma("sp", wg[:, :], wg_d[:, :], R=[wg_d], W=[wg])
        kb.dma("sp", bg[:, :], bg_d[:, :], R=[bg_d], W=[bg])
    xin = [kb.sb([128, D], F32, "xin") for _ in range(2)]
    pst = [kb.ps([128, 1024], F32, "pst")]
    for tt in range(NT):
        xi = xin[tt % 2]
        kb.dma("sp", xi[:, :], x[tt * 128:(tt + 1) * 128, :], R=[x], W=[xi])
        pt = pst[0]
        for kc in range(8):
            kb.op("pe", lambda: nc.tensor.transpose(pt[:, kc * 128:(kc + 1) * 128], xi[:, kc * 128:(kc + 1) * 128],
                                                    ident[:, :]), R=[xi, ident], W=[pt])
        for hf in range(2):
            src = pt[:, hf * 512:(hf + 1) * 512].rearrange("p (k t) -> p k t", k=4)
            dst = xT[:, hf * 4:(hf + 1) * 4, tt * 128:(tt + 1) * 128]
            if hf == 0:
                kb.op("dve", lambda: nc.vector.tensor_copy(out=dst, in_=src), R=[pt], W=[xT])
            else:
                kb.op("act", lambda: nc.scalar.copy(out=dst, in_=src), R=[pt], W=[xT])
    psf = [kb.ps([128, 512], F32, "psf") for _ in range(2)]
    stf = [kb.sb([128, 512], BF16, "stf") for _ in range(3)]
    cnt = 0
    row = 0
    blocks = list(fm)
    for (c0, ncol) in blocks:
        for tg in range(TPC // 512):
            pp = psf[cnt % 2]
            st = stf[cnt % 3]
            for kc in range(8):
                kb.op("pe", lambda: nc.tensor.matmul(pp[:ncol, :], lhsT=wsb[:, kc, c0:c0 + ncol],
                                                     rhs=xT[:, kc, tg * 512:(tg + 1) * 512],
                                                     start=(kc == 0), stop=(kc == 7)), R=[wsb, xT], W=[pp])
            if cnt % 2 == 0:
                kb.op("dve", lambda: nc.vector.tensor_copy(out=st[:ncol, :], in_=pp[:ncol, :]), R=[pp], W=[st])
            else:
                kb.op("act", lambda: nc.scalar.copy(out=st[:ncol, :], in_=pp[:ncol, :]), R=[pp], W=[st])
            kb.dma("pool" if cnt % 2 else "sp", yT[row:row + ncol, tg * 512:(tg + 1) * 512], st[:ncol, :],
                   R=[st], W=[yT])
            cnt += 1
        row += ncol
    if gate is not None:
        g0 = gate[0]
        for tg in range(TPC // 512):
            pp = psf[cnt % 2]
            for kc in range(8):
                kb.op("pe", lambda: nc.tensor.matmul(pp[:16, :], lhsT=wsb[:, kc, g0:g0 + 16],
                                                     rhs=xT[:, kc, tg * 512:(tg + 1) * 512],
                                                     start=(kc == 0), stop=(kc == 7)), R=[wsb, xT], W=[pp])
            kb.op("dve", lambda: nc.vector.tensor_copy(out=gaT[:, tg * 512:(tg + 1) * 512], in_=pp[:16, :]),
                  R=[pp], W=[gaT])
            cnt += 1
    pstm = [kb.ps([128, 512], F32, "pstm") for _ in range(2)]
    stb = [kb.sb([128, 512], BF16, "stb") for _ in range(3)]
    st32 = [kb.sb([128, 512], F32, "st32") for _ in range(3)]
    cnt = 0
    for tt in range(NT):
        for kind, lst, dst_d in (("b", tmb, ytb), ("f", tmf, ytf)):
            off = 0
            for (c0, ncol) in lst:
                pp = pstm[cnt % 2]
                st = (stb if kind == "b" else st32)[cnt % 3]
                for kc in range(8):
                    kb.op("pe", lambda: nc.tensor.matmul(pp[:, :ncol], lhsT=xT[:, kc, tt * 128:(tt + 1) * 128],
                                                         rhs=wsb[:, kc, c0:c0 + ncol],
                                                         start=(kc == 0), stop=(kc == 7)), R=[wsb, xT], W=[pp])
                if cnt % 2 == 0:
                    kb.op("dve", lambda: nc.vector.tensor_copy(out=st[:, :ncol], in_=pp[:, :ncol]), R=[pp], W=[st])
                else:
                    kb.op("act", lambda: nc.scalar.copy(out=st[:, :ncol], in_=pp[:, :ncol]), R=[pp], W=[st])
                kb.dma("pool" if cnt % 2 else "sp", dst_d[tt * 128:(tt + 1) * 128, off:off + ncol], st[:, :ncol],
                       R=[st], W=[dst_d])
                off += ncol
                cnt += 1
        if gate is not None:
            off = sum(n for _, n in tmf)
            pp = pstm[cnt % 2]
            st = st32[cnt % 3]
            kb.op("pe", lambda: nc.tensor.matmul(pp[:, :256], lhsT=gaT[:, tt * 128:(tt + 1) * 128], rhs=wg[:, :],
                                                 start=True, stop=True), R=[gaT, wg], W=[pp])
            kb.op("dve", lambda: nc.vector.tensor_tensor(out=st[:, :256], in0=pp[:, :256], in1=bg[:, :], op=ALU.add),
                  R=[pp, bg], W=[st])
            kb.op("act", lambda: nc.scalar.activation(out=st[:, :256], in_=st[:, :256], func=AF.Exp, scale=-1.0),
                  R=[st], W=[st])
            kb.op("act", lambda: nc.scalar.activation(out=st[:, :256], in_=st[:, :256], func=AF.Ln, bias=1.0),
                  R=[st], W=[st])
            kb.op("dve", lambda: nc.vector.tensor_scalar(out=st[:, :256], in0=st[:, :256], scalar1=-1.0 / 16.0,
                                                         scalar2=None, op0=ALU.mult), R=[st], W=[st])
            kb.dma("sp", ytf[tt * 128:(tt + 1) * 128, off:off + 256], st[:, :256], R=[st], W=[ytf])
            cnt += 1
    return kb.finish()


def blocks_of(c0, n, bs):
    out = []
    while n > 0:
        k = min(bs, n)
        out.append((c0, k))
        c0 += k
        n -= k
    return out


def w_kc_layout(w):
    W = w.shape[1]
    return np.ascontiguousarray(w.reshape(8, 128, W).transpose(1, 0, 2))


IDENT = np.eye(128, dtype=np.float32)


def run_A(x_flat, w_bf, fm_ranges, tmb_ranges, tmf_ranges, gate=None, wg=None, bg=None):
    W = w_bf.shape[1]
    fm = [b for (c0, n) in fm_ranges for b in blocks_of(c0, n, 128)]
    tmb = [b for (c0, n) in tmb_ranges for b in blocks_of(c0, n, 512)]
    tmf = [b for (c0, n) in tmf_ranges for b in blocks_of(c0, n, 512)]
    nc = build_A(W, fm, tmb, tmf, gate)
    wl = w_kc_layout(w_bf)
    maps = []
    for c in range(NCORES):
        m = {"x": np.ascontiguousarray(x_flat[c * TPC:(c + 1) * TPC]), "w": wl, "ident": IDENT}
        if gate is not None:
            m["wg"] = np.ascontiguousarray(wg)
            m["bg"] = np.ascontiguousarray(np.broadcast_to(bg[None, :], (128, 256)))
        maps.append(m)
    res = run_spmd(nc, maps)
    yT = np.concatenate([res[c]["yT"] for c in range(NCORES)], axis=1)
    ytb = np.concatenate([res[c]["ytb"] for c in range(NCORES)], axis=0)
    ytf = np.concatenate([res[c]["ytf"] for c in range(NCORES)], axis=0)
    return yT, ytb, ytf


def layer_norm(kb, h, gt, bt, out_ap, out_buf, scr):
    nc = kb.nc
    stats, mv, sd = scr
    for c in range(2):
        kb.op("dve", lambda: nc.vector.bn_stats(out=stats[:, c, :], in_=h[:, c * 512:(c + 1) * 512]), R=[h], W=[stats])
    kb.op("dve", lambda: nc.vector.bn_aggr(out=mv[:, :], in_=stats[:, :, :].rearrange("p a b -> p (a b)")),
          R=[stats], W=[mv])
    kb.op("act", lambda: nc.scalar.activation(out=sd[:, 0:1], in_=mv[:, 1:2], func=AF.Sqrt, bias=kb.eps_col[:, 0:1]),
          R=[mv, kb.eps_col], W=[sd])
    kb.op("dve", lambda: nc.vector.reciprocal(out=sd[:, 1:2], in_=sd[:, 0:1]), R=[sd], W=[sd])
    kb.op("dve", lambda: nc.vector.tensor_scalar(out=h[:, :], in0=h[:, :], scalar1=mv[:, 0:1], scalar2=sd[:, 1:2],
                                                 op0=ALU.subtract, op1=ALU.mult), R=[h, mv, sd], W=[h])
    kb.op("pool", lambda: nc.gpsimd.tensor_tensor(out=h[:, :], in0=h[:, :], in1=gt[1], op=ALU.mult),
          R=[h, gt[0]], W=[h])
    kb.op("dve", lambda: nc.vector.tensor_tensor(out=out_ap, in0=h[:, :], in1=bt[1], op=ALU.add),
          R=[h, bt[0]], W=[out_buf])


def build_C(n_exp, nf_unit, n_units_per_exp):
    kb = KB()
    nc = kb.nc
    moe = n_exp > 1
    NT = TPC // 128
    TG = 512
    NG = TPC // TG
    nfu = nf_unit
    n_chunks = n_exp * n_units_per_exp * nfu
    oT_d = kb.dram_in("oT", [128, 8, TPC], BF16)
    x_d = kb.dram_in("x", [TPC, D], F32)
    wo_d = kb.dram_in("wo", [128, 8, D], BF16)
    lnp_d = kb.dram_in("lnp", [128, 4, D], F32)
    wf_d = kb.dram_in("wf", [n_chunks, 128, 3072], BF16)
    ident_d = kb.dram_in("ident", [128, 128], F32)
    out_d = kb.dram_out("out", [TPC, D], F32)
    ident = kb.sb([128, 128], F32, "ident")
    wo = kb.sb([128, 8, D], BF16, "wo")
    lnp = kb.sb([128, 4, D], F32, "lnp")
    kb.eps_col = kb.sb([128, 1], F32, "eps")
    kb.op("dve", lambda: nc.vector.memset(kb.eps_col[:, :], LN_EPS), W=[kb.eps_col])
    kb.dma("sp", ident[:, :], ident_d[:, :], R=[ident_d], W=[ident])
    kb.dma("sp", wo[:, :, :], wo_d[:, :, :], R=[wo_d], W=[wo])
    kb.dma("pool", lnp[:, :, :], lnp_d[:, :, :], R=[lnp_d], W=[lnp])
    if moe:
        wr_d = kb.dram_in("wr", [128, 8, 8], F32)
        wr = kb.sb([128, 8, 8], F32, "wr")
        kb.dma("sp", wr[:, :, :], wr_d[:, :, :], R=[wr_d], W=[wr])
        x1T32 = kb.sb([128, 8, 128], F32, "x1T32")
        comb = [kb.sb([128, 8], F32, "comb") for _ in range(4)]
        rt = kb.sb([128, 40], F32, "rt")
    oT = [kb.sb([128, 8, TG], BF16, "oT") for _ in range(2)]
    xt = [kb.sb([128, D], F32, "xt") for _ in range(2)]
    h = kb.sb([128, D], F32, "h")
    x1g = [kb.sb([128, D], F32, "x1g") for _ in range(4)]
    x1T = kb.sb([128, 8, TG], BF16, "x1T")
    aT = [kb.sb([128, TG], BF16, "aT") for _ in range(nfu)]
    w2 = [kb.sb([128, D], BF16, "w2") for _ in range(nfu)]
    w13 = [kb.sb([128, 2048], BF16, "w13") for _ in range(3)]
    yacc = [kb.sb([128, D], F32, "yacc") for _ in range(4)]
    sil = [kb.sb([128, TG], F32, "sil") for _ in range(2)]
    ost = [kb.sb([128, D], F32, "ost") for _ in range(2)]
    scr = (kb.sb([128, 2, 6], F32, "stats"), kb.sb([128, 2], F32, "mv"), kb.sb([128, 2], F32, "sd"))
    X = [kb.ps([128, 512], F32, "X") for _ in range(4)]
    Y = [kb.ps([128, 512], F32, "Y") for _ in range(2)]
    wcnt = 0
    for g in range(NG):
        og = oT[g % 2]
        kb.dma("pool", og[:, :, :], oT_d[:, :, g * TG:(g + 1) * TG], R=[oT_d], W=[og])
        for tt in range(4):
            tok0 = g * TG + tt * 128
            xi = xt[tt % 2]
            kb.dma("sp", xi[:, :], x_d[tok0:tok0 + 128, :], R=[x_d], W=[xi])
            for hf in range(2):
                for kc in range(8):
                    kb.op("pe", lambda: nc.tensor.matmul(Y[hf][:, :], lhsT=og[:, kc, tt * 128:(tt + 1) * 128],
                                                         rhs=wo[:, kc, hf * 512:(hf + 1) * 512],
                                                         start=(kc == 0), stop=(kc == 7)), R=[og, wo], W=[Y[hf]])
                kb.op("dve", lambda: nc.vector.scalar_tensor_tensor(out=h[:, hf * 512:(hf + 1) * 512],
                                                                    in0=xi[:, hf * 512:(hf + 1) * 512], scalar=ALPHA,
                                                                    in1=Y[hf][:, :], op0=ALU.mult, op1=ALU.add),
                      R=[xi, Y[hf]], W=[h])
            x1 = x1g[tt]
            layer_norm(kb, h, (lnp, lnp[:, 0, :]), (lnp, lnp[:, 1, :]), x1[:, :], x1, scr)
            for kc in range(8):
                pt = X[kc // 4]
                kb.op("pe", lambda: nc.tensor.transpose(pt[:, (kc % 4) * 128:(kc % 4 + 1) * 128],
                                                        x1[:, kc * 128:(kc + 1) * 128], ident[:, :]),
                      R=[x1, ident], W=[pt])
            for hf in range(2):
                src = X[hf][:, :].rearrange("p (k t) -> p k t", k=4)
                dst = x1T[:, hf * 4:(hf + 1) * 4, tt * 128:(tt + 1) * 128]
                if hf == 0:
                    kb.op("dve", lambda: nc.vector.tensor_copy(out=dst, in_=src), R=[X[hf]], W=[x1T])
                else:
                    kb.op("act", lambda: nc.scalar.copy(out=dst, in_=src), R=[X[hf]], W=[x1T])
                if moe:
                    kb.op("pool" if False else "dve",
                          lambda: nc.vector.tensor_copy(out=x1T32[:, hf * 4:(hf + 1) * 4, :], in_=src),
                          R=[X[hf]], W=[x1T32])
            if moe:
                pr = X[2]
                for kc in range(8):
                    kb.op("pe", lambda: nc.tensor.matmul(pr[:, :8], lhsT=x1T32[:, kc, :], rhs=wr[:, kc, :],
                                                         start=(kc == 0), stop=(kc == 7)), R=[x1T32, wr], W=[pr])
                cb = comb[tt]
                lg, mx, tmp, oh = rt[:, 0:8], rt[:, 8:16], rt[:, 16:24], rt[:, 24:32]
                sc = rt[:, 32:40]
                kb.op("dve", lambda: nc.vector.tensor_copy(out=lg, in_=pr[:, :8]), R=[pr], W=[rt])
                kb.op("dve", lambda: nc.vector.max(out=mx, in_=lg), R=[rt], W=[rt])
                kb.op("dve", lambda: nc.vector.tensor_tensor(out=sc[:, 0:1], in0=mx[:, 1:2], in1=mx[:, 0:1],
                                                             op=ALU.subtract), R=[rt], W=[rt])
                kb.op("act", lambda: nc.scalar.activation(out=sc[:, 1:2], in_=sc[:, 0:1], func=AF.Exp), R=[rt], W=[rt])
                kb.op("dve", lambda: nc.vector.tensor_scalar(out=sc[:, 2:3], in0=sc[:, 1:2], scalar1=1.0, scalar2=None,
                                                             op0=ALU.add), R=[rt], W=[rt])
                kb.op("dve", lambda: nc.vector.reciprocal(out=sc[:, 3:4], in_=sc[:, 2:3]), R=[rt], W=[rt])
                kb.op("dve", lambda: nc.vector.tensor_tensor(out=sc[:, 4:5], in0=sc[:, 1:2], in1=sc[:, 3:4],
                                                             op=ALU.mult), R=[rt], W=[rt])
                kb.op("dve", lambda: nc.vector.tensor_scalar(out=tmp, in0=lg, scalar1=mx[:, 0:1], scalar2=sc[:, 3:4],
                                                             op0=ALU.is_equal, op1=ALU.mult), R=[rt], W=[rt])
                kb.op("dve", lambda: nc.vector.tensor_scalar(out=oh, in0=lg, scalar1=mx[:, 1:2], scalar2=sc[:, 4:5],
                                                             op0=ALU.is_equal, op1=ALU.mult), R=[rt], W=[rt])
                kb.op("dve", lambda: nc.vector.tensor_tensor(out=cb[:, :], in0=tmp, in1=oh, op=ALU.add),
                      R=[rt], W=[cb])
        first = True
        for e in range(n_exp):
            for u in range(n_units_per_exp):
                base = (e * n_units_per_exp + u) * nfu
                for f in range(nfu):
                    wc = w13[wcnt % 3]
                    q = "sp" if wcnt % 2 == 0 else "pool"
                    kb.dma(q, wc[:, :], wf_d[base + f, :, 0:2048], R=[wf_d], W=[wc])
                    kb.dma("pool" if wcnt % 2 == 0 else "sp", w2[f][:, :], wf_d[base + f, :, 2048:3072],
                           R=[wf_d], W=[w2[f]])
                    h1, h3 = X[(wcnt % 2) * 2], X[(wcnt % 2) * 2 + 1]
                    for kc in range(8):
                        kb.op("pe", lambda: nc.tensor.matmul(h1[:, :], lhsT=wc[:, kc * 128:(kc + 1) * 128],
                                                             rhs=x1T[:, kc, :], start=(kc == 0), stop=(kc == 7)),
                              R=[wc, x1T], W=[h1])
                    for kc in range(8):
                        kb.op("pe", lambda: nc.tensor.matmul(h3[:, :], lhsT=wc[:, 1024 + kc * 128:1024 + (kc + 1) * 128],
                                                             rhs=x1T[:, kc, :], start=(kc == 0), stop=(kc == 7)),
                              R=[wc, x1T], W=[h3])
                    s = sil[wcnt % 2]
                    kb.op("act", lambda: nc.scalar.activation(out=s[:, :], in_=h1[:, :], func=AF.Silu), R=[h1], W=[s])
                    kb.op("dve", lambda: nc.vector.tensor_tensor(out=aT[f][:, :], in0=s[:, :], in1=h3[:, :],
                                                                 op=ALU.mult), R=[s, h3], W=[aT[f]])
                    wcnt += 1
                for tt in range(4):
                    for hf in range(2):
                        py = Y[hf]
                        for f in range(nfu):
                            kb.op("pe", lambda: nc.tensor.matmul(py[:, :], lhsT=aT[f][:, tt * 128:(tt + 1) * 128],
                                                                 rhs=w2[f][:, hf * 512:(hf + 1) * 512],
                                                                 start=(f == 0), stop=(f == nfu - 1)),
                                  R=[aT[f], w2[f]], W=[py])
                        ya = yacc[tt]
                        ysl = ya[:, hf * 512:(hf + 1) * 512]
                        if moe:
                            cs = comb[tt][:, e:e + 1]
                            if first:
                                kb.op("dve", lambda: nc.vector.tensor_scalar(out=ysl, in0=py[:, :], scalar1=cs,
                                                                             scalar2=None, op0=ALU.mult),
                                      R=[py, comb[tt]], W=[ya])
                            else:
                                kb.op("dve", lambda: nc.vector.scalar_tensor_tensor(out=ysl, in0=py[:, :], scalar=cs,
                                                                                    in1=ysl, op0=ALU.mult, op1=ALU.add),
                                      R=[py, comb[tt], ya], W=[ya])
                        else:
                            if first:
                                kb.op("dve", lambda: nc.vector.tensor_copy(out=ysl, in_=py[:, :]), R=[py], W=[ya])
                            else:
                                kb.op("dve", lambda: nc.vector.tensor_tensor(out=ysl, in0=py[:, :], in1=ysl, op=ALU.add),
                                      R=[py, ya], W=[ya])
                first = False
        for tt in range(4):
            tok0 = g * TG + tt * 128
            kb.op("dve", lambda: nc.vector.scalar_tensor_tensor(out=h[:, :], in0=x1g[tt][:, :], scalar=ALPHA,
                                                                in1=yacc[tt][:, :], op0=ALU.mult, op1=ALU.add),
                  R=[x1g[tt], yacc[tt]], W=[h])
            o = ost[tt % 2]
            layer_norm(kb, h, (lnp, lnp[:, 2, :]), (lnp, lnp[:, 3, :]), o[:, :], o, scr)
            kb.dma("sp", out_d[tok0:tok0 + 128, :], o[:, :], R=[o], W=[out_d])
    return kb.finish()


def ffn_chunk_layout(w1, w3, w2):
    F = w1.shape[1]
    nf = F // 128
    a = w1.reshape(8, 128, nf, 128).transpose(2, 1, 0, 3).reshape(nf, 128, 1024)
    b = w3.reshape(8, 128, nf, 128).transpose(2, 1, 0, 3).reshape(nf, 128, 1024)
    c = w2.reshape(nf, 128, 1024)
    return np.ascontiguousarray(np.concatenate([a, b, c], axis=2))


def run_C(o_flat_bf, x_flat, wo_bf, lnp4, wf, n_exp, nf_unit, n_units, wr=None):
    nc = build_C(n_exp, nf_unit, n_units)
    wol = w_kc_layout(wo_bf)
    lnb = np.ascontiguousarray(np.broadcast_to(lnp4[None, :, :], (128, 4, D))).astype(np.float32)
    maps = []
    for c in range(NCORES):
        oc = o_flat_bf[c * TPC:(c + 1) * TPC]
        oT = np.ascontiguousarray(oc.reshape(TPC, 8, 128).transpose(2, 1, 0))
        m = {"oT": oT, "x": np.ascontiguousarray(x_flat[c * TPC:(c + 1) * TPC]), "wo": wol, "lnp": lnb,
             "wf": wf, "ident": IDENT}
        if wr is not None:
            m["wr"] = np.ascontiguousarray(wr.reshape(8, 128, 8).transpose(1, 0, 2))
        maps.append(m)
    res = run_spmd(nc, maps)
    return np.concatenate([res[c]["out"] for c in range(NCORES)], axis=0)


from concourse.bass_types import AP as _AP

NQB = SEQ // 128
NDEL = 2304


def rel_bucket_np(d):
    d = np.maximum(d, 0)
    df = np.maximum(d, 1).astype(np.float32)
    large = 16 + (np.log(df / np.float32(16.0)).astype(np.float32) / np.float32(math.log(2048 / 16))
                  * np.float32(16)).astype(np.int32)
    large = np.minimum(large, 31)
    return np.where(d < 16, d, large)


def dil_const():
    C = np.zeros((32, NDEL), np.float32)
    for idx in range(NDEL):
        dl = idx - 127
        if dl < 0 or dl > 2048:
            continue
        mult = 0
        for (w, dd) in ((128, 1), (512, 4), (2048, 16)):
            if dl <= w and dl % dd == 0:
                mult += 1
        if mult:
            C[int(rel_bucket_np(np.array([dl]))[0]), idx] = mult
    return C


class AttnCtx:
    def __init__(self, kb, n_s=4, n_o=2, grouped=False, fox=False):
        self.kb = kb
        self.S = [kb.ps([128, 512], F32, "S") for _ in range(n_s)]
        self.O = [kb.ps([128, 512], F32, "O") for _ in range(n_o)]
        self.P32 = [kb.sb([128, 128], F32, "P32") for _ in range(4)]
        self.Pb = [kb.sb([128, 128], BF16, "Pb") for _ in range(6)]
        self.pending = []
        self.LA = 3
        self.LAG = 2
        self.cpg = 0
        self.cpf = 0
        if grouped:
            self.P32g = [kb.sb([128, 512], F32, "P32g") for _ in range(3)]
            self.Pbg = [kb.sb([128, 512], BF16, "Pbg") for _ in range(4)]
        if fox:
            self.P32f = [kb.sb([128, 256], F32, "P32f") for _ in range(3)]
            self.Pbf = [kb.sb([128, 256], BF16, "Pbf") for _ in range(6)]
        self.rc = [kb.sb([128, 1], F32, "rc") for _ in range(2)]
        self.cs = 0
        self.co = 0
        self.cp = 0


def attn_qblock(ax, qT, kT, Vaug, h, i, jlist, bias_of, mask_of, ost, ocol, after=None):
    kb = ax.kb
    nc = kb.nc
    O = ax.O[ax.co % len(ax.O)]
    rc = ax.rc[ax.co % 2]
    ax.co += 1
    hp = slice(h * 64, (h + 1) * 64)
    nj = len(jlist)
    for n, j in enumerate(jlist):
        S = ax.S[ax.cs % len(ax.S)]
        ax.cs += 1
        kb.op("pe", lambda: nc.tensor.matmul(S[:, :128], lhsT=kT[hp, j * 128:(j + 1) * 128],
                                             rhs=qT[hp, i * 128:(i + 1) * 128], start=True, stop=True),
              R=[kT, qT], W=[S])
        Pb = ax.Pb[ax.cp % len(ax.Pb)]
        b = bias_of(j) if bias_of is not None else None
        m = mask_of(j) if mask_of is not None else None
        if m is not None and not isinstance(m, list):
            m = [m]
        if m is not None and len(m) == 0:
            m = None
        tgt = Pb if m is None else ax.P32[ax.cp % len(ax.P32)]
        ax.cp += 1
        if b is None:
            kb.op("act", lambda: nc.scalar.activation(out=tgt[:, :], in_=S[:, :128], func=AF.Exp, scale=0.125),
                  R=[S], W=[tgt])
        else:
            kb.op("act", lambda: nc.scalar.activation(out=tgt[:, :], in_=S[:, :128], func=AF.Exp, bias=b[1],
                                                      scale=0.125), R=[S, b[0]], W=[tgt])
        if m is not None:
            for mm in m[:-1]:
                kb.op("pool", lambda: nc.gpsimd.tensor_tensor(out=tgt[:, :], in0=tgt[:, :], in1=mm[1], op=ALU.mult),
                      R=[tgt, mm[0]], W=[tgt])
            kb.op("dve", lambda: nc.vector.tensor_tensor(out=Pb[:, :], in0=tgt[:, :], in1=m[-1][1], op=ALU.mult),
                  R=[tgt, m[-1][0]], W=[Pb])

        def pv(Pb=Pb, j=j, n=n):
            kb.op("pe", lambda: nc.tensor.matmul(O[:, :65], lhsT=Pb[:, :], rhs=Vaug[:, j, h, :],
                                                 start=(n == 0), stop=(n == nj - 1)), R=[Pb, Vaug], W=[O])
            if n == nj - 1:
                kb.op("dve", lambda: nc.vector.reciprocal(out=rc[:, :], in_=O[:, 64:65]), R=[O], W=[rc])
                kb.op("dve", lambda: nc.vector.tensor_scalar(out=ost[:, ocol:ocol + 64], in0=O[:, 0:64],
                                                             scalar1=rc[:, 0:1], scalar2=None, op0=ALU.mult),
                      R=[O, rc], W=[ost])
                if after is not None:
                    after()

        ax.pending.append(pv)
        while len(ax.pending) > ax.LA:
            ax.pending.pop(0)()


def attn_qgroup(ax, qT, kT, Vaug, h, i, groups, ost, ocol, after=None):
    kb = ax.kb
    nc = kb.nc
    O = ax.O[ax.co % len(ax.O)]
    rc = ax.rc[ax.co % 2]
    ax.co += 1
    hp = slice(h * 64, (h + 1) * 64)
    ng = len(groups)
    for gi, g in enumerate(groups):
        js = g["js"]
        wd = 128 * len(js)
        S = ax.S[ax.cs % len(ax.S)]
        ax.cs += 1
        for n, j in enumerate(js):
            kb.op("pe", lambda: nc.tensor.matmul(S[:, n * 128:(n + 1) * 128], lhsT=kT[hp, j * 128:(j + 1) * 128],
                                                 rhs=qT[hp, i * 128:(i + 1) * 128], start=True, stop=True),
                  R=[kT, qT], W=[S])
        Pb = ax.Pbg[ax.cpg % len(ax.Pbg)]
        has_mask = (g.get("pool_masks") is not None) or (g.get("dve_mask") is not None)
        tgt = ax.P32g[ax.cpg % len(ax.P32g)] if has_mask else Pb
        ax.cpg += 1
        b = g.get("bias")
        if b is None:
            kb.op("act", lambda: nc.scalar.activation(out=tgt[:, :wd], in_=S[:, :wd], func=AF.Exp, scale=0.125),
                  R=[S], W=[tgt])
        else:
            kb.op("act", lambda: nc.scalar.activation(out=tgt[:, :wd], in_=S[:, :wd], func=AF.Exp, bias=b[1],
                                                      scale=0.125), R=[S, b[0]], W=[tgt])
        if has_mask:
            pm = g.get("pool_masks")
            dm = g.get("dve_mask")
            if pm is not None:
                for n, mm in enumerate(pm):
                    last = (dm is None) and False
                    kb.op("pool", lambda: nc.gpsimd.tensor_tensor(out=tgt[:, n * 128:(n + 1) * 128],
                                                                  in0=tgt[:, n * 128:(n + 1) * 128], in1=mm[1],
                                                                  op=ALU.mult), R=[tgt, mm[0]], W=[tgt])
            if dm is not None:
                kb.op("dve", lambda: nc.vector.tensor_tensor(out=Pb[:, :wd], in0=tgt[:, :wd], in1=dm[1], op=ALU.mult),
                      R=[tgt, dm[0]], W=[Pb])
            else:
                kb.op("dve", lambda: nc.vector.tensor_copy(out=Pb[:, :wd], in_=tgt[:, :wd]), R=[tgt], W=[Pb])

        def pv(Pb=Pb, js=js, gi=gi):
            for n, j in enumerate(js):
                kb.op("pe", lambda: nc.tensor.matmul(O[:, :65], lhsT=Pb[:, n * 128:(n + 1) * 128], rhs=Vaug[:, j, h, :],
                                                     start=(gi == 0 and n == 0),
                                                     stop=(gi == ng - 1 and n == len(js) - 1)), R=[Pb, Vaug], W=[O])
            if gi == ng - 1:
                kb.op("dve", lambda: nc.vector.reciprocal(out=rc[:, :], in_=O[:, 64:65]), R=[O], W=[rc])
                kb.op("dve", lambda: nc.vector.tensor_scalar(out=ost[:, ocol:ocol + 64], in0=O[:, 0:64],
                                                             scalar1=rc[:, 0:1], scalar2=None, op0=ALU.mult),
                      R=[O, rc], W=[ost])
                if after is not None:
                    after()

        ax.pending.append(pv)
        while len(ax.pending) > ax.LAG:
            ax.pending.pop(0)()


def attn_fox_pair(ax, qT, kT, Vaug, h, i0, B, tri2, osts, ocol, after=None):
    kb = ax.kb
    nc = kb.nc
    i1 = i0 + 1
    O0, O1 = ax.O[0], ax.O[1]
    rc0, rc1 = ax.rc[0], ax.rc[1]
    hp = slice(h * 64, (h + 1) * 64)
    for j in range(0, i1 + 1):
        wide = (j <= i0)
        wd = 256 if wide else 128
        q0 = i0 * 128 if wide else i1 * 128
        S = ax.S[ax.cs % len(ax.S)]
        ax.cs += 1
        kb.op("pe", lambda: nc.tensor.matmul(S[:, :wd], lhsT=kT[hp, j * 128:(j + 1) * 128],
                                             rhs=qT[hp, q0:q0 + wd], start=True, stop=True), R=[kT, qT], W=[S])
        Pb = ax.Pbf[ax.cpf % len(ax.Pbf)]
        diag = (j >= i0)
        tgt = ax.P32f[ax.cpf % len(ax.P32f)] if diag else Pb
        ax.cpf += 1
        kb.op("act", lambda: nc.scalar.activation(out=tgt[:, :wd], in_=S[:, :wd], func=AF.Exp, bias=B[:, j:j + 1],
                                                  scale=0.125), R=[S, B], W=[tgt])
        if diag:
            kb.op("dve", lambda: nc.vector.tensor_tensor(out=Pb[:, :wd], in0=tgt[:, :wd], in1=tri2[:, :wd], op=ALU.mult),
                  R=[tgt, tri2], W=[Pb])

        def pv(Pb=Pb, j=j, wide=wide):
            if wide:
                kb.op("pe", lambda: nc.tensor.matmul(O0[:, :65], lhsT=Pb[:, 0:128], rhs=Vaug[:, j, h, :],
                                                     start=(j == 0), stop=(j == i0)), R=[Pb, Vaug], W=[O0])
                kb.op("pe", lambda: nc.tensor.matmul(O1[:, :65], lhsT=Pb[:, 128:256], rhs=Vaug[:, j, h, :],
                                                     start=(j == 0), stop=False), R=[Pb, Vaug], W=[O1])
            else:
                kb.op("pe", lambda: nc.tensor.matmul(O1[:, :65], lhsT=Pb[:, 0:128], rhs=Vaug[:, j, h, :],
                                                     start=False, stop=True), R=[Pb, Vaug], W=[O1])
                for (O, rc, ost) in ((O0, rc0, osts[0]), (O1, rc1, osts[1])):
                    kb.op("dve", lambda: nc.vector.reciprocal(out=rc[:, :], in_=O[:, 64:65]), R=[O], W=[rc])
                    kb.op("dve", lambda: nc.vector.tensor_scalar(out=ost[:, ocol:ocol + 64], in0=O[:, 0:64],
                                                                 scalar1=rc[:, 0:1], scalar2=None, op0=ALU.mult),
                          R=[O, rc], W=[ost])
                if after is not None:
                    after()

        ax.pending.append(pv)
        while len(ax.pending) > ax.LA:
            ax.pending.pop(0)()


def attn_flush(ax):
    while ax.pending:
        ax.pending.pop(0)()


def load_vaug(kb, v_d, name):
    nc = kb.nc
    Vaug = kb.sb([128, NQB, 2, 65], BF16, name)
    kb.op("pool", lambda: nc.gpsimd.memset(Vaug[:, :, :, :], 1.0), W=[Vaug])
    for c in range(4):
        js = slice(c * 16, (c + 1) * 16)
        kb.dma("sp" if c % 2 == 0 else "pool", Vaug[:, js, :, 0:64],
               v_d[:, js, :].rearrange("p j (h d) -> p j h d", h=2), R=[v_d], W=[Vaug])
    return Vaug


def build_B_cd():
    kb = KB()
    nc = kb.nc
    qc_d = kb.dram_in("qc", [128, SEQ], BF16)
    kc_d = kb.dram_in("kc", [128, SEQ], BF16)
    vc_d = kb.dram_in("vc", [128, NQB, 128], BF16)
    qd_d = kb.dram_in("qd", [128, SEQ], BF16)
    kd_d = kb.dram_in("kd", [128, SEQ], BF16)
    vd_d = kb.dram_in("vd", [128, NQB, 128], BF16)
    fc_d = kb.dram_in("fc", [128, NQB, 2], F32)
    bf_d = kb.dram_in("bf", [128, 2], F32)
    tri_d = kb.dram_in("tri", [128, 128], F32)
    rel_d = kb.dram_in("rel", [32, 2], F32)
    C_d = kb.dram_in("dilc", [32, NDEL], F32)
    oc_d = kb.dram_out("oc", [SEQ, 128], BF16)
    od_d = kb.dram_out("od", [SEQ, 128], BF16)
    E_d = kb.dram_tmp("Escr", [2, NDEL], F32)

    tri = kb.sb([128, 128], F32, "tri")
    ones = kb.sb([128, 128], F32, "ones")
    kb.dma("sp", tri[:, :], tri_d[:, :], R=[tri_d], W=[tri])
    kb.op("dve", lambda: nc.vector.memset(ones[:, :], 1.0), W=[ones])
    ax = AttnCtx(kb)
    rel = kb.sb([32, 2], F32, "rel")
    Cs = kb.sb([32, NDEL], F32, "Cs")
    Es = kb.sb([2, NDEL], F32, "Es")
    kb.dma("sp", rel[:, :], rel_d[:, :], R=[rel_d], W=[rel])
    kb.dma("sp", Cs[:, :], C_d[:, :], R=[C_d], W=[Cs])
    kb.op("act", lambda: nc.scalar.activation(out=rel[:, :], in_=rel[:, :], func=AF.Exp), R=[rel], W=[rel])
    for c in range((NDEL + 511) // 512):
        w = min(512, NDEL - c * 512)
        pp = ax.S[c % 3]
        kb.op("pe", lambda: nc.tensor.matmul(pp[:2, :w], lhsT=rel[:, :], rhs=Cs[:, c * 512:c * 512 + w],
                                             start=True, stop=True), R=[rel, Cs], W=[pp])
        kb.op("dve", lambda: nc.vector.tensor_copy(out=Es[:, c * 512:c * 512 + w], in_=pp[:2, :w]), R=[pp], W=[Es])
    kb.dma("sp", E_d[:, :], Es[:, :], R=[Es], W=[E_d])
    TT = [kb.sb([128, 17 * 128], F32, "TT") for _ in range(2)]
    for h in range(2):
        src = _AP(tensor=E_d.t.tensor, offset=h * NDEL, ap=[[1, 128], [1, 17 * 128]])
        kb.dma("sp", TT[h][:, :], src, R=[E_d], W=[TT[h]])
    fc = kb.sb([128, NQB, 2], F32, "fc")
    bf = kb.sb([128, 2], F32, "bf")
    lf = kb.sb([128, 2, NQB], F32, "lf")
    kb.dma("sp", fc[:, :, :], fc_d[:, :, :], R=[fc_d], W=[fc])
    kb.dma("sp", bf[:, :], bf_d[:, :], R=[bf_d], W=[bf])
    for h in range(2):
        kb.op("dve", lambda: nc.vector.tensor_scalar(out=lf[:, h, :], in0=fc[:, :, h], scalar1=bf[:, h:h + 1],
                                                     scalar2=None, op0=ALU.add), R=[fc, bf], W=[lf])
    kb.op("act", lambda: nc.scalar.activation(out=lf[:, :, :], in_=lf[:, :, :], func=AF.Exp, scale=-1.0), R=[lf], W=[lf])
    kb.op("act", lambda: nc.scalar.activation(out=lf[:, :, :], in_=lf[:, :, :], func=AF.Ln, bias=1.0), R=[lf], W=[lf])
    kb.op("dve", lambda: nc.vector.tensor_scalar(out=lf[:, :, :], in0=lf[:, :, :], scalar1=-1.0, scalar2=None,
                                                 op0=ALU.mult), R=[lf], W=[lf])
    lf2 = lf[:, :, :].rearrange("p h j -> p (h j)")
    p1, p2 = ax.O[0], ax.O[1]
    kb.op("pe", lambda: nc.tensor.matmul(p1[:, :128], lhsT=tri[:, :], rhs=lf2, start=True, stop=True),
          R=[tri, lf], W=[p1])
    kb.op("pe", lambda: nc.tensor.matmul(p2[:, :128], lhsT=ones[:, :], rhs=lf2, start=True, stop=True),
          R=[ones, lf], W=[p2])
    tot = kb.sb([128, 2, NQB], F32, "tot")
    carry = kb.sb([128, 2, NQB], F32, "carry")
    negF = kb.sb([128, 2, NQB], F32, "negF")
    kb.op("dve", lambda: nc.vector.tensor_copy(out=tot[:, :, :].rearrange("p h j -> p (h j)"), in_=p2[:, :128]),
          R=[p2], W=[tot])
    for h in range(2):
        kb.op("dve", lambda: nc.vector.tensor_tensor_scan(out=carry[:, h, :], data0=ones[:, :NQB], data1=tot[:, h, :],
                                                          initial=0.0, op0=ALU.mult, op1=ALU.add),
              R=[ones, tot], W=[carry])
    kb.op("dve", lambda: nc.vector.tensor_tensor(out=carry[:, :, :], in0=carry[:, :, :], in1=tot[:, :, :],
                                                 op=ALU.subtract), R=[carry, tot], W=[carry])
    kb.op("dve", lambda: nc.vector.tensor_tensor(out=negF[:, :, :].rearrange("p h j -> p (h j)"), in0=p1[:, :128],
                                                 in1=carry[:, :, :].rearrange("p h j -> p (h j)"), op=ALU.add),
          R=[p1, carry], W=[negF])
    kb.op("dve", lambda: nc.vector.tensor_scalar(out=negF[:, :, :], in0=negF[:, :, :], scalar1=-1.0, scalar2=None,
                                                 op0=ALU.mult), R=[negF], W=[negF])
    qc = kb.sb([128, SEQ], BF16, "qc")
    kc = kb.sb([128, SEQ], BF16, "kc")
    qd = kb.sb([128, SEQ], BF16, "qd")
    kd = kb.sb([128, SEQ], BF16, "kd")
    for n, (s, d_) in enumerate(((qd, qd_d), (kd, kd_d), (qc, qc_d), (kc, kc_d))):
        for c in range(2):
            kb.dma("sp" if (n + c) % 2 == 0 else "pool", s[:, c * 4096:(c + 1) * 4096], d_[:, c * 4096:(c + 1) * 4096],
                   R=[d_], W=[s])
    Vd = load_vaug(kb, vd_d, "Vd")
    Vc = load_vaug(kb, vc_d, "Vc")
    ostd = [kb.sb([128, 128], BF16, "ostd") for _ in range(2)]
    ostc = [kb.sb([128, 128], BF16, "ostc") for _ in range(2)]
    Bi = [kb.sb([128, NQB], F32, "Bi") for _ in range(3)]
    nb = 0
    for i in range(NQB):
        od = ostd[i % 2]
        for h in range(2):
            j0 = max(0, i - 16)
            attn_qblock(ax, qd, kd, Vd, h, i, list(range(j0, i + 1)), None,
                        lambda j, h=h, i=i: (TT[h], TT[h][:, (i - j) * 128:(i - j + 1) * 128]), od, h * 64,
                        after=(None if h == 0 else
                               (lambda i=i, od=od: kb.dma("pool", od_d[i * 128:(i + 1) * 128, :], od[:, :],
                                                          R=[od], W=[od_d]))))
        oc = ostc[i % 2]
        for h in range(2):
            B = Bi[nb % 3]
            nb += 1
            kb.op("dve", lambda: nc.vector.tensor_scalar(out=B[:, :i + 1], in0=negF[:, h, :i + 1],
                                                         scalar1=carry[:, h, i:i + 1], scalar2=None, op0=ALU.add),
                  R=[negF, carry], W=[B])
            attn_qblock(ax, qc, kc, Vc, h, i, list(range(0, i + 1)),
                        lambda j, B=B: (B, B[:, j:j + 1]),
                        lambda j, i=i: ((tri, tri[:, :]) if j == i else None), oc, h * 64,
                        after=(None if h == 0 else
                               (lambda i=i, oc=oc: kb.dma("sp", oc_d[i * 128:(i + 1) * 128, :], oc[:, :],
                                                          R=[oc], W=[oc_d]))))
    attn_flush(ax)
    return kb.finish()


TRI = np.triu(np.ones((128, 128), np.float32))


def to_pj(a):
    n = a.shape[1]
    return np.ascontiguousarray(a.reshape(NQB, 128, n).transpose(1, 0, 2))


def run_B_cd(yT, ytb, ytf, b_forget, rel_table):
    nc = build_B_cd()
    C = dil_const()
    maps = []
    for c in range(NCORES):
        b, m = c // 4, c % 4
        ts = slice(b * SEQ, (b + 1) * SEQ)
        rs = lambda base: slice(base + m * 128, base + (m + 1) * 128)
        maps.append({
            "qc": np.ascontiguousarray(yT[rs(0), ts]), "kc": np.ascontiguousarray(yT[rs(512), ts]),
            "qd": np.ascontiguousarray(yT[rs(1024), ts]),
            "kd": np.ascontiguousarray(yT[rs(1536), ts].reshape(128, NQB, 128)[:, :, ::-1].reshape(128, SEQ)),
            "vc": to_pj(ytb[ts, m * 128:(m + 1) * 128]), "vd": np.ascontiguousarray(to_pj(ytb[ts, 512 + m * 128:512 + (m + 1) * 128])[::-1]),
            "fc": to_pj(ytf[ts, 2 * m:2 * m + 2]),
            "bf": np.ascontiguousarray(np.broadcast_to(b_forget[None, 2 * m:2 * m + 2], (128, 2))).astype(np.float32),
            "tri": TRI, "rel": np.ascontiguousarray(rel_table[:, 2 * m:2 * m + 2]), "dilc": C,
        })
    res = run_spmd(nc, maps)
    o = np.zeros((BATCH * SEQ, D), NPBF)
    for c in range(NCORES):
        b, m = c // 4, c % 4
        o[b * SEQ:(b + 1) * SEQ, m * 128:(m + 1) * 128] = res[c]["oc"]
        o[b * SEQ:(b + 1) * SEQ, 512 + m * 128:512 + (m + 1) * 128] = res[c]["od"]
    return o


def build_B_gla():
    kb = KB()
    nc = kb.nc
    qT_d = kb.dram_in("qT", [64, SEQ], BF16)
    kT_d = kb.dram_in("kT", [64, SEQ], BF16)
    k_d = kb.dram_in("k", [128, NQB, 64], BF16)
    v_d = kb.dram_in("v", [128, NQB, 128], BF16)
    r_d = kb.dram_in("r", [128, NQB, 128], F32)
    g_d = kb.dram_in("g", [128, NQB, 64], F32)
    gn_d = kb.dram_in("gn", [128, 128], F32)
    tri_d = kb.dram_in("tri", [128, 128], F32)
    o_d = kb.dram_out("o", [SEQ, 128], BF16)
    qT = kb.sb([64, SEQ], BF16, "qT")
    kT = kb.sb([64, SEQ], BF16, "kT")
    ktm = kb.sb([128, NQB, 64], BF16, "ktm")
    v = kb.sb([128, NQB, 128], BF16, "v")
    r = kb.sb([128, NQB, 128], F32, "r")
    g = kb.sb([128, NQB, 64], F32, "g")
    gn = kb.sb([128, 128], F32, "gn")
    tri = kb.sb([128, 128], F32, "tri")
    eps = kb.sb([128, 1], F32, "eps")
    kb.op("dve", lambda: nc.vector.memset(eps[:, :], LN_EPS), W=[eps])
    kb.dma("sp", tri[:, :], tri_d[:, :], R=[tri_d], W=[tri])
    kb.dma("sp", g[:, :, :], g_d[:, :, :], R=[g_d], W=[g])
    kb.dma("pool", qT[:, :], qT_d[:, :], R=[qT_d], W=[qT])
    kb.dma("sp", kT[:, :], kT_d[:, :], R=[kT_d], W=[kT])
    kb.dma("pool", ktm[:, :, :], k_d[:, :, :], R=[k_d], W=[ktm])
    kb.dma("sp", v[:, :, :], v_d[:, :, :], R=[v_d], W=[v])
    kb.dma("pool", r[:, :, :], r_d[:, :, :], R=[r_d], W=[r])
    kb.dma("sp", gn[:, :], gn_d[:, :], R=[gn_d], W=[gn])
    kb.op("act", lambda: nc.scalar.activation(out=r[:, :, :], in_=r[:, :, :], func=AF.Silu), R=[r], W=[r])
    PG = [kb.ps([128, 512], F32, "PG") for _ in range(2)]
    PGT = [kb.ps([128, 512], F32, "PGT") for _ in range(2)]
    PA = [kb.ps([128, 512], F32, "PA") for _ in range(2)]
    PO = kb.ps([128, 512], F32, "PO")
    PU = kb.ps([128, 512], F32, "PU")
    eGT = [kb.sb([64, 128], F32, "eGT") for _ in range(2)]
    enGT = [kb.sb([64, 128], F32, "enGT") for _ in range(2)]
    enG = [kb.sb([128, 64], F32, "enG") for _ in range(2)]
    qgT = [kb.sb([64, 128], BF16, "qgT") for _ in range(2)]
    kgT = [kb.sb([64, 128], BF16, "kgT") for _ in range(2)]
    kg = [kb.sb([128, 64], BF16, "kg") for _ in range(2)]
    Am = [kb.sb([128, 128], BF16, "Am") for _ in range(2)]
    S32 = kb.sb([64, 128], F32, "S32")
    Sbf = kb.sb([64, 128], BF16, "Sbf")
    st6 = [kb.sb([128, 6], F32, "st6") for _ in range(2)]
    mv = [kb.sb([128, 4], F32, "mv") for _ in range(2)]
    of = [kb.sb([128, 128], F32, "of") for _ in range(2)]
    ost = [kb.sb([128, 128], BF16, "ost") for _ in range(2)]
    for c in range(NQB):
        p = c % 2
        cs = slice(c * 128, (c + 1) * 128)
        kb.op("pe", lambda: nc.tensor.matmul(PG[p][:, :64], lhsT=tri[:, :], rhs=g[:, c, :], start=True, stop=True),
              R=[tri, g], W=[PG[p]])
        kb.op("pe", lambda: nc.tensor.matmul(PGT[p][:64, :128], lhsT=g[:, c, :], rhs=tri[:, :], start=True, stop=True),
              R=[tri, g], W=[PGT[p]])
        kb.op("act", lambda: nc.scalar.activation(out=eGT[p][:, :], in_=PGT[p][:64, :128], func=AF.Exp),
              R=[PGT[p]], W=[eGT[p]])
        kb.op("act", lambda: nc.scalar.activation(out=enGT[p][:, :], in_=PGT[p][:64, :128], func=AF.Exp, scale=-1.0),
              R=[PGT[p]], W=[enGT[p]])
        kb.op("act", lambda: nc.scalar.activation(out=enG[p][:, :], in_=PG[p][:, :64], func=AF.Exp, scale=-1.0),
              R=[PG[p]], W=[enG[p]])
        kb.op("dve", lambda: nc.vector.scalar_tensor_tensor(out=qgT[p][:, :], in0=qT[:, cs], scalar=0.125,
                                                            in1=eGT[p][:, :], op0=ALU.mult, op1=ALU.mult),
              R=[qT, eGT[p]], W=[qgT[p]])
        kb.op("dve", lambda: nc.vector.tensor_tensor(out=kgT[p][:, :], in0=kT[:, cs], in1=enGT[p][:, :], op=ALU.mult),
              R=[kT, enGT[p]], W=[kgT[p]])
        kb.op("dve", lambda: nc.vector.tensor_tensor(out=kg[p][:, :], in0=ktm[:, c, :], in1=enG[p][:, :], op=ALU.mult),
              R=[ktm, enG[p]], W=[kg[p]])
        kb.op("pe", lambda: nc.tensor.matmul(PA[p][:, :128], lhsT=kgT[p][:, :], rhs=qgT[p][:, :], start=True, stop=True),
              R=[kgT[p], qgT[p]], W=[PA[p]])
        kb.op("dve", lambda: nc.vector.tensor_tensor(out=Am[p][:, :], in0=PA[p][:, :128], in1=tri[:, :], op=ALU.mult),
              R=[PA[p], tri], W=[Am[p]])
        kb.op("pe", lambda: nc.tensor.matmul(PO[:, :128], lhsT=Am[p][:, :], rhs=v[:, c, :], start=True, stop=(c == 0)),
              R=[Am[p], v], W=[PO])
        if c > 0:
            kb.op("pe", lambda: nc.tensor.matmul(PO[:, :128], lhsT=qgT[p][:, :], rhs=Sbf[:, :], start=False, stop=True),
                  R=[qgT[p], Sbf], W=[PO])
        if c < NQB - 1:
            kb.op("pe", lambda: nc.tensor.matmul(PU[:64, :128], lhsT=kg[p][:, :], rhs=v[:, c, :], start=True, stop=True),
                  R=[kg[p], v], W=[PU])
            eGl = eGT[p][:, 127:128]
            if c == 0:
                kb.op("dve", lambda: nc.vector.tensor_scalar(out=S32[:, :], in0=PU[:64, :128], scalar1=eGl, scalar2=None,
                                                             op0=ALU.mult), R=[PU, eGT[p]], W=[S32])
            else:
                kb.op("dve", lambda: nc.vector.tensor_scalar(out=S32[:, :], in0=S32[:, :], scalar1=eGl, scalar2=None,
                                                             op0=ALU.mult), R=[S32, eGT[p]], W=[S32])
                kb.op("dve", lambda: nc.vector.scalar_tensor_tensor(out=S32[:, :], in0=PU[:64, :128], scalar=eGl,
                                                                    in1=S32[:, :], op0=ALU.mult, op1=ALU.add),
                      R=[PU, eGT[p], S32], W=[S32])
            kb.op("dve", lambda: nc.vector.tensor_copy(out=Sbf[:, :], in_=S32[:, :]), R=[S32], W=[Sbf])
        kb.op("dve", lambda: nc.vector.bn_stats(out=st6[p][:, :], in_=PO[:, :128]), R=[PO], W=[st6[p]])
        kb.op("dve", lambda: nc.vector.bn_aggr(out=mv[p][:, 0:2], in_=st6[p][:, :]), R=[st6[p]], W=[mv[p]])
        kb.op("dve", lambda: nc.vector.scalar_tensor_tensor(out=mv[p][:, 2:3], in0=mv[p][:, 0:1], scalar=mv[p][:, 0:1],
                                                            in1=mv[p][:, 1:2], op0=ALU.mult, op1=ALU.add),
              R=[mv[p]], W=[mv[p]])
        kb.op("act", lambda: nc.scalar.activation(out=mv[p][:, 3:4], in_=mv[p][:, 2:3], func=AF.Ln, bias=eps[:, 0:1]),
              R=[mv[p], eps], W=[mv[p]])
        kb.op("act", lambda: nc.scalar.activation(out=mv[p][:, 3:4], in_=mv[p][:, 3:4], func=AF.Exp, scale=-0.5),
              R=[mv[p]], W=[mv[p]])
        kb.op("dve", lambda: nc.vector.scalar_tensor_tensor(out=of[p][:, :], in0=PO[:, :128], scalar=mv[p][:, 3:4],
                                                            in1=gn[:, :], op0=ALU.mult, op1=ALU.mult),
              R=[PO, mv[p], gn], W=[of[p]])
        kb.op("pool", lambda: nc.gpsimd.tensor_tensor(out=ost[p][:, :], in0=of[p][:, :], in1=r[:, c, :], op=ALU.mult),
              R=[of[p], r], W=[ost[p]])
        kb.dma("sp", o_d[cs, :], ost[p][:, :], R=[ost[p]], W=[o_d])
    return kb.finish()


def run_B_gla(yT, ytb, ytf, g_norm):
    nc = build_B_gla()
    maps = []
    for c in range(NCORES):
        b, h = c // 4, c % 4
        ts = slice(b * SEQ, (b + 1) * SEQ)
        maps.append({
            "qT": np.ascontiguousarray(yT[h * 64:(h + 1) * 64, ts]),
            "kT": np.ascontiguousarray(yT[256 + h * 64:256 + (h + 1) * 64, ts]),
            "k": to_pj(ytb[ts, h * 64:(h + 1) * 64]),
            "v": to_pj(ytb[ts, 256 + h * 128:256 + (h + 1) * 128]),
            "r": to_pj(ytf[ts, h * 128:(h + 1) * 128]),
            "g": to_pj(ytf[ts, 520 + h * 64:520 + (h + 1) * 64]),
            "gn": np.ascontiguousarray(np.broadcast_to(g_norm[None, :], (128, 128))).astype(np.float32),
            "tri": TRI,
        })
    res = run_spmd(nc, maps)
    o = np.zeros((BATCH * SEQ, 512), NPBF)
    for c in range(NCORES):
        b, h = c // 4, c % 4
        o[b * SEQ:(b + 1) * SEQ, h * 128:(h + 1) * 128] = res[c]["o"]
    return o


NBIS = 20
TOPK = 256


def build_B_dsa1(act_split=True):
    kb = KB()
    nc = kb.nc
    NK = 16
    qiT_d = kb.dram_in("qiT", [64, 8, NK * 128], BF16)
    kiT_d = kb.dram_in("kiT", [64, SEQ], BF16)
    wi_d = kb.dram_in("wi", [128, NK, 8], F32)
    cm_d = kb.dram_in("cmask", [128, 512], F32)
    idb_d = kb.dram_in("identb", [128, 128], BF16)
    stp_d = kb.dram_in("steps", [128, NBIS], F32)
    M_d = kb.dram_out("M", [NK, 128, SEQ], BF16)
    qiT = kb.sb([64, 8, NK * 128], BF16, "qiT")
    kiT = kb.sb([64, SEQ], BF16, "kiT")
    wi = kb.sb([128, NK, 8], F32, "wi")
    absw = kb.sb([128, NK, 8], F32, "absw")
    sgn = kb.sb([128, NK, 8], F32, "sgn")
    cm = kb.sb([128, 512], F32, "cm")
    idb = kb.sb([128, 128], BF16, "idb")
    stp = kb.sb([128, NBIS], F32, "stp")
    kb.dma("sp", qiT[:, :, :], qiT_d[:, :, :], R=[qiT_d], W=[qiT])
    kb.dma("pool", kiT[:, :], kiT_d[:, :], R=[kiT_d], W=[kiT])
    kb.dma("sp", wi[:, :, :], wi_d[:, :, :], R=[wi_d], W=[wi])
    kb.dma("sp", cm[:, :], cm_d[:, :], R=[cm_d], W=[cm])
    kb.dma("sp", idb[:, :], idb_d[:, :], R=[idb_d], W=[idb])
    kb.dma("sp", stp[:, :], stp_d[:, :], R=[stp_d], W=[stp])
    kb.op("act", lambda: nc.scalar.activation(out=absw[:, :, :], in_=wi[:, :, :], func=AF.Abs), R=[wi], W=[absw])
    kb.op("act", lambda: nc.scalar.activation(out=sgn[:, :, :], in_=wi[:, :, :], func=AF.Sign), R=[wi], W=[sgn])
    PS = [kb.ps([128, 512], F32, "PS") for _ in range(3)]
    PT = [kb.ps([128, 1024], BF16, "PT") for _ in range(2)]

    def write_mask(k, mt, Lk):
        kb.dma("sp", M_d[k, :, :Lk], mt[:, :Lk], R=[mt], W=[M_d])

    dsa1_body(kb, qiT, kiT, absw, sgn, cm, idb, stp, PS, PT, write_mask, act_split)
    return kb.finish()


def dsa1_body(kb, qiT, kiT, absw, sgn, cm, idb, stp, PS, PT, write_mask, act_split=True):
    nc = kb.nc
    NK = 16
    score = [kb.sb([128, SEQ], F32, "score") for _ in range(2)]
    selb = kb.sb([128, SEQ], BF16, "selb")
    junk2 = kb.sb([128, 5120], BF16, "junk2")
    MT = [kb.sb([128, SEQ], BF16, "MT") for _ in range(2)]
    rl = [kb.sb([128, 512], F32, "rl") for _ in range(3)]
    bs = [kb.sb([128, 16], F32, "bs") for _ in range(2)]
    stk = [kb.sb([128, NBIS], F32, "stk") for _ in range(2)]
    midb = [kb.sb([128, 1], F32, "midb") for _ in range(2)]
    cntd = [kb.sb([128, 1], F32, "cntd") for _ in range(2)]
    cnta = [kb.sb([128, 1], F32, "cnta") for _ in range(2)]
    tmpb = [kb.sb([128, 1], F32, "tmpb") for _ in range(2)]
    geb = [kb.sb([128, 1], F32, "geb") for _ in range(2)]
    st = {"rl": 0, "pt": 0}

    def scoring(k):
        sc_ = score[k % 2]
        for c in range(k + 1):
            for hi in range(8):
                pp = PS[st["rl"] % 3]
                r_ = rl[st["rl"] % 3]
                st["rl"] += 1
                kb.op("pe", lambda: nc.tensor.matmul(pp[:, :], lhsT=qiT[:, hi, k * 128:(k + 1) * 128],
                                                     rhs=kiT[:, c * 512:(c + 1) * 512], start=True, stop=True),
                      R=[qiT, kiT], W=[pp])
                kb.op("act", lambda: nc.scalar.activation(out=r_[:, :], in_=pp[:, :], func=AF.Relu,
                                                          scale=absw[:, k, hi:hi + 1]), R=[pp, absw], W=[r_])
                ssl = sc_[:, c * 512:(c + 1) * 512]
                if hi == 0:
                    kb.op("dve", lambda: nc.vector.tensor_scalar(out=ssl, in0=r_[:, :], scalar1=sgn[:, k, 0:1],
                                                                 scalar2=None, op0=ALU.mult), R=[r_, sgn], W=[sc_])
                else:
                    kb.op("dve", lambda: nc.vector.scalar_tensor_tensor(out=ssl, in0=r_[:, :],
                                                                        scalar=sgn[:, k, hi:hi + 1], in1=ssl,
                                                                        op0=ALU.mult, op1=ALU.add),
                          R=[r_, sgn, sc_], W=[sc_])

    def setup(k):
        L = 512 * (k + 1)
        sc_, b_, sk = score[k % 2], bs[k % 2], stk[k % 2]
        kb.op("dve", lambda: nc.vector.tensor_reduce(out=b_[:, 0:1], in_=sc_[:, :L], axis=AX.X, op=ALU.min),
              R=[sc_], W=[b_])
        kb.op("dve", lambda: nc.vector.tensor_reduce(out=b_[:, 1:2], in_=sc_[:, :L], axis=AX.X, op=ALU.max),
              R=[sc_], W=[b_])
        kb.op("dve", lambda: nc.vector.tensor_tensor(out=sc_[:, L - 512:L], in0=sc_[:, L - 512:L], in1=cm[:, :],
                                                     op=ALU.add), R=[sc_, cm], W=[sc_])
        kb.op("dve", lambda: nc.vector.tensor_tensor(out=b_[:, 2:3], in0=b_[:, 1:2], in1=b_[:, 0:1], op=ALU.subtract),
              R=[b_], W=[b_])
        kb.op("dve", lambda: nc.vector.tensor_scalar(out=sk[:, :], in0=stp[:, :], scalar1=b_[:, 2:3], scalar2=None,
                                                     op0=ALU.mult), R=[stp, b_], W=[sk])
        kb.op("dve", lambda: nc.vector.scalar_tensor_tensor(out=midb[k % 2][:, 0:1], in0=b_[:, 2:3], scalar=0.5,
                                                            in1=b_[:, 0:1], op0=ALU.mult, op1=ALU.add),
              R=[b_], W=[midb[k % 2]])

    def iteration(k, n):
        L = 512 * (k + 1)
        p = k % 2
        sc_, sk = score[p], stk[p]
        Ld = (L * 2 // 5) if act_split else L
        if act_split:
            kb.op("act", lambda: nc.scalar.activation(out=junk2[:, :L - Ld], in_=sc_[:, Ld:L], func=AF.Sign,
                                                      bias=midb[p][:, 0:1], scale=-1.0, accum_out=cnta[p][:, 0:1]),
                  R=[sc_, midb[p]], W=[junk2, cnta[p]])
        kb.op("dve", lambda: nc.vector.tensor_scalar(out=selb[:, :Ld], in0=sc_[:, :Ld], scalar1=midb[p][:, 0:1],
                                                     scalar2=0.0, op0=ALU.is_ge, op1=ALU.add,
                                                     accum_out=cntd[p][:, 0:1]),
              R=[sc_, midb[p]], W=[selb, cntd[p]])
        thr = TOPK - 0.25
        if act_split:
            kb.op("dve", lambda: nc.vector.scalar_tensor_tensor(out=tmpb[p][:, 0:1], in0=cnta[p][:, 0:1], scalar=-0.5,
                                                                in1=cntd[p][:, 0:1], op0=ALU.mult, op1=ALU.add),
                  R=[cnta[p], cntd[p]], W=[tmpb[p]])
            thr -= 0.5 * (L - Ld)
            src, srcb = tmpb[p][:, 0:1], tmpb[p]
        else:
            src, srcb = cntd[p][:, 0:1], cntd[p]
        kb.op("dve", lambda: nc.vector.tensor_scalar(out=geb[p][:, 0:1], in0=src, scalar1=thr, scalar2=-0.5,
                                                     op0=ALU.is_ge, op1=ALU.add), R=[srcb], W=[geb[p]])
        kb.op("dve", lambda: nc.vector.scalar_tensor_tensor(out=midb[p][:, 0:1], in0=geb[p][:, 0:1],
                                                            scalar=sk[:, n:n + 1], in1=midb[p][:, 0:1],
                                                            op0=ALU.mult, op1=ALU.add),
              R=[geb[p], sk, midb[p]], W=[midb[p]])

    def finish_block(k):
        L = 512 * (k + 1)
        sc_, b_, sk = score[k % 2], bs[k % 2], stk[k % 2]
        kb.op("dve", lambda: nc.vector.scalar_tensor_tensor(out=b_[:, 8:9], in0=sk[:, NBIS - 1:NBIS], scalar=-0.5,
                                                            in1=midb[k % 2][:, 0:1], op0=ALU.mult, op1=ALU.add),
              R=[midb[k % 2], sk], W=[b_])
        kb.op("dve", lambda: nc.vector.tensor_scalar(out=selb[:, :L], in0=sc_[:, :L], scalar1=b_[:, 8:9], scalar2=None,
                                                     op0=ALU.is_ge), R=[sc_, b_], W=[selb])
        mt = MT[k % 2]
        nkb = 4 * (k + 1)
        for j0 in range(0, nkb, 8):
            pt = PT[st["pt"] % 2]
            for jj in range(8):
                j = j0 + jj
                if j >= nkb:
                    break
                kb.op("pe", lambda: nc.tensor.transpose(pt[:, jj * 128:(jj + 1) * 128], selb[:, j * 128:(j + 1) * 128],
                                                        idb[:, :]), R=[selb, idb], W=[pt])
            wdt = min(8, nkb - j0) * 128
            if st["pt"] % 2 == 0:
                kb.op("act", lambda: nc.scalar.copy(out=mt[:, j0 * 128:j0 * 128 + wdt], in_=pt[:, :wdt]), R=[pt], W=[mt])
            else:
                kb.op("dve", lambda: nc.vector.tensor_copy(out=mt[:, j0 * 128:j0 * 128 + wdt], in_=pt[:, :wdt]),
                      R=[pt], W=[mt])
            st["pt"] += 1
        write_mask(k, mt, L)

    for k0 in range(0, NK, 2):
        ks = (k0, k0 + 1)
        for k in ks:
            scoring(k)
        for k in ks:
            setup(k)
        for n in range(NBIS):
            for k in ks:
                iteration(k, n)
        for k in ks:
            finish_block(k)


def run_B_dsa1(yT, ytf):
    nc = build_B_dsa1()
    steps = np.ascontiguousarray(np.broadcast_to((0.5 ** np.arange(1, NBIS + 1))[None, :], (128, NBIS))).astype(np.float32)
    maps = []
    for c in range(NCORES):
        b, m = c // 4, c % 4
        ts = slice(b * SEQ, (b + 1) * SEQ)
        tok = (np.arange(16)[:, None] * 512 + m * 128 + np.arange(128)[None, :]).reshape(-1) + b * SEQ
        qi = yT[1536:2048][:, tok].reshape(8, 64, 2048).transpose(1, 0, 2)
        wi = ytf[tok, 512:520].reshape(16, 128, 8).transpose(1, 0, 2)
        cm = np.where(np.arange(512)[None, :] <= (128 * m + np.arange(128))[:, None], 0.0, NEG).astype(np.float32)
        maps.append({"qiT": np.ascontiguousarray(qi), "kiT": np.ascontiguousarray(yT[2048:2112, ts]),
                     "wi": np.ascontiguousarray(wi), "cmask": cm, "identb": IDENT.astype(NPBF), "steps": steps})
    res = run_spmd(nc, maps)
    out = []
    for b in range(BATCH):
        M = np.zeros((NQB, 128, SEQ), NPBF)
        for m in range(4):
            M[m::4] = res[b * 4 + m]["M"]
        out.append(M)
    return out


def dsa_const():
    C = np.zeros((32, NDEL), np.float32)
    dl = np.arange(NDEL) - 127
    ok = dl >= 0
    C[rel_bucket_np(dl)[ok], np.arange(NDEL)[ok]] = 1.0
    return C


NPACK = NQB * (NQB + 1) // 2


def build_B_dsa2():
    kb = KB()
    nc = kb.nc
    q_d = kb.dram_in("q", [128, SEQ], BF16)
    k_d = kb.dram_in("k", [128, SEQ], BF16)
    v_d = kb.dram_in("v", [128, NQB, 128], BF16)
    rel_d = kb.dram_in("rel", [32, 2], F32)
    rf_d = kb.dram_in("relfar", [128, 2], F32)
    C_d = kb.dram_in("dsac", [32, NDEL], F32)
    M_d = kb.dram_in("Mp", [NPACK * 128 * 128], BF16)
    o_d = kb.dram_out("o", [SEQ, 128], BF16)
    E_d = kb.dram_tmp("Escr", [2, NDEL], F32)
    ax = AttnCtx(kb)
    rel = kb.sb([32, 2], F32, "rel")
    rf = kb.sb([128, 2], F32, "rf")
    Cs = kb.sb([32, NDEL], F32, "Cs")
    Es = kb.sb([2, NDEL], F32, "Es")
    kb.dma("sp", rel[:, :], rel_d[:, :], R=[rel_d], W=[rel])
    kb.dma("sp", rf[:, :], rf_d[:, :], R=[rf_d], W=[rf])
    kb.dma("sp", Cs[:, :], C_d[:, :], R=[C_d], W=[Cs])
    kb.op("act", lambda: nc.scalar.activation(out=rel[:, :], in_=rel[:, :], func=AF.Exp), R=[rel], W=[rel])
    for c in range((NDEL + 511) // 512):
        w = min(512, NDEL - c * 512)
        pp = ax.S[c % 3]
        kb.op("pe", lambda: nc.tensor.matmul(pp[:2, :w], lhsT=rel[:, :], rhs=Cs[:, c * 512:c * 512 + w],
                                             start=True, stop=True), R=[rel, Cs], W=[pp])
        kb.op("dve", lambda: nc.vector.tensor_copy(out=Es[:, c * 512:c * 512 + w], in_=pp[:2, :w]), R=[pp], W=[Es])
    kb.dma("sp", E_d[:, :], Es[:, :], R=[Es], W=[E_d])
    TT = [kb.sb([128, 17 * 128], F32, "TT") for _ in range(2)]
    for h in range(2):
        src = _AP(tensor=E_d.t.tensor, offset=h * NDEL, ap=[[1, 128], [1, 17 * 128]])
        kb.dma("sp", TT[h][:, :], src, R=[E_d], W=[TT[h]])
    q = kb.sb([128, SEQ], BF16, "q")
    k = kb.sb([128, SEQ], BF16, "k")
    for n, (s, d_) in enumerate(((q, q_d), (k, k_d))):
        for c in range(2):
            kb.dma("sp" if (n + c) % 2 == 0 else "pool", s[:, c * 4096:(c + 1) * 4096], d_[:, c * 4096:(c + 1) * 4096],
                   R=[d_], W=[s])
    V = load_vaug(kb, v_d, "V")
    Ms = [kb.sb([128, SEQ], BF16, "Ms") for _ in range(2)]
    ost = [kb.sb([128, 128], BF16, "ost") for _ in range(2)]
    for i in range(NQB):
        ms = Ms[i % 2]
        W_ = (i + 1) * 128
        off = (i * (i + 1) // 2) * 128 * 128
        src = _AP(tensor=M_d.t.tensor, offset=off, ap=[[W_, 128], [1, W_]])
        kb.dma("pool" if i % 2 else "sp", ms[:, :W_], src, R=[M_d], W=[ms])
        o = ost[i % 2]
        for h in range(2):
            def bias_of(j, h=h, i=i):
                return (rf, rf[:, h:h + 1]) if i - j > 16 else None

            def mask_of(j, h=h, i=i, ms=ms):
                sel = (ms, ms[:, j * 128:(j + 1) * 128])
                if i - j > 16:
                    return [sel]
                return [(TT[h], TT[h][:, (i - j) * 128:(i - j + 1) * 128]), sel]

            attn_qblock(ax, q, k, V, h, i, list(range(0, i + 1)), bias_of, mask_of, o, h * 64,
                        after=(None if h == 0 else
                               (lambda i=i, o=o: kb.dma("sp", o_d[i * 128:(i + 1) * 128, :], o[:, :], R=[o], W=[o_d]))))
    attn_flush(ax)
    return kb.finish()


def run_B_dsa2(yT, ytb, Msel, rel_table):
    nc = build_B_dsa2()
    C = dsa_const()
    packed = []
    for b in range(BATCH):
        M = Msel[b][:, ::-1, :]
        packed.append(np.concatenate([np.ascontiguousarray(M[i][:, :(i + 1) * 128]).reshape(-1) for i in range(NQB)]))
    maps = []
    for c in range(NCORES):
        b, m = c // 4, c % 4
        ts = slice(b * SEQ, (b + 1) * SEQ)
        maps.append({
            "q": np.ascontiguousarray(yT[512 + m * 128:512 + (m + 1) * 128, ts]),
            "k": np.ascontiguousarray(yT[1024 + m * 128:1024 + (m + 1) * 128, ts].reshape(128, NQB, 128)[:, :, ::-1]
                                      .reshape(128, SEQ)),
            "v": np.ascontiguousarray(to_pj(ytb[ts, 768 + m * 128:768 + (m + 1) * 128])[::-1]),
            "rel": np.ascontiguousarray(rel_table[:, 2 * m:2 * m + 2]),
            "relfar": np.ascontiguousarray(np.broadcast_to(rel_table[31:32, 2 * m:2 * m + 2], (128, 2))).astype(np.float32),
            "dsac": C, "Mp": packed[b],
        })
    res = run_spmd(nc, maps)
    o = np.zeros((BATCH * SEQ, 512), NPBF)
    for c in range(NCORES):
        b, m = c // 4, c % 4
        o[b * SEQ:(b + 1) * SEQ, m * 128:(m + 1) * 128] = res[c]["o"]
    return o


def kernel_unfused(x, ln_g, ln_b, rel_table, w_in_ab, w_gate_a, b_gate_a, g_norm_a, w_out_ab,
           w_in_cd, b_forget, w_out_cd, w1_dense, w3_dense, w2_dense,
           w_router, w1_moe, w3_moe, w2_moe):
    f32 = lambda a: np.ascontiguousarray(np.asarray(a, dtype=np.float32))
    x = f32(x)
    rel_table = f32(rel_table)
    big = [f32(w_in_ab), f32(w_in_cd), f32(w_out_ab), f32(w_out_cd), f32(w1_dense), f32(w3_dense), f32(w2_dense),
           f32(w1_moe), f32(w3_moe), f32(w2_moe)]
    (wi_ab, wi_cd, wo_ab, wo_cd, w1d, w3d, w2d, w1m, w3m, w2m) = cast_weights(big)
    del big
    xf = x.reshape(BATCH * SEQ, D)
    for layer in range(DEPTH):
        j = layer // 2
        lnp4 = np.stack([ln_g[layer, 0], ln_b[layer, 0], ln_g[layer, 1], ln_b[layer, 1]]).astype(np.float32)
        if layer % 2 == 0:
            yT, ytb, ytf = run_A(xf, wi_ab[j],
                                 [(0, 256), (256, 256), (1552, 512), (2064, 512), (3088, 512), (3600, 64)],
                                 [(256, 256), (512, 512), (2576, 512)],
                                 [(1024, 512), (3664, 8)],
                                 gate=(1536,), wg=f32(w_gate_a[j]), bg=f32(b_gate_a[j]))
            oa = run_B_gla(yT, ytb, ytf, f32(g_norm_a[j]))
            Msel = run_B_dsa1(yT, ytf)
            ob = run_B_dsa2(yT, ytb, Msel, rel_table)
            del Msel
            o = np.concatenate([oa, ob], axis=1)
            wf = ffn_chunk_layout(w1d[j], w3d[j], w2d[j])
            xf = run_C(o, xf, wo_ab[j], lnp4, wf, 1, 11, 2)
        else:
            yT, ytb, ytf = run_A(xf, wi_cd[j],
                                 [(0, 512), (512, 512), (1544, 512), (2056, 512)],
                                 [(1024, 512), (2568, 512)],
                                 [(1536, 8)])
            o = run_B_cd(yT, ytb, ytf, f32(b_forget[j]), rel_table)
            wf = np.concatenate([ffn_chunk_layout(w1m[j, e], w3m[j, e], w2m[j, e]) for e in range(8)], axis=0)
            xf = run_C(o, xf, wo_cd[j], lnp4, wf, 8, 14, 2, wr=f32(w_router[j]))
    return xf.reshape(BATCH, SEQ, D).astype(np.float32)


I32 = mybir.dt.int32

import os
SKIP = os.environ.get('FZ_SKIP', '')
GROUPS = [[0, 1, 2, 3], [4, 5, 6, 7]]
LK = [512 * (k + 1) for k in range(16)]
MOFF = [128 * sum(LK[:k]) for k in range(16)]
MTOT = 128 * sum(LK)
MPARTS = []
_g = 0
for _k in range(16):
    _r0 = MOFF[_k] // 512
    _n = 128 * LK[_k] // 512
    _halves = 1 if _k < 8 else 2
    for _h in range(_halves):
        _nh = _n // _halves
        MPARTS.append((_k, _h, _r0 + _h * _nh, _nh, _g))
        _g += 4 * _nh
MG = {(p[0], p[1]): p for p in MPARTS}


class KBF(KB):
    def __init__(self):
        super().__init__()
        self.ccsem = self.es.enter_context(self.nc.semaphore("ccsem"))
        self.cccnt = 0
        self.stack = [self.es]

    def sb(self, shape, dt, name="sb"):
        return Buf(self.stack[-1].enter_context(self.nc.sbuf_tensor(self._nm(name), list(shape), dt)))

    def ps(self, shape, dt=F32, name="ps"):
        b = Buf(self.stack[-1].enter_context(self.nc.psum_tensor(self._nm(name), list(shape), dt)))
        b.psum = True
        return b

    def barrier(self):
        evs = []
        for q in ("sp", "act", "pool"):
            for i in range(self.NDS):
                if self.dcnt[q][i] > 0:
                    evs.append((self.dsem[q][i], self.dcnt[q][i], "d%s%d" % (q, i)))
        for e in ("pe", "dve", "act", "pool", "sp"):
            if self.ecnt[e] > 0:
                evs.append((self.esem[e], self.ecnt[e], e))
        if self.cccnt > 0:
            evs.append((self.ccsem, self.cccnt, "cc"))
        for e in ("pe", "dve", "act", "pool", "sp"):
            for ev in evs:
                if ev[2] == e:
                    continue
                self._wait(e, ev)

    def scope(self):
        kb = self

        class _S:
            def __enter__(s):
                st = ExitStack()
                kb.stack.append(st)
                return st

            def __exit__(s, *a):
                kb.barrier()
                st = kb.stack.pop()
                st.close()
                return False

        return _S()

    def dram_tmp(self, name, shape, dt):
        return Buf(self.nc.dram_tensor(name, list(shape), dt).ap())

    def collective(self, kind, src, dst, src_ap=None, dst_ap=None, track_src=True):
        self._deps("pool", [src], [dst])
        sa = src.t if src_ap is None else src_ap
        da = dst.t if dst_ap is None else dst_ap
        ins = self.nc.gpsimd.collective_compute(kind, ALU.bypass, replica_groups=GROUPS,
                                                ins=[sa.opt()], outs=[da.opt()])
        self.cccnt += CC_INC
        ins.then_inc(self.ccsem, CC_INC)
        ev = (self.ccsem, self.cccnt, "cc")
        _kbf_mark(self, ev, [src] if track_src else [], [dst])
        return ev


CC_INC = 1


def load_w_cast(kb, dst, src_d, ncols):
    for kc in range(8):
        kb.dma("pool", dst[:, kc, :ncols], src_d[:, kc, :ncols], R=[src_d], Wd=[dst])


def load_xT_chunk(kb, x_, xT_all, c):
    r, t0 = c // 4, (c % 4) * 512
    for cf in range(4):
        row0 = (cf * 4 + r) * 256
        kb.dma("sp", x_[:, 2 * cf:2 * cf + 2, :],
               xT_all[row0:row0 + 256, t0:t0 + 512].rearrange("(k p) t -> p k t", p=128),
               R=[xT_all], Wd=[x_] if cf else (), W=[x_] if cf == 0 else ())


def emit_xT(kb, ax_ps, ident, xtile, tt, stg, xT_loc, xTs_loc):
    nc = kb.nc
    for kc in range(8):
        pt = ax_ps[kc // 4]
        kb.op("pe", lambda: nc.tensor.transpose(pt[:, (kc % 4) * 128:(kc % 4 + 1) * 128],
                                                xtile[:, kc * 128:(kc + 1) * 128], ident[:, :]),
              R=[xtile, ident], W=[pt])
    q = tt % 4
    for hf in range(2):
        src = ax_ps[hf][:, :].rearrange("p (k t) -> p k t", k=4)
        dst = stg[:, hf * 4:(hf + 1) * 4, q * 128:(q + 1) * 128]
        if hf == 0:
            kb.op("dve", lambda: nc.vector.tensor_copy(out=dst, in_=src), R=[ax_ps[hf]], W=[stg])
        else:
            kb.op("act", lambda: nc.scalar.copy(out=dst, in_=src), R=[ax_ps[hf]], W=[stg])
    if q == 3:
        g = tt // 4
        kb.dma("sp", xT_loc[:, g * 512:(g + 1) * 512].rearrange("(k p) t -> p k t", p=128), stg[:, :, :],
               R=[stg], Wd=[xT_loc])
        if xTs_loc is not None:
            for j in range(4):
                kb.dma("sp", xTs_loc[j * D:(j + 1) * D, g * 128:(g + 1) * 128].rearrange("(k p) t -> p k t", p=128),
                       stg[:, :, j * 128:(j + 1) * 128], R=[stg], Wd=[xTs_loc])


def toeplitz_load(kb, TT, E_d, h, q="act"):
    for s in range(128):
        kb.dma(q if s % 2 == 0 else "sp", TT[s:s + 1, :], E_d[h:h + 1, 127 - s:127 - s + 17 * 128], R=[E_d], Wd=[TT])


def build_E_table(kb, ax, rel_d, C_d, E_d):
    nc = kb.nc
    rel = kb.sb([32, 2], F32, "rel")
    Cs = kb.sb([32, NDEL], F32, "Cs")
    Es = kb.sb([2, NDEL], F32, "Es")
    kb.dma("sp", rel[:, :], rel_d[:, :], R=[rel_d], W=[rel])
    kb.dma("sp", Cs[:, :], C_d[:, :], R=[C_d], W=[Cs])
    kb.op("act", lambda: nc.scalar.activation(out=rel[:, :], in_=rel[:, :], func=AF.Exp), R=[rel], W=[rel])
    for c in range((NDEL + 511) // 512):
        w = min(512, NDEL - c * 512)
        pp = ax.S[c % 3]
        kb.op("pe", lambda: nc.tensor.matmul(pp[:2, :w], lhsT=rel[:, :], rhs=Cs[:, c * 512:c * 512 + w],
                                             start=True, stop=True), R=[rel, Cs], W=[pp])
        kb.op("dve", lambda: nc.vector.tensor_copy(out=Es[:, c * 512:c * 512 + w], in_=pp[:2, :w]), R=[pp], W=[Es])
    kb.dma("sp", E_d[:, :], Es[:, :], R=[Es], W=[E_d])


def oT_exchange(kb, oT_loc, oT_all, j):
    kb.collective("AllGather", oT_loc, oT_all, oT_loc[j * 1024:(j + 1) * 1024, :],
                  oT_all[j * 4096:(j + 1) * 4096, :], track_src=False)


class OTOut:
    def __init__(self, kb, identb, oT_loc, row0, name):
        self.kb, self.identb, self.oT_loc, self.row0 = kb, identb, oT_loc, row0
        self.pt = [kb.ps([128, 1024], BF16, "ptO" + name) for _ in range(1)]
        self.stg = [kb.sb([128, 512], BF16, "stgO" + name) for _ in range(2)]
        self.n = 0

    def put(self, i, ost):
        kb, nc = self.kb, self.kb.nc
        g = i // 4
        st = self.stg[g % 2]
        pt = self.pt[0]
        q = i % 4
        kb.op("pe", lambda: nc.tensor.transpose(pt[:, q * 128:(q + 1) * 128], ost[:, :], self.identb[:, :]),
              R=[ost, self.identb], W=[pt])
        if q == 3:
            kb.op("act", lambda: nc.scalar.copy(out=st[:, :], in_=pt[:, :512]), R=[pt], W=[st])
            rank, grp = g // 4, g % 4
            r0 = (rank * 4 + grp) * 256 + self.row0
            kb.dma("sp", self.oT_loc[r0:r0 + 128, :], st[:, :], R=[st], Wd=[self.oT_loc])


def phase_cd(kb, L, xT_all, oT_loc, oT_all, cst):
    nc = kb.nc
    with kb.scope():
        wcd_d = L["wcd"]
        NCOL = 770
        w = kb.sb([128, 8, NCOL], BF16, "wcd")
        load_w_cast(kb, w, wcd_d, NCOL)
        tri = kb.sb([128, 128], F32, "tri")
        ones = kb.sb([128, 128], F32, "ones")
        identb = kb.sb([128, 128], BF16, "identb")
        kb.dma("sp", tri[:, :], cst["tri"][:, :], R=[cst["tri"]], W=[tri])
        kb.dma("sp", identb[:, :], cst["identb"][:, :], R=[cst["identb"]], W=[identb])
        kb.op("dve", lambda: nc.vector.memset(ones[:, :], 1.0), W=[ones])
        ax = AttnCtx(kb, n_s=4, n_o=2, grouped=True, fox=True)
        E_d = L["Escr"]
        with kb.scope():
            build_E_table(kb, ax, L["rel2"], cst["dilc"], E_d)
        TT = [kb.sb([128, 17 * 128], F32, "TT") for _ in range(2)]
        for h in range(2):
            if "toep" in SKIP:
                kb.op("dve", lambda: nc.vector.memset(TT[h][:, :], 1.0), W=[TT[h]])
            else:
                toeplitz_load(kb, TT[h], E_d, h)
        qc = kb.sb([128, SEQ], BF16, "qc")
        kc_ = kb.sb([128, SEQ], BF16, "kc")
        qd = kb.sb([128, SEQ], BF16, "qd")
        kd = kb.sb([128, SEQ], BF16, "kd")
        Vc = kb.sb([128, NQB, 2, 65], BF16, "Vc")
        Vd = kb.sb([128, NQB, 2, 65], BF16, "Vd")
        fc = kb.sb([128, NQB, 2], F32, "fc")
        kb.op("pool", lambda: nc.gpsimd.memset(Vc[:, :, :, :], 1.0), W=[Vc])
        kb.op("pool", lambda: nc.gpsimd.memset(Vd[:, :, :, :], 1.0), W=[Vd])
        xc = [kb.sb([128, 8, 512], BF16, "xc") for _ in range(2)]
        n_ev = 0
        passes = [(True, True)] if "twopass" not in SKIP else [(True, False), (False, True)]
        NCH = int(os.environ.get("FZ_NCH", "16"))
        for c2 in range((NCH if "proj" not in SKIP else 0) * len(passes)):
            c = c2 % NCH
            do_fm, do_tm = passes[c2 // NCH]
            x_ = xc[c % 2]
            load_xT_chunk(kb, x_, xT_all, c)
            for bi, dst in enumerate((qc, kc_, qd, kd) if ("projfm" not in SKIP and do_fm) else ()):
                pp = ax.S[n_ev % 4]
                for k8 in range(8):
                    kb.op("pe", lambda: nc.tensor.matmul(pp[:, :], lhsT=w[:, k8, bi * 128:(bi + 1) * 128],
                                                         rhs=x_[:, k8, :], start=(k8 == 0), stop=(k8 == 7)),
                          R=[w, x_], W=[pp])
                d_ = dst[:, c * 512:(c + 1) * 512]
                if n_ev % 2 == 0 or "fmdve" in SKIP:
                    kb.op("dve", lambda: nc.vector.tensor_copy(out=d_, in_=pp[:, :]), R=[pp], W=[dst])
                else:
                    kb.op("act", lambda: nc.scalar.copy(out=d_, in_=pp[:, :]), R=[pp], W=[dst])
                n_ev += 1
            for t4 in range(4 if ("projtm" not in SKIP and do_tm) else 0):
                j = c * 4 + t4
                pp = ax.S[n_ev % 4]
                n_ev += 1
                for k8 in range(8):
                    kb.op("pe", lambda: nc.tensor.matmul(pp[:, :258], lhsT=x_[:, k8, t4 * 128:(t4 + 1) * 128],
                                                         rhs=w[:, k8, 512:770], start=(k8 == 0), stop=(k8 == 7)),
                          R=[w, x_], W=[pp])
                kb.op("dve", lambda: nc.vector.tensor_copy(out=Vc[:, j, :, 0:64],
                                                           in_=pp[:, 0:128].rearrange("p (h d) -> p h d", h=2)),
                      R=[pp], W=[Vc])
                if "vddve" in SKIP:
                    kb.op("dve", lambda: nc.vector.tensor_copy(out=Vd[:, j, :, 0:64],
                                                               in_=pp[:, 128:256].rearrange("p (h d) -> p h d", h=2)),
                          R=[pp], W=[Vd])
                else:
                    kb.op("act", lambda: nc.scalar.copy(out=Vd[:, j, :, 0:64],
                                                        in_=pp[:, 128:256].rearrange("p (h d) -> p h d", h=2)),
                          R=[pp], W=[Vd])
                kb.op("dve", lambda: nc.vector.tensor_copy(out=fc[:, j, :], in_=pp[:, 256:258]), R=[pp], W=[fc])
        bf = kb.sb([128, 2], F32, "bf")
        lf = kb.sb([128, 2, NQB], F32, "lf")
        kb.dma("sp", bf[:, :], L["bf"][:, :], R=[L["bf"]], W=[bf])
        for h in range(2):
            kb.op("dve", lambda: nc.vector.tensor_scalar(out=lf[:, h, :], in0=fc[:, :, h], scalar1=bf[:, h:h + 1],
                                                         scalar2=None, op0=ALU.add), R=[fc, bf], W=[lf])
        kb.op("act", lambda: nc.scalar.activation(out=lf[:, :, :], in_=lf[:, :, :], func=AF.Exp, scale=-1.0),
              R=[lf], W=[lf])
        kb.op("act", lambda: nc.scalar.activation(out=lf[:, :, :], in_=lf[:, :, :], func=AF.Ln, bias=1.0),
              R=[lf], W=[lf])
        kb.op("dve", lambda: nc.vector.tensor_scalar(out=lf[:, :, :], in0=lf[:, :, :], scalar1=-1.0, scalar2=None,
                                                     op0=ALU.mult), R=[lf], W=[lf])
        lf2 = lf[:, :, :].rearrange("p h j -> p (h j)")
        p1, p2 = ax.O[0], ax.O[1]
        kb.op("pe", lambda: nc.tensor.matmul(p1[:, :128], lhsT=tri[:, :], rhs=lf2, start=True, stop=True),
              R=[tri, lf], W=[p1])
        kb.op("pe", lambda: nc.tensor.matmul(p2[:, :128], lhsT=ones[:, :], rhs=lf2, start=True, stop=True),
              R=[ones, lf], W=[p2])
        tot = kb.sb([128, 2, NQB], F32, "tot")
        carry = kb.sb([128, 2, NQB], F32, "carry")
        negF = kb.sb([128, 2, NQB], F32, "negF")
        kb.op("dve", lambda: nc.vector.tensor_copy(out=tot[:, :, :].rearrange("p h j -> p (h j)"), in_=p2[:, :128]),
              R=[p2], W=[tot])
        for h in range(2):
            kb.op("dve", lambda: nc.vector.tensor_tensor_scan(out=carry[:, h, :], data0=ones[:, :NQB],
                                                              data1=tot[:, h, :], initial=0.0, op0=ALU.mult,
                                                              op1=ALU.add), R=[ones, tot], W=[carry])
        kb.op("dve", lambda: nc.vector.tensor_tensor(out=carry[:, :, :], in0=carry[:, :, :], in1=tot[:, :, :],
                                                     op=ALU.subtract), R=[carry, tot], W=[carry])
        kb.op("dve", lambda: nc.vector.tensor_tensor(out=negF[:, :, :].rearrange("p h j -> p (h j)"), in0=p1[:, :128],
                                                     in1=carry[:, :, :].rearrange("p h j -> p (h j)"), op=ALU.add),
              R=[p1, carry], W=[negF])
        kb.op("dve", lambda: nc.vector.tensor_scalar(out=negF[:, :, :], in0=negF[:, :, :], scalar1=-1.0, scalar2=None,
                                                     op0=ALU.mult), R=[negF], W=[negF])
        outc = OTOut(kb, identb, oT_loc, 0, "c")
        outd = OTOut(kb, identb, oT_loc, 128, "d")
        ostd = [kb.sb([128, 128], BF16, "ostd") for _ in range(2)]
        ostc = [kb.sb([128, 128], BF16, "ostc") for _ in range(2)]
        Bi = [kb.sb([128, NQB], F32, "Bi") for _ in range(3)]
        tri2 = kb.sb([128, 256], F32, "tri2")
        kb.op("dve", lambda: nc.vector.memset(tri2[:, :], 1.0), W=[tri2])
        kb.op("dve", lambda: nc.vector.tensor_copy(out=tri2[:, 0:128], in_=tri[:, :]), R=[tri], W=[tri2])
        nb = 0
        for p in range(NQB // 2 if "attn" not in SKIP else 0):
            for i in (2 * p, 2 * p + 1):
                od = ostd[i % 2]
                for h in range(2):
                    jdesc = list(range(i, max(0, i - 16) - 1, -1))
                    groups = []
                    for a in range(0, len(jdesc), 4):
                        js = jdesc[a:a + 4]
                        d0 = i - js[0]
                        groups.append({"js": js, "dve_mask": (TT[h], TT[h][:, d0 * 128:(d0 + len(js)) * 128])})
                    attn_qgroup(ax, qd, kd, Vd, h, i, groups, od, h * 64,
                                after=(None if h == 0 else (lambda i=i, od=od: outd.put(i, od))))
            i0_ = 2 * p
            for h in range(2):
                B = Bi[nb % 3]
                nb += 1
                kb.op("dve", lambda: nc.vector.tensor_scalar(out=B[:, :i0_ + 2], in0=negF[:, h, :i0_ + 2],
                                                             scalar1=carry[:, h, i0_:i0_ + 1], scalar2=None,
                                                             op0=ALU.add), R=[negF, carry], W=[B])

                def fin(i0_=i0_):
                    outc.put(i0_, ostc[0])
                    outc.put(i0_ + 1, ostc[1])
                    if (i0_ + 1) % 16 == 15:
                        oT_exchange(kb, oT_loc, oT_all, (i0_ + 1) // 16)

                attn_fox_pair(ax, qc, kc_, Vc, h, i0_, B, tri2, (ostc[0], ostc[1]), h * 64,
                              after=(None if h == 0 else fin))
        attn_flush(ax)


def phase_gla(kb, L, xT_all, oT_loc, cst):
    nc = kb.nc
    with kb.scope():
        tri = kb.sb([128, 128], F32, "tri")
        identb = kb.sb([128, 128], BF16, "identb")
        gn = kb.sb([128, 128], F32, "gn")
        eps = kb.sb([128, 1], F32, "eps")
        kb.dma("sp", tri[:, :], cst["tri"][:, :], R=[cst["tri"]], W=[tri])
        kb.dma("sp", identb[:, :], cst["identb"][:, :], R=[cst["identb"]], W=[identb])
        kb.dma("sp", gn[:, :], L["gn"][:, :], R=[L["gn"]], W=[gn])
        kb.op("dve", lambda: nc.vector.memset(eps[:, :], LN_EPS), W=[eps])
        qT = kb.sb([64, SEQ], BF16, "qT")
        kT = kb.sb([64, SEQ], BF16, "kT")
        ktm = kb.sb([128, NQB, 64], BF16, "ktm")
        v = kb.sb([128, NQB, 128], BF16, "v")
        r = kb.sb([128, NQB, 128], F32, "r")
        g = kb.sb([128, NQB, 64], F32, "g")
        PG = [kb.ps([128, 512], F32, "PG")]
        PGT = [kb.ps([128, 512], F32, "PGT")]
        PA = [kb.ps([128, 512], F32, "PA") for _ in range(2)]
        PO = kb.ps([128, 512], F32, "PO")
        PU = kb.ps([128, 512], F32, "PU")
        out = OTOut(kb, identb, oT_loc, 0, "g")
        with kb.scope():
            NCOL = 464
            w = kb.sb([128, 8, NCOL], BF16, "wgl")
            load_w_cast(kb, w, L["wgl"], NCOL)
            wg = kb.sb([16, 64], F32, "wg")
            bg = kb.sb([128, 64], F32, "bg")
            kb.dma("sp", wg[:, :], L["wg"][:, :], R=[L["wg"]], W=[wg])
            kb.dma("sp", bg[:, :], L["bg"][:, :], R=[L["bg"]], W=[bg])
            xc = [kb.sb([128, 8, 512], BF16, "xc") for _ in range(2)]
            gaT = [kb.sb([16, 512], F32, "gaT") for _ in range(2)]
            zt = [kb.sb([128, 64], F32, "zt") for _ in range(2)]
            pr = [PA[0], PA[1], PO]
            n_ev = 0
            for c in range(16):
                x_ = xc[c % 2]
                ga_ = gaT[c % 2]
                load_xT_chunk(kb, x_, xT_all, c)
                for (c0, ncol, dst) in ((0, 64, qT), (64, 64, kT), (128, 16, ga_)):
                    pp = pr[n_ev % 3]
                    for k8 in range(8):
                        kb.op("pe", lambda: nc.tensor.matmul(pp[:ncol, :], lhsT=w[:, k8, c0:c0 + ncol], rhs=x_[:, k8, :],
                                                             start=(k8 == 0), stop=(k8 == 7)), R=[w, x_], W=[pp])
                    d_ = dst[:, c * 512:(c + 1) * 512] if dst is not ga_ else ga_[:, :]
                    if n_ev % 2 == 0:
                        kb.op("dve", lambda: nc.vector.tensor_copy(out=d_, in_=pp[:ncol, :]), R=[pp], W=[dst])
                    else:
                        kb.op("act", lambda: nc.scalar.copy(out=d_, in_=pp[:ncol, :]), R=[pp], W=[dst])
                    n_ev += 1
                for t4 in range(4):
                    j = c * 4 + t4
                    pp = pr[n_ev % 3]
                    n_ev += 1
                    for k8 in range(8):
                        kb.op("pe", lambda: nc.tensor.matmul(pp[:, :320], lhsT=x_[:, k8, t4 * 128:(t4 + 1) * 128],
                                                             rhs=w[:, k8, 144:464], start=(k8 == 0), stop=(k8 == 7)),
                              R=[w, x_], W=[pp])
                    kb.op("dve", lambda: nc.vector.tensor_copy(out=ktm[:, j, :], in_=pp[:, 0:64]), R=[pp], W=[ktm])
                    kb.op("dve", lambda: nc.vector.tensor_copy(out=v[:, j, :], in_=pp[:, 64:192]), R=[pp], W=[v])
                    kb.op("act", lambda: nc.scalar.activation(out=r[:, j, :], in_=pp[:, 192:320], func=AF.Silu),
                          R=[pp], W=[r])
                    pq = pr[n_ev % 3]
                    n_ev += 1
                    z = zt[j % 2]
                    kb.op("pe", lambda: nc.tensor.matmul(pq[:, :64], lhsT=ga_[:, t4 * 128:(t4 + 1) * 128], rhs=wg[:, :],
                                                         start=True, stop=True), R=[ga_, wg], W=[pq])
                    kb.op("dve", lambda: nc.vector.tensor_tensor(out=z[:, :], in0=pq[:, :64], in1=bg[:, :], op=ALU.add),
                          R=[pq, bg], W=[z])
                    kb.op("act", lambda: nc.scalar.activation(out=z[:, :], in_=z[:, :], func=AF.Exp, scale=-1.0),
                          R=[z], W=[z])
                    kb.op("act", lambda: nc.scalar.activation(out=z[:, :], in_=z[:, :], func=AF.Ln, bias=1.0),
                          R=[z], W=[z])
                    kb.op("dve", lambda: nc.vector.tensor_scalar(out=g[:, j, :], in0=z[:, :], scalar1=-1.0 / 16.0,
                                                                 scalar2=None, op0=ALU.mult), R=[z], W=[g])
        eGT = [kb.sb([64, 128], F32, "eGT") for _ in range(2)]
        enGT = [kb.sb([64, 128], F32, "enGT") for _ in range(2)]
        enG = [kb.sb([128, 64], F32, "enG") for _ in range(2)]
        qgT = [kb.sb([64, 128], BF16, "qgT") for _ in range(2)]
        kgT = [kb.sb([64, 128], BF16, "kgT") for _ in range(2)]
        kg = [kb.sb([128, 64], BF16, "kg") for _ in range(2)]
        Am = [kb.sb([128, 128], BF16, "Am") for _ in range(2)]
        S32 = kb.sb([64, 128], F32, "S32")
        Sbf = kb.sb([64, 128], BF16, "Sbf")
        st6 = [kb.sb([128, 6], F32, "st6") for _ in range(2)]
        mv = [kb.sb([128, 4], F32, "mv") for _ in range(2)]
        of = [kb.sb([128, 128], F32, "of") for _ in range(2)]
        ost = [kb.sb([128, 128], BF16, "ost") for _ in range(2)]
        for c in range(NQB):
            p = c % 2
            cs = slice(c * 128, (c + 1) * 128)
            kb.op("pe", lambda: nc.tensor.matmul(PG[0][:, :64], lhsT=tri[:, :], rhs=g[:, c, :], start=True, stop=True),
                  R=[tri, g], W=[PG[0]])
            kb.op("pe", lambda: nc.tensor.matmul(PGT[0][:64, :128], lhsT=g[:, c, :], rhs=tri[:, :], start=True, stop=True),
                  R=[tri, g], W=[PGT[0]])
            kb.op("act", lambda: nc.scalar.activation(out=eGT[p][:, :], in_=PGT[0][:64, :128], func=AF.Exp),
                  R=[PGT[0]], W=[eGT[p]])
            kb.op("act", lambda: nc.scalar.activation(out=enGT[p][:, :], in_=PGT[0][:64, :128], func=AF.Exp, scale=-1.0),
                  R=[PGT[0]], W=[enGT[p]])
            kb.op("act", lambda: nc.scalar.activation(out=enG[p][:, :], in_=PG[0][:, :64], func=AF.Exp, scale=-1.0),
                  R=[PG[0]], W=[enG[p]])
            kb.op("dve", lambda: nc.vector.scalar_tensor_tensor(out=qgT[p][:, :], in0=qT[:, cs], scalar=0.125,
                                                                in1=eGT[p][:, :], op0=ALU.mult, op1=ALU.mult),
                  R=[qT, eGT[p]], W=[qgT[p]])
            kb.op("dve", lambda: nc.vector.tensor_tensor(out=kgT[p][:, :], in0=kT[:, cs], in1=enGT[p][:, :], op=ALU.mult),
                  R=[kT, enGT[p]], W=[kgT[p]])
            kb.op("dve", lambda: nc.vector.tensor_tensor(out=kg[p][:, :], in0=ktm[:, c, :], in1=enG[p][:, :], op=ALU.mult),
                  R=[ktm, enG[p]], W=[kg[p]])
            kb.op("pe", lambda: nc.tensor.matmul(PA[p][:, :128], lhsT=kgT[p][:, :], rhs=qgT[p][:, :], start=True, stop=True),
                  R=[kgT[p], qgT[p]], W=[PA[p]])
            kb.op("dve", lambda: nc.vector.tensor_tensor(out=Am[p][:, :], in0=PA[p][:, :128], in1=tri[:, :], op=ALU.mult),
                  R=[PA[p], tri], W=[Am[p]])
            kb.op("pe", lambda: nc.tensor.matmul(PO[:, :128], lhsT=Am[p][:, :], rhs=v[:, c, :], start=True, stop=(c == 0)),
                  R=[Am[p], v], W=[PO])
            if c > 0:
                kb.op("pe", lambda: nc.tensor.matmul(PO[:, :128], lhsT=qgT[p][:, :], rhs=Sbf[:, :], start=False, stop=True),
                      R=[qgT[p], Sbf], W=[PO])
            if c < NQB - 1:
                kb.op("pe", lambda: nc.tensor.matmul(PU[:64, :128], lhsT=kg[p][:, :], rhs=v[:, c, :], start=True, stop=True),
                      R=[kg[p], v], W=[PU])
                eGl = eGT[p][:, 127:128]
                if c == 0:
                    kb.op("dve", lambda: nc.vector.tensor_scalar(out=S32[:, :], in0=PU[:64, :128], scalar1=eGl,
                                                                 scalar2=None, op0=ALU.mult), R=[PU, eGT[p]], W=[S32])
                else:
                    kb.op("dve", lambda: nc.vector.tensor_scalar(out=S32[:, :], in0=S32[:, :], scalar1=eGl, scalar2=None,
                                                                 op0=ALU.mult), R=[S32, eGT[p]], W=[S32])
                    kb.op("dve", lambda: nc.vector.scalar_tensor_tensor(out=S32[:, :], in0=PU[:64, :128], scalar=eGl,
                                                                        in1=S32[:, :], op0=ALU.mult, op1=ALU.add),
                          R=[PU, eGT[p], S32], W=[S32])
                kb.op("dve", lambda: nc.vector.tensor_copy(out=Sbf[:, :], in_=S32[:, :]), R=[S32], W=[Sbf])
            kb.op("dve", lambda: nc.vector.bn_stats(out=st6[p][:, :], in_=PO[:, :128]), R=[PO], W=[st6[p]])
            kb.op("dve", lambda: nc.vector.bn_aggr(out=mv[p][:, 0:2], in_=st6[p][:, :]), R=[st6[p]], W=[mv[p]])
            kb.op("dve", lambda: nc.vector.scalar_tensor_tensor(out=mv[p][:, 2:3], in0=mv[p][:, 0:1], scalar=mv[p][:, 0:1],
                                                                in1=mv[p][:, 1:2], op0=ALU.mult, op1=ALU.add),
                  R=[mv[p]], W=[mv[p]])
            kb.op("act", lambda: nc.scalar.activation(out=mv[p][:, 3:4], in_=mv[p][:, 2:3], func=AF.Ln, bias=eps[:, 0:1]),
                  R=[mv[p], eps], W=[mv[p]])
            kb.op("act", lambda: nc.scalar.activation(out=mv[p][:, 3:4], in_=mv[p][:, 3:4], func=AF.Exp, scale=-0.5),
                  R=[mv[p]], W=[mv[p]])
            kb.op("dve", lambda: nc.vector.scalar_tensor_tensor(out=of[p][:, :], in0=PO[:, :128], scalar=mv[p][:, 3:4],
                                                                in1=gn[:, :], op0=ALU.mult, op1=ALU.mult),
                  R=[PO, mv[p], gn], W=[of[p]])
            kb.op("pool", lambda: nc.gpsimd.tensor_tensor(out=ost[p][:, :], in0=of[p][:, :], in1=r[:, c, :], op=ALU.mult),
                  R=[of[p], r], W=[ost[p]])
            out.put(c, ost[p])


def _kbf_deps(self, e, R, W, Wd=()):
    evs = []
    for b in R:
        evs.extend(b.wl())
        if getattr(b, "psum", False):
            evs.extend(x for x in b.r if x[2] != e)
    for b in W:
        evs.extend(b.wl())
        evs.extend(b.r)
    for b in Wd:
        evs.extend(b.r)
        evs.extend(getattr(b, "w_excl", []))
    best = {}
    for ev in evs:
        k = ev[2]
        if e == "pe" and k == "pe":
            continue
        if k not in best or best[k][1] < ev[1]:
            best[k] = ev
    for ev in best.values():
        self._wait(e, ev)


def _buf_wl(self):
    if self.w is None:
        return []
    return self.w if isinstance(self.w, list) else [self.w]


Buf.wl = _buf_wl


def _kbf_mark(self, ev, R, W, Wd=()):
    KB._mark(self, ev, R, W)
    for b in W:
        b.w = [ev]
        b.w_excl = [ev]
    for b in Wd:
        cur = b.wl()
        cur = [x for x in cur if x[2] != ev[2]] + [ev]
        b.w = cur
        b.r = []


def _kbf_dma(self, q, out, in_, R=(), W=(), Wd=(), **kw):
    _kbf_deps(self, q, R, W, Wd)
    i = self.dnext[q]
    self.dnext[q] = (i + 1) % self.NDS
    key = "d%s%d" % (q, i)
    if self.dcnt[q][i] > 0:
        self._wait(q, (self.dsem[q][i], self.dcnt[q][i], key))
    self.dcnt[q][i] += 16
    self.eng[q].dma_start(out=out, in_=in_, **kw).then_inc(self.dsem[q][i], 16)
    ev = (self.dsem[q][i], self.dcnt[q][i], key)
    _kbf_mark(self, ev, R, W, Wd)
    return ev


def _kbf_op(self, e, fn, R=(), W=()):
    _kbf_deps(self, e, R, W)
    ins = fn()
    self.ecnt[e] += 1
    ins.then_inc(self.esem[e], 1)
    ev = (self.esem[e], self.ecnt[e], e)
    _kbf_mark(self, ev, R, W)
    return ev


def _kbf_idma(self, out, in_, idx_ap, R=(), W=(), Wd=()):
    q = "pool"
    _kbf_deps(self, q, R, W, Wd)
    i = self.dnext[q]
    self.dnext[q] = (i + 1) % self.NDS
    key = "d%s%d" % (q, i)
    if self.dcnt[q][i] > 0:
        self._wait(q, (self.dsem[q][i], self.dcnt[q][i], key))
    self.dcnt[q][i] += 16
    self.nc.gpsimd.indirect_dma_start(out=out, out_offset=None, in_=in_,
                                      in_offset=bass.IndirectOffsetOnAxis(ap=idx_ap, axis=0)
                                      ).then_inc(self.dsem[q][i], 16)
    ev = (self.dsem[q][i], self.dcnt[q][i], key)
    _kbf_mark(self, ev, R, W, Wd)
    return ev


KBF.idma = _kbf_idma
KBF._deps = lambda self, e, R, W: _kbf_deps(self, e, R, W)
KBF.dma = _kbf_dma
KBF.op = _kbf_op


def phase_dsa1(kb, L, xT_all, xTs_all, M_loc, M_all, cst, act_split=True):
    nc = kb.nc
    NK = 16
    with kb.scope():
        qiT = kb.sb([64, 8, NK * 128], BF16, "qiT")
        kiT = kb.sb([64, SEQ], BF16, "kiT")
        wi = kb.sb([128, NK, 8], F32, "wi")
        absw = kb.sb([128, NK, 8], F32, "absw")
        sgn = kb.sb([128, NK, 8], F32, "sgn")
        cm = kb.sb([128, 512], F32, "cm")
        idb = kb.sb([128, 128], BF16, "idb")
        stp = kb.sb([128, NBIS], F32, "stp")
        kb.dma("sp", cm[:, :], cst["cmask"][:, :], R=[cst["cmask"]], W=[cm])
        kb.dma("sp", idb[:, :], cst["identb"][:, :], R=[cst["identb"]], W=[idb])
        kb.dma("sp", stp[:, :], cst["steps"][:, :], R=[cst["steps"]], W=[stp])
        idxs = kb.sb([128, 32], I32, "idxs")
        kb.dma("sp", idxs[:, :], cst["idx_s"][:, :], R=[cst["idx_s"]], W=[idxs])
        PS = [kb.ps([128, 512], F32, "PS") for _ in range(3)]
        PT = [kb.ps([128, 1024], BF16, "PT") for _ in range(2)]
        with kb.scope():
            NCOL = 584
            w = kb.sb([128, 8, NCOL], BF16, "wd1")
            load_w_cast(kb, w, L["wd1"], NCOL)
            xc = [kb.sb([128, 8, 512], BF16, "xc") for _ in range(2)]
            n_ev = 0
            for c in range(16):
                x_ = xc[c % 2]
                load_xT_chunk(kb, x_, xT_all, c)
                pp = PS[n_ev % 3]
                n_ev += 1
                for k8 in range(8):
                    kb.op("pe", lambda: nc.tensor.matmul(pp[:64, :], lhsT=w[:, k8, 512:576], rhs=x_[:, k8, :],
                                                         start=(k8 == 0), stop=(k8 == 7)), R=[w, x_], W=[pp])
                kb.op("dve", lambda: nc.vector.tensor_copy(out=kiT[:, c * 512:(c + 1) * 512], in_=pp[:64, :]),
                      R=[pp], W=[kiT])
            for r in range(4):
                x_ = xc[r % 2]
                for k8 in range(8):
                    kb.idma(x_[:, k8, :], xTs_all[:, :], idxs[:, r * 8 + k8:r * 8 + k8 + 1], R=[xTs_all, idxs],
                            Wd=[x_] if k8 else (), W=[x_] if k8 == 0 else ())
                for hi in range(8):
                    pp = PS[n_ev % 3]
                    n_ev += 1
                    for k8 in range(8):
                        kb.op("pe", lambda: nc.tensor.matmul(pp[:64, :], lhsT=w[:, k8, hi * 64:(hi + 1) * 64],
                                                             rhs=x_[:, k8, :], start=(k8 == 0), stop=(k8 == 7)),
                              R=[w, x_], W=[pp])
                    if hi % 2 == 0:
                        kb.op("dve", lambda: nc.vector.tensor_copy(out=qiT[:, hi, r * 512:(r + 1) * 512], in_=pp[:64, :]),
                              R=[pp], W=[qiT])
                    else:
                        kb.op("act", lambda: nc.scalar.copy(out=qiT[:, hi, r * 512:(r + 1) * 512], in_=pp[:64, :]),
                              R=[pp], W=[qiT])
                for t4 in range(4):
                    pp = PS[n_ev % 3]
                    n_ev += 1
                    for k8 in range(8):
                        kb.op("pe", lambda: nc.tensor.matmul(pp[:, :8], lhsT=x_[:, k8, t4 * 128:(t4 + 1) * 128],
                                                             rhs=w[:, k8, 576:584], start=(k8 == 0), stop=(k8 == 7)),
                              R=[w, x_], W=[pp])
                    kb.op("dve", lambda: nc.vector.tensor_copy(out=wi[:, r * 4 + t4, :], in_=pp[:, :8]), R=[pp], W=[wi])
        kb.op("act", lambda: nc.scalar.activation(out=absw[:, :, :], in_=wi[:, :, :], func=AF.Abs), R=[wi], W=[absw])
        kb.op("act", lambda: nc.scalar.activation(out=sgn[:, :, :], in_=wi[:, :, :], func=AF.Sign), R=[wi], W=[sgn])
        def write_mask(k, mt, Lk):
            dst = _AP(tensor=M_loc.t.tensor, offset=MOFF[k], ap=[[Lk, 128], [1, Lk]])
            kb.dma("sp", dst, mt[:, :Lk], R=[mt], Wd=[M_loc])
            for (k2, hf, lrow, nrow, grow) in MPARTS:
                if k2 == k:
                    kb.collective("AllGather", M_loc, M_all, M_loc[lrow:lrow + nrow, :],
                                  M_all[grow:grow + 4 * nrow, :], track_src=False)

        dsa1_body(kb, qiT, kiT, absw, sgn, cm, idb, stp, PS, PT, write_mask, act_split)


def phase_dsa2(kb, L, xT_all, M_all, oT_loc, oT_all, cst):
    nc = kb.nc
    with kb.scope():
        identb = kb.sb([128, 128], BF16, "identb")
        kb.dma("sp", identb[:, :], cst["identb"][:, :], R=[cst["identb"]], W=[identb])
        rf = kb.sb([128, 2], F32, "rf")
        kb.dma("sp", rf[:, :], L["relfar"][:, :], R=[L["relfar"]], W=[rf])
        ax = AttnCtx(kb, n_s=4, n_o=2, grouped=True)
        E_d = L["Escr"]
        with kb.scope():
            build_E_table(kb, ax, L["rel2"], cst["dsac"], E_d)
        TT = [kb.sb([128, 17 * 128], F32, "TT") for _ in range(2)]
        for h in range(2):
            toeplitz_load(kb, TT[h], E_d, h)
        q = kb.sb([128, SEQ], BF16, "q")
        k = kb.sb([128, SEQ], BF16, "k")
        V = kb.sb([128, NQB, 2, 65], BF16, "V")
        kb.op("pool", lambda: nc.gpsimd.memset(V[:, :, :, :], 1.0), W=[V])
        with kb.scope():
            NCOL = 384
            w = kb.sb([128, 8, NCOL], BF16, "wd2")
            load_w_cast(kb, w, L["wd2"], NCOL)
            xc = [kb.sb([128, 8, 512], BF16, "xc") for _ in range(2)]
            n_ev = 0
            for c in range(16):
                x_ = xc[c % 2]
                load_xT_chunk(kb, x_, xT_all, c)
                for bi, dst in enumerate((q, k)):
                    pp = ax.S[n_ev % 3]
                    for k8 in range(8):
                        kb.op("pe", lambda: nc.tensor.matmul(pp[:, :], lhsT=w[:, k8, bi * 128:(bi + 1) * 128],
                                                             rhs=x_[:, k8, :], start=(k8 == 0), stop=(k8 == 7)),
                              R=[w, x_], W=[pp])
                    d_ = dst[:, c * 512:(c + 1) * 512]
                    if n_ev % 2 == 0:
                        kb.op("dve", lambda: nc.vector.tensor_copy(out=d_, in_=pp[:, :]), R=[pp], W=[dst])
                    else:
                        kb.op("act", lambda: nc.scalar.copy(out=d_, in_=pp[:, :]), R=[pp], W=[dst])
                    n_ev += 1
                for t4 in range(4):
                    j = c * 4 + t4
                    pp = ax.S[n_ev % 3]
                    n_ev += 1
                    for k8 in range(8):
                        kb.op("pe", lambda: nc.tensor.matmul(pp[:, :128], lhsT=x_[:, k8, t4 * 128:(t4 + 1) * 128],
                                                             rhs=w[:, k8, 256:384], start=(k8 == 0), stop=(k8 == 7)),
                              R=[w, x_], W=[pp])
                    kb.op("dve" if t4 % 2 else "act",
                          (lambda: nc.vector.tensor_copy(out=V[:, j, :, 0:64],
                                                         in_=pp[:, 0:128].rearrange("p (h d) -> p h d", h=2)))
                          if t4 % 2 else
                          (lambda: nc.scalar.copy(out=V[:, j, :, 0:64],
                                                  in_=pp[:, 0:128].rearrange("p (h d) -> p h d", h=2))),
                          R=[pp], W=[V])
        Ms = [kb.sb([128, SEQ], BF16, "Ms") for _ in range(2)]
        ost = [kb.sb([128, 128], BF16, "ost") for _ in range(2)]
        out = OTOut(kb, identb, oT_loc, 128, "b")
        for i in range(NQB):
            ms = Ms[i % 2]
            W_ = (i + 1) * 128
            r_, k_ = i % 4, i // 4
            nh = 1 if k_ < 8 else 2
            for hf in range(nh):
                (_, _, lrow, nrow, grow) = MG[(k_, hf)]
                ns = 128 // nh
                src = _AP(tensor=M_all.t.tensor, offset=(grow + r_ * nrow) * 512, ap=[[LK[k_], ns], [1, W_]])
                kb.dma("sp", ms[hf * ns:(hf + 1) * ns, :W_], src, R=[M_all],
                       Wd=[ms] if hf else (), W=[ms] if hf == 0 else ())
            o = ost[i % 2]
            for h in range(2):
                groups = []
                jn0 = max(0, i - 16)
                for a in range(0, jn0, 4):
                    js = list(range(a, min(a + 4, jn0)))
                    groups.append({"js": js, "bias": (rf, rf[:, h:h + 1]),
                                   "dve_mask": (ms, ms[:, js[0] * 128:(js[-1] + 1) * 128])})
                for a in range(jn0, i + 1, 4):
                    js = list(range(a, min(a + 4, i + 1)))
                    groups.append({"js": js,
                                   "pool_masks": [(TT[h], TT[h][:, (i - j) * 128:(i - j + 1) * 128]) for j in js],
                                   "dve_mask": (ms, ms[:, js[0] * 128:(js[-1] + 1) * 128])})
                def fin2(i=i, o=o):
                    out.put(i, o)
                    if i % 16 == 15:
                        oT_exchange(kb, oT_loc, oT_all, i // 16)

                attn_qgroup(ax, q, k, V, h, i, groups, o, h * 64, after=(None if h == 0 else fin2))
        attn_flush(ax)


def phase_c(kb, L, moe, oT_all, x_src, x_dst, xT_loc, xTs_loc, cst, last):
    nc = kb.nc
    n_exp, nfu, n_units = (8, 14, 2) if moe else (1, 11, 2)
    TG = 512
    NG = TPC // TG
    with kb.scope():
        wf_d = L["wf"]
        ident = kb.sb([128, 128], F32, "ident")
        wo = kb.sb([128, 8, D], BF16, "wo")
        lnp = kb.sb([128, 4, D], F32, "lnp")
        kb.eps_col = kb.sb([128, 1], F32, "eps")
        kb.op("dve", lambda: nc.vector.memset(kb.eps_col[:, :], LN_EPS), W=[kb.eps_col])
        kb.dma("sp", ident[:, :], cst["ident"][:, :], R=[cst["ident"]], W=[ident])
        load_w_cast(kb, wo, L["wo"], D)
        kb.dma("sp", lnp[:, :, :], L["lnp"][:, :, :], R=[L["lnp"]], W=[lnp])
        idxo = kb.sb([128, 32], I32, "idxo")
        kb.dma("sp", idxo[:, :], cst["idx_o"][:, :], R=[cst["idx_o"]], W=[idxo])
        if moe:
            wr = kb.sb([128, 8, 8], F32, "wr")
            kb.dma("sp", wr[:, :, :], L["wr"][:, :, :], R=[L["wr"]], W=[wr])
            x1T32 = kb.sb([128, 8, 128], F32, "x1T32")
            comb = [kb.sb([128, 8], F32, "comb") for _ in range(4)]
            rt = kb.sb([128, 40], F32, "rt")
        oT = [kb.sb([128, 8, TG], BF16, "oT") for _ in range(1)]
        xt = [kb.sb([128, D], F32, "xt") for _ in range(2)]
        h = kb.sb([128, D], F32, "h")
        x1g = [kb.sb([128, D], F32, "x1g") for _ in range(4)]
        x1T = kb.sb([128, 8, TG], BF16, "x1T")
        aT = [kb.sb([128, TG], BF16, "aT") for _ in range(nfu)]
        w2 = [kb.sb([128, D], BF16, "w2") for _ in range(nfu)]
        w13 = [kb.sb([128, 2048], BF16, "w13") for _ in range(3)]
        yacc = [kb.sb([128, D], F32, "yacc") for _ in range(4)]
        sil = [kb.sb([128, TG], F32, "sil") for _ in range(2)]
        ost = [kb.sb([128, D], F32, "ost") for _ in range(2)]
        stg = kb.sb([128, 8, 512], BF16, "stgx")
        scr = (kb.sb([128, 2, 6], F32, "stats"), kb.sb([128, 2], F32, "mv"), kb.sb([128, 2], F32, "sd"))
        X = [kb.ps([128, 512], F32, "X") for _ in range(4)]
        Y = [kb.ps([128, 512], F32, "Y") for _ in range(2)]
        wcnt = 0
        for g in range(NG):
            og = oT[0]
            for k8 in range(8):
                kb.idma(og[:, k8, :], oT_all[:, :], idxo[:, g * 8 + k8:g * 8 + k8 + 1], R=[oT_all, idxo],
                        Wd=[og] if k8 else (), W=[og] if k8 == 0 else ())
            for tt in range(4):
                tok0 = g * TG + tt * 128
                xi = xt[tt % 2]
                kb.dma("sp", xi[:, :], x_src[tok0:tok0 + 128, :], R=[x_src], W=[xi])
                for hf in range(2):
                    for kc in range(8):
                        kb.op("pe", lambda: nc.tensor.matmul(Y[hf][:, :], lhsT=og[:, kc, tt * 128:(tt + 1) * 128],
                                                             rhs=wo[:, kc, hf * 512:(hf + 1) * 512],
                                                             start=(kc == 0), stop=(kc == 7)), R=[og, wo], W=[Y[hf]])
                    kb.op("dve", lambda: nc.vector.scalar_tensor_tensor(out=h[:, hf * 512:(hf + 1) * 512],
                                                                        in0=xi[:, hf * 512:(hf + 1) * 512], scalar=ALPHA,
                                                                        in1=Y[hf][:, :], op0=ALU.mult, op1=ALU.add),
                          R=[xi, Y[hf]], W=[h])
                x1 = x1g[tt]
                layer_norm(kb, h, (lnp, lnp[:, 0, :]), (lnp, lnp[:, 1, :]), x1[:, :], x1, scr)
                for kc in range(8):
                    pt = X[kc // 4]
                    kb.op("pe", lambda: nc.tensor.transpose(pt[:, (kc % 4) * 128:(kc % 4 + 1) * 128],
                                                            x1[:, kc * 128:(kc + 1) * 128], ident[:, :]),
                          R=[x1, ident], W=[pt])
                for hf in range(2):
                    src = X[hf][:, :].rearrange("p (k t) -> p k t", k=4)
                    dst = x1T[:, hf * 4:(hf + 1) * 4, tt * 128:(tt + 1) * 128]
                    if hf == 0:
                        kb.op("dve", lambda: nc.vector.tensor_copy(out=dst, in_=src), R=[X[hf]], W=[x1T])
                    else:
                        kb.op("act", lambda: nc.scalar.copy(out=dst, in_=src), R=[X[hf]], W=[x1T])
                    if moe:
                        kb.op("dve", lambda: nc.vector.tensor_copy(out=x1T32[:, hf * 4:(hf + 1) * 4, :], in_=src),
                              R=[X[hf]], W=[x1T32])
                if moe:
                    pr = X[2]
                    for kc in range(8):
                        kb.op("pe", lambda: nc.tensor.matmul(pr[:, :8], lhsT=x1T32[:, kc, :], rhs=wr[:, kc, :],
                                                             start=(kc == 0), stop=(kc == 7)), R=[x1T32, wr], W=[pr])
                    cb = comb[tt]
                    lg, mx, tmp, oh = rt[:, 0:8], rt[:, 8:16], rt[:, 16:24], rt[:, 24:32]
                    sc = rt[:, 32:40]
                    kb.op("dve", lambda: nc.vector.tensor_copy(out=lg, in_=pr[:, :8]), R=[pr], W=[rt])
                    kb.op("dve", lambda: nc.vector.max(out=mx, in_=lg), R=[rt], W=[rt])
                    kb.op("dve", lambda: nc.vector.tensor_tensor(out=sc[:, 0:1], in0=mx[:, 1:2], in1=mx[:, 0:1],
                                                                 op=ALU.subtract), R=[rt], W=[rt])
                    kb.op("act", lambda: nc.scalar.activation(out=sc[:, 1:2], in_=sc[:, 0:1], func=AF.Exp),
                          R=[rt], W=[rt])
                    kb.op("dve", lambda: nc.vector.tensor_scalar(out=sc[:, 2:3], in0=sc[:, 1:2], scalar1=1.0,
                                                                 scalar2=None, op0=ALU.add), R=[rt], W=[rt])
                    kb.op("dve", lambda: nc.vector.reciprocal(out=sc[:, 3:4], in_=sc[:, 2:3]), R=[rt], W=[rt])
                    kb.op("dve", lambda: nc.vector.tensor_tensor(out=sc[:, 4:5], in0=sc[:, 1:2], in1=sc[:, 3:4],
                                                                 op=ALU.mult), R=[rt], W=[rt])
                    kb.op("dve", lambda: nc.vector.tensor_scalar(out=tmp, in0=lg, scalar1=mx[:, 0:1],
                                                                 scalar2=sc[:, 3:4], op0=ALU.is_equal, op1=ALU.mult),
                          R=[rt], W=[rt])
                    kb.op("dve", lambda: nc.vector.tensor_scalar(out=oh, in0=lg, scalar1=mx[:, 1:2],
                                                                 scalar2=sc[:, 4:5], op0=ALU.is_equal, op1=ALU.mult),
                          R=[rt], W=[rt])
                    kb.op("dve", lambda: nc.vector.tensor_tensor(out=cb[:, :], in0=tmp, in1=oh, op=ALU.add),
                          R=[rt], W=[cb])
            first = True
            for e in range(n_exp):
                for u in range(n_units):
                    base = (e * n_units + u) * nfu
                    for f in range(nfu):
                        wc = w13[wcnt % 3]
                        kb.dma("pool", wc[:, :], wf_d[base + f, :, 0:2048], R=[wf_d], W=[wc])
                        kb.dma("pool", w2[f][:, :], wf_d[base + f, :, 2048:3072], R=[wf_d], W=[w2[f]])
                        h1, h3 = X[(wcnt % 2) * 2], X[(wcnt % 2) * 2 + 1]
                        for kc in range(8):
                            kb.op("pe", lambda: nc.tensor.matmul(h1[:, :], lhsT=wc[:, kc * 128:(kc + 1) * 128],
                                                                 rhs=x1T[:, kc, :], start=(kc == 0), stop=(kc == 7)),
                                  R=[wc, x1T], W=[h1])
                        for kc in range(8):
                            kb.op("pe", lambda: nc.tensor.matmul(h3[:, :],
                                                                 lhsT=wc[:, 1024 + kc * 128:1024 + (kc + 1) * 128],
                                                                 rhs=x1T[:, kc, :], start=(kc == 0), stop=(kc == 7)),
                                  R=[wc, x1T], W=[h3])
                        s = sil[wcnt % 2]
                        kb.op("act", lambda: nc.scalar.activation(out=s[:, :], in_=h1[:, :], func=AF.Silu),
                              R=[h1], W=[s])
                        kb.op("dve", lambda: nc.vector.tensor_tensor(out=aT[f][:, :], in0=s[:, :], in1=h3[:, :],
                                                                     op=ALU.mult), R=[s, h3], W=[aT[f]])
                        wcnt += 1
                    for tt in range(4):
                        for hf in range(2):
                            py = Y[hf]
                            for f in range(nfu):
                                kb.op("pe", lambda: nc.tensor.matmul(py[:, :], lhsT=aT[f][:, tt * 128:(tt + 1) * 128],
                                                                     rhs=w2[f][:, hf * 512:(hf + 1) * 512],
                                                                     start=(f == 0), stop=(f == nfu - 1)),
                                      R=[aT[f], w2[f]], W=[py])
                            ya = yacc[tt]
                            ysl = ya[:, hf * 512:(hf + 1) * 512]
                            if moe:
                                cs = comb[tt][:, e:e + 1]
                                if first:
                                    kb.op("dve", lambda: nc.vector.tensor_scalar(out=ysl, in0=py[:, :], scalar1=cs,
                                                                                 scalar2=None, op0=ALU.mult),
                                          R=[py, comb[tt]], W=[ya])
                                else:
                                    kb.op("dve", lambda: nc.vector.scalar_tensor_tensor(out=ysl, in0=py[:, :], scalar=cs,
                                                                                        in1=ysl, op0=ALU.mult,
                                                                                        op1=ALU.add),
                                          R=[py, comb[tt], ya], W=[ya])
                            else:
                                if first:
                                    kb.op("dve", lambda: nc.vector.tensor_copy(out=ysl, in_=py[:, :]), R=[py], W=[ya])
                                else:
                                    kb.op("dve", lambda: nc.vector.tensor_tensor(out=ysl, in0=py[:, :], in1=ysl,
                                                                                 op=ALU.add), R=[py, ya], W=[ya])
                    first = False
            for tt in range(4):
                tok0 = g * TG + tt * 128
                kb.op("dve", lambda: nc.vector.scalar_tensor_tensor(out=h[:, :], in0=x1g[tt][:, :], scalar=ALPHA,
                                                                    in1=yacc[tt][:, :], op0=ALU.mult, op1=ALU.add),
                      R=[x1g[tt], yacc[tt]], W=[h])
                o = ost[tt % 2]
                layer_norm(kb, h, (lnp, lnp[:, 2, :]), (lnp, lnp[:, 3, :]), o[:, :], o, scr)
                kb.dma("sp", x_dst[tok0:tok0 + 128, :], o[:, :], R=[o], Wd=[x_dst])
                if not last:
                    emit_xT(kb, X, ident, o, g * 4 + tt, stg, xT_loc, xTs_loc)


def _dbg_out(kb, x_d, out_d):
    for tt in range(4):
        kb.dma("sp", out_d[tt * 512:(tt + 1) * 512, :], x_d[tt * 512:(tt + 1) * 512, :], R=[x_d], Wd=[out_d])
    return kb.finish()


PROFILE_SCOPES = False


class _NullScope:
    def __enter__(self):
        return self

    def __exit__(self, *a):
        return False


def _scope(nc, name):
    return nc.named_scope(name) if PROFILE_SCOPES else _NullScope()


def build_fused(layers=(0, 1, 2, 3), upto=9):
    kb = KBF()
    nc = kb.nc
    x_d = kb.dram_in("x", [TPC, D], F32)
    out_d = kb.dram_out("out", [TPC, D], F32)
    cst = {"tri": kb.dram_in("tri", [128, 128], F32), "identb": kb.dram_in("identb", [128, 128], BF16),
           "ident": kb.dram_in("ident", [128, 128], F32), "dilc": kb.dram_in("dilc", [32, NDEL], F32),
           "dsac": kb.dram_in("dsac", [32, NDEL], F32), "cmask": kb.dram_in("cmask", [128, 512], F32),
           "steps": kb.dram_in("steps", [128, NBIS], F32), "idx_s": kb.dram_in("idx_s", [128, 32], I32),
           "idx_o": kb.dram_in("idx_o", [128, 32], I32)}
    Ls = {}
    for l in layers:
        L = {}
        pre = "L%d_" % l
        moe = (l % 2 == 1)
        if l % 2 == 0:
            L["wgl"] = kb.dram_in(pre + "wgl", [128, 8, 464], F32)
            L["wg"] = kb.dram_in(pre + "wg", [16, 64], F32)
            L["bg"] = kb.dram_in(pre + "bg", [128, 64], F32)
            L["gn"] = kb.dram_in(pre + "gn", [128, 128], F32)
            L["wd1"] = kb.dram_in(pre + "wd1", [128, 8, 584], F32)
            L["wd2"] = kb.dram_in(pre + "wd2", [128, 8, 384], F32)
            L["relfar"] = kb.dram_in(pre + "relfar", [128, 2], F32)
        else:
            L["wcd"] = kb.dram_in(pre + "wcd", [128, 8, 770], F32)
            L["bf"] = kb.dram_in(pre + "bf", [128, 2], F32)
            L["wr"] = kb.dram_in(pre + "wr", [128, 8, 8], F32)
        L["rel2"] = kb.dram_in(pre + "rel2", [32, 2], F32)
        L["wo"] = kb.dram_in(pre + "wo", [128, 8, D], F32)
        L["lnp"] = kb.dram_in(pre + "lnp", [128, 4, D], F32)
        L["wf"] = kb.dram_in(pre + "wf", [(224 if moe else 22) if upto >= 9 else 1, 128, 3072], F32)
        L["Escr"] = kb.dram_tmp(pre + "Escr", [2, NDEL], F32)
        Ls[l] = L
    xres = [kb.dram_tmp("xres0", [TPC, D], F32), kb.dram_tmp("xres1", [TPC, D], F32)]
    xT_loc = kb.dram_tmp("xT_loc", [D, TPC], BF16)
    xT_all = kb.dram_tmp("xT_all", [4 * D, TPC], BF16)
    xTs_loc = kb.dram_tmp("xTs_loc", [4 * D, 512], BF16)
    xTs_all = kb.dram_tmp("xTs_all", [16 * D, 512], BF16)
    oT_loc = kb.dram_tmp("oT_loc", [16 * 256, 512], BF16)
    oT_all = kb.dram_tmp("oT_all", [64 * 256, 512], BF16)
    M_loc = kb.dram_tmp("M_loc", [MTOT // 512, 512], BF16)
    M_all = kb.dram_tmp("M_all", [4 * MTOT // 512, 512], BF16)
    assert MPARTS[-1][4] + 4 * MPARTS[-1][3] == 4 * MTOT // 512
    with kb.scope():
        ident = kb.sb([128, 128], F32, "ident")
        kb.dma("sp", ident[:, :], cst["ident"][:, :], R=[cst["ident"]], W=[ident])
        xin = [kb.sb([128, D], F32, "xin") for _ in range(2)]
        stg = kb.sb([128, 8, 512], BF16, "stgx")
        X = [kb.ps([128, 512], F32, "X") for _ in range(2)]
        for tt in range(TPC // 128):
            xi = xin[tt % 2]
            kb.dma("sp", xi[:, :], x_d[tt * 128:(tt + 1) * 128, :], R=[x_d], W=[xi])
            emit_xT(kb, X, ident, xi, tt, stg, xT_loc, xTs_loc)
    if upto == 0:
        return _dbg_out(kb, x_d, out_d)
    x_src = x_d
    for n, l in enumerate(layers):
        L = Ls[l]
        last = (n == len(layers) - 1)
        x_dst = out_d if last else xres[n % 2]
        for cf in range(4):
            kb.collective("AllGather", xT_loc, xT_all, xT_loc[cf * 256:(cf + 1) * 256, :],
                          xT_all[cf * 1024:(cf + 1) * 1024, :])
        if upto == 1:
            return _dbg_out(kb, x_d, out_d)
        if l % 2 == 0:
            for j in range(4):
                kb.collective("AllGather", xTs_loc, xTs_all, xTs_loc[j * 1024:(j + 1) * 1024, :],
                              xTs_all[j * 4096:(j + 1) * 4096, :])
            with _scope(nc, "L%d_gla" % l):
                phase_gla(kb, L, xT_all, oT_loc, cst)
            with _scope(nc, "L%d_dsa1" % l):
                phase_dsa1(kb, L, xT_all, xTs_all, M_loc, M_all, cst)
            with _scope(nc, "L%d_dsa2" % l):
                phase_dsa2(kb, L, xT_all, M_all, oT_loc, oT_all, cst)
        else:
            with _scope(nc, "L%d_cd" % l):
                phase_cd(kb, L, xT_all, oT_loc, oT_all, cst)
        if upto == 2:
            return _dbg_out(kb, x_d, out_d)
        if upto == 3:
            return _dbg_out(kb, x_d, out_d)
        with _scope(nc, "L%d_c" % l):
            phase_c(kb, L, l % 2 == 1, oT_all, x_src, x_dst, xT_loc, xTs_loc, cst, last)
        x_src = x_dst
    return kb.finish()


def fused_inputs(x, ln_g, ln_b, rel_table, w_in_ab, w_gate_a, b_gate_a, g_norm_a, w_out_ab,
                 w_in_cd, b_forget, w_out_cd, w1_dense, w3_dense, w2_dense,
                 w_router, w1_moe, w3_moe, w2_moe, layers=(0, 1, 2, 3)):
    f32 = lambda a: np.ascontiguousarray(np.asarray(a, dtype=np.float32))
    bc = lambda v, n=128: np.ascontiguousarray(np.broadcast_to(np.asarray(v, np.float32)[None, :], (n, len(v))))
    xf = f32(x).reshape(BATCH * SEQ, D)
    rel_table = f32(rel_table)
    steps = bc(0.5 ** np.arange(1, NBIS + 1))
    perm = np.zeros(D, np.int64)
    for src in range(4):
        for rr in range(256):
            perm[src * 256 + rr] = src * 128 + rr if rr < 128 else 512 + src * 128 + (rr - 128)
    shared = {"tri": TRI, "identb": IDENT.astype(NPBF), "ident": IDENT, "dilc": dil_const(), "dsac": dsa_const(),
              "steps": steps}
    per_layer_shared = {}
    for l in layers:
        j = l // 2
        pre = "L%d_" % l
        d = {}
        d[pre + "lnp"] = np.ascontiguousarray(np.broadcast_to(
            np.stack([ln_g[l, 0], ln_b[l, 0], ln_g[l, 1], ln_b[l, 1]]).astype(np.float32)[None], (128, 4, D)))
        if l % 2 == 0:
            d[pre + "wo"] = w_kc_layout(f32(w_out_ab[j])[perm])
            d[pre + "wf"] = ffn_chunk_layout(f32(w1_dense[j]), f32(w3_dense[j]), f32(w2_dense[j]))
            d[pre + "gn"] = bc(g_norm_a[j])
        else:
            d[pre + "wo"] = w_kc_layout(f32(w_out_cd[j])[perm])
            d[pre + "wf"] = np.concatenate([ffn_chunk_layout(f32(w1_moe[j, e]), f32(w3_moe[j, e]), f32(w2_moe[j, e]))
                                            for e in range(8)], axis=0)
            d[pre + "wr"] = np.ascontiguousarray(f32(w_router[j]).reshape(8, 128, 8).transpose(1, 0, 2))
        per_layer_shared.update(d)
    maps = []
    for c in range(NCORES):
        b, m = c // 4, c % 4
        mp = dict(shared)
        mp.update(per_layer_shared)
        mp["x"] = np.ascontiguousarray(xf[c * TPC:(c + 1) * TPC])
        pp_ = np.arange(128)[:, None]
        rr_, k8_ = np.arange(4)[None, :, None], np.arange(8)[None, None, :]
        mp["idx_s"] = np.ascontiguousarray(((m * 4 + rr_) * 1024 + k8_ * 128 + pp_[:, :, None]).reshape(128, 32)
                                           .astype(np.int32))
        G_ = k8_ * 128 + pp_[:, :, None]
        mp["idx_o"] = np.ascontiguousarray(((m * 4 + G_ // 256) * 1024 + rr_ * 256 + G_ % 256).reshape(128, 32)
                                           .astype(np.int32))
        mp["cmask"] = np.where(np.arange(512)[None, :] <= (128 * m + np.arange(128))[:, None], 0.0, NEG).astype(np.float32)
        for l in layers:
            j = l // 2
            pre = "L%d_" % l
            mp[pre + "rel2"] = np.ascontiguousarray(rel_table[:, 2 * m:2 * m + 2])
            if l % 2 == 0:
                w = f32(w_in_ab[j])
                cs = lambda a, n: w[:, a:a + n]
                wgl = np.concatenate([cs(m * 64, 64), cs(256 + m * 64, 64), cs(1536, 16), cs(256 + m * 64, 64),
                                      cs(512 + m * 128, 128), cs(1024 + m * 128, 128)], axis=1)
                wd1 = np.concatenate([cs(3088, 512), cs(3600, 64), cs(3664, 8)], axis=1)
                wd2 = np.concatenate([cs(1552 + m * 128, 128), cs(2064 + m * 128, 128), cs(2576 + m * 128, 128)], axis=1)
                mp[pre + "wgl"] = w_kc_layout(wgl)
                mp[pre + "wd1"] = w_kc_layout(wd1)
                mp[pre + "wd2"] = w_kc_layout(wd2)
                mp[pre + "wg"] = np.ascontiguousarray(f32(w_gate_a[j])[:, m * 64:(m + 1) * 64])
                mp[pre + "bg"] = bc(f32(b_gate_a[j])[m * 64:(m + 1) * 64])
                mp[pre + "relfar"] = bc(rel_table[31, 2 * m:2 * m + 2])
            else:
                w = f32(w_in_cd[j])
                cs = lambda a, n: w[:, a:a + n]
                wcd = np.concatenate([cs(m * 128, 128), cs(512 + m * 128, 128), cs(1544 + m * 128, 128),
                                      cs(2056 + m * 128, 128), cs(1024 + m * 128, 128), cs(2568 + m * 128, 128),
                                      cs(1536 + 2 * m, 2)], axis=1)
                mp[pre + "wcd"] = w_kc_layout(wcd)
                mp[pre + "bf"] = bc(f32(b_forget[j])[2 * m:2 * m + 2])
        maps.append(mp)
    return maps


def kernel_fused(**inputs):
    nc = build_fused()
    maps = fused_inputs(**inputs)
    res = run_spmd(nc, maps)
    out = np.concatenate([res[c]["out"] for c in range(NCORES)], axis=0)
    return out.reshape(BATCH, SEQ, D).astype(np.float32)


def kernel(**inputs):
    return kernel_fused(**inputs)
```

```python
import math
from contextlib import ExitStack
import numpy as np
import ml_dtypes
import concourse.bass as bass
import concourse.mybir as mybir
from concourse.bass_utils import run_bass_kernel_spmd

F32 = mybir.dt.float32
BF16 = mybir.dt.bfloat16
AF = mybir.ActivationFunctionType
ALU = mybir.AluOpType
AX = mybir.AxisListType
NPBF = ml_dtypes.bfloat16

NCORES = 8
D = 1024
SEQ = 8192
BATCH = 2
DEPTH = 4
ALPHA = (2 * DEPTH) ** 0.25
LN_EPS = 1e-5
NEG = -1.0e30


class Buf:
    def __init__(self, t):
        self.t = t
        self.w = None
        self.r = []

    def __getitem__(self, idx):
        return self.t[idx]


class KB:
    NDS = 6

    def __init__(self):
        self.nc = bass.Bass("TRN2", target_bir_lowering=False)
        nc = self.nc
        self.es = ExitStack()
        self.eng = {"pe": nc.tensor, "dve": nc.vector, "act": nc.scalar, "pool": nc.gpsimd, "sp": nc.sync}
        self.esem = {}
        self.ecnt = {}
        self.seen = {e: {} for e in self.eng}
        for e in self.eng:
            self.esem[e] = self.es.enter_context(nc.semaphore("sem_" + e))
            self.ecnt[e] = 0
        self.dsem = {}
        self.dcnt = {}
        self.dnext = {}
        for q in ("sp", "act", "pool"):
            self.dsem[q] = [self.es.enter_context(nc.semaphore("dsem_%s%d" % (q, i))) for i in range(self.NDS)]
            self.dcnt[q] = [0] * self.NDS
            self.dnext[q] = 0
        self.n_names = 0
        self.outs = []

    def _nm(self, p):
        self.n_names += 1
        return "%s_%d" % (p, self.n_names)

    def dram_in(self, name, shape, dt):
        return Buf(self.nc.dram_tensor(name, list(shape), dt, kind="ExternalInput").ap())

    def dram_out(self, name, shape, dt):
        b = Buf(self.nc.dram_tensor(name, list(shape), dt, kind="ExternalOutput").ap())
        self.outs.append(b)
        return b

    def dram_tmp(self, name, shape, dt):
        return Buf(self.nc.dram_tensor(name, list(shape), dt, kind="Internal").ap())

    def sb(self, shape, dt, name="sb"):
        return Buf(self.es.enter_context(self.nc.sbuf_tensor(self._nm(name), list(shape), dt)))

    def ps(self, shape, dt=F32, name="ps"):
        return Buf(self.es.enter_context(self.nc.psum_tensor(self._nm(name), list(shape), dt)))

    def _wait(self, e, ev):
        if ev is None:
            return
        sem, val, key = ev
        if self.seen[e].get(key, 0) >= val:
            return
        self.eng[e].wait_ge(sem, val)
        self.seen[e][key] = val

    def _deps(self, e, R, W):
        evs = []
        for b in R:
            if b.w is not None:
                evs.append(b.w)
        for b in W:
            if b.w is not None:
                evs.append(b.w)
            evs.extend(b.r)
        best = {}
        for ev in evs:
            k = ev[2]
            if e == "pe" and k == "pe":
                continue
            if k not in best or best[k][1] < ev[1]:
                best[k] = ev
        for ev in best.values():
            self._wait(e, ev)

    def _mark(self, ev, R, W):
        for b in R:
            b.r.append(ev)
            if len(b.r) > 24:
                best = {}
                for x in b.r:
                    if x[2] not in best or best[x[2]][1] < x[1]:
                        best[x[2]] = x
                b.r = list(best.values())
        for b in W:
            b.w = ev
            b.r = []

    def op(self, e, fn, R=(), W=()):
        self._deps(e, R, W)
        ins = fn()
        self.ecnt[e] += 1
        ins.then_inc(self.esem[e], 1)
        ev = (self.esem[e], self.ecnt[e], e)
        self.seen[e][e] = max(self.seen[e].get(e, 0), 0)
        self._mark(ev, R, W)
        return ev

    def dma(self, q, out, in_, R=(), W=(), **kw):
        self._deps(q, R, W)
        i = self.dnext[q]
        self.dnext[q] = (i + 1) % self.NDS
        key = "d%s%d" % (q, i)
        if self.dcnt[q][i] > 0:
            self._wait(q, (self.dsem[q][i], self.dcnt[q][i], key))
        self.dcnt[q][i] += 16
        self.eng[q].dma_start(out=out, in_=in_, **kw).then_inc(self.dsem[q][i], 16)
        ev = (self.dsem[q][i], self.dcnt[q][i], key)
        self._mark(ev, R, W)
        return ev

    def finish(self):
        for q in ("sp", "act", "pool"):
            for i in range(self.NDS):
                if self.dcnt[q][i] > 0:
                    self._wait("sp", (self.dsem[q][i], self.dcnt[q][i], "d%s%d" % (q, i)))
        for e in ("pe", "dve", "act", "pool"):
            if self.ecnt[e] > 0:
                self._wait("sp", (self.esem[e], self.ecnt[e], e))
        self.es.close()
        return self.nc


def run_spmd(nc, in_maps):
    res = run_bass_kernel_spmd(nc, in_maps, core_ids=list(range(NCORES)))
    return res.results


def build_cast(n):
    kb = KB()
    nc = kb.nc
    CH = 2048
    src = kb.dram_in("src", [128, n], F32)
    dst = kb.dram_out("dst", [128, n], BF16)
    NB = 3
    tin = [kb.sb([128, CH], F32, "tin") for _ in range(NB)]
    tout = [kb.sb([128, CH], BF16, "tout") for _ in range(NB)]
    nch = (n + CH - 1) // CH
    for c in range(nch):
        c0 = c * CH
        w = min(CH, n - c0)
        a, b = tin[c % NB], tout[c % NB]
        kb.dma("sp", a[:, :w], src[:, c0:c0 + w], R=[src], W=[a])
        if c % 2 == 0:
            kb.op("dve", lambda: nc.vector.tensor_copy(out=b[:, :w], in_=a[:, :w]), R=[a], W=[b])
        else:
            kb.op("act", lambda: nc.scalar.copy(out=b[:, :w], in_=a[:, :w]), R=[a], W=[b])
        kb.dma("pool", dst[:, c0:c0 + w], b[:, :w], R=[b], W=[dst])
    return kb.finish()


def cast_weights(arrs):
    flats = [np.ascontiguousarray(a).reshape(NCORES, 128, -1) for a in arrs]
    ns = [f.shape[2] for f in flats]
    cat = np.concatenate(flats, axis=2)
    n = cat.shape[2]
    nc = build_cast(n)
    res = run_spmd(nc, [{"src": np.ascontiguousarray(cat[c])} for c in range(NCORES)])
    out = np.stack([res[c]["dst"] for c in range(NCORES)], axis=0)
    outs = []
    o = 0
    for a, k in zip(arrs, ns):
        outs.append(out[:, :, o:o + k].reshape(a.shape))
        o += k
    return outs


TPC = 2048


def build_A(W, fm, tmb, tmf, gate=None):
    kb = KB()
    nc = kb.nc
    NT = TPC // 128
    n_fm = sum(n for _, n in fm)
    n_tmb = sum(n for _, n in tmb)
    n_tmf = sum(n for _, n in tmf) + (256 if gate is not None else 0)
    x = kb.dram_in("x", [TPC, D], F32)
    w = kb.dram_in("w", [128, 8, W], BF16)
    ident_d = kb.dram_in("ident", [128, 128], F32)
    yT = kb.dram_out("yT", [n_fm, TPC], BF16)
    ytb = kb.dram_out("ytb", [TPC, max(n_tmb, 1)], BF16)
    ytf = kb.dram_out("ytf", [TPC, max(n_tmf, 1)], F32)
    wsb = kb.sb([128, 8, W], BF16, "w")
    ident = kb.sb([128, 128], F32, "ident")
    xT = kb.sb([128, 8, TPC], BF16, "xT")
    kb.dma("sp", ident[:, :], ident_d[:, :], R=[ident_d], W=[ident])
    for kc in range(8):
        kb.dma("pool" if kc % 2 else "sp", wsb[:, kc, :], w[:, kc, :], R=[w], W=[wsb])
    if gate is not None:
        wg_d = kb.dram_in("wg", [16, 256], F32)
        bg_d = kb.dram_in("bg", [128, 256], F32)
        wg = kb.sb([16, 256], F32, "wg")
        bg = kb.sb([128, 256], F32, "bg")
        gaT = kb.sb([16, TPC], F32, "gaT")
        w32 = kb.sb([128, 8, 16], F32, "w32")
        kb.dma("sp", wg[:, :], wg_d[:, :], R=[wg_d], W=[wg])
        kb.dma("sp", bg[:, :], bg_d[:, :], R=[bg_d], W=[bg])
    xin = [kb.sb([128, D], F32, "xin") for _ in range(2)]
    pst = [kb.ps([128, 1024], F32, "pst")]
    for tt in range(NT):
        xi = xin[tt % 2]
        kb.dma("sp", xi[:, :], x[tt * 128:(tt + 1) * 128, :], R=[x], W=[xi])
        pt = pst[0]
        for kc in range(8):
            kb.op("pe", lambda: nc.tensor.transpose(pt[:, kc * 128:(kc + 1) * 128], xi[:, kc * 128:(kc + 1) * 128],
                                                    ident[:, :]), R=[xi, ident], W=[pt])
        for hf in range(2):
            src = pt[:, hf * 512:(hf + 1) * 512].rearrange("p (k t) -> p k t", k=4)
            dst = xT[:, hf * 4:(hf + 1) * 4, tt * 128:(tt + 1) * 128]
            if hf == 0:
                kb.op("dve", lambda: nc.vector.tensor_copy(out=dst, in_=src), R=[pt], W=[xT])
            else:
                kb.op("act", lambda: nc.scalar.copy(out=dst, in_=src), R=[pt], W=[xT])
    psf = [kb.ps([128, 512], F32, "psf") for _ in range(2)]
    stf = [kb.sb([128, 512], BF16, "stf") for _ in range(3)]
    cnt = 0
    row = 0
    blocks = list(fm)
    for (c0, ncol) in blocks:
        for tg in range(TPC // 512):
            pp = psf[cnt % 2]
            st = stf[cnt % 3]
            for kc in range(8):
                kb.op("pe", lambda: nc.tensor.matmul(pp[:ncol, :], lhsT=wsb[:, kc, c0:c0 + ncol],
                                                     rhs=xT[:, kc, tg * 512:(tg + 1) * 512],
                                                     start=(kc == 0), stop=(kc == 7)), R=[wsb, xT], W=[pp])
            if cnt % 2 == 0:
                kb.op("dve", lambda: nc.vector.tensor_copy(out=st[:ncol, :], in_=pp[:ncol, :]), R=[pp], W=[st])
            else:
                kb.op("act", lambda: nc.scalar.copy(out=st[:ncol, :], in_=pp[:ncol, :]), R=[pp], W=[st])
            kb.dma("pool" if cnt % 2 else "sp", yT[row:row + ncol, tg * 512:(tg + 1) * 512], st[:ncol, :],
                   R=[st], W=[yT])
            cnt += 1
        row += ncol
    if gate is not None:
        g0 = gate[0]
        for tg in range(TPC // 512):
            pp = psf[cnt % 2]
            for kc in range(8):
                kb.op("pe", lambda: nc.tensor.matmul(pp[:16, :], lhsT=wsb[:, kc, g0:g0 + 16],
                                                     rhs=xT[:, kc, tg * 512:(tg + 1) * 512],
                                                     start=(kc == 0), stop=(kc == 7)), R=[wsb, xT], W=[pp])
            kb.op("dve", lambda: nc.vector.tensor_copy(out=gaT[:, tg * 512:(tg + 1) * 512], in_=pp[:16, :]),
                  R=[pp], W=[gaT])
            cnt += 1
    pstm = [kb.ps([128, 512], F32, "pstm") for _ in range(2)]
    stb = [kb.sb([128, 512], BF16, "stb") for _ in range(3)]
    st32 = [kb.sb([128, 512], F32, "st32") for _ in range(3)]
    cnt = 0
    for tt in range(NT):
        for kind, lst, dst_d in (("b", tmb, ytb), ("f", tmf, ytf)):
            off = 0
            for (c0, ncol) in lst:
                pp = pstm[cnt % 2]
                st = (stb if kind == "b" else st32)[cnt % 3]
                for kc in range(8):
                    kb.op("pe", lambda: nc.tensor.matmul(pp[:, :ncol], lhsT=xT[:, kc, tt * 128:(tt + 1) * 128],
                                                         rhs=wsb[:, kc, c0:c0 + ncol],
                                                         start=(kc == 0), stop=(kc == 7)), R=[wsb, xT], W=[pp])
                if cnt % 2 == 0:
                    kb.op("dve", lambda: nc.vector.tensor_copy(out=st[:, :ncol], in_=pp[:, :ncol]), R=[pp], W=[st])
                else:
                    kb.op("act", lambda: nc.scalar.copy(out=st[:, :ncol], in_=pp[:, :ncol]), R=[pp], W=[st])
                kb.dma("pool" if cnt % 2 else "sp", dst_d[tt * 128:(tt + 1) * 128, off:off + ncol], st[:, :ncol],
                       R=[st], W=[dst_d])
                off += ncol
                cnt += 1
        if gate is not None:
            off = sum(n for _, n in tmf)
            pp = pstm[cnt % 2]
            st = st32[cnt % 3]
            kb.op("pe", lambda: nc.tensor.matmul(pp[:, :256], lhsT=gaT[:, tt * 128:(tt + 1) * 128], rhs=wg[:, :],
                                                 start=True, stop=True), R=[gaT, wg], W=[pp])
            kb.op("dve", lambda: nc.vector.tensor_tensor(out=st[:, :256], in0=pp[:, :256], in1=bg[:, :], op=ALU.add),
                  R=[pp, bg], W=[st])
            kb.op("act", lambda: nc.scalar.activation(out=st[:, :256], in_=st[:, :256], func=AF.Exp, scale=-1.0),
                  R=[st], W=[st])
            kb.op("act", lambda: nc.scalar.activation(out=st[:, :256], in_=st[:, :256], func=AF.Ln, bias=1.0),
                  R=[st], W=[st])
            kb.op("dve", lambda: nc.vector.tensor_scalar(out=st[:, :256], in0=st[:, :256], scalar1=-1.0 / 16.0,
                                                         scalar2=None, op0=ALU.mult), R=[st], W=[st])
            kb.dma("sp", ytf[tt * 128:(tt + 1) * 128, off:off + 256], st[:, :256], R=[st], W=[ytf])
            cnt += 1
    return kb.finish()


def blocks_of(c0, n, bs):
    out = []
    while n > 0:
        k = min(bs, n)
        out.append((c0, k))
        c0 += k
        n -= k
    return out


def w_kc_layout(w):
    W = w.shape[1]
    return np.ascontiguousarray(w.reshape(8, 128, W).transpose(1, 0, 2))


IDENT = np.eye(128, dtype=np.float32)


def run_A(x_flat, w_bf, fm_ranges, tmb_ranges, tmf_ranges, gate=None, wg=None, bg=None):
    W = w_bf.shape[1]
    fm = [b for (c0, n) in fm_ranges for b in blocks_of(c0, n, 128)]
    tmb = [b for (c0, n) in tmb_ranges for b in blocks_of(c0, n, 512)]
    tmf = [b for (c0, n) in tmf_ranges for b in blocks_of(c0, n, 512)]
    nc = build_A(W, fm, tmb, tmf, gate)
    wl = w_kc_layout(w_bf)
    maps = []
    for c in range(NCORES):
        m = {"x": np.ascontiguousarray(x_flat[c * TPC:(c + 1) * TPC]), "w": wl, "ident": IDENT}
        if gate is not None:
            m["wg"] = np.ascontiguousarray(wg)
            m["bg"] = np.ascontiguousarray(np.broadcast_to(bg[None, :], (128, 256)))
        maps.append(m)
    res = run_spmd(nc, maps)
    yT = np.concatenate([res[c]["yT"] for c in range(NCORES)], axis=1)
    ytb = np.concatenate([res[c]["ytb"] for c in range(NCORES)], axis=0)
    ytf = np.concatenate([res[c]["ytf"] for c in range(NCORES)], axis=0)
    return yT, ytb, ytf


def layer_norm(kb, h, gt, bt, out_ap, out_buf, scr):
    nc = kb.nc
    stats, mv, sd = scr
    for c in range(2):
        kb.op("dve", lambda: nc.vector.bn_stats(out=stats[:, c, :], in_=h[:, c * 512:(c + 1) * 512]), R=[h], W=[stats])
    kb.op("dve", lambda: nc.vector.bn_aggr(out=mv[:, :], in_=stats[:, :, :].rearrange("p a b -> p (a b)")),
          R=[stats], W=[mv])
    kb.op("act", lambda: nc.scalar.activation(out=sd[:, 0:1], in_=mv[:, 1:2], func=AF.Sqrt, bias=kb.eps_col[:, 0:1]),
          R=[mv, kb.eps_col], W=[sd])
    kb.op("dve", lambda: nc.vector.reciprocal(out=sd[:, 1:2], in_=sd[:, 0:1]), R=[sd], W=[sd])
    kb.op("dve", lambda: nc.vector.tensor_scalar(out=h[:, :], in0=h[:, :], scalar1=mv[:, 0:1], scalar2=sd[:, 1:2],
                                                 op0=ALU.subtract, op1=ALU.mult), R=[h, mv, sd], W=[h])
    kb.op("pool", lambda: nc.gpsimd.tensor_tensor(out=h[:, :], in0=h[:, :], in1=gt[1], op=ALU.mult),
          R=[h, gt[0]], W=[h])
    kb.op("dve", lambda: nc.vector.tensor_tensor(out=out_ap, in0=h[:, :], in1=bt[1], op=ALU.add),
          R=[h, bt[0]], W=[out_buf])


def build_C(n_exp, nf_unit, n_units_per_exp):
    kb = KB()
    nc = kb.nc
    moe = n_exp > 1
    NT = TPC // 128
    TG = 512
    NG = TPC // TG
    nfu = nf_unit
    n_chunks = n_exp * n_units_per_exp * nfu
    oT_d = kb.dram_in("oT", [128, 8, TPC], BF16)
    x_d = kb.dram_in("x", [TPC, D], F32)
    wo_d = kb.dram_in("wo", [128, 8, D], BF16)
    lnp_d = kb.dram_in("lnp", [128, 4, D], F32)
    wf_d = kb.dram_in("wf", [n_chunks, 128, 3072], BF16)
    ident_d = kb.dram_in("ident", [128, 128], F32)
    out_d = kb.dram_out("out", [TPC, D], F32)
    ident = kb.sb([128, 128], F32, "ident")
    wo = kb.sb([128, 8, D], BF16, "wo")
    lnp = kb.sb([128, 4, D], F32, "lnp")
    kb.eps_col = kb.sb([128, 1], F32, "eps")
    kb.op("dve", lambda: nc.vector.memset(kb.eps_col[:, :], LN_EPS), W=[kb.eps_col])
    kb.dma("sp", ident[:, :], ident_d[:, :], R=[ident_d], W=[ident])
    kb.dma("sp", wo[:, :, :], wo_d[:, :, :], R=[wo_d], W=[wo])
    kb.dma("pool", lnp[:, :, :], lnp_d[:, :, :], R=[lnp_d], W=[lnp])
    if moe:
        wr_d = kb.dram_in("wr", [128, 8, 8], F32)
        wr = kb.sb([128, 8, 8], F32, "wr")
        kb.dma("sp", wr[:, :, :], wr_d[:, :, :], R=[wr_d], W=[wr])
        x1T32 = kb.sb([128, 8, 128], F32, "x1T32")
        comb = [kb.sb([128, 8], F32, "comb") for _ in range(4)]
        rt = kb.sb([128, 40], F32, "rt")
    oT = [kb.sb([128, 8, TG], BF16, "oT") for _ in range(2)]
    xt = [kb.sb([128, D], F32, "xt") for _ in range(2)]
    h = kb.sb([128, D], F32, "h")
    x1g = [kb.sb([128, D], F32, "x1g") for _ in range(4)]
    x1T = kb.sb([128, 8, TG], BF16, "x1T")
    aT = [kb.sb([128, TG], BF16, "aT") for _ in range(nfu)]
    w2 = [kb.sb([128, D], BF16, "w2") for _ in range(nfu)]
    w13 = [kb.sb([128, 2048], BF16, "w13") for _ in range(3)]
    yacc = [kb.sb([128, D], F32, "yacc") for _ in range(4)]
    sil = [kb.sb([128, TG], F32, "sil") for _ in range(2)]
    ost = [kb.sb([128, D], F32, "ost") for _ in range(2)]
    scr = (kb.sb([128, 2, 6], F32, "stats"), kb.sb([128, 2], F32, "mv"), kb.sb([128, 2], F32, "sd"))
    X = [kb.ps([128, 512], F32, "X") for _ in range(4)]
    Y = [kb.ps([128, 512], F32, "Y") for _ in range(2)]
    wcnt = 0
    for g in range(NG):
        og = oT[g % 2]
        kb.dma("pool", og[:, :, :], oT_d[:, :, g * TG:(g + 1) * TG], R=[oT_d], W=[og])
        for tt in range(4):
            tok0 = g * TG + tt * 128
            xi = xt[tt % 2]
            kb.dma("sp", xi[:, :], x_d[tok0:tok0 + 128, :], R=[x_d], W=[xi])
            for hf in range(2):
                for kc in range(8):
                    kb.op("pe", lambda: nc.tensor.matmul(Y[hf][:, :], lhsT=og[:, kc, tt * 128:(tt + 1) * 128],
                                                         rhs=wo[:, kc, hf * 512:(hf + 1) * 512],
                                                         start=(kc == 0), stop=(kc == 7)), R=[og, wo], W=[Y[hf]])
                kb.op("dve", lambda: nc.vector.scalar_tensor_tensor(out=h[:, hf * 512:(hf + 1) * 512],
                                                                    in0=xi[:, hf * 512:(hf + 1) * 512], scalar=ALPHA,
                                                                    in1=Y[hf][:, :], op0=ALU.mult, op1=ALU.add),
                      R=[xi, Y[hf]], W=[h])
            x1 = x1g[tt]
            layer_norm(kb, h, (lnp, lnp[:, 0, :]), (lnp, lnp[:, 1, :]), x1[:, :], x1, scr)
            for kc in range(8):
                pt = X[kc // 4]
                kb.op("pe", lambda: nc.tensor.transpose(pt[:, (kc % 4) * 128:(kc % 4 + 1) * 128],
                                                        x1[:, kc * 128:(kc + 1) * 128], ident[:, :]),
                      R=[x1, ident], W=[pt])
            for hf in range(2):
                src = X[hf][:, :].rearrange("p (k t) -> p k t", k=4)
                dst = x1T[:, hf * 4:(hf + 1) * 4, tt * 128:(tt + 1) * 128]
                if hf == 0:
                    kb.op("dve", lambda: nc.vector.tensor_copy(out=dst, in_=src), R=[X[hf]], W=[x1T])
                else:
                    kb.op("act", lambda: nc.scalar.copy(out=dst, in_=src), R=[X[hf]], W=[x1T])
                if moe:
                    kb.op("pool" if False else "dve",
                          lambda: nc.vector.tensor_copy(out=x1T32[:, hf * 4:(hf + 1) * 4, :], in_=src),
                          R=[X[hf]], W=[x1T32])
            if moe:
                pr = X[2]
                for kc in range(8):
                    kb.op("pe", lambda: nc.tensor.matmul(pr[:, :8], lhsT=x1T32[:, kc, :], rhs=wr[:, kc, :],
                                                         start=(kc == 0), stop=(kc == 7)), R=[x1T32, wr], W=[pr])
                cb = comb[tt]
                lg, mx, tmp, oh = rt[:, 0:8], rt[:, 8:16], rt[:, 16:24], rt[:, 24:32]
                sc = rt[:, 32:40]
                kb.op("dve", lambda: nc.vector.tensor_copy(out=lg, in_=pr[:, :8]), R=[pr], W=[rt])
                kb.op("dve", lambda: nc.vector.max(out=mx, in_=lg), R=[rt], W=[rt])
                kb.op("dve", lambda: nc.vector.tensor_tensor(out=sc[:, 0:1], in0=mx[:, 1:2], in1=mx[:, 0:1],
                                                             op=ALU.subtract), R=[rt], W=[rt])
                kb.op("act", lambda: nc.scalar.activation(out=sc[:, 1:2], in_=sc[:, 0:1], func=AF.Exp), R=[rt], W=[rt])
                kb.op("dve", lambda: nc.vector.tensor_scalar(out=sc[:, 2:3], in0=sc[:, 1:2], scalar1=1.0, scalar2=None,
                                                             op0=ALU.add), R=[rt], W=[rt])
                kb.op("dve", lambda: nc.vector.reciprocal(out=sc[:, 3:4], in_=sc[:, 2:3]), R=[rt], W=[rt])
                kb.op("dve", lambda: nc.vector.tensor_tensor(out=sc[:, 4:5], in0=sc[:, 1:2], in1=sc[:, 3:4],
                                                             op=ALU.mult), R=[rt], W=[rt])
                kb.op("dve", lambda: nc.vector.tensor_scalar(out=tmp, in0=lg, scalar1=mx[:, 0:1], scalar2=sc[:, 3:4],
                                                             op0=ALU.is_equal, op1=ALU.mult), R=[rt], W=[rt])
                kb.op("dve", lambda: nc.vector.tensor_scalar(out=oh, in0=lg, scalar1=mx[:, 1:2], scalar2=sc[:, 4:5],
                                                             op0=ALU.is_equal, op1=ALU.mult), R=[rt], W=[rt])
                kb.op("dve", lambda: nc.vector.tensor_tensor(out=cb[:, :], in0=tmp, in1=oh, op=ALU.add),
                      R=[rt], W=[cb])
        first = True
        for e in range(n_exp):
            for u in range(n_units_per_exp):
                base = (e * n_units_per_exp + u) * nfu
                for f in range(nfu):
                    wc = w13[wcnt % 3]
                    q = "sp" if wcnt % 2 == 0 else "pool"
                    kb.dma(q, wc[:, :], wf_d[base + f, :, 0:2048], R=[wf_d], W=[wc])
                    kb.dma("pool" if wcnt % 2 == 0 else "sp", w2[f][:, :], wf_d[base + f, :, 2048:3072],
                           R=[wf_d], W=[w2[f]])
                    h1, h3 = X[(wcnt % 2) * 2], X[(wcnt % 2) * 2 + 1]
                    for kc in range(8):
                        kb.op("pe", lambda: nc.tensor.matmul(h1[:, :], lhsT=wc[:, kc * 128:(kc + 1) * 128],
                                                             rhs=x1T[:, kc, :], start=(kc == 0), stop=(kc == 7)),
                              R=[wc, x1T], W=[h1])
                    for kc in range(8):
                        kb.op("pe", lambda: nc.tensor.matmul(h3[:, :], lhsT=wc[:, 1024 + kc * 128:1024 + (kc + 1) * 128],
                                                             rhs=x1T[:, kc, :], start=(kc == 0), stop=(kc == 7)),
                              R=[wc, x1T], W=[h3])
                    s = sil[wcnt % 2]
                    kb.op("act", lambda: nc.scalar.activation(out=s[:, :], in_=h1[:, :], func=AF.Silu), R=[h1], W=[s])
                    kb.op("dve", lambda: nc.vector.tensor_tensor(out=aT[f][:, :], in0=s[:, :], in1=h3[:, :],
                                                                 op=ALU.mult), R=[s, h3], W=[aT[f]])
                    wcnt += 1
                for tt in range(4):
                    for hf in range(2):
                        py = Y[hf]
                        for f in range(nfu):
                            kb.op("pe", lambda: nc.tensor.matmul(py[:, :], lhsT=aT[f][:, tt * 128:(tt + 1) * 128],
                                                                 rhs=w2[f][:, hf * 512:(hf + 1) * 512],
                                                                 start=(f == 0), stop=(f == nfu - 1)),
                                  R=[aT[f], w2[f]], W=[py])
                        ya = yacc[tt]
                        ysl = ya[:, hf * 512:(hf + 1) * 512]
                        if moe:
                            cs = comb[tt][:, e:e + 1]
                            if first:
                                kb.op("dve", lambda: nc.vector.tensor_scalar(out=ysl, in0=py[:, :], scalar1=cs,
                                                                             scalar2=None, op0=ALU.mult),
                                      R=[py, comb[tt]], W=[ya])
                            else:
                                kb.op("dve", lambda: nc.vector.scalar_tensor_tensor(out=ysl, in0=py[:, :], scalar=cs,
                                                                                    in1=ysl, op0=ALU.mult, op1=ALU.add),
                                      R=[py, comb[tt], ya], W=[ya])
                        else:
                            if first:
                                kb.op("dve", lambda: nc.vector.tensor_copy(out=ysl, in_=py[:, :]), R=[py], W=[ya])
                            else:
                                kb.op("dve", lambda: nc.vector.tensor_tensor(out=ysl, in0=py[:, :], in1=ysl, op=ALU.add),
                                      R=[py, ya], W=[ya])
                first = False
        for tt in range(4):
            tok0 = g * TG + tt * 128
            kb.op("dve", lambda: nc.vector.scalar_tensor_tensor(out=h[:, :], in0=x1g[tt][:, :], scalar=ALPHA,
                                                                in1=yacc[tt][:, :], op0=ALU.mult, op1=ALU.add),
                  R=[x1g[tt], yacc[tt]], W=[h])
            o = ost[tt % 2]
            layer_norm(kb, h, (lnp, lnp[:, 2, :]), (lnp, lnp[:, 3, :]), o[:, :], o, scr)
            kb.dma("sp", out_d[tok0:tok0 + 128, :], o[:, :], R=[o], W=[out_d])
    return kb.finish()


def ffn_chunk_layout(w1, w3, w2):
    F = w1.shape[1]
    nf = F // 128
    a = w1.reshape(8, 128, nf, 128).transpose(2, 1, 0, 3).reshape(nf, 128, 1024)
    b = w3.reshape(8, 128, nf, 128).transpose(2, 1, 0, 3).reshape(nf, 128, 1024)
    c = w2.reshape(nf, 128, 1024)
    return np.ascontiguousarray(np.concatenate([a, b, c], axis=2))


def run_C(o_flat_bf, x_flat, wo_bf, lnp4, wf, n_exp, nf_unit, n_units, wr=None):
    nc = build_C(n_exp, nf_unit, n_units)
    wol = w_kc_layout(wo_bf)
    lnb = np.ascontiguousarray(np.broadcast_to(lnp4[None, :, :], (128, 4, D))).astype(np.float32)
    maps = []
    for c in range(NCORES):
        oc = o_flat_bf[c * TPC:(c + 1) * TPC]
        oT = np.ascontiguousarray(oc.reshape(TPC, 8, 128).transpose(2, 1, 0))
        m = {"oT": oT, "x": np.ascontiguousarray(x_flat[c * TPC:(c + 1) * TPC]), "wo": wol, "lnp": lnb,
             "wf": wf, "ident": IDENT}
        if wr is not None:
            m["wr"] = np.ascontiguousarray(wr.reshape(8, 128, 8).transpose(1, 0, 2))
        maps.append(m)
    res = run_spmd(nc, maps)
    return np.concatenate([res[c]["out"] for c in range(NCORES)], axis=0)


from concourse.bass_types import AP as _AP

NQB = SEQ // 128
NDEL = 2304


def rel_bucket_np(d):
    d = np.maximum(d, 0)
    df = np.maximum(d, 1).astype(np.float32)
    large = 16 + (np.log(df / np.float32(16.0)).astype(np.float32) / np.float32(math.log(2048 / 16))
                  * np.float32(16)).astype(np.int32)
    large = np.minimum(large, 31)
    return np.where(d < 16, d, large)


def dil_const():
    C = np.zeros((32, NDEL), np.float32)
    for idx in range(NDEL):
        dl = idx - 127
        if dl < 0 or dl > 2048:
            continue
        mult = 0
        for (w, dd) in ((128, 1), (512, 4), (2048, 16)):
            if dl <= w and dl % dd == 0:
                mult += 1
        if mult:
            C[int(rel_bucket_np(np.array([dl]))[0]), idx] = mult
    return C


class AttnCtx:
    def __init__(self, kb, n_s=4, n_o=2, grouped=False, fox=False):
        self.kb = kb
        self.S = [kb.ps([128, 512], F32, "S") for _ in range(n_s)]
        self.O = [kb.ps([128, 512], F32, "O") for _ in range(n_o)]
        self.P32 = [kb.sb([128, 128], F32, "P32") for _ in range(4)]
        self.Pb = [kb.sb([128, 128], BF16, "Pb") for _ in range(6)]
        self.pending = []
        self.LA = 3
        self.LAG = 2
        self.cpg = 0
        self.cpf = 0
        if grouped:
            self.P32g = [kb.sb([128, 512], F32, "P32g") for _ in range(3)]
            self.Pbg = [kb.sb([128, 512], BF16, "Pbg") for _ in range(4)]
        if fox:
            self.P32f = [kb.sb([128, 256], F32, "P32f") for _ in range(3)]
            self.Pbf = [kb.sb([128, 256], BF16, "Pbf") for _ in range(6)]
        self.rc = [kb.sb([128, 1], F32, "rc") for _ in range(2)]
        self.cs = 0
        self.co = 0
        self.cp = 0


def attn_qblock(ax, qT, kT, Vaug, h, i, jlist, bias_of, mask_of, ost, ocol, after=None):
    kb = ax.kb
    nc = kb.nc
    O = ax.O[ax.co % len(ax.O)]
    rc = ax.rc[ax.co % 2]
    ax.co += 1
    hp = slice(h * 64, (h + 1) * 64)
    nj = len(jlist)
    for n, j in enumerate(jlist):
        S = ax.S[ax.cs % len(ax.S)]
        ax.cs += 1
        kb.op("pe", lambda: nc.tensor.matmul(S[:, :128], lhsT=kT[hp, j * 128:(j + 1) * 128],
                                             rhs=qT[hp, i * 128:(i + 1) * 128], start=True, stop=True),
              R=[kT, qT], W=[S])
        Pb = ax.Pb[ax.cp % len(ax.Pb)]
        b = bias_of(j) if bias_of is not None else None
        m = mask_of(j) if mask_of is not None else None
        if m is not None and not isinstance(m, list):
            m = [m]
        if m is not None and len(m) == 0:
            m = None
        tgt = Pb if m is None else ax.P32[ax.cp % len(ax.P32)]
        ax.cp += 1
        if b is None:
            kb.op("act", lambda: nc.scalar.activation(out=tgt[:, :], in_=S[:, :128], func=AF.Exp, scale=0.125),
                  R=[S], W=[tgt])
        else:
            kb.op("act", lambda: nc.scalar.activation(out=tgt[:, :], in_=S[:, :128], func=AF.Exp, bias=b[1],
                                                      scale=0.125), R=[S, b[0]], W=[tgt])
        if m is not None:
            for mm in m[:-1]:
                kb.op("pool", lambda: nc.gpsimd.tensor_tensor(out=tgt[:, :], in0=tgt[:, :], in1=mm[1], op=ALU.mult),
                      R=[tgt, mm[0]], W=[tgt])
            kb.op("dve", lambda: nc.vector.tensor_tensor(out=Pb[:, :], in0=tgt[:, :], in1=m[-1][1], op=ALU.mult),
                  R=[tgt, m[-1][0]], W=[Pb])

        def pv(Pb=Pb, j=j, n=n):
            kb.op("pe", lambda: nc.tensor.matmul(O[:, :65], lhsT=Pb[:, :], rhs=Vaug[:, j, h, :],
                                                 start=(n == 0), stop=(n == nj - 1)), R=[Pb, Vaug], W=[O])
            if n == nj - 1:
                kb.op("dve", lambda: nc.vector.reciprocal(out=rc[:, :], in_=O[:, 64:65]), R=[O], W=[rc])
                kb.op("dve", lambda: nc.vector.tensor_scalar(out=ost[:, ocol:ocol + 64], in0=O[:, 0:64],
                                                             scalar1=rc[:, 0:1], scalar2=None, op0=ALU.mult),
                      R=[O, rc], W=[ost])
                if after is not None:
                    after()

        ax.pending.append(pv)
        while len(ax.pending) > ax.LA:
            ax.pending.pop(0)()


def attn_qgroup(ax, qT, kT, Vaug, h, i, groups, ost, ocol, after=None):
    kb = ax.kb
    nc = kb.nc
    O = ax.O[ax.co % len(ax.O)]
    rc = ax.rc[ax.co % 2]
    ax.co += 1
    hp = slice(h * 64, (h + 1) * 64)
    ng = len(groups)
    for gi, g in enumerate(groups):
        js = g["js"]
        wd = 128 * len(js)
        S = ax.S[ax.cs % len(ax.S)]
        ax.cs += 1
        for n, j in enumerate(js):
            kb.op("pe", lambda: nc.tensor.matmul(S[:, n * 128:(n + 1) * 128], lhsT=kT[hp, j * 128:(j + 1) * 128],
                                                 rhs=qT[hp, i * 128:(i + 1) * 128], start=True, stop=True),
                  R=[kT, qT], W=[S])
        Pb = ax.Pbg[ax.cpg % len(ax.Pbg)]
        has_mask = (g.get("pool_masks") is not None) or (g.get("dve_mask") is not None)
        tgt = ax.P32g[ax.cpg % len(ax.P32g)] if has_mask else Pb
        ax.cpg += 1
        b = g.get("bias")
        if b is None:
            kb.op("act", lambda: nc.scalar.activation(out=tgt[:, :wd], in_=S[:, :wd], func=AF.Exp, scale=0.125),
                  R=[S], W=[tgt])
        else:
            kb.op("act", lambda: nc.scalar.activation(out=tgt[:, :wd], in_=S[:, :wd], func=AF.Exp, bias=b[1],
                                                      scale=0.125), R=[S, b[0]], W=[tgt])
        if has_mask:
            pm = g.get("pool_masks")
            dm = g.get("dve_mask")
            if pm is not None:
                for n, mm in enumerate(pm):
                    last = (dm is None) and False
                    kb.op("pool", lambda: nc.gpsimd.tensor_tensor(out=tgt[:, n * 128:(n + 1) * 128],
                                                                  in0=tgt[:, n * 128:(n + 1) * 128], in1=mm[1],
                                                                  op=ALU.mult), R=[tgt, mm[0]], W=[tgt])
            if dm is not None:
                kb.op("dve", lambda: nc.vector.tensor_tensor(out=Pb[:, :wd], in0=tgt[:, :wd], in1=dm[1], op=ALU.mult),
                      R=[tgt, dm[0]], W=[Pb])
            else:
                kb.op("dve", lambda: nc.vector.tensor_copy(out=Pb[:, :wd], in_=tgt[:, :wd]), R=[tgt], W=[Pb])

        def pv(Pb=Pb, js=js, gi=gi):
            for n, j in enumerate(js):
                kb.op("pe", lambda: nc.tensor.matmul(O[:, :65], lhsT=Pb[:, n * 128:(n + 1) * 128], rhs=Vaug[:, j, h, :],
                                                     start=(gi == 0 and n == 0),
                                                     stop=(gi == ng - 1 and n == len(js) - 1)), R=[Pb, Vaug], W=[O])
            if gi == ng - 1:
                kb.op("dve", lambda: nc.vector.reciprocal(out=rc[:, :], in_=O[:, 64:65]), R=[O], W=[rc])
                kb.op("dve", lambda: nc.vector.tensor_scalar(out=ost[:, ocol:ocol + 64], in0=O[:, 0:64],
                                                             scalar1=rc[:, 0:1], scalar2=None, op0=ALU.mult),
                      R=[O, rc], W=[ost])
                if after is not None:
                    after()

        ax.pending.append(pv)
        while len(ax.pending) > ax.LAG:
            ax.pending.pop(0)()


def attn_fox_pair(ax, qT, kT, Vaug, h, i0, B, tri2, osts, ocol, after=None):
    kb = ax.kb
    nc = kb.nc
    i1 = i0 + 1
    O0, O1 = ax.O[0], ax.O[1]
    rc0, rc1 = ax.rc[0], ax.rc[1]
    hp = slice(h * 64, (h + 1) * 64)
    for j in range(0, i1 + 1):
        wide = (j <= i0)
        wd = 256 if wide else 128
        q0 = i0 * 128 if wide else i1 * 128
        S = ax.S[ax.cs % len(ax.S)]
        ax.cs += 1
        kb.op("pe", lambda: nc.tensor.matmul(S[:, :wd], lhsT=kT[hp, j * 128:(j + 1) * 128],
                                             rhs=qT[hp, q0:q0 + wd], start=True, stop=True), R=[kT, qT], W=[S])
        Pb = ax.Pbf[ax.cpf % len(ax.Pbf)]
        diag = (j >= i0)
        tgt = ax.P32f[ax.cpf % len(ax.P32f)] if diag else Pb
        ax.cpf += 1
        kb.op("act", lambda: nc.scalar.activation(out=tgt[:, :wd], in_=S[:, :wd], func=AF.Exp, bias=B[:, j:j + 1],
                                                  scale=0.125), R=[S, B], W=[tgt])
        if diag:
            kb.op("dve", lambda: nc.vector.tensor_tensor(out=Pb[:, :wd], in0=tgt[:, :wd], in1=tri2[:, :wd], op=ALU.mult),
                  R=[tgt, tri2], W=[Pb])

        def pv(Pb=Pb, j=j, wide=wide):
            if wide:
                kb.op("pe", lambda: nc.tensor.matmul(O0[:, :65], lhsT=Pb[:, 0:128], rhs=Vaug[:, j, h, :],
                                                     start=(j == 0), stop=(j == i0)), R=[Pb, Vaug], W=[O0])
                kb.op("pe", lambda: nc.tensor.matmul(O1[:, :65], lhsT=Pb[:, 128:256], rhs=Vaug[:, j, h, :],
                                                     start=(j == 0), stop=False), R=[Pb, Vaug], W=[O1])
            else:
                kb.op("pe", lambda: nc.tensor.matmul(O1[:, :65], lhsT=Pb[:, 0:128], rhs=Vaug[:, j, h, :],
                                                     start=False, stop=True), R=[Pb, Vaug], W=[O1])
                for (O, rc, ost) in ((O0, rc0, osts[0]), (O1, rc1, osts[1])):
                    kb.op("dve", lambda: nc.vector.reciprocal(out=rc[:, :], in_=O[:, 64:65]), R=[O], W=[rc])
                    kb.op("dve", lambda: nc.vector.tensor_scalar(out=ost[:, ocol:ocol + 64], in0=O[:, 0:64],
                                                                 scalar1=rc[:, 0:1], scalar2=None, op0=ALU.mult),
                          R=[O, rc], W=[ost])
                if after is not None:
                    after()

        ax.pending.append(pv)
        while len(ax.pending) > ax.LA:
            ax.pending.pop(0)()


def attn_flush(ax):
    while ax.pending:
        ax.pending.pop(0)()


def load_vaug(kb, v_d, name):
    nc = kb.nc
    Vaug = kb.sb([128, NQB, 2, 65], BF16, name)
    kb.op("pool", lambda: nc.gpsimd.memset(Vaug[:, :, :, :], 1.0), W=[Vaug])
    for c in range(4):
        js = slice(c * 16, (c + 1) * 16)
        kb.dma("sp" if c % 2 == 0 else "pool", Vaug[:, js, :, 0:64],
               v_d[:, js, :].rearrange("p j (h d) -> p j h d", h=2), R=[v_d], W=[Vaug])
    return Vaug


def build_B_cd():
    kb = KB()
    nc = kb.nc
    qc_d = kb.dram_in("qc", [128, SEQ], BF16)
    kc_d = kb.dram_in("kc", [128, SEQ], BF16)
    vc_d = kb.dram_in("vc", [128, NQB, 128], BF16)
    qd_d = kb.dram_in("qd", [128, SEQ], BF16)
    kd_d = kb.dram_in("kd", [128, SEQ], BF16)
    vd_d = kb.dram_in("vd", [128, NQB, 128], BF16)
    fc_d = kb.dram_in("fc", [128, NQB, 2], F32)
    bf_d = kb.dram_in("bf", [128, 2], F32)
    tri_d = kb.dram_in("tri", [128, 128], F32)
    rel_d = kb.dram_in("rel", [32, 2], F32)
    C_d = kb.dram_in("dilc", [32, NDEL], F32)
    oc_d = kb.dram_out("oc", [SEQ, 128], BF16)
    od_d = kb.dram_out("od", [SEQ, 128], BF16)
    E_d = kb.dram_tmp("Escr", [2, NDEL], F32)

    tri = kb.sb([128, 128], F32, "tri")
    ones = kb.sb([128, 128], F32, "ones")
    kb.dma("sp", tri[:, :], tri_d[:, :], R=[tri_d], W=[tri])
    kb.op("dve", lambda: nc.vector.memset(ones[:, :], 1.0), W=[ones])
    ax = AttnCtx(kb)
    rel = kb.sb([32, 2], F32, "rel")
    Cs = kb.sb([32, NDEL], F32, "Cs")
    Es = kb.sb([2, NDEL], F32, "Es")
    kb.dma("sp", rel[:, :], rel_d[:, :], R=[rel_d], W=[rel])
    kb.dma("sp", Cs[:, :], C_d[:, :], R=[C_d], W=[Cs])
    kb.op("act", lambda: nc.scalar.activation(out=rel[:, :], in_=rel[:, :], func=AF.Exp), R=[rel], W=[rel])
    for c in range((NDEL + 511) // 512):
        w = min(512, NDEL - c * 512)
        pp = ax.S[c % 3]
        kb.op("pe", lambda: nc.tensor.matmul(pp[:2, :w], lhsT=rel[:, :], rhs=Cs[:, c * 512:c * 512 + w],
                                             start=True, stop=True), R=[rel, Cs], W=[pp])
        kb.op("dve", lambda: nc.vector.tensor_copy(out=Es[:, c * 512:c * 512 + w], in_=pp[:2, :w]), R=[pp], W=[Es])
    kb.dma("sp", E_d[:, :], Es[:, :], R=[Es], W=[E_d])
    TT = [kb.sb([128, 17 * 128], F32, "TT") for _ in range(2)]
    for h in range(2):
        src = _AP(tensor=E_d.t.tensor, offset=h * NDEL, ap=[[1, 128], [1, 17 * 128]])
        kb.dma("sp", TT[h][:, :], src, R=[E_d], W=[TT[h]])
    fc = kb.sb([128, NQB, 2], F32, "fc")
    bf = kb.sb([128, 2], F32, "bf")
    lf = kb.sb([128, 2, NQB], F32, "lf")
    kb.dma("sp", fc[:, :, :], fc_d[:, :, :], R=[fc_d], W=[fc])
    kb.dma("sp", bf[:, :], bf_d[:, :], R=[bf_d], W=[bf])
    for h in range(2):
        kb.op("dve", lambda: nc.vector.tensor_scalar(out=lf[:, h, :], in0=fc[:, :, h], scalar1=bf[:, h:h + 1],
                                                     scalar2=None, op0=ALU.add), R=[fc, bf], W=[lf])
    kb.op("act", lambda: nc.scalar.activation(out=lf[:, :, :], in_=lf[:, :, :], func=AF.Exp, scale=-1.0), R=[lf], W=[lf])
    kb.op("act", lambda: nc.scalar.activation(out=lf[:, :, :], in_=lf[:, :, :], func=AF.Ln, bias=1.0), R=[lf], W=[lf])
    kb.op("dve", lambda: nc.vector.tensor_scalar(out=lf[:, :, :], in0=lf[:, :, :], scalar1=-1.0, scalar2=None,
                                                 op0=ALU.mult), R=[lf], W=[lf])
    lf2 = lf[:, :, :].rearrange("p h j -> p (h j)")
    p1, p2 = ax.O[0], ax.O[1]
    kb.op("pe", lambda: nc.tensor.matmul(p1[:, :128], lhsT=tri[:, :], rhs=lf2, start=True, stop=True),
          R=[tri, lf], W=[p1])
    kb.op("pe", lambda: nc.tensor.matmul(p2[:, :128], lhsT=ones[:, :], rhs=lf2, start=True, stop=True),
          R=[ones, lf], W=[p2])
    tot = kb.sb([128, 2, NQB], F32, "tot")
    carry = kb.sb([128, 2, NQB], F32, "carry")
    negF = kb.sb([128, 2, NQB], F32, "negF")
    kb.op("dve", lambda: nc.vector.tensor_copy(out=tot[:, :, :].rearrange("p h j -> p (h j)"), in_=p2[:, :128]),
          R=[p2], W=[tot])
    for h in range(2):
        kb.op("dve", lambda: nc.vector.tensor_tensor_scan(out=carry[:, h, :], data0=ones[:, :NQB], data1=tot[:, h, :],
                                                          initial=0.0, op0=ALU.mult, op1=ALU.add),
              R=[ones, tot], W=[carry])
    kb.op("dve", lambda: nc.vector.tensor_tensor(out=carry[:, :, :], in0=carry[:, :, :], in1=tot[:, :, :],
                                                 op=ALU.subtract), R=[carry, tot], W=[carry])
    kb.op("dve", lambda: nc.vector.tensor_tensor(out=negF[:, :, :].rearrange("p h j -> p (h j)"), in0=p1[:, :128],
                                                 in1=carry[:, :, :].rearrange("p h j -> p (h j)"), op=ALU.add),
          R=[p1, carry], W=[negF])
    kb.op("dve", lambda: nc.vector.tensor_scalar(out=negF[:, :, :], in0=negF[:, :, :], scalar1=-1.0, scalar2=None,
                                                 op0=ALU.mult), R=[negF], W=[negF])
    qc = kb.sb([128, SEQ], BF16, "qc")
    kc = kb.sb([128, SEQ], BF16, "kc")
    qd = kb.sb([128, SEQ], BF16, "qd")
    kd = kb.sb([128, SEQ], BF16, "kd")
    for n, (s, d_) in enumerate(((qd, qd_d), (kd, kd_d), (qc, qc_d), (kc, kc_d))):
        for c in range(2):
            kb.dma("sp" if (n + c) % 2 == 0 else "pool", s[:, c * 4096:(c + 1) * 4096], d_[:, c * 4096:(c + 1) * 4096],
                   R=[d_], W=[s])
    Vd = load_vaug(kb, vd_d, "Vd")
    Vc = load_vaug(kb, vc_d, "Vc")
    ostd = [kb.sb([128, 128], BF16, "ostd") for _ in range(2)]
    ostc = [kb.sb([128, 128], BF16, "ostc") for _ in range(2)]
    Bi = [kb.sb([128, NQB], F32, "Bi") for _ in range(3)]
    nb = 0
    for i in range(NQB):
        od = ostd[i % 2]
        for h in range(2):
            j0 = max(0, i - 16)
            attn_qblock(ax, qd, kd, Vd, h, i, list(range(j0, i + 1)), None,
                        lambda j, h=h, i=i: (TT[h], TT[h][:, (i - j) * 128:(i - j + 1) * 128]), od, h * 64,
                        after=(None if h == 0 else
                               (lambda i=i, od=od: kb.dma("pool", od_d[i * 128:(i + 1) * 128, :], od[:, :],
                                                          R=[od], W=[od_d]))))
        oc = ostc[i % 2]
        for h in range(2):
            B = Bi[nb % 3]
            nb += 1
            kb.op("dve", lambda: nc.vector.tensor_scalar(out=B[:, :i + 1], in0=negF[:, h, :i + 1],
                                                         scalar1=carry[:, h, i:i + 1], scalar2=None, op0=ALU.add),
                  R=[negF, carry], W=[B])
            attn_qblock(ax, qc, kc, Vc, h, i, list(range(0, i + 1)),
                        lambda j, B=B: (B, B[:, j:j + 1]),
                        lambda j, i=i: ((tri, tri[:, :]) if j == i else None), oc, h * 64,
                        after=(None if h == 0 else
                               (lambda i=i, oc=oc: kb.dma("sp", oc_d[i * 128:(i + 1) * 128, :], oc[:, :],
                                                          R=[oc], W=[oc_d]))))
    attn_flush(ax)
    return kb.finish()


TRI = np.triu(np.ones((128, 128), np.float32))


def to_pj(a):
    n = a.shape[1]
    return np.ascontiguousarray(a.reshape(NQB, 128, n).transpose(1, 0, 2))


def run_B_cd(yT, ytb, ytf, b_forget, rel_table):
    nc = build_B_cd()
    C = dil_const()
    maps = []
    for c in range(NCORES):
        b, m = c // 4, c % 4
        ts = slice(b * SEQ, (b + 1) * SEQ)
        rs = lambda base: slice(base + m * 128, base + (m + 1) * 128)
        maps.append({
            "qc": np.ascontiguousarray(yT[rs(0), ts]), "kc": np.ascontiguousarray(yT[rs(512), ts]),
            "qd": np.ascontiguousarray(yT[rs(1024), ts]),
            "kd": np.ascontiguousarray(yT[rs(1536), ts].reshape(128, NQB, 128)[:, :, ::-1].reshape(128, SEQ)),
            "vc": to_pj(ytb[ts, m * 128:(m + 1) * 128]), "vd": np.ascontiguousarray(to_pj(ytb[ts, 512 + m * 128:512 + (m + 1) * 128])[::-1]),
            "fc": to_pj(ytf[ts, 2 * m:2 * m + 2]),
            "bf": np.ascontiguousarray(np.broadcast_to(b_forget[None, 2 * m:2 * m + 2], (128, 2))).astype(np.float32),
            "tri": TRI, "rel": np.ascontiguousarray(rel_table[:, 2 * m:2 * m + 2]), "dilc": C,
        })
    res = run_spmd(nc, maps)
    o = np.zeros((BATCH * SEQ, D), NPBF)
    for c in range(NCORES):
        b, m = c // 4, c % 4
        o[b * SEQ:(b + 1) * SEQ, m * 128:(m + 1) * 128] = res[c]["oc"]
        o[b * SEQ:(b + 1) * SEQ, 512 + m * 128:512 + (m + 1) * 128] = res[c]["od"]
    return o


def build_B_gla():
    kb = KB()
    nc = kb.nc
    qT_d = kb.dram_in("qT", [64, SEQ], BF16)
    kT_d = kb.dram_in("kT", [64, SEQ], BF16)
    k_d = kb.dram_in("k", [128, NQB, 64], BF16)
    v_d = kb.dram_in("v", [128, NQB, 128], BF16)
    r_d = kb.dram_in("r", [128, NQB, 128], F32)
    g_d = kb.dram_in("g", [128, NQB, 64], F32)
    gn_d = kb.dram_in("gn", [128, 128], F32)
    tri_d = kb.dram_in("tri", [128, 128], F32)
    o_d = kb.dram_out("o", [SEQ, 128], BF16)
    qT = kb.sb([64, SEQ], BF16, "qT")
    kT = kb.sb([64, SEQ], BF16, "kT")
    ktm = kb.sb([128, NQB, 64], BF16, "ktm")
    v = kb.sb([128, NQB, 128], BF16, "v")
    r = kb.sb([128, NQB, 128], F32, "r")
    g = kb.sb([128, NQB, 64], F32, "g")
    gn = kb.sb([128, 128], F32, "gn")
    tri = kb.sb([128, 128], F32, "tri")
    eps = kb.sb([128, 1], F32, "eps")
    kb.op("dve", lambda: nc.vector.memset(eps[:, :], LN_EPS), W=[eps])
    kb.dma("sp", tri[:, :], tri_d[:, :], R=[tri_d], W=[tri])
    kb.dma("sp", g[:, :, :], g_d[:, :, :], R=[g_d], W=[g])
    kb.dma("pool", qT[:, :], qT_d[:, :], R=[qT_d], W=[qT])
    kb.dma("sp", kT[:, :], kT_d[:, :], R=[kT_d], W=[kT])
    kb.dma("pool", ktm[:, :, :], k_d[:, :, :], R=[k_d], W=[ktm])
    kb.dma("sp", v[:, :, :], v_d[:, :, :], R=[v_d], W=[v])
    kb.dma("pool", r[:, :, :], r_d[:, :, :], R=[r_d], W=[r])
    kb.dma("sp", gn[:, :], gn_d[:, :], R=[gn_d], W=[gn])
    kb.op("act", lambda: nc.scalar.activation(out=r[:, :, :], in_=r[:, :, :], func=AF.Silu), R=[r], W=[r])
    PG = [kb.ps([128, 512], F32, "PG") for _ in range(2)]
    PGT = [kb.ps([128, 512], F32, "PGT") for _ in range(2)]
    PA = [kb.ps([128, 512], F32, "PA") for _ in range(2)]
    PO = kb.ps([128, 512], F32, "PO")
    PU = kb.ps([128, 512], F32, "PU")
    eGT = [kb.sb([64, 128], F32, "eGT") for _ in range(2)]
    enGT = [kb.sb([64, 128], F32, "enGT") for _ in range(2)]
    enG = [kb.sb([128, 64], F32, "enG") for _ in range(2)]
    qgT = [kb.sb([64, 128], BF16, "qgT") for _ in range(2)]
    kgT = [kb.sb([64, 128], BF16, "kgT") for _ in range(2)]
    kg = [kb.sb([128, 64], BF16, "kg") for _ in range(2)]
    Am = [kb.sb([128, 128], BF16, "Am") for _ in range(2)]
    S32 = kb.sb([64, 128], F32, "S32")
    Sbf = kb.sb([64, 128], BF16, "Sbf")
    st6 = [kb.sb([128, 6], F32, "st6") for _ in range(2)]
    mv = [kb.sb([128, 4], F32, "mv") for _ in range(2)]
    of = [kb.sb([128, 128], F32, "of") for _ in range(2)]
    ost = [kb.sb([128, 128], BF16, "ost") for _ in range(2)]
    for c in range(NQB):
        p = c % 2
        cs = slice(c * 128, (c + 1) * 128)
        kb.op("pe", lambda: nc.tensor.matmul(PG[p][:, :64], lhsT=tri[:, :], rhs=g[:, c, :], start=True, stop=True),
              R=[tri, g], W=[PG[p]])
        kb.op("pe", lambda: nc.tensor.matmul(PGT[p][:64, :128], lhsT=g[:, c, :], rhs=tri[:, :], start=True, stop=True),
              R=[tri, g], W=[PGT[p]])
        kb.op("act", lambda: nc.scalar.activation(out=eGT[p][:, :], in_=PGT[p][:64, :128], func=AF.Exp),
              R=[PGT[p]], W=[eGT[p]])
        kb.op("act", lambda: nc.scalar.activation(out=enGT[p][:, :], in_=PGT[p][:64, :128], func=AF.Exp, scale=-1.0),
              R=[PGT[p]], W=[enGT[p]])
        kb.op("act", lambda: nc.scalar.activation(out=enG[p][:, :], in_=PG[p][:, :64], func=AF.Exp, scale=-1.0),
              R=[PG[p]], W=[enG[p]])
        kb.op("dve", lambda: nc.vector.scalar_tensor_tensor(out=qgT[p][:, :], in0=qT[:, cs], scalar=0.125,
                                                            in1=eGT[p][:, :], op0=ALU.mult, op1=ALU.mult),
              R=[qT, eGT[p]], W=[qgT[p]])
        kb.op("dve", lambda: nc.vector.tensor_tensor(out=kgT[p][:, :], in0=kT[:, cs], in1=enGT[p][:, :], op=ALU.mult),
              R=[kT, enGT[p]], W=[kgT[p]])
        kb.op("dve", lambda: nc.vector.tensor_tensor(out=kg[p][:, :], in0=ktm[:, c, :], in1=enG[p][:, :], op=ALU.mult),
              R=[ktm, enG[p]], W=[kg[p]])
        kb.op("pe", lambda: nc.tensor.matmul(PA[p][:, :128], lhsT=kgT[p][:, :], rhs=qgT[p][:, :], start=True, stop=True),
              R=[kgT[p], qgT[p]], W=[PA[p]])
        kb.op("dve", lambda: nc.vector.tensor_tensor(out=Am[p][:, :], in0=PA[p][:, :128], in1=tri[:, :], op=ALU.mult),
              R=[PA[p], tri], W=[Am[p]])
        kb.op("pe", lambda: nc.tensor.matmul(PO[:, :128], lhsT=Am[p][:, :], rhs=v[:, c, :], start=True, stop=(c == 0)),
              R=[Am[p], v], W=[PO])
        if c > 0:
            kb.op("pe", lambda: nc.tensor.matmul(PO[:, :128], lhsT=qgT[p][:, :], rhs=Sbf[:, :], start=False, stop=True),
                  R=[qgT[p], Sbf], W=[PO])
        if c < NQB - 1:
            kb.op("pe", lambda: nc.tensor.matmul(PU[:64, :128], lhsT=kg[p][:, :], rhs=v[:, c, :], start=True, stop=True),
                  R=[kg[p], v], W=[PU])
            eGl = eGT[p][:, 127:128]
            if c == 0:
                kb.op("dve", lambda: nc.vector.tensor_scalar(out=S32[:, :], in0=PU[:64, :128], scalar1=eGl, scalar2=None,
                                                             op0=ALU.mult), R=[PU, eGT[p]], W=[S32])
            else:
                kb.op("dve", lambda: nc.vector.tensor_scalar(out=S32[:, :], in0=S32[:, :], scalar1=eGl, scalar2=None,
                                                             op0=ALU.mult), R=[S32, eGT[p]], W=[S32])
                kb.op("dve", lambda: nc.vector.scalar_tensor_tensor(out=S32[:, :], in0=PU[:64, :128], scalar=eGl,
                                                                    in1=S32[:, :], op0=ALU.mult, op1=ALU.add),
                      R=[PU, eGT[p], S32], W=[S32])
            kb.op("dve", lambda: nc.vector.tensor_copy(out=Sbf[:, :], in_=S32[:, :]), R=[S32], W=[Sbf])
        kb.op("dve", lambda: nc.vector.bn_stats(out=st6[p][:, :], in_=PO[:, :128]), R=[PO], W=[st6[p]])
        kb.op("dve", lambda: nc.vector.bn_aggr(out=mv[p][:, 0:2], in_=st6[p][:, :]), R=[st6[p]], W=[mv[p]])
        kb.op("dve", lambda: nc.vector.scalar_tensor_tensor(out=mv[p][:, 2:3], in0=mv[p][:, 0:1], scalar=mv[p][:, 0:1],
                                                            in1=mv[p][:, 1:2], op0=ALU.mult, op1=ALU.add),
              R=[mv[p]], W=[mv[p]])
        kb.op("act", lambda: nc.scalar.activation(out=mv[p][:, 3:4], in_=mv[p][:, 2:3], func=AF.Ln, bias=eps[:, 0:1]),
              R=[mv[p], eps], W=[mv[p]])
        kb.op("act", lambda: nc.scalar.activation(out=mv[p][:, 3:4], in_=mv[p][:, 3:4], func=AF.Exp, scale=-0.5),
              R=[mv[p]], W=[mv[p]])
        kb.op("dve", lambda: nc.vector.scalar_tensor_tensor(out=of[p][:, :], in0=PO[:, :128], scalar=mv[p][:, 3:4],
                                                            in1=gn[:, :], op0=ALU.mult, op1=ALU.mult),
              R=[PO, mv[p], gn], W=[of[p]])
        kb.op("pool", lambda: nc.gpsimd.tensor_tensor(out=ost[p][:, :], in0=of[p][:, :], in1=r[:, c, :], op=ALU.mult),
              R=[of[p], r], W=[ost[p]])
        kb.dma("sp", o_d[cs, :], ost[p][:, :], R=[ost[p]], W=[o_d])
    return kb.finish()


def run_B_gla(yT, ytb, ytf, g_norm):
    nc = build_B_gla()
    maps = []
    for c in range(NCORES):
        b, h = c // 4, c % 4
        ts = slice(b * SEQ, (b + 1) * SEQ)
        maps.append({
            "qT": np.ascontiguousarray(yT[h * 64:(h + 1) * 64, ts]),
            "kT": np.ascontiguousarray(yT[256 + h * 64:256 + (h + 1) * 64, ts]),
            "k": to_pj(ytb[ts, h * 64:(h + 1) * 64]),
            "v": to_pj(ytb[ts, 256 + h * 128:256 + (h + 1) * 128]),
            "r": to_pj(ytf[ts, h * 128:(h + 1) * 128]),
            "g": to_pj(ytf[ts, 520 + h * 64:520 + (h + 1) * 64]),
            "gn": np.ascontiguousarray(np.broadcast_to(g_norm[None, :], (128, 128))).astype(np.float32),
            "tri": TRI,
        })
    res = run_spmd(nc, maps)
    o = np.zeros((BATCH * SEQ, 512), NPBF)
    for c in range(NCORES):
        b, h = c // 4, c % 4
        o[b * SEQ:(b + 1) * SEQ, h * 128:(h + 1) * 128] = res[c]["o"]
    return o


NBIS = 20
TOPK = 256


def build_B_dsa1(act_split=True):
    kb = KB()
    nc = kb.nc
    NK = 16
    qiT_d = kb.dram_in("qiT", [64, 8, NK * 128], BF16)
    kiT_d = kb.dram_in("kiT", [64, SEQ], BF16)
    wi_d = kb.dram_in("wi", [128, NK, 8], F32)
    cm_d = kb.dram_in("cmask", [128, 512], F32)
    idb_d = kb.dram_in("identb", [128, 128], BF16)
    stp_d = kb.dram_in("steps", [128, NBIS], F32)
    M_d = kb.dram_out("M", [NK, 128, SEQ], BF16)
    qiT = kb.sb([64, 8, NK * 128], BF16, "qiT")
    kiT = kb.sb([64, SEQ], BF16, "kiT")
    wi = kb.sb([128, NK, 8], F32, "wi")
    absw = kb.sb([128, NK, 8], F32, "absw")
    sgn = kb.sb([128, NK, 8], F32, "sgn")
    cm = kb.sb([128, 512], F32, "cm")
    idb = kb.sb([128, 128], BF16, "idb")
    stp = kb.sb([128, NBIS], F32, "stp")
    kb.dma("sp", qiT[:, :, :], qiT_d[:, :, :], R=[qiT_d], W=[qiT])
    kb.dma("pool", kiT[:, :], kiT_d[:, :], R=[kiT_d], W=[kiT])
    kb.dma("sp", wi[:, :, :], wi_d[:, :, :], R=[wi_d], W=[wi])
    kb.dma("sp", cm[:, :], cm_d[:, :], R=[cm_d], W=[cm])
    kb.dma("sp", idb[:, :], idb_d[:, :], R=[idb_d], W=[idb])
    kb.dma("sp", stp[:, :], stp_d[:, :], R=[stp_d], W=[stp])
    kb.op("act", lambda: nc.scalar.activation(out=absw[:, :, :], in_=wi[:, :, :], func=AF.Abs), R=[wi], W=[absw])
    kb.op("act", lambda: nc.scalar.activation(out=sgn[:, :, :], in_=wi[:, :, :], func=AF.Sign), R=[wi], W=[sgn])
    PS = [kb.ps([128, 512], F32, "PS") for _ in range(3)]
    PT = [kb.ps([128, 1024], BF16, "PT") for _ in range(2)]

    def write_mask(k, mt, Lk):
        kb.dma("sp", M_d[k, :, :Lk], mt[:, :Lk], R=[mt], W=[M_d])

    dsa1_body(kb, qiT, kiT, absw, sgn, cm, idb, stp, PS, PT, write_mask, act_split)
    return kb.finish()


def dsa1_body(kb, qiT, kiT, absw, sgn, cm, idb, stp, PS, PT, write_mask, act_split=True):
    nc = kb.nc
    NK = 16
    score = [kb.sb([128, SEQ], F32, "score") for _ in range(2)]
    selb = kb.sb([128, SEQ], BF16, "selb")
    junk2 = kb.sb([128, 5120], BF16, "junk2")
    MT = [kb.sb([128, SEQ], BF16, "MT") for _ in range(2)]
    rl = [kb.sb([128, 512], F32, "rl") for _ in range(3)]
    bs = [kb.sb([128, 16], F32, "bs") for _ in range(2)]
    stk = [kb.sb([128, NBIS], F32, "stk") for _ in range(2)]
    midb = [kb.sb([128, 1], F32, "midb") for _ in range(2)]
    cntd = [kb.sb([128, 1], F32, "cntd") for _ in range(2)]
    cnta = [kb.sb([128, 1], F32, "cnta") for _ in range(2)]
    tmpb = [kb.sb([128, 1], F32, "tmpb") for _ in range(2)]
    geb = [kb.sb([128, 1], F32, "geb") for _ in range(2)]
    st = {"rl": 0, "pt": 0}

    def scoring(k):
        sc_ = score[k % 2]
        for c in range(k + 1):
            for hi in range(8):
                pp = PS[st["rl"] % 3]
                r_ = rl[st["rl"] % 3]
                st["rl"] += 1
                kb.op("pe", lambda: nc.tensor.matmul(pp[:, :], lhsT=qiT[:, hi, k * 128:(k + 1) * 128],
                                                     rhs=kiT[:, c * 512:(c + 1) * 512], start=True, stop=True),
                      R=[qiT, kiT], W=[pp])
                kb.op("act", lambda: nc.scalar.activation(out=r_[:, :], in_=pp[:, :], func=AF.Relu,
                                                          scale=absw[:, k, hi:hi + 1]), R=[pp, absw], W=[r_])
                ssl = sc_[:, c * 512:(c + 1) * 512]
                if hi == 0:
                    kb.op("dve", lambda: nc.vector.tensor_scalar(out=ssl, in0=r_[:, :], scalar1=sgn[:, k, 0:1],
                                                                 scalar2=None, op0=ALU.mult), R=[r_, sgn], W=[sc_])
                else:
                    kb.op("dve", lambda: nc.vector.scalar_tensor_tensor(out=ssl, in0=r_[:, :],
                                                                        scalar=sgn[:, k, hi:hi + 1], in1=ssl,
                                                                        op0=ALU.mult, op1=ALU.add),
                          R=[r_, sgn, sc_], W=[sc_])

    def setup(k):
        L = 512 * (k + 1)
        sc_, b_, sk = score[k % 2], bs[k % 2], stk[k % 2]
        kb.op("dve", lambda: nc.vector.tensor_reduce(out=b_[:, 0:1], in_=sc_[:, :L], axis=AX.X, op=ALU.min),
              R=[sc_], W=[b_])
        kb.op("dve", lambda: nc.vector.tensor_reduce(out=b_[:, 1:2], in_=sc_[:, :L], axis=AX.X, op=ALU.max),
              R=[sc_], W=[b_])
        kb.op("dve", lambda: nc.vector.tensor_tensor(out=sc_[:, L - 512:L], in0=sc_[:, L - 512:L], in1=cm[:, :],
                                                     op=ALU.add), R=[sc_, cm], W=[sc_])
        kb.op("dve", lambda: nc.vector.tensor_tensor(out=b_[:, 2:3], in0=b_[:, 1:2], in1=b_[:, 0:1], op=ALU.subtract),
              R=[b_], W=[b_])
        kb.op("dve", lambda: nc.vector.tensor_scalar(out=sk[:, :], in0=stp[:, :], scalar1=b_[:, 2:3], scalar2=None,
                                                     op0=ALU.mult), R=[stp, b_], W=[sk])
        kb.op("dve", lambda: nc.vector.scalar_tensor_tensor(out=midb[k % 2][:, 0:1], in0=b_[:, 2:3], scalar=0.5,
                                                            in1=b_[:, 0:1], op0=ALU.mult, op1=ALU.add),
              R=[b_], W=[midb[k % 2]])

    def iteration(k, n):
        L = 512 * (k + 1)
        p = k % 2
        sc_, sk = score[p], stk[p]
        Ld = (L * 2 // 5) if act_split else L
        if act_split:
            kb.op("act", lambda: nc.scalar.activation(out=junk2[:, :L - Ld], in_=sc_[:, Ld:L], func=AF.Sign,
                                                      bias=midb[p][:, 0:1], scale=-1.0, accum_out=cnta[p][:, 0:1]),
                  R=[sc_, midb[p]], W=[junk2, cnta[p]])
        kb.op("dve", lambda: nc.vector.tensor_scalar(out=selb[:, :Ld], in0=sc_[:, :Ld], scalar1=midb[p][:, 0:1],
                                                     scalar2=0.0, op0=ALU.is_ge, op1=ALU.add,
                                                     accum_out=cntd[p][:, 0:1]),
              R=[sc_, midb[p]], W=[selb, cntd[p]])
        thr = TOPK - 0.25
        if act_split:
            kb.op("dve", lambda: nc.vector.scalar_tensor_tensor(out=tmpb[p][:, 0:1], in0=cnta[p][:, 0:1], scalar=-0.5,
                                                                in1=cntd[p][:, 0:1], op0=ALU.mult, op1=ALU.add),
                  R=[cnta[p], cntd[p]], W=[tmpb[p]])
            thr -= 0.5 * (L - Ld)
            src, srcb = tmpb[p][:, 0:1], tmpb[p]
        else:
            src, srcb = cntd[p][:, 0:1], cntd[p]
        kb.op("dve", lambda: nc.vector.tensor_scalar(out=geb[p][:, 0:1], in0=src, scalar1=thr, scalar2=-0.5,
                                                     op0=ALU.is_ge, op1=ALU.add), R=[srcb], W=[geb[p]])
        kb.op("dve", lambda: nc.vector.scalar_tensor_tensor(out=midb[p][:, 0:1], in0=geb[p][:, 0:1],
                                                            scalar=sk[:, n:n + 1], in1=midb[p][:, 0:1],
                                                            op0=ALU.mult, op1=ALU.add),
              R=[geb[p], sk, midb[p]], W=[midb[p]])

    def finish_block(k):
        L = 512 * (k + 1)
        sc_, b_, sk = score[k % 2], bs[k % 2], stk[k % 2]
        kb.op("dve", lambda: nc.vector.scalar_tensor_tensor(out=b_[:, 8:9], in0=sk[:, NBIS - 1:NBIS], scalar=-0.5,
                                                            in1=midb[k % 2][:, 0:1], op0=ALU.mult, op1=ALU.add),
              R=[midb[k % 2], sk], W=[b_])
        kb.op("dve", lambda: nc.vector.tensor_scalar(out=selb[:, :L], in0=sc_[:, :L], scalar1=b_[:, 8:9], scalar2=None,
                                                     op0=ALU.is_ge), R=[sc_, b_], W=[selb])
        mt = MT[k % 2]
        nkb = 4 * (k + 1)
        for j0 in range(0, nkb, 8):
            pt = PT[st["pt"] % 2]
            for jj in range(8):
                j = j0 + jj
                if j >= nkb:
                    break
                kb.op("pe", lambda: nc.tensor.transpose(pt[:, jj * 128:(jj + 1) * 128], selb[:, j * 128:(j + 1) * 128],
                                                        idb[:, :]), R=[selb, idb], W=[pt])
            wdt = min(8, nkb - j0) * 128
            if st["pt"] % 2 == 0:
                kb.op("act", lambda: nc.scalar.copy(out=mt[:, j0 * 128:j0 * 128 + wdt], in_=pt[:, :wdt]), R=[pt], W=[mt])
            else:
                kb.op("dve", lambda: nc.vector.tensor_copy(out=mt[:, j0 * 128:j0 * 128 + wdt], in_=pt[:, :wdt]),
                      R=[pt], W=[mt])
            st["pt"] += 1
        write_mask(k, mt, L)

    for k0 in range(0, NK, 2):
        ks = (k0, k0 + 1)
        for k in ks:
            scoring(k)
        for k in ks:
            setup(k)
        for n in range(NBIS):
            for k in ks:
                iteration(k, n)
        for k in ks:
            finish_block(k)


def run_B_dsa1(yT, ytf):
    nc = build_B_dsa1()
    steps = np.ascontiguousarray(np.broadcast_to((0.5 ** np.arange(1, NBIS + 1))[None, :], (128, NBIS))).astype(np.float32)
    maps = []
    for c in range(NCORES):
        b, m = c // 4, c % 4
        ts = slice(b * SEQ, (b + 1) * SEQ)
        tok = (np.arange(16)[:, None] * 512 + m * 128 + np.arange(128)[None, :]).reshape(-1) + b * SEQ
        qi = yT[1536:2048][:, tok].reshape(8, 64, 2048).transpose(1, 0, 2)
        wi = ytf[tok, 512:520].reshape(16, 128, 8).transpose(1, 0, 2)
        cm = np.where(np.arange(512)[None, :] <= (128 * m + np.arange(128))[:, None], 0.0, NEG).astype(np.float32)
        maps.append({"qiT": np.ascontiguousarray(qi), "kiT": np.ascontiguousarray(yT[2048:2112, ts]),
                     "wi": np.ascontiguousarray(wi), "cmask": cm, "identb": IDENT.astype(NPBF), "steps": steps})
    res = run_spmd(nc, maps)
    out = []
    for b in range(BATCH):
        M = np.zeros((NQB, 128, SEQ), NPBF)
        for m in range(4):
            M[m::4] = res[b * 4 + m]["M"]
        out.append(M)
    return out


def dsa_const():
    C = np.zeros((32, NDEL), np.float32)
    dl = np.arange(NDEL) - 127
    ok = dl >= 0
    C[rel_bucket_np(dl)[ok], np.arange(NDEL)[ok]] = 1.0
    return C


NPACK = NQB * (NQB + 1) // 2


def build_B_dsa2():
    kb = KB()
    nc = kb.nc
    q_d = kb.dram_in("q", [128, SEQ], BF16)
    k_d = kb.dram_in("k", [128, SEQ], BF16)
    v_d = kb.dram_in("v", [128, NQB, 128], BF16)
    rel_d = kb.dram_in("rel", [32, 2], F32)
    rf_d = kb.dram_in("relfar", [128, 2], F32)
    C_d = kb.dram_in("dsac", [32, NDEL], F32)
    M_d = kb.dram_in("Mp", [NPACK * 128 * 128], BF16)
    o_d = kb.dram_out("o", [SEQ, 128], BF16)
    E_d = kb.dram_tmp("Escr", [2, NDEL], F32)
    ax = AttnCtx(kb)
    rel = kb.sb([32, 2], F32, "rel")
    rf = kb.sb([128, 2], F32, "rf")
    Cs = kb.sb([32, NDEL], F32, "Cs")
    Es = kb.sb([2, NDEL], F32, "Es")
    kb.dma("sp", rel[:, :], rel_d[:, :], R=[rel_d], W=[rel])
    kb.dma("sp", rf[:, :], rf_d[:, :], R=[rf_d], W=[rf])
    kb.dma("sp", Cs[:, :], C_d[:, :], R=[C_d], W=[Cs])
    kb.op("act", lambda: nc.scalar.activation(out=rel[:, :], in_=rel[:, :], func=AF.Exp), R=[rel], W=[rel])
    for c in range((NDEL + 511) // 512):
        w = min(512, NDEL - c * 512)
        pp = ax.S[c % 3]
        kb.op("pe", lambda: nc.tensor.matmul(pp[:2, :w], lhsT=rel[:, :], rhs=Cs[:, c * 512:c * 512 + w],
                                             start=True, stop=True), R=[rel, Cs], W=[pp])
        kb.op("dve", lambda: nc.vector.tensor_copy(out=Es[:, c * 512:c * 512 + w], in_=pp[:2, :w]), R=[pp], W=[Es])
    kb.dma("sp", E_d[:, :], Es[:, :], R=[Es], W=[E_d])
    TT = [kb.sb([128, 17 * 128], F32, "TT") for _ in range(2)]
    for h in range(2):
        src = _AP(tensor=E_d.t.tensor, offset=h * NDEL, ap=[[1, 128], [1, 17 * 128]])
        kb.dma("sp", TT[h][:, :], src, R=[E_d], W=[TT[h]])
    q = kb.sb([128, SEQ], BF16, "q")
    k = kb.sb([128, SEQ], BF16, "k")
    for n, (s, d_) in enumerate(((q, q_d), (k, k_d))):
        for c in range(2):
            kb.dma("sp" if (n + c) % 2 == 0 else "pool", s[:, c * 4096:(c + 1) * 4096], d_[:, c * 4096:(c + 1) * 4096],
                   R=[d_], W=[s])
    V = load_vaug(kb, v_d, "V")
    Ms = [kb.sb([128, SEQ], BF16, "Ms") for _ in range(2)]
    ost = [kb.sb([128, 128], BF16, "ost") for _ in range(2)]
    for i in range(NQB):
        ms = Ms[i % 2]
        W_ = (i + 1) * 128
        off = (i * (i + 1) // 2) * 128 * 128
        src = _AP(tensor=M_d.t.tensor, offset=off, ap=[[W_, 128], [1, W_]])
        kb.dma("pool" if i % 2 else "sp", ms[:, :W_], src, R=[M_d], W=[ms])
        o = ost[i % 2]
        for h in range(2):
            def bias_of(j, h=h, i=i):
                return (rf, rf[:, h:h + 1]) if i - j > 16 else None

            def mask_of(j, h=h, i=i, ms=ms):
                sel = (ms, ms[:, j * 128:(j + 1) * 128])
                if i - j > 16:
                    return [sel]
                return [(TT[h], TT[h][:, (i - j) * 128:(i - j + 1) * 128]), sel]

            attn_qblock(ax, q, k, V, h, i, list(range(0, i + 1)), bias_of, mask_of, o, h * 64,
                        after=(None if h == 0 else
                               (lambda i=i, o=o: kb.dma("sp", o_d[i * 128:(i + 1) * 128, :], o[:, :], R=[o], W=[o_d]))))
    attn_flush(ax)
    return kb.finish()


def run_B_dsa2(yT, ytb, Msel, rel_table):
    nc = build_B_dsa2()
    C = dsa_const()
    packed = []
    for b in range(BATCH):
        M = Msel[b][:, ::-1, :]
        packed.append(np.concatenate([np.ascontiguousarray(M[i][:, :(i + 1) * 128]).reshape(-1) for i in range(NQB)]))
    maps = []
    for c in range(NCORES):
        b, m = c // 4, c % 4
        ts = slice(b * SEQ, (b + 1) * SEQ)
        maps.append({
            "q": np.ascontiguousarray(yT[512 + m * 128:512 + (m + 1) * 128, ts]),
            "k": np.ascontiguousarray(yT[1024 + m * 128:1024 + (m + 1) * 128, ts].reshape(128, NQB, 128)[:, :, ::-1]
                                      .reshape(128, SEQ)),
            "v": np.ascontiguousarray(to_pj(ytb[ts, 768 + m * 128:768 + (m + 1) * 128])[::-1]),
            "rel": np.ascontiguousarray(rel_table[:, 2 * m:2 * m + 2]),
            "relfar": np.ascontiguousarray(np.broadcast_to(rel_table[31:32, 2 * m:2 * m + 2], (128, 2))).astype(np.float32),
            "dsac": C, "Mp": packed[b],
        })
    res = run_spmd(nc, maps)
    o = np.zeros((BATCH * SEQ, 512), NPBF)
    for c in range(NCORES):
        b, m = c // 4, c % 4
        o[b * SEQ:(b + 1) * SEQ, m * 128:(m + 1) * 128] = res[c]["o"]
    return o


def kernel_unfused(x, ln_g, ln_b, rel_table, w_in_ab, w_gate_a, b_gate_a, g_norm_a, w_out_ab,
           w_in_cd, b_forget, w_out_cd, w1_dense, w3_dense, w2_dense,
           w_router, w1_moe, w3_moe, w2_moe):
    f32 = lambda a: np.ascontiguousarray(np.asarray(a, dtype=np.float32))
    x = f32(x)
    rel_table = f32(rel_table)
    big = [f32(w_in_ab), f32(w_in_cd), f32(w_out_ab), f32(w_out_cd), f32(w1_dense), f32(w3_dense), f32(w2_dense),
           f32(w1_moe), f32(w3_moe), f32(w2_moe)]
    (wi_ab, wi_cd, wo_ab, wo_cd, w1d, w3d, w2d, w1m, w3m, w2m) = cast_weights(big)
    del big
    xf = x.reshape(BATCH * SEQ, D)
    for layer in range(DEPTH):
        j = layer // 2
        lnp4 = np.stack([ln_g[layer, 0], ln_b[layer, 0], ln_g[layer, 1], ln_b[layer, 1]]).astype(np.float32)
        if layer % 2 == 0:
            yT, ytb, ytf = run_A(xf, wi_ab[j],
                                 [(0, 256), (256, 256), (1552, 512), (2064, 512), (3088, 512), (3600, 64)],
                                 [(256, 256), (512, 512), (2576, 512)],
                                 [(1024, 512), (3664, 8)],
                                 gate=(1536,), wg=f32(w_gate_a[j]), bg=f32(b_gate_a[j]))
            oa = run_B_gla(yT, ytb, ytf, f32(g_norm_a[j]))
            Msel = run_B_dsa1(yT, ytf)
            ob = run_B_dsa2(yT, ytb, Msel, rel_table)
            del Msel
            o = np.concatenate([oa, ob], axis=1)
            wf = ffn_chunk_layout(w1d[j], w3d[j], w2d[j])
            xf = run_C(o, xf, wo_ab[j], lnp4, wf, 1, 11, 2)
        else:
            yT, ytb, ytf = run_A(xf, wi_cd[j],
                                 [(0, 512), (512, 512), (1544, 512), (2056, 512)],
                                 [(1024, 512), (2568, 512)],
                                 [(1536, 8)])
            o = run_B_cd(yT, ytb, ytf, f32(b_forget[j]), rel_table)
            wf = np.concatenate([ffn_chunk_layout(w1m[j, e], w3m[j, e], w2m[j, e]) for e in range(8)], axis=0)
            xf = run_C(o, xf, wo_cd[j], lnp4, wf, 8, 14, 2, wr=f32(w_router[j]))
    return xf.reshape(BATCH, SEQ, D).astype(np.float32)


I32 = mybir.dt.int32

import os
SKIP = os.environ.get('FZ_SKIP', '')
GROUPS = [[0, 1, 2, 3], [4, 5, 6, 7]]
LK = [512 * (k + 1) for k in range(16)]
MOFF = [128 * sum(LK[:k]) for k in range(16)]
MTOT = 128 * sum(LK)
MPARTS = []
_g = 0
for _k in range(16):
    _r0 = MOFF[_k] // 512
    _n = 128 * LK[_k] // 512
    _halves = 1 if _k < 8 else 2
    for _h in range(_halves):
        _nh = _n // _halves
        MPARTS.append((_k, _h, _r0 + _h * _nh, _nh, _g))
        _g += 4 * _nh
MG = {(p[0], p[1]): p for p in MPARTS}


class KBF(KB):
    def __init__(self):
        super().__init__()
        self.ccsem = self.es.enter_context(self.nc.semaphore("ccsem"))
        self.cccnt = 0
        self.stack = [self.es]

    def sb(self, shape, dt, name="sb"):
        return Buf(self.stack[-1].enter_context(self.nc.sbuf_tensor(self._nm(name), list(shape), dt)))

    def ps(self, shape, dt=F32, name="ps"):
        b = Buf(self.stack[-1].enter_context(self.nc.psum_tensor(self._nm(name), list(shape), dt)))
        b.psum = True
        return b

    def barrier(self):
        evs = []
        for q in ("sp", "act", "pool"):
            for i in range(self.NDS):
                if self.dcnt[q][i] > 0:
                    evs.append((self.dsem[q][i], self.dcnt[q][i], "d%s%d" % (q, i)))
        for e in ("pe", "dve", "act", "pool", "sp"):
            if self.ecnt[e] > 0:
                evs.append((self.esem[e], self.ecnt[e], e))
        if self.cccnt > 0:
            evs.append((self.ccsem, self.cccnt, "cc"))
        for e in ("pe", "dve", "act", "pool", "sp"):
            for ev in evs:
                if ev[2] == e:
                    continue
                self._wait(e, ev)

    def scope(self):
        kb = self

        class _S:
            def __enter__(s):
                st = ExitStack()
                kb.stack.append(st)
                return st

            def __exit__(s, *a):
                kb.barrier()
                st = kb.stack.pop()
                st.close()
                return False

        return _S()

    def dram_tmp(self, name, shape, dt):
        return Buf(self.nc.dram_tensor(name, list(shape), dt).ap())

    def collective(self, kind, src, dst, src_ap=None, dst_ap=None, track_src=True):
        self._deps("pool", [src], [dst])
        sa = src.t if src_ap is None else src_ap
        da = dst.t if dst_ap is None else dst_ap
        ins = self.nc.gpsimd.collective_compute(kind, ALU.bypass, replica_groups=GROUPS,
                                                ins=[sa.opt()], outs=[da.opt()])
        self.cccnt += CC_INC
        ins.then_inc(self.ccsem, CC_INC)
        ev = (self.ccsem, self.cccnt, "cc")
        _kbf_mark(self, ev, [src] if track_src else [], [dst])
        return ev


CC_INC = 1


def load_w_cast(kb, dst, src_d, ncols):
    for kc in range(8):
        kb.dma("pool", dst[:, kc, :ncols], src_d[:, kc, :ncols], R=[src_d], Wd=[dst])


def load_xT_chunk(kb, x_, xT_all, c):
    r, t0 = c // 4, (c % 4) * 512
    for cf in range(4):
        row0 = (cf * 4 + r) * 256
        kb.dma("sp", x_[:, 2 * cf:2 * cf + 2, :],
               xT_all[row0:row0 + 256, t0:t0 + 512].rearrange("(k p) t -> p k t", p=128),
               R=[xT_all], Wd=[x_] if cf else (), W=[x_] if cf == 0 else ())


def emit_xT(kb, ax_ps, ident, xtile, tt, stg, xT_loc, xTs_loc):
    nc = kb.nc
    for kc in range(8):
        pt = ax_ps[kc // 4]
        kb.op("pe", lambda: nc.tensor.transpose(pt[:, (kc % 4) * 128:(kc % 4 + 1) * 128],
                                                xtile[:, kc * 128:(kc + 1) * 128], ident[:, :]),
              R=[xtile, ident], W=[pt])
    q = tt % 4
    for hf in range(2):
        src = ax_ps[hf][:, :].rearrange("p (k t) -> p k t", k=4)
        dst = stg[:, hf * 4:(hf + 1) * 4, q * 128:(q + 1) * 128]
        if hf == 0:
            kb.op("dve", lambda: nc.vector.tensor_copy(out=dst, in_=src), R=[ax_ps[hf]], W=[stg])
        else:
            kb.op("act", lambda: nc.scalar.copy(out=dst, in_=src), R=[ax_ps[hf]], W=[stg])
    if q == 3:
        g = tt // 4
        kb.dma("sp", xT_loc[:, g * 512:(g + 1) * 512].rearrange("(k p) t -> p k t", p=128), stg[:, :, :],
               R=[stg], Wd=[xT_loc])
        if xTs_loc is not None:
            for j in range(4):
                kb.dma("sp", xTs_loc[j * D:(j + 1) * D, g * 128:(g + 1) * 128].rearrange("(k p) t -> p k t", p=128),
                       stg[:, :, j * 128:(j + 1) * 128], R=[stg], Wd=[xTs_loc])


def toeplitz_load(kb, TT, E_d, h, q="act"):
    for s in range(128):
        kb.dma(q if s % 2 == 0 else "sp", TT[s:s + 1, :], E_d[h:h + 1, 127 - s:127 - s + 17 * 128], R=[E_d], Wd=[TT])


def build_E_table(kb, ax, rel_d, C_d, E_d):
    nc = kb.nc
    rel = kb.sb([32, 2], F32, "rel")
    Cs = kb.sb([32, NDEL], F32, "Cs")
    Es = kb.sb([2, NDEL], F32, "Es")
    kb.dma("sp", rel[:, :], rel_d[:, :], R=[rel_d], W=[rel])
    kb.dma("sp", Cs[:, :], C_d[:, :], R=[C_d], W=[Cs])
    kb.op("act", lambda: nc.scalar.activation(out=rel[:, :], in_=rel[:, :], func=AF.Exp), R=[rel], W=[rel])
    for c in range((NDEL + 511) // 512):
        w = min(512, NDEL - c * 512)
        pp = ax.S[c % 3]
        kb.op("pe", lambda: nc.tensor.matmul(pp[:2, :w], lhsT=rel[:, :], rhs=Cs[:, c * 512:c * 512 + w],
                                             start=True, stop=True), R=[rel, Cs], W=[pp])
        kb.op("dve", lambda: nc.vector.tensor_copy(out=Es[:, c * 512:c * 512 + w], in_=pp[:2, :w]), R=[pp], W=[Es])
    kb.dma("sp", E_d[:, :], Es[:, :], R=[Es], W=[E_d])


def oT_exchange(kb, oT_loc, oT_all, j):
    kb.collective("AllGather", oT_loc, oT_all, oT_loc[j * 1024:(j + 1) * 1024, :],
                  oT_all[j * 4096:(j + 1) * 4096, :], track_src=False)


class OTOut:
    def __init__(self, kb, identb, oT_loc, row0, name):
        self.kb, self.identb, self.oT_loc, self.row0 = kb, identb, oT_loc, row0
        self.pt = [kb.ps([128, 1024], BF16, "ptO" + name) for _ in range(1)]
        self.stg = [kb.sb([128, 512], BF16, "stgO" + name) for _ in range(2)]
        self.n = 0

    def put(self, i, ost):
        kb, nc = self.kb, self.kb.nc
        g = i // 4
        st = self.stg[g % 2]
        pt = self.pt[0]
        q = i % 4
        kb.op("pe", lambda: nc.tensor.transpose(pt[:, q * 128:(q + 1) * 128], ost[:, :], self.identb[:, :]),
              R=[ost, self.identb], W=[pt])
        if q == 3:
            kb.op("act", lambda: nc.scalar.copy(out=st[:, :], in_=pt[:, :512]), R=[pt], W=[st])
            rank, grp = g // 4, g % 4
            r0 = (rank * 4 + grp) * 256 + self.row0
            kb.dma("sp", self.oT_loc[r0:r0 + 128, :], st[:, :], R=[st], Wd=[self.oT_loc])


def phase_cd(kb, L, xT_all, oT_loc, oT_all, cst):
    nc = kb.nc
    with kb.scope():
        wcd_d = L["wcd"]
        NCOL = 770
        w = kb.sb([128, 8, NCOL], BF16, "wcd")
        load_w_cast(kb, w, wcd_d, NCOL)
        tri = kb.sb([128, 128], F32, "tri")
        ones = kb.sb([128, 128], F32, "ones")
        identb = kb.sb([128, 128], BF16, "identb")
        kb.dma("sp", tri[:, :], cst["tri"][:, :], R=[cst["tri"]], W=[tri])
        kb.dma("sp", identb[:, :], cst["identb"][:, :], R=[cst["identb"]], W=[identb])
        kb.op("dve", lambda: nc.vector.memset(ones[:, :], 1.0), W=[ones])
        ax = AttnCtx(kb, n_s=4, n_o=2, grouped=True, fox=True)
        E_d = L["Escr"]
        with kb.scope():
            build_E_table(kb, ax, L["rel2"], cst["dilc"], E_d)
        TT = [kb.sb([128, 17 * 128], F32, "TT") for _ in range(2)]
        for h in range(2):
            if "toep" in SKIP:
                kb.op("dve", lambda: nc.vector.memset(TT[h][:, :], 1.0), W=[TT[h]])
            else:
                toeplitz_load(kb, TT[h], E_d, h)
        qc = kb.sb([128, SEQ], BF16, "qc")
        kc_ = kb.sb([128, SEQ], BF16, "kc")
        qd = kb.sb([128, SEQ], BF16, "qd")
        kd = kb.sb([128, SEQ], BF16, "kd")
        Vc = kb.sb([128, NQB, 2, 65], BF16, "Vc")
        Vd = kb.sb([128, NQB, 2, 65], BF16, "Vd")
        fc = kb.sb([128, NQB, 2], F32, "fc")
        kb.op("pool", lambda: nc.gpsimd.memset(Vc[:, :, :, :], 1.0), W=[Vc])
        kb.op("pool", lambda: nc.gpsimd.memset(Vd[:, :, :, :], 1.0), W=[Vd])
        xc = [kb.sb([128, 8, 512], BF16, "xc") for _ in range(2)]
        n_ev = 0
        passes = [(True, True)] if "twopass" not in SKIP else [(True, False), (False, True)]
        NCH = int(os.environ.get("FZ_NCH", "16"))
        for c2 in range((NCH if "proj" not in SKIP else 0) * len(passes)):
            c = c2 % NCH
            do_fm, do_tm = passes[c2 // NCH]
            x_ = xc[c % 2]
            load_xT_chunk(kb, x_, xT_all, c)
            for bi, dst in enumerate((qc, kc_, qd, kd) if ("projfm" not in SKIP and do_fm) else ()):
                pp = ax.S[n_ev % 4]
                for k8 in range(8):
                    kb.op("pe", lambda: nc.tensor.matmul(pp[:, :], lhsT=w[:, k8, bi * 128:(bi + 1) * 128],
                                                         rhs=x_[:, k8, :], start=(k8 == 0), stop=(k8 == 7)),
                          R=[w, x_], W=[pp])
                d_ = dst[:, c * 512:(c + 1) * 512]
                if n_ev % 2 == 0 or "fmdve" in SKIP:
                    kb.op("dve", lambda: nc.vector.tensor_copy(out=d_, in_=pp[:, :]), R=[pp], W=[dst])
                else:
                    kb.op("act", lambda: nc.scalar.copy(out=d_, in_=pp[:, :]), R=[pp], W=[dst])
                n_ev += 1
            for t4 in range(4 if ("projtm" not in SKIP and do_tm) else 0):
                j = c * 4 + t4
                pp = ax.S[n_ev % 4]
                n_ev += 1
                for k8 in range(8):
                    kb.op("pe", lambda: nc.tensor.matmul(pp[:, :258], lhsT=x_[:, k8, t4 * 128:(t4 + 1) * 128],
                                                         rhs=w[:, k8, 512:770], start=(k8 == 0), stop=(k8 == 7)),
                          R=[w, x_], W=[pp])
                kb.op("dve", lambda: nc.vector.tensor_copy(out=Vc[:, j, :, 0:64],
                                                           in_=pp[:, 0:128].rearrange("p (h d) -> p h d", h=2)),
                      R=[pp], W=[Vc])
                if "vddve" in SKIP:
                    kb.op("dve", lambda: nc.vector.tensor_copy(out=Vd[:, j, :, 0:64],
                                                               in_=pp[:, 128:256].rearrange("p (h d) -> p h d", h=2)),
                          R=[pp], W=[Vd])
                else:
                    kb.op("act", lambda: nc.scalar.copy(out=Vd[:, j, :, 0:64],
                                                        in_=pp[:, 128:256].rearrange("p (h d) -> p h d", h=2)),
                          R=[pp], W=[Vd])
                kb.op("dve", lambda: nc.vector.tensor_copy(out=fc[:, j, :], in_=pp[:, 256:258]), R=[pp], W=[fc])
        bf = kb.sb([128, 2], F32, "bf")
        lf = kb.sb([128, 2, NQB], F32, "lf")
        kb.dma("sp", bf[:, :], L["bf"][:, :], R=[L["bf"]], W=[bf])
        for h in range(2):
            kb.op("dve", lambda: nc.vector.tensor_scalar(out=lf[:, h, :], in0=fc[:, :, h], scalar1=bf[:, h:h + 1],
                                                         scalar2=None, op0=ALU.add), R=[fc, bf], W=[lf])
        kb.op("act", lambda: nc.scalar.activation(out=lf[:, :, :], in_=lf[:, :, :], func=AF.Exp, scale=-1.0),
              R=[lf], W=[lf])
        kb.op("act", lambda: nc.scalar.activation(out=lf[:, :, :], in_=lf[:, :, :], func=AF.Ln, bias=1.0),
              R=[lf], W=[lf])
        kb.op("dve", lambda: nc.vector.tensor_scalar(out=lf[:, :, :], in0=lf[:, :, :], scalar1=-1.0, scalar2=None,
                                                     op0=ALU.mult), R=[lf], W=[lf])
        lf2 = lf[:, :, :].rearrange("p h j -> p (h j)")
        p1, p2 = ax.O[0], ax.O[1]
        kb.op("pe", lambda: nc.tensor.matmul(p1[:, :128], lhsT=tri[:, :], rhs=lf2, start=True, stop=True),
              R=[tri, lf], W=[p1])
        kb.op("pe", lambda: nc.tensor.matmul(p2[:, :128], lhsT=ones[:, :], rhs=lf2, start=True, stop=True),
              R=[ones, lf], W=[p2])
        tot = kb.sb([128, 2, NQB], F32, "tot")
        carry = kb.sb([128, 2, NQB], F32, "carry")
        negF = kb.sb([128, 2, NQB], F32, "negF")
        kb.op("dve", lambda: nc.vector.tensor_copy(out=tot[:, :, :].rearrange("p h j -> p (h j)"), in_=p2[:, :128]),
              R=[p2], W=[tot])
        for h in range(2):
            kb.op("dve", lambda: nc.vector.tensor_tensor_scan(out=carry[:, h, :], data0=ones[:, :NQB],
                                                              data1=tot[:, h, :], initial=0.0, op0=ALU.mult,
                                                              op1=ALU.add), R=[ones, tot], W=[carry])
        kb.op("dve", lambda: nc.vector.tensor_tensor(out=carry[:, :, :], in0=carry[:, :, :], in1=tot[:, :, :],
                                                     op=ALU.subtract), R=[carry, tot], W=[carry])
        kb.op("dve", lambda: nc.vector.tensor_tensor(out=negF[:, :, :].rearrange("p h j -> p (h j)"), in0=p1[:, :128],
                                                     in1=carry[:, :, :].rearrange("p h j -> p (h j)"), op=ALU.add),
              R=[p1, carry], W=[negF])
        kb.op("dve", lambda: nc.vector.tensor_scalar(out=negF[:, :, :], in0=negF[:, :, :], scalar1=-1.0, scalar2=None,
                                                     op0=ALU.mult), R=[negF], W=[negF])
        outc = OTOut(kb, identb, oT_loc, 0, "c")
        outd = OTOut(kb, identb, oT_loc, 128, "d")
        ostd = [kb.sb([128, 128], BF16, "ostd") for _ in range(2)]
        ostc = [kb.sb([128, 128], BF16, "ostc") for _ in range(2)]
        Bi = [kb.sb([128, NQB], F32, "Bi") for _ in range(3)]
        tri2 = kb.sb([128, 256], F32, "tri2")
        kb.op("dve", lambda: nc.vector.memset(tri2[:, :], 1.0), W=[tri2])
        kb.op("dve", lambda: nc.vector.tensor_copy(out=tri2[:, 0:128], in_=tri[:, :]), R=[tri], W=[tri2])
        nb = 0
        for p in range(NQB // 2 if "attn" not in SKIP else 0):
            for i in (2 * p, 2 * p + 1):
                od = ostd[i % 2]
                for h in range(2):
                    jdesc = list(range(i, max(0, i - 16) - 1, -1))
                    groups = []
                    for a in range(0, len(jdesc), 4):
                        js = jdesc[a:a + 4]
                        d0 = i - js[0]
                        groups.append({"js": js, "dve_mask": (TT[h], TT[h][:, d0 * 128:(d0 + len(js)) * 128])})
                    attn_qgroup(ax, qd, kd, Vd, h, i, groups, od, h * 64,
                                after=(None if h == 0 else (lambda i=i, od=od: outd.put(i, od))))
            i0_ = 2 * p
            for h in range(2):
                B = Bi[nb % 3]
                nb += 1
                kb.op("dve", lambda: nc.vector.tensor_scalar(out=B[:, :i0_ + 2], in0=negF[:, h, :i0_ + 2],
                                                             scalar1=carry[:, h, i0_:i0_ + 1], scalar2=None,
                                                             op0=ALU.add), R=[negF, carry], W=[B])

                def fin(i0_=i0_):
                    outc.put(i0_, ostc[0])
                    outc.put(i0_ + 1, ostc[1])
                    if (i0_ + 1) % 16 == 15:
                        oT_exchange(kb, oT_loc, oT_all, (i0_ + 1) // 16)

                attn_fox_pair(ax, qc, kc_, Vc, h, i0_, B, tri2, (ostc[0], ostc[1]), h * 64,
                              after=(None if h == 0 else fin))
        attn_flush(ax)


def phase_gla(kb, L, xT_all, oT_loc, cst):
    nc = kb.nc
    with kb.scope():
        tri = kb.sb([128, 128], F32, "tri")
        identb = kb.sb([128, 128], BF16, "identb")
        gn = kb.sb([128, 128], F32, "gn")
        eps = kb.sb([128, 1], F32, "eps")
        kb.dma("sp", tri[:, :], cst["tri"][:, :], R=[cst["tri"]], W=[tri])
        kb.dma("sp", identb[:, :], cst["identb"][:, :], R=[cst["identb"]], W=[identb])
        kb.dma("sp", gn[:, :], L["gn"][:, :], R=[L["gn"]], W=[gn])
        kb.op("dve", lambda: nc.vector.memset(eps[:, :], LN_EPS), W=[eps])
        qT = kb.sb([64, SEQ], BF16, "qT")
        kT = kb.sb([64, SEQ], BF16, "kT")
        ktm = kb.sb([128, NQB, 64], BF16, "ktm")
        v = kb.sb([128, NQB, 128], BF16, "v")
        r = kb.sb([128, NQB, 128], F32, "r")
        g = kb.sb([128, NQB, 64], F32, "g")
        PG = [kb.ps([128, 512], F32, "PG")]
        PGT = [kb.ps([128, 512], F32, "PGT")]
        PA = [kb.ps([128, 512], F32, "PA") for _ in range(2)]
        PO = kb.ps([128, 512], F32, "PO")
        PU = kb.ps([128, 512], F32, "PU")
        out = OTOut(kb, identb, oT_loc, 0, "g")
        with kb.scope():
            NCOL = 464
            w = kb.sb([128, 8, NCOL], BF16, "wgl")
            load_w_cast(kb, w, L["wgl"], NCOL)
            wg = kb.sb([16, 64], F32, "wg")
            bg = kb.sb([128, 64], F32, "bg")
            kb.dma("sp", wg[:, :], L["wg"][:, :], R=[L["wg"]], W=[wg])
            kb.dma("sp", bg[:, :], L["bg"][:, :], R=[L["bg"]], W=[bg])
            xc = [kb.sb([128, 8, 512], BF16, "xc") for _ in range(2)]
            gaT = [kb.sb([16, 512], F32, "gaT") for _ in range(2)]
            zt = [kb.sb([128, 64], F32, "zt") for _ in range(2)]
            pr = [PA[0], PA[1], PO]
            n_ev = 0
            for c in range(16):
                x_ = xc[c % 2]
                ga_ = gaT[c % 2]
                load_xT_chunk(kb, x_, xT_all, c)
                for (c0, ncol, dst) in ((0, 64, qT), (64, 64, kT), (128, 16, ga_)):
                    pp = pr[n_ev % 3]
                    for k8 in range(8):
                        kb.op("pe", lambda: nc.tensor.matmul(pp[:ncol, :], lhsT=w[:, k8, c0:c0 + ncol], rhs=x_[:, k8, :],
                                                             start=(k8 == 0), stop=(k8 == 7)), R=[w, x_], W=[pp])
                    d_ = dst[:, c * 512:(c + 1) * 512] if dst is not ga_ else ga_[:, :]
                    if n_ev % 2 == 0:
                        kb.op("dve", lambda: nc.vector.tensor_copy(out=d_, in_=pp[:ncol, :]), R=[pp], W=[dst])
                    else:
                        kb.op("act", lambda: nc.scalar.copy(out=d_, in_=pp[:ncol, :]), R=[pp], W=[dst])
                    n_ev += 1
                for t4 in range(4):
                    j = c * 4 + t4
                    pp = pr[n_ev % 3]
                    n_ev += 1
                    for k8 in range(8):
                        kb.op("pe", lambda: nc.tensor.matmul(pp[:, :320], lhsT=x_[:, k8, t4 * 128:(t4 + 1) * 128],
                                                             rhs=w[:, k8, 144:464], start=(k8 == 0), stop=(k8 == 7)),
                              R=[w, x_], W=[pp])
                    kb.op("dve", lambda: nc.vector.tensor_copy(out=ktm[:, j, :], in_=pp[:, 0:64]), R=[pp], W=[ktm])
                    kb.op("dve", lambda: nc.vector.tensor_copy(out=v[:, j, :], in_=pp[:, 64:192]), R=[pp], W=[v])
                    kb.op("act", lambda: nc.scalar.activation(out=r[:, j, :], in_=pp[:, 192:320], func=AF.Silu),
                          R=[pp], W=[r])
                    pq = pr[n_ev % 3]
                    n_ev += 1
                    z = zt[j % 2]
                    kb.op("pe", lambda: nc.tensor.matmul(pq[:, :64], lhsT=ga_[:, t4 * 128:(t4 + 1) * 128], rhs=wg[:, :],
                                                         start=True, stop=True), R=[ga_, wg], W=[pq])
                    kb.op("dve", lambda: nc.vector.tensor_tensor(out=z[:, :], in0=pq[:, :64], in1=bg[:, :], op=ALU.add),
                          R=[pq, bg], W=[z])
                    kb.op("act", lambda: nc.scalar.activation(out=z[:, :], in_=z[:, :], func=AF.Exp, scale=-1.0),
                          R=[z], W=[z])
                    kb.op("act", lambda: nc.scalar.activation(out=z[:, :], in_=z[:, :], func=AF.Ln, bias=1.0),
                          R=[z], W=[z])
                    kb.op("dve", lambda: nc.vector.tensor_scalar(out=g[:, j, :], in0=z[:, :], scalar1=-1.0 / 16.0,
                                                                 scalar2=None, op0=ALU.mult), R=[z], W=[g])
        eGT = [kb.sb([64, 128], F32, "eGT") for _ in range(2)]
        enGT = [kb.sb([64, 128], F32, "enGT") for _ in range(2)]
        enG = [kb.sb([128, 64], F32, "enG") for _ in range(2)]
        qgT = [kb.sb([64, 128], BF16, "qgT") for _ in range(2)]
        kgT = [kb.sb([64, 128], BF16, "kgT") for _ in range(2)]
        kg = [kb.sb([128, 64], BF16, "kg") for _ in range(2)]
        Am = [kb.sb([128, 128], BF16, "Am") for _ in range(2)]
        S32 = kb.sb([64, 128], F32, "S32")
        Sbf = kb.sb([64, 128], BF16, "Sbf")
        st6 = [kb.sb([128, 6], F32, "st6") for _ in range(2)]
        mv = [kb.sb([128, 4], F32, "mv") for _ in range(2)]
        of = [kb.sb([128, 128], F32, "of") for _ in range(2)]
        ost = [kb.sb([128, 128], BF16, "ost") for _ in range(2)]
        for c in range(NQB):
            p = c % 2
            cs = slice(c * 128, (c + 1) * 128)
            kb.op("pe", lambda: nc.tensor.matmul(PG[0][:, :64], lhsT=tri[:, :], rhs=g[:, c, :], start=True, stop=True),
                  R=[tri, g], W=[PG[0]])
            kb.op("pe", lambda: nc.tensor.matmul(PGT[0][:64, :128], lhsT=g[:, c, :], rhs=tri[:, :], start=True, stop=True),
                  R=[tri, g], W=[PGT[0]])
            kb.op("act", lambda: nc.scalar.activation(out=eGT[p][:, :], in_=PGT[0][:64, :128], func=AF.Exp),
                  R=[PGT[0]], W=[eGT[p]])
            kb.op("act", lambda: nc.scalar.activation(out=enGT[p][:, :], in_=PGT[0][:64, :128], func=AF.Exp, scale=-1.0),
                  R=[PGT[0]], W=[enGT[p]])
            kb.op("act", lambda: nc.scalar.activation(out=enG[p][:, :], in_=PG[0][:, :64], func=AF.Exp, scale=-1.0),
                  R=[PG[0]], W=[enG[p]])
            kb.op("dve", lambda: nc.vector.scalar_tensor_tensor(out=qgT[p][:, :], in0=qT[:, cs], scalar=0.125,
                                                                in1=eGT[p][:, :], op0=ALU.mult, op1=ALU.mult),
                  R=[qT, eGT[p]], W=[qgT[p]])
            kb.op("dve", lambda: nc.vector.tensor_tensor(out=kgT[p][:, :], in0=kT[:, cs], in1=enGT[p][:, :], op=ALU.mult),
                  R=[kT, enGT[p]], W=[kgT[p]])
            kb.op("dve", lambda: nc.vector.tensor_tensor(out=kg[p][:, :], in0=ktm[:, c, :], in1=enG[p][:, :], op=ALU.mult),
                  R=[ktm, enG[p]], W=[kg[p]])
            kb.op("pe", lambda: nc.tensor.matmul(PA[p][:, :128], lhsT=kgT[p][:, :], rhs=qgT[p][:, :], start=True, stop=True),
                  R=[kgT[p], qgT[p]], W=[PA[p]])
            kb.op("dve", lambda: nc.vector.tensor_tensor(out=Am[p][:, :], in0=PA[p][:, :128], in1=tri[:, :], op=ALU.mult),
                  R=[PA[p], tri], W=[Am[p]])
            kb.op("pe", lambda: nc.tensor.matmul(PO[:, :128], lhsT=Am[p][:, :], rhs=v[:, c, :], start=True, stop=(c == 0)),
                  R=[Am[p], v], W=[PO])
            if c > 0:
                kb.op("pe", lambda: nc.tensor.matmul(PO[:, :128], lhsT=qgT[p][:, :], rhs=Sbf[:, :], start=False, stop=True),
                      R=[qgT[p], Sbf], W=[PO])
            if c < NQB - 1:
                kb.op("pe", lambda: nc.tensor.matmul(PU[:64, :128], lhsT=kg[p][:, :], rhs=v[:, c, :], start=True, stop=True),
                      R=[kg[p], v], W=[PU])
                eGl = eGT[p][:, 127:128]
                if c == 0:
                    kb.op("dve", lambda: nc.vector.tensor_scalar(out=S32[:, :], in0=PU[:64, :128], scalar1=eGl,
                                                                 scalar2=None, op0=ALU.mult), R=[PU, eGT[p]], W=[S32])
                else:
                    kb.op("dve", lambda: nc.vector.tensor_scalar(out=S32[:, :], in0=S32[:, :], scalar1=eGl, scalar2=None,
                                                                 op0=ALU.mult), R=[S32, eGT[p]], W=[S32])
                    kb.op("dve", lambda: nc.vector.scalar_tensor_tensor(out=S32[:, :], in0=PU[:64, :128], scalar=eGl,
                                                                        in1=S32[:, :], op0=ALU.mult, op1=ALU.add),
                          R=[PU, eGT[p], S32], W=[S32])
                kb.op("dve", lambda: nc.vector.tensor_copy(out=Sbf[:, :], in_=S32[:, :]), R=[S32], W=[Sbf])
            kb.op("dve", lambda: nc.vector.bn_stats(out=st6[p][:, :], in_=PO[:, :128]), R=[PO], W=[st6[p]])
            kb.op("dve", lambda: nc.vector.bn_aggr(out=mv[p][:, 0:2], in_=st6[p][:, :]), R=[st6[p]], W=[mv[p]])
            kb.op("dve", lambda: nc.vector.scalar_tensor_tensor(out=mv[p][:, 2:3], in0=mv[p][:, 0:1], scalar=mv[p][:, 0:1],
                                                                in1=mv[p][:, 1:2], op0=ALU.mult, op1=ALU.add),
                  R=[mv[p]], W=[mv[p]])
            kb.op("act", lambda: nc.scalar.activation(out=mv[p][:, 3:4], in_=mv[p][:, 2:3], func=AF.Ln, bias=eps[:, 0:1]),
                  R=[mv[p], eps], W=[mv[p]])
            kb.op("act", lambda: nc.scalar.activation(out=mv[p][:, 3:4], in_=mv[p][:, 3:4], func=AF.Exp, scale=-0.5),
                  R=[mv[p]], W=[mv[p]])
            kb.op("dve", lambda: nc.vector.scalar_tensor_tensor(out=of[p][:, :], in0=PO[:, :128], scalar=mv[p][:, 3:4],
                                                                in1=gn[:, :], op0=ALU.mult, op1=ALU.mult),
                  R=[PO, mv[p], gn], W=[of[p]])
            kb.op("pool", lambda: nc.gpsimd.tensor_tensor(out=ost[p][:, :], in0=of[p][:, :], in1=r[:, c, :], op=ALU.mult),
                  R=[of[p], r], W=[ost[p]])
            out.put(c, ost[p])


def _kbf_deps(self, e, R, W, Wd=()):
    evs = []
    for b in R:
        evs.extend(b.wl())
        if getattr(b, "psum", False):
            evs.extend(x for x in b.r if x[2] != e)
    for b in W:
        evs.extend(b.wl())
        evs.extend(b.r)
    for b in Wd:
        evs.extend(b.r)
        evs.extend(getattr(b, "w_excl", []))
    best = {}
    for ev in evs:
        k = ev[2]
        if e == "pe" and k == "pe":
            continue
        if k not in best or best[k][1] < ev[1]:
            best[k] = ev
    for ev in best.values():
        self._wait(e, ev)


def _buf_wl(self):
    if self.w is None:
        return []
    return self.w if isinstance(self.w, list) else [self.w]


Buf.wl = _buf_wl


def _kbf_mark(self, ev, R, W, Wd=()):
    KB._mark(self, ev, R, W)
    for b in W:
        b.w = [ev]
        b.w_excl = [ev]
    for b in Wd:
        cur = b.wl()
        cur = [x for x in cur if x[2] != ev[2]] + [ev]
        b.w = cur
        b.r = []


def _kbf_dma(self, q, out, in_, R=(), W=(), Wd=(), **kw):
    _kbf_deps(self, q, R, W, Wd)
    i = self.dnext[q]
    self.dnext[q] = (i + 1) % self.NDS
    key = "d%s%d" % (q, i)
    if self.dcnt[q][i] > 0:
        self._wait(q, (self.dsem[q][i], self.dcnt[q][i], key))
    self.dcnt[q][i] += 16
    self.eng[q].dma_start(out=out, in_=in_, **kw).then_inc(self.dsem[q][i], 16)
    ev = (self.dsem[q][i], self.dcnt[q][i], key)
    _kbf_mark(self, ev, R, W, Wd)
    return ev


def _kbf_op(self, e, fn, R=(), W=()):
    _kbf_deps(self, e, R, W)
    ins = fn()
    self.ecnt[e] += 1
    ins.then_inc(self.esem[e], 1)
    ev = (self.esem[e], self.ecnt[e], e)
    _kbf_mark(self, ev, R, W)
    return ev


def _kbf_idma(self, out, in_, idx_ap, R=(), W=(), Wd=()):
    q = "pool"
    _kbf_deps(self, q, R, W, Wd)
    i = self.dnext[q]
    self.dnext[q] = (i + 1) % self.NDS
    key = "d%s%d" % (q, i)
    if self.dcnt[q][i] > 0:
        self._wait(q, (self.dsem[q][i], self.dcnt[q][i], key))
    self.dcnt[q][i] += 16
    self.nc.gpsimd.indirect_dma_start(out=out, out_offset=None, in_=in_,
                                      in_offset=bass.IndirectOffsetOnAxis(ap=idx_ap, axis=0)
                                      ).then_inc(self.dsem[q][i], 16)
    ev = (self.dsem[q][i], self.dcnt[q][i], key)
    _kbf_mark(self, ev, R, W, Wd)
    return ev


KBF.idma = _kbf_idma
KBF._deps = lambda self, e, R, W: _kbf_deps(self, e, R, W)
KBF.dma = _kbf_dma
KBF.op = _kbf_op


def phase_dsa1(kb, L, xT_all, xTs_all, M_loc, M_all, cst, act_split=True):
    nc = kb.nc
    NK = 16
    with kb.scope():
        qiT = kb.sb([64, 8, NK * 128], BF16, "qiT")
        kiT = kb.sb([64, SEQ], BF16, "kiT")
        wi = kb.sb([128, NK, 8], F32, "wi")
        absw = kb.sb([128, NK, 8], F32, "absw")
        sgn = kb.sb([128, NK, 8], F32, "sgn")
        cm = kb.sb([128, 512], F32, "cm")
        idb = kb.sb([128, 128], BF16, "idb")
        stp = kb.sb([128, NBIS], F32, "stp")
        kb.dma("sp", cm[:, :], cst["cmask"][:, :], R=[cst["cmask"]], W=[cm])
        kb.dma("sp", idb[:, :], cst["identb"][:, :], R=[cst["identb"]], W=[idb])
        kb.dma("sp", stp[:, :], cst["steps"][:, :], R=[cst["steps"]], W=[stp])
        idxs = kb.sb([128, 32], I32, "idxs")
        kb.dma("sp", idxs[:, :], cst["idx_s"][:, :], R=[cst["idx_s"]], W=[idxs])
        PS = [kb.ps([128, 512], F32, "PS") for _ in range(3)]
        PT = [kb.ps([128, 1024], BF16, "PT") for _ in range(2)]
        with kb.scope():
            NCOL = 584
            w = kb.sb([128, 8, NCOL], BF16, "wd1")
            load_w_cast(kb, w, L["wd1"], NCOL)
            xc = [kb.sb([128, 8, 512], BF16, "xc") for _ in range(2)]
            n_ev = 0
            for c in range(16):
                x_ = xc[c % 2]
                load_xT_chunk(kb, x_, xT_all, c)
                pp = PS[n_ev % 3]
                n_ev += 1
                for k8 in range(8):
                    kb.op("pe", lambda: nc.tensor.matmul(pp[:64, :], lhsT=w[:, k8, 512:576], rhs=x_[:, k8, :],
                                                         start=(k8 == 0), stop=(k8 == 7)), R=[w, x_], W=[pp])
                kb.op("dve", lambda: nc.vector.tensor_copy(out=kiT[:, c * 512:(c + 1) * 512], in_=pp[:64, :]),
                      R=[pp], W=[kiT])
            for r in range(4):
                x_ = xc[r % 2]
                for k8 in range(8):
                    kb.idma(x_[:, k8, :], xTs_all[:, :], idxs[:, r * 8 + k8:r * 8 + k8 + 1], R=[xTs_all, idxs],
                            Wd=[x_] if k8 else (), W=[x_] if k8 == 0 else ())
                for hi in range(8):
                    pp = PS[n_ev % 3]
                    n_ev += 1
                    for k8 in range(8):
                        kb.op("pe", lambda: nc.tensor.matmul(pp[:64, :], lhsT=w[:, k8, hi * 64:(hi + 1) * 64],
                                                             rhs=x_[:, k8, :], start=(k8 == 0), stop=(k8 == 7)),
                              R=[w, x_], W=[pp])
                    if hi % 2 == 0:
                        kb.op("dve", lambda: nc.vector.tensor_copy(out=qiT[:, hi, r * 512:(r + 1) * 512], in_=pp[:64, :]),
                              R=[pp], W=[qiT])
                    else:
                        kb.op("act", lambda: nc.scalar.copy(out=qiT[:, hi, r * 512:(r + 1) * 512], in_=pp[:64, :]),
                              R=[pp], W=[qiT])
                for t4 in range(4):
                    pp = PS[n_ev % 3]
                    n_ev += 1
                    for k8 in range(8):
                        kb.op("pe", lambda: nc.tensor.matmul(pp[:, :8], lhsT=x_[:, k8, t4 * 128:(t4 + 1) * 128],
                                                             rhs=w[:, k8, 576:584], start=(k8 == 0), stop=(k8 == 7)),
                              R=[w, x_], W=[pp])
                    kb.op("dve", lambda: nc.vector.tensor_copy(out=wi[:, r * 4 + t4, :], in_=pp[:, :8]), R=[pp], W=[wi])
        kb.op("act", lambda: nc.scalar.activation(out=absw[:, :, :], in_=wi[:, :, :], func=AF.Abs), R=[wi], W=[absw])
        kb.op("act", lambda: nc.scalar.activation(out=sgn[:, :, :], in_=wi[:, :, :], func=AF.Sign), R=[wi], W=[sgn])
        def write_mask(k, mt, Lk):
            dst = _AP(tensor=M_loc.t.tensor, offset=MOFF[k], ap=[[Lk, 128], [1, Lk]])
            kb.dma("sp", dst, mt[:, :Lk], R=[mt], Wd=[M_loc])
            for (k2, hf, lrow, nrow, grow) in MPARTS:
                if k2 == k:
                    kb.collective("AllGather", M_loc, M_all, M_loc[lrow:lrow + nrow, :],
                                  M_all[grow:grow + 4 * nrow, :], track_src=False)

        dsa1_body(kb, qiT, kiT, absw, sgn, cm, idb, stp, PS, PT, write_mask, act_split)


def phase_dsa2(kb, L, xT_all, M_all, oT_loc, oT_all, cst):
    nc = kb.nc
    with kb.scope():
        identb = kb.sb([128, 128], BF16, "identb")
        kb.dma("sp", identb[:, :], cst["identb"][:, :], R=[cst["identb"]], W=[identb])
        rf = kb.sb([128, 2], F32, "rf")
        kb.dma("sp", rf[:, :], L["relfar"][:, :], R=[L["relfar"]], W=[rf])
        ax = AttnCtx(kb, n_s=4, n_o=2, grouped=True)
        E_d = L["Escr"]
        with kb.scope():
            build_E_table(kb, ax, L["rel2"], cst["dsac"], E_d)
        TT = [kb.sb([128, 17 * 128], F32, "TT") for _ in range(2)]
        for h in range(2):
            toeplitz_load(kb, TT[h], E_d, h)
        q = kb.sb([128, SEQ], BF16, "q")
        k = kb.sb([128, SEQ], BF16, "k")
        V = kb.sb([128, NQB, 2, 65], BF16, "V")
        kb.op("pool", lambda: nc.gpsimd.memset(V[:, :, :, :], 1.0), W=[V])
        with kb.scope():
            NCOL = 384
            w = kb.sb([128, 8, NCOL], BF16, "wd2")
            load_w_cast(kb, w, L["wd2"], NCOL)
            xc = [kb.sb([128, 8, 512], BF16, "xc") for _ in range(2)]
            n_ev = 0
            for c in range(16):
                x_ = xc[c % 2]
                load_xT_chunk(kb, x_, xT_all, c)
                for bi, dst in enumerate((q, k)):
                    pp = ax.S[n_ev % 3]
                    for k8 in range(8):
                        kb.op("pe", lambda: nc.tensor.matmul(pp[:, :], lhsT=w[:, k8, bi * 128:(bi + 1) * 128],
                                                             rhs=x_[:, k8, :], start=(k8 == 0), stop=(k8 == 7)),
                              R=[w, x_], W=[pp])
                    d_ = dst[:, c * 512:(c + 1) * 512]
                    if n_ev % 2 == 0:
                        kb.op("dve", lambda: nc.vector.tensor_copy(out=d_, in_=pp[:, :]), R=[pp], W=[dst])
                    else:
                        kb.op("act", lambda: nc.scalar.copy(out=d_, in_=pp[:, :]), R=[pp], W=[dst])
                    n_ev += 1
                for t4 in range(4):
                    j = c * 4 + t4
                    pp = ax.S[n_ev % 3]
                    n_ev += 1
                    for k8 in range(8):
                        kb.op("pe", lambda: nc.tensor.matmul(pp[:, :128], lhsT=x_[:, k8, t4 * 128:(t4 + 1) * 128],
                                                             rhs=w[:, k8, 256:384], start=(k8 == 0), stop=(k8 == 7)),
                              R=[w, x_], W=[pp])
                    kb.op("dve" if t4 % 2 else "act",
                          (lambda: nc.vector.tensor_copy(out=V[:, j, :, 0:64],
                                                         in_=pp[:, 0:128].rearrange("p (h d) -> p h d", h=2)))
                          if t4 % 2 else
                          (lambda: nc.scalar.copy(out=V[:, j, :, 0:64],
                                                  in_=pp[:, 0:128].rearrange("p (h d) -> p h d", h=2))),
                          R=[pp], W=[V])
        Ms = [kb.sb([128, SEQ], BF16, "Ms") for _ in range(2)]
        ost = [kb.sb([128, 128], BF16, "ost") for _ in range(2)]
        out = OTOut(kb, identb, oT_loc, 128, "b")
        for i in range(NQB):
            ms = Ms[i % 2]
            W_ = (i + 1) * 128
            r_, k_ = i % 4, i // 4
            nh = 1 if k_ < 8 else 2
            for hf in range(nh):
                (_, _, lrow, nrow, grow) = MG[(k_, hf)]
                ns = 128 // nh
                src = _AP(tensor=M_all.t.tensor, offset=(grow + r_ * nrow) * 512, ap=[[LK[k_], ns], [1, W_]])
                kb.dma("sp", ms[hf * ns:(hf + 1) * ns, :W_], src, R=[M_all],
                       Wd=[ms] if hf else (), W=[ms] if hf == 0 else ())
            o = ost[i % 2]
            for h in range(2):
                groups = []
                jn0 = max(0, i - 16)
                for a in range(0, jn0, 4):
                    js = list(range(a, min(a + 4, jn0)))
                    groups.append({"js": js, "bias": (rf, rf[:, h:h + 1]),
                                   "dve_mask": (ms, ms[:, js[0] * 128:(js[-1] + 1) * 128])})
                for a in range(jn0, i + 1, 4):
                    js = list(range(a, min(a + 4, i + 1)))
                    groups.append({"js": js,
                                   "pool_masks": [(TT[h], TT[h][:, (i - j) * 128:(i - j + 1) * 128]) for j in js],
                                   "dve_mask": (ms, ms[:, js[0] * 128:(js[-1] + 1) * 128])})
                def fin2(i=i, o=o):
                    out.put(i, o)
                    if i % 16 == 15:
                        oT_exchange(kb, oT_loc, oT_all, i // 16)

                attn_qgroup(ax, q, k, V, h, i, groups, o, h * 64, after=(None if h == 0 else fin2))
        attn_flush(ax)


def phase_c(kb, L, moe, oT_all, x_src, x_dst, xT_loc, xTs_loc, cst, last):
    nc = kb.nc
    n_exp, nfu, n_units = (8, 14, 2) if moe else (1, 11, 2)
    TG = 512
    NG = TPC // TG
    with kb.scope():
        wf_d = L["wf"]
        ident = kb.sb([128, 128], F32, "ident")
        wo = kb.sb([128, 8, D], BF16, "wo")
        lnp = kb.sb([128, 4, D], F32, "lnp")
        kb.eps_col = kb.sb([128, 1], F32, "eps")
        kb.op("dve", lambda: nc.vector.memset(kb.eps_col[:, :], LN_EPS), W=[kb.eps_col])
        kb.dma("sp", ident[:, :], cst["ident"][:, :], R=[cst["ident"]], W=[ident])
        load_w_cast(kb, wo, L["wo"], D)
        kb.dma("sp", lnp[:, :, :], L["lnp"][:, :, :], R=[L["lnp"]], W=[lnp])
        idxo = kb.sb([128, 32], I32, "idxo")
        kb.dma("sp", idxo[:, :], cst["idx_o"][:, :], R=[cst["idx_o"]], W=[idxo])
        if moe:
            wr = kb.sb([128, 8, 8], F32, "wr")
            kb.dma("sp", wr[:, :, :], L["wr"][:, :, :], R=[L["wr"]], W=[wr])
            x1T32 = kb.sb([128, 8, 128], F32, "x1T32")
            comb = [kb.sb([128, 8], F32, "comb") for _ in range(4)]
            rt = kb.sb([128, 40], F32, "rt")
        oT = [kb.sb([128, 8, TG], BF16, "oT") for _ in range(1)]
        xt = [kb.sb([128, D], F32, "xt") for _ in range(2)]
        h = kb.sb([128, D], F32, "h")
        x1g = [kb.sb([128, D], F32, "x1g") for _ in range(4)]
        x1T = kb.sb([128, 8, TG], BF16, "x1T")
        aT = [kb.sb([128, TG], BF16, "aT") for _ in range(nfu)]
        w2 = [kb.sb([128, D], BF16, "w2") for _ in range(nfu)]
        w13 = [kb.sb([128, 2048], BF16, "w13") for _ in range(4)]
        yacc = [kb.sb([128, D], F32, "yacc") for _ in range(4)]
        sil = [kb.sb([128, TG], F32, "sil") for _ in range(2)]
        ost = [kb.sb([128, D], F32, "ost") for _ in range(2)]
        stg = kb.sb([128, 8, 512], BF16, "stgx")
        scr = (kb.sb([128, 2, 6], F32, "stats"), kb.sb([128, 2], F32, "mv"), kb.sb([128, 2], F32, "sd"))
        X = [kb.ps([128, 512], F32, "X") for _ in range(4)]
        Y = [kb.ps([128, 512], F32, "Y") for _ in range(2)]
        wcnt = 0
        for g in range(NG):
            og = oT[0]
            for k8 in range(8):
                kb.idma(og[:, k8, :], oT_all[:, :], idxo[:, g * 8 + k8:g * 8 + k8 + 1], R=[oT_all, idxo],
                        Wd=[og] if k8 else (), W=[og] if k8 == 0 else ())
            for tt in range(4):
                tok0 = g * TG + tt * 128
                xi = xt[tt % 2]
                kb.dma("sp", xi[:, :], x_src[tok0:tok0 + 128, :], R=[x_src], W=[xi])
                for hf in range(2):
                    for kc in range(8):
                        kb.op("pe", lambda: nc.tensor.matmul(Y[hf][:, :], lhsT=og[:, kc, tt * 128:(tt + 1) * 128],
                                                             rhs=wo[:, kc, hf * 512:(hf + 1) * 512],
                                                             start=(kc == 0), stop=(kc == 7)), R=[og, wo], W=[Y[hf]])
                    kb.op("dve", lambda: nc.vector.scalar_tensor_tensor(out=h[:, hf * 512:(hf + 1) * 512],
                                                                        in0=xi[:, hf * 512:(hf + 1) * 512], scalar=ALPHA,
                                                                        in1=Y[hf][:, :], op0=ALU.mult, op1=ALU.add),
                          R=[xi, Y[hf]], W=[h])
                x1 = x1g[tt]
                layer_norm(kb, h, (lnp, lnp[:, 0, :]), (lnp, lnp[:, 1, :]), x1[:, :], x1, scr)
                for kc in range(8):
                    pt = X[kc // 4]
                    kb.op("pe", lambda: nc.tensor.transpose(pt[:, (kc % 4) * 128:(kc % 4 + 1) * 128],
                                                            x1[:, kc * 128:(kc + 1) * 128], ident[:, :]),
                          R=[x1, ident], W=[pt])
                for hf in range(2):
                    src = X[hf][:, :].rearrange("p (k t) -> p k t", k=4)
                    dst = x1T[:, hf * 4:(hf + 1) * 4, tt * 128:(tt + 1) * 128]
                    if hf == 0:
                        kb.op("dve", lambda: nc.vector.tensor_copy(out=dst, in_=src), R=[X[hf]], W=[x1T])
                    else:
                        kb.op("act", lambda: nc.scalar.copy(out=dst, in_=src), R=[X[hf]], W=[x1T])
                    if moe:
                        kb.op("dve", lambda: nc.vector.tensor_copy(out=x1T32[:, hf * 4:(hf + 1) * 4, :], in_=src),
                              R=[X[hf]], W=[x1T32])
                if moe:
                    pr = X[2]
                    for kc in range(8):
                        kb.op("pe", lambda: nc.tensor.matmul(pr[:, :8], lhsT=x1T32[:, kc, :], rhs=wr[:, kc, :],
                                                             start=(kc == 0), stop=(kc == 7)), R=[x1T32, wr], W=[pr])
                    cb = comb[tt]
                    lg, mx, tmp, oh = rt[:, 0:8], rt[:, 8:16], rt[:, 16:24], rt[:, 24:32]
                    sc = rt[:, 32:40]
                    kb.op("dve", lambda: nc.vector.tensor_copy(out=lg, in_=pr[:, :8]), R=[pr], W=[rt])
                    kb.op("dve", lambda: nc.vector.max(out=mx, in_=lg), R=[rt], W=[rt])
                    kb.op("dve", lambda: nc.vector.tensor_tensor(out=sc[:, 0:1], in0=mx[:, 1:2], in1=mx[:, 0:1],
                                                                 op=ALU.subtract), R=[rt], W=[rt])
                    kb.op("act", lambda: nc.scalar.activation(out=sc[:, 1:2], in_=sc[:, 0:1], func=AF.Exp),
                          R=[rt], W=[rt])
                    kb.op("dve", lambda: nc.vector.tensor_scalar(out=sc[:, 2:3], in0=sc[:, 1:2], scalar1=1.0,
                                                                 scalar2=None, op0=ALU.add), R=[rt], W=[rt])
                    kb.op("dve", lambda: nc.vector.reciprocal(out=sc[:, 3:4], in_=sc[:, 2:3]), R=[rt], W=[rt])
                    kb.op("dve", lambda: nc.vector.tensor_tensor(out=sc[:, 4:5], in0=sc[:, 1:2], in1=sc[:, 3:4],
                                                                 op=ALU.mult), R=[rt], W=[rt])
                    kb.op("dve", lambda: nc.vector.tensor_scalar(out=tmp, in0=lg, scalar1=mx[:, 0:1],
                                                                 scalar2=sc[:, 3:4], op0=ALU.is_equal, op1=ALU.mult),
                          R=[rt], W=[rt])
                    kb.op("dve", lambda: nc.vector.tensor_scalar(out=oh, in0=lg, scalar1=mx[:, 1:2],
                                                                 scalar2=sc[:, 4:5], op0=ALU.is_equal, op1=ALU.mult),
                          R=[rt], W=[rt])
                    kb.op("dve", lambda: nc.vector.tensor_tensor(out=cb[:, :], in0=tmp, in1=oh, op=ALU.add),
                          R=[rt], W=[cb])
            first = True
            for e in range(n_exp):
                for u in range(n_units):
                    base = (e * n_units + u) * nfu
                    LAGW = 3
                    wcs = {}
                    for f in range(nfu):
                        if f == 0:
                            for f2 in range(min(LAGW, nfu)):
                                wcs[f2] = w13[(wcnt + f2) % 4]
                                kb.dma("pool", wcs[f2][:, :], wf_d[base + f2, :, 0:2048], R=[wf_d], W=[wcs[f2]])
                        if f + LAGW < nfu:
                            wcs[f + LAGW] = w13[(wcnt + LAGW) % 4]
                            kb.dma("pool", wcs[f + LAGW][:, :], wf_d[base + f + LAGW, :, 0:2048], R=[wf_d],
                                   W=[wcs[f + LAGW]])
                        kb.dma("pool", w2[f][:, :], wf_d[base + f, :, 2048:3072], R=[wf_d], W=[w2[f]])
                        wc = wcs[f]
                        h1, h3 = X[(wcnt % 2) * 2], X[(wcnt % 2) * 2 + 1]
                        for kc in range(8):
                            kb.op("pe", lambda: nc.tensor.matmul(h1[:, :], lhsT=wc[:, kc * 128:(kc + 1) * 128],
                                                                 rhs=x1T[:, kc, :], start=(kc == 0), stop=(kc == 7)),
                                  R=[wc, x1T], W=[h1])
                        for kc in range(8):
                            kb.op("pe", lambda: nc.tensor.matmul(h3[:, :],
                                                                 lhsT=wc[:, 1024 + kc * 128:1024 + (kc + 1) * 128],
                                                                 rhs=x1T[:, kc, :], start=(kc == 0), stop=(kc == 7)),
                                  R=[wc, x1T], W=[h3])
                        s = sil[wcnt % 2]
                        kb.op("act", lambda: nc.scalar.activation(out=s[:, :], in_=h1[:, :], func=AF.Silu),
                              R=[h1], W=[s])
                        kb.op("dve", lambda: nc.vector.tensor_tensor(out=aT[f][:, :], in0=s[:, :], in1=h3[:, :],
                                                                     op=ALU.mult), R=[s, h3], W=[aT[f]])
                        wcnt += 1
                    for tt in range(4):
                        for hf in range(2):
                            py = Y[hf]
                            for f in range(nfu):
                                kb.op("pe", lambda: nc.tensor.matmul(py[:, :], lhsT=aT[f][:, tt * 128:(tt + 1) * 128],
                                                                     rhs=w2[f][:, hf * 512:(hf + 1) * 512],
                                                                     start=(f == 0), stop=(f == nfu - 1)),
                                      R=[aT[f], w2[f]], W=[py])
                            ya = yacc[tt]
                            ysl = ya[:, hf * 512:(hf + 1) * 512]
                            if moe:
                                cs = comb[tt][:, e:e + 1]
                                if first:
                                    kb.op("dve", lambda: nc.vector.tensor_scalar(out=ysl, in0=py[:, :], scalar1=cs,
                                                                                 scalar2=None, op0=ALU.mult),
                                          R=[py, comb[tt]], W=[ya])
                                else:
                                    kb.op("dve", lambda: nc.vector.scalar_tensor_tensor(out=ysl, in0=py[:, :], scalar=cs,
                                                                                        in1=ysl, op0=ALU.mult,
                                                                                        op1=ALU.add),
                                          R=[py, comb[tt], ya], W=[ya])
                            else:
                                if first:
                                    kb.op("dve", lambda: nc.vector.tensor_copy(out=ysl, in_=py[:, :]), R=[py], W=[ya])
                                else:
                                    kb.op("dve", lambda: nc.vector.tensor_tensor(out=ysl, in0=py[:, :], in1=ysl,
                                                                                 op=ALU.add), R=[py, ya], W=[ya])
                    first = False
            for tt in range(4):
                tok0 = g * TG + tt * 128
                kb.op("dve", lambda: nc.vector.scalar_tensor_tensor(out=h[:, :], in0=x1g[tt][:, :], scalar=ALPHA,
                                                                    in1=yacc[tt][:, :], op0=ALU.mult, op1=ALU.add),
                      R=[x1g[tt], yacc[tt]], W=[h])
                o = ost[tt % 2]
                layer_norm(kb, h, (lnp, lnp[:, 2, :]), (lnp, lnp[:, 3, :]), o[:, :], o, scr)
                kb.dma("sp", x_dst[tok0:tok0 + 128, :], o[:, :], R=[o], Wd=[x_dst])
                if not last:
                    emit_xT(kb, X, ident, o, g * 4 + tt, stg, xT_loc, xTs_loc)


def _dbg_out(kb, x_d, out_d):
    for tt in range(4):
        kb.dma("sp", out_d[tt * 512:(tt + 1) * 512, :], x_d[tt * 512:(tt + 1) * 512, :], R=[x_d], Wd=[out_d])
    return kb.finish()


PROFILE_SCOPES = False


class _NullScope:
    def __enter__(self):
        return self

    def __exit__(self, *a):
        return False


def _scope(nc, name):
    return nc.named_scope(name) if PROFILE_SCOPES else _NullScope()


def build_fused(layers=(0, 1, 2, 3), upto=9):
    kb = KBF()
    nc = kb.nc
    x_d = kb.dram_in("x", [TPC, D], F32)
    out_d = kb.dram_out("out", [TPC, D], F32)
    cst = {"tri": kb.dram_in("tri", [128, 128], F32), "identb": kb.dram_in("identb", [128, 128], BF16),
           "ident": kb.dram_in("ident", [128, 128], F32), "dilc": kb.dram_in("dilc", [32, NDEL], F32),
           "dsac": kb.dram_in("dsac", [32, NDEL], F32), "cmask": kb.dram_in("cmask", [128, 512], F32),
           "steps": kb.dram_in("steps", [128, NBIS], F32), "idx_s": kb.dram_in("idx_s", [128, 32], I32),
           "idx_o": kb.dram_in("idx_o", [128, 32], I32)}
    Ls = {}
    for l in layers:
        L = {}
        pre = "L%d_" % l
        moe = (l % 2 == 1)
        if l % 2 == 0:
            L["wgl"] = kb.dram_in(pre + "wgl", [128, 8, 464], F32)
            L["wg"] = kb.dram_in(pre + "wg", [16, 64], F32)
            L["bg"] = kb.dram_in(pre + "bg", [128, 64], F32)
            L["gn"] = kb.dram_in(pre + "gn", [128, 128], F32)
            L["wd1"] = kb.dram_in(pre + "wd1", [128, 8, 584], F32)
            L["wd2"] = kb.dram_in(pre + "wd2", [128, 8, 384], F32)
            L["relfar"] = kb.dram_in(pre + "relfar", [128, 2], F32)
        else:
            L["wcd"] = kb.dram_in(pre + "wcd", [128, 8, 770], F32)
            L["bf"] = kb.dram_in(pre + "bf", [128, 2], F32)
            L["wr"] = kb.dram_in(pre + "wr", [128, 8, 8], F32)
        L["rel2"] = kb.dram_in(pre + "rel2", [32, 2], F32)
        L["wo"] = kb.dram_in(pre + "wo", [128, 8, D], F32)
        L["lnp"] = kb.dram_in(pre + "lnp", [128, 4, D], F32)
        L["wf"] = kb.dram_in(pre + "wf", [(224 if moe else 22) if upto >= 9 else 1, 128, 3072], F32)
        L["Escr"] = kb.dram_tmp(pre + "Escr", [2, NDEL], F32)
        Ls[l] = L
    xres = [kb.dram_tmp("xres0", [TPC, D], F32), kb.dram_tmp("xres1", [TPC, D], F32)]
    xT_loc = kb.dram_tmp("xT_loc", [D, TPC], BF16)
    xT_all = kb.dram_tmp("xT_all", [4 * D, TPC], BF16)
    xTs_loc = kb.dram_tmp("xTs_loc", [4 * D, 512], BF16)
    xTs_all = kb.dram_tmp("xTs_all", [16 * D, 512], BF16)
    oT_loc = kb.dram_tmp("oT_loc", [16 * 256, 512], BF16)
    oT_all = kb.dram_tmp("oT_all", [64 * 256, 512], BF16)
    M_loc = kb.dram_tmp("M_loc", [MTOT // 512, 512], BF16)
    M_all = kb.dram_tmp("M_all", [4 * MTOT // 512, 512], BF16)
    assert MPARTS[-1][4] + 4 * MPARTS[-1][3] == 4 * MTOT // 512
    with kb.scope():
        ident = kb.sb([128, 128], F32, "ident")
        kb.dma("sp", ident[:, :], cst["ident"][:, :], R=[cst["ident"]], W=[ident])
        xin = [kb.sb([128, D], F32, "xin") for _ in range(2)]
        stg = kb.sb([128, 8, 512], BF16, "stgx")
        X = [kb.ps([128, 512], F32, "X") for _ in range(2)]
        for tt in range(TPC // 128):
            xi = xin[tt % 2]
            kb.dma("sp", xi[:, :], x_d[tt * 128:(tt + 1) * 128, :], R=[x_d], W=[xi])
            emit_xT(kb, X, ident, xi, tt, stg, xT_loc, xTs_loc)
    if upto == 0:
        return _dbg_out(kb, x_d, out_d)
    x_src = x_d
    for n, l in enumerate(layers):
        L = Ls[l]
        last = (n == len(layers) - 1)
        x_dst = out_d if last else xres[n % 2]
        for cf in range(4):
            kb.collective("AllGather", xT_loc, xT_all, xT_loc[cf * 256:(cf + 1) * 256, :],
                          xT_all[cf * 1024:(cf + 1) * 1024, :])
        if upto == 1:
            return _dbg_out(kb, x_d, out_d)
        if l % 2 == 0:
            for j in range(4):
                kb.collective("AllGather", xTs_loc, xTs_all, xTs_loc[j * 1024:(j + 1) * 1024, :],
                              xTs_all[j * 4096:(j + 1) * 4096, :])
            with _scope(nc, "L%d_gla" % l):
                phase_gla(kb, L, xT_all, oT_loc, cst)
            with _scope(nc, "L%d_dsa1" % l):
                phase_dsa1(kb, L, xT_all, xTs_all, M_loc, M_all, cst)
            with _scope(nc, "L%d_dsa2" % l):
                phase_dsa2(kb, L, xT_all, M_all, oT_loc, oT_all, cst)
        else:
            with _scope(nc, "L%d_cd" % l):
                phase_cd(kb, L, xT_all, oT_loc, oT_all, cst)
        if upto == 2:
            return _dbg_out(kb, x_d, out_d)
        if upto == 3:
            return _dbg_out(kb, x_d, out_d)
        with _scope(nc, "L%d_c" % l):
            phase_c(kb, L, l % 2 == 1, oT_all, x_src, x_dst, xT_loc, xTs_loc, cst, last)
        x_src = x_dst
    return kb.finish()


def fused_inputs(x, ln_g, ln_b, rel_table, w_in_ab, w_gate_a, b_gate_a, g_norm_a, w_out_ab,
                 w_in_cd, b_forget, w_out_cd, w1_dense, w3_dense, w2_dense,
                 w_router, w1_moe, w3_moe, w2_moe, layers=(0, 1, 2, 3)):
    f32 = lambda a: np.ascontiguousarray(np.asarray(a, dtype=np.float32))
    bc = lambda v, n=128: np.ascontiguousarray(np.broadcast_to(np.asarray(v, np.float32)[None, :], (n, len(v))))
    xf = f32(x).reshape(BATCH * SEQ, D)
    rel_table = f32(rel_table)
    steps = bc(0.5 ** np.arange(1, NBIS + 1))
    perm = np.zeros(D, np.int64)
    for src in range(4):
        for rr in range(256):
            perm[src * 256 + rr] = src * 128 + rr if rr < 128 else 512 + src * 128 + (rr - 128)
    shared = {"tri": TRI, "identb": IDENT.astype(NPBF), "ident": IDENT, "dilc": dil_const(), "dsac": dsa_const(),
              "steps": steps}
    per_layer_shared = {}
    for l in layers:
        j = l // 2
        pre = "L%d_" % l
        d = {}
        d[pre + "lnp"] = np.ascontiguousarray(np.broadcast_to(
            np.stack([ln_g[l, 0], ln_b[l, 0], ln_g[l, 1], ln_b[l, 1]]).astype(np.float32)[None], (128, 4, D)))
        if l % 2 == 0:
            d[pre + "wo"] = w_kc_layout(f32(w_out_ab[j])[perm])
            d[pre + "wf"] = ffn_chunk_layout(f32(w1_dense[j]), f32(w3_dense[j]), f32(w2_dense[j]))
            d[pre + "gn"] = bc(g_norm_a[j])
        else:
            d[pre + "wo"] = w_kc_layout(f32(w_out_cd[j])[perm])
            d[pre + "wf"] = np.concatenate([ffn_chunk_layout(f32(w1_moe[j, e]), f32(w3_moe[j, e]), f32(w2_moe[j, e]))
                                            for e in range(8)], axis=0)
            d[pre + "wr"] = np.ascontiguousarray(f32(w_router[j]).reshape(8, 128, 8).transpose(1, 0, 2))
        per_layer_shared.update(d)
    maps = []
    for c in range(NCORES):
        b, m = c // 4, c % 4
        mp = dict(shared)
        mp.update(per_layer_shared)
        mp["x"] = np.ascontiguousarray(xf[c * TPC:(c + 1) * TPC])
        pp_ = np.arange(128)[:, None]
        rr_, k8_ = np.arange(4)[None, :, None], np.arange(8)[None, None, :]
        mp["idx_s"] = np.ascontiguousarray(((m * 4 + rr_) * 1024 + k8_ * 128 + pp_[:, :, None]).reshape(128, 32)
                                           .astype(np.int32))
        G_ = k8_ * 128 + pp_[:, :, None]
        mp["idx_o"] = np.ascontiguousarray(((m * 4 + G_ // 256) * 1024 + rr_ * 256 + G_ % 256).reshape(128, 32)
                                           .astype(np.int32))
        mp["cmask"] = np.where(np.arange(512)[None, :] <= (128 * m + np.arange(128))[:, None], 0.0, NEG).astype(np.float32)
        for l in layers:
            j = l // 2
            pre = "L%d_" % l
            mp[pre + "rel2"] = np.ascontiguousarray(rel_table[:, 2 * m:2 * m + 2])
            if l % 2 == 0:
                w = f32(w_in_ab[j])
                cs = lambda a, n: w[:, a:a + n]
                wgl = np.concatenate([cs(m * 64, 64), cs(256 + m * 64, 64), cs(1536, 16), cs(256 + m * 64, 64),
                                      cs(512 + m * 128, 128), cs(1024 + m * 128, 128)], axis=1)
                wd1 = np.concatenate([cs(3088, 512), cs(3600, 64), cs(3664, 8)], axis=1)
                wd2 = np.concatenate([cs(1552 + m * 128, 128), cs(2064 + m * 128, 128), cs(2576 + m * 128, 128)], axis=1)
                mp[pre + "wgl"] = w_kc_layout(wgl)
                mp[pre + "wd1"] = w_kc_layout(wd1)
                mp[pre + "wd2"] = w_kc_layout(wd2)
                mp[pre + "wg"] = np.ascontiguousarray(f32(w_gate_a[j])[:, m * 64:(m + 1) * 64])
                mp[pre + "bg"] = bc(f32(b_gate_a[j])[m * 64:(m + 1) * 64])
                mp[pre + "relfar"] = bc(rel_table[31, 2 * m:2 * m + 2])
            else:
                w = f32(w_in_cd[j])
                cs = lambda a, n: w[:, a:a + n]
                wcd = np.concatenate([cs(m * 128, 128), cs(512 + m * 128, 128), cs(1544 + m * 128, 128),
                                      cs(2056 + m * 128, 128), cs(1024 + m * 128, 128), cs(2568 + m * 128, 128),
                                      cs(1536 + 2 * m, 2)], axis=1)
                mp[pre + "wcd"] = w_kc_layout(wcd)
                mp[pre + "bf"] = bc(f32(b_forget[j])[2 * m:2 * m + 2])
        maps.append(mp)
    return maps


def kernel_fused(**inputs):
    nc = build_fused()
    maps = fused_inputs(**inputs)
    res = run_spmd(nc, maps)
    out = np.concatenate([res[c]["out"] for c in range(NCORES)], axis=0)
    return out.reshape(BATCH, SEQ, D).astype(np.float32)


def kernel(**inputs):
    return kernel_fused(**inputs)
```

```python
import math
from contextlib import ExitStack
import numpy as np
import ml_dtypes
import concourse.bass as bass
import concourse.mybir as mybir
from concourse.bass_utils import run_bass_kernel_spmd

F32 = mybir.dt.float32
BF16 = mybir.dt.bfloat16
AF = mybir.ActivationFunctionType
ALU = mybir.AluOpType
AX = mybir.AxisListType
NPBF = ml_dtypes.bfloat16

NCORES = 8
D = 1024
SEQ = 8192
BATCH = 2
DEPTH = 4
ALPHA = (2 * DEPTH) ** 0.25
LN_EPS = 1e-5
NEG = -1.0e30


class Buf:
    def __init__(self, t):
        self.t = t
        self.w = None
        self.r = []

    def __getitem__(self, idx):
        return self.t[idx]


class KB:
    NDS = 6

    def __init__(self):
        self.nc = bass.Bass("TRN2", target_bir_lowering=False)
        nc = self.nc
        self.es = ExitStack()
        self.eng = {"pe": nc.tensor, "dve": nc.vector, "act": nc.scalar, "pool": nc.gpsimd, "sp": nc.sync}
        self.esem = {}
        self.ecnt = {}
        self.seen = {e: {} for e in self.eng}
        for e in self.eng:
            self.esem[e] = self.es.enter_context(nc.semaphore("sem_" + e))
            self.ecnt[e] = 0
        self.dsem = {}
        self.dcnt = {}
        self.dnext = {}
        for q in ("sp", "act", "pool"):
            self.dsem[q] = [self.es.enter_context(nc.semaphore("dsem_%s%d" % (q, i))) for i in range(self.NDS)]
            self.dcnt[q] = [0] * self.NDS
            self.dnext[q] = 0
        self.n_names = 0
        self.outs = []

    def _nm(self, p):
        self.n_names += 1
        return "%s_%d" % (p, self.n_names)

    def dram_in(self, name, shape, dt):
        return Buf(self.nc.dram_tensor(name, list(shape), dt, kind="ExternalInput").ap())

    def dram_out(self, name, shape, dt):
        b = Buf(self.nc.dram_tensor(name, list(shape), dt, kind="ExternalOutput").ap())
        self.outs.append(b)
        return b

    def dram_tmp(self, name, shape, dt):
        return Buf(self.nc.dram_tensor(name, list(shape), dt, kind="Internal").ap())

    def sb(self, shape, dt, name="sb"):
        return Buf(self.es.enter_context(self.nc.sbuf_tensor(self._nm(name), list(shape), dt)))

    def ps(self, shape, dt=F32, name="ps"):
        return Buf(self.es.enter_context(self.nc.psum_tensor(self._nm(name), list(shape), dt)))

    def _wait(self, e, ev):
        if ev is None:
            return
        sem, val, key = ev
        if self.seen[e].get(key, 0) >= val:
            return
        self.eng[e].wait_ge(sem, val)
        self.seen[e][key] = val

    def _deps(self, e, R, W):
        evs = []
        for b in R:
            if b.w is not None:
                evs.append(b.w)
        for b in W:
            if b.w is not None:
                evs.append(b.w)
            evs.extend(b.r)
        best = {}
        for ev in evs:
            k = ev[2]
            if e == "pe" and k == "pe":
                continue
            if k not in best or best[k][1] < ev[1]:
                best[k] = ev
        for ev in best.values():
            self._wait(e, ev)

    def _mark(self, ev, R, W):
        for b in R:
            b.r.append(ev)
            if len(b.r) > 24:
                best = {}
                for x in b.r:
                    if x[2] not in best or best[x[2]][1] < x[1]:
                        best[x[2]] = x
                b.r = list(best.values())
        for b in W:
            b.w = ev
            b.r = []

    def op(self, e, fn, R=(), W=()):
        self._deps(e, R, W)
        ins = fn()
        self.ecnt[e] += 1
        ins.then_inc(self.esem[e], 1)
        ev = (self.esem[e], self.ecnt[e], e)
        self.seen[e][e] = max(self.seen[e].get(e, 0), 0)
        self._mark(ev, R, W)
        return ev

    def dma(self, q, out, in_, R=(), W=(), **kw):
        self._deps(q, R, W)
        i = self.dnext[q]
        self.dnext[q] = (i + 1) % self.NDS
        key = "d%s%d" % (q, i)
        if self.dcnt[q][i] > 0:
            self._wait(q, (self.dsem[q][i], self.dcnt[q][i], key))
        self.dcnt[q][i] += 16
        self.eng[q].dma_start(out=out, in_=in_, **kw).then_inc(self.dsem[q][i], 16)
        ev = (self.dsem[q][i], self.dcnt[q][i], key)
        self._mark(ev, R, W)
        return ev

    def finish(self):
        for q in ("sp", "act", "pool"):
            for i in range(self.NDS):
                if self.dcnt[q][i] > 0:
                    self._wait("sp", (self.dsem[q][i], self.dcnt[q][i], "d%s%d" % (q, i)))
        for e in ("pe", "dve", "act", "pool"):
            if self.ecnt[e] > 0:
                self._wait("sp", (self.esem[e], self.ecnt[e], e))
        self.es.close()
        return self.nc


def run_spmd(nc, in_maps):
    res = run_bass_kernel_spmd(nc, in_maps, core_ids=list(range(NCORES)))
    return res.results


def build_cast(n):
    kb = KB()
    nc = kb.nc
    CH = 2048
    src = kb.dram_in("src", [128, n], F32)
    dst = kb.dram_out("dst", [128, n], BF16)
    NB = 3
    tin = [kb.sb([128, CH], F32, "tin") for _ in range(NB)]
    tout = [kb.sb([128, CH], BF16, "tout") for _ in range(NB)]
    nch = (n + CH - 1) // CH
    for c in range(nch):
        c0 = c * CH
        w = min(CH, n - c0)
        a, b = tin[c % NB], tout[c % NB]
        kb.dma("sp", a[:, :w], src[:, c0:c0 + w], R=[src], W=[a])
        if c % 2 == 0:
            kb.op("dve", lambda: nc.vector.tensor_copy(out=b[:, :w], in_=a[:, :w]), R=[a], W=[b])
        else:
            kb.op("act", lambda: nc.scalar.copy(out=b[:, :w], in_=a[:, :w]), R=[a], W=[b])
        kb.dma("pool", dst[:, c0:c0 + w], b[:, :w], R=[b], W=[dst])
    return kb.finish()


def cast_weights(arrs):
    flats = [np.ascontiguousarray(a).reshape(NCORES, 128, -1) for a in arrs]
    ns = [f.shape[2] for f in flats]
    cat = np.concatenate(flats, axis=2)
    n = cat.shape[2]
    nc = build_cast(n)
    res = run_spmd(nc, [{"src": np.ascontiguousarray(cat[c])} for c in range(NCORES)])
    out = np.stack([res[c]["dst"] for c in range(NCORES)], axis=0)
    outs = []
    o = 0
    for a, k in zip(arrs, ns):
        outs.append(out[:, :, o:o + k].reshape(a.shape))
        o += k
    return outs


TPC = 2048


def build_A(W, fm, tmb, tmf, gate=None):
    kb = KB()
    nc = kb.nc
    NT = TPC // 128
    n_fm = sum(n for _, n in fm)
    n_tmb = sum(n for _, n in tmb)
    n_tmf = sum(n for _, n in tmf) + (256 if gate is not None else 0)
    x = kb.dram_in("x", [TPC, D], F32)
    w = kb.dram_in("w", [128, 8, W], BF16)
    ident_d = kb.dram_in("ident", [128, 128], F32)
    yT = kb.dram_out("yT", [n_fm, TPC], BF16)
    ytb = kb.dram_out("ytb", [TPC, max(n_tmb, 1)], BF16)
    ytf = kb.dram_out("ytf", [TPC, max(n_tmf, 1)], F32)
    wsb = kb.sb([128, 8, W], BF16, "w")
    ident = kb.sb([128, 128], F32, "ident")
    xT = kb.sb([128, 8, TPC], BF16, "xT")
    kb.dma("sp", ident[:, :], ident_d[:, :], R=[ident_d], W=[ident])
    for kc in range(8):
        kb.dma("pool" if kc % 2 else "sp", wsb[:, kc, :], w[:, kc, :], R=[w], W=[wsb])
    if gate is not None:
        wg_d = kb.dram_in("wg", [16, 256], F32)
        bg_d = kb.dram_in("bg", [128, 256], F32)
        wg = kb.sb([16, 256], F32, "wg")
        bg = kb.sb([128, 256], F32, "bg")
        gaT = kb.sb([16, TPC], F32, "gaT")
        w32 = kb.sb([128, 8, 16], F32, "w32")
        kb.dma("sp", wg[:, :], wg_d[:, :], R=[wg_d], W=[wg])
        kb.dma("sp", bg[:, :], bg_d[:, :], R=[bg_d], W=[bg])
    xin = [kb.sb([128, D], F32, "xin") for _ in range(2)]
    pst = [kb.ps([128, 1024], F32, "pst")]
    for tt in range(NT):
        xi = xin[tt % 2]
        kb.dma("sp", xi[:, :], x[tt * 128:(tt + 1) * 128, :], R=[x], W=[xi])
        pt = pst[0]
        for kc in range(8):
            kb.op("pe", lambda: nc.tensor.transpose(pt[:, kc * 128:(kc + 1) * 128], xi[:, kc * 128:(kc + 1) * 128],
                                                    ident[:, :]), R=[xi, ident], W=[pt])
        for hf in range(2):
            src = pt[:, hf * 512:(hf + 1) * 512].rearrange("p (k t) -> p k t", k=4)
            dst = xT[:, hf * 4:(hf + 1) * 4, tt * 128:(tt + 1) * 128]
            if hf == 0:
                kb.op("dve", lambda: nc.vector.tensor_copy(out=dst, in_=src), R=[pt], W=[xT])
            else:
                kb.op("act", lambda: nc.scalar.copy(out=dst, in_=src), R=[pt], W=[xT])
    psf = [kb.ps([128, 512], F32, "psf") for _ in range(2)]
    stf = [kb.sb([128, 512], BF16, "stf") for _ in range(3)]
    cnt = 0
    row = 0
    blocks = list(fm)
    for (c0, ncol) in blocks:
        for tg in range(TPC // 512):
            pp = psf[cnt % 2]
            st = stf[cnt % 3]
            for kc in range(8):
                kb.op("pe", lambda: nc.tensor.matmul(pp[:ncol, :], lhsT=wsb[:, kc, c0:c0 + ncol],
                                                     rhs=xT[:, kc, tg * 512:(tg + 1) * 512],
                                                     start=(kc == 0), stop=(kc == 7)), R=[wsb, xT], W=[pp])
            if cnt % 2 == 0:
                kb.op("dve", lambda: nc.vector.tensor_copy(out=st[:ncol, :], in_=pp[:ncol, :]), R=[pp], W=[st])
            else:
                kb.op("act", lambda: nc.scalar.copy(out=st[:ncol, :], in_=pp[:ncol, :]), R=[pp], W=[st])
            kb.dma("pool" if cnt % 2 else "sp", yT[row:row + ncol, tg * 512:(tg + 1) * 512], st[:ncol, :],
                   R=[st], W=[yT])
            cnt += 1
        row += ncol
    if gate is not None:
        g0 = gate[0]
        for tg in range(TPC // 512):
            pp = psf[cnt % 2]
            for kc in range(8):
                kb.op("pe", lambda: nc.tensor.matmul(pp[:16, :], lhsT=wsb[:, kc, g0:g0 + 16],
                                                     rhs=xT[:, kc, tg * 512:(tg + 1) * 512],
                                                     start=(kc == 0), stop=(kc == 7)), R=[wsb, xT], W=[pp])
            kb.op("dve", lambda: nc.vector.tensor_copy(out=gaT[:, tg * 512:(tg + 1) * 512], in_=pp[:16, :]),
                  R=[pp], W=[gaT])
            cnt += 1
    pstm = [kb.ps([128, 512], F32, "pstm") for _ in range(2)]
    stb = [kb.sb([128, 512], BF16, "stb") for _ in range(3)]
    st32 = [kb.sb([128, 512], F32, "st32") for _ in range(3)]
    cnt = 0
    for tt in range(NT):
        for kind, lst, dst_d in (("b", tmb, ytb), ("f", tmf, ytf)):
            off = 0
            for (c0, ncol) in lst:
                pp = pstm[cnt % 2]
                st = (stb if kind == "b" else st32)[cnt % 3]
                for kc in range(8):
                    kb.op("pe", lambda: nc.tensor.matmul(pp[:, :ncol], lhsT=xT[:, kc, tt * 128:(tt + 1) * 128],
                                                         rhs=wsb[:, kc, c0:c0 + ncol],
                                                         start=(kc == 0), stop=(kc == 7)), R=[wsb, xT], W=[pp])
                if cnt % 2 == 0:
                    kb.op("dve", lambda: nc.vector.tensor_copy(out=st[:, :ncol], in_=pp[:, :ncol]), R=[pp], W=[st])
                else:
                    kb.op("act", lambda: nc.scalar.copy(out=st[:, :ncol], in_=pp[:, :ncol]), R=[pp], W=[st])
                kb.dma("pool" if cnt % 2 else "sp", dst_d[tt * 128:(tt + 1) * 128, off:off + ncol], st[:, :ncol],
                       R=[st], W=[dst_d])
                off += ncol
                cnt += 1
        if gate is not None:
            off = sum(n for _, n in tmf)
            pp = pstm[cnt % 2]
            st = st32[cnt % 3]
            kb.op("pe", lambda: nc.tensor.matmul(pp[:, :256], lhsT=gaT[:, tt * 128:(tt + 1) * 128], rhs=wg[:, :],
                                                 start=True, stop=True), R=[gaT, wg], W=[pp])
            kb.op("dve", lambda: nc.vector.tensor_tensor(out=st[:, :256], in0=pp[:, :256], in1=bg[:, :], op=ALU.add),
                  R=[pp, bg], W=[st])
            kb.op("act", lambda: nc.scalar.activation(out=st[:, :256], in_=st[:, :256], func=AF.Exp, scale=-1.0),
                  R=[st], W=[st])
            kb.op("act", lambda: nc.scalar.activation(out=st[:, :256], in_=st[:, :256], func=AF.Ln, bias=1.0),
                  R=[st], W=[st])
            kb.op("dve", lambda: nc.vector.tensor_scalar(out=st[:, :256], in0=st[:, :256], scalar1=-1.0 / 16.0,
                                                         scalar2=None, op0=ALU.mult), R=[st], W=[st])
            kb.dma("sp", ytf[tt * 128:(tt + 1) * 128, off:off + 256], st[:, :256], R=[st], W=[ytf])
            cnt += 1
    return kb.finish()


def blocks_of(c0, n, bs):
    out = []
    while n > 0:
        k = min(bs, n)
        out.append((c0, k))
        c0 += k
        n -= k
    return out


def w_kc_layout(w):
    W = w.shape[1]
    return np.ascontiguousarray(w.reshape(8, 128, W).transpose(1, 0, 2))


IDENT = np.eye(128, dtype=np.float32)


def run_A(x_flat, w_bf, fm_ranges, tmb_ranges, tmf_ranges, gate=None, wg=None, bg=None):
    W = w_bf.shape[1]
    fm = [b for (c0, n) in fm_ranges for b in blocks_of(c0, n, 128)]
    tmb = [b for (c0, n) in tmb_ranges for b in blocks_of(c0, n, 512)]
    tmf = [b for (c0, n) in tmf_ranges for b in blocks_of(c0, n, 512)]
    nc = build_A(W, fm, tmb, tmf, gate)
    wl = w_kc_layout(w_bf)
    maps = []
    for c in range(NCORES):
        m = {"x": np.ascontiguousarray(x_flat[c * TPC:(c + 1) * TPC]), "w": wl, "ident": IDENT}
        if gate is not None:
            m["wg"] = np.ascontiguousarray(wg)
            m["bg"] = np.ascontiguousarray(np.broadcast_to(bg[None, :], (128, 256)))
        maps.append(m)
    res = run_spmd(nc, maps)
    yT = np.concatenate([res[c]["yT"] for c in range(NCORES)], axis=1)
    ytb = np.concatenate([res[c]["ytb"] for c in range(NCORES)], axis=0)
    ytf = np.concatenate([res[c]["ytf"] for c in range(NCORES)], axis=0)
    return yT, ytb, ytf


def layer_norm(kb, h, gt, bt, out_ap, out_buf, scr):
    nc = kb.nc
    stats, mv, sd = scr
    for c in range(2):
        kb.op("dve", lambda: nc.vector.bn_stats(out=stats[:, c, :], in_=h[:, c * 512:(c + 1) * 512]), R=[h], W=[stats])
    kb.op("dve", lambda: nc.vector.bn_aggr(out=mv[:, :], in_=stats[:, :, :].rearrange("p a b -> p (a b)")),
          R=[stats], W=[mv])
    kb.op("act", lambda: nc.scalar.activation(out=sd[:, 0:1], in_=mv[:, 1:2], func=AF.Sqrt, bias=kb.eps_col[:, 0:1]),
          R=[mv, kb.eps_col], W=[sd])
    kb.op("dve", lambda: nc.vector.reciprocal(out=sd[:, 1:2], in_=sd[:, 0:1]), R=[sd], W=[sd])
    kb.op("dve", lambda: nc.vector.tensor_scalar(out=h[:, :], in0=h[:, :], scalar1=mv[:, 0:1], scalar2=sd[:, 1:2],
                                                 op0=ALU.subtract, op1=ALU.mult), R=[h, mv, sd], W=[h])
    kb.op("pool", lambda: nc.gpsimd.tensor_tensor(out=h[:, :], in0=h[:, :], in1=gt[1], op=ALU.mult),
          R=[h, gt[0]], W=[h])
    kb.op("dve", lambda: nc.vector.tensor_tensor(out=out_ap, in0=h[:, :], in1=bt[1], op=ALU.add),
          R=[h, bt[0]], W=[out_buf])


def build_C(n_exp, nf_unit, n_units_per_exp):
    kb = KB()
    nc = kb.nc
    moe = n_exp > 1
    NT = TPC // 128
    TG = 512
    NG = TPC // TG
    nfu = nf_unit
    n_chunks = n_exp * n_units_per_exp * nfu
    oT_d = kb.dram_in("oT", [128, 8, TPC], BF16)
    x_d = kb.dram_in("x", [TPC, D], F32)
    wo_d = kb.dram_in("wo", [128, 8, D], BF16)
    lnp_d = kb.dram_in("lnp", [128, 4, D], F32)
    wf_d = kb.dram_in("wf", [n_chunks, 128, 3072], BF16)
    ident_d = kb.dram_in("ident", [128, 128], F32)
    out_d = kb.dram_out("out", [TPC, D], F32)
    ident = kb.sb([128, 128], F32, "ident")
    wo = kb.sb([128, 8, D], BF16, "wo")
    lnp = kb.sb([128, 4, D], F32, "lnp")
    kb.eps_col = kb.sb([128, 1], F32, "eps")
    kb.op("dve", lambda: nc.vector.memset(kb.eps_col[:, :], LN_EPS), W=[kb.eps_col])
    kb.dma("sp", ident[:, :], ident_d[:, :], R=[ident_d], W=[ident])
    kb.dma("sp", wo[:, :, :], wo_d[:, :, :], R=[wo_d], W=[wo])
    kb.dma("pool", lnp[:, :, :], lnp_d[:, :, :], R=[lnp_d], W=[lnp])
    if moe:
        wr_d = kb.dram_in("wr", [128, 8, 8], F32)
        wr = kb.sb([128, 8, 8], F32, "wr")
        kb.dma("sp", wr[:, :, :], wr_d[:, :, :], R=[wr_d], W=[wr])
        x1T32 = kb.sb([128, 8, 128], F32, "x1T32")
        comb = [kb.sb([128, 8], F32, "comb") for _ in range(4)]
        rt = kb.sb([128, 40], F32, "rt")
    oT = [kb.sb([128, 8, TG], BF16, "oT") for _ in range(2)]
    xt = [kb.sb([128, D], F32, "xt") for _ in range(2)]
    h = kb.sb([128, D], F32, "h")
    x1g = [kb.sb([128, D], F32, "x1g") for _ in range(4)]
    x1T = kb.sb([128, 8, TG], BF16, "x1T")
    aT = [kb.sb([128, TG], BF16, "aT") for _ in range(nfu)]
    w2 = [kb.sb([128, D], BF16, "w2") for _ in range(nfu)]
    w13 = [kb.sb([128, 2048], BF16, "w13") for _ in range(3)]
    yacc = [kb.sb([128, D], F32, "yacc") for _ in range(4)]
    sil = [kb.sb([128, TG], F32, "sil") for _ in range(2)]
    ost = [kb.sb([128, D], F32, "ost") for _ in range(2)]
    scr = (kb.sb([128, 2, 6], F32, "stats"), kb.sb([128, 2], F32, "mv"), kb.sb([128, 2], F32, "sd"))
    X = [kb.ps([128, 512], F32, "X") for _ in range(4)]
    Y = [kb.ps([128, 512], F32, "Y") for _ in range(2)]
    wcnt = 0
    for g in range(NG):
        og = oT[g % 2]
        kb.dma("pool", og[:, :, :], oT_d[:, :, g * TG:(g + 1) * TG], R=[oT_d], W=[og])
        for tt in range(4):
            tok0 = g * TG + tt * 128
            xi = xt[tt % 2]
            kb.dma("sp", xi[:, :], x_d[tok0:tok0 + 128, :], R=[x_d], W=[xi])
            for hf in range(2):
                for kc in range(8):
                    kb.op("pe", lambda: nc.tensor.matmul(Y[hf][:, :], lhsT=og[:, kc, tt * 128:(tt + 1) * 128],
                                                         rhs=wo[:, kc, hf * 512:(hf + 1) * 512],
                                                         start=(kc == 0), stop=(kc == 7)), R=[og, wo], W=[Y[hf]])
                kb.op("dve", lambda: nc.vector.scalar_tensor_tensor(out=h[:, hf * 512:(hf + 1) * 512],
                                                                    in0=xi[:, hf * 512:(hf + 1) * 512], scalar=ALPHA,
                                                                    in1=Y[hf][:, :], op0=ALU.mult, op1=ALU.add),
                      R=[xi, Y[hf]], W=[h])
            x1 = x1g[tt]
            layer_norm(kb, h, (lnp, lnp[:, 0, :]), (lnp, lnp[:, 1, :]), x1[:, :], x1, scr)
            for kc in range(8):
                pt = X[kc // 4]
                kb.op("pe", lambda: nc.tensor.transpose(pt[:, (kc % 4) * 128:(kc % 4 + 1) * 128],
                                                        x1[:, kc * 128:(kc + 1) * 128], ident[:, :]),
                      R=[x1, ident], W=[pt])
            for hf in range(2):
                src = X[hf][:, :].rearrange("p (k t) -> p k t", k=4)
                dst = x1T[:, hf * 4:(hf + 1) * 4, tt * 128:(tt + 1) * 128]
                if hf == 0:
                    kb.op("dve", lambda: nc.vector.tensor_copy(out=dst, in_=src), R=[X[hf]], W=[x1T])
                else:
                    kb.op("act", lambda: nc.scalar.copy(out=dst, in_=src), R=[X[hf]], W=[x1T])
                if moe:
                    kb.op("pool" if False else "dve",
                          lambda: nc.vector.tensor_copy(out=x1T32[:, hf * 4:(hf + 1) * 4, :], in_=src),
                          R=[X[hf]], W=[x1T32])
            if moe:
                pr = X[2]
                for kc in range(8):
                    kb.op("pe", lambda: nc.tensor.matmul(pr[:, :8], lhsT=x1T32[:, kc, :], rhs=wr[:, kc, :],
                                                         start=(kc == 0), stop=(kc == 7)), R=[x1T32, wr], W=[pr])
                cb = comb[tt]
                lg, mx, tmp, oh = rt[:, 0:8], rt[:, 8:16], rt[:, 16:24], rt[:, 24:32]
                sc = rt[:, 32:40]
                kb.op("dve", lambda: nc.vector.tensor_copy(out=lg, in_=pr[:, :8]), R=[pr], W=[rt])
                kb.op("dve", lambda: nc.vector.max(out=mx, in_=lg), R=[rt], W=[rt])
                kb.op("dve", lambda: nc.vector.tensor_tensor(out=sc[:, 0:1], in0=mx[:, 1:2], in1=mx[:, 0:1],
                                                             op=ALU.subtract), R=[rt], W=[rt])
                kb.op("act", lambda: nc.scalar.activation(out=sc[:, 1:2], in_=sc[:, 0:1], func=AF.Exp), R=[rt], W=[rt])
                kb.op("dve", lambda: nc.vector.tensor_scalar(out=sc[:, 2:3], in0=sc[:, 1:2], scalar1=1.0, scalar2=None,
                                                             op0=ALU.add), R=[rt], W=[rt])
                kb.op("dve", lambda: nc.vector.reciprocal(out=sc[:, 3:4], in_=sc[:, 2:3]), R=[rt], W=[rt])
                kb.op("dve", lambda: nc.vector.tensor_tensor(out=sc[:, 4:5], in0=sc[:, 1:2], in1=sc[:, 3:4],
                                                             op=ALU.mult), R=[rt], W=[rt])
                kb.op("dve", lambda: nc.vector.tensor_scalar(out=tmp, in0=lg, scalar1=mx[:, 0:1], scalar2=sc[:, 3:4],
                                                             op0=ALU.is_equal, op1=ALU.mult), R=[rt], W=[rt])
                kb.op("dve", lambda: nc.vector.tensor_scalar(out=oh, in0=lg, scalar1=mx[:, 1:2], scalar2=sc[:, 4:5],
                                                             op0=ALU.is_equal, op1=ALU.mult), R=[rt], W=[rt])
                kb.op("dve", lambda: nc.vector.tensor_tensor(out=cb[:, :], in0=tmp, in1=oh, op=ALU.add),
                      R=[rt], W=[cb])
        first = True
        for e in range(n_exp):
            for u in range(n_units_per_exp):
                base = (e * n_units_per_exp + u) * nfu
                for f in range(nfu):
                    wc = w13[wcnt % 3]
                    q = "sp" if wcnt % 2 == 0 else "pool"
                    kb.dma(q, wc[:, :], wf_d[base + f, :, 0:2048], R=[wf_d], W=[wc])
                    kb.dma("pool" if wcnt % 2 == 0 else "sp", w2[f][:, :], wf_d[base + f, :, 2048:3072],
                           R=[wf_d], W=[w2[f]])
                    h1, h3 = X[(wcnt % 2) * 2], X[(wcnt % 2) * 2 + 1]
                    for kc in range(8):
                        kb.op("pe", lambda: nc.tensor.matmul(h1[:, :], lhsT=wc[:, kc * 128:(kc + 1) * 128],
                                                             rhs=x1T[:, kc, :], start=(kc == 0), stop=(kc == 7)),
                              R=[wc, x1T], W=[h1])
                    for kc in range(8):
                        kb.op("pe", lambda: nc.tensor.matmul(h3[:, :], lhsT=wc[:, 1024 + kc * 128:1024 + (kc + 1) * 128],
                                                             rhs=x1T[:, kc, :], start=(kc == 0), stop=(kc == 7)),
                              R=[wc, x1T], W=[h3])
                    s = sil[wcnt % 2]
                    kb.op("act", lambda: nc.scalar.activation(out=s[:, :], in_=h1[:, :], func=AF.Silu), R=[h1], W=[s])
                    kb.op("dve", lambda: nc.vector.tensor_tensor(out=aT[f][:, :], in0=s[:, :], in1=h3[:, :],
                                                                 op=ALU.mult), R=[s, h3], W=[aT[f]])
                    wcnt += 1
                for tt in range(4):
                    for hf in range(2):
                        py = Y[hf]
                        for f in range(nfu):
                            kb.op("pe", lambda: nc.tensor.matmul(py[:, :], lhsT=aT[f][:, tt * 128:(tt + 1) * 128],
                                                                 rhs=w2[f][:, hf * 512:(hf + 1) * 512],
                                                                 start=(f == 0), stop=(f == nfu - 1)),
                                  R=[aT[f], w2[f]], W=[py])
                        ya = yacc[tt]
                        ysl = ya[:, hf * 512:(hf + 1) * 512]
                        if moe:
                            cs = comb[tt][:, e:e + 1]
                            if first:
                                kb.op("dve", lambda: nc.vector.tensor_scalar(out=ysl, in0=py[:, :], scalar1=cs,
                                                                             scalar2=None, op0=ALU.mult),
                                      R=[py, comb[tt]], W=[ya])
                            else:
                                kb.op("dve", lambda: nc.vector.scalar_tensor_tensor(out=ysl, in0=py[:, :], scalar=cs,
                                                                                    in1=ysl, op0=ALU.mult, op1=ALU.add),
                                      R=[py, comb[tt], ya], W=[ya])
                        else:
                            if first:
                                kb.op("dve", lambda: nc.vector.tensor_copy(out=ysl, in_=py[:, :]), R=[py], W=[ya])
                            else:
                                kb.op("dve", lambda: nc.vector.tensor_tensor(out=ysl, in0=py[:, :], in1=ysl, op=ALU.add),
                                      R=[py, ya], W=[ya])
                first = False
        for tt in range(4):
            tok0 = g * TG + tt * 128
            kb.op("dve", lambda: nc.vector.scalar_tensor_tensor(out=h[:, :], in0=x1g[tt][:, :], scalar=ALPHA,
                                                                in1=yacc[tt][:, :], op0=ALU.mult, op1=ALU.add),
                  R=[x1g[tt], yacc[tt]], W=[h])
            o = ost[tt % 2]
            layer_norm(kb, h, (lnp, lnp[:, 2, :]), (lnp, lnp[:, 3, :]), o[:, :], o, scr)
            kb.dma("sp", out_d[tok0:tok0 + 128, :], o[:, :], R=[o], W=[out_d])
    return kb.finish()


def ffn_chunk_layout(w1, w3, w2):
    F = w1.shape[1]
    nf = F // 128
    a = w1.reshape(8, 128, nf, 128).transpose(2, 1, 0, 3).reshape(nf, 128, 1024)
    b = w3.reshape(8, 128, nf, 128).transpose(2, 1, 0, 3).reshape(nf, 128, 1024)
    c = w2.reshape(nf, 128, 1024)
    return np.ascontiguousarray(np.concatenate([a, b, c], axis=2))


def run_C(o_flat_bf, x_flat, wo_bf, lnp4, wf, n_exp, nf_unit, n_units, wr=None):
    nc = build_C(n_exp, nf_unit, n_units)
    wol = w_kc_layout(wo_bf)
    lnb = np.ascontiguousarray(np.broadcast_to(lnp4[None, :, :], (128, 4, D))).astype(np.float32)
    maps = []
    for c in range(NCORES):
        oc = o_flat_bf[c * TPC:(c + 1) * TPC]
        oT = np.ascontiguousarray(oc.reshape(TPC, 8, 128).transpose(2, 1, 0))
        m = {"oT": oT, "x": np.ascontiguousarray(x_flat[c * TPC:(c + 1) * TPC]), "wo": wol, "lnp": lnb,
             "wf": wf, "ident": IDENT}
        if wr is not None:
            m["wr"] = np.ascontiguousarray(wr.reshape(8, 128, 8).transpose(1, 0, 2))
        maps.append(m)
    res = run_spmd(nc, maps)
    return np.concatenate([res[c]["out"] for c in range(NCORES)], axis=0)


from concourse.bass_types import AP as _AP

NQB = SEQ // 128
NDEL = 2304


def rel_bucket_np(d):
    d = np.maximum(d, 0)
    df = np.maximum(d, 1).astype(np.float32)
    large = 16 + (np.log(df / np.float32(16.0)).astype(np.float32) / np.float32(math.log(2048 / 16))
                  * np.float32(16)).astype(np.int32)
    large = np.minimum(large, 31)
    return np.where(d < 16, d, large)


def dil_const():
    C = np.zeros((32, NDEL), np.float32)
    for idx in range(NDEL):
        dl = idx - 127
        if dl < 0 or dl > 2048:
            continue
        mult = 0
        for (w, dd) in ((128, 1), (512, 4), (2048, 16)):
            if dl <= w and dl % dd == 0:
                mult += 1
        if mult:
            C[int(rel_bucket_np(np.array([dl]))[0]), idx] = mult
    return C


class AttnCtx:
    def __init__(self, kb, n_s=4, n_o=2, grouped=False, fox=False):
        self.kb = kb
        self.S = [kb.ps([128, 512], F32, "S") for _ in range(n_s)]
        self.O = [kb.ps([128, 512], F32, "O") for _ in range(n_o)]
        self.P32 = [kb.sb([128, 128], F32, "P32") for _ in range(4)]
        self.Pb = [kb.sb([128, 128], BF16, "Pb") for _ in range(6)]
        self.pending = []
        self.LA = 4
        self.LAG = 3
        self.cpg = 0
        self.cpf = 0
        if grouped:
            self.P32g = [kb.sb([128, 512], F32, "P32g") for _ in range(3)]
            self.Pbg = [kb.sb([128, 512], BF16, "Pbg") for _ in range(4)]
        if fox:
            self.P32f = [kb.sb([128, 256], F32, "P32f") for _ in range(3)]
            self.Pbf = [kb.sb([128, 256], BF16, "Pbf") for _ in range(6)]
        self.rc = [kb.sb([128, 1], F32, "rc") for _ in range(2)]
        self.cs = 0
        self.co = 0
        self.cp = 0


def attn_qblock(ax, qT, kT, Vaug, h, i, jlist, bias_of, mask_of, ost, ocol, after=None):
    kb = ax.kb
    nc = kb.nc
    O = ax.O[ax.co % len(ax.O)]
    rc = ax.rc[ax.co % 2]
    ax.co += 1
    hp = slice(h * 64, (h + 1) * 64)
    nj = len(jlist)
    for n, j in enumerate(jlist):
        S = ax.S[ax.cs % len(ax.S)]
        ax.cs += 1
        kb.op("pe", lambda: nc.tensor.matmul(S[:, :128], lhsT=kT[hp, j * 128:(j + 1) * 128],
                                             rhs=qT[hp, i * 128:(i + 1) * 128], start=True, stop=True),
              R=[kT, qT], W=[S])
        Pb = ax.Pb[ax.cp % len(ax.Pb)]
        b = bias_of(j) if bias_of is not None else None
        m = mask_of(j) if mask_of is not None else None
        if m is not None and not isinstance(m, list):
            m = [m]
        if m is not None and len(m) == 0:
            m = None
        tgt = Pb if m is None else ax.P32[ax.cp % len(ax.P32)]
        ax.cp += 1
        if b is None:
            kb.op("act", lambda: nc.scalar.activation(out=tgt[:, :], in_=S[:, :128], func=AF.Exp, scale=0.125),
                  R=[S], W=[tgt])
        else:
            kb.op("act", lambda: nc.scalar.activation(out=tgt[:, :], in_=S[:, :128], func=AF.Exp, bias=b[1],
                                                      scale=0.125), R=[S, b[0]], W=[tgt])
        if m is not None:
            for mm in m[:-1]:
                kb.op("pool", lambda: nc.gpsimd.tensor_tensor(out=tgt[:, :], in0=tgt[:, :], in1=mm[1], op=ALU.mult),
                      R=[tgt, mm[0]], W=[tgt])
            kb.op("dve", lambda: nc.vector.tensor_tensor(out=Pb[:, :], in0=tgt[:, :], in1=m[-1][1], op=ALU.mult),
                  R=[tgt, m[-1][0]], W=[Pb])

        def pv(Pb=Pb, j=j, n=n):
            kb.op("pe", lambda: nc.tensor.matmul(O[:, :65], lhsT=Pb[:, :], rhs=Vaug[:, j, h, :],
                                                 start=(n == 0), stop=(n == nj - 1)), R=[Pb, Vaug], W=[O])
            if n == nj - 1:
                kb.op("dve", lambda: nc.vector.reciprocal(out=rc[:, :], in_=O[:, 64:65]), R=[O], W=[rc])
                kb.op("dve", lambda: nc.vector.tensor_scalar(out=ost[:, ocol:ocol + 64], in0=O[:, 0:64],
                                                             scalar1=rc[:, 0:1], scalar2=None, op0=ALU.mult),
                      R=[O, rc], W=[ost])
                if after is not None:
                    after()

        ax.pending.append(pv)
        while len(ax.pending) > ax.LA:
            ax.pending.pop(0)()


def attn_qgroup(ax, qT, kT, Vaug, h, i, groups, ost, ocol, after=None):
    kb = ax.kb
    nc = kb.nc
    O = ax.O[ax.co % len(ax.O)]
    rc = ax.rc[ax.co % 2]
    ax.co += 1
    hp = slice(h * 64, (h + 1) * 64)
    ng = len(groups)
    for gi, g in enumerate(groups):
        js = g["js"]
        wd = 128 * len(js)
        S = ax.S[ax.cs % len(ax.S)]
        ax.cs += 1
        for n, j in enumerate(js):
            kb.op("pe", lambda: nc.tensor.matmul(S[:, n * 128:(n + 1) * 128], lhsT=kT[hp, j * 128:(j + 1) * 128],
                                                 rhs=qT[hp, i * 128:(i + 1) * 128], start=True, stop=True),
                  R=[kT, qT], W=[S])
        Pb = ax.Pbg[ax.cpg % len(ax.Pbg)]
        has_mask = (g.get("pool_masks") is not None) or (g.get("dve_mask") is not None)
        tgt = ax.P32g[ax.cpg % len(ax.P32g)] if has_mask else Pb
        ax.cpg += 1
        b = g.get("bias")
        if b is None:
            kb.op("act", lambda: nc.scalar.activation(out=tgt[:, :wd], in_=S[:, :wd], func=AF.Exp, scale=0.125),
                  R=[S], W=[tgt])
        else:
            kb.op("act", lambda: nc.scalar.activation(out=tgt[:, :wd], in_=S[:, :wd], func=AF.Exp, bias=b[1],
                                                      scale=0.125), R=[S, b[0]], W=[tgt])
        if has_mask:
            pm = g.get("pool_masks")
            dm = g.get("dve_mask")
            if pm is not None:
                for n, mm in enumerate(pm):
                    last = (dm is None) and False
                    kb.op("pool", lambda: nc.gpsimd.tensor_tensor(out=tgt[:, n * 128:(n + 1) * 128],
                                                                  in0=tgt[:, n * 128:(n + 1) * 128], in1=mm[1],
                                                                  op=ALU.mult), R=[tgt, mm[0]], W=[tgt])
            if dm is not None:
                kb.op("dve", lambda: nc.vector.tensor_tensor(out=Pb[:, :wd], in0=tgt[:, :wd], in1=dm[1], op=ALU.mult),
                      R=[tgt, dm[0]], W=[Pb])
            else:
                kb.op("dve", lambda: nc.vector.tensor_copy(out=Pb[:, :wd], in_=tgt[:, :wd]), R=[tgt], W=[Pb])

        def pv(Pb=Pb, js=js, gi=gi):
            for n, j in enumerate(js):
                kb.op("pe", lambda: nc.tensor.matmul(O[:, :65], lhsT=Pb[:, n * 128:(n + 1) * 128], rhs=Vaug[:, j, h, :],
                                                     start=(gi == 0 and n == 0),
                                                     stop=(gi == ng - 1 and n == len(js) - 1)), R=[Pb, Vaug], W=[O])
            if gi == ng - 1:
                kb.op("dve", lambda: nc.vector.reciprocal(out=rc[:, :], in_=O[:, 64:65]), R=[O], W=[rc])
                kb.op("dve", lambda: nc.vector.tensor_scalar(out=ost[:, ocol:ocol + 64], in0=O[:, 0:64],
                                                             scalar1=rc[:, 0:1], scalar2=None, op0=ALU.mult),
                      R=[O, rc], W=[ost])
                if after is not None:
                    after()

        ax.pending.append(pv)
        while len(ax.pending) > ax.LAG:
            ax.pending.pop(0)()


def attn_fox_pair(ax, qT, kT, Vaug, h, i0, B, tri2, osts, ocol, after=None):
    kb = ax.kb
    nc = kb.nc
    i1 = i0 + 1
    O0, O1 = ax.O[0], ax.O[1]
    rc0, rc1 = ax.rc[0], ax.rc[1]
    hp = slice(h * 64, (h + 1) * 64)
    for j in range(0, i1 + 1):
        wide = (j <= i0)
        wd = 256 if wide else 128
        q0 = i0 * 128 if wide else i1 * 128
        S = ax.S[ax.cs % len(ax.S)]
        ax.cs += 1
        kb.op("pe", lambda: nc.tensor.matmul(S[:, :wd], lhsT=kT[hp, j * 128:(j + 1) * 128],
                                             rhs=qT[hp, q0:q0 + wd], start=True, stop=True), R=[kT, qT], W=[S])
        Pb = ax.Pbf[ax.cpf % len(ax.Pbf)]
        diag = (j >= i0)
        tgt = ax.P32f[ax.cpf % len(ax.P32f)] if diag else Pb
        ax.cpf += 1
        kb.op("act", lambda: nc.scalar.activation(out=tgt[:, :wd], in_=S[:, :wd], func=AF.Exp, bias=B[:, j:j + 1],
                                                  scale=0.125), R=[S, B], W=[tgt])
        if diag:
            kb.op("dve", lambda: nc.vector.tensor_tensor(out=Pb[:, :wd], in0=tgt[:, :wd], in1=tri2[:, :wd], op=ALU.mult),
                  R=[tgt, tri2], W=[Pb])

        def pv(Pb=Pb, j=j, wide=wide):
            if wide:
                kb.op("pe", lambda: nc.tensor.matmul(O0[:, :65], lhsT=Pb[:, 0:128], rhs=Vaug[:, j, h, :],
                                                     start=(j == 0), stop=(j == i0)), R=[Pb, Vaug], W=[O0])
                kb.op("pe", lambda: nc.tensor.matmul(O1[:, :65], lhsT=Pb[:, 128:256], rhs=Vaug[:, j, h, :],
                                                     start=(j == 0), stop=False), R=[Pb, Vaug], W=[O1])
            else:
                kb.op("pe", lambda: nc.tensor.matmul(O1[:, :65], lhsT=Pb[:, 0:128], rhs=Vaug[:, j, h, :],
                                                     start=False, stop=True), R=[Pb, Vaug], W=[O1])
                for (O, rc, ost) in ((O0, rc0, osts[0]), (O1, rc1, osts[1])):
                    kb.op("dve", lambda: nc.vector.reciprocal(out=rc[:, :], in_=O[:, 64:65]), R=[O], W=[rc])
                    kb.op("dve", lambda: nc.vector.tensor_scalar(out=ost[:, ocol:ocol + 64], in0=O[:, 0:64],
                                                                 scalar1=rc[:, 0:1], scalar2=None, op0=ALU.mult),
                          R=[O, rc], W=[ost])
                if after is not None:
                    after()

        ax.pending.append(pv)
        while len(ax.pending) > ax.LA:
            ax.pending.pop(0)()


def attn_flush(ax):
    while ax.pending:
        ax.pending.pop(0)()


def load_vaug(kb, v_d, name):
    nc = kb.nc
    Vaug = kb.sb([128, NQB, 2, 65], BF16, name)
    kb.op("pool", lambda: nc.gpsimd.memset(Vaug[:, :, :, :], 1.0), W=[Vaug])
    for c in range(4):
        js = slice(c * 16, (c + 1) * 16)
        kb.dma("sp" if c % 2 == 0 else "pool", Vaug[:, js, :, 0:64],
               v_d[:, js, :].rearrange("p j (h d) -> p j h d", h=2), R=[v_d], W=[Vaug])
    return Vaug


def build_B_cd():
    kb = KB()
    nc = kb.nc
    qc_d = kb.dram_in("qc", [128, SEQ], BF16)
    kc_d = kb.dram_in("kc", [128, SEQ], BF16)
    vc_d = kb.dram_in("vc", [128, NQB, 128], BF16)
    qd_d = kb.dram_in("qd", [128, SEQ], BF16)
    kd_d = kb.dram_in("kd", [128, SEQ], BF16)
    vd_d = kb.dram_in("vd", [128, NQB, 128], BF16)
    fc_d = kb.dram_in("fc", [128, NQB, 2], F32)
    bf_d = kb.dram_in("bf", [128, 2], F32)
    tri_d = kb.dram_in("tri", [128, 128], F32)
    rel_d = kb.dram_in("rel", [32, 2], F32)
    C_d = kb.dram_in("dilc", [32, NDEL], F32)
    oc_d = kb.dram_out("oc", [SEQ, 128], BF16)
    od_d = kb.dram_out("od", [SEQ, 128], BF16)
    E_d = kb.dram_tmp("Escr", [2, NDEL], F32)

    tri = kb.sb([128, 128], F32, "tri")
    ones = kb.sb([128, 128], F32, "ones")
    kb.dma("sp", tri[:, :], tri_d[:, :], R=[tri_d], W=[tri])
    kb.op("dve", lambda: nc.vector.memset(ones[:, :], 1.0), W=[ones])
    ax = AttnCtx(kb)
    rel = kb.sb([32, 2], F32, "rel")
    Cs = kb.sb([32, NDEL], F32, "Cs")
    Es = kb.sb([2, NDEL], F32, "Es")
    kb.dma("sp", rel[:, :], rel_d[:, :], R=[rel_d], W=[rel])
    kb.dma("sp", Cs[:, :], C_d[:, :], R=[C_d], W=[Cs])
    kb.op("act", lambda: nc.scalar.activation(out=rel[:, :], in_=rel[:, :], func=AF.Exp), R=[rel], W=[rel])
    for c in range((NDEL + 511) // 512):
        w = min(512, NDEL - c * 512)
        pp = ax.S[c % 3]
        kb.op("pe", lambda: nc.tensor.matmul(pp[:2, :w], lhsT=rel[:, :], rhs=Cs[:, c * 512:c * 512 + w],
                                             start=True, stop=True), R=[rel, Cs], W=[pp])
        kb.op("dve", lambda: nc.vector.tensor_copy(out=Es[:, c * 512:c * 512 + w], in_=pp[:2, :w]), R=[pp], W=[Es])
    kb.dma("sp", E_d[:, :], Es[:, :], R=[Es], W=[E_d])
    TT = [kb.sb([128, 17 * 128], F32, "TT") for _ in range(2)]
    for h in range(2):
        src = _AP(tensor=E_d.t.tensor, offset=h * NDEL, ap=[[1, 128], [1, 17 * 128]])
        kb.dma("sp", TT[h][:, :], src, R=[E_d], W=[TT[h]])
    fc = kb.sb([128, NQB, 2], F32, "fc")
    bf = kb.sb([128, 2], F32, "bf")
    lf = kb.sb([128, 2, NQB], F32, "lf")
    kb.dma("sp", fc[:, :, :], fc_d[:, :, :], R=[fc_d], W=[fc])
    kb.dma("sp", bf[:, :], bf_d[:, :], R=[bf_d], W=[bf])
    for h in range(2):
        kb.op("dve", lambda: nc.vector.tensor_scalar(out=lf[:, h, :], in0=fc[:, :, h], scalar1=bf[:, h:h + 1],
                                                     scalar2=None, op0=ALU.add), R=[fc, bf], W=[lf])
    kb.op("act", lambda: nc.scalar.activation(out=lf[:, :, :], in_=lf[:, :, :], func=AF.Exp, scale=-1.0), R=[lf], W=[lf])
    kb.op("act", lambda: nc.scalar.activation(out=lf[:, :, :], in_=lf[:, :, :], func=AF.Ln, bias=1.0), R=[lf], W=[lf])
    kb.op("dve", lambda: nc.vector.tensor_scalar(out=lf[:, :, :], in0=lf[:, :, :], scalar1=-1.0, scalar2=None,
                                                 op0=ALU.mult), R=[lf], W=[lf])
    lf2 = lf[:, :, :].rearrange("p h j -> p (h j)")
    p1, p2 = ax.O[0], ax.O[1]
    kb.op("pe", lambda: nc.tensor.matmul(p1[:, :128], lhsT=tri[:, :], rhs=lf2, start=True, stop=True),
          R=[tri, lf], W=[p1])
    kb.op("pe", lambda: nc.tensor.matmul(p2[:, :128], lhsT=ones[:, :], rhs=lf2, start=True, stop=True),
          R=[ones, lf], W=[p2])
    tot = kb.sb([128, 2, NQB], F32, "tot")
    carry = kb.sb([128, 2, NQB], F32, "carry")
    negF = kb.sb([128, 2, NQB], F32, "negF")
    kb.op("dve", lambda: nc.vector.tensor_copy(out=tot[:, :, :].rearrange("p h j -> p (h j)"), in_=p2[:, :128]),
          R=[p2], W=[tot])
    for h in range(2):
        kb.op("dve", lambda: nc.vector.tensor_tensor_scan(out=carry[:, h, :], data0=ones[:, :NQB], data1=tot[:, h, :],
                                                          initial=0.0, op0=ALU.mult, op1=ALU.add),
              R=[ones, tot], W=[carry])
    kb.op("dve", lambda: nc.vector.tensor_tensor(out=carry[:, :, :], in0=carry[:, :, :], in1=tot[:, :, :],
                                                 op=ALU.subtract), R=[carry, tot], W=[carry])
    kb.op("dve", lambda: nc.vector.tensor_tensor(out=negF[:, :, :].rearrange("p h j -> p (h j)"), in0=p1[:, :128],
                                                 in1=carry[:, :, :].rearrange("p h j -> p (h j)"), op=ALU.add),
          R=[p1, carry], W=[negF])
    kb.op("dve", lambda: nc.vector.tensor_scalar(out=negF[:, :, :], in0=negF[:, :, :], scalar1=-1.0, scalar2=None,
                                                 op0=ALU.mult), R=[negF], W=[negF])
    qc = kb.sb([128, SEQ], BF16, "qc")
    kc = kb.sb([128, SEQ], BF16, "kc")
    qd = kb.sb([128, SEQ], BF16, "qd")
    kd = kb.sb([128, SEQ], BF16, "kd")
    for n, (s, d_) in enumerate(((qd, qd_d), (kd, kd_d), (qc, qc_d), (kc, kc_d))):
        for c in range(2):
            kb.dma("sp" if (n + c) % 2 == 0 else "pool", s[:, c * 4096:(c + 1) * 4096], d_[:, c * 4096:(c + 1) * 4096],
                   R=[d_], W=[s])
    Vd = load_vaug(kb, vd_d, "Vd")
    Vc = load_vaug(kb, vc_d, "Vc")
    ostd = [kb.sb([128, 128], BF16, "ostd") for _ in range(2)]
    ostc = [kb.sb([128, 128], BF16, "ostc") for _ in range(2)]
    Bi = [kb.sb([128, NQB], F32, "Bi") for _ in range(3)]
    nb = 0
    for i in range(NQB):
        od = ostd[i % 2]
        for h in range(2):
            j0 = max(0, i - 16)
            attn_qblock(ax, qd, kd, Vd, h, i, list(range(j0, i + 1)), None,
                        lambda j, h=h, i=i: (TT[h], TT[h][:, (i - j) * 128:(i - j + 1) * 128]), od, h * 64,
                        after=(None if h == 0 else
                               (lambda i=i, od=od: kb.dma("pool", od_d[i * 128:(i + 1) * 128, :], od[:, :],
                                                          R=[od], W=[od_d]))))
        oc = ostc[i % 2]
        for h in range(2):
            B = Bi[nb % 3]
            nb += 1
            kb.op("dve", lambda: nc.vector.tensor_scalar(out=B[:, :i + 1], in0=negF[:, h, :i + 1],
                                                         scalar1=carry[:, h, i:i + 1], scalar2=None, op0=ALU.add),
                  R=[negF, carry], W=[B])
            attn_qblock(ax, qc, kc, Vc, h, i, list(range(0, i + 1)),
                        lambda j, B=B: (B, B[:, j:j + 1]),
                        lambda j, i=i: ((tri, tri[:, :]) if j == i else None), oc, h * 64,
                        after=(None if h == 0 else
                               (lambda i=i, oc=oc: kb.dma("sp", oc_d[i * 128:(i + 1) * 128, :], oc[:, :],
                                                          R=[oc], W=[oc_d]))))
    attn_flush(ax)
    return kb.finish()


TRI = np.triu(np.ones((128, 128), np.float32))


def to_pj(a):
    n = a.shape[1]
    return np.ascontiguousarray(a.reshape(NQB, 128, n).transpose(1, 0, 2))


def run_B_cd(yT, ytb, ytf, b_forget, rel_table):
    nc = build_B_cd()
    C = dil_const()
    maps = []
    for c in range(NCORES):
        b, m = c // 4, c % 4
        ts = slice(b * SEQ, (b + 1) * SEQ)
        rs = lambda base: slice(base + m * 128, base + (m + 1) * 128)
        maps.append({
            "qc": np.ascontiguousarray(yT[rs(0), ts]), "kc": np.ascontiguousarray(yT[rs(512), ts]),
            "qd": np.ascontiguousarray(yT[rs(1024), ts]),
            "kd": np.ascontiguousarray(yT[rs(1536), ts].reshape(128, NQB, 128)[:, :, ::-1].reshape(128, SEQ)),
            "vc": to_pj(ytb[ts, m * 128:(m + 1) * 128]), "vd": np.ascontiguousarray(to_pj(ytb[ts, 512 + m * 128:512 + (m + 1) * 128])[::-1]),
            "fc": to_pj(ytf[ts, 2 * m:2 * m + 2]),
            "bf": np.ascontiguousarray(np.broadcast_to(b_forget[None, 2 * m:2 * m + 2], (128, 2))).astype(np.float32),
            "tri": TRI, "rel": np.ascontiguousarray(rel_table[:, 2 * m:2 * m + 2]), "dilc": C,
        })
    res = run_spmd(nc, maps)
    o = np.zeros((BATCH * SEQ, D), NPBF)
    for c in range(NCORES):
        b, m = c // 4, c % 4
        o[b * SEQ:(b + 1) * SEQ, m * 128:(m + 1) * 128] = res[c]["oc"]
        o[b * SEQ:(b + 1) * SEQ, 512 + m * 128:512 + (m + 1) * 128] = res[c]["od"]
    return o


def build_B_gla():
    kb = KB()
    nc = kb.nc
    qT_d = kb.dram_in("qT", [64, SEQ], BF16)
    kT_d = kb.dram_in("kT", [64, SEQ], BF16)
    k_d = kb.dram_in("k", [128, NQB, 64], BF16)
    v_d = kb.dram_in("v", [128, NQB, 128], BF16)
    r_d = kb.dram_in("r", [128, NQB, 128], F32)
    g_d = kb.dram_in("g", [128, NQB, 64], F32)
    gn_d = kb.dram_in("gn", [128, 128], F32)
    tri_d = kb.dram_in("tri", [128, 128], F32)
    o_d = kb.dram_out("o", [SEQ, 128], BF16)
    qT = kb.sb([64, SEQ], BF16, "qT")
    kT = kb.sb([64, SEQ], BF16, "kT")
    ktm = kb.sb([128, NQB, 64], BF16, "ktm")
    v = kb.sb([128, NQB, 128], BF16, "v")
    r = kb.sb([128, NQB, 128], F32, "r")
    g = kb.sb([128, NQB, 64], F32, "g")
    gn = kb.sb([128, 128], F32, "gn")
    tri = kb.sb([128, 128], F32, "tri")
    eps = kb.sb([128, 1], F32, "eps")
    kb.op("dve", lambda: nc.vector.memset(eps[:, :], LN_EPS), W=[eps])
    kb.dma("sp", tri[:, :], tri_d[:, :], R=[tri_d], W=[tri])
    kb.dma("sp", g[:, :, :], g_d[:, :, :], R=[g_d], W=[g])
    kb.dma("pool", qT[:, :], qT_d[:, :], R=[qT_d], W=[qT])
    kb.dma("sp", kT[:, :], kT_d[:, :], R=[kT_d], W=[kT])
    kb.dma("pool", ktm[:, :, :], k_d[:, :, :], R=[k_d], W=[ktm])
    kb.dma("sp", v[:, :, :], v_d[:, :, :], R=[v_d], W=[v])
    kb.dma("pool", r[:, :, :], r_d[:, :, :], R=[r_d], W=[r])
    kb.dma("sp", gn[:, :], gn_d[:, :], R=[gn_d], W=[gn])
    kb.op("act", lambda: nc.scalar.activation(out=r[:, :, :], in_=r[:, :, :], func=AF.Silu), R=[r], W=[r])
    PG = [kb.ps([128, 512], F32, "PG") for _ in range(2)]
    PGT = [kb.ps([128, 512], F32, "PGT") for _ in range(2)]
    PA = [kb.ps([128, 512], F32, "PA") for _ in range(2)]
    PO = kb.ps([128, 512], F32, "PO")
    PU = kb.ps([128, 512], F32, "PU")
    eGT = [kb.sb([64, 128], F32, "eGT") for _ in range(2)]
    enGT = [kb.sb([64, 128], F32, "enGT") for _ in range(2)]
    enG = [kb.sb([128, 64], F32, "enG") for _ in range(2)]
    qgT = [kb.sb([64, 128], BF16, "qgT") for _ in range(2)]
    kgT = [kb.sb([64, 128], BF16, "kgT") for _ in range(2)]
    kg = [kb.sb([128, 64], BF16, "kg") for _ in range(2)]
    Am = [kb.sb([128, 128], BF16, "Am") for _ in range(2)]
    S32 = kb.sb([64, 128], F32, "S32")
    Sbf = kb.sb([64, 128], BF16, "Sbf")
    st6 = [kb.sb([128, 6], F32, "st6") for _ in range(2)]
    mv = [kb.sb([128, 4], F32, "mv") for _ in range(2)]
    of = [kb.sb([128, 128], F32, "of") for _ in range(2)]
    ost = [kb.sb([128, 128], BF16, "ost") for _ in range(2)]
    for c in range(NQB):
        p = c % 2
        cs = slice(c * 128, (c + 1) * 128)
        kb.op("pe", lambda: nc.tensor.matmul(PG[p][:, :64], lhsT=tri[:, :], rhs=g[:, c, :], start=True, stop=True),
              R=[tri, g], W=[PG[p]])
        kb.op("pe", lambda: nc.tensor.matmul(PGT[p][:64, :128], lhsT=g[:, c, :], rhs=tri[:, :], start=True, stop=True),
              R=[tri, g], W=[PGT[p]])
        kb.op("act", lambda: nc.scalar.activation(out=eGT[p][:, :], in_=PGT[p][:64, :128], func=AF.Exp),
              R=[PGT[p]], W=[eGT[p]])
        kb.op("act", lambda: nc.scalar.activation(out=enGT[p][:, :], in_=PGT[p][:64, :128], func=AF.Exp, scale=-1.0),
              R=[PGT[p]], W=[enGT[p]])
        kb.op("act", lambda: nc.scalar.activation(out=enG[p][:, :], in_=PG[p][:, :64], func=AF.Exp, scale=-1.0),
              R=[PG[p]], W=[enG[p]])
        kb.op("dve", lambda: nc.vector.scalar_tensor_tensor(out=qgT[p][:, :], in0=qT[:, cs], scalar=0.125,
                                                            in1=eGT[p][:, :], op0=ALU.mult, op1=ALU.mult),
              R=[qT, eGT[p]], W=[qgT[p]])
        kb.op("dve", lambda: nc.vector.tensor_tensor(out=kgT[p][:, :], in0=kT[:, cs], in1=enGT[p][:, :], op=ALU.mult),
              R=[kT, enGT[p]], W=[kgT[p]])
        kb.op("dve", lambda: nc.vector.tensor_tensor(out=kg[p][:, :], in0=ktm[:, c, :], in1=enG[p][:, :], op=ALU.mult),
              R=[ktm, enG[p]], W=[kg[p]])
        kb.op("pe", lambda: nc.tensor.matmul(PA[p][:, :128], lhsT=kgT[p][:, :], rhs=qgT[p][:, :], start=True, stop=True),
              R=[kgT[p], qgT[p]], W=[PA[p]])
        kb.op("dve", lambda: nc.vector.tensor_tensor(out=Am[p][:, :], in0=PA[p][:, :128], in1=tri[:, :], op=ALU.mult),
              R=[PA[p], tri], W=[Am[p]])
        kb.op("pe", lambda: nc.tensor.matmul(PO[:, :128], lhsT=Am[p][:, :], rhs=v[:, c, :], start=True, stop=(c == 0)),
              R=[Am[p], v], W=[PO])
        if c > 0:
            kb.op("pe", lambda: nc.tensor.matmul(PO[:, :128], lhsT=qgT[p][:, :], rhs=Sbf[:, :], start=False, stop=True),
                  R=[qgT[p], Sbf], W=[PO])
        if c < NQB - 1:
            kb.op("pe", lambda: nc.tensor.matmul(PU[:64, :128], lhsT=kg[p][:, :], rhs=v[:, c, :], start=True, stop=True),
                  R=[kg[p], v], W=[PU])
            eGl = eGT[p][:, 127:128]
            if c == 0:
                kb.op("dve", lambda: nc.vector.tensor_scalar(out=S32[:, :], in0=PU[:64, :128], scalar1=eGl, scalar2=None,
                                                             op0=ALU.mult), R=[PU, eGT[p]], W=[S32])
            else:
                kb.op("dve", lambda: nc.vector.tensor_scalar(out=S32[:, :], in0=S32[:, :], scalar1=eGl, scalar2=None,
                                                             op0=ALU.mult), R=[S32, eGT[p]], W=[S32])
                kb.op("dve", lambda: nc.vector.scalar_tensor_tensor(out=S32[:, :], in0=PU[:64, :128], scalar=eGl,
                                                                    in1=S32[:, :], op0=ALU.mult, op1=ALU.add),
                      R=[PU, eGT[p], S32], W=[S32])
            kb.op("dve", lambda: nc.vector.tensor_copy(out=Sbf[:, :], in_=S32[:, :]), R=[S32], W=[Sbf])
        kb.op("dve", lambda: nc.vector.bn_stats(out=st6[p][:, :], in_=PO[:, :128]), R=[PO], W=[st6[p]])
        kb.op("dve", lambda: nc.vector.bn_aggr(out=mv[p][:, 0:2], in_=st6[p][:, :]), R=[st6[p]], W=[mv[p]])
        kb.op("dve", lambda: nc.vector.scalar_tensor_tensor(out=mv[p][:, 2:3], in0=mv[p][:, 0:1], scalar=mv[p][:, 0:1],
                                                            in1=mv[p][:, 1:2], op0=ALU.mult, op1=ALU.add),
              R=[mv[p]], W=[mv[p]])
        kb.op("act", lambda: nc.scalar.activation(out=mv[p][:, 3:4], in_=mv[p][:, 2:3], func=AF.Ln, bias=eps[:, 0:1]),
              R=[mv[p], eps], W=[mv[p]])
        kb.op("act", lambda: nc.scalar.activation(out=mv[p][:, 3:4], in_=mv[p][:, 3:4], func=AF.Exp, scale=-0.5),
              R=[mv[p]], W=[mv[p]])
        kb.op("dve", lambda: nc.vector.scalar_tensor_tensor(out=of[p][:, :], in0=PO[:, :128], scalar=mv[p][:, 3:4],
                                                            in1=gn[:, :], op0=ALU.mult, op1=ALU.mult),
              R=[PO, mv[p], gn], W=[of[p]])
        kb.op("pool", lambda: nc.gpsimd.tensor_tensor(out=ost[p][:, :], in0=of[p][:, :], in1=r[:, c, :], op=ALU.mult),
              R=[of[p], r], W=[ost[p]])
        kb.dma("sp", o_d[cs, :], ost[p][:, :], R=[ost[p]], W=[o_d])
    return kb.finish()


def run_B_gla(yT, ytb, ytf, g_norm):
    nc = build_B_gla()
    maps = []
    for c in range(NCORES):
        b, h = c // 4, c % 4
        ts = slice(b * SEQ, (b + 1) * SEQ)
        maps.append({
            "qT": np.ascontiguousarray(yT[h * 64:(h + 1) * 64, ts]),
            "kT": np.ascontiguousarray(yT[256 + h * 64:256 + (h + 1) * 64, ts]),
            "k": to_pj(ytb[ts, h * 64:(h + 1) * 64]),
            "v": to_pj(ytb[ts, 256 + h * 128:256 + (h + 1) * 128]),
            "r": to_pj(ytf[ts, h * 128:(h + 1) * 128]),
            "g": to_pj(ytf[ts, 520 + h * 64:520 + (h + 1) * 64]),
            "gn": np.ascontiguousarray(np.broadcast_to(g_norm[None, :], (128, 128))).astype(np.float32),
            "tri": TRI,
        })
    res = run_spmd(nc, maps)
    o = np.zeros((BATCH * SEQ, 512), NPBF)
    for c in range(NCORES):
        b, h = c // 4, c % 4
        o[b * SEQ:(b + 1) * SEQ, h * 128:(h + 1) * 128] = res[c]["o"]
    return o


NBIS = 20
TOPK = 256


def build_B_dsa1(act_split=True):
    kb = KB()
    nc = kb.nc
    NK = 16
    qiT_d = kb.dram_in("qiT", [64, 8, NK * 128], BF16)
    kiT_d = kb.dram_in("kiT", [64, SEQ], BF16)
    wi_d = kb.dram_in("wi", [128, NK, 8], F32)
    cm_d = kb.dram_in("cmask", [128, 512], F32)
    idb_d = kb.dram_in("identb", [128, 128], BF16)
    stp_d = kb.dram_in("steps", [128, NBIS], F32)
    M_d = kb.dram_out("M", [NK, 128, SEQ], BF16)
    qiT = kb.sb([64, 8, NK * 128], BF16, "qiT")
    kiT = kb.sb([64, SEQ], BF16, "kiT")
    wi = kb.sb([128, NK, 8], F32, "wi")
    absw = kb.sb([128, NK, 8], F32, "absw")
    sgn = kb.sb([128, NK, 8], F32, "sgn")
    cm = kb.sb([128, 512], F32, "cm")
    idb = kb.sb([128, 128], BF16, "idb")
    stp = kb.sb([128, NBIS], F32, "stp")
    kb.dma("sp", qiT[:, :, :], qiT_d[:, :, :], R=[qiT_d], W=[qiT])
    kb.dma("pool", kiT[:, :], kiT_d[:, :], R=[kiT_d], W=[kiT])
    kb.dma("sp", wi[:, :, :], wi_d[:, :, :], R=[wi_d], W=[wi])
    kb.dma("sp", cm[:, :], cm_d[:, :], R=[cm_d], W=[cm])
    kb.dma("sp", idb[:, :], idb_d[:, :], R=[idb_d], W=[idb])
    kb.dma("sp", stp[:, :], stp_d[:, :], R=[stp_d], W=[stp])
    kb.op("act", lambda: nc.scalar.activation(out=absw[:, :, :], in_=wi[:, :, :], func=AF.Abs), R=[wi], W=[absw])
    kb.op("act", lambda: nc.scalar.activation(out=sgn[:, :, :], in_=wi[:, :, :], func=AF.Sign), R=[wi], W=[sgn])
    PS = [kb.ps([128, 512], F32, "PS") for _ in range(3)]
    PT = [kb.ps([128, 1024], BF16, "PT") for _ in range(2)]

    def write_mask(k, mt, Lk):
        kb.dma("sp", M_d[k, :, :Lk], mt[:, :Lk], R=[mt], W=[M_d])

    dsa1_body(kb, qiT, kiT, absw, sgn, cm, idb, stp, PS, PT, write_mask, act_split)
    return kb.finish()


def dsa1_body(kb, qiT, kiT, absw, sgn, cm, idb, stp, PS, PT, write_mask, act_split=True):
    nc = kb.nc
    NK = 16
    score = [kb.sb([128, SEQ], F32, "score") for _ in range(2)]
    selb = kb.sb([128, SEQ], BF16, "selb")
    junk2 = kb.sb([128, 5120], BF16, "junk2")
    MT = [kb.sb([128, SEQ], BF16, "MT") for _ in range(2)]
    rl = [kb.sb([128, 512], F32, "rl") for _ in range(3)]
    bs = [kb.sb([128, 16], F32, "bs") for _ in range(2)]
    stk = [kb.sb([128, NBIS], F32, "stk") for _ in range(2)]
    midb = [kb.sb([128, 1], F32, "midb") for _ in range(2)]
    cntd = [kb.sb([128, 1], F32, "cntd") for _ in range(2)]
    cnta = [kb.sb([128, 1], F32, "cnta") for _ in range(2)]
    tmpb = [kb.sb([128, 1], F32, "tmpb") for _ in range(2)]
    geb = [kb.sb([128, 1], F32, "geb") for _ in range(2)]
    st = {"rl": 0, "pt": 0}

    def scoring(k):
        sc_ = score[k % 2]
        for c in range(k + 1):
            for hi in range(8):
                pp = PS[st["rl"] % 3]
                r_ = rl[st["rl"] % 3]
                st["rl"] += 1
                kb.op("pe", lambda: nc.tensor.matmul(pp[:, :], lhsT=qiT[:, hi, k * 128:(k + 1) * 128],
                                                     rhs=kiT[:, c * 512:(c + 1) * 512], start=True, stop=True),
                      R=[qiT, kiT], W=[pp])
                kb.op("act", lambda: nc.scalar.activation(out=r_[:, :], in_=pp[:, :], func=AF.Relu,
                                                          scale=absw[:, k, hi:hi + 1]), R=[pp, absw], W=[r_])
                ssl = sc_[:, c * 512:(c + 1) * 512]
                if hi == 0:
                    kb.op("dve", lambda: nc.vector.tensor_scalar(out=ssl, in0=r_[:, :], scalar1=sgn[:, k, 0:1],
                                                                 scalar2=None, op0=ALU.mult), R=[r_, sgn], W=[sc_])
                else:
                    kb.op("dve", lambda: nc.vector.scalar_tensor_tensor(out=ssl, in0=r_[:, :],
                                                                        scalar=sgn[:, k, hi:hi + 1], in1=ssl,
                                                                        op0=ALU.mult, op1=ALU.add),
                          R=[r_, sgn, sc_], W=[sc_])

    def setup(k):
        L = 512 * (k + 1)
        sc_, b_, sk = score[k % 2], bs[k % 2], stk[k % 2]
        kb.op("dve", lambda: nc.vector.tensor_reduce(out=b_[:, 0:1], in_=sc_[:, :L], axis=AX.X, op=ALU.min),
              R=[sc_], W=[b_])
        kb.op("dve", lambda: nc.vector.tensor_reduce(out=b_[:, 1:2], in_=sc_[:, :L], axis=AX.X, op=ALU.max),
              R=[sc_], W=[b_])
        kb.op("dve", lambda: nc.vector.tensor_tensor(out=sc_[:, L - 512:L], in0=sc_[:, L - 512:L], in1=cm[:, :],
                                                     op=ALU.add), R=[sc_, cm], W=[sc_])
        kb.op("dve", lambda: nc.vector.tensor_tensor(out=b_[:, 2:3], in0=b_[:, 1:2], in1=b_[:, 0:1], op=ALU.subtract),
              R=[b_], W=[b_])
        kb.op("dve", lambda: nc.vector.tensor_scalar(out=sk[:, :], in0=stp[:, :], scalar1=b_[:, 2:3], scalar2=None,
                                                     op0=ALU.mult), R=[stp, b_], W=[sk])
        kb.op("dve", lambda: nc.vector.scalar_tensor_tensor(out=midb[k % 2][:, 0:1], in0=b_[:, 2:3], scalar=0.5,
                                                            in1=b_[:, 0:1], op0=ALU.mult, op1=ALU.add),
              R=[b_], W=[midb[k % 2]])

    def iteration(k, n):
        L = 512 * (k + 1)
        p = k % 2
        sc_, sk = score[p], stk[p]
        Ld = (L * 2 // 5) if act_split else L
        if act_split:
            kb.op("act", lambda: nc.scalar.activation(out=junk2[:, :L - Ld], in_=sc_[:, Ld:L], func=AF.Sign,
                                                      bias=midb[p][:, 0:1], scale=-1.0, accum_out=cnta[p][:, 0:1]),
                  R=[sc_, midb[p]], W=[junk2, cnta[p]])
        kb.op("dve", lambda: nc.vector.tensor_scalar(out=selb[:, :Ld], in0=sc_[:, :Ld], scalar1=midb[p][:, 0:1],
                                                     scalar2=0.0, op0=ALU.is_ge, op1=ALU.add,
                                                     accum_out=cntd[p][:, 0:1]),
              R=[sc_, midb[p]], W=[selb, cntd[p]])
        thr = TOPK - 0.25
        if act_split:
            kb.op("dve", lambda: nc.vector.scalar_tensor_tensor(out=tmpb[p][:, 0:1], in0=cnta[p][:, 0:1], scalar=-0.5,
                                                                in1=cntd[p][:, 0:1], op0=ALU.mult, op1=ALU.add),
                  R=[cnta[p], cntd[p]], W=[tmpb[p]])
            thr -= 0.5 * (L - Ld)
            src, srcb = tmpb[p][:, 0:1], tmpb[p]
        else:
            src, srcb = cntd[p][:, 0:1], cntd[p]
        kb.op("dve", lambda: nc.vector.tensor_scalar(out=geb[p][:, 0:1], in0=src, scalar1=thr, scalar2=-0.5,
                                                     op0=ALU.is_ge, op1=ALU.add), R=[srcb], W=[geb[p]])
        kb.op("dve", lambda: nc.vector.scalar_tensor_tensor(out=midb[p][:, 0:1], in0=geb[p][:, 0:1],
                                                            scalar=sk[:, n:n + 1], in1=midb[p][:, 0:1],
                                                            op0=ALU.mult, op1=ALU.add),
              R=[geb[p], sk, midb[p]], W=[midb[p]])

    def finish_block(k):
        L = 512 * (k + 1)
        sc_, b_, sk = score[k % 2], bs[k % 2], stk[k % 2]
        kb.op("dve", lambda: nc.vector.scalar_tensor_tensor(out=b_[:, 8:9], in0=sk[:, NBIS - 1:NBIS], scalar=-0.5,
                                                            in1=midb[k % 2][:, 0:1], op0=ALU.mult, op1=ALU.add),
              R=[midb[k % 2], sk], W=[b_])
        kb.op("dve", lambda: nc.vector.tensor_scalar(out=selb[:, :L], in0=sc_[:, :L], scalar1=b_[:, 8:9], scalar2=None,
                                                     op0=ALU.is_ge), R=[sc_, b_], W=[selb])
        mt = MT[k % 2]
        nkb = 4 * (k + 1)
        for j0 in range(0, nkb, 8):
            pt = PT[st["pt"] % 2]
            for jj in range(8):
                j = j0 + jj
                if j >= nkb:
                    break
                kb.op("pe", lambda: nc.tensor.transpose(pt[:, jj * 128:(jj + 1) * 128], selb[:, j * 128:(j + 1) * 128],
                                                        idb[:, :]), R=[selb, idb], W=[pt])
            wdt = min(8, nkb - j0) * 128
            if st["pt"] % 2 == 0:
                kb.op("act", lambda: nc.scalar.copy(out=mt[:, j0 * 128:j0 * 128 + wdt], in_=pt[:, :wdt]), R=[pt], W=[mt])
            else:
                kb.op("dve", lambda: nc.vector.tensor_copy(out=mt[:, j0 * 128:j0 * 128 + wdt], in_=pt[:, :wdt]),
                      R=[pt], W=[mt])
            st["pt"] += 1
        write_mask(k, mt, L)

    for k0 in range(0, NK, 2):
        ks = (k0, k0 + 1)
        for k in ks:
            scoring(k)
        for k in ks:
            setup(k)
        for n in range(NBIS):
            for k in ks:
                iteration(k, n)
        for k in ks:
            finish_block(k)


def run_B_dsa1(yT, ytf):
    nc = build_B_dsa1()
    steps = np.ascontiguousarray(np.broadcast_to((0.5 ** np.arange(1, NBIS + 1))[None, :], (128, NBIS))).astype(np.float32)
    maps = []
    for c in range(NCORES):
        b, m = c // 4, c % 4
        ts = slice(b * SEQ, (b + 1) * SEQ)
        tok = (np.arange(16)[:, None] * 512 + m * 128 + np.arange(128)[None, :]).reshape(-1) + b * SEQ
        qi = yT[1536:2048][:, tok].reshape(8, 64, 2048).transpose(1, 0, 2)
        wi = ytf[tok, 512:520].reshape(16, 128, 8).transpose(1, 0, 2)
        cm = np.where(np.arange(512)[None, :] <= (128 * m + np.arange(128))[:, None], 0.0, NEG).astype(np.float32)
        maps.append({"qiT": np.ascontiguousarray(qi), "kiT": np.ascontiguousarray(yT[2048:2112, ts]),
                     "wi": np.ascontiguousarray(wi), "cmask": cm, "identb": IDENT.astype(NPBF), "steps": steps})
    res = run_spmd(nc, maps)
    out = []
    for b in range(BATCH):
        M = np.zeros((NQB, 128, SEQ), NPBF)
        for m in range(4):
            M[m::4] = res[b * 4 + m]["M"]
        out.append(M)
    return out


def dsa_const():
    C = np.zeros((32, NDEL), np.float32)
    dl = np.arange(NDEL) - 127
    ok = dl >= 0
    C[rel_bucket_np(dl)[ok], np.arange(NDEL)[ok]] = 1.0
    return C


NPACK = NQB * (NQB + 1) // 2


def build_B_dsa2():
    kb = KB()
    nc = kb.nc
    q_d = kb.dram_in("q", [128, SEQ], BF16)
    k_d = kb.dram_in("k", [128, SEQ], BF16)
    v_d = kb.dram_in("v", [128, NQB, 128], BF16)
    rel_d = kb.dram_in("rel", [32, 2], F32)
    rf_d = kb.dram_in("relfar", [128, 2], F32)
    C_d = kb.dram_in("dsac", [32, NDEL], F32)
    M_d = kb.dram_in("Mp", [NPACK * 128 * 128], BF16)
    o_d = kb.dram_out("o", [SEQ, 128], BF16)
    E_d = kb.dram_tmp("Escr", [2, NDEL], F32)
    ax = AttnCtx(kb)
    rel = kb.sb([32, 2], F32, "rel")
    rf = kb.sb([128, 2], F32, "rf")
    Cs = kb.sb([32, NDEL], F32, "Cs")
    Es = kb.sb([2, NDEL], F32, "Es")
    kb.dma("sp", rel[:, :], rel_d[:, :], R=[rel_d], W=[rel])
    kb.dma("sp", rf[:, :], rf_d[:, :], R=[rf_d], W=[rf])
    kb.dma("sp", Cs[:, :], C_d[:, :], R=[C_d], W=[Cs])
    kb.op("act", lambda: nc.scalar.activation(out=rel[:, :], in_=rel[:, :], func=AF.Exp), R=[rel], W=[rel])
    for c in range((NDEL + 511) // 512):
        w = min(512, NDEL - c * 512)
        pp = ax.S[c % 3]
        kb.op("pe", lambda: nc.tensor.matmul(pp[:2, :w], lhsT=rel[:, :], rhs=Cs[:, c * 512:c * 512 + w],
                                             start=True, stop=True), R=[rel, Cs], W=[pp])
        kb.op("dve", lambda: nc.vector.tensor_copy(out=Es[:, c * 512:c * 512 + w], in_=pp[:2, :w]), R=[pp], W=[Es])
    kb.dma("sp", E_d[:, :], Es[:, :], R=[Es], W=[E_d])
    TT = [kb.sb([128, 17 * 128], F32, "TT") for _ in range(2)]
    for h in range(2):
        src = _AP(tensor=E_d.t.tensor, offset=h * NDEL, ap=[[1, 128], [1, 17 * 128]])
        kb.dma("sp", TT[h][:, :], src, R=[E_d], W=[TT[h]])
    q = kb.sb([128, SEQ], BF16, "q")
    k = kb.sb([128, SEQ], BF16, "k")
    for n, (s, d_) in enumerate(((q, q_d), (k, k_d))):
        for c in range(2):
            kb.dma("sp" if (n + c) % 2 == 0 else "pool", s[:, c * 4096:(c + 1) * 4096], d_[:, c * 4096:(c + 1) * 4096],
                   R=[d_], W=[s])
    V = load_vaug(kb, v_d, "V")
    Ms = [kb.sb([128, SEQ], BF16, "Ms") for _ in range(2)]
    ost = [kb.sb([128, 128], BF16, "ost") for _ in range(2)]
    for i in range(NQB):
        ms = Ms[i % 2]
        W_ = (i + 1) * 128
        off = (i * (i + 1) // 2) * 128 * 128
        src = _AP(tensor=M_d.t.tensor, offset=off, ap=[[W_, 128], [1, W_]])
        kb.dma("pool" if i % 2 else "sp", ms[:, :W_], src, R=[M_d], W=[ms])
        o = ost[i % 2]
        for h in range(2):
            def bias_of(j, h=h, i=i):
                return (rf, rf[:, h:h + 1]) if i - j > 16 else None

            def mask_of(j, h=h, i=i, ms=ms):
                sel = (ms, ms[:, j * 128:(j + 1) * 128])
                if i - j > 16:
                    return [sel]
                return [(TT[h], TT[h][:, (i - j) * 128:(i - j + 1) * 128]), sel]

            attn_qblock(ax, q, k, V, h, i, list(range(0, i + 1)), bias_of, mask_of, o, h * 64,
                        after=(None if h == 0 else
                               (lambda i=i, o=o: kb.dma("sp", o_d[i * 128:(i + 1) * 128, :], o[:, :], R=[o], W=[o_d]))))
    attn_flush(ax)
    return kb.finish()


def run_B_dsa2(yT, ytb, Msel, rel_table):
    nc = build_B_dsa2()
    C = dsa_const()
    packed = []
    for b in range(BATCH):
        M = Msel[b][:, ::-1, :]
        packed.append(np.concatenate([np.ascontiguousarray(M[i][:, :(i + 1) * 128]).reshape(-1) for i in range(NQB)]))
    maps = []
    for c in range(NCORES):
        b, m = c // 4, c % 4
        ts = slice(b * SEQ, (b + 1) * SEQ)
        maps.append({
            "q": np.ascontiguousarray(yT[512 + m * 128:512 + (m + 1) * 128, ts]),
            "k": np.ascontiguousarray(yT[1024 + m * 128:1024 + (m + 1) * 128, ts].reshape(128, NQB, 128)[:, :, ::-1]
                                      .reshape(128, SEQ)),
            "v": np.ascontiguousarray(to_pj(ytb[ts, 768 + m * 128:768 + (m + 1) * 128])[::-1]),
            "rel": np.ascontiguousarray(rel_table[:, 2 * m:2 * m + 2]),
            "relfar": np.ascontiguousarray(np.broadcast_to(rel_table[31:32, 2 * m:2 * m + 2], (128, 2))).astype(np.float32),
            "dsac": C, "Mp": packed[b],
        })
    res = run_spmd(nc, maps)
    o = np.zeros((BATCH * SEQ, 512), NPBF)
    for c in range(NCORES):
        b, m = c // 4, c % 4
        o[b * SEQ:(b + 1) * SEQ, m * 128:(m + 1) * 128] = res[c]["o"]
    return o


def kernel_unfused(x, ln_g, ln_b, rel_table, w_in_ab, w_gate_a, b_gate_a, g_norm_a, w_out_ab,
           w_in_cd, b_forget, w_out_cd, w1_dense, w3_dense, w2_dense,
           w_router, w1_moe, w3_moe, w2_moe):
    f32 = lambda a: np.ascontiguousarray(np.asarray(a, dtype=np.float32))
    x = f32(x)
    rel_table = f32(rel_table)
    big = [f32(w_in_ab), f32(w_in_cd), f32(w_out_ab), f32(w_out_cd), f32(w1_dense), f32(w3_dense), f32(w2_dense),
           f32(w1_moe), f32(w3_moe), f32(w2_moe)]
    (wi_ab, wi_cd, wo_ab, wo_cd, w1d, w3d, w2d, w1m, w3m, w2m) = cast_weights(big)
    del big
    xf = x.reshape(BATCH * SEQ, D)
    for layer in range(DEPTH):
        j = layer // 2
        lnp4 = np.stack([ln_g[layer, 0], ln_b[layer, 0], ln_g[layer, 1], ln_b[layer, 1]]).astype(np.float32)
        if layer % 2 == 0:
            yT, ytb, ytf = run_A(xf, wi_ab[j],
                                 [(0, 256), (256, 256), (1552, 512), (2064, 512), (3088, 512), (3600, 64)],
                                 [(256, 256), (512, 512), (2576, 512)],
                                 [(1024, 512), (3664, 8)],
                                 gate=(1536,), wg=f32(w_gate_a[j]), bg=f32(b_gate_a[j]))
            oa = run_B_gla(yT, ytb, ytf, f32(g_norm_a[j]))
            Msel = run_B_dsa1(yT, ytf)
            ob = run_B_dsa2(yT, ytb, Msel, rel_table)
            del Msel
            o = np.concatenate([oa, ob], axis=1)
            wf = ffn_chunk_layout(w1d[j], w3d[j], w2d[j])
            xf = run_C(o, xf, wo_ab[j], lnp4, wf, 1, 11, 2)
        else:
            yT, ytb, ytf = run_A(xf, wi_cd[j],
                                 [(0, 512), (512, 512), (1544, 512), (2056, 512)],
                                 [(1024, 512), (2568, 512)],
                                 [(1536, 8)])
            o = run_B_cd(yT, ytb, ytf, f32(b_forget[j]), rel_table)
            wf = np.concatenate([ffn_chunk_layout(w1m[j, e], w3m[j, e], w2m[j, e]) for e in range(8)], axis=0)
            xf = run_C(o, xf, wo_cd[j], lnp4, wf, 8, 14, 2, wr=f32(w_router[j]))
    return xf.reshape(BATCH, SEQ, D).astype(np.float32)


I32 = mybir.dt.int32

import os
SKIP = os.environ.get('FZ_SKIP', '')
GROUPS = [[0, 1, 2, 3], [4, 5, 6, 7]]
LK = [512 * (k + 1) for k in range(16)]
MOFF = [128 * sum(LK[:k]) for k in range(16)]
MTOT = 128 * sum(LK)
MPARTS = []
_g = 0
for _k in range(16):
    _r0 = MOFF[_k] // 512
    _n = 128 * LK[_k] // 512
    _halves = 1 if _k < 8 else 2
    for _h in range(_halves):
        _nh = _n // _halves
        MPARTS.append((_k, _h, _r0 + _h * _nh, _nh, _g))
        _g += 4 * _nh
MG = {(p[0], p[1]): p for p in MPARTS}


class KBF(KB):
    def __init__(self):
        super().__init__()
        self.ccsem = self.es.enter_context(self.nc.semaphore("ccsem"))
        self.cccnt = 0
        self.stack = [self.es]

    def sb(self, shape, dt, name="sb"):
        return Buf(self.stack[-1].enter_context(self.nc.sbuf_tensor(self._nm(name), list(shape), dt)))

    def ps(self, shape, dt=F32, name="ps"):
        b = Buf(self.stack[-1].enter_context(self.nc.psum_tensor(self._nm(name), list(shape), dt)))
        b.psum = True
        return b

    def barrier(self):
        evs = []
        for q in ("sp", "act", "pool"):
            for i in range(self.NDS):
                if self.dcnt[q][i] > 0:
                    evs.append((self.dsem[q][i], self.dcnt[q][i], "d%s%d" % (q, i)))
        for e in ("pe", "dve", "act", "pool", "sp"):
            if self.ecnt[e] > 0:
                evs.append((self.esem[e], self.ecnt[e], e))
        if self.cccnt > 0:
            evs.append((self.ccsem, self.cccnt, "cc"))
        for e in ("pe", "dve", "act", "pool", "sp"):
            for ev in evs:
                if ev[2] == e:
                    continue
                self._wait(e, ev)

    def scope(self):
        kb = self

        class _S:
            def __enter__(s):
                st = ExitStack()
                kb.stack.append(st)
                return st

            def __exit__(s, *a):
                kb.barrier()
                st = kb.stack.pop()
                st.close()
                return False

        return _S()

    def dram_tmp(self, name, shape, dt):
        return Buf(self.nc.dram_tensor(name, list(shape), dt).ap())

    def collective(self, kind, src, dst, src_ap=None, dst_ap=None, track_src=True):
        self._deps("pool", [src], [dst])
        sa = src.t if src_ap is None else src_ap
        da = dst.t if dst_ap is None else dst_ap
        ins = self.nc.gpsimd.collective_compute(kind, ALU.bypass, replica_groups=GROUPS,
                                                ins=[sa.opt()], outs=[da.opt()])
        self.cccnt += CC_INC
        ins.then_inc(self.ccsem, CC_INC)
        ev = (self.ccsem, self.cccnt, "cc")
        _kbf_mark(self, ev, [src] if track_src else [], [dst])
        return ev


CC_INC = 1


def load_w_cast(kb, dst, src_d, ncols):
    for kc in range(8):
        kb.dma("pool", dst[:, kc, :ncols], src_d[:, kc, :ncols], R=[src_d], Wd=[dst])


def load_xT_chunk(kb, x_, xT_all, c):
    r, t0 = c // 4, (c % 4) * 512
    for cf in range(4):
        row0 = (cf * 4 + r) * 256
        kb.dma("sp", x_[:, 2 * cf:2 * cf + 2, :],
               xT_all[row0:row0 + 256, t0:t0 + 512].rearrange("(k p) t -> p k t", p=128),
               R=[xT_all], Wd=[x_] if cf else (), W=[x_] if cf == 0 else ())


def emit_xT(kb, ax_ps, ident, xtile, tt, stg, xT_loc, xTs_loc):
    nc = kb.nc
    for kc in range(8):
        pt = ax_ps[kc // 4]
        kb.op("pe", lambda: nc.tensor.transpose(pt[:, (kc % 4) * 128:(kc % 4 + 1) * 128],
                                                xtile[:, kc * 128:(kc + 1) * 128], ident[:, :]),
              R=[xtile, ident], W=[pt])
    q = tt % 4
    for hf in range(2):
        src = ax_ps[hf][:, :].rearrange("p (k t) -> p k t", k=4)
        dst = stg[:, hf * 4:(hf + 1) * 4, q * 128:(q + 1) * 128]
        if hf == 0:
            kb.op("dve", lambda: nc.vector.tensor_copy(out=dst, in_=src), R=[ax_ps[hf]], W=[stg])
        else:
            kb.op("act", lambda: nc.scalar.copy(out=dst, in_=src), R=[ax_ps[hf]], W=[stg])
    if q == 3:
        g = tt // 4
        kb.dma("sp", xT_loc[:, g * 512:(g + 1) * 512].rearrange("(k p) t -> p k t", p=128), stg[:, :, :],
               R=[stg], Wd=[xT_loc])
        if xTs_loc is not None:
            for j in range(4):
                kb.dma("sp", xTs_loc[j * D:(j + 1) * D, g * 128:(g + 1) * 128].rearrange("(k p) t -> p k t", p=128),
                       stg[:, :, j * 128:(j + 1) * 128], R=[stg], Wd=[xTs_loc])


def toeplitz_load(kb, TT, E_d, h, q="act"):
    for s in range(128):
        kb.dma(q if s % 2 == 0 else "sp", TT[s:s + 1, :], E_d[h:h + 1, 127 - s:127 - s + 17 * 128], R=[E_d], Wd=[TT])


def build_E_table(kb, ax, rel_d, C_d, E_d):
    nc = kb.nc
    rel = kb.sb([32, 2], F32, "rel")
    Cs = kb.sb([32, NDEL], F32, "Cs")
    Es = kb.sb([2, NDEL], F32, "Es")
    kb.dma("sp", rel[:, :], rel_d[:, :], R=[rel_d], W=[rel])
    kb.dma("sp", Cs[:, :], C_d[:, :], R=[C_d], W=[Cs])
    kb.op("act", lambda: nc.scalar.activation(out=rel[:, :], in_=rel[:, :], func=AF.Exp), R=[rel], W=[rel])
    for c in range((NDEL + 511) // 512):
        w = min(512, NDEL - c * 512)
        pp = ax.S[c % 3]
        kb.op("pe", lambda: nc.tensor.matmul(pp[:2, :w], lhsT=rel[:, :], rhs=Cs[:, c * 512:c * 512 + w],
                                             start=True, stop=True), R=[rel, Cs], W=[pp])
        kb.op("dve", lambda: nc.vector.tensor_copy(out=Es[:, c * 512:c * 512 + w], in_=pp[:2, :w]), R=[pp], W=[Es])
    kb.dma("sp", E_d[:, :], Es[:, :], R=[Es], W=[E_d])


def oT_exchange(kb, oT_loc, oT_all, j):
    kb.collective("AllGather", oT_loc, oT_all, oT_loc[j * 1024:(j + 1) * 1024, :],
                  oT_all[j * 4096:(j + 1) * 4096, :], track_src=False)


class OTOut:
    def __init__(self, kb, identb, oT_loc, row0, name):
        self.kb, self.identb, self.oT_loc, self.row0 = kb, identb, oT_loc, row0
        self.pt = [kb.ps([128, 1024], BF16, "ptO" + name) for _ in range(1)]
        self.stg = [kb.sb([128, 512], BF16, "stgO" + name) for _ in range(2)]
        self.n = 0

    def put(self, i, ost):
        kb, nc = self.kb, self.kb.nc
        g = i // 4
        st = self.stg[g % 2]
        pt = self.pt[0]
        q = i % 4
        kb.op("pe", lambda: nc.tensor.transpose(pt[:, q * 128:(q + 1) * 128], ost[:, :], self.identb[:, :]),
              R=[ost, self.identb], W=[pt])
        if q == 3:
            kb.op("act", lambda: nc.scalar.copy(out=st[:, :], in_=pt[:, :512]), R=[pt], W=[st])
            rank, grp = g // 4, g % 4
            r0 = (rank * 4 + grp) * 256 + self.row0
            kb.dma("sp", self.oT_loc[r0:r0 + 128, :], st[:, :], R=[st], Wd=[self.oT_loc])


def phase_cd(kb, L, xT_all, oT_loc, oT_all, cst):
    nc = kb.nc
    with kb.scope():
        wcd_d = L["wcd"]
        NCOL = 770
        w = kb.sb([128, 8, NCOL], BF16, "wcd")
        load_w_cast(kb, w, wcd_d, NCOL)
        tri = kb.sb([128, 128], F32, "tri")
        ones = kb.sb([128, 128], F32, "ones")
        identb = kb.sb([128, 128], BF16, "identb")
        kb.dma("sp", tri[:, :], cst["tri"][:, :], R=[cst["tri"]], W=[tri])
        kb.dma("sp", identb[:, :], cst["identb"][:, :], R=[cst["identb"]], W=[identb])
        kb.op("dve", lambda: nc.vector.memset(ones[:, :], 1.0), W=[ones])
        ax = AttnCtx(kb, n_s=4, n_o=2, grouped=True, fox=True)
        E_d = L["Escr"]
        with kb.scope():
            build_E_table(kb, ax, L["rel2"], cst["dilc"], E_d)
        TT = [kb.sb([128, 17 * 128], F32, "TT") for _ in range(2)]
        for h in range(2):
            if "toep" in SKIP:
                kb.op("dve", lambda: nc.vector.memset(TT[h][:, :], 1.0), W=[TT[h]])
            else:
                toeplitz_load(kb, TT[h], E_d, h)
        qc = kb.sb([128, SEQ], BF16, "qc")
        kc_ = kb.sb([128, SEQ], BF16, "kc")
        qd = kb.sb([128, SEQ], BF16, "qd")
        kd = kb.sb([128, SEQ], BF16, "kd")
        Vc = kb.sb([128, NQB, 2, 65], BF16, "Vc")
        Vd = kb.sb([128, NQB, 2, 65], BF16, "Vd")
        fc = kb.sb([128, NQB, 2], F32, "fc")
        kb.op("pool", lambda: nc.gpsimd.memset(Vc[:, :, :, :], 1.0), W=[Vc])
        kb.op("pool", lambda: nc.gpsimd.memset(Vd[:, :, :, :], 1.0), W=[Vd])
        xc = [kb.sb([128, 8, 512], BF16, "xc") for _ in range(2)]
        n_ev = 0
        passes = [(True, True)] if "twopass" not in SKIP else [(True, False), (False, True)]
        NCH = int(os.environ.get("FZ_NCH", "16"))
        for c2 in range((NCH if "proj" not in SKIP else 0) * len(passes)):
            c = c2 % NCH
            do_fm, do_tm = passes[c2 // NCH]
            x_ = xc[c % 2]
            load_xT_chunk(kb, x_, xT_all, c)
            for bi, dst in enumerate((qc, kc_, qd, kd) if ("projfm" not in SKIP and do_fm) else ()):
                pp = ax.S[n_ev % 4]
                for k8 in range(8):
                    kb.op("pe", lambda: nc.tensor.matmul(pp[:, :], lhsT=w[:, k8, bi * 128:(bi + 1) * 128],
                                                         rhs=x_[:, k8, :], start=(k8 == 0), stop=(k8 == 7)),
                          R=[w, x_], W=[pp])
                d_ = dst[:, c * 512:(c + 1) * 512]
                if n_ev % 2 == 0 or "fmdve" in SKIP:
                    kb.op("dve", lambda: nc.vector.tensor_copy(out=d_, in_=pp[:, :]), R=[pp], W=[dst])
                else:
                    kb.op("act", lambda: nc.scalar.copy(out=d_, in_=pp[:, :]), R=[pp], W=[dst])
                n_ev += 1
            for t4 in range(4 if ("projtm" not in SKIP and do_tm) else 0):
                j = c * 4 + t4
                pp = ax.S[n_ev % 4]
                n_ev += 1
                for k8 in range(8):
                    kb.op("pe", lambda: nc.tensor.matmul(pp[:, :258], lhsT=x_[:, k8, t4 * 128:(t4 + 1) * 128],
                                                         rhs=w[:, k8, 512:770], start=(k8 == 0), stop=(k8 == 7)),
                          R=[w, x_], W=[pp])
                kb.op("dve", lambda: nc.vector.tensor_copy(out=Vc[:, j, :, 0:64],
                                                           in_=pp[:, 0:128].rearrange("p (h d) -> p h d", h=2)),
                      R=[pp], W=[Vc])
                if "vddve" in SKIP:
                    kb.op("dve", lambda: nc.vector.tensor_copy(out=Vd[:, j, :, 0:64],
                                                               in_=pp[:, 128:256].rearrange("p (h d) -> p h d", h=2)),
                          R=[pp], W=[Vd])
                else:
                    kb.op("act", lambda: nc.scalar.copy(out=Vd[:, j, :, 0:64],
                                                        in_=pp[:, 128:256].rearrange("p (h d) -> p h d", h=2)),
                          R=[pp], W=[Vd])
                kb.op("dve", lambda: nc.vector.tensor_copy(out=fc[:, j, :], in_=pp[:, 256:258]), R=[pp], W=[fc])
        bf = kb.sb([128, 2], F32, "bf")
        lf = kb.sb([128, 2, NQB], F32, "lf")
        kb.dma("sp", bf[:, :], L["bf"][:, :], R=[L["bf"]], W=[bf])
        for h in range(2):
            kb.op("dve", lambda: nc.vector.tensor_scalar(out=lf[:, h, :], in0=fc[:, :, h], scalar1=bf[:, h:h + 1],
                                                         scalar2=None, op0=ALU.add), R=[fc, bf], W=[lf])
        kb.op("act", lambda: nc.scalar.activation(out=lf[:, :, :], in_=lf[:, :, :], func=AF.Exp, scale=-1.0),
              R=[lf], W=[lf])
        kb.op("act", lambda: nc.scalar.activation(out=lf[:, :, :], in_=lf[:, :, :], func=AF.Ln, bias=1.0),
              R=[lf], W=[lf])
        kb.op("dve", lambda: nc.vector.tensor_scalar(out=lf[:, :, :], in0=lf[:, :, :], scalar1=-1.0, scalar2=None,
                                                     op0=ALU.mult), R=[lf], W=[lf])
        lf2 = lf[:, :, :].rearrange("p h j -> p (h j)")
        p1, p2 = ax.O[0], ax.O[1]
        kb.op("pe", lambda: nc.tensor.matmul(p1[:, :128], lhsT=tri[:, :], rhs=lf2, start=True, stop=True),
              R=[tri, lf], W=[p1])
        kb.op("pe", lambda: nc.tensor.matmul(p2[:, :128], lhsT=ones[:, :], rhs=lf2, start=True, stop=True),
              R=[ones, lf], W=[p2])
        tot = kb.sb([128, 2, NQB], F32, "tot")
        carry = kb.sb([128, 2, NQB], F32, "carry")
        negF = kb.sb([128, 2, NQB], F32, "negF")
        kb.op("dve", lambda: nc.vector.tensor_copy(out=tot[:, :, :].rearrange("p h j -> p (h j)"), in_=p2[:, :128]),
              R=[p2], W=[tot])
        for h in range(2):
            kb.op("dve", lambda: nc.vector.tensor_tensor_scan(out=carry[:, h, :], data0=ones[:, :NQB],
                                                              data1=tot[:, h, :], initial=0.0, op0=ALU.mult,
                                                              op1=ALU.add), R=[ones, tot], W=[carry])
        kb.op("dve", lambda: nc.vector.tensor_tensor(out=carry[:, :, :], in0=carry[:, :, :], in1=tot[:, :, :],
                                                     op=ALU.subtract), R=[carry, tot], W=[carry])
        kb.op("dve", lambda: nc.vector.tensor_tensor(out=negF[:, :, :].rearrange("p h j -> p (h j)"), in0=p1[:, :128],
                                                     in1=carry[:, :, :].rearrange("p h j -> p (h j)"), op=ALU.add),
              R=[p1, carry], W=[negF])
        kb.op("dve", lambda: nc.vector.tensor_scalar(out=negF[:, :, :], in0=negF[:, :, :], scalar1=-1.0, scalar2=None,
                                                     op0=ALU.mult), R=[negF], W=[negF])
        outc = OTOut(kb, identb, oT_loc, 0, "c")
        outd = OTOut(kb, identb, oT_loc, 128, "d")
        ostd = [kb.sb([128, 128], BF16, "ostd") for _ in range(2)]
        ostc = [kb.sb([128, 128], BF16, "ostc") for _ in range(2)]
        Bi = [kb.sb([128, NQB], F32, "Bi") for _ in range(3)]
        tri2 = kb.sb([128, 256], F32, "tri2")
        kb.op("dve", lambda: nc.vector.memset(tri2[:, :], 1.0), W=[tri2])
        kb.op("dve", lambda: nc.vector.tensor_copy(out=tri2[:, 0:128], in_=tri[:, :]), R=[tri], W=[tri2])
        nb = 0
        for p in range(NQB // 2 if "attn" not in SKIP else 0):
            for i in (2 * p, 2 * p + 1):
                od = ostd[i % 2]
                for h in range(2):
                    jdesc = list(range(i, max(0, i - 16) - 1, -1))
                    groups = []
                    for a in range(0, len(jdesc), 4):
                        js = jdesc[a:a + 4]
                        d0 = i - js[0]
                        groups.append({"js": js, "dve_mask": (TT[h], TT[h][:, d0 * 128:(d0 + len(js)) * 128])})
                    attn_qgroup(ax, qd, kd, Vd, h, i, groups, od, h * 64,
                                after=(None if h == 0 else (lambda i=i, od=od: outd.put(i, od))))
            i0_ = 2 * p
            for h in range(2):
                B = Bi[nb % 3]
                nb += 1
                kb.op("dve", lambda: nc.vector.tensor_scalar(out=B[:, :i0_ + 2], in0=negF[:, h, :i0_ + 2],
                                                             scalar1=carry[:, h, i0_:i0_ + 1], scalar2=None,
                                                             op0=ALU.add), R=[negF, carry], W=[B])

                def fin(i0_=i0_):
                    outc.put(i0_, ostc[0])
                    outc.put(i0_ + 1, ostc[1])
                    if (i0_ + 1) % 16 == 15:
                        oT_exchange(kb, oT_loc, oT_all, (i0_ + 1) // 16)

                attn_fox_pair(ax, qc, kc_, Vc, h, i0_, B, tri2, (ostc[0], ostc[1]), h * 64,
                              after=(None if h == 0 else fin))
        attn_flush(ax)


def phase_gla(kb, L, xT_all, oT_loc, cst):
    nc = kb.nc
    with kb.scope():
        tri = kb.sb([128, 128], F32, "tri")
        identb = kb.sb([128, 128], BF16, "identb")
        gn = kb.sb([128, 128], F32, "gn")
        eps = kb.sb([128, 1], F32, "eps")
        kb.dma("sp", tri[:, :], cst["tri"][:, :], R=[cst["tri"]], W=[tri])
        kb.dma("sp", identb[:, :], cst["identb"][:, :], R=[cst["identb"]], W=[identb])
        kb.dma("sp", gn[:, :], L["gn"][:, :], R=[L["gn"]], W=[gn])
        kb.op("dve", lambda: nc.vector.memset(eps[:, :], LN_EPS), W=[eps])
        qT = kb.sb([64, SEQ], BF16, "qT")
        kT = kb.sb([64, SEQ], BF16, "kT")
        ktm = kb.sb([128, NQB, 64], BF16, "ktm")
        v = kb.sb([128, NQB, 128], BF16, "v")
        r = kb.sb([128, NQB, 128], F32, "r")
        g = kb.sb([128, NQB, 64], F32, "g")
        PG = [kb.ps([128, 512], F32, "PG")]
        PGT = [kb.ps([128, 512], F32, "PGT")]
        PA = [kb.ps([128, 512], F32, "PA") for _ in range(2)]
        PO = kb.ps([128, 512], F32, "PO")
        PU = kb.ps([128, 512], F32, "PU")
        out = OTOut(kb, identb, oT_loc, 0, "g")
        with kb.scope():
            NCOL = 464
            w = kb.sb([128, 8, NCOL], BF16, "wgl")
            load_w_cast(kb, w, L["wgl"], NCOL)
            wg = kb.sb([16, 64], F32, "wg")
            bg = kb.sb([128, 64], F32, "bg")
            kb.dma("sp", wg[:, :], L["wg"][:, :], R=[L["wg"]], W=[wg])
            kb.dma("sp", bg[:, :], L["bg"][:, :], R=[L["bg"]], W=[bg])
            xc = [kb.sb([128, 8, 512], BF16, "xc") for _ in range(2)]
            gaT = [kb.sb([16, 512], F32, "gaT") for _ in range(2)]
            zt = [kb.sb([128, 64], F32, "zt") for _ in range(2)]
            pr = [PA[0], PA[1], PO]
            n_ev = 0
            for c in range(16):
                x_ = xc[c % 2]
                ga_ = gaT[c % 2]
                load_xT_chunk(kb, x_, xT_all, c)
                for (c0, ncol, dst) in ((0, 64, qT), (64, 64, kT), (128, 16, ga_)):
                    pp = pr[n_ev % 3]
                    for k8 in range(8):
                        kb.op("pe", lambda: nc.tensor.matmul(pp[:ncol, :], lhsT=w[:, k8, c0:c0 + ncol], rhs=x_[:, k8, :],
                                                             start=(k8 == 0), stop=(k8 == 7)), R=[w, x_], W=[pp])
                    d_ = dst[:, c * 512:(c + 1) * 512] if dst is not ga_ else ga_[:, :]
                    if n_ev % 2 == 0:
                        kb.op("dve", lambda: nc.vector.tensor_copy(out=d_, in_=pp[:ncol, :]), R=[pp], W=[dst])
                    else:
                        kb.op("act", lambda: nc.scalar.copy(out=d_, in_=pp[:ncol, :]), R=[pp], W=[dst])
                    n_ev += 1
                for t4 in range(4):
                    j = c * 4 + t4
                    pp = pr[n_ev % 3]
                    n_ev += 1
                    for k8 in range(8):
                        kb.op("pe", lambda: nc.tensor.matmul(pp[:, :320], lhsT=x_[:, k8, t4 * 128:(t4 + 1) * 128],
                                                             rhs=w[:, k8, 144:464], start=(k8 == 0), stop=(k8 == 7)),
                              R=[w, x_], W=[pp])
                    kb.op("dve", lambda: nc.vector.tensor_copy(out=ktm[:, j, :], in_=pp[:, 0:64]), R=[pp], W=[ktm])
                    kb.op("dve", lambda: nc.vector.tensor_copy(out=v[:, j, :], in_=pp[:, 64:192]), R=[pp], W=[v])
                    kb.op("act", lambda: nc.scalar.activation(out=r[:, j, :], in_=pp[:, 192:320], func=AF.Silu),
                          R=[pp], W=[r])
                    pq = pr[n_ev % 3]
                    n_ev += 1
                    z = zt[j % 2]
                    kb.op("pe", lambda: nc.tensor.matmul(pq[:, :64], lhsT=ga_[:, t4 * 128:(t4 + 1) * 128], rhs=wg[:, :],
                                                         start=True, stop=True), R=[ga_, wg], W=[pq])
                    kb.op("dve", lambda: nc.vector.tensor_tensor(out=z[:, :], in0=pq[:, :64], in1=bg[:, :], op=ALU.add),
                          R=[pq, bg], W=[z])
                    kb.op("act", lambda: nc.scalar.activation(out=z[:, :], in_=z[:, :], func=AF.Exp, scale=-1.0),
                          R=[z], W=[z])
                    kb.op("act", lambda: nc.scalar.activation(out=z[:, :], in_=z[:, :], func=AF.Ln, bias=1.0),
                          R=[z], W=[z])
                    kb.op("dve", lambda: nc.vector.tensor_scalar(out=g[:, j, :], in0=z[:, :], scalar1=-1.0 / 16.0,
                                                                 scalar2=None, op0=ALU.mult), R=[z], W=[g])
        eGT = [kb.sb([64, 128], F32, "eGT") for _ in range(2)]
        enGT = [kb.sb([64, 128], F32, "enGT") for _ in range(2)]
        enG = [kb.sb([128, 64], F32, "enG") for _ in range(2)]
        qgT = [kb.sb([64, 128], BF16, "qgT") for _ in range(2)]
        kgT = [kb.sb([64, 128], BF16, "kgT") for _ in range(2)]
        kg = [kb.sb([128, 64], BF16, "kg") for _ in range(2)]
        Am = [kb.sb([128, 128], BF16, "Am") for _ in range(2)]
        S32 = kb.sb([64, 128], F32, "S32")
        Sbf = kb.sb([64, 128], BF16, "Sbf")
        st6 = [kb.sb([128, 6], F32, "st6") for _ in range(2)]
        mv = [kb.sb([128, 4], F32, "mv") for _ in range(2)]
        of = [kb.sb([128, 128], F32, "of") for _ in range(2)]
        ost = [kb.sb([128, 128], BF16, "ost") for _ in range(2)]
        for c in range(NQB):
            p = c % 2
            cs = slice(c * 128, (c + 1) * 128)
            kb.op("pe", lambda: nc.tensor.matmul(PG[0][:, :64], lhsT=tri[:, :], rhs=g[:, c, :], start=True, stop=True),
                  R=[tri, g], W=[PG[0]])
            kb.op("pe", lambda: nc.tensor.matmul(PGT[0][:64, :128], lhsT=g[:, c, :], rhs=tri[:, :], start=True, stop=True),
                  R=[tri, g], W=[PGT[0]])
            kb.op("act", lambda: nc.scalar.activation(out=eGT[p][:, :], in_=PGT[0][:64, :128], func=AF.Exp),
                  R=[PGT[0]], W=[eGT[p]])
            kb.op("act", lambda: nc.scalar.activation(out=enGT[p][:, :], in_=PGT[0][:64, :128], func=AF.Exp, scale=-1.0),
                  R=[PGT[0]], W=[enGT[p]])
            kb.op("act", lambda: nc.scalar.activation(out=enG[p][:, :], in_=PG[0][:, :64], func=AF.Exp, scale=-1.0),
                  R=[PG[0]], W=[enG[p]])
            kb.op("dve", lambda: nc.vector.scalar_tensor_tensor(out=qgT[p][:, :], in0=qT[:, cs], scalar=0.125,
                                                                in1=eGT[p][:, :], op0=ALU.mult, op1=ALU.mult),
                  R=[qT, eGT[p]], W=[qgT[p]])
            kb.op("dve", lambda: nc.vector.tensor_tensor(out=kgT[p][:, :], in0=kT[:, cs], in1=enGT[p][:, :], op=ALU.mult),
                  R=[kT, enGT[p]], W=[kgT[p]])
            kb.op("dve", lambda: nc.vector.tensor_tensor(out=kg[p][:, :], in0=ktm[:, c, :], in1=enG[p][:, :], op=ALU.mult),
                  R=[ktm, enG[p]], W=[kg[p]])
            kb.op("pe", lambda: nc.tensor.matmul(PA[p][:, :128], lhsT=kgT[p][:, :], rhs=qgT[p][:, :], start=True, stop=True),
                  R=[kgT[p], qgT[p]], W=[PA[p]])
            kb.op("dve", lambda: nc.vector.tensor_tensor(out=Am[p][:, :], in0=PA[p][:, :128], in1=tri[:, :], op=ALU.mult),
                  R=[PA[p], tri], W=[Am[p]])
            kb.op("pe", lambda: nc.tensor.matmul(PO[:, :128], lhsT=Am[p][:, :], rhs=v[:, c, :], start=True, stop=(c == 0)),
                  R=[Am[p], v], W=[PO])
            if c > 0:
                kb.op("pe", lambda: nc.tensor.matmul(PO[:, :128], lhsT=qgT[p][:, :], rhs=Sbf[:, :], start=False, stop=True),
                      R=[qgT[p], Sbf], W=[PO])
            if c < NQB - 1:
                kb.op("pe", lambda: nc.tensor.matmul(PU[:64, :128], lhsT=kg[p][:, :], rhs=v[:, c, :], start=True, stop=True),
                      R=[kg[p], v], W=[PU])
                eGl = eGT[p][:, 127:128]
                if c == 0:
                    kb.op("dve", lambda: nc.vector.tensor_scalar(out=S32[:, :], in0=PU[:64, :128], scalar1=eGl,
                                                                 scalar2=None, op0=ALU.mult), R=[PU, eGT[p]], W=[S32])
                else:
                    kb.op("dve", lambda: nc.vector.tensor_scalar(out=S32[:, :], in0=S32[:, :], scalar1=eGl, scalar2=None,
                                                                 op0=ALU.mult), R=[S32, eGT[p]], W=[S32])
                    kb.op("dve", lambda: nc.vector.scalar_tensor_tensor(out=S32[:, :], in0=PU[:64, :128], scalar=eGl,
                                                                        in1=S32[:, :], op0=ALU.mult, op1=ALU.add),
                          R=[PU, eGT[p], S32], W=[S32])
                kb.op("dve", lambda: nc.vector.tensor_copy(out=Sbf[:, :], in_=S32[:, :]), R=[S32], W=[Sbf])
            kb.op("dve", lambda: nc.vector.bn_stats(out=st6[p][:, :], in_=PO[:, :128]), R=[PO], W=[st6[p]])
            kb.op("dve", lambda: nc.vector.bn_aggr(out=mv[p][:, 0:2], in_=st6[p][:, :]), R=[st6[p]], W=[mv[p]])
            kb.op("dve", lambda: nc.vector.scalar_tensor_tensor(out=mv[p][:, 2:3], in0=mv[p][:, 0:1], scalar=mv[p][:, 0:1],
                                                                in1=mv[p][:, 1:2], op0=ALU.mult, op1=ALU.add),
                  R=[mv[p]], W=[mv[p]])
            kb.op("act", lambda: nc.scalar.activation(out=mv[p][:, 3:4], in_=mv[p][:, 2:3], func=AF.Ln, bias=eps[:, 0:1]),
                  R=[mv[p], eps], W=[mv[p]])
            kb.op("act", lambda: nc.scalar.activation(out=mv[p][:, 3:4], in_=mv[p][:, 3:4], func=AF.Exp, scale=-0.5),
                  R=[mv[p]], W=[mv[p]])
            kb.op("dve", lambda: nc.vector.scalar_tensor_tensor(out=of[p][:, :], in0=PO[:, :128], scalar=mv[p][:, 3:4],
                                                                in1=gn[:, :], op0=ALU.mult, op1=ALU.mult),
                  R=[PO, mv[p], gn], W=[of[p]])
            kb.op("pool", lambda: nc.gpsimd.tensor_tensor(out=ost[p][:, :], in0=of[p][:, :], in1=r[:, c, :], op=ALU.mult),
                  R=[of[p], r], W=[ost[p]])
            out.put(c, ost[p])


def _kbf_deps(self, e, R, W, Wd=()):
    evs = []
    for b in R:
        evs.extend(b.wl())
        if getattr(b, "psum", False):
            evs.extend(x for x in b.r if x[2] != e)
    for b in W:
        evs.extend(b.wl())
        evs.extend(b.r)
    for b in Wd:
        evs.extend(b.r)
        evs.extend(getattr(b, "w_excl", []))
    best = {}
    for ev in evs:
        k = ev[2]
        if e == "pe" and k == "pe":
            continue
        if k not in best or best[k][1] < ev[1]:
            best[k] = ev
    for ev in best.values():
        self._wait(e, ev)


def _buf_wl(self):
    if self.w is None:
        return []
    return self.w if isinstance(self.w, list) else [self.w]


Buf.wl = _buf_wl


def _kbf_mark(self, ev, R, W, Wd=()):
    KB._mark(self, ev, R, W)
    for b in W:
        b.w = [ev]
        b.w_excl = [ev]
    for b in Wd:
        cur = b.wl()
        cur = [x for x in cur if x[2] != ev[2]] + [ev]
        b.w = cur
        b.r = []


def _kbf_dma(self, q, out, in_, R=(), W=(), Wd=(), **kw):
    _kbf_deps(self, q, R, W, Wd)
    i = self.dnext[q]
    self.dnext[q] = (i + 1) % self.NDS
    key = "d%s%d" % (q, i)
    if self.dcnt[q][i] > 0:
        self._wait(q, (self.dsem[q][i], self.dcnt[q][i], key))
    self.dcnt[q][i] += 16
    self.eng[q].dma_start(out=out, in_=in_, **kw).then_inc(self.dsem[q][i], 16)
    ev = (self.dsem[q][i], self.dcnt[q][i], key)
    _kbf_mark(self, ev, R, W, Wd)
    return ev


def _kbf_op(self, e, fn, R=(), W=()):
    _kbf_deps(self, e, R, W)
    ins = fn()
    self.ecnt[e] += 1
    ins.then_inc(self.esem[e], 1)
    ev = (self.esem[e], self.ecnt[e], e)
    _kbf_mark(self, ev, R, W)
    return ev


def _kbf_idma(self, out, in_, idx_ap, R=(), W=(), Wd=()):
    q = "pool"
    _kbf_deps(self, q, R, W, Wd)
    i = self.dnext[q]
    self.dnext[q] = (i + 1) % self.NDS
    key = "d%s%d" % (q, i)
    if self.dcnt[q][i] > 0:
        self._wait(q, (self.dsem[q][i], self.dcnt[q][i], key))
    self.dcnt[q][i] += 16
    self.nc.gpsimd.indirect_dma_start(out=out, out_offset=None, in_=in_,
                                      in_offset=bass.IndirectOffsetOnAxis(ap=idx_ap, axis=0)
                                      ).then_inc(self.dsem[q][i], 16)
    ev = (self.dsem[q][i], self.dcnt[q][i], key)
    _kbf_mark(self, ev, R, W, Wd)
    return ev


KBF.idma = _kbf_idma
KBF._deps = lambda self, e, R, W: _kbf_deps(self, e, R, W)
KBF.dma = _kbf_dma
KBF.op = _kbf_op


def phase_dsa1(kb, L, xT_all, xTs_all, M_loc, M_all, cst, act_split=True):
    nc = kb.nc
    NK = 16
    with kb.scope():
        qiT = kb.sb([64, 8, NK * 128], BF16, "qiT")
        kiT = kb.sb([64, SEQ], BF16, "kiT")
        wi = kb.sb([128, NK, 8], F32, "wi")
        absw = kb.sb([128, NK, 8], F32, "absw")
        sgn = kb.sb([128, NK, 8], F32, "sgn")
        cm = kb.sb([128, 512], F32, "cm")
        idb = kb.sb([128, 128], BF16, "idb")
        stp = kb.sb([128, NBIS], F32, "stp")
        kb.dma("sp", cm[:, :], cst["cmask"][:, :], R=[cst["cmask"]], W=[cm])
        kb.dma("sp", idb[:, :], cst["identb"][:, :], R=[cst["identb"]], W=[idb])
        kb.dma("sp", stp[:, :], cst["steps"][:, :], R=[cst["steps"]], W=[stp])
        idxs = kb.sb([128, 32], I32, "idxs")
        kb.dma("sp", idxs[:, :], cst["idx_s"][:, :], R=[cst["idx_s"]], W=[idxs])
        PS = [kb.ps([128, 512], F32, "PS") for _ in range(3)]
        PT = [kb.ps([128, 1024], BF16, "PT") for _ in range(2)]
        with kb.scope():
            NCOL = 584
            w = kb.sb([128, 8, NCOL], BF16, "wd1")
            load_w_cast(kb, w, L["wd1"], NCOL)
            xc = [kb.sb([128, 8, 512], BF16, "xc") for _ in range(2)]
            n_ev = 0
            for c in range(16):
                x_ = xc[c % 2]
                load_xT_chunk(kb, x_, xT_all, c)
                pp = PS[n_ev % 3]
                n_ev += 1
                for k8 in range(8):
                    kb.op("pe", lambda: nc.tensor.matmul(pp[:64, :], lhsT=w[:, k8, 512:576], rhs=x_[:, k8, :],
                                                         start=(k8 == 0), stop=(k8 == 7)), R=[w, x_], W=[pp])
                kb.op("dve", lambda: nc.vector.tensor_copy(out=kiT[:, c * 512:(c + 1) * 512], in_=pp[:64, :]),
                      R=[pp], W=[kiT])
            for r in range(4):
                x_ = xc[r % 2]
                for k8 in range(8):
                    kb.idma(x_[:, k8, :], xTs_all[:, :], idxs[:, r * 8 + k8:r * 8 + k8 + 1], R=[xTs_all, idxs],
                            Wd=[x_] if k8 else (), W=[x_] if k8 == 0 else ())
                for hi in range(8):
                    pp = PS[n_ev % 3]
                    n_ev += 1
                    for k8 in range(8):
                        kb.op("pe", lambda: nc.tensor.matmul(pp[:64, :], lhsT=w[:, k8, hi * 64:(hi + 1) * 64],
                                                             rhs=x_[:, k8, :], start=(k8 == 0), stop=(k8 == 7)),
                              R=[w, x_], W=[pp])
                    if hi % 2 == 0:
                        kb.op("dve", lambda: nc.vector.tensor_copy(out=qiT[:, hi, r * 512:(r + 1) * 512], in_=pp[:64, :]),
                              R=[pp], W=[qiT])
                    else:
                        kb.op("act", lambda: nc.scalar.copy(out=qiT[:, hi, r * 512:(r + 1) * 512], in_=pp[:64, :]),
                              R=[pp], W=[qiT])
                for t4 in range(4):
                    pp = PS[n_ev % 3]
                    n_ev += 1
                    for k8 in range(8):
                        kb.op("pe", lambda: nc.tensor.matmul(pp[:, :8], lhsT=x_[:, k8, t4 * 128:(t4 + 1) * 128],
                                                             rhs=w[:, k8, 576:584], start=(k8 == 0), stop=(k8 == 7)),
                              R=[w, x_], W=[pp])
                    kb.op("dve", lambda: nc.vector.tensor_copy(out=wi[:, r * 4 + t4, :], in_=pp[:, :8]), R=[pp], W=[wi])
        kb.op("act", lambda: nc.scalar.activation(out=absw[:, :, :], in_=wi[:, :, :], func=AF.Abs), R=[wi], W=[absw])
        kb.op("act", lambda: nc.scalar.activation(out=sgn[:, :, :], in_=wi[:, :, :], func=AF.Sign), R=[wi], W=[sgn])
        def write_mask(k, mt, Lk):
            dst = _AP(tensor=M_loc.t.tensor, offset=MOFF[k], ap=[[Lk, 128], [1, Lk]])
            kb.dma("sp", dst, mt[:, :Lk], R=[mt], Wd=[M_loc])
            for (k2, hf, lrow, nrow, grow) in MPARTS:
                if k2 == k:
                    kb.collective("AllGather", M_loc, M_all, M_loc[lrow:lrow + nrow, :],
                                  M_all[grow:grow + 4 * nrow, :], track_src=False)

        dsa1_body(kb, qiT, kiT, absw, sgn, cm, idb, stp, PS, PT, write_mask, act_split)


def phase_dsa2(kb, L, xT_all, M_all, oT_loc, oT_all, cst):
    nc = kb.nc
    with kb.scope():
        identb = kb.sb([128, 128], BF16, "identb")
        kb.dma("sp", identb[:, :], cst["identb"][:, :], R=[cst["identb"]], W=[identb])
        rf = kb.sb([128, 2], F32, "rf")
        kb.dma("sp", rf[:, :], L["relfar"][:, :], R=[L["relfar"]], W=[rf])
        ax = AttnCtx(kb, n_s=4, n_o=2, grouped=True)
        E_d = L["Escr"]
        with kb.scope():
            build_E_table(kb, ax, L["rel2"], cst["dsac"], E_d)
        TT = [kb.sb([128, 17 * 128], F32, "TT") for _ in range(2)]
        for h in range(2):
            toeplitz_load(kb, TT[h], E_d, h)
        q = kb.sb([128, SEQ], BF16, "q")
        k = kb.sb([128, SEQ], BF16, "k")
        V = kb.sb([128, NQB, 2, 65], BF16, "V")
        kb.op("pool", lambda: nc.gpsimd.memset(V[:, :, :, :], 1.0), W=[V])
        with kb.scope():
            NCOL = 384
            w = kb.sb([128, 8, NCOL], BF16, "wd2")
            load_w_cast(kb, w, L["wd2"], NCOL)
            xc = [kb.sb([128, 8, 512], BF16, "xc") for _ in range(2)]
            n_ev = 0
            for c in range(16):
                x_ = xc[c % 2]
                load_xT_chunk(kb, x_, xT_all, c)
                for bi, dst in enumerate((q, k)):
                    pp = ax.S[n_ev % 3]
                    for k8 in range(8):
                        kb.op("pe", lambda: nc.tensor.matmul(pp[:, :], lhsT=w[:, k8, bi * 128:(bi + 1) * 128],
                                                             rhs=x_[:, k8, :], start=(k8 == 0), stop=(k8 == 7)),
                              R=[w, x_], W=[pp])
                    d_ = dst[:, c * 512:(c + 1) * 512]
                    if n_ev % 2 == 0:
                        kb.op("dve", lambda: nc.vector.tensor_copy(out=d_, in_=pp[:, :]), R=[pp], W=[dst])
                    else:
                        kb.op("act", lambda: nc.scalar.copy(out=d_, in_=pp[:, :]), R=[pp], W=[dst])
                    n_ev += 1
                for t4 in range(4):
                    j = c * 4 + t4
                    pp = ax.S[n_ev % 3]
                    n_ev += 1
                    for k8 in range(8):
                        kb.op("pe", lambda: nc.tensor.matmul(pp[:, :128], lhsT=x_[:, k8, t4 * 128:(t4 + 1) * 128],
                                                             rhs=w[:, k8, 256:384], start=(k8 == 0), stop=(k8 == 7)),
                              R=[w, x_], W=[pp])
                    kb.op("dve" if t4 % 2 else "act",
                          (lambda: nc.vector.tensor_copy(out=V[:, j, :, 0:64],
                                                         in_=pp[:, 0:128].rearrange("p (h d) -> p h d", h=2)))
                          if t4 % 2 else
                          (lambda: nc.scalar.copy(out=V[:, j, :, 0:64],
                                                  in_=pp[:, 0:128].rearrange("p (h d) -> p h d", h=2))),
                          R=[pp], W=[V])
        Ms = [kb.sb([128, SEQ], BF16, "Ms") for _ in range(2)]
        ost = [kb.sb([128, 128], BF16, "ost") for _ in range(2)]
        out = OTOut(kb, identb, oT_loc, 128, "b")
        for i in range(NQB):
            ms = Ms[i % 2]
            W_ = (i + 1) * 128
            r_, k_ = i % 4, i // 4
            nh = 1 if k_ < 8 else 2
            for hf in range(nh):
                (_, _, lrow, nrow, grow) = MG[(k_, hf)]
                ns = 128 // nh
                src = _AP(tensor=M_all.t.tensor, offset=(grow + r_ * nrow) * 512, ap=[[LK[k_], ns], [1, W_]])
                kb.dma("sp", ms[hf * ns:(hf + 1) * ns, :W_], src, R=[M_all],
                       Wd=[ms] if hf else (), W=[ms] if hf == 0 else ())
            o = ost[i % 2]
            for h in range(2):
                groups = []
                jn0 = max(0, i - 16)
                for a in range(0, jn0, 4):
                    js = list(range(a, min(a + 4, jn0)))
                    groups.append({"js": js, "bias": (rf, rf[:, h:h + 1]),
                                   "dve_mask": (ms, ms[:, js[0] * 128:(js[-1] + 1) * 128])})
                for a in range(jn0, i + 1, 4):
                    js = list(range(a, min(a + 4, i + 1)))
                    groups.append({"js": js,
                                   "pool_masks": [(TT[h], TT[h][:, (i - j) * 128:(i - j + 1) * 128]) for j in js],
                                   "dve_mask": (ms, ms[:, js[0] * 128:(js[-1] + 1) * 128])})
                def fin2(i=i, o=o):
                    out.put(i, o)
                    if i % 16 == 15:
                        oT_exchange(kb, oT_loc, oT_all, i // 16)

                attn_qgroup(ax, q, k, V, h, i, groups, o, h * 64, after=(None if h == 0 else fin2))
        attn_flush(ax)


def phase_c(kb, L, moe, oT_all, x_src, x_dst, xT_loc, xTs_loc, cst, last):
    nc = kb.nc
    n_exp, nfu, n_units = (8, 14, 2) if moe else (1, 11, 2)
    TG = 512
    NG = TPC // TG
    with kb.scope():
        wf_d = L["wf"]
        ident = kb.sb([128, 128], F32, "ident")
        wo = kb.sb([128, 8, D], BF16, "wo")
        lnp = kb.sb([128, 4, D], F32, "lnp")
        kb.eps_col = kb.sb([128, 1], F32, "eps")
        kb.op("dve", lambda: nc.vector.memset(kb.eps_col[:, :], LN_EPS), W=[kb.eps_col])
        kb.dma("sp", ident[:, :], cst["ident"][:, :], R=[cst["ident"]], W=[ident])
        load_w_cast(kb, wo, L["wo"], D)
        kb.dma("sp", lnp[:, :, :], L["lnp"][:, :, :], R=[L["lnp"]], W=[lnp])
        idxo = kb.sb([128, 32], I32, "idxo")
        kb.dma("sp", idxo[:, :], cst["idx_o"][:, :], R=[cst["idx_o"]], W=[idxo])
        if moe:
            wr = kb.sb([128, 8, 8], F32, "wr")
            kb.dma("sp", wr[:, :, :], L["wr"][:, :, :], R=[L["wr"]], W=[wr])
            x1T32 = kb.sb([128, 8, 128], F32, "x1T32")
            comb = [kb.sb([128, 8], F32, "comb") for _ in range(4)]
            rt = kb.sb([128, 40], F32, "rt")
        oT = [kb.sb([128, 8, TG], BF16, "oT") for _ in range(1)]
        xt = [kb.sb([128, D], F32, "xt") for _ in range(2)]
        h = kb.sb([128, D], F32, "h")
        x1g = [kb.sb([128, D], F32, "x1g") for _ in range(4)]
        x1T = kb.sb([128, 8, TG], BF16, "x1T")
        aT = [kb.sb([128, TG], BF16, "aT") for _ in range(nfu)]
        w2 = [kb.sb([128, D], BF16, "w2") for _ in range(nfu)]
        w13 = [kb.sb([128, 2048], BF16, "w13") for _ in range(4)]
        yacc = [kb.sb([128, D], F32, "yacc") for _ in range(4)]
        sil = [kb.sb([128, TG], F32, "sil") for _ in range(2)]
        ost = [kb.sb([128, D], F32, "ost") for _ in range(2)]
        stg = kb.sb([128, 8, 512], BF16, "stgx")
        scr = (kb.sb([128, 2, 6], F32, "stats"), kb.sb([128, 2], F32, "mv"), kb.sb([128, 2], F32, "sd"))
        X = [kb.ps([128, 512], F32, "X") for _ in range(4)]
        Y = [kb.ps([128, 512], F32, "Y") for _ in range(2)]
        wcnt = 0
        for g in range(NG):
            og = oT[0]
            for k8 in range(8):
                kb.idma(og[:, k8, :], oT_all[:, :], idxo[:, g * 8 + k8:g * 8 + k8 + 1], R=[oT_all, idxo],
                        Wd=[og] if k8 else (), W=[og] if k8 == 0 else ())
            for tt in range(4):
                tok0 = g * TG + tt * 128
                xi = xt[tt % 2]
                kb.dma("sp", xi[:, :], x_src[tok0:tok0 + 128, :], R=[x_src], W=[xi])
                for hf in range(2):
                    for kc in range(8):
                        kb.op("pe", lambda: nc.tensor.matmul(Y[hf][:, :], lhsT=og[:, kc, tt * 128:(tt + 1) * 128],
                                                             rhs=wo[:, kc, hf * 512:(hf + 1) * 512],
                                                             start=(kc == 0), stop=(kc == 7)), R=[og, wo], W=[Y[hf]])
                    kb.op("dve", lambda: nc.vector.scalar_tensor_tensor(out=h[:, hf * 512:(hf + 1) * 512],
                                                                        in0=xi[:, hf * 512:(hf + 1) * 512], scalar=ALPHA,
                                                                        in1=Y[hf][:, :], op0=ALU.mult, op1=ALU.add),
                          R=[xi, Y[hf]], W=[h])
                x1 = x1g[tt]
                layer_norm(kb, h, (lnp, lnp[:, 0, :]), (lnp, lnp[:, 1, :]), x1[:, :], x1, scr)
                for kc in range(8):
                    pt = X[kc // 4]
                    kb.op("pe", lambda: nc.tensor.transpose(pt[:, (kc % 4) * 128:(kc % 4 + 1) * 128],
                                                            x1[:, kc * 128:(kc + 1) * 128], ident[:, :]),
                          R=[x1, ident], W=[pt])
                for hf in range(2):
                    src = X[hf][:, :].rearrange("p (k t) -> p k t", k=4)
                    dst = x1T[:, hf * 4:(hf + 1) * 4, tt * 128:(tt + 1) * 128]
                    if hf == 0:
                        kb.op("dve", lambda: nc.vector.tensor_copy(out=dst, in_=src), R=[X[hf]], W=[x1T])
                    else:
                        kb.op("act", lambda: nc.scalar.copy(out=dst, in_=src), R=[X[hf]], W=[x1T])
                    if moe:
                        kb.op("dve", lambda: nc.vector.tensor_copy(out=x1T32[:, hf * 4:(hf + 1) * 4, :], in_=src),
                              R=[X[hf]], W=[x1T32])
                if moe:
                    pr = X[2]
                    for kc in range(8):
                        kb.op("pe", lambda: nc.tensor.matmul(pr[:, :8], lhsT=x1T32[:, kc, :], rhs=wr[:, kc, :],
                                                             start=(kc == 0), stop=(kc == 7)), R=[x1T32, wr], W=[pr])
                    cb = comb[tt]
                    lg, mx, tmp, oh = rt[:, 0:8], rt[:, 8:16], rt[:, 16:24], rt[:, 24:32]
                    sc = rt[:, 32:40]
                    kb.op("dve", lambda: nc.vector.tensor_copy(out=lg, in_=pr[:, :8]), R=[pr], W=[rt])
                    kb.op("dve", lambda: nc.vector.max(out=mx, in_=lg), R=[rt], W=[rt])
                    kb.op("dve", lambda: nc.vector.tensor_tensor(out=sc[:, 0:1], in0=mx[:, 1:2], in1=mx[:, 0:1],
                                                                 op=ALU.subtract), R=[rt], W=[rt])
                    kb.op("act", lambda: nc.scalar.activation(out=sc[:, 1:2], in_=sc[:, 0:1], func=AF.Exp),
                          R=[rt], W=[rt])
                    kb.op("dve", lambda: nc.vector.tensor_scalar(out=sc[:, 2:3], in0=sc[:, 1:2], scalar1=1.0,
                                                                 scalar2=None, op0=ALU.add), R=[rt], W=[rt])
                    kb.op("dve", lambda: nc.vector.reciprocal(out=sc[:, 3:4], in_=sc[:, 2:3]), R=[rt], W=[rt])
                    kb.op("dve", lambda: nc.vector.tensor_tensor(out=sc[:, 4:5], in0=sc[:, 1:2], in1=sc[:, 3:4],
                                                                 op=ALU.mult), R=[rt], W=[rt])
                    kb.op("dve", lambda: nc.vector.tensor_scalar(out=tmp, in0=lg, scalar1=mx[:, 0:1],
                                                                 scalar2=sc[:, 3:4], op0=ALU.is_equal, op1=ALU.mult),
                          R=[rt], W=[rt])
                    kb.op("dve", lambda: nc.vector.tensor_scalar(out=oh, in0=lg, scalar1=mx[:, 1:2],
                                                                 scalar2=sc[:, 4:5], op0=ALU.is_equal, op1=ALU.mult),
                          R=[rt], W=[rt])
                    kb.op("dve", lambda: nc.vector.tensor_tensor(out=cb[:, :], in0=tmp, in1=oh, op=ALU.add),
                          R=[rt], W=[cb])
            first = True
            for e in range(n_exp):
                for u in range(n_units):
                    base = (e * n_units + u) * nfu
                    LAGW = 3
                    wcs = {}
                    for f in range(nfu):
                        if f == 0:
                            for f2 in range(min(LAGW, nfu)):
                                wcs[f2] = w13[(wcnt + f2) % 4]
                                kb.dma("pool", wcs[f2][:, :], wf_d[base + f2, :, 0:2048], R=[wf_d], W=[wcs[f2]])
                        if f + LAGW < nfu:
                            wcs[f + LAGW] = w13[(wcnt + LAGW) % 4]
                            kb.dma("pool", wcs[f + LAGW][:, :], wf_d[base + f + LAGW, :, 0:2048], R=[wf_d],
                                   W=[wcs[f + LAGW]])
                        kb.dma("pool", w2[f][:, :], wf_d[base + f, :, 2048:3072], R=[wf_d], W=[w2[f]])
                        wc = wcs[f]
                        h1, h3 = X[(wcnt % 2) * 2], X[(wcnt % 2) * 2 + 1]
                        for kc in range(8):
                            kb.op("pe", lambda: nc.tensor.matmul(h1[:, :], lhsT=wc[:, kc * 128:(kc + 1) * 128],
                                                                 rhs=x1T[:, kc, :], start=(kc == 0), stop=(kc == 7)),
                                  R=[wc, x1T], W=[h1])
                        for kc in range(8):
                            kb.op("pe", lambda: nc.tensor.matmul(h3[:, :],
                                                                 lhsT=wc[:, 1024 + kc * 128:1024 + (kc + 1) * 128],
                                                                 rhs=x1T[:, kc, :], start=(kc == 0), stop=(kc == 7)),
                                  R=[wc, x1T], W=[h3])
                        s = sil[wcnt % 2]
                        kb.op("act", lambda: nc.scalar.activation(out=s[:, :], in_=h1[:, :], func=AF.Silu),
                              R=[h1], W=[s])
                        kb.op("dve", lambda: nc.vector.tensor_tensor(out=aT[f][:, :], in0=s[:, :], in1=h3[:, :],
                                                                     op=ALU.mult), R=[s, h3], W=[aT[f]])
                        wcnt += 1
                    for tt in range(4):
                        for hf in range(2):
                            py = Y[hf]
                            for f in range(nfu):
                                kb.op("pe", lambda: nc.tensor.matmul(py[:, :], lhsT=aT[f][:, tt * 128:(tt + 1) * 128],
                                                                     rhs=w2[f][:, hf * 512:(hf + 1) * 512],
                                                                     start=(f == 0), stop=(f == nfu - 1)),
                                      R=[aT[f], w2[f]], W=[py])
                            ya = yacc[tt]
                            ysl = ya[:, hf * 512:(hf + 1) * 512]
                            if moe:
                                cs = comb[tt][:, e:e + 1]
                                if first:
                                    kb.op("dve", lambda: nc.vector.tensor_scalar(out=ysl, in0=py[:, :], scalar1=cs,
                                                                                 scalar2=None, op0=ALU.mult),
                                          R=[py, comb[tt]], W=[ya])
                                else:
                                    kb.op("dve", lambda: nc.vector.scalar_tensor_tensor(out=ysl, in0=py[:, :], scalar=cs,
                                                                                        in1=ysl, op0=ALU.mult,
                                                                                        op1=ALU.add),
                                          R=[py, comb[tt], ya], W=[ya])
                            else:
                                if first:
                                    kb.op("dve", lambda: nc.vector.tensor_copy(out=ysl, in_=py[:, :]), R=[py], W=[ya])
                                else:
                                    kb.op("dve", lambda: nc.vector.tensor_tensor(out=ysl, in0=py[:, :], in1=ysl,
                                                                                 op=ALU.add), R=[py, ya], W=[ya])
                    first = False
            for tt in range(4):
                tok0 = g * TG + tt * 128
                kb.op("dve", lambda: nc.vector.scalar_tensor_tensor(out=h[:, :], in0=x1g[tt][:, :], scalar=ALPHA,
                                                                    in1=yacc[tt][:, :], op0=ALU.mult, op1=ALU.add),
                      R=[x1g[tt], yacc[tt]], W=[h])
                o = ost[tt % 2]
                layer_norm(kb, h, (lnp, lnp[:, 2, :]), (lnp, lnp[:, 3, :]), o[:, :], o, scr)
                kb.dma("sp", x_dst[tok0:tok0 + 128, :], o[:, :], R=[o], Wd=[x_dst])
                if not last:
                    emit_xT(kb, X, ident, o, g * 4 + tt, stg, xT_loc, xTs_loc)


def _dbg_out(kb, x_d, out_d):
    for tt in range(4):
        kb.dma("sp", out_d[tt * 512:(tt + 1) * 512, :], x_d[tt * 512:(tt + 1) * 512, :], R=[x_d], Wd=[out_d])
    return kb.finish()


PROFILE_SCOPES = False


class _NullScope:
    def __enter__(self):
        return self

    def __exit__(self, *a):
        return False


def _scope(nc, name):
    return nc.named_scope(name) if PROFILE_SCOPES else _NullScope()


def build_fused(layers=(0, 1, 2, 3), upto=9):
    kb = KBF()
    nc = kb.nc
    x_d = kb.dram_in("x", [TPC, D], F32)
    out_d = kb.dram_out("out", [TPC, D], F32)
    cst = {"tri": kb.dram_in("tri", [128, 128], F32), "identb": kb.dram_in("identb", [128, 128], BF16),
           "ident": kb.dram_in("ident", [128, 128], F32), "dilc": kb.dram_in("dilc", [32, NDEL], F32),
           "dsac": kb.dram_in("dsac", [32, NDEL], F32), "cmask": kb.dram_in("cmask", [128, 512], F32),
           "steps": kb.dram_in("steps", [128, NBIS], F32), "idx_s": kb.dram_in("idx_s", [128, 32], I32),
           "idx_o": kb.dram_in("idx_o", [128, 32], I32)}
    Ls = {}
    for l in layers:
        L = {}
        pre = "L%d_" % l
        moe = (l % 2 == 1)
        if l % 2 == 0:
            L["wgl"] = kb.dram_in(pre + "wgl", [128, 8, 464], F32)
            L["wg"] = kb.dram_in(pre + "wg", [16, 64], F32)
            L["bg"] = kb.dram_in(pre + "bg", [128, 64], F32)
            L["gn"] = kb.dram_in(pre + "gn", [128, 128], F32)
            L["wd1"] = kb.dram_in(pre + "wd1", [128, 8, 584], F32)
            L["wd2"] = kb.dram_in(pre + "wd2", [128, 8, 384], F32)
            L["relfar"] = kb.dram_in(pre + "relfar", [128, 2], F32)
        else:
            L["wcd"] = kb.dram_in(pre + "wcd", [128, 8, 770], F32)
            L["bf"] = kb.dram_in(pre + "bf", [128, 2], F32)
            L["wr"] = kb.dram_in(pre + "wr", [128, 8, 8], F32)
        L["rel2"] = kb.dram_in(pre + "rel2", [32, 2], F32)
        L["wo"] = kb.dram_in(pre + "wo", [128, 8, D], F32)
        L["lnp"] = kb.dram_in(pre + "lnp", [128, 4, D], F32)
        L["wf"] = kb.dram_in(pre + "wf", [(224 if moe else 22) if upto >= 9 else 1, 128, 3072], F32)
        L["Escr"] = kb.dram_tmp(pre + "Escr", [2, NDEL], F32)
        Ls[l] = L
    xres = [kb.dram_tmp("xres0", [TPC, D], F32), kb.dram_tmp("xres1", [TPC, D], F32)]
    xT_loc = kb.dram_tmp("xT_loc", [D, TPC], BF16)
    xT_all = kb.dram_tmp("xT_all", [4 * D, TPC], BF16)
    xTs_loc = kb.dram_tmp("xTs_loc", [4 * D, 512], BF16)
    xTs_all = kb.dram_tmp("xTs_all", [16 * D, 512], BF16)
    oT_loc = kb.dram_tmp("oT_loc", [16 * 256, 512], BF16)
    oT_all = kb.dram_tmp("oT_all", [64 * 256, 512], BF16)
    M_loc = kb.dram_tmp("M_loc", [MTOT // 512, 512], BF16)
    M_all = kb.dram_tmp("M_all", [4 * MTOT // 512, 512], BF16)
    assert MPARTS[-1][4] + 4 * MPARTS[-1][3] == 4 * MTOT // 512
    with kb.scope():
        ident = kb.sb([128, 128], F32, "ident")
        kb.dma("sp", ident[:, :], cst["ident"][:, :], R=[cst["ident"]], W=[ident])
        xin = [kb.sb([128, D], F32, "xin") for _ in range(2)]
        stg = kb.sb([128, 8, 512], BF16, "stgx")
        X = [kb.ps([128, 512], F32, "X") for _ in range(2)]
        for tt in range(TPC // 128):
            xi = xin[tt % 2]
            kb.dma("sp", xi[:, :], x_d[tt * 128:(tt + 1) * 128, :], R=[x_d], W=[xi])
            emit_xT(kb, X, ident, xi, tt, stg, xT_loc, xTs_loc)
    if upto == 0:
        return _dbg_out(kb, x_d, out_d)
    x_src = x_d
    for n, l in enumerate(layers):
        L = Ls[l]
        last = (n == len(layers) - 1)
        x_dst = out_d if last else xres[n % 2]
        for cf in range(4):
            kb.collective("AllGather", xT_loc, xT_all, xT_loc[cf * 256:(cf + 1) * 256, :],
                          xT_all[cf * 1024:(cf + 1) * 1024, :])
        if upto == 1:
            return _dbg_out(kb, x_d, out_d)
        if l % 2 == 0:
            for j in range(4):
                kb.collective("AllGather", xTs_loc, xTs_all, xTs_loc[j * 1024:(j + 1) * 1024, :],
                              xTs_all[j * 4096:(j + 1) * 4096, :])
            with _scope(nc, "L%d_gla" % l):
                phase_gla(kb, L, xT_all, oT_loc, cst)
            with _scope(nc, "L%d_dsa1" % l):
                phase_dsa1(kb, L, xT_all, xTs_all, M_loc, M_all, cst)
            with _scope(nc, "L%d_dsa2" % l):
                phase_dsa2(kb, L, xT_all, M_all, oT_loc, oT_all, cst)
        else:
            with _scope(nc, "L%d_cd" % l):
                phase_cd(kb, L, xT_all, oT_loc, oT_all, cst)
        if upto == 2:
            return _dbg_out(kb, x_d, out_d)
        if upto == 3:
            return _dbg_out(kb, x_d, out_d)
        with _scope(nc, "L%d_c" % l):
            phase_c(kb, L, l % 2 == 1, oT_all, x_src, x_dst, xT_loc, xTs_loc, cst, last)
        x_src = x_dst
    return kb.finish()


def fused_inputs(x, ln_g, ln_b, rel_table, w_in_ab, w_gate_a, b_gate_a, g_norm_a, w_out_ab,
                 w_in_cd, b_forget, w_out_cd, w1_dense, w3_dense, w2_dense,
                 w_router, w1_moe, w3_moe, w2_moe, layers=(0, 1, 2, 3)):
    f32 = lambda a: np.ascontiguousarray(np.asarray(a, dtype=np.float32))
    bc = lambda v, n=128: np.ascontiguousarray(np.broadcast_to(np.asarray(v, np.float32)[None, :], (n, len(v))))
    xf = f32(x).reshape(BATCH * SEQ, D)
    rel_table = f32(rel_table)
    steps = bc(0.5 ** np.arange(1, NBIS + 1))
    perm = np.zeros(D, np.int64)
    for src in range(4):
        for rr in range(256):
            perm[src * 256 + rr] = src * 128 + rr if rr < 128 else 512 + src * 128 + (rr - 128)
    shared = {"tri": TRI, "identb": IDENT.astype(NPBF), "ident": IDENT, "dilc": dil_const(), "dsac": dsa_const(),
              "steps": steps}
    per_layer_shared = {}
    for l in layers:
        j = l // 2
        pre = "L%d_" % l
        d = {}
        d[pre + "lnp"] = np.ascontiguousarray(np.broadcast_to(
            np.stack([ln_g[l, 0], ln_b[l, 0], ln_g[l, 1], ln_b[l, 1]]).astype(np.float32)[None], (128, 4, D)))
        if l % 2 == 0:
            d[pre + "wo"] = w_kc_layout(f32(w_out_ab[j])[perm])
            d[pre + "wf"] = ffn_chunk_layout(f32(w1_dense[j]), f32(w3_dense[j]), f32(w2_dense[j]))
            d[pre + "gn"] = bc(g_norm_a[j])
        else:
            d[pre + "wo"] = w_kc_layout(f32(w_out_cd[j])[perm])
            d[pre + "wf"] = np.concatenate([ffn_chunk_layout(f32(w1_moe[j, e]), f32(w3_moe[j, e]), f32(w2_moe[j, e]))
                                            for e in range(8)], axis=0)
            d[pre + "wr"] = np.ascontiguousarray(f32(w_router[j]).reshape(8, 128, 8).transpose(1, 0, 2))
        per_layer_shared.update(d)
    maps = []
    for c in range(NCORES):
        b, m = c // 4, c % 4
        mp = dict(shared)
        mp.update(per_layer_shared)
        mp["x"] = np.ascontiguousarray(xf[c * TPC:(c + 1) * TPC])
        pp_ = np.arange(128)[:, None]
        rr_, k8_ = np.arange(4)[None, :, None], np.arange(8)[None, None, :]
        mp["idx_s"] = np.ascontiguousarray(((m * 4 + rr_) * 1024 + k8_ * 128 + pp_[:, :, None]).reshape(128, 32)
                                           .astype(np.int32))
        G_ = k8_ * 128 + pp_[:, :, None]
        mp["idx_o"] = np.ascontiguousarray(((m * 4 + G_ // 256) * 1024 + rr_ * 256 + G_ % 256).reshape(128, 32)
                                           .astype(np.int32))
        mp["cmask"] = np.where(np.arange(512)[None, :] <= (128 * m + np.arange(128))[:, None], 0.0, NEG).astype(np.float32)
        for l in layers:
            j = l // 2
            pre = "L%d_" % l
            mp[pre + "rel2"] = np.ascontiguousarray(rel_table[:, 2 * m:2 * m + 2])
            if l % 2 == 0:
                w = f32(w_in_ab[j])
                cs = lambda a, n: w[:, a:a + n]
                wgl = np.concatenate([cs(m * 64, 64), cs(256 + m * 64, 64), cs(1536, 16), cs(256 + m * 64, 64),
                                      cs(512 + m * 128, 128), cs(1024 + m * 128, 128)], axis=1)
                wd1 = np.concatenate([cs(3088, 512), cs(3600, 64), cs(3664, 8)], axis=1)
                wd2 = np.concatenate([cs(1552 + m * 128, 128), cs(2064 + m * 128, 128), cs(2576 + m * 128, 128)], axis=1)
                mp[pre + "wgl"] = w_kc_layout(wgl)
                mp[pre + "wd1"] = w_kc_layout(wd1)
                mp[pre + "wd2"] = w_kc_layout(wd2)
                mp[pre + "wg"] = np.ascontiguousarray(f32(w_gate_a[j])[:, m * 64:(m + 1) * 64])
                mp[pre + "bg"] = bc(f32(b_gate_a[j])[m * 64:(m + 1) * 64])
                mp[pre + "relfar"] = bc(rel_table[31, 2 * m:2 * m + 2])
            else:
                w = f32(w_in_cd[j])
                cs = lambda a, n: w[:, a:a + n]
                wcd = np.concatenate([cs(m * 128, 128), cs(512 + m * 128, 128), cs(1544 + m * 128, 128),
                                      cs(2056 + m * 128, 128), cs(1024 + m * 128, 128), cs(2568 + m * 128, 128),
                                      cs(1536 + 2 * m, 2)], axis=1)
                mp[pre + "wcd"] = w_kc_layout(wcd)
                mp[pre + "bf"] = bc(f32(b_forget[j])[2 * m:2 * m + 2])
        maps.append(mp)
    return maps


def kernel_fused(**inputs):
    nc = build_fused()
    maps = fused_inputs(**inputs)
    res = run_spmd(nc, maps)
    out = np.concatenate([res[c]["out"] for c in range(NCORES)], axis=0)
    return out.reshape(BATCH, SEQ, D).astype(np.float32)


def kernel(**inputs):
    return kernel_fused(**inputs)
```
